# Optimizing a Trainium2 kernel written in Bass

```python
import jax, jax.numpy as jnp
from jax import lax
import numpy as np

D_MODEL = 2048
BATCH = 2
SEQ = 4096
DEPTH = 1

N_META = 16
N_ATT_HEADS = 8
N_KV_HEADS = 2
ATT_GROUP = N_ATT_HEADS // N_KV_HEADS
ATT_HEAD_DIM = 128
N_IDX_HEADS = 16
IDX_HEAD_DIM = 64
TOPK_MAX = 256
Q_BLOCK = 128
HG_HEADS = 8
HG_KEY_DIM = 128
HG_VAL_DIM = 128
HG_CHUNK = 64
D_FF = 5632
LN_EPS = 1e-5
RMS_EPS = 1e-6

ATT_Q_W = N_ATT_HEADS * ATT_HEAD_DIM
ATT_KV_W = N_KV_HEADS * ATT_HEAD_DIM
IDX_Q_W = N_IDX_HEADS * IDX_HEAD_DIM
HG_K_W = HG_HEADS * HG_KEY_DIM
HG_V_W = HG_HEADS * HG_VAL_DIM
IN_SPLITS = (ATT_Q_W, ATT_KV_W, ATT_KV_W, IDX_Q_W, IDX_HEAD_DIM, N_IDX_HEADS, HG_K_W, HG_K_W, HG_V_W, HG_V_W, D_MODEL, D_MODEL)
D_IN_PROJ = sum(IN_SPLITS)

kernel_name = 'hybrid_dsa_hgrn2_macaron_block'


def layer_norm(x, g, b):
    xf = x.astype(jnp.float32)
    mu = jnp.mean(xf, -1, keepdims=True)
    var = jnp.mean(jnp.square(xf - mu), -1, keepdims=True)
    return ((xf - mu) * lax.rsqrt(var + LN_EPS) * g + b).astype(x.dtype)


def swiglu(x, w_gate, w_up, w_down):
    return (jax.nn.silu(x @ w_gate) * (x @ w_up)) @ w_down


def alibi_slopes(n):
    return jnp.exp2(-8.0 * (jnp.arange(n, dtype=jnp.float32) + 1.0) / n)


def dsa_attention(q, k, v, iq, ik, iw):
    B, T = q.shape[0], q.shape[1]
    L = T - N_META
    k_sel = min(TOPK_MAX, L // 4)
    slopes = alibi_slopes(N_ATT_HEADS).reshape(N_KV_HEADS, ATT_GROUP)
    ik_real = ik[:, N_META:].astype(jnp.float32)
    key_pos = N_META + jnp.arange(L)
    meta_pos = jnp.arange(N_META)

    def block(t0, qlen):
        t_pos = t0 + jnp.arange(qlen)
        qb = lax.dynamic_slice_in_dim(q, t0, qlen, axis=1)
        iqb = lax.dynamic_slice_in_dim(iq, t0, qlen, axis=1).astype(jnp.float32)
        iwb = lax.dynamic_slice_in_dim(iw, t0, qlen, axis=1).astype(jnp.float32)
        s = jnp.einsum('bqhd,bsd->bqhs', iqb, ik_real) * (IDX_HEAD_DIM ** -0.5)
        score = jnp.einsum('bqhs,bqh->bqs', jax.nn.relu(s), iwb)
        score = jnp.where(key_pos[None, None, :] <= t_pos[None, :, None], score, -jnp.inf)
        vals, sel = lax.top_k(score, k_sel)
        pos = jnp.concatenate([jnp.broadcast_to(meta_pos, (B, qlen, N_META)), sel + N_META], -1)
        valid = jnp.concatenate([jnp.broadcast_to(meta_pos[None, None, :] <= t_pos[None, :, None], (B, qlen, N_META)), jnp.isfinite(vals)], -1)
        kg = jax.vmap(lambda kb, pb: kb[pb])(k, pos)
        vg = jax.vmap(lambda vb, pb: vb[pb])(v, pos)
        logits = jnp.einsum('bqkgd,bqnkd->bqkgn', qb, kg).astype(jnp.float32) * (ATT_HEAD_DIM ** -0.5)
        dist = jnp.abs(t_pos[None, :, None] - pos).astype(jnp.float32)
        logits = logits - slopes[None, None, :, :, None] * dist[:, :, None, None, :]
        logits = jnp.where(valid[:, :, None, None, :], logits, -jnp.inf)
        p = jax.nn.softmax(logits, axis=-1).astype(v.dtype)
        return jnp.einsum('bqkgn,bqnkd->bqkgd', p, vg)

    meta_out = block(0, N_META)
    n_blocks = L // Q_BLOCK
    real = lax.map(lambda i: block(N_META + i * Q_BLOCK, Q_BLOCK), jnp.arange(n_blocks))
    real = jnp.moveaxis(real, 0, 1).reshape(B, L, N_KV_HEADS, ATT_GROUP, ATT_HEAD_DIM)
    return jnp.concatenate([meta_out, real], axis=1).reshape(B, T, ATT_Q_W)


def _hgrn2_chunk(state, chunk):
    q, k, v, logf = chunk
    b = jnp.cumsum(logf, axis=2)
    causal = jnp.tril(jnp.ones((HG_CHUNK, HG_CHUNK), dtype=bool))
    diff = b[:, :, :, None, :] - b[:, :, None, :, :]
    decay = jnp.exp(jnp.where(causal[None, None, :, :, None], diff, -jnp.inf))
    scores = jnp.einsum('bhtd,bhsd,bhtsd->bhts', q, k, decay)
    o = jnp.einsum('bhts,bhsv->bhtv', scores, v) + jnp.einsum('bhtd,bhdv->bhtv', q * jnp.exp(b), state)
    b_last = b[:, :, -1:, :]
    state = jnp.exp(b_last[:, :, 0, :])[..., None] * state + jnp.einsum('bhsd,bhsv->bhdv', k * jnp.exp(b_last - b), v)
    return state, o


def hgrn2(q, f, i, gate, lb, norm_g):
    B, T = q.shape[0], q.shape[1]
    f32 = jnp.float32
    fg = lb + (1.0 - lb) * jax.nn.sigmoid(f.astype(f32))
    qh = jax.nn.silu(q.astype(f32)).reshape(B, T, HG_HEADS, HG_KEY_DIM)
    kh = (1.0 - fg).reshape(B, T, HG_HEADS, HG_KEY_DIM)
    logf = jnp.log(fg).reshape(B, T, HG_HEADS, HG_KEY_DIM)
    vh = i.astype(f32).reshape(B, T, HG_HEADS, HG_VAL_DIM)
    pad = HG_CHUNK - N_META

    def to_chunks(a):
        a = jnp.pad(a, ((0, 0), (pad, 0), (0, 0), (0, 0)))
        n = a.shape[1] // HG_CHUNK
        return a.reshape(B, n, HG_CHUNK, HG_HEADS, a.shape[-1]).transpose(1, 0, 3, 2, 4)

    state0 = jnp.zeros((B, HG_HEADS, HG_KEY_DIM, HG_VAL_DIM), f32)
    _, o = lax.scan(_hgrn2_chunk, state0, (to_chunks(qh), to_chunks(kh), to_chunks(vh), to_chunks(logf)))
    o = o.transpose(1, 0, 3, 2, 4).reshape(B, -1, HG_HEADS, HG_VAL_DIM)[:, pad:]
    o = o * lax.rsqrt(jnp.mean(jnp.square(o), -1, keepdims=True) + RMS_EPS) * norm_g
    o = o * jax.nn.silu(gate.astype(f32)).reshape(B, T, HG_HEADS, HG_VAL_DIM)
    return o.reshape(B, T, HG_V_W).astype(gate.dtype)


def hybrid_mixer(h, w_in, idx_k_norm_g, idx_k_norm_b, lb, hg_norm_g, w_branch_att, w_branch_hg, w_out):
    B, T, _ = h.shape
    offsets = np.cumsum(IN_SPLITS)[:-1].tolist()
    aq, ak, av, iq, ik, iw, hq, hf, hi, hgate, g_att, g_hg = jnp.split(h @ w_in, offsets, axis=-1)
    aq = aq.reshape(B, T, N_KV_HEADS, ATT_GROUP, ATT_HEAD_DIM)
    ak = ak.reshape(B, T, N_KV_HEADS, ATT_HEAD_DIM)
    av = av.reshape(B, T, N_KV_HEADS, ATT_HEAD_DIM)
    iq = iq.reshape(B, T, N_IDX_HEADS, IDX_HEAD_DIM)
    ik = layer_norm(ik, idx_k_norm_g, idx_k_norm_b)
    iw = iw * (N_IDX_HEADS ** -0.5)
    y_att = dsa_attention(aq, ak, av, iq, ik, iw)
    y_hg = hgrn2(hq, hf, hi, hgate, lb, hg_norm_g)
    merged = jax.nn.sigmoid(g_att) * (y_att @ w_branch_att) + jax.nn.sigmoid(g_hg) * (y_hg @ w_branch_hg)
    return merged @ w_out


def setup_inputs(seed: int = 0) -> dict:
    key = jax.random.key(seed)
    ks = jax.random.split(key, 24)
    f32 = jnp.float32
    beta = (8.0 * DEPTH) ** -0.25

    def dense(k, shape, fan_in, scale=1.0):
        return jax.random.normal(k, shape, f32) * (scale * fan_in ** -0.5)

    def gain(k, shape):
        return 1.0 + 0.02 * jax.random.normal(k, shape, f32)

    def bias(k, shape):
        return 0.02 * jax.random.normal(k, shape, f32)

    return {
        'x': jax.random.normal(ks[0], (BATCH, SEQ, D_MODEL), f32),
        'meta': jax.random.normal(ks[1], (N_META, D_MODEL), f32),
        'ffn1_w_gate': dense(ks[2], (DEPTH, D_MODEL, D_FF), D_MODEL),
        'ffn1_w_up': dense(ks[3], (DEPTH, D_MODEL, D_FF), D_MODEL),
        'ffn1_w_down': dense(ks[4], (DEPTH, D_FF, D_MODEL), D_FF, beta),
        'ln1_g': gain(ks[5], (DEPTH, D_MODEL)),
        'ln1_b': bias(ks[6], (DEPTH, D_MODEL)),
        'w_in': dense(ks[7], (DEPTH, D_MODEL, D_IN_PROJ), D_MODEL),
        'idx_k_norm_g': gain(ks[8], (DEPTH, IDX_HEAD_DIM)),
        'idx_k_norm_b': bias(ks[9], (DEPTH, IDX_HEAD_DIM)),
        'hg_lb_logits': 0.1 * jax.random.normal(ks[10], (DEPTH + 1, HG_K_W), f32),
        'hg_norm_g': gain(ks[11], (DEPTH, HG_HEADS, HG_VAL_DIM)),
        'w_branch_att': dense(ks[12], (DEPTH, ATT_Q_W, D_MODEL), ATT_Q_W),
        'w_branch_hg': dense(ks[13], (DEPTH, HG_V_W, D_MODEL), HG_V_W),
        'w_out': dense(ks[14], (DEPTH, D_MODEL, D_MODEL), D_MODEL, beta),
        'ln2_g': gain(ks[15], (DEPTH, D_MODEL)),
        'ln2_b': bias(ks[16], (DEPTH, D_MODEL)),
        'ffn2_w_gate': dense(ks[17], (DEPTH, D_MODEL, D_FF), D_MODEL),
        'ffn2_w_up': dense(ks[18], (DEPTH, D_MODEL, D_FF), D_MODEL),
        'ffn2_w_down': dense(ks[19], (DEPTH, D_FF, D_MODEL), D_FF, beta),
        'ln3_g': gain(ks[20], (DEPTH, D_MODEL)),
        'ln3_b': bias(ks[21], (DEPTH, D_MODEL)),
    }


def reference(x, meta, ffn1_w_gate, ffn1_w_up, ffn1_w_down, ln1_g, ln1_b, w_in, idx_k_norm_g, idx_k_norm_b, hg_lb_logits, hg_norm_g, w_branch_att, w_branch_hg, w_out, ln2_g, ln2_b, ffn2_w_gate, ffn2_w_up, ffn2_w_down, ln3_g, ln3_b):
    B = x.shape[0]
    alpha = (2.0 * DEPTH) ** 0.25
    lb_all = jnp.cumsum(jax.nn.softmax(hg_lb_logits.astype(jnp.float32), axis=0), axis=0)
    h = jnp.concatenate([jnp.broadcast_to(meta[None].astype(x.dtype), (B, N_META, D_MODEL)), x], axis=1)
    for l in range(DEPTH):
        h = layer_norm(alpha * h + 0.5 * swiglu(h, ffn1_w_gate[l], ffn1_w_up[l], ffn1_w_down[l]), ln1_g[l], ln1_b[l])
        mix = hybrid_mixer(h, w_in[l], idx_k_norm_g[l], idx_k_norm_b[l], lb_all[l], hg_norm_g[l], w_branch_att[l], w_branch_hg[l], w_out[l])
        h = layer_norm(alpha * h + mix, ln2_g[l], ln2_b[l])
        h = layer_norm(alpha * h + 0.5 * swiglu(h, ffn2_w_gate[l], ffn2_w_up[l], ffn2_w_down[l]), ln3_g[l], ln3_b[l])
    return h[:, N_META:]
```

```python
import os
import numpy as np
import concourse.bass as bass
import concourse.mybir as mybir
from concourse.bass_utils import run_bass_kernel_spmd

F32 = mybir.dt.float32
BF16 = mybir.dt.bfloat16
AF = mybir.ActivationFunctionType
ALU = mybir.AluOpType
AX = mybir.AxisListType

D = 2048
DFF = 5632
NMETA = 16
NREAL = 1024
NT = NREAL + NMETA
KC = D // 128
FC = DFF // 128
TBS = [(0, 512), (512, 1024), (1024, 1040)]
ALPHA = 2.0 ** 0.25
LN_EPS = 1e-5
RMS_EPS = 1e-6

ENGS = ("pe", "act", "dve", "pool", "sp")


class Op:
    __slots__ = ("eng", "fn", "deps", "is_dma", "dsem", "dval", "sig", "sigidx", "waits")

    def __init__(self, eng, fn, is_dma=False):
        self.eng = eng
        self.fn = fn
        self.deps = []
        self.is_dma = is_dma
        self.dsem = None
        self.dval = 0
        self.sig = False
        self.sigidx = 0
        self.waits = []


class Sched:
    def __init__(self, n_dma_sems=40, same_engine_sync=True):
        self.ops = {e: [] for e in ENGS}
        self.lastw = {}
        self.readers = {}
        self.n_dma = 0
        self.n_dma_sems = n_dma_sems
        self.dma_hist = {}
        self.same_engine_sync = same_engine_sync
        self.n_coll = 0

    def _add(self, op, reads, writes):
        deps = set()
        for k in reads:
            w = self.lastw.get(k)
            if w is not None:
                deps.add(w)
        for k in writes:
            w = self.lastw.get(k)
            if w is not None:
                deps.add(w)
            for r in self.readers.get(k, ()):
                deps.add(r)
        op.deps = list(deps)
        for k in reads:
            self.readers.setdefault(k, []).append(op)
        for k in writes:
            self.lastw[k] = op
            self.readers[k] = []
        self.ops[op.eng].append(op)
        return op

    def op(self, eng, fn, reads=(), writes=()):
        return self._add(Op(eng, fn), reads, writes)

    def dma(self, q, fn, reads=(), writes=()):
        op = Op(q, fn, is_dma=True)
        slot = self.n_dma % self.n_dma_sems
        op.dsem = slot
        op.dval = 16 * (self.n_dma // self.n_dma_sems + 1)
        self.n_dma += 1
        self._add(op, reads, writes)
        prev = self.dma_hist.get(slot)
        if prev is not None:
            op.deps.append(prev)
        self.dma_hist[slot] = op
        return op

    def coll(self, fn, reads=(), writes=()):
        op = Op("pool", fn, is_dma=True)
        op.dsem = self.n_dma_sems + self.n_coll
        op.dval = 1
        self.n_coll += 1
        self._add(op, reads, writes)
        self.dma_hist[op.dsem] = op
        return op

    def barrier(self):
        lasts = []
        for e in ENGS:
            for o in reversed(self.ops[e]):
                if not o.is_dma and o.fn is not None:
                    lasts.append(o)
                    break
        dmas = list(self.dma_hist.values())
        for e in ENGS:
            op = Op(e, None)
            op.deps = [o for o in lasts if o.eng != e] + dmas
            self.ops[e].append(op)
        self.lastw = {}
        self.readers = {}

    def _skip(self, d, op):
        return d.eng == op.eng and (d.eng in ("pe", "sp") or not self.same_engine_sync)

    def finalize(self):
        for e in ENGS:
            for op in self.ops[e]:
                for d in op.deps:
                    if not d.is_dma and not self._skip(d, op):
                        d.sig = True
        for e in ENGS:
            c = 0
            for op in self.ops[e]:
                if op.sig:
                    c += 1
                    op.sigidx = c
        for e in ENGS:
            waited = {}
            for op in self.ops[e]:
                need = {}
                for d in op.deps:
                    if d.is_dma:
                        key, val = ("d", d.dsem), d.dval
                    else:
                        if self._skip(d, op):
                            continue
                        key, val = ("e", d.eng), d.sigidx
                    if waited.get(key, 0) >= val:
                        continue
                    if need.get(key, 0) < val:
                        need[key] = val
                for k, v in need.items():
                    waited[k] = v
                op.waits = list(need.items())

    def emit(self, block, esems, dsems):
        self.finalize()
        regs = {"pe": block.tensor, "act": block.scalar, "dve": block.vector,
                "pool": block.gpsimd, "sp": block.sync}
        final = {d.dsem: d.dval for d in self.dma_hist.values()}

        def make(e):
            ops = self.ops[e]

            def body(eng):
                for op in ops:
                    for (kind, which), val in op.waits:
                        eng.wait_ge(dsems[which] if kind == "d" else esems[which], val)
                    if op.fn is None:
                        continue
                    ins = op.fn(eng)
                    if op.is_dma:
                        ins.then_inc(dsems[op.dsem], 16 if op.dsem < self.n_dma_sems else 1)
                    elif op.sig:
                        ins.then_inc(esems[e], 1)
                if e == "sp":
                    for slot, val in final.items():
                        eng.wait_ge(dsems[slot], val)
            return body

        for e in ENGS:
            regs[e](make(e))


ARENA_F32 = 50816
CONST_F32 = 2304


class Builder:
    def __init__(self, stage):
        self.stage = stage
        self.nc = bass.Bass("TRN2", target_bir_lowering=False)
        self.S = Sched()
        self.dram = {}

    def din(self, name, shape, dt=F32):
        self.dram[name] = self.nc.dram_tensor(name, list(shape), dt, kind="ExternalInput").ap()
        return self.dram[name]

    def dout(self, name, shape, dt=F32):
        self.dram[name] = self.nc.dram_tensor(name, list(shape), dt, kind="ExternalOutput").ap()
        return self.dram[name]

    def dscratch(self, name, shape, dt=F32):
        self.dram[name] = self.nc.dram_tensor(name, list(shape), dt, kind="Internal").ap()
        return self.dram[name]

    def view(self, off_bytes, dt, shape):
        n = 1
        for s in shape:
            n *= s
        esz = 4 if dt == F32 else 2
        assert off_bytes % 4 == 0
        nbytes = n * esz
        assert nbytes % 4 == 0
        assert off_bytes + nbytes <= ARENA_F32 * 4, (off_bytes, nbytes)
        v = self.arena[:, off_bytes // 4:(off_bytes + nbytes) // 4]
        if dt != F32:
            v = v.bitcast(dt)
        if len(shape) == 2:
            v = v.rearrange("p (a b) -> p a b", b=shape[1])
        elif len(shape) == 3:
            v = v.rearrange("p (a b c) -> p a b c", b=shape[1], c=shape[2])
        return v

    def ln_apply(self, tag, z, tbs, cg, cb, toff, eps_eff, strm_out=None, alias_key=None, resid_out=None,
                 final_out=None):
        S, ps = self.S, self.ps
        zsq = [self.view(toff + i * 2048, F32, [512]) for i in range(2)]
        meanb = self.view(toff + 4096, F32, [512])
        rstdb = self.view(toff + 6144, F32, [512])
        t1 = [self.view(toff + 8192 + i * 2048, F32, [512]) for i in range(2)]
        t2 = [self.view(toff + 12288 + i * 2048, F32, [512]) for i in range(2)]
        o32 = [self.view(toff + 16384 + i * 2048, F32, [512]) for i in range(2)]
        ones = self.ones_f32
        for ti, (t0, t1e) in enumerate(tbs):
            n = t1e - t0
            pm, pq = ps[:, 6, 0:n], ps[:, 7, 0:n]
            for dc in range(KC):
                zq = zsq[dc % 2]
                S.op("act", lambda e, zq=zq, dc=dc, t0=t0, t1e=t1e, n=n: e.activation(
                    out=zq[:, 0:n], in_=z[:, dc, t0:t1e], func=AF.Square),
                    reads=[(tag, "z", dc, ti)], writes=[(tag, "zsq", dc % 2)])
                S.op("pe", lambda e, pm=pm, dc=dc, t0=t0, t1e=t1e: e.matmul(
                    pm, lhsT=ones, rhs=z[:, dc, t0:t1e], start=(dc == 0), stop=(dc == KC - 1)),
                    reads=[(tag, "z", dc, ti)], writes=[("ps", 6)])
                S.op("pe", lambda e, pq=pq, zq=zq, dc=dc, n=n: e.matmul(
                    pq, lhsT=ones, rhs=zq[:, 0:n], start=(dc == 0), stop=(dc == KC - 1)),
                    reads=[(tag, "zsq", dc % 2)], writes=[("ps", 7)])
            S.op("act", lambda e, pm=pm, n=n: e.activation(out=meanb[:, 0:n], in_=pm, func=AF.Copy),
                 reads=[("ps", 6)], writes=[(tag, "meanb")])
            S.op("dve", lambda e, n=n: e.tensor_tensor(out=rstdb[:, 0:n], in0=meanb[:, 0:n], in1=meanb[:, 0:n],
                                                       op=ALU.mult),
                 reads=[(tag, "meanb")], writes=[(tag, "rstdb")])
            S.op("dve", lambda e, pq=pq, n=n: e.tensor_tensor(out=rstdb[:, 0:n], in0=pq, in1=rstdb[:, 0:n],
                                                              op=ALU.subtract),
                 reads=[("ps", 7), (tag, "rstdb")], writes=[(tag, "rstdb")])
            S.op("act", lambda e, n=n: e.activation(out=rstdb[:, 0:n], in_=rstdb[:, 0:n], func=AF.Sqrt,
                                                    bias=self.eps_cols[eps_eff], scale=1.0),
                 reads=[(tag, "rstdb")], writes=[(tag, "rstdb")])
            S.op("dve", lambda e, n=n: e.reciprocal(out=rstdb[:, 0:n], in_=rstdb[:, 0:n]),
                 reads=[(tag, "rstdb")], writes=[(tag, "rstdb")])
            for dc in range(KC):
                a, b_, o = t1[dc % 2], t2[dc % 2], o32[dc % 2]
                S.op("pool", lambda e, a=a, dc=dc, t0=t0, t1e=t1e, n=n: e.tensor_tensor(
                    out=a[:, 0:n], in0=z[:, dc, t0:t1e], in1=meanb[:, 0:n], op=ALU.subtract),
                    reads=[(tag, "z", dc, ti), (tag, "meanb")], writes=[(tag, "t1", dc % 2)])
                S.op("dve", lambda e, a=a, b_=b_, n=n: e.tensor_tensor(
                    out=b_[:, 0:n], in0=a[:, 0:n], in1=rstdb[:, 0:n], op=ALU.mult),
                    reads=[(tag, "t1", dc % 2), (tag, "rstdb")], writes=[(tag, "t2", dc % 2)])
                S.op("act", lambda e, b_=b_, o=o, dc=dc, n=n: e.activation(
                    out=o[:, 0:n], in_=b_[:, 0:n], func=AF.Identity,
                    bias=cb[:, dc:dc + 1], scale=cg[:, dc:dc + 1]),
                    reads=[(tag, "t2", dc % 2)], writes=[(tag, "o32", dc % 2)])
                if strm_out is not None:
                    wk = [(tag, "sout", dc, ti)] + ([(tag, alias_key, dc, ti)] if alias_key else [])
                    S.op("act", lambda e, b_=b_, dc=dc, t0=t0, t1e=t1e, n=n: e.activation(
                        out=strm_out[:, dc, t0:t1e], in_=b_[:, 0:n], func=AF.Identity,
                        bias=cb[:, dc:dc + 1], scale=cg[:, dc:dc + 1]),
                        reads=[(tag, "t2", dc % 2)], writes=wk)
                dst = resid_out if final_out is None else final_out
                if final_out is None or t0 < NREAL:
                    S.dma("sp", lambda e, o=o, dc=dc, t0=t0, t1e=t1e, n=n, dst=dst: e.dma_start(
                        out=dst[dc * 128:(dc + 1) * 128, t0:t1e], in_=o[:, 0:n]),
                        reads=[(tag, "o32", dc % 2)])

    def ffn_phase(self, tag, ntok, tbs, strm_in, strm_out_off, wg, wu, wd, resid, cg, cb,
                  resid_out=None, final_out=None):
        S, nc = self.S, self.nc
        ps = self.ps
        C_OFF = 33280
        B_OFF = 66560
        D_OFF = B_OFF + FC * NT * 2
        T_OFF = D_OFF + 2 * FC * 128 * 2
        hT = self.view(B_OFF, BF16, [FC, ntok])
        wgu = [self.view(C_OFF + i * 16384, BF16, [2, KC, 256]) for i in range(2)]
        wdb = [self.view(D_OFF + i * FC * 128 * 2, BF16, [FC, 128]) for i in range(2)]
        z = self.view(0, F32, [KC, ntok])
        sil = [self.view(T_OFF + 8192 + i * 2048, F32, [512]) for i in range(2)]
        strm_out = self.view(strm_out_off, BF16, [KC, ntok]) if final_out is None else None
        c_scale = 0.5 / ALPHA
        eps_eff = LN_EPS / (ALPHA * ALPHA)
        nb = len(tbs)

        for g in range(FC // 2):
            wb = wgu[g % 2]
            kb = (tag, "wgu", g % 2)
            S.dma("pool", lambda e, wb=wb, g=g: e.dma_start(out=wb[:, 0], in_=wg[g]), writes=[(kb, 0)])
            S.dma("pool", lambda e, wb=wb, g=g: e.dma_start(out=wb[:, 1], in_=wu[g]), writes=[(kb, 1)])
            for fcl in range(2):
                fc = 2 * g + fcl
                for ti, (t0, t1e) in enumerate(tbs):
                    n = t1e - t0
                    pb = (fc * nb + ti) % 2
                    pg, pu = ps[:, 2 * pb, 0:n], ps[:, 2 * pb + 1, 0:n]
                    for k in range(KC):
                        S.op("pe", lambda e, pg=pg, wb=wb, k=k, fcl=fcl, t0=t0, t1e=t1e: e.matmul(
                            pg, lhsT=wb[:, 0, k, fcl * 128:(fcl + 1) * 128], rhs=strm_in[:, k, t0:t1e],
                            start=(k == 0), stop=(k == KC - 1)),
                            reads=[(kb, 0), (tag, "sin")], writes=[("ps", 2 * pb)])
                    for k in range(KC):
                        S.op("pe", lambda e, pu=pu, wb=wb, k=k, fcl=fcl, t0=t0, t1e=t1e: e.matmul(
                            pu, lhsT=wb[:, 1, k, fcl * 128:(fcl + 1) * 128], rhs=strm_in[:, k, t0:t1e],
                            start=(k == 0), stop=(k == KC - 1)),
                            reads=[(kb, 1), (tag, "sin")], writes=[("ps", 2 * pb + 1)])
                    sb = sil[pb]
                    S.op("act", lambda e, sb=sb, pg=pg, n=n: e.activation(out=sb[:, 0:n], in_=pg, func=AF.Silu),
                         reads=[("ps", 2 * pb)], writes=[(tag, "sil", pb)])
                    S.op("dve", lambda e, sb=sb, pu=pu, n=n, fc=fc, t0=t0, t1e=t1e: e.tensor_tensor(
                        out=hT[:, fc, t0:t1e], in0=sb[:, 0:n], in1=pu, op=ALU.mult),
                        reads=[(tag, "sil", pb), ("ps", 2 * pb + 1)], writes=[(tag, "hT", fc, ti)])

        S.barrier()
        for dc in range(KC):
            wb = wdb[dc % 2]
            kb = (tag, "wd", dc % 2)
            S.dma("pool", lambda e, wb=wb, dc=dc: e.dma_start(out=wb, in_=wd[dc]), writes=[kb])
            S.dma("sp", lambda e, dc=dc: e.dma_start(out=z[:, dc, :], in_=resid[dc * 128:(dc + 1) * 128, 0:ntok]),
                  writes=[(tag, "z", dc, ti) for ti in range(nb)])
            for ti, (t0, t1e) in enumerate(tbs):
                n = t1e - t0
                pb = 4 + (dc * nb + ti) % 2
                py = ps[:, pb, 0:n]
                for f in range(FC):
                    S.op("pe", lambda e, py=py, wb=wb, f=f, t0=t0, t1e=t1e: e.matmul(
                        py, lhsT=wb[:, f, :], rhs=hT[:, f, t0:t1e], start=(f == 0), stop=(f == FC - 1)),
                        reads=[kb, (tag, "hT", f, ti)], writes=[("ps", pb)])
                S.op("dve", lambda e, py=py, dc=dc, t0=t0, t1e=t1e: e.scalar_tensor_tensor(
                    out=z[:, dc, t0:t1e], in0=py, scalar=c_scale, in1=z[:, dc, t0:t1e],
                    op0=ALU.mult, op1=ALU.add),
                    reads=[("ps", pb), (tag, "z", dc, ti)], writes=[(tag, "z", dc, ti)])
        self.ln_apply(tag, z, tbs, cg, cb, T_OFF, eps_eff, strm_out=strm_out, alias_key="hT",
                      resid_out=resid_out, final_out=final_out)
        S.barrier()

    def proj_fm(self, tag, strm, gi, wbufs, tbs, evac, banks=(0, 1), parity=[0]):
        S, ps = self.S, self.ps
        wb = wbufs[parity[0] % 2]
        kb = ("wb", parity[0] % 2)
        parity[0] += 1
        S.dma("pool", lambda e, wb=wb, gi=gi: e.dma_start(out=wb, in_=self.win[gi]), writes=[kb])
        cnt = 0
        for half in range(2):
            for ti, (t0, t1e) in enumerate(tbs):
                n = t1e - t0
                bk = banks[cnt % len(banks)]
                cnt += 1
                pv = ps[:, bk, 0:n]
                for k in range(KC):
                    S.op("pe", lambda e, pv=pv, wb=wb, k=k, half=half, t0=t0, t1e=t1e: e.matmul(
                        pv, lhsT=wb[:, k, half * 128:(half + 1) * 128], rhs=strm[:, k, t0:t1e],
                        start=(k == 0), stop=(k == KC - 1)),
                        reads=[kb, "strm"], writes=[("ps", bk)])
                evac(half, ti, t0, t1e, pv, bk)

    def proj_tm(self, tag, strm, gi, wbufs, tls, ncols, evac, banks=(0, 1), parity=[0]):
        S, ps = self.S, self.ps
        wb = wbufs[parity[0] % 2]
        kb = ("wb", parity[0] % 2)
        parity[0] += 1
        S.dma("pool", lambda e, wb=wb, gi=gi: e.dma_start(out=wb, in_=self.win[gi]), writes=[kb])
        for cnt, (i, c0, n) in enumerate(tls):
            bk = banks[cnt % len(banks)]
            pv = ps[0:n, bk, 0:ncols]
            for k in range(KC):
                S.op("pe", lambda e, pv=pv, wb=wb, k=k, c0=c0, n=n: e.matmul(
                    pv, lhsT=strm[:, k, c0:c0 + n], rhs=wb[:, k, 0:ncols],
                    start=(k == 0), stop=(k == KC - 1)),
                    reads=[kb, "strm"], writes=[("ps", bk)])
            evac(i, c0, n, pv, bk)

    def hgrn_m1(self, strm):
        S, ps, nc = self.S, self.ps, self.nc
        R1 = 99840
        wbufs = [self.view(R1 + i * 8192, BF16, [KC, 256]) for i in range(2)]
        off = [R1 + 16384]

        def alloc(dt, shape):
            n = 1
            for x in shape:
                n *= x
            nb = n * (4 if dt == F32 else 2)
            nb = (nb + 63) // 64 * 64
            v = self.view(off[0], dt, shape)
            off[0] += nb
            return v
        logf = alloc(F32, [2, NT]); Bg = alloc(F32, [2, NT]); Bsh = alloc(F32, [2, NT])
        tA = alloc(F32, [2, NT]); tB = alloc(F32, [2, NT])
        kk = alloc(BF16, [2, NT]); qs = alloc(BF16, [2, NT]); qt = alloc(BF16, [2, NT])
        kt = alloc(BF16, [2, NT]); kh64 = alloc(BF16, [2, NT]); kh128 = alloc(BF16, [2, NT])
        vv = alloc(BF16, [9, 256])
        PT = alloc(BF16, [2, 128]); khT = alloc(BF16, [2, 128])
        xs_st = alloc(BF16, [9, 256]); xa_st = alloc(F32, [9, 2])
        Qc = self.view(0, BF16, [8, NREAL]); oloc = self.view(16384, BF16, [8, NREAL])
        cv = self.cv
        lbc, omlc = self.lbc, self.omlc
        psb = lambda bk: ps[:, bk, :].bitcast(BF16)
        TL = [(i, 128 * i, 128) for i in range(8)] + [(8, NREAL, NMETA)]
        flat = lambda v: v.rearrange("p h t -> p (h t)")
        r64 = lambda v: v[:, :, 0:NREAL].rearrange("p h (c t) -> p h c t", t=64)
        r128 = lambda v: v[:, :, 0:NREAL].rearrange("p h (c t) -> p h c t", t=128)
        mt = lambda v: v[:, :, NREAL:NT]

        for hp in range(4):
            T = ("m1", hp)
            def ev_f(half, ti, t0, t1e, pv, bk, hp=hp):
                h = 2 * hp + half
                S.op("act", lambda e: e.activation(out=tA[:, half, t0:t1e], in_=pv, func=AF.Sigmoid),
                     reads=[("ps", bk)], writes=[("tA", half, ti)])
                S.op("dve", lambda e: e.tensor_scalar(out=tA[:, half, t0:t1e], in0=tA[:, half, t0:t1e],
                                                      scalar1=omlc[:, h:h + 1], scalar2=lbc[:, h:h + 1],
                                                      op0=ALU.mult, op1=ALU.add),
                     reads=[("tA", half, ti), "lb"], writes=[("tA", half, ti)])
                S.op("act", lambda e: e.activation(out=logf[:, half, t0:t1e], in_=tA[:, half, t0:t1e], func=AF.Ln),
                     reads=[("tA", half, ti)], writes=[("logf", half, ti)])
                S.op("pool", lambda e: e.tensor_scalar(out=kk[:, half, t0:t1e], in0=tA[:, half, t0:t1e],
                                                       scalar1=-1.0, scalar2=1.0, op0=ALU.mult, op1=ALU.add),
                     reads=[("tA", half, ti)], writes=[("kk", half, ti)])
            self.proj_fm(T, strm, self.gidx["hf%d" % hp], wbufs, TBS, ev_f)

            def ev_q(half, ti, t0, t1e, pv, bk):
                S.op("act", lambda e: e.activation(out=qs[:, half, t0:t1e], in_=pv, func=AF.Silu),
                     reads=[("ps", bk)], writes=[("qs", half, ti)])
            self.proj_fm(T, strm, self.gidx["hq%d" % hp], wbufs, TBS, ev_q)

            def ev_v(i, c0, n, pv, bk):
                S.op("act", lambda e: e.activation(out=vv[0:n, i, :], in_=pv, func=AF.Copy),
                     reads=[("ps", bk)], writes=[("vv", i)])
            self.proj_tm(T, strm, self.gidx["hi%d" % hp], wbufs, TL, 256, ev_v)

            allk = lambda nm: [(nm, hl, ti) for hl in range(2) for ti in range(3)]
            S.op("dve", lambda e: e.tensor_tensor_scan(out=flat(Bg), data0=flat(logf), data1=flat(logf),
                                                       initial=0.0, op0=ALU.add, op1=ALU.min),
                 reads=allk("logf"), writes=["Bg"])
            S.op("pool", lambda e: e.memset(flat(Bsh)[:, 0:1], 0.0), writes=["Bsh0"])
            S.op("pool", lambda e: e.tensor_copy(out=flat(Bsh)[:, 1:2 * NT], in_=flat(Bg)[:, 0:2 * NT - 1]),
                 reads=["Bg"], writes=["Bsh"])
            S.op("dve", lambda e: e.tensor_tensor(out=r64(tB), in0=r64(Bg),
                                                  in1=r64(Bsh)[:, :, :, 0:1].to_broadcast([128, 2, 16, 64]),
                                                  op=ALU.subtract),
                 reads=["Bg", "Bsh", "Bsh0"], writes=["tBr"])
            S.op("dve", lambda e: e.tensor_tensor(out=mt(tB), in0=mt(Bg),
                                                  in1=mt(Bsh)[:, :, 0:1].to_broadcast([128, 2, NMETA]),
                                                  op=ALU.subtract),
                 reads=["Bg", "Bsh", "Bsh0"], writes=["tBm"])
            S.op("pool", lambda e: e.tensor_tensor(out=r128(tA), in0=r128(Bg),
                                                   in1=r128(Bsh)[:, :, :, 0:1].to_broadcast([128, 2, 8, 128]),
                                                   op=ALU.subtract),
                 reads=["Bg", "Bsh", "Bsh0"] + allk("tA"), writes=["tAr"] + allk("tA"))
            S.op("pool", lambda e: e.tensor_copy(out=mt(tA), in_=mt(tB)),
                 reads=["tBm"], writes=["tAm"])
            TBk, TAk = ["tBr", "tBm"], ["tAr", "tAm"] + allk("tA")
            S.op("act", lambda e: e.activation(out=flat(Bg), in_=flat(tB), func=AF.Exp),
                 reads=TBk + ["Bsh", "tAr"], writes=["Bg"])
            S.op("dve", lambda e: e.tensor_tensor(out=flat(qt), in0=flat(qs), in1=flat(Bg), op=ALU.mult),
                 reads=["Bg"] + allk("qs"), writes=["qt"])
            S.op("act", lambda e: e.activation(out=flat(Bsh), in_=flat(tB), func=AF.Exp, scale=-1.0),
                 reads=TBk + ["Bsh", "tAr", "Bsh0"], writes=["Bsh", "Bsh0"])
            S.op("dve", lambda e: e.tensor_tensor(out=flat(kt), in0=flat(kk), in1=flat(Bsh), op=ALU.mult),
                 reads=["Bsh"] + allk("kk"), writes=["kt"])
            S.op("dve", lambda e: e.tensor_tensor(out=r64(Bg), in0=r64(tB),
                                                  in1=r64(tB)[:, :, :, 63:64].to_broadcast([128, 2, 16, 64]),
                                                  op=ALU.subtract),
                 reads=TBk + ["qt"], writes=["Bg"])
            S.op("act", lambda e: e.activation(out=r64(Bg), in_=r64(Bg), func=AF.Exp, scale=-1.0),
                 reads=["Bg"], writes=["Bg"])
            S.op("dve", lambda e: e.tensor_tensor(out=r64(kh64), in0=r64(kk), in1=r64(Bg), op=ALU.mult),
                 reads=["Bg"] + allk("kk"), writes=["kh64"])
            S.op("act", lambda e: e.activation(out=flat(Bsh), in_=flat(tA), func=AF.Exp),
                 reads=TAk + ["kt"], writes=["Bsh"])
            S.op("dve", lambda e, hp=hp: e.tensor_tensor(out=Qc[:, 2 * hp:2 * hp + 2, :], in0=qs[:, :, 0:NREAL],
                                                         in1=Bsh[:, :, 0:NREAL], op=ALU.mult),
                 reads=["Bsh"] + allk("qs"), writes=[("Qc", hp)])
            S.op("pool", lambda e: e.tensor_copy(out=xa_st[:, 0:8, :].rearrange("p i h -> p h i"),
                                                 in_=r128(Bsh)[:, :, :, 127]),
                 reads=["Bsh"], writes=["xa_st"])
            S.op("pool", lambda e: e.tensor_copy(out=xa_st[:, 8, :], in_=Bsh[:, :, NT - 1]),
                 reads=["Bsh"], writes=["xa_st"])
            S.op("dve", lambda e: e.tensor_tensor(out=r128(Bg), in0=r128(tA),
                                                  in1=r128(tA)[:, :, :, 127:128].to_broadcast([128, 2, 8, 128]),
                                                  op=ALU.subtract),
                 reads=TAk + ["kh64"], writes=["Bg"])
            S.op("dve", lambda e: e.tensor_tensor(out=mt(Bg), in0=mt(tA),
                                                  in1=mt(tA)[:, :, NMETA - 1:NMETA].to_broadcast([128, 2, NMETA]),
                                                  op=ALU.subtract),
                 reads=TAk + ["kh64"], writes=["Bg"])
            S.op("act", lambda e: e.activation(out=flat(Bg), in_=flat(Bg), func=AF.Exp, scale=-1.0),
                 reads=["Bg"], writes=["Bg"])
            S.op("dve", lambda e: e.tensor_tensor(out=flat(kh128), in0=flat(kk), in1=flat(Bg), op=ALU.mult),
                 reads=["Bg"] + allk("kk"), writes=["kh128"])

            for (i, c0, n) in TL:
                if n == 128:
                    for hl in range(2):
                        o0 = hl * 128
                        S.op("pe", lambda e, hl=hl, o0=o0, c0=c0: e.matmul(
                            ps[:, 2, o0:o0 + 64], lhsT=kt[:, hl, c0:c0 + 128], rhs=qt[:, hl, c0:c0 + 64],
                            start=True, stop=True), reads=["kt", "qt"], writes=[("ps", 2)])
                        S.op("pe", lambda e, hl=hl, o0=o0, c0=c0: e.matmul(
                            ps[0:64, 2, o0 + 64:o0 + 128], lhsT=kh64[:, hl, c0:c0 + 64],
                            rhs=qt[:, hl, c0 + 64:c0 + 128], start=True, stop=True),
                            reads=["kh64", "qt"], writes=[("ps", 2)])
                        S.op("pe", lambda e, hl=hl, o0=o0, c0=c0: e.matmul(
                            ps[64:128, 2, o0 + 64:o0 + 128], lhsT=kt[:, hl, c0 + 64:c0 + 128],
                            rhs=qt[:, hl, c0 + 64:c0 + 128], start=True, stop=True),
                            reads=["kt", "qt"], writes=[("ps", 2)])
                    for hl in range(2):
                        S.op("dve", lambda e, hl=hl: e.tensor_tensor(
                            out=PT[:, hl, :], in0=ps[:, 2, hl * 128:(hl + 1) * 128], in1=self.mask2, op=ALU.mult),
                            reads=[("ps", 2), "mask2"], writes=[("PT", hl)])
                    for hl in range(2):
                        S.op("pe", lambda e, hl=hl, i=i: e.matmul(
                            ps[:, 3, hl * 128:(hl + 1) * 128], lhsT=vv[:, i, hl * 128:(hl + 1) * 128],
                            rhs=PT[:, hl, :], start=True, stop=True),
                            reads=[("vv", i), ("PT", hl)], writes=[("ps", 3)])
                    S.op("act", lambda e, hp=hp, c0=c0: e.activation(
                        out=oloc[:, 2 * hp:2 * hp + 2, c0:c0 + 128],
                        in_=ps[:, 3, 0:256].rearrange("p (h t) -> p h t", h=2), func=AF.Copy),
                        reads=[("ps", 3)], writes=[("oloc", hp, i)])
                for hl in range(2):
                    S.op("pe", lambda e, hl=hl, c0=c0, n=n: e.transpose(
                        out=psb(4)[0:n, hl * 128:(hl + 1) * 128], in_=kh128[:, hl, c0:c0 + n],
                        identity=self.ident_bf),
                        reads=["kh128", "ident"], writes=[("ps", 4)])
                S.op("dve", lambda e, n=n: e.tensor_copy(out=khT[0:n].rearrange("p h d -> p (h d)"),
                                                         in_=psb(4)[0:n, 0:256]),
                     reads=[("ps", 4)], writes=["khT"])
                for hl in range(2):
                    S.op("pe", lambda e, hl=hl, i=i, n=n: e.matmul(
                        ps[:, 5, hl * 128:(hl + 1) * 128], lhsT=khT[0:n, hl, :],
                        rhs=vv[0:n, i, hl * 128:(hl + 1) * 128], start=True, stop=True),
                        reads=["khT", ("vv", i)], writes=[("ps", 5)])
                S.op("dve", lambda e, i=i: e.tensor_copy(out=xs_st[:, i, :], in_=ps[:, 5, 0:256]),
                     reads=[("ps", 5)], writes=[("xs_st", i)])
            for q3 in range(3):
                S.dma("sp", lambda e, hp=hp, q3=q3: e.dma_start(
                    out=self.xs[q3].rearrange("p (i c) -> p i c", i=3)[:, :, hp * 256:(hp + 1) * 256],
                    in_=xs_st[:, 3 * q3:3 * q3 + 3, :]),
                    reads=[("xs_st", i) for i in range(9)], writes=[("xs", hp, q3)])
            S.dma("sp", lambda e, hp=hp: e.dma_start(
                out=self.xa.rearrange("p (i c) -> p i c", i=9)[:, :, 2 * hp:2 * hp + 2], in_=xa_st),
                reads=["xa_st"], writes=[("xa", hp)])
        S.barrier()
        rg = [[0, 1, 2, 3], [4, 5, 6, 7]]
        for q3 in range(3):
            S.coll(lambda e, q3=q3: e.collective_compute("AllGather", ALU.bypass, replica_groups=rg,
                                                         ins=[self.xs[q3]], outs=[self.xg[q3]]), writes=[("xg", q3)])
        S.coll(lambda e: e.collective_compute("AllGather", ALU.bypass, replica_groups=rg,
                                              ins=[self.xa], outs=[self.xag]), writes=["xag"])

    def hgrn_m2(self, strm):
        S, ps, nc = self.S, self.ps, self.nc
        R1 = 99840
        wbufs = [self.view(R1 + i * 8192, BF16, [KC, 256]) for i in range(2)]
        off = [R1 + 16384]

        def alloc(dt, shape):
            n = 1
            for x in shape:
                n *= x
            nb = n * (4 if dt == F32 else 2)
            nb = (nb + 63) // 64 * 64
            v = self.view(off[0], dt, shape)
            off[0] += nb
            return v
        sgate = alloc(BF16, [8, NREAL])
        Scur = alloc(F32, [8, 128]); SmF = alloc(F32, [8, 128])
        SAb = [alloc(BF16, [8, 128]) for _ in range(3)]
        Aall = alloc(F32, [4, 72])
        OF = alloc(F32, [8, 128]); OSQ = alloc(F32, [8, 128]); RS = alloc(F32, [8, 128])
        Qc = self.view(0, BF16, [8, NREAL]); oloc = self.view(16384, BF16, [8, NREAL])
        yhg = self.view(32768, BF16, [8, NREAL]); Smine = self.view(49152, BF16, [8, 8, 128])
        f2 = lambda v: v.rearrange("p h t -> p (h t)")
        RTB = TBS[0:2]

        for g4 in range(4):
            def ev_g(half, ti, t0, t1e, pv, bk, g4=g4):
                h = 2 * g4 + half
                S.op("act", lambda e: e.activation(out=sgate[:, h, t0:t1e], in_=pv, func=AF.Silu),
                     reads=[("ps", bk)], writes=[("sgate", h, ti)])
            self.proj_fm("m2", strm, self.gidx["hg%d" % g4], wbufs, RTB, ev_g)

        S.dma("sp", lambda e: e.dma_start(out=Aall, in_=self.xag.rearrange("(r p) c -> p r c", p=128)),
              reads=["xag"], writes=["Aall"])
        xg3 = [x_.rearrange("(r p) (i c) -> r p i c", p=128, i=3) for x_ in self.xg]
        S.dma("sp", lambda e: e.dma_start(out=f2(SAb[2]), in_=xg3[2][0, :, 2, :]), reads=[("xg", 2)],
              writes=[("SAb", 2)])
        S.op("dve", lambda e: e.tensor_copy(out=f2(Scur), in_=f2(SAb[2])), reads=[("SAb", 2)], writes=["Scur"])
        for g in range(32):
            r, i = g % 4, g // 4
            sb = SAb[g % 3]
            S.dma("sp", lambda e, sb=sb, r=r, i=i: e.dma_start(out=f2(sb), in_=xg3[i // 3][r, :, i % 3, :]),
                  reads=[("xg", i // 3)], writes=[("SAb", g % 3)])
            if r == 0:
                S.op("dve", lambda e: e.tensor_scalar(out=f2(SmF), in0=f2(Scur), scalar1=self.selc[:, 0:1],
                                                      scalar2=None, op0=ALU.mult),
                     reads=["Scur", "sel"], writes=["SmF"])
            else:
                dst = SmF if r < 3 else Smine[:, i]
                S.op("dve", lambda e, r=r, dst=dst: e.scalar_tensor_tensor(
                    out=f2(dst), in0=f2(Scur), scalar=self.selc[:, r:r + 1], in1=f2(SmF),
                    op0=ALU.mult, op1=ALU.add),
                    reads=["Scur", "sel", "SmF"], writes=(["SmF"] if r < 3 else [("Smine", i)]))
            if g < 31:
                for h in range(8):
                    S.op("dve", lambda e, h=h, r=r, i=i, sb=sb: e.scalar_tensor_tensor(
                        out=Scur[:, h, :], in0=Scur[:, h, :], scalar=Aall[:, r, i * 8 + h:i * 8 + h + 1],
                        in1=sb[:, h, :], op0=ALU.mult, op1=ALU.add),
                        reads=["Scur", "Aall", ("SAb", g % 3)], writes=["Scur"])

        for i in range(8):
            c0 = 128 * i
            for h in range(8):
                bk = 2 + h // 4
                S.op("pe", lambda e, h=h, i=i, c0=c0, bk=bk: e.matmul(
                    ps[:, bk, (h % 4) * 128:(h % 4 + 1) * 128], lhsT=Smine[:, i, h, :], rhs=Qc[:, h, c0:c0 + 128],
                    start=True, stop=True),
                    reads=[("Smine", i), "Qc"], writes=[("ps", bk)])
            S.op("dve", lambda e, c0=c0: e.tensor_tensor(
                out=OF, in0=ps[:, 2:4, :].rearrange("p a (h t) -> p (a h) t", h=4), in1=oloc[:, :, c0:c0 + 128],
                op=ALU.add),
                reads=[("ps", 2), ("ps", 3), "oloc"], writes=["OF"])
            S.op("act", lambda e: e.activation(out=f2(OSQ), in_=f2(OF), func=AF.Square),
                 reads=["OF"], writes=["OSQ"])
            for a in range(2):
                S.op("pe", lambda e, a=a: e.matmul(ps[:, 4 + a, :], lhsT=self.ones128, rhs=f2(OSQ)[:, a * 512:(a + 1) * 512],
                                                   start=True, stop=True),
                     reads=["OSQ", "ones128"], writes=[("ps", 4 + a)])
            S.op("act", lambda e: e.activation(out=f2(RS), in_=ps[:, 4:6, :].rearrange("p a b -> p (a b)"),
                                               func=AF.Ln, bias=self.eps_rms, scale=1.0),
                 reads=[("ps", 4), ("ps", 5), "eps"], writes=["RS"])
            S.op("act", lambda e: e.activation(out=f2(RS), in_=f2(RS), func=AF.Exp, scale=-0.5),
                 reads=["RS"], writes=["RS"])
            S.op("dve", lambda e: e.tensor_tensor(out=f2(OF), in0=f2(OF), in1=f2(RS), op=ALU.mult),
                 reads=["OF", "RS"], writes=["OF"])
            S.op("dve", lambda e, c0=c0: e.tensor_tensor(out=OF, in0=OF, in1=sgate[:, :, c0:c0 + 128], op=ALU.mult),
                 reads=["OF"] + [("sgate", h, c0 // 512) for h in range(8)], writes=["OF"])
            for h in range(8):
                S.op("pool", lambda e, h=h, c0=c0: e.tensor_scalar(
                    out=yhg[:, h, c0:c0 + 128], in0=OF[:, h, :], scalar1=self.gnc[:, h:h + 1], scalar2=None,
                    op0=ALU.mult),
                    reads=["OF", "gn"], writes=[("yhg", i)])
        S.barrier()

    def attn_m3(self, strm):
        S, ps, nc = self.S, self.ps, self.nc
        R1 = 99840
        psb = lambda bk: ps[:, bk, :].bitcast(BF16)
        K_all = self.view(R1, BF16, [2, 4112])
        V_all = self.view(R1 + 16448, BF16, [33, 258])
        IK_all = self.view(R1 + 33536, BF16, [4096])
        AugK = self.view(R1 + 41728, BF16, [4112])
        qT = self.view(R1 + 49952, BF16, [8, NREAL])
        iqT = self.view(R1 + 66336, BF16, [8, NREAL])
        sc = self.view(R1 + 82720, F32, [4096])
        wbufs = [self.view(R1 + 82720 + i * 8192, BF16, [KC, 256]) for i in range(2)]
        Dg = self.view(R1 + 99104, BF16, [16, 128])
        yatt = self.view(0, BF16, [8, NREAL])
        mb = self.view(16384, BF16, [4096])
        mbT = self.view(24576, BF16, [32, 128])
        junk = self.view(49152, BF16, [4096])
        rh = [self.view(57344 + q * 1024, BF16, [512]) for q in range(4)]
        ya = self.view(57344, BF16, [8, 128])
        PTb = [self.view(61440 + q * 2048, BF16, [1024]) for q in range(2)]
        cbt = self.view(65536, BF16, [4, 128])
        kst = self.view(49152, BF16, [2, NREAL])
        vst = self.view(49152 + 4096, BF16, [8, 258])
        ikst = self.view(49152 + 4096 + 4160, BF16, [NREAL])
        iktmp = self.view(49152 + 10304, F32, [64])
        ikn2 = self.view(49152 + 10304 + 256, BF16, [128])
        cst = self.cst
        AugQ = cst[:, 664:1176].bitcast(BF16)
        AugR = cst[:, 1176:1688].bitcast(BF16)
        wq = cst[:, 1688:1816].rearrange("p (i h) -> p i h", h=16)
        H = cst[:, 1816:1848]
        mx = cst[:, 1848:2008].rearrange("p (h c) -> p h c", h=8)
        mrow = cst[:, 2008:2016]; cc = cst[:, 2016:2024]; rs = cst[:, 2024:2032]
        Bt = cst[:, 2032:2033]; Wc = cst[:, 2033:2034]; mid = cst[:, 2034:2035]; cnt = cst[:, 2035:2036]
        u2 = cst[:, 2036:2037]; tau = cst[:, 2037:2038]; rstd1 = cst[:, 2038:2039]
        AQc = cst[:, 2040:2104].rearrange("p (i h) -> p i h", h=8)
        pw = cst[:, 2104:2136]
        gik = cst[:, 2136:2200]; bik = cst[:, 2200:2264]
        st6 = cst[:, 2264:2270]; mv = cst[:, 2270:2272]
        TL = [(i, 128 * i, 128) for i in range(8)] + [(8, NREAL, NMETA)]
        RTB = TBS[0:2]
        NB = 20

        S.dma("sp", lambda e: e.dma_start(out=cst[:, 2040:2264], in_=self.catt), writes=["catt"])
        S.op("pool", lambda e: e.memset(AugK[0:65, :], 0.0), writes=["AugK"])
        S.op("pool", lambda e: e.memset(AugQ[0:65, :], 0.0), writes=["AugQ"])
        S.op("pool", lambda e: e.memset(AugR[0:65, :], 0.0), writes=["AugR"])
        for rr in range(3):
            S.dma("pool", lambda e, rr=rr: e.dma_start(out=AugK[32 * rr:32 * rr + 1, :], in_=self.augk[rr:rr + 1, :]),
                  writes=["AugK"])
        for rr in range(2):
            S.dma("pool", lambda e, rr=rr: e.dma_start(out=AugQ[32 * rr:32 * rr + 1, :], in_=self.augs[rr:rr + 1, :]),
                  writes=["AugQ"])
            S.dma("pool", lambda e, rr=rr: e.dma_start(out=AugR[32 * rr:32 * rr + 1, :], in_=self.augs[rr:rr + 1, :]),
                  writes=["AugR"])
        S.dma("pool", lambda e: e.dma_start(out=cbt, in_=self.cbt_d.rearrange("p (r s) -> p r s", r=4)), writes=["cbt"])
        S.op("dve", lambda e: e.memset(vst.rearrange("p i (k c) -> p i k c", k=2)[:, :, :, 128:129], 1.0),
             writes=["vst1"])
        S.op("dve", lambda e: e.memset(V_all[:, 32, :].rearrange("p (k c) -> p k c", k=2)[:, :, 128:129], 1.0),
             writes=["V1"])

        def ev_k(half, ti, t0, t1e, pv, bk):
            if ti < 2:
                S.op("act", lambda e: e.activation(out=kst[:, half, t0:t1e], in_=pv, func=AF.Copy),
                     reads=[("ps", bk)], writes=[("kst", half, ti)])
            else:
                S.op("act", lambda e: e.activation(out=K_all[:, half, 4096:4112], in_=pv, func=AF.Copy),
                     reads=[("ps", bk)], writes=[("Kmeta", half)])
        self.proj_fm("m3", strm, self.gidx["ak"], wbufs, TBS, ev_k)

        def ev_v(i, c0, n, pv, bk):
            src = pv.rearrange("p (k c) -> p k c", k=2)
            if i < 8:
                dst = vst[:, i, :].rearrange("p (k c) -> p k c", k=2)[:, :, 0:128]
                S.op("act", lambda e: e.activation(out=dst, in_=src, func=AF.Copy),
                     reads=[("ps", bk), "vst1"], writes=[("vst", i)])
            else:
                dst = V_all[0:n, 32, :].rearrange("p (k c) -> p k c", k=2)[:, :, 0:128]
                S.op("act", lambda e: e.activation(out=dst, in_=src, func=AF.Copy),
                     reads=[("ps", bk), "V1"], writes=["Vmeta"])
        self.proj_tm("m3", strm, self.gidx["av"], wbufs, TL, 256, ev_v)

        def ev_ik(i, c0, n, pv, bk):
            S.op("dve", lambda e: e.bn_stats(out=st6, in_=pv[:, 0:64]), reads=[("ps", bk)], writes=["st6"])
            S.op("dve", lambda e: e.bn_aggr(out=mv, in_=st6), reads=["st6"], writes=["mv"])
            S.op("act", lambda e: e.activation(out=rstd1, in_=mv[:, 1:2], func=AF.Sqrt, bias=self.eps_ik, scale=1.0),
                 reads=["mv", "eps"], writes=["rstd1"])
            S.op("dve", lambda e: e.reciprocal(out=rstd1, in_=rstd1), reads=["rstd1"], writes=["rstd1"])
            S.op("dve", lambda e: e.tensor_scalar(out=iktmp, in0=pv[:, 0:64], scalar1=mv[:, 0:1], scalar2=rstd1,
                                                  op0=ALU.subtract, op1=ALU.mult),
                 reads=[("ps", bk), "mv", "rstd1"], writes=["iktmp"])
            S.op("dve", lambda e: e.tensor_tensor(out=iktmp, in0=iktmp, in1=gik, op=ALU.mult),
                 reads=["iktmp", "catt"], writes=["iktmp"])
            S.op("dve", lambda e: e.tensor_tensor(out=ikn2[:, 0:64], in0=iktmp, in1=bik, op=ALU.add),
                 reads=["iktmp", "catt"], writes=["ikn2a"])
            S.op("pool", lambda e: e.tensor_copy(out=ikn2[:, 64:128], in_=ikn2[:, 0:64]),
                 reads=["ikn2a"], writes=["ikn2b"])
            S.op("act", lambda e, i=i: e.activation(out=wq[:, i, :], in_=pv[:, 64:80], func=AF.Copy,
                                                    scale=0.25 * 0.125),
                 reads=[("ps", bk)], writes=[("wq", i)])
            S.op("pe", lambda e: e.transpose(out=psb(2)[:, 0:128], in_=ikn2, identity=self.ident_bf),
                 reads=["ikn2a", "ikn2b", "ident"], writes=[("ps", 2)])
            S.op("act", lambda e, c0=c0: e.activation(out=ikst[:, c0:c0 + 128], in_=psb(2)[:, 0:128], func=AF.Copy),
                 reads=[("ps", 2)], writes=[("ikst", i)])
        self.proj_tm("m3", strm, self.gidx["ikw"], wbufs, TL[0:8], 80, ev_ik)

        S.dma("sp", lambda e: e.dma_start(out=self.ks.rearrange("p (k t) -> p k t", k=2), in_=kst),
              reads=[("kst", hh, ti) for hh in range(2) for ti in range(2)], writes=["ks"])
        S.dma("sp", lambda e: e.dma_start(out=self.vs[:, 0:2064].rearrange("p (i c) -> p i c", i=8), in_=vst),
              reads=[("vst", i) for i in range(8)] + ["vst1"], writes=["vs"])
        S.dma("sp", lambda e: e.dma_start(out=self.vs[:, 2064:3088], in_=ikst),
              reads=[("ikst", i) for i in range(8)], writes=["vs2"])
        S.barrier()
        rg = [[0, 1, 2, 3], [4, 5, 6, 7]]
        S.coll(lambda e: e.collective_compute("AllGather", ALU.bypass, replica_groups=rg,
                                              ins=[self.ks], outs=[self.kg]), writes=["kg"])
        S.coll(lambda e: e.collective_compute("AllGather", ALU.bypass, replica_groups=rg,
                                              ins=[self.vs], outs=[self.vg]), writes=["vg"])

        for g4 in range(4):
            def ev_q(half, ti, t0, t1e, pv, bk, g4=g4):
                h = 2 * g4 + half
                S.op("act", lambda e: e.activation(out=qT[:, h, t0:t1e], in_=pv, func=AF.Copy, scale=128.0 ** -0.5),
                     reads=[("ps", bk)], writes=[("qT", h, ti)])
            self.proj_fm("m3", strm, self.gidx["aq%d" % g4], wbufs, RTB, ev_q)
        for g4 in range(4):
            def ev_iq(half, ti, t0, t1e, pv, bk, g4=g4):
                h = 2 * g4 + half
                S.op("dve", lambda e: e.tensor_copy(out=iqT[:, h, t0:t1e], in_=pv),
                     reads=[("ps", bk)], writes=[("iqT", h, ti)])
            self.proj_fm("m3", strm, self.gidx["iq%d" % g4], wbufs, RTB, ev_iq)

        for r in range(4):
            S.dma("sp", lambda e, r=r: e.dma_start(
                out=K_all[:, :, r * 1024:(r + 1) * 1024],
                in_=self.kg[r * 128:(r + 1) * 128, :].rearrange("p (k t) -> p k t", k=2)),
                reads=["kg"], writes=["K_all"])
            S.dma("sp", lambda e, r=r: e.dma_start(
                out=V_all[:, r * 8:(r + 1) * 8, :],
                in_=self.vg[r * 128:(r + 1) * 128, 0:2064].rearrange("p (i c) -> p i c", i=8)),
                reads=["vg"], writes=["V_all"])
            S.dma("sp", lambda e, r=r: e.dma_start(
                out=IK_all[:, r * 1024:(r + 1) * 1024], in_=self.vg[r * 128:(r + 1) * 128, 2064:3088]),
                reads=["vg"], writes=["IK_all"])
        S.barrier()

        sc4 = sc.rearrange("p (r c) -> p r c", r=4)
        mb4 = mb.rearrange("p (r c) -> p r c", r=4)
        jk4 = junk.rearrange("p (r c) -> p r c", r=4)
        def qblock(i):
                q0 = 128 * i
                nk = 128 * (i + 1)
                pieces = [(r, c0, min(512, nk - c0)) for r in range(4) for c0 in range(0, nk, 512)]
                scv, mbv, jkv = sc4[:, :, 0:nk], mb4[:, :, 0:nk], jk4[:, :, 0:nk]
                S.dma("pool", lambda e, i=i: e.dma_start(out=AugQ[64:65, :], in_=self.augq[i:i + 1, :]), writes=["AugQ"])
                for h in range(16):
                    S.op("pool", lambda e, h=h, i=i: e.tensor_scalar(out=Dg[:, h, :], in0=self.ident_bf,
                                                                     scalar1=wq[:, i, h:h + 1], scalar2=None, op0=ALU.mult),
                         reads=["ident", ("wq", i)], writes=["Dg"])
                for pi, (r, c0, cn) in enumerate(pieces):
                    col0 = r * 1024 + c0
                    accb = 4 + pi % 2
                    for h in range(16):
                        bk = h % 4
                        hb = h % 2
                        S.op("pe", lambda e, bk=bk, hb=hb, h=h, col0=col0, cn=cn, q0=q0: e.matmul(
                            ps[:, bk, 0:cn], lhsT=iqT[hb * 64:(hb + 1) * 64, h // 2, q0:q0 + 128],
                            rhs=IK_all[hb * 64:(hb + 1) * 64, col0:col0 + cn], start=True, stop=True),
                            reads=["IK_all", "iqT"], writes=[("ps", bk)])
                        if h % 2 == 0:
                            S.op("act", lambda e, bk=bk, cn=cn: e.activation(out=rh[bk][:, 0:cn], in_=ps[:, bk, 0:cn],
                                                                             func=AF.Relu),
                                 reads=[("ps", bk)], writes=[("rh", bk)])
                        else:
                            S.op("dve", lambda e, bk=bk, cn=cn: e.tensor_scalar(out=rh[bk][:, 0:cn], in0=ps[:, bk, 0:cn],
                                                                                scalar1=0.0, scalar2=None, op0=ALU.max),
                                 reads=[("ps", bk)], writes=[("rh", bk)])
                        S.op("pe", lambda e, accb=accb, h=h, bk=bk, cn=cn: e.matmul(
                            ps[:, accb, 0:cn], lhsT=Dg[:, h, :], rhs=rh[bk][:, 0:cn], start=(h == 0), stop=(h == 15)),
                            reads=["Dg", ("rh", bk)], writes=[("ps", accb)])
                    S.op("act", lambda e, accb=accb, col0=col0, cn=cn: e.activation(
                        out=sc[:, col0:col0 + cn], in_=ps[:, accb, 0:cn], func=AF.Copy),
                        reads=[("ps", accb)], writes=["sc"])
                S.op("dve", lambda e, scv=scv: e.reduce_max(out=Bt, in_=scv, axis=AX.XY, apply_absolute_value=True),
                     reads=["sc"], writes=["Bt"])
                S.op("dve", lambda e, i=i: e.tensor_tensor(out=sc4[:, :, q0:q0 + 128], in0=sc4[:, :, q0:q0 + 128], in1=cbt,
                                                           op=ALU.add),
                     reads=["sc", "cbt", "Bt"], writes=["sc"])
                S.op("dve", lambda e: e.tensor_scalar(out=Wc, in0=Bt, scalar1=2.0002, scalar2=1e-6,
                                                      op0=ALU.mult, op1=ALU.add), reads=["Bt"], writes=["Wc"])
                S.op("dve", lambda e: e.tensor_scalar(out=H[:, 0:NB + 1], in0=pw[:, 0:NB + 1], scalar1=Wc, scalar2=None,
                                                      op0=ALU.mult), reads=["Wc", "catt"], writes=["H"])
                S.op("dve", lambda e: e.memset(mid, 0.0), writes=["mid"])
                for k in range(NB):
                    S.op("dve", lambda e, scv=scv, jkv=jkv: e.tensor_scalar(
                        out=jkv, in0=scv, scalar1=mid, scalar2=0.0, op0=ALU.is_ge, op1=ALU.add, accum_out=cnt),
                        reads=["sc", "mid"], writes=["junk", "cnt"])
                    S.op("dve", lambda e, k=k: e.tensor_scalar(out=u2, in0=cnt, scalar1=256.0, scalar2=H[:, k:k + 1],
                                                               op0=ALU.is_ge, op1=ALU.mult),
                         reads=["cnt", "H"], writes=["u2"])
                    S.op("dve", lambda e, k=k: e.scalar_tensor_tensor(out=mid, in0=mid, scalar=H[:, k + 1:k + 2], in1=u2,
                                                                      op0=ALU.subtract, op1=ALU.add),
                         reads=["mid", "H", "u2"], writes=["mid"])
                S.op("dve", lambda e: e.tensor_tensor(out=tau, in0=mid, in1=H[:, NB:NB + 1], op=ALU.subtract),
                     reads=["mid", "H"], writes=["tau"])
                S.op("dve", lambda e, scv=scv, mbv=mbv: e.tensor_scalar(
                    out=mbv, in0=scv, scalar1=tau, scalar2=-30000.0, op0=ALU.is_lt, op1=ALU.mult),
                    reads=["sc", "tau"], writes=["mb"])
                kts = [(r, ip) for r in range(4) for ip in range(i + 1)]
                for g0 in range(0, len(kts), 8):
                    grp = kts[g0:g0 + 8]
                    bk = 6 + (g0 // 8) % 2
                    for s_, (r, ip) in enumerate(grp):
                        S.op("pe", lambda e, bk=bk, s_=s_, r=r, ip=ip: e.transpose(
                            out=psb(bk)[:, s_ * 128:(s_ + 1) * 128], in_=mb[:, r * 1024 + ip * 128:r * 1024 + ip * 128 + 128],
                            identity=self.ident_bf),
                            reads=["mb", "ident"], writes=[("ps", bk)])
                    for s_, (r, ip) in enumerate(grp):
                        S.op("act", lambda e, bk=bk, s_=s_, r=r, ip=ip: e.activation(
                            out=mbT[:, r * 8 + ip, :], in_=psb(bk)[:, s_ * 128:(s_ + 1) * 128], func=AF.Copy),
                            reads=[("ps", bk)], writes=["mbT"])
                pcs = pieces + [(4, 0, NMETA)]
                for h in range(8):
                    kvh = h // 4
                    for pi, (r, c0, cn) in enumerate(pcs):
                        col0 = r * 1024 + c0
                        bk = (h * len(pcs) + pi) % 4
                        meta = (r == 4)
                        S.op("pe", lambda e, bk=bk, h=h, kvh=kvh, col0=col0, cn=cn: e.matmul(
                            ps[:, bk, 0:cn], lhsT=qT[:, h, q0:q0 + 128], rhs=K_all[:, kvh, col0:col0 + cn],
                            start=True, stop=False),
                            reads=["K_all", "qT", ("Kmeta", kvh)], writes=[("ps", bk)])
                        S.op("pe", lambda e, bk=bk, h=h, col0=col0, cn=cn, meta=meta: e.matmul(
                            ps[:, bk, 0:cn], lhsT=AugQ[0:65, h * 128:(h + 1) * 128], rhs=AugK[0:65, col0:col0 + cn],
                            start=False, stop=meta),
                            reads=["AugQ", "AugK"], writes=[("ps", bk)])
                        if not meta:
                            S.op("pe", lambda e, bk=bk, col0=col0, cn=cn: e.matmul(
                                ps[:, bk, 0:cn], lhsT=self.ident_bf, rhs=mb[:, col0:col0 + cn], start=False, stop=True),
                                reads=["mb", "ident"], writes=[("ps", bk)])
                        S.op("dve", lambda e, bk=bk, h=h, pi=pi, cn=cn: e.reduce_max(
                            out=mx[:, h, pi:pi + 1], in_=ps[:, bk, 0:cn], axis=AX.X),
                            reads=[("ps", bk)], writes=["mx"])
                S.op("dve", lambda e, npc=len(pcs): e.reduce_max(out=mrow, in_=mx[:, :, 0:npc], axis=AX.X),
                     reads=["mx"], writes=["mrow"])
                S.op("dve", lambda e, i=i: e.tensor_tensor(out=cc, in0=AQc[:, i, :], in1=mrow, op=ALU.subtract),
                     reads=["mrow", "catt"], writes=["cc"])
                for h in range(8):
                    S.op("pool", lambda e, h=h: e.tensor_scalar(out=Dg[:, h, :], in0=self.ident_bf, scalar1=cc[:, h:h + 1],
                                                                scalar2=None, op0=ALU.mult),
                         reads=["ident", "cc"], writes=["Dg"])
                for a in range(2):
                    S.op("pe", lambda e, a=a: e.matmul(ps[:, 4 + a, :], lhsT=self.ones_bf,
                                                       rhs=Dg[:, 4 * a:4 * a + 4, :].rearrange("p h t -> p (h t)"),
                                                       start=True, stop=True),
                         reads=["Dg", "ones_bf"], writes=[("ps", 4 + a)])
                S.op("dve", lambda e: e.tensor_copy(out=AugR[64:65, :],
                                                    in_=ps[64:65, 4:6, :].rearrange("p a b -> p (a b)")),
                     reads=[("ps", 4), ("ps", 5)], writes=["AugR"])
                ktl = [(r * 1024 + ip * 128, r * 8 + ip, 128) for r in range(4) for ip in range(i + 1)] + [(4096, 32, NMETA)]
                Oreg = lambda h: ps[:, 5 + h // 3, (h % 3) * 129:(h % 3 + 1) * 129]
                S.op("dve", lambda e: e.memset(ps[:, 5:8, :].rearrange("p a b -> p (a b)"), 0.0),
                     writes=[("ps", 5), ("ps", 6), ("ps", 7)])
                for qi, (col0, vt, n) in enumerate(ktl):
                    meta = (n == NMETA)
                    pair = (0, 1) if qi % 2 == 0 else (2, 3)
                    for h in range(8):
                        kvh = h // 4
                        out = ps[0:n, pair[h // 4], (h % 4) * 128:(h % 4 + 1) * 128]
                        S.op("pe", lambda e, out=out, kvh=kvh, col0=col0, n=n, h=h: e.matmul(
                            out, lhsT=K_all[:, kvh, col0:col0 + n], rhs=qT[:, h, q0:q0 + 128], start=True, stop=False),
                            reads=["K_all", "qT", ("Kmeta", kvh)], writes=[("ps", pair[h // 4])])
                        S.op("pe", lambda e, out=out, col0=col0, n=n, h=h, meta=meta: e.matmul(
                            out, lhsT=AugK[0:65, col0:col0 + n], rhs=AugR[0:65, h * 128:(h + 1) * 128],
                            start=False, stop=meta),
                            reads=["AugK", "AugR"], writes=[("ps", pair[h // 4])])
                        if not meta:
                            S.op("pe", lambda e, out=out, vt=vt: e.matmul(
                                out, lhsT=self.ident_bf, rhs=mbT[:, vt, :], start=False, stop=True),
                                reads=["mbT", "ident"], writes=[("ps", pair[h // 4])])
                    pt = PTb[qi % 2]
                    S.op("act", lambda e, pt=pt, pair=pair, n=n: e.activation(
                        out=pt[0:n, :], in_=ps[0:n, pair[0]:pair[0] + 2, :].rearrange("p a b -> p (a b)"), func=AF.Exp),
                        reads=[("ps", pair[0]), ("ps", pair[1])], writes=[("PTb", qi % 2)])
                    for h in range(8):
                        kvh = h // 4
                        S.op("pe", lambda e, h=h, kvh=kvh, pt=pt, vt=vt, n=n, qi=qi: e.matmul(
                            Oreg(h), lhsT=pt[0:n, h * 128:(h + 1) * 128], rhs=V_all[0:n, vt, kvh * 129:(kvh + 1) * 129],
                            start=False, stop=(qi == len(ktl) - 1)),
                            reads=[("PTb", qi % 2), "V_all", "Vmeta"], writes=[("ps", 5 + h // 3)])
                for b3 in range(3):
                    nh = 3 if b3 < 2 else 2
                    Ov = ps[:, 5 + b3, 0:nh * 129].rearrange("p (h c) -> p h c", c=129)
                    S.op("dve", lambda e, Ov=Ov, b3=b3, nh=nh: e.reciprocal(out=rs[:, 3 * b3:3 * b3 + nh], in_=Ov[:, :, 128]),
                         reads=[("ps", 5 + b3)], writes=[("rs", b3)])
                    S.op("dve", lambda e, Ov=Ov, b3=b3, nh=nh: e.tensor_tensor(
                        out=ya[:, 3 * b3:3 * b3 + nh, :], in0=Ov[:, :, 0:128],
                        in1=rs[:, 3 * b3:3 * b3 + nh].unsqueeze(2).to_broadcast([128, nh, 128]), op=ALU.mult),
                        reads=[("ps", 5 + b3), ("rs", b3)] + [("rh", q) for q in range(4)], writes=["ya"] + [("rh", q) for q in range(4)])
                for h in range(8):
                    S.op("pe", lambda e, h=h: e.transpose(out=psb(4)[:, h * 128:(h + 1) * 128], in_=ya[:, h, :],
                                                          identity=self.ident_bf),
                         reads=["ya", "ident"] + [("rh", q) for q in range(4)], writes=[("ps", 4)])
                S.op("act", lambda e, q0=q0: e.activation(out=yatt[:, :, q0:q0 + 128],
                                                          in_=psb(4).rearrange("p (h t) -> p h t", h=8), func=AF.Copy),
                     reads=[("ps", 4)], writes=[("yatt", i)])

        for i in range(8):
            qblock(i)
            if i == 0 and self.stage == 4:
                d2 = self.dbg2
                S.barrier()
                S.dma("sp", lambda e: e.dma_start(out=d2[:, 0:4096], in_=sc), reads=[])
                S.dma("sp", lambda e: e.dma_start(out=d2[:, 4096:4096 + 264], in_=cst[:, 1816:2080]), reads=[])
                S.dma("pool", lambda e: e.dma_start(out=d2[:, 4400:4400 + 4096], in_=mb), reads=[])
                S.dma("pool", lambda e: e.dma_start(out=d2[0:65, 8500:8500 + 1024], in_=AugR[0:65, :]), reads=[])
                S.dma("pool", lambda e: e.dma_start(out=d2[0:65, 9600:9600 + 1024], in_=AugQ[0:65, :]), reads=[])
                S.dma("pool", lambda e: e.dma_start(out=d2[:, 10700:10700 + 1024], in_=ya.rearrange("p h d -> p (h d)")), reads=[])
                S.barrier()
        S.barrier()

    def merge_m4(self, strm, cg, cb):
        S, ps, nc = self.S, self.ps, self.nc
        R1 = 99840
        RTB = TBS[0:2]
        yatt = self.view(0, BF16, [8, NREAL]); yhg = self.view(32768, BF16, [8, NREAL])
        merged = self.view(R1, BF16, [KC, NREAL])
        o = R1 + 32768
        wga = [self.view(o + q * 8192, BF16, [KC, 256]) for q in range(2)]
        wgh = [self.view(o + 16384 + q * 8192, BF16, [KC, 256]) for q in range(2)]
        wba = [self.view(o + 32768 + q * 4096, BF16, [8, 256]) for q in range(2)]
        wbh = [self.view(o + 40960 + q * 4096, BF16, [8, 256]) for q in range(2)]
        tm = [self.view(o + 49152 + q * 2048, F32, [512]) for q in range(4)]
        for mg in range(8):
            q = mg % 2
            S.dma("pool", lambda e, q=q, mg=mg: e.dma_start(out=wga[q], in_=self.win[self.gidx["ga%d" % mg]]),
                  writes=[("wga", q)])
            S.dma("pool", lambda e, q=q, mg=mg: e.dma_start(out=wgh[q], in_=self.win[self.gidx["gh%d" % mg]]),
                  writes=[("wgh", q)])
            S.dma("pool", lambda e, q=q, mg=mg: e.dma_start(out=wba[q], in_=self.wba_d[mg]), writes=[("wba", q)])
            S.dma("pool", lambda e, q=q, mg=mg: e.dma_start(out=wbh[q], in_=self.wbh_d[mg]), writes=[("wbh", q)])
            for half in range(2):
                mc = 2 * mg + half
                hs = slice(half * 128, (half + 1) * 128)
                for ti, (t0, t1e) in enumerate(RTB):
                    pp = (half * 2 + ti) % 2
                    b0 = 4 * pp
                    for k in range(KC):
                        S.op("pe", lambda e, b0=b0, q=q, k=k, hs=hs, t0=t0, t1e=t1e: e.matmul(
                            ps[:, b0, :], lhsT=wga[q][:, k, hs], rhs=strm[:, k, t0:t1e],
                            start=(k == 0), stop=(k == KC - 1)),
                            reads=[("wga", q), "strm"], writes=[("ps", b0)])
                    for k in range(8):
                        S.op("pe", lambda e, b0=b0, q=q, k=k, hs=hs, t0=t0, t1e=t1e: e.matmul(
                            ps[:, b0 + 1, :], lhsT=wba[q][:, k, hs], rhs=yatt[:, k, t0:t1e],
                            start=(k == 0), stop=(k == 7)),
                            reads=[("wba", q), "yatt"], writes=[("ps", b0 + 1)])
                    for k in range(KC):
                        S.op("pe", lambda e, b0=b0, q=q, k=k, hs=hs, t0=t0, t1e=t1e: e.matmul(
                            ps[:, b0 + 2, :], lhsT=wgh[q][:, k, hs], rhs=strm[:, k, t0:t1e],
                            start=(k == 0), stop=(k == KC - 1)),
                            reads=[("wgh", q), "strm"], writes=[("ps", b0 + 2)])
                    for k in range(8):
                        S.op("pe", lambda e, b0=b0, q=q, k=k, hs=hs, t0=t0, t1e=t1e: e.matmul(
                            ps[:, b0 + 3, :], lhsT=wbh[q][:, k, hs], rhs=yhg[:, k, t0:t1e],
                            start=(k == 0), stop=(k == 7)),
                            reads=[("wbh", q), "yhg"], writes=[("ps", b0 + 3)])
                    ta, th = tm[2 * pp], tm[2 * pp + 1]
                    S.op("act", lambda e, ta=ta, b0=b0: e.activation(out=ta, in_=ps[:, b0, :], func=AF.Sigmoid),
                         reads=[("ps", b0)], writes=[("tm", 2 * pp)])
                    S.op("dve", lambda e, ta=ta, b0=b0: e.tensor_tensor(out=ta, in0=ta, in1=ps[:, b0 + 1, :], op=ALU.mult),
                         reads=[("tm", 2 * pp), ("ps", b0 + 1)], writes=[("tm", 2 * pp)])
                    S.op("act", lambda e, th=th, b0=b0: e.activation(out=th, in_=ps[:, b0 + 2, :], func=AF.Sigmoid),
                         reads=[("ps", b0 + 2)], writes=[("tm", 2 * pp + 1)])
                    S.op("dve", lambda e, th=th, b0=b0: e.tensor_tensor(out=th, in0=th, in1=ps[:, b0 + 3, :], op=ALU.mult),
                         reads=[("tm", 2 * pp + 1), ("ps", b0 + 3)], writes=[("tm", 2 * pp + 1)])
                    S.op("pool", lambda e, ta=ta, th=th, mc=mc, t0=t0, t1e=t1e: e.tensor_tensor(
                        out=merged[:, mc, t0:t1e], in0=ta, in1=th, op=ALU.add),
                        reads=[("tm", 2 * pp), ("tm", 2 * pp + 1)], writes=[("merged", mc, ti)])
        S.barrier()
        z = self.view(R1 + 32768, F32, [KC, NREAL])
        wo = [self.view(53760 + q * 4096, BF16, [KC, 128]) for q in range(2)]
        strm_out = self.view(0, BF16, [KC, NREAL])
        for dc in range(KC):
            q = dc % 2
            S.dma("pool", lambda e, q=q, dc=dc: e.dma_start(out=wo[q], in_=self.wo_d[dc]), writes=[("wo", q)])
            S.dma("sp", lambda e, dc=dc: e.dma_start(out=z[:, dc, :], in_=self.h1s[dc * 128:(dc + 1) * 128, 0:NREAL]),
                  writes=[("l2", "z", dc, ti) for ti in range(2)])
            for ti, (t0, t1e) in enumerate(RTB):
                pb = (dc * 2 + ti) % 2
                for k in range(KC):
                    S.op("pe", lambda e, pb=pb, q=q, k=k, t0=t0, t1e=t1e: e.matmul(
                        ps[:, pb, :], lhsT=wo[q][:, k, :], rhs=merged[:, k, t0:t1e],
                        start=(k == 0), stop=(k == KC - 1)),
                        reads=[("wo", q), ("merged", k, ti)], writes=[("ps", pb)])
                S.op("dve", lambda e, pb=pb, dc=dc, t0=t0, t1e=t1e: e.scalar_tensor_tensor(
                    out=z[:, dc, t0:t1e], in0=ps[:, pb, :], scalar=1.0 / ALPHA, in1=z[:, dc, t0:t1e],
                    op0=ALU.mult, op1=ALU.add),
                    reads=[("ps", pb), ("l2", "z", dc, ti)], writes=[("l2", "z", dc, ti)])
        self.ln_apply("l2", z, RTB, cg, cb, 33280, LN_EPS / (ALPHA * ALPHA), strm_out=strm_out,
                      resid_out=self.h2s)
        S.barrier()

    def eps_col(self, val):
        return self.eps_cols[val]

    def build(self):
        nc, S = self.nc, self.S
        stage = self.stage
        xT = self.din("xT", [D, NT])
        wg1 = self.din("wg1", [FC // 2, 128, KC, 256])
        wu1 = self.din("wu1", [FC // 2, 128, KC, 256])
        wd1 = self.din("wd1", [KC, 128, FC, 128])
        wg2 = self.din("wg2", [FC // 2, 128, KC, 256])
        wu2 = self.din("wu2", [FC // 2, 128, KC, 256])
        wd2 = self.din("wd2", [KC, 128, FC, 128])
        cvec = self.din("cvec", [128, 128])
        cmat = self.din("cmat", [128, 384])
        self.catt = self.din("catt", [128, 224])
        self.augk = self.din("augk", [3, 4112])
        self.augs = self.din("augs", [2, 1024])
        self.augq = self.din("augq", [8, 1024])
        self.cbt_d = self.din("cbt", [128, 512])
        self.win = self.din("win", [len(GROUPS), 128, KC, 256])
        self.wba_d = self.din("wba", [8, 128, 8, 256])
        self.wbh_d = self.din("wbh", [8, 128, 8, 256])
        self.wo_d = self.din("wo", [KC, 128, KC, 128])
        self.gidx = {nm: i for i, (nm, _, _) in enumerate(GROUPS)}
        self.h1s = h1s = self.dscratch("h1s", [D, NT])
        self.h2s = self.dscratch("h2s", [D, NREAL])
        self.xs = [self.dscratch("xs%d" % q, [128, 3 * 1024], BF16) for q in range(3)]
        self.xg = [self.dscratch("xg%d" % q, [512, 3 * 1024], BF16) for q in range(3)]
        self.xa = self.dscratch("xa", [128, 72])
        self.xag = self.dscratch("xag", [512, 72])
        self.ks = self.dscratch("ks", [128, 2048], BF16)
        self.kg = self.dscratch("kg", [512, 2048], BF16)
        self.vs = self.dscratch("vs", [128, 3088], BF16)
        self.vg = self.dscratch("vg", [512, 3088], BF16)
        if stage == 1:
            dbg = self.dout("dbg", [D, NT])
        elif stage in (3, 4):
            dbg = self.dout("dbg", [128, 8 * NREAL])
            self.dbg2 = self.dout("dbg2", [128, 12000])
        elif stage == 5:
            dbg = self.dout("dbg", [D, NREAL])
        else:
            outT = self.dout("outT", [D, NREAL])

        from contextlib import ExitStack
        with ExitStack() as es:
            self.arena = es.enter_context(nc.sbuf_tensor("arena", [128, ARENA_F32], F32))
            self.cst = es.enter_context(nc.sbuf_tensor("cst", [128, CONST_F32], F32))
            self.ps = es.enter_context(nc.psum_tensor("ps", [128, 8, 512], F32))
            esems = {e: es.enter_context(nc.semaphore("sem_" + e)) for e in ENGS}
            dsems = [es.enter_context(nc.semaphore("dsem%d" % i)) for i in range(S.n_dma_sems + 8)]
            block = es.enter_context(nc.Block())
            cst = self.cst
            self.ones_f32 = cst[:, 0:128]
            cv = self.cv = cst[:, 128:256]
            epsA = cst[:, 256:257]
            self.eps_rms = cst[:, 257:258]
            self.eps_ik = cst[:, 258:259]
            self.eps_cols = {LN_EPS / (ALPHA * ALPHA): epsA}
            self.ident_bf = cst[:, 264:328].bitcast(BF16)
            self.ones_bf = cst[:, 328:392].bitcast(BF16)
            self.mask2 = cst[:, 392:520]
            self.ones128 = cst[:, 520:648]
            self.lbc = cst[:, 648:656]
            self.omlc = cst[:, 656:664]
            self.gnc = cv[:, 112:120]
            self.selc = cv[:, 120:124]
            S.op("dve", lambda e: e.memset(self.ones_f32, 1.0 / D), writes=["ones"])
            S.op("dve", lambda e: e.memset(self.ones128, 1.0 / 128), writes=["ones128"])
            S.op("dve", lambda e: e.memset(epsA, LN_EPS / (ALPHA * ALPHA)), writes=["eps"])
            S.op("dve", lambda e: e.memset(self.eps_rms, RMS_EPS), writes=["eps"])
            S.op("dve", lambda e: e.memset(self.eps_ik, LN_EPS), writes=["eps"])
            S.dma("sp", lambda e: e.dma_start(out=cv, in_=cvec), writes=["cv"])
            S.dma("sp", lambda e: e.dma_start(out=self.mask2, in_=cmat[:, 256:384]), writes=["mask2"])
            S.dma("pool", lambda e: e.dma_start(out=self.ident_bf, in_=cmat[:, 0:128]), writes=["ident"])
            S.dma("pool", lambda e: e.dma_start(out=self.ones_bf, in_=cmat[:, 128:256]), writes=["ones_bf"])
            S.op("dve", lambda e: e.tensor_tensor(out=self.lbc, in0=cv[:, 96:104], in1=cv[:, 104:112], op=ALU.subtract),
                 reads=["cv"], writes=["lb"])
            S.op("act", lambda e: e.activation(out=self.lbc, in_=self.lbc, func=AF.Sigmoid), reads=["lb"], writes=["lb"])
            S.op("dve", lambda e: e.tensor_scalar(out=self.omlc, in0=self.lbc, scalar1=-1.0, scalar2=1.0,
                                                  op0=ALU.mult, op1=ALU.add), reads=["lb"], writes=["lb"])
            strm0 = self.view(0, BF16, [KC, NT])
            S.dma("pool", lambda e: e.dma_start(out=strm0, in_=xT.rearrange("(k p) t -> p k t", p=128)),
                  writes=[("f1", "sin")])
            S.barrier()
            self.ffn_phase("f1", NT, TBS, strm0, 66560, wg1, wu1, wd1, xT, cv[:, 0:16], cv[:, 16:32],
                           resid_out=(dbg if stage == 1 else h1s))
            strm1 = self.view(66560, BF16, [KC, NT])
            if stage >= 2:
                self.hgrn_m1(strm1)
                self.hgrn_m2(strm1)
            if stage == 3:
                yhg = self.view(32768, BF16, [8 * NREAL])
                S.dma("pool", lambda e: e.dma_start(out=dbg, in_=yhg), reads=[])
            if stage >= 4:
                self.attn_m3(strm1)
            if stage == 4:
                yat = self.view(0, BF16, [8 * NREAL])
                S.dma("pool", lambda e: e.dma_start(out=dbg, in_=yat), reads=[])
            if stage >= 5:
                if stage == 5:
                    self.h2s = dbg
                self.merge_m4(strm1, cv[:, 32:48], cv[:, 48:64])
            if stage >= 6:
                strm2 = self.view(0, BF16, [KC, NREAL])
                self.ffn_phase("f2", NREAL, TBS[0:2], strm2, 66560, wg2, wu2, wd2, self.h2s, cv[:, 64:80],
                               cv[:, 80:96], final_out=outT)
            S.emit(block, esems, dsems)
        return nc


def _lay_gu(w):
    return np.ascontiguousarray(w.reshape(KC, 128, FC // 2, 256).transpose(2, 1, 0, 3))


def _lay_d(w):
    return np.ascontiguousarray(w.reshape(FC, 128, KC, 128).transpose(2, 1, 0, 3))


def _fm(v):
    return np.ascontiguousarray(v.reshape(KC, 128).T)


def _core_tokens(x, meta, c):
    b, j = c // 4, c % 4
    blocks = [x[b, 128 * (4 * i + j):128 * (4 * i + j) + 128] for i in range(8)]
    tok = np.concatenate(blocks + [meta], axis=0)
    return np.ascontiguousarray(tok.T)


def _mk_groups():
    g = []
    for hp in range(4):
        g += [("hf%d" % hp, 3664 + 256 * hp, 256), ("hq%d" % hp, 2640 + 256 * hp, 256),
              ("hi%d" % hp, 4688 + 256 * hp, 256)]
    for i in range(4):
        g.append(("hg%d" % i, 5712 + 256 * i, 256))
    g += [("ak", 1024, 256), ("av", 1280, 256), ("ikw", 2560, 80)]
    for i in range(4):
        g.append(("aq%d" % i, 256 * i, 256))
    for i in range(4):
        g.append(("iq%d" % i, 1536 + 256 * i, 256))
    for i in range(8):
        g.append(("ga%d" % i, 6736 + 256 * i, 256))
        g.append(("gh%d" % i, 8784 + 256 * i, 256))
    return g


GROUPS = _mk_groups()


def _lay_win(w):
    out = np.zeros((len(GROUPS), 128, KC, 256), np.float32)
    for gi, (nm, c0, nc_) in enumerate(GROUPS):
        out[gi, :, :, 0:nc_] = w[:, c0:c0 + nc_].reshape(KC, 128, nc_).transpose(1, 0, 2)
    return out


def prepare(inputs, stage):
    f = lambda k: np.asarray(inputs[k], dtype=np.float32)
    x, meta = f("x"), f("meta")
    shared = {
        "wg1": _lay_gu(f("ffn1_w_gate")[0]), "wu1": _lay_gu(f("ffn1_w_up")[0]),
        "wd1": _lay_d(f("ffn1_w_down")[0]),
        "win": _lay_win(f("w_in")[0]),
        "wg2": _lay_gu(f("ffn2_w_gate")[0]), "wu2": _lay_gu(f("ffn2_w_up")[0]),
        "wd2": _lay_d(f("ffn2_w_down")[0]),
        "wba": np.ascontiguousarray(f("w_branch_att")[0].reshape(8, 128, 8, 256).transpose(2, 1, 0, 3)),
        "wbh": np.ascontiguousarray(f("w_branch_hg")[0].reshape(8, 128, 8, 256).transpose(2, 1, 0, 3)),
        "wo": np.ascontiguousarray(f("w_out")[0].reshape(KC, 128, KC, 128).transpose(2, 1, 0, 3)),
    }
    slopes = (2.0 ** -(np.arange(8) + 1.0)).astype(np.float32)
    c = np.arange(4096)
    kpos = np.concatenate([16 + 128 * (4 * ((c % 1024) // 128) + c // 1024) + c % 128, np.arange(16)]).astype(np.float32)
    augk = np.stack([np.floor(kpos / 64.0), kpos % 64.0, np.ones_like(kpos)], 0).astype(np.float32)
    augs = np.stack([np.repeat(64.0 * slopes, 128), np.repeat(slopes, 128)], 0).astype(np.float32)
    shared["augk"] = augk
    shared["augs"] = augs
    cvec = np.zeros((128, 128), np.float32)
    cvec[:, 0:16] = _fm(f("ln1_g")[0]); cvec[:, 16:32] = _fm(f("ln1_b")[0])
    cvec[:, 32:48] = _fm(f("ln2_g")[0]); cvec[:, 48:64] = _fm(f("ln2_b")[0])
    cvec[:, 64:80] = _fm(f("ln3_g")[0]); cvec[:, 80:96] = _fm(f("ln3_b")[0])
    lbl = f("hg_lb_logits")
    cvec[:, 96:104] = lbl[0].reshape(8, 128).T
    cvec[:, 104:112] = lbl[1].reshape(8, 128).T
    cvec[:, 112:120] = f("hg_norm_g")[0].T
    cmat = np.zeros((128, 384), np.float32)
    cmat[:, 0:128] = np.eye(128, dtype=np.float32)
    cmat[:, 128:256] = 1.0
    sidx = np.arange(128)[:, None]; tidx = np.arange(128)[None, :]
    cmat[:, 256:384] = (((sidx <= tidx) & ((sidx // 64) == (tidx // 64))) | ((sidx < 64) & (tidx >= 64))).astype(np.float32)
    shared["cmat"] = cmat
    maps = []
    for c in range(8):
        m = dict(shared)
        m["xT"] = _core_tokens(x, meta, c)
        cv = cvec.copy()
        cv[:, 120 + (c % 4)] = 1.0
        m["cvec"] = cv
        j = c % 4
        p = np.arange(128, dtype=np.float32)
        qpos = np.stack([16 + 128 * (4 * i + j) + p for i in range(8)], 0)
        m["augq"] = np.ascontiguousarray((-slopes[None, :, None] * qpos[:, None, :]).reshape(8, 1024).astype(np.float32))
        catt = np.zeros((128, 224), np.float32)
        catt[:, 0:64] = (-qpos.T[:, :, None] * slopes[None, None, :]).reshape(128, 64)
        catt[:, 64:96] = (2.0 ** -(np.arange(32) + 1.0))[None, :]
        catt[:, 96:160] = f("idx_k_norm_g")[0][None, :]
        catt[:, 160:224] = f("idx_k_norm_b")[0][None, :]
        m["catt"] = catt
        tt = np.arange(128)[:, None, None]; rr = np.arange(4)[None, :, None]; ss = np.arange(128)[None, None, :]
        m["cbt"] = np.where(128 * (rr - j) + (ss - tt) > 0, -1e30, 0.0).astype(np.float32).reshape(128, 512)
        maps.append(m)
    return maps


_NC_CACHE = {}


def kernel(**inputs):
    stage = int(os.environ.get("KSTAGE", "9"))
    if stage not in _NC_CACHE:
        _NC_CACHE[stage] = Builder(stage).build()
    nc = _NC_CACHE[stage]
    maps = prepare(inputs, stage)
    res = run_bass_kernel_spmd(nc, maps, core_ids=list(range(8)))
    if stage == 4:
        return [(r["dbg"], r["dbg2"]) for r in res.results]
    if stage < 6:
        return [r["dbg"] for r in res.results]
    out = np.zeros((2, 4096, D), np.float32)
    for c in range(8):
        b, j = c // 4, c % 4
        o = res.results[c]["outT"]
        for i in range(8):
            g = 4 * i + j
            out[b, 128 * g:128 * g + 128] = o[:, 128 * i:128 * i + 128].T
    return out
```

```python
import os
import numpy as np
import concourse.bass as bass
import concourse.mybir as mybir
from concourse.bass_utils import run_bass_kernel_spmd

F32 = mybir.dt.float32
BF16 = mybir.dt.bfloat16
AF = mybir.ActivationFunctionType
ALU = mybir.AluOpType
AX = mybir.AxisListType

D = 2048
DFF = 5632
NMETA = 16
NREAL = 1024
NT = NREAL + NMETA
KC = D // 128
FC = DFF // 128
TBS = [(0, 512), (512, 1024), (1024, 1040)]
ALPHA = 2.0 ** 0.25
LN_EPS = 1e-5
RMS_EPS = 1e-6

ENGS = ("pe", "act", "dve", "pool", "sp")


class Op:
    __slots__ = ("eng", "fn", "deps", "is_dma", "dsem", "dval", "sig", "sigidx", "waits")

    def __init__(self, eng, fn, is_dma=False):
        self.eng = eng
        self.fn = fn
        self.deps = []
        self.is_dma = is_dma
        self.dsem = None
        self.dval = 0
        self.sig = False
        self.sigidx = 0
        self.waits = []


class Sched:
    def __init__(self, n_dma_sems=40, same_engine_sync=True):
        self.ops = {e: [] for e in ENGS}
        self.lastw = {}
        self.readers = {}
        self.n_dma = 0
        self.n_dma_sems = n_dma_sems
        self.dma_hist = {}
        self.same_engine_sync = same_engine_sync
        self.n_coll = 0

    def _add(self, op, reads, writes):
        deps = set()
        for k in reads:
            w = self.lastw.get(k)
            if w is not None:
                deps.add(w)
        for k in writes:
            w = self.lastw.get(k)
            if w is not None:
                deps.add(w)
            for r in self.readers.get(k, ()):
                deps.add(r)
        op.deps = list(deps)
        for k in reads:
            self.readers.setdefault(k, []).append(op)
        for k in writes:
            self.lastw[k] = op
            self.readers[k] = []
        self.ops[op.eng].append(op)
        return op

    def op(self, eng, fn, reads=(), writes=()):
        return self._add(Op(eng, fn), reads, writes)

    def dma(self, q, fn, reads=(), writes=()):
        op = Op(q, fn, is_dma=True)
        slot = self.n_dma % self.n_dma_sems
        op.dsem = slot
        op.dval = 16 * (self.n_dma // self.n_dma_sems + 1)
        self.n_dma += 1
        self._add(op, reads, writes)
        prev = self.dma_hist.get(slot)
        if prev is not None:
            op.deps.append(prev)
        self.dma_hist[slot] = op
        return op

    def coll(self, fn, reads=(), writes=()):
        op = Op("pool", fn, is_dma=True)
        op.dsem = self.n_dma_sems + self.n_coll
        op.dval = 1
        self.n_coll += 1
        self._add(op, reads, writes)
        self.dma_hist[op.dsem] = op
        return op

    def barrier(self):
        lasts = []
        for e in ENGS:
            for o in reversed(self.ops[e]):
                if not o.is_dma and o.fn is not None:
                    lasts.append(o)
                    break
        dmas = list(self.dma_hist.values())
        for e in ENGS:
            op = Op(e, None)
            op.deps = [o for o in lasts if o.eng != e] + dmas
            self.ops[e].append(op)
        self.lastw = {}
        self.readers = {}

    def _skip(self, d, op):
        return d.eng == op.eng and (d.eng in ("pe", "sp") or not self.same_engine_sync)

    def finalize(self):
        for e in ENGS:
            for op in self.ops[e]:
                for d in op.deps:
                    if not d.is_dma and not self._skip(d, op):
                        d.sig = True
        for e in ENGS:
            c = 0
            for op in self.ops[e]:
                if op.sig:
                    c += 1
                    op.sigidx = c
        for e in ENGS:
            waited = {}
            for op in self.ops[e]:
                need = {}
                for d in op.deps:
                    if d.is_dma:
                        key, val = ("d", d.dsem), d.dval
                    else:
                        if self._skip(d, op):
                            continue
                        key, val = ("e", d.eng), d.sigidx
                    if waited.get(key, 0) >= val:
                        continue
                    if need.get(key, 0) < val:
                        need[key] = val
                for k, v in need.items():
                    waited[k] = v
                op.waits = list(need.items())

    def emit(self, block, esems, dsems):
        self.finalize()
        regs = {"pe": block.tensor, "act": block.scalar, "dve": block.vector,
                "pool": block.gpsimd, "sp": block.sync}
        final = {d.dsem: d.dval for d in self.dma_hist.values()}

        def make(e):
            ops = self.ops[e]

            def body(eng):
                for op in ops:
                    for (kind, which), val in op.waits:
                        eng.wait_ge(dsems[which] if kind == "d" else esems[which], val)
                    if op.fn is None:
                        continue
                    ins = op.fn(eng)
                    if op.is_dma:
                        ins.then_inc(dsems[op.dsem], 16 if op.dsem < self.n_dma_sems else 1)
                    elif op.sig:
                        ins.then_inc(esems[e], 1)
                if e == "sp":
                    for slot, val in final.items():
                        eng.wait_ge(dsems[slot], val)
            return body

        for e in ENGS:
            regs[e](make(e))


ARENA_F32 = 50816
CONST_F32 = 2304


class Builder:
    def __init__(self, stage):
        self.stage = stage
        self.nc = bass.Bass("TRN2", target_bir_lowering=False)
        self.S = Sched()
        self.dram = {}

    def din(self, name, shape, dt=F32):
        self.dram[name] = self.nc.dram_tensor(name, list(shape), dt, kind="ExternalInput").ap()
        return self.dram[name]

    def dout(self, name, shape, dt=F32):
        self.dram[name] = self.nc.dram_tensor(name, list(shape), dt, kind="ExternalOutput").ap()
        return self.dram[name]

    def dscratch(self, name, shape, dt=F32):
        self.dram[name] = self.nc.dram_tensor(name, list(shape), dt, kind="Internal").ap()
        return self.dram[name]

    def view(self, off_bytes, dt, shape):
        n = 1
        for s in shape:
            n *= s
        esz = 4 if dt == F32 else 2
        assert off_bytes % 4 == 0
        nbytes = n * esz
        assert nbytes % 4 == 0
        assert off_bytes + nbytes <= ARENA_F32 * 4, (off_bytes, nbytes)
        v = self.arena[:, off_bytes // 4:(off_bytes + nbytes) // 4]
        if dt != F32:
            v = v.bitcast(dt)
        if len(shape) == 2:
            v = v.rearrange("p (a b) -> p a b", b=shape[1])
        elif len(shape) == 3:
            v = v.rearrange("p (a b c) -> p a b c", b=shape[1], c=shape[2])
        return v

    def ln_apply(self, tag, z, tbs, cg, cb, toff, eps_eff, strm_out=None, alias_key=None, resid_out=None,
                 final_out=None):
        S, ps = self.S, self.ps
        zsq = [self.view(toff + i * 2048, F32, [512]) for i in range(2)]
        meanb = self.view(toff + 4096, F32, [512])
        rstdb = self.view(toff + 6144, F32, [512])
        t1 = [self.view(toff + 8192 + i * 2048, F32, [512]) for i in range(2)]
        t2 = [self.view(toff + 12288 + i * 2048, F32, [512]) for i in range(2)]
        o32 = [self.view(toff + 16384 + i * 2048, F32, [512]) for i in range(2)]
        ones = self.ones_f32
        for ti, (t0, t1e) in enumerate(tbs):
            n = t1e - t0
            pm, pq = ps[:, 6, 0:n], ps[:, 7, 0:n]
            for dc in range(KC):
                zq = zsq[dc % 2]
                S.op("act", lambda e, zq=zq, dc=dc, t0=t0, t1e=t1e, n=n: e.activation(
                    out=zq[:, 0:n], in_=z[:, dc, t0:t1e], func=AF.Square),
                    reads=[(tag, "z", dc, ti)], writes=[(tag, "zsq", dc % 2)])
                S.op("pe", lambda e, pm=pm, dc=dc, t0=t0, t1e=t1e: e.matmul(
                    pm, lhsT=ones, rhs=z[:, dc, t0:t1e], start=(dc == 0), stop=(dc == KC - 1)),
                    reads=[(tag, "z", dc, ti)], writes=[("ps", 6)])
                S.op("pe", lambda e, pq=pq, zq=zq, dc=dc, n=n: e.matmul(
                    pq, lhsT=ones, rhs=zq[:, 0:n], start=(dc == 0), stop=(dc == KC - 1)),
                    reads=[(tag, "zsq", dc % 2)], writes=[("ps", 7)])
            S.op("act", lambda e, pm=pm, n=n: e.activation(out=meanb[:, 0:n], in_=pm, func=AF.Copy),
                 reads=[("ps", 6)], writes=[(tag, "meanb")])
            S.op("dve", lambda e, n=n: e.tensor_tensor(out=rstdb[:, 0:n], in0=meanb[:, 0:n], in1=meanb[:, 0:n],
                                                       op=ALU.mult),
                 reads=[(tag, "meanb")], writes=[(tag, "rstdb")])
            S.op("dve", lambda e, pq=pq, n=n: e.tensor_tensor(out=rstdb[:, 0:n], in0=pq, in1=rstdb[:, 0:n],
                                                              op=ALU.subtract),
                 reads=[("ps", 7), (tag, "rstdb")], writes=[(tag, "rstdb")])
            S.op("act", lambda e, n=n: e.activation(out=rstdb[:, 0:n], in_=rstdb[:, 0:n], func=AF.Sqrt,
                                                    bias=self.eps_cols[eps_eff], scale=1.0),
                 reads=[(tag, "rstdb")], writes=[(tag, "rstdb")])
            S.op("dve", lambda e, n=n: e.reciprocal(out=rstdb[:, 0:n], in_=rstdb[:, 0:n]),
                 reads=[(tag, "rstdb")], writes=[(tag, "rstdb")])
            for dc in range(KC):
                a, b_, o = t1[dc % 2], t2[dc % 2], o32[dc % 2]
                S.op("pool", lambda e, a=a, dc=dc, t0=t0, t1e=t1e, n=n: e.tensor_tensor(
                    out=a[:, 0:n], in0=z[:, dc, t0:t1e], in1=meanb[:, 0:n], op=ALU.subtract),
                    reads=[(tag, "z", dc, ti), (tag, "meanb")], writes=[(tag, "t1", dc % 2)])
                S.op("dve", lambda e, a=a, b_=b_, n=n: e.tensor_tensor(
                    out=b_[:, 0:n], in0=a[:, 0:n], in1=rstdb[:, 0:n], op=ALU.mult),
                    reads=[(tag, "t1", dc % 2), (tag, "rstdb")], writes=[(tag, "t2", dc % 2)])
                S.op("act", lambda e, b_=b_, o=o, dc=dc, n=n: e.activation(
                    out=o[:, 0:n], in_=b_[:, 0:n], func=AF.Identity,
                    bias=cb[:, dc:dc + 1], scale=cg[:, dc:dc + 1]),
                    reads=[(tag, "t2", dc % 2)], writes=[(tag, "o32", dc % 2)])
                if strm_out is not None:
                    wk = [(tag, "sout", dc, ti)] + ([(tag, alias_key, dc, ti)] if alias_key else [])
                    S.op("act", lambda e, b_=b_, dc=dc, t0=t0, t1e=t1e, n=n: e.activation(
                        out=strm_out[:, dc, t0:t1e], in_=b_[:, 0:n], func=AF.Identity,
                        bias=cb[:, dc:dc + 1], scale=cg[:, dc:dc + 1]),
                        reads=[(tag, "t2", dc % 2)], writes=wk)
                dst = resid_out if final_out is None else final_out
                if final_out is None or t0 < NREAL:
                    S.dma("sp", lambda e, o=o, dc=dc, t0=t0, t1e=t1e, n=n, dst=dst: e.dma_start(
                        out=dst[dc * 128:(dc + 1) * 128, t0:t1e], in_=o[:, 0:n]),
                        reads=[(tag, "o32", dc % 2)])

    def ffn_phase(self, tag, ntok, tbs, strm_in, strm_out_off, wg, wu, wd, resid, cg, cb,
                  resid_out=None, final_out=None):
        S, nc = self.S, self.nc
        ps = self.ps
        C_OFF = 33280
        B_OFF = 66560
        D_OFF = B_OFF + FC * NT * 2
        T_OFF = D_OFF + 2 * FC * 128 * 2
        hT = self.view(B_OFF, BF16, [FC, ntok])
        wgu = [self.view(C_OFF + i * 16384, BF16, [2, KC, 256]) for i in range(2)]
        wdb = [self.view(D_OFF + i * FC * 128 * 2, BF16, [FC, 128]) for i in range(2)]
        z = self.view(0, F32, [KC, ntok])
        sil = [self.view(T_OFF + 8192 + i * 2048, F32, [512]) for i in range(2)]
        strm_out = self.view(strm_out_off, BF16, [KC, ntok]) if final_out is None else None
        c_scale = 0.5 / ALPHA
        eps_eff = LN_EPS / (ALPHA * ALPHA)
        nb = len(tbs)

        for g in range(FC // 2):
            wb = wgu[g % 2]
            kb = (tag, "wgu", g % 2)
            S.dma("pool", lambda e, wb=wb, g=g: e.dma_start(out=wb[:, 0], in_=wg[g]), writes=[(kb, 0)])
            S.dma("pool", lambda e, wb=wb, g=g: e.dma_start(out=wb[:, 1], in_=wu[g]), writes=[(kb, 1)])
            for fcl in range(2):
                fc = 2 * g + fcl
                for ti, (t0, t1e) in enumerate(tbs):
                    n = t1e - t0
                    pb = (fc * nb + ti) % 2
                    pg, pu = ps[:, 2 * pb, 0:n], ps[:, 2 * pb + 1, 0:n]
                    for k in range(KC):
                        S.op("pe", lambda e, pg=pg, wb=wb, k=k, fcl=fcl, t0=t0, t1e=t1e: e.matmul(
                            pg, lhsT=wb[:, 0, k, fcl * 128:(fcl + 1) * 128], rhs=strm_in[:, k, t0:t1e],
                            start=(k == 0), stop=(k == KC - 1)),
                            reads=[(kb, 0), (tag, "sin")], writes=[("ps", 2 * pb)])
                    for k in range(KC):
                        S.op("pe", lambda e, pu=pu, wb=wb, k=k, fcl=fcl, t0=t0, t1e=t1e: e.matmul(
                            pu, lhsT=wb[:, 1, k, fcl * 128:(fcl + 1) * 128], rhs=strm_in[:, k, t0:t1e],
                            start=(k == 0), stop=(k == KC - 1)),
                            reads=[(kb, 1), (tag, "sin")], writes=[("ps", 2 * pb + 1)])
                    sb = sil[pb]
                    S.op("act", lambda e, sb=sb, pg=pg, n=n: e.activation(out=sb[:, 0:n], in_=pg, func=AF.Silu),
                         reads=[("ps", 2 * pb)], writes=[(tag, "sil", pb)])
                    S.op("dve", lambda e, sb=sb, pu=pu, n=n, fc=fc, t0=t0, t1e=t1e: e.tensor_tensor(
                        out=hT[:, fc, t0:t1e], in0=sb[:, 0:n], in1=pu, op=ALU.mult),
                        reads=[(tag, "sil", pb), ("ps", 2 * pb + 1)], writes=[(tag, "hT", fc, ti)])

        S.barrier()
        for dc in range(KC):
            wb = wdb[dc % 2]
            kb = (tag, "wd", dc % 2)
            S.dma("pool", lambda e, wb=wb, dc=dc: e.dma_start(out=wb, in_=wd[dc]), writes=[kb])
            S.dma("sp", lambda e, dc=dc: e.dma_start(out=z[:, dc, :], in_=resid[dc * 128:(dc + 1) * 128, 0:ntok]),
                  writes=[(tag, "z", dc, ti) for ti in range(nb)])
            for ti, (t0, t1e) in enumerate(tbs):
                n = t1e - t0
                pb = 4 + (dc * nb + ti) % 2
                py = ps[:, pb, 0:n]
                for f in range(FC):
                    S.op("pe", lambda e, py=py, wb=wb, f=f, t0=t0, t1e=t1e: e.matmul(
                        py, lhsT=wb[:, f, :], rhs=hT[:, f, t0:t1e], start=(f == 0), stop=(f == FC - 1)),
                        reads=[kb, (tag, "hT", f, ti)], writes=[("ps", pb)])
                S.op("dve", lambda e, py=py, dc=dc, t0=t0, t1e=t1e: e.scalar_tensor_tensor(
                    out=z[:, dc, t0:t1e], in0=py, scalar=c_scale, in1=z[:, dc, t0:t1e],
                    op0=ALU.mult, op1=ALU.add),
                    reads=[("ps", pb), (tag, "z", dc, ti)], writes=[(tag, "z", dc, ti)])
        self.ln_apply(tag, z, tbs, cg, cb, T_OFF, eps_eff, strm_out=strm_out, alias_key="hT",
                      resid_out=resid_out, final_out=final_out)
        S.barrier()

    def proj_fm(self, tag, strm, gi, wbufs, tbs, evac, banks=(0, 1), parity=[0]):
        S, ps = self.S, self.ps
        wb = wbufs[parity[0] % 2]
        kb = ("wb", parity[0] % 2)
        parity[0] += 1
        S.dma("pool", lambda e, wb=wb, gi=gi: e.dma_start(out=wb, in_=self.win[gi]), writes=[kb])
        cnt = 0
        for half in range(2):
            for ti, (t0, t1e) in enumerate(tbs):
                n = t1e - t0
                bk = banks[cnt % len(banks)]
                cnt += 1
                pv = ps[:, bk, 0:n]
                for k in range(KC):
                    S.op("pe", lambda e, pv=pv, wb=wb, k=k, half=half, t0=t0, t1e=t1e: e.matmul(
                        pv, lhsT=wb[:, k, half * 128:(half + 1) * 128], rhs=strm[:, k, t0:t1e],
                        start=(k == 0), stop=(k == KC - 1)),
                        reads=[kb, "strm"], writes=[("ps", bk)])
                evac(half, ti, t0, t1e, pv, bk)

    def proj_tm(self, tag, strm, gi, wbufs, tls, ncols, evac, banks=(0, 1), parity=[0]):
        S, ps = self.S, self.ps
        wb = wbufs[parity[0] % 2]
        kb = ("wb", parity[0] % 2)
        parity[0] += 1
        S.dma("pool", lambda e, wb=wb, gi=gi: e.dma_start(out=wb, in_=self.win[gi]), writes=[kb])
        for cnt, (i, c0, n) in enumerate(tls):
            bk = banks[cnt % len(banks)]
            pv = ps[0:n, bk, 0:ncols]
            for k in range(KC):
                S.op("pe", lambda e, pv=pv, wb=wb, k=k, c0=c0, n=n: e.matmul(
                    pv, lhsT=strm[:, k, c0:c0 + n], rhs=wb[:, k, 0:ncols],
                    start=(k == 0), stop=(k == KC - 1)),
                    reads=[kb, "strm"], writes=[("ps", bk)])
            evac(i, c0, n, pv, bk)

    def hgrn_m1(self, strm):
        S, ps, nc = self.S, self.ps, self.nc
        R1 = 99840
        wbufs = [self.view(R1 + i * 8192, BF16, [KC, 256]) for i in range(2)]
        off = [R1 + 16384]

        def alloc(dt, shape):
            n = 1
            for x in shape:
                n *= x
            nb = n * (4 if dt == F32 else 2)
            nb = (nb + 63) // 64 * 64
            v = self.view(off[0], dt, shape)
            off[0] += nb
            return v
        logf = alloc(F32, [2, NT]); Bg = alloc(F32, [2, NT]); Bsh = alloc(F32, [2, NT])
        tA = alloc(F32, [2, NT]); tB = alloc(F32, [2, NT])
        kk = alloc(BF16, [2, NT]); qs = alloc(BF16, [2, NT]); qt = alloc(BF16, [2, NT])
        kt = alloc(BF16, [2, NT]); kh64 = alloc(BF16, [2, NT]); kh128 = alloc(BF16, [2, NT])
        vv = alloc(BF16, [9, 256])
        PT = alloc(BF16, [2, 128]); khT = alloc(BF16, [2, 128])
        xs_st = alloc(BF16, [9, 256]); xa_st = alloc(F32, [9, 2])
        Qc = self.view(0, BF16, [8, NREAL]); oloc = self.view(16384, BF16, [8, NREAL])
        cv = self.cv
        lbc, omlc = self.lbc, self.omlc
        psb = lambda bk: ps[:, bk, :].bitcast(BF16)
        TL = [(i, 128 * i, 128) for i in range(8)] + [(8, NREAL, NMETA)]
        flat = lambda v: v.rearrange("p h t -> p (h t)")
        r64 = lambda v: v[:, :, 0:NREAL].rearrange("p h (c t) -> p h c t", t=64)
        r128 = lambda v: v[:, :, 0:NREAL].rearrange("p h (c t) -> p h c t", t=128)
        mt = lambda v: v[:, :, NREAL:NT]

        for hp in range(4):
            T = ("m1", hp)
            def ev_f(half, ti, t0, t1e, pv, bk, hp=hp):
                h = 2 * hp + half
                S.op("act", lambda e: e.activation(out=tA[:, half, t0:t1e], in_=pv, func=AF.Sigmoid),
                     reads=[("ps", bk)], writes=[("tA", half, ti)])
                S.op("dve", lambda e: e.tensor_scalar(out=tA[:, half, t0:t1e], in0=tA[:, half, t0:t1e],
                                                      scalar1=omlc[:, h:h + 1], scalar2=lbc[:, h:h + 1],
                                                      op0=ALU.mult, op1=ALU.add),
                     reads=[("tA", half, ti), "lb"], writes=[("tA", half, ti)])
                S.op("act", lambda e: e.activation(out=logf[:, half, t0:t1e], in_=tA[:, half, t0:t1e], func=AF.Ln),
                     reads=[("tA", half, ti)], writes=[("logf", half, ti)])
                S.op("pool", lambda e: e.tensor_scalar(out=kk[:, half, t0:t1e], in0=tA[:, half, t0:t1e],
                                                       scalar1=-1.0, scalar2=1.0, op0=ALU.mult, op1=ALU.add),
                     reads=[("tA", half, ti)], writes=[("kk", half, ti)])
            self.proj_fm(T, strm, self.gidx["hf%d" % hp], wbufs, TBS, ev_f)

            def ev_q(half, ti, t0, t1e, pv, bk):
                S.op("act", lambda e: e.activation(out=qs[:, half, t0:t1e], in_=pv, func=AF.Silu),
                     reads=[("ps", bk)], writes=[("qs", half, ti)])
            self.proj_fm(T, strm, self.gidx["hq%d" % hp], wbufs, TBS, ev_q)

            def ev_v(i, c0, n, pv, bk):
                S.op("act", lambda e: e.activation(out=vv[0:n, i, :], in_=pv, func=AF.Copy),
                     reads=[("ps", bk)], writes=[("vv", i)])
            self.proj_tm(T, strm, self.gidx["hi%d" % hp], wbufs, TL, 256, ev_v)

            allk = lambda nm: [(nm, hl, ti) for hl in range(2) for ti in range(3)]
            S.op("dve", lambda e: e.tensor_tensor_scan(out=flat(Bg), data0=flat(logf), data1=flat(logf),
                                                       initial=0.0, op0=ALU.add, op1=ALU.min),
                 reads=allk("logf"), writes=["Bg"])
            S.op("pool", lambda e: e.memset(flat(Bsh)[:, 0:1], 0.0), writes=["Bsh0"])
            S.op("pool", lambda e: e.tensor_copy(out=flat(Bsh)[:, 1:2 * NT], in_=flat(Bg)[:, 0:2 * NT - 1]),
                 reads=["Bg"], writes=["Bsh"])
            S.op("dve", lambda e: e.tensor_tensor(out=r64(tB), in0=r64(Bg),
                                                  in1=r64(Bsh)[:, :, :, 0:1].to_broadcast([128, 2, 16, 64]),
                                                  op=ALU.subtract),
                 reads=["Bg", "Bsh", "Bsh0"], writes=["tBr"])
            S.op("dve", lambda e: e.tensor_tensor(out=mt(tB), in0=mt(Bg),
                                                  in1=mt(Bsh)[:, :, 0:1].to_broadcast([128, 2, NMETA]),
                                                  op=ALU.subtract),
                 reads=["Bg", "Bsh", "Bsh0"], writes=["tBm"])
            S.op("pool", lambda e: e.tensor_tensor(out=r128(tA), in0=r128(Bg),
                                                   in1=r128(Bsh)[:, :, :, 0:1].to_broadcast([128, 2, 8, 128]),
                                                   op=ALU.subtract),
                 reads=["Bg", "Bsh", "Bsh0"] + allk("tA"), writes=["tAr"] + allk("tA"))
            S.op("pool", lambda e: e.tensor_copy(out=mt(tA), in_=mt(tB)),
                 reads=["tBm"], writes=["tAm"])
            TBk, TAk = ["tBr", "tBm"], ["tAr", "tAm"] + allk("tA")
            S.op("act", lambda e: e.activation(out=flat(Bg), in_=flat(tB), func=AF.Exp),
                 reads=TBk + ["Bsh", "tAr"], writes=["Bg"])
            S.op("dve", lambda e: e.tensor_tensor(out=flat(qt), in0=flat(qs), in1=flat(Bg), op=ALU.mult),
                 reads=["Bg"] + allk("qs"), writes=["qt"])
            S.op("act", lambda e: e.activation(out=flat(Bsh), in_=flat(tB), func=AF.Exp, scale=-1.0),
                 reads=TBk + ["Bsh", "tAr", "Bsh0"], writes=["Bsh", "Bsh0"])
            S.op("dve", lambda e: e.tensor_tensor(out=flat(kt), in0=flat(kk), in1=flat(Bsh), op=ALU.mult),
                 reads=["Bsh"] + allk("kk"), writes=["kt"])
            S.op("dve", lambda e: e.tensor_tensor(out=r64(Bg), in0=r64(tB),
                                                  in1=r64(tB)[:, :, :, 63:64].to_broadcast([128, 2, 16, 64]),
                                                  op=ALU.subtract),
                 reads=TBk + ["qt"], writes=["Bg"])
            S.op("act", lambda e: e.activation(out=r64(Bg), in_=r64(Bg), func=AF.Exp, scale=-1.0),
                 reads=["Bg"], writes=["Bg"])
            S.op("dve", lambda e: e.tensor_tensor(out=r64(kh64), in0=r64(kk), in1=r64(Bg), op=ALU.mult),
                 reads=["Bg"] + allk("kk"), writes=["kh64"])
            S.op("act", lambda e: e.activation(out=flat(Bsh), in_=flat(tA), func=AF.Exp),
                 reads=TAk + ["kt"], writes=["Bsh"])
            S.op("dve", lambda e, hp=hp: e.tensor_tensor(out=Qc[:, 2 * hp:2 * hp + 2, :], in0=qs[:, :, 0:NREAL],
                                                         in1=Bsh[:, :, 0:NREAL], op=ALU.mult),
                 reads=["Bsh"] + allk("qs"), writes=[("Qc", hp)])
            S.op("pool", lambda e: e.tensor_copy(out=xa_st[:, 0:8, :].rearrange("p i h -> p h i"),
                                                 in_=r128(Bsh)[:, :, :, 127]),
                 reads=["Bsh"], writes=["xa_st"])
            S.op("pool", lambda e: e.tensor_copy(out=xa_st[:, 8, :], in_=Bsh[:, :, NT - 1]),
                 reads=["Bsh"], writes=["xa_st"])
            S.op("dve", lambda e: e.tensor_tensor(out=r128(Bg), in0=r128(tA),
                                                  in1=r128(tA)[:, :, :, 127:128].to_broadcast([128, 2, 8, 128]),
                                                  op=ALU.subtract),
                 reads=TAk + ["kh64"], writes=["Bg"])
            S.op("dve", lambda e: e.tensor_tensor(out=mt(Bg), in0=mt(tA),
                                                  in1=mt(tA)[:, :, NMETA - 1:NMETA].to_broadcast([128, 2, NMETA]),
                                                  op=ALU.subtract),
                 reads=TAk + ["kh64"], writes=["Bg"])
            S.op("act", lambda e: e.activation(out=flat(Bg), in_=flat(Bg), func=AF.Exp, scale=-1.0),
                 reads=["Bg"], writes=["Bg"])
            S.op("dve", lambda e: e.tensor_tensor(out=flat(kh128), in0=flat(kk), in1=flat(Bg), op=ALU.mult),
                 reads=["Bg"] + allk("kk"), writes=["kh128"])

            for (i, c0, n) in TL:
                if n == 128:
                    for hl in range(2):
                        o0 = hl * 128
                        S.op("pe", lambda e, hl=hl, o0=o0, c0=c0: e.matmul(
                            ps[:, 2, o0:o0 + 64], lhsT=kt[:, hl, c0:c0 + 128], rhs=qt[:, hl, c0:c0 + 64],
                            start=True, stop=True), reads=["kt", "qt"], writes=[("ps", 2)])
                        S.op("pe", lambda e, hl=hl, o0=o0, c0=c0: e.matmul(
                            ps[0:64, 2, o0 + 64:o0 + 128], lhsT=kh64[:, hl, c0:c0 + 64],
                            rhs=qt[:, hl, c0 + 64:c0 + 128], start=True, stop=True),
                            reads=["kh64", "qt"], writes=[("ps", 2)])
                        S.op("pe", lambda e, hl=hl, o0=o0, c0=c0: e.matmul(
                            ps[64:128, 2, o0 + 64:o0 + 128], lhsT=kt[:, hl, c0 + 64:c0 + 128],
                            rhs=qt[:, hl, c0 + 64:c0 + 128], start=True, stop=True),
                            reads=["kt", "qt"], writes=[("ps", 2)])
                    for hl in range(2):
                        S.op("dve", lambda e, hl=hl: e.tensor_tensor(
                            out=PT[:, hl, :], in0=ps[:, 2, hl * 128:(hl + 1) * 128], in1=self.mask2, op=ALU.mult),
                            reads=[("ps", 2), "mask2"], writes=[("PT", hl)])
                    for hl in range(2):
                        S.op("pe", lambda e, hl=hl, i=i: e.matmul(
                            ps[:, 3, hl * 128:(hl + 1) * 128], lhsT=vv[:, i, hl * 128:(hl + 1) * 128],
                            rhs=PT[:, hl, :], start=True, stop=True),
                            reads=[("vv", i), ("PT", hl)], writes=[("ps", 3)])
                    S.op("act", lambda e, hp=hp, c0=c0: e.activation(
                        out=oloc[:, 2 * hp:2 * hp + 2, c0:c0 + 128],
                        in_=ps[:, 3, 0:256].rearrange("p (h t) -> p h t", h=2), func=AF.Copy),
                        reads=[("ps", 3)], writes=[("oloc", hp, i)])
                for hl in range(2):
                    S.op("pe", lambda e, hl=hl, c0=c0, n=n: e.transpose(
                        out=psb(4)[0:n, hl * 128:(hl + 1) * 128], in_=kh128[:, hl, c0:c0 + n],
                        identity=self.ident_bf),
                        reads=["kh128", "ident"], writes=[("ps", 4)])
                S.op("dve", lambda e, n=n: e.tensor_copy(out=khT[0:n].rearrange("p h d -> p (h d)"),
                                                         in_=psb(4)[0:n, 0:256]),
                     reads=[("ps", 4)], writes=["khT"])
                for hl in range(2):
                    S.op("pe", lambda e, hl=hl, i=i, n=n: e.matmul(
                        ps[:, 5, hl * 128:(hl + 1) * 128], lhsT=khT[0:n, hl, :],
                        rhs=vv[0:n, i, hl * 128:(hl + 1) * 128], start=True, stop=True),
                        reads=["khT", ("vv", i)], writes=[("ps", 5)])
                S.op("dve", lambda e, i=i: e.tensor_copy(out=xs_st[:, i, :], in_=ps[:, 5, 0:256]),
                     reads=[("ps", 5)], writes=[("xs_st", i)])
            for q3 in range(3):
                S.dma("sp", lambda e, hp=hp, q3=q3: e.dma_start(
                    out=self.xs[q3].rearrange("p (i c) -> p i c", i=3)[:, :, hp * 256:(hp + 1) * 256],
                    in_=xs_st[:, 3 * q3:3 * q3 + 3, :]),
                    reads=[("xs_st", i) for i in range(9)], writes=[("xs", hp, q3)])
            S.dma("sp", lambda e, hp=hp: e.dma_start(
                out=self.xa.rearrange("p (i c) -> p i c", i=9)[:, :, 2 * hp:2 * hp + 2], in_=xa_st),
                reads=["xa_st"], writes=[("xa", hp)])
        S.barrier()
        rg = [[0, 1, 2, 3], [4, 5, 6, 7]]
        for q3 in range(3):
            S.coll(lambda e, q3=q3: e.collective_compute("AllGather", ALU.bypass, replica_groups=rg,
                                                         ins=[self.xs[q3]], outs=[self.xg[q3]]), writes=[("xg", q3)])
        S.coll(lambda e: e.collective_compute("AllGather", ALU.bypass, replica_groups=rg,
                                              ins=[self.xa], outs=[self.xag]), writes=["xag"])

    def hgrn_m2(self, strm):
        S, ps, nc = self.S, self.ps, self.nc
        R1 = 99840
        wbufs = [self.view(R1 + i * 8192, BF16, [KC, 256]) for i in range(2)]
        off = [R1 + 16384]

        def alloc(dt, shape):
            n = 1
            for x in shape:
                n *= x
            nb = n * (4 if dt == F32 else 2)
            nb = (nb + 63) // 64 * 64
            v = self.view(off[0], dt, shape)
            off[0] += nb
            return v
        sgate = alloc(BF16, [8, NREAL])
        Scur = alloc(F32, [8, 128]); SmF = alloc(F32, [8, 128])
        SAb = [alloc(BF16, [8, 128]) for _ in range(3)]
        Aall = alloc(F32, [4, 72])
        OF = alloc(F32, [8, 128]); OSQ = alloc(F32, [8, 128]); RS = alloc(F32, [8, 128])
        Qc = self.view(0, BF16, [8, NREAL]); oloc = self.view(16384, BF16, [8, NREAL])
        yhg = self.view(32768, BF16, [8, NREAL]); Smine = self.view(49152, BF16, [8, 8, 128])
        f2 = lambda v: v.rearrange("p h t -> p (h t)")
        RTB = TBS[0:2]

        for g4 in range(4):
            def ev_g(half, ti, t0, t1e, pv, bk, g4=g4):
                h = 2 * g4 + half
                S.op("act", lambda e: e.activation(out=sgate[:, h, t0:t1e], in_=pv, func=AF.Silu),
                     reads=[("ps", bk)], writes=[("sgate", h, ti)])
            self.proj_fm("m2", strm, self.gidx["hg%d" % g4], wbufs, RTB, ev_g)

        S.dma("sp", lambda e: e.dma_start(out=Aall, in_=self.xag.rearrange("(r p) c -> p r c", p=128)),
              reads=["xag"], writes=["Aall"])
        xg3 = [x_.rearrange("(r p) (i c) -> r p i c", p=128, i=3) for x_ in self.xg]
        S.dma("sp", lambda e: e.dma_start(out=f2(SAb[2]), in_=xg3[2][0, :, 2, :]), reads=[("xg", 2)],
              writes=[("SAb", 2)])
        S.op("dve", lambda e: e.tensor_copy(out=f2(Scur), in_=f2(SAb[2])), reads=[("SAb", 2)], writes=["Scur"])
        for g in range(32):
            r, i = g % 4, g // 4
            sb = SAb[g % 3]
            S.dma("sp", lambda e, sb=sb, r=r, i=i: e.dma_start(out=f2(sb), in_=xg3[i // 3][r, :, i % 3, :]),
                  reads=[("xg", i // 3)], writes=[("SAb", g % 3)])
            if r == 0:
                S.op("dve", lambda e: e.tensor_scalar(out=f2(SmF), in0=f2(Scur), scalar1=self.selc[:, 0:1],
                                                      scalar2=None, op0=ALU.mult),
                     reads=["Scur", "sel"], writes=["SmF"])
            else:
                dst = SmF if r < 3 else Smine[:, i]
                S.op("dve", lambda e, r=r, dst=dst: e.scalar_tensor_tensor(
                    out=f2(dst), in0=f2(Scur), scalar=self.selc[:, r:r + 1], in1=f2(SmF),
                    op0=ALU.mult, op1=ALU.add),
                    reads=["Scur", "sel", "SmF"], writes=(["SmF"] if r < 3 else [("Smine", i)]))
            if g < 31:
                for h in range(8):
                    S.op("dve", lambda e, h=h, r=r, i=i, sb=sb: e.scalar_tensor_tensor(
                        out=Scur[:, h, :], in0=Scur[:, h, :], scalar=Aall[:, r, i * 8 + h:i * 8 + h + 1],
                        in1=sb[:, h, :], op0=ALU.mult, op1=ALU.add),
                        reads=["Scur", "Aall", ("SAb", g % 3)], writes=["Scur"])

        for i in range(8):
            c0 = 128 * i
            for h in range(8):
                bk = 2 + h // 4
                S.op("pe", lambda e, h=h, i=i, c0=c0, bk=bk: e.matmul(
                    ps[:, bk, (h % 4) * 128:(h % 4 + 1) * 128], lhsT=Smine[:, i, h, :], rhs=Qc[:, h, c0:c0 + 128],
                    start=True, stop=True),
                    reads=[("Smine", i), "Qc"], writes=[("ps", bk)])
            S.op("dve", lambda e, c0=c0: e.tensor_tensor(
                out=OF, in0=ps[:, 2:4, :].rearrange("p a (h t) -> p (a h) t", h=4), in1=oloc[:, :, c0:c0 + 128],
                op=ALU.add),
                reads=[("ps", 2), ("ps", 3), "oloc"], writes=["OF"])
            S.op("act", lambda e: e.activation(out=f2(OSQ), in_=f2(OF), func=AF.Square),
                 reads=["OF"], writes=["OSQ"])
            for a in range(2):
                S.op("pe", lambda e, a=a: e.matmul(ps[:, 4 + a, :], lhsT=self.ones128, rhs=f2(OSQ)[:, a * 512:(a + 1) * 512],
                                                   start=True, stop=True),
                     reads=["OSQ", "ones128"], writes=[("ps", 4 + a)])
            S.op("act", lambda e: e.activation(out=f2(RS), in_=ps[:, 4:6, :].rearrange("p a b -> p (a b)"),
                                               func=AF.Ln, bias=self.eps_rms, scale=1.0),
                 reads=[("ps", 4), ("ps", 5), "eps"], writes=["RS"])
            S.op("act", lambda e: e.activation(out=f2(RS), in_=f2(RS), func=AF.Exp, scale=-0.5),
                 reads=["RS"], writes=["RS"])
            S.op("dve", lambda e: e.tensor_tensor(out=f2(OF), in0=f2(OF), in1=f2(RS), op=ALU.mult),
                 reads=["OF", "RS"], writes=["OF"])
            S.op("dve", lambda e, c0=c0: e.tensor_tensor(out=OF, in0=OF, in1=sgate[:, :, c0:c0 + 128], op=ALU.mult),
                 reads=["OF"] + [("sgate", h, c0 // 512) for h in range(8)], writes=["OF"])
            for h in range(8):
                S.op("pool", lambda e, h=h, c0=c0: e.tensor_scalar(
                    out=yhg[:, h, c0:c0 + 128], in0=OF[:, h, :], scalar1=self.gnc[:, h:h + 1], scalar2=None,
                    op0=ALU.mult),
                    reads=["OF", "gn"], writes=[("yhg", i)])
        S.barrier()

    def attn_m3(self, strm):
        S, ps, nc = self.S, self.ps, self.nc
        R1 = 99840
        psb = lambda bk: ps[:, bk, :].bitcast(BF16)
        K_all = self.view(R1, BF16, [2, 4112])
        V_all = self.view(R1 + 16448, BF16, [33, 258])
        IK_all = self.view(R1 + 33536, BF16, [4096])
        AugK = self.view(R1 + 41728, BF16, [4112])
        qT = self.view(R1 + 49952, BF16, [8, NREAL])
        iqT = self.view(R1 + 66336, BF16, [8, NREAL])
        sc = self.view(R1 + 82720, F32, [4096])
        wbufs = [self.view(R1 + 82720 + i * 8192, BF16, [KC, 256]) for i in range(2)]
        Dg = self.view(R1 + 99104, BF16, [16, 128])
        yatt = self.view(0, BF16, [8, NREAL])
        mb = self.view(16384, BF16, [4096])
        mbT = self.view(24576, BF16, [32, 128])
        junk = self.view(49152, BF16, [4096])
        rh = [self.view(57344 + q * 1024, BF16, [512]) for q in range(4)]
        ya = self.view(61440, BF16, [8, 128])
        PTb = [self.view(61440 + q * 2048, BF16, [1024]) for q in range(2)]
        cbt = self.view(65536, BF16, [4, 128])
        kst = self.view(49152, BF16, [2, NREAL])
        vst = self.view(49152 + 4096, BF16, [8, 258])
        ikst = self.view(49152 + 4096 + 4160, BF16, [NREAL])
        iktmp = self.view(49152 + 10304, F32, [64])
        ikn2 = self.view(49152 + 10304 + 256, BF16, [128])
        cst = self.cst
        AugQ = cst[:, 664:1176].bitcast(BF16)
        AugR = cst[:, 1176:1688].bitcast(BF16)
        wq = cst[:, 1688:1816].rearrange("p (i h) -> p i h", h=16)
        H = cst[:, 1816:1848]
        mx = cst[:, 1848:2008].rearrange("p (h c) -> p h c", h=8)
        mrow = cst[:, 2008:2016]; cc = cst[:, 2016:2024]; rs = cst[:, 2024:2032]
        Bt = cst[:, 2032:2033]; Wc = cst[:, 2033:2034]; mid = cst[:, 2034:2035]; cnt = cst[:, 2035:2036]
        u2 = cst[:, 2036:2037]; tau = cst[:, 2037:2038]; rstd1 = cst[:, 2038:2039]
        AQc = cst[:, 2040:2104].rearrange("p (i h) -> p i h", h=8)
        pw = cst[:, 2104:2136]
        gik = cst[:, 2136:2200]; bik = cst[:, 2200:2264]
        st6 = cst[:, 2264:2270]; mv = cst[:, 2270:2272]
        TL = [(i, 128 * i, 128) for i in range(8)] + [(8, NREAL, NMETA)]
        RTB = TBS[0:2]
        NB = 20

        S.dma("sp", lambda e: e.dma_start(out=cst[:, 2040:2264], in_=self.catt), writes=["catt"])
        S.op("pool", lambda e: e.memset(AugK[0:65, :], 0.0), writes=["AugK"])
        S.op("pool", lambda e: e.memset(AugQ[0:65, :], 0.0), writes=["AugQ"])
        S.op("pool", lambda e: e.memset(AugR[0:65, :], 0.0), writes=["AugR"])
        for rr in range(3):
            S.dma("pool", lambda e, rr=rr: e.dma_start(out=AugK[32 * rr:32 * rr + 1, :], in_=self.augk[rr:rr + 1, :]),
                  writes=["AugK"])
        for rr in range(2):
            S.dma("pool", lambda e, rr=rr: e.dma_start(out=AugQ[32 * rr:32 * rr + 1, :], in_=self.augs[rr:rr + 1, :]),
                  writes=["AugQ"])
            S.dma("pool", lambda e, rr=rr: e.dma_start(out=AugR[32 * rr:32 * rr + 1, :], in_=self.augs[rr:rr + 1, :]),
                  writes=["AugR"])
        S.dma("pool", lambda e: e.dma_start(out=cbt, in_=self.cbt_d.rearrange("p (r s) -> p r s", r=4)), writes=["cbt"])
        S.op("dve", lambda e: e.memset(vst.rearrange("p i (k c) -> p i k c", k=2)[:, :, :, 128:129], 1.0),
             writes=["vst1"])
        S.op("dve", lambda e: e.memset(V_all[:, 32, :].rearrange("p (k c) -> p k c", k=2)[:, :, 128:129], 1.0),
             writes=["V1"])

        def ev_k(half, ti, t0, t1e, pv, bk):
            if ti < 2:
                S.op("act", lambda e: e.activation(out=kst[:, half, t0:t1e], in_=pv, func=AF.Copy),
                     reads=[("ps", bk)], writes=[("kst", half, ti)])
            else:
                S.op("act", lambda e: e.activation(out=K_all[:, half, 4096:4112], in_=pv, func=AF.Copy),
                     reads=[("ps", bk)], writes=[("Kmeta", half)])
        self.proj_fm("m3", strm, self.gidx["ak"], wbufs, TBS, ev_k)

        def ev_v(i, c0, n, pv, bk):
            src = pv.rearrange("p (k c) -> p k c", k=2)
            if i < 8:
                dst = vst[:, i, :].rearrange("p (k c) -> p k c", k=2)[:, :, 0:128]
                S.op("act", lambda e: e.activation(out=dst, in_=src, func=AF.Copy),
                     reads=[("ps", bk), "vst1"], writes=[("vst", i)])
            else:
                dst = V_all[0:n, 32, :].rearrange("p (k c) -> p k c", k=2)[:, :, 0:128]
                S.op("act", lambda e: e.activation(out=dst, in_=src, func=AF.Copy),
                     reads=[("ps", bk), "V1"], writes=["Vmeta"])
        self.proj_tm("m3", strm, self.gidx["av"], wbufs, TL, 256, ev_v)

        def ev_ik(i, c0, n, pv, bk):
            S.op("dve", lambda e: e.bn_stats(out=st6, in_=pv[:, 0:64]), reads=[("ps", bk)], writes=["st6"])
            S.op("dve", lambda e: e.bn_aggr(out=mv, in_=st6), reads=["st6"], writes=["mv"])
            S.op("act", lambda e: e.activation(out=rstd1, in_=mv[:, 1:2], func=AF.Sqrt, bias=self.eps_ik, scale=1.0),
                 reads=["mv", "eps"], writes=["rstd1"])
            S.op("dve", lambda e: e.reciprocal(out=rstd1, in_=rstd1), reads=["rstd1"], writes=["rstd1"])
            S.op("dve", lambda e: e.tensor_scalar(out=iktmp, in0=pv[:, 0:64], scalar1=mv[:, 0:1], scalar2=rstd1,
                                                  op0=ALU.subtract, op1=ALU.mult),
                 reads=[("ps", bk), "mv", "rstd1"], writes=["iktmp"])
            S.op("dve", lambda e: e.tensor_tensor(out=iktmp, in0=iktmp, in1=gik, op=ALU.mult),
                 reads=["iktmp", "catt"], writes=["iktmp"])
            S.op("dve", lambda e: e.tensor_tensor(out=ikn2[:, 0:64], in0=iktmp, in1=bik, op=ALU.add),
                 reads=["iktmp", "catt"], writes=["ikn2a"])
            S.op("pool", lambda e: e.tensor_copy(out=ikn2[:, 64:128], in_=ikn2[:, 0:64]),
                 reads=["ikn2a"], writes=["ikn2b"])
            S.op("act", lambda e, i=i: e.activation(out=wq[:, i, :], in_=pv[:, 64:80], func=AF.Copy,
                                                    scale=0.25 * 0.125),
                 reads=[("ps", bk)], writes=[("wq", i)])
            S.op("pe", lambda e: e.transpose(out=psb(2)[:, 0:128], in_=ikn2, identity=self.ident_bf),
                 reads=["ikn2a", "ikn2b", "ident"], writes=[("ps", 2)])
            S.op("act", lambda e, c0=c0: e.activation(out=ikst[:, c0:c0 + 128], in_=psb(2)[:, 0:128], func=AF.Copy),
                 reads=[("ps", 2)], writes=[("ikst", i)])
        self.proj_tm("m3", strm, self.gidx["ikw"], wbufs, TL[0:8], 80, ev_ik)

        S.dma("sp", lambda e: e.dma_start(out=self.ks.rearrange("p (k t) -> p k t", k=2), in_=kst),
              reads=[("kst", hh, ti) for hh in range(2) for ti in range(2)], writes=["ks"])
        S.dma("sp", lambda e: e.dma_start(out=self.vs[:, 0:2064].rearrange("p (i c) -> p i c", i=8), in_=vst),
              reads=[("vst", i) for i in range(8)] + ["vst1"], writes=["vs"])
        S.dma("sp", lambda e: e.dma_start(out=self.vs[:, 2064:3088], in_=ikst),
              reads=[("ikst", i) for i in range(8)], writes=["vs2"])
        S.barrier()
        rg = [[0, 1, 2, 3], [4, 5, 6, 7]]
        S.coll(lambda e: e.collective_compute("AllGather", ALU.bypass, replica_groups=rg,
                                              ins=[self.ks], outs=[self.kg]), writes=["kg"])
        S.coll(lambda e: e.collective_compute("AllGather", ALU.bypass, replica_groups=rg,
                                              ins=[self.vs], outs=[self.vg]), writes=["vg"])

        for g4 in range(4):
            def ev_q(half, ti, t0, t1e, pv, bk, g4=g4):
                h = 2 * g4 + half
                S.op("act", lambda e: e.activation(out=qT[:, h, t0:t1e], in_=pv, func=AF.Copy, scale=128.0 ** -0.5),
                     reads=[("ps", bk)], writes=[("qT", h, ti)])
            self.proj_fm("m3", strm, self.gidx["aq%d" % g4], wbufs, RTB, ev_q)
        for g4 in range(4):
            def ev_iq(half, ti, t0, t1e, pv, bk, g4=g4):
                h = 2 * g4 + half
                S.op("dve", lambda e: e.tensor_copy(out=iqT[:, h, t0:t1e], in_=pv),
                     reads=[("ps", bk)], writes=[("iqT", h, ti)])
            self.proj_fm("m3", strm, self.gidx["iq%d" % g4], wbufs, RTB, ev_iq)

        for r in range(4):
            S.dma("sp", lambda e, r=r: e.dma_start(
                out=K_all[:, :, r * 1024:(r + 1) * 1024],
                in_=self.kg[r * 128:(r + 1) * 128, :].rearrange("p (k t) -> p k t", k=2)),
                reads=["kg"], writes=["K_all"])
            S.dma("sp", lambda e, r=r: e.dma_start(
                out=V_all[:, r * 8:(r + 1) * 8, :],
                in_=self.vg[r * 128:(r + 1) * 128, 0:2064].rearrange("p (i c) -> p i c", i=8)),
                reads=["vg"], writes=["V_all"])
            S.dma("sp", lambda e, r=r: e.dma_start(
                out=IK_all[:, r * 1024:(r + 1) * 1024], in_=self.vg[r * 128:(r + 1) * 128, 2064:3088]),
                reads=["vg"], writes=["IK_all"])
        S.barrier()

        sc4 = sc.rearrange("p (r c) -> p r c", r=4)
        mb4 = mb.rearrange("p (r c) -> p r c", r=4)
        jk4 = junk.rearrange("p (r c) -> p r c", r=4)
        def geom(i):
            q0 = 128 * i
            nk = 128 * (i + 1)
            pieces = [(r, c0, min(512, nk - c0)) for r in range(4) for c0 in range(0, nk, 512)]
            return q0, nk, pieces

        def st_idx(i):
            q0, nk, pieces = geom(i)
            for h in range(16):
                S.op("pool", lambda e, h=h: e.tensor_scalar(out=Dg[:, h, :], in0=self.ident_bf,
                                                            scalar1=wq[:, i, h:h + 1], scalar2=None, op0=ALU.mult),
                     reads=["ident", ("wq", i)], writes=["Dg"])
            for pi, (r, c0, cn) in enumerate(pieces):
                col0 = r * 1024 + c0
                accb = 4 + pi % 2

                def head_mm(h, cn=cn, col0=col0):
                    bk, hb = h % 4, h % 2
                    S.op("pe", lambda e: e.matmul(
                        ps[:, bk, 0:cn], lhsT=iqT[hb * 64:(hb + 1) * 64, h // 2, q0:q0 + 128],
                        rhs=IK_all[hb * 64:(hb + 1) * 64, col0:col0 + cn], start=True, stop=True),
                        reads=["IK_all", "iqT"], writes=[("ps", bk)])
                    S.op("act", lambda e: e.activation(out=rh[bk][:, 0:cn], in_=ps[:, bk, 0:cn], func=AF.Relu),
                         reads=[("ps", bk)], writes=[("rh", bk)])

                def head_acc(h, cn=cn, accb=accb):
                    bk = h % 4
                    S.op("pe", lambda e: e.matmul(
                        ps[:, accb, 0:cn], lhsT=Dg[:, h, :], rhs=rh[bk][:, 0:cn], start=(h == 0), stop=(h == 15)),
                        reads=["Dg", ("rh", bk)], writes=[("ps", accb)])
                for h in range(16):
                    head_mm(h)
                    if h >= 2:
                        head_acc(h - 2)
                head_acc(14)
                head_acc(15)
                S.op("act", lambda e, accb=accb, col0=col0, cn=cn: e.activation(
                    out=sc[:, col0:col0 + cn], in_=ps[:, accb, 0:cn], func=AF.Copy),
                    reads=[("ps", accb)], writes=["sc"])

        def st_bis(i):
            q0, nk, pieces = geom(i)
            scv, mbv, jkv = sc4[:, :, 0:nk], mb4[:, :, 0:nk], jk4[:, :, 0:nk]
            S.op("dve", lambda e: e.reduce_max(out=Bt, in_=scv, axis=AX.XY, apply_absolute_value=True),
                 reads=["sc"], writes=["Bt"])
            S.op("dve", lambda e: e.tensor_tensor(out=sc4[:, :, q0:q0 + 128], in0=sc4[:, :, q0:q0 + 128], in1=cbt,
                                                  op=ALU.add),
                 reads=["sc", "cbt", "Bt"], writes=["sc"])
            S.op("dve", lambda e: e.tensor_scalar(out=Wc, in0=Bt, scalar1=2.0002, scalar2=1e-6,
                                                  op0=ALU.mult, op1=ALU.add), reads=["Bt"], writes=["Wc"])
            S.op("dve", lambda e: e.tensor_scalar(out=H[:, 0:NB + 1], in0=pw[:, 0:NB + 1], scalar1=Wc, scalar2=None,
                                                  op0=ALU.mult), reads=["Wc", "catt"], writes=["H"])
            S.op("dve", lambda e: e.memset(mid, 0.0), writes=["mid"])
            for k in range(NB):
                S.op("dve", lambda e: e.tensor_scalar(
                    out=jkv, in0=scv, scalar1=mid, scalar2=0.0, op0=ALU.is_ge, op1=ALU.add, accum_out=cnt),
                    reads=["sc", "mid"], writes=["junk", "cnt"])
                S.op("dve", lambda e, k=k: e.tensor_scalar(out=u2, in0=cnt, scalar1=256.0, scalar2=H[:, k:k + 1],
                                                           op0=ALU.is_ge, op1=ALU.mult),
                     reads=["cnt", "H"], writes=["u2"])
                S.op("dve", lambda e, k=k: e.scalar_tensor_tensor(out=mid, in0=mid, scalar=H[:, k + 1:k + 2], in1=u2,
                                                                  op0=ALU.subtract, op1=ALU.add),
                     reads=["mid", "H", "u2"], writes=["mid"])
            S.op("dve", lambda e: e.tensor_tensor(out=tau, in0=mid, in1=H[:, NB:NB + 1], op=ALU.subtract),
                 reads=["mid", "H"], writes=["tau"])
            S.op("dve", lambda e: e.tensor_scalar(
                out=mbv, in0=scv, scalar1=tau, scalar2=-30000.0, op0=ALU.is_lt, op1=ALU.mult),
                reads=["sc", "tau"], writes=["mb"])

        def st_mbT(i):
            kts = [(r, ip) for r in range(4) for ip in range(i + 1)]
            for g0 in range(0, len(kts), 8):
                grp = kts[g0:g0 + 8]
                bk = 6 + (g0 // 8) % 2
                for s_, (r, ip) in enumerate(grp):
                    S.op("pe", lambda e, bk=bk, s_=s_, r=r, ip=ip: e.transpose(
                        out=psb(bk)[:, s_ * 128:(s_ + 1) * 128], in_=mb[:, r * 1024 + ip * 128:r * 1024 + ip * 128 + 128],
                        identity=self.ident_bf),
                        reads=["mb", "ident"], writes=[("ps", bk)])
                for s_, (r, ip) in enumerate(grp):
                    S.op("act", lambda e, bk=bk, s_=s_, r=r, ip=ip: e.activation(
                        out=mbT[:, r * 8 + ip, :], in_=psb(bk)[:, s_ * 128:(s_ + 1) * 128], func=AF.Copy),
                        reads=[("ps", bk)], writes=["mbT"])

        def st_passA(i):
            q0, nk, pieces = geom(i)
            S.op("dve", lambda e: e.memset(ps[:, 5:8, :].rearrange("p a b -> p (a b)"), 0.0),
                 writes=[("ps", 5), ("ps", 6), ("ps", 7)])
            S.dma("pool", lambda e: e.dma_start(out=AugQ[64:65, :], in_=self.augq[i:i + 1, :]), writes=["AugQ"])
            pcs = pieces + [(4, 0, NMETA)]
            for h in range(8):
                kvh = h // 4
                for pi, (r, c0, cn) in enumerate(pcs):
                    col0 = r * 1024 + c0
                    bk = (h * len(pcs) + pi) % 4
                    meta = (r == 4)
                    S.op("pe", lambda e, bk=bk, h=h, kvh=kvh, col0=col0, cn=cn: e.matmul(
                        ps[:, bk, 0:cn], lhsT=qT[:, h, q0:q0 + 128], rhs=K_all[:, kvh, col0:col0 + cn],
                        start=True, stop=False),
                        reads=["K_all", "qT", ("Kmeta", kvh)], writes=[("ps", bk)])
                    S.op("pe", lambda e, bk=bk, h=h, col0=col0, cn=cn, meta=meta: e.matmul(
                        ps[:, bk, 0:cn], lhsT=AugQ[0:65, h * 128:(h + 1) * 128], rhs=AugK[0:65, col0:col0 + cn],
                        start=False, stop=meta),
                        reads=["AugQ", "AugK"], writes=[("ps", bk)])
                    if not meta:
                        S.op("pe", lambda e, bk=bk, col0=col0, cn=cn: e.matmul(
                            ps[:, bk, 0:cn], lhsT=self.ident_bf, rhs=mb[:, col0:col0 + cn], start=False, stop=True),
                            reads=["mb", "ident"], writes=[("ps", bk)])
                    S.op("dve", lambda e, bk=bk, h=h, pi=pi, cn=cn: e.reduce_max(
                        out=mx[:, h, pi:pi + 1], in_=ps[:, bk, 0:cn], axis=AX.X),
                        reads=[("ps", bk)], writes=["mx"])
            S.op("dve", lambda e: e.reduce_max(out=mrow, in_=mx[:, :, 0:len(pcs)], axis=AX.X),
                 reads=["mx"], writes=["mrow"])
            S.op("dve", lambda e: e.tensor_tensor(out=cc, in0=AQc[:, i, :], in1=mrow, op=ALU.subtract),
                 reads=["mrow", "catt"], writes=["cc"])
            for h in range(8):
                S.op("pool", lambda e, h=h: e.tensor_scalar(out=Dg[:, h, :], in0=self.ident_bf, scalar1=cc[:, h:h + 1],
                                                            scalar2=None, op0=ALU.mult),
                     reads=["ident", "cc"], writes=["Dg"])
            for a_ in range(2):
                S.op("pe", lambda e, a_=a_: e.matmul(ps[:, 4, :], lhsT=self.ones_bf,
                                                     rhs=Dg[:, 4 * a_:4 * a_ + 4, :].rearrange("p h t -> p (h t)"),
                                                     start=True, stop=True),
                     reads=["Dg", "ones_bf"], writes=[("ps", 4)])
                S.op("dve", lambda e, a_=a_: e.tensor_copy(out=AugR[64:65, a_ * 512:(a_ + 1) * 512], in_=ps[64:65, 4, :]),
                     reads=[("ps", 4)], writes=["AugR"])

        def st_passB(i):
            q0, nk, pieces = geom(i)
            ktl = [(r * 1024 + ip * 128, r * 8 + ip, 128) for r in range(4) for ip in range(i + 1)] + [(4096, 32, NMETA)]
            Oreg = lambda h: ps[:, 5 + h // 3, (h % 3) * 129:(h % 3 + 1) * 129]

            def logits(qi):
                col0, vt, n = ktl[qi]
                meta = (n == NMETA)
                pair = (0, 1) if qi % 2 == 0 else (2, 3)
                for h in range(8):
                    kvh = h // 4
                    out = ps[0:n, pair[h // 4], (h % 4) * 128:(h % 4 + 1) * 128]
                    S.op("pe", lambda e, out=out, kvh=kvh, h=h: e.matmul(
                        out, lhsT=K_all[:, kvh, col0:col0 + n], rhs=qT[:, h, q0:q0 + 128], start=True, stop=False),
                        reads=["K_all", "qT", ("Kmeta", kvh)], writes=[("ps", pair[h // 4])])
                    S.op("pe", lambda e, out=out, h=h: e.matmul(
                        out, lhsT=AugK[0:65, col0:col0 + n], rhs=AugR[0:65, h * 128:(h + 1) * 128],
                        start=False, stop=meta),
                        reads=["AugK", "AugR"], writes=[("ps", pair[h // 4])])
                    if not meta:
                        S.op("pe", lambda e, out=out: e.matmul(
                            out, lhsT=self.ident_bf, rhs=mbT[:, vt, :], start=False, stop=True),
                            reads=["mbT", "ident"], writes=[("ps", pair[h // 4])])
                pt = PTb[qi % 2]
                S.op("act", lambda e: e.activation(
                    out=pt[0:n, :], in_=ps[0:n, pair[0]:pair[0] + 2, :].rearrange("p a b -> p (a b)"), func=AF.Exp),
                    reads=[("ps", pair[0]), ("ps", pair[1])], writes=[("PTb", qi % 2)])

            def pv(qi):
                col0, vt, n = ktl[qi]
                pt = PTb[qi % 2]
                for h in range(8):
                    kvh = h // 4
                    S.op("pe", lambda e, h=h, kvh=kvh: e.matmul(
                        Oreg(h), lhsT=pt[0:n, h * 128:(h + 1) * 128], rhs=V_all[0:n, vt, kvh * 129:(kvh + 1) * 129],
                        start=False, stop=(qi == len(ktl) - 1)),
                        reads=[("PTb", qi % 2), "V_all", "Vmeta"], writes=[("ps", 5 + h // 3)])
            logits(0)
            for qi in range(len(ktl)):
                if qi + 1 < len(ktl):
                    logits(qi + 1)
                pv(qi)

        def st_fin(i):
            q0 = 128 * i
            for b3 in range(3):
                nh = 3 if b3 < 2 else 2
                Ov = ps[:, 5 + b3, 0:nh * 129].rearrange("p (h c) -> p h c", c=129)
                S.op("dve", lambda e, Ov=Ov, b3=b3, nh=nh: e.reciprocal(out=rs[:, 3 * b3:3 * b3 + nh], in_=Ov[:, :, 128]),
                     reads=[("ps", 5 + b3)], writes=[("rs", b3)])
                S.op("dve", lambda e, Ov=Ov, b3=b3, nh=nh: e.tensor_tensor(
                    out=ya[:, 3 * b3:3 * b3 + nh, :], in0=Ov[:, :, 0:128],
                    in1=rs[:, 3 * b3:3 * b3 + nh].unsqueeze(2).to_broadcast([128, nh, 128]), op=ALU.mult),
                    reads=[("ps", 5 + b3), ("rs", b3)], writes=[("ya", b3), ("PTb", 0)])
            for h in range(8):
                S.op("pe", lambda e, h=h: e.transpose(out=psb(4)[:, h * 128:(h + 1) * 128], in_=ya[:, h, :],
                                                      identity=self.ident_bf),
                     reads=[("ya", h // 3), ("PTb", 0), "ident"], writes=[("ps", 4)])
            S.op("act", lambda e: e.activation(out=yatt[:, :, q0:q0 + 128],
                                               in_=psb(4).rearrange("p (h t) -> p h t", h=8), func=AF.Copy),
                 reads=[("ps", 4)], writes=[("yatt", i)])

        st_idx(0)
        st_bis(0)
        st_mbT(0)
        for i in range(8):
            if i + 1 < 8:
                st_idx(i + 1)
            st_passA(i)
            if i + 1 < 8:
                st_bis(i + 1)
            st_passB(i)
            st_fin(i)
            if i + 1 < 8:
                st_mbT(i + 1)
        S.barrier()

    def merge_m4(self, strm, cg, cb):
        S, ps, nc = self.S, self.ps, self.nc
        R1 = 99840
        RTB = TBS[0:2]
        yatt = self.view(0, BF16, [8, NREAL]); yhg = self.view(32768, BF16, [8, NREAL])
        merged = self.view(R1, BF16, [KC, NREAL])
        o = R1 + 32768
        wga = [self.view(o + q * 8192, BF16, [KC, 256]) for q in range(2)]
        wgh = [self.view(o + 16384 + q * 8192, BF16, [KC, 256]) for q in range(2)]
        wba = [self.view(o + 32768 + q * 4096, BF16, [8, 256]) for q in range(2)]
        wbh = [self.view(o + 40960 + q * 4096, BF16, [8, 256]) for q in range(2)]
        tm = [self.view(o + 49152 + q * 2048, F32, [512]) for q in range(4)]
        for mg in range(8):
            q = mg % 2
            S.dma("pool", lambda e, q=q, mg=mg: e.dma_start(out=wga[q], in_=self.win[self.gidx["ga%d" % mg]]),
                  writes=[("wga", q)])
            S.dma("pool", lambda e, q=q, mg=mg: e.dma_start(out=wgh[q], in_=self.win[self.gidx["gh%d" % mg]]),
                  writes=[("wgh", q)])
            S.dma("pool", lambda e, q=q, mg=mg: e.dma_start(out=wba[q], in_=self.wba_d[mg]), writes=[("wba", q)])
            S.dma("pool", lambda e, q=q, mg=mg: e.dma_start(out=wbh[q], in_=self.wbh_d[mg]), writes=[("wbh", q)])
            for half in range(2):
                mc = 2 * mg + half
                hs = slice(half * 128, (half + 1) * 128)
                for ti, (t0, t1e) in enumerate(RTB):
                    pp = (half * 2 + ti) % 2
                    b0 = 4 * pp
                    for k in range(KC):
                        S.op("pe", lambda e, b0=b0, q=q, k=k, hs=hs, t0=t0, t1e=t1e: e.matmul(
                            ps[:, b0, :], lhsT=wga[q][:, k, hs], rhs=strm[:, k, t0:t1e],
                            start=(k == 0), stop=(k == KC - 1)),
                            reads=[("wga", q), "strm"], writes=[("ps", b0)])
                    for k in range(8):
                        S.op("pe", lambda e, b0=b0, q=q, k=k, hs=hs, t0=t0, t1e=t1e: e.matmul(
                            ps[:, b0 + 1, :], lhsT=wba[q][:, k, hs], rhs=yatt[:, k, t0:t1e],
                            start=(k == 0), stop=(k == 7)),
                            reads=[("wba", q), "yatt"], writes=[("ps", b0 + 1)])
                    for k in range(KC):
                        S.op("pe", lambda e, b0=b0, q=q, k=k, hs=hs, t0=t0, t1e=t1e: e.matmul(
                            ps[:, b0 + 2, :], lhsT=wgh[q][:, k, hs], rhs=strm[:, k, t0:t1e],
                            start=(k == 0), stop=(k == KC - 1)),
                            reads=[("wgh", q), "strm"], writes=[("ps", b0 + 2)])
                    for k in range(8):
                        S.op("pe", lambda e, b0=b0, q=q, k=k, hs=hs, t0=t0, t1e=t1e: e.matmul(
                            ps[:, b0 + 3, :], lhsT=wbh[q][:, k, hs], rhs=yhg[:, k, t0:t1e],
                            start=(k == 0), stop=(k == 7)),
                            reads=[("wbh", q), "yhg"], writes=[("ps", b0 + 3)])
                    ta, th = tm[2 * pp], tm[2 * pp + 1]
                    S.op("act", lambda e, ta=ta, b0=b0: e.activation(out=ta, in_=ps[:, b0, :], func=AF.Sigmoid),
                         reads=[("ps", b0)], writes=[("tm", 2 * pp)])
                    S.op("dve", lambda e, ta=ta, b0=b0: e.tensor_tensor(out=ta, in0=ta, in1=ps[:, b0 + 1, :], op=ALU.mult),
                         reads=[("tm", 2 * pp), ("ps", b0 + 1)], writes=[("tm", 2 * pp)])
                    S.op("act", lambda e, th=th, b0=b0: e.activation(out=th, in_=ps[:, b0 + 2, :], func=AF.Sigmoid),
                         reads=[("ps", b0 + 2)], writes=[("tm", 2 * pp + 1)])
                    S.op("dve", lambda e, th=th, b0=b0: e.tensor_tensor(out=th, in0=th, in1=ps[:, b0 + 3, :], op=ALU.mult),
                         reads=[("tm", 2 * pp + 1), ("ps", b0 + 3)], writes=[("tm", 2 * pp + 1)])
                    S.op("pool", lambda e, ta=ta, th=th, mc=mc, t0=t0, t1e=t1e: e.tensor_tensor(
                        out=merged[:, mc, t0:t1e], in0=ta, in1=th, op=ALU.add),
                        reads=[("tm", 2 * pp), ("tm", 2 * pp + 1)], writes=[("merged", mc, ti)])
        S.barrier()
        z = self.view(R1 + 32768, F32, [KC, NREAL])
        wo = [self.view(53760 + q * 4096, BF16, [KC, 128]) for q in range(2)]
        strm_out = self.view(0, BF16, [KC, NREAL])
        for dc in range(KC):
            q = dc % 2
            S.dma("pool", lambda e, q=q, dc=dc: e.dma_start(out=wo[q], in_=self.wo_d[dc]), writes=[("wo", q)])
            S.dma("sp", lambda e, dc=dc: e.dma_start(out=z[:, dc, :], in_=self.h1s[dc * 128:(dc + 1) * 128, 0:NREAL]),
                  writes=[("l2", "z", dc, ti) for ti in range(2)])
            for ti, (t0, t1e) in enumerate(RTB):
                pb = (dc * 2 + ti) % 2
                for k in range(KC):
                    S.op("pe", lambda e, pb=pb, q=q, k=k, t0=t0, t1e=t1e: e.matmul(
                        ps[:, pb, :], lhsT=wo[q][:, k, :], rhs=merged[:, k, t0:t1e],
                        start=(k == 0), stop=(k == KC - 1)),
                        reads=[("wo", q), ("merged", k, ti)], writes=[("ps", pb)])
                S.op("dve", lambda e, pb=pb, dc=dc, t0=t0, t1e=t1e: e.scalar_tensor_tensor(
                    out=z[:, dc, t0:t1e], in0=ps[:, pb, :], scalar=1.0 / ALPHA, in1=z[:, dc, t0:t1e],
                    op0=ALU.mult, op1=ALU.add),
                    reads=[("ps", pb), ("l2", "z", dc, ti)], writes=[("l2", "z", dc, ti)])
        self.ln_apply("l2", z, RTB, cg, cb, 33280, LN_EPS / (ALPHA * ALPHA), strm_out=strm_out,
                      resid_out=self.h2s)
        S.barrier()

    def eps_col(self, val):
        return self.eps_cols[val]

    def build(self):
        nc, S = self.nc, self.S
        stage = self.stage
        xT = self.din("xT", [D, NT])
        wg1 = self.din("wg1", [FC // 2, 128, KC, 256])
        wu1 = self.din("wu1", [FC // 2, 128, KC, 256])
        wd1 = self.din("wd1", [KC, 128, FC, 128])
        wg2 = self.din("wg2", [FC // 2, 128, KC, 256])
        wu2 = self.din("wu2", [FC // 2, 128, KC, 256])
        wd2 = self.din("wd2", [KC, 128, FC, 128])
        cvec = self.din("cvec", [128, 128])
        cmat = self.din("cmat", [128, 384])
        self.catt = self.din("catt", [128, 224])
        self.augk = self.din("augk", [3, 4112])
        self.augs = self.din("augs", [2, 1024])
        self.augq = self.din("augq", [8, 1024])
        self.cbt_d = self.din("cbt", [128, 512])
        self.win = self.din("win", [len(GROUPS), 128, KC, 256])
        self.wba_d = self.din("wba", [8, 128, 8, 256])
        self.wbh_d = self.din("wbh", [8, 128, 8, 256])
        self.wo_d = self.din("wo", [KC, 128, KC, 128])
        self.gidx = {nm: i for i, (nm, _, _) in enumerate(GROUPS)}
        self.h1s = h1s = self.dscratch("h1s", [D, NT])
        self.h2s = self.dscratch("h2s", [D, NREAL])
        self.xs = [self.dscratch("xs%d" % q, [128, 3 * 1024], BF16) for q in range(3)]
        self.xg = [self.dscratch("xg%d" % q, [512, 3 * 1024], BF16) for q in range(3)]
        self.xa = self.dscratch("xa", [128, 72])
        self.xag = self.dscratch("xag", [512, 72])
        self.ks = self.dscratch("ks", [128, 2048], BF16)
        self.kg = self.dscratch("kg", [512, 2048], BF16)
        self.vs = self.dscratch("vs", [128, 3088], BF16)
        self.vg = self.dscratch("vg", [512, 3088], BF16)
        if stage == 1:
            dbg = self.dout("dbg", [D, NT])
        elif stage in (3, 4):
            dbg = self.dout("dbg", [128, 8 * NREAL])
            self.dbg2 = self.dout("dbg2", [128, 12000])
        elif stage == 5:
            dbg = self.dout("dbg", [D, NREAL])
        else:
            outT = self.dout("outT", [D, NREAL])

        from contextlib import ExitStack
        with ExitStack() as es:
            self.arena = es.enter_context(nc.sbuf_tensor("arena", [128, ARENA_F32], F32))
            self.cst = es.enter_context(nc.sbuf_tensor("cst", [128, CONST_F32], F32))
            self.ps = es.enter_context(nc.psum_tensor("ps", [128, 8, 512], F32))
            esems = {e: es.enter_context(nc.semaphore("sem_" + e)) for e in ENGS}
            dsems = [es.enter_context(nc.semaphore("dsem%d" % i)) for i in range(S.n_dma_sems + 8)]
            block = es.enter_context(nc.Block())
            cst = self.cst
            self.ones_f32 = cst[:, 0:128]
            cv = self.cv = cst[:, 128:256]
            epsA = cst[:, 256:257]
            self.eps_rms = cst[:, 257:258]
            self.eps_ik = cst[:, 258:259]
            self.eps_cols = {LN_EPS / (ALPHA * ALPHA): epsA}
            self.ident_bf = cst[:, 264:328].bitcast(BF16)
            self.ones_bf = cst[:, 328:392].bitcast(BF16)
            self.mask2 = cst[:, 392:520]
            self.ones128 = cst[:, 520:648]
            self.lbc = cst[:, 648:656]
            self.omlc = cst[:, 656:664]
            self.gnc = cv[:, 112:120]
            self.selc = cv[:, 120:124]
            S.op("dve", lambda e: e.memset(self.ones_f32, 1.0 / D), writes=["ones"])
            S.op("dve", lambda e: e.memset(self.ones128, 1.0 / 128), writes=["ones128"])
            S.op("dve", lambda e: e.memset(epsA, LN_EPS / (ALPHA * ALPHA)), writes=["eps"])
            S.op("dve", lambda e: e.memset(self.eps_rms, RMS_EPS), writes=["eps"])
            S.op("dve", lambda e: e.memset(self.eps_ik, LN_EPS), writes=["eps"])
            S.dma("sp", lambda e: e.dma_start(out=cv, in_=cvec), writes=["cv"])
            S.dma("sp", lambda e: e.dma_start(out=self.mask2, in_=cmat[:, 256:384]), writes=["mask2"])
            S.dma("pool", lambda e: e.dma_start(out=self.ident_bf, in_=cmat[:, 0:128]), writes=["ident"])
            S.dma("pool", lambda e: e.dma_start(out=self.ones_bf, in_=cmat[:, 128:256]), writes=["ones_bf"])
            S.op("dve", lambda e: e.tensor_tensor(out=self.lbc, in0=cv[:, 96:104], in1=cv[:, 104:112], op=ALU.subtract),
                 reads=["cv"], writes=["lb"])
            S.op("act", lambda e: e.activation(out=self.lbc, in_=self.lbc, func=AF.Sigmoid), reads=["lb"], writes=["lb"])
            S.op("dve", lambda e: e.tensor_scalar(out=self.omlc, in0=self.lbc, scalar1=-1.0, scalar2=1.0,
                                                  op0=ALU.mult, op1=ALU.add), reads=["lb"], writes=["lb"])
            strm0 = self.view(0, BF16, [KC, NT])
            S.dma("pool", lambda e: e.dma_start(out=strm0, in_=xT.rearrange("(k p) t -> p k t", p=128)),
                  writes=[("f1", "sin")])
            S.barrier()
            self.ffn_phase("f1", NT, TBS, strm0, 66560, wg1, wu1, wd1, xT, cv[:, 0:16], cv[:, 16:32],
                           resid_out=(dbg if stage == 1 else h1s))
            strm1 = self.view(66560, BF16, [KC, NT])
            if stage >= 2:
                self.hgrn_m1(strm1)
                self.hgrn_m2(strm1)
            if stage == 3:
                yhg = self.view(32768, BF16, [8 * NREAL])
                S.dma("pool", lambda e: e.dma_start(out=dbg, in_=yhg), reads=[])
            if stage >= 4:
                self.attn_m3(strm1)
            if stage == 4:
                yat = self.view(0, BF16, [8 * NREAL])
                S.dma("pool", lambda e: e.dma_start(out=dbg, in_=yat), reads=[])
            if stage >= 5:
                if stage == 5:
                    self.h2s = dbg
                self.merge_m4(strm1, cv[:, 32:48], cv[:, 48:64])
            if stage >= 6:
                strm2 = self.view(0, BF16, [KC, NREAL])
                self.ffn_phase("f2", NREAL, TBS[0:2], strm2, 66560, wg2, wu2, wd2, self.h2s, cv[:, 64:80],
                               cv[:, 80:96], final_out=outT)
            S.emit(block, esems, dsems)
        return nc


def _lay_gu(w):
    return np.ascontiguousarray(w.reshape(KC, 128, FC // 2, 256).transpose(2, 1, 0, 3))


def _lay_d(w):
    return np.ascontiguousarray(w.reshape(FC, 128, KC, 128).transpose(2, 1, 0, 3))


def _fm(v):
    return np.ascontiguousarray(v.reshape(KC, 128).T)


def _core_tokens(x, meta, c):
    b, j = c // 4, c % 4
    blocks = [x[b, 128 * (4 * i + j):128 * (4 * i + j) + 128] for i in range(8)]
    tok = np.concatenate(blocks + [meta], axis=0)
    return np.ascontiguousarray(tok.T)


def _mk_groups():
    g = []
    for hp in range(4):
        g += [("hf%d" % hp, 3664 + 256 * hp, 256), ("hq%d" % hp, 2640 + 256 * hp, 256),
              ("hi%d" % hp, 4688 + 256 * hp, 256)]
    for i in range(4):
        g.append(("hg%d" % i, 5712 + 256 * i, 256))
    g += [("ak", 1024, 256), ("av", 1280, 256), ("ikw", 2560, 80)]
    for i in range(4):
        g.append(("aq%d" % i, 256 * i, 256))
    for i in range(4):
        g.append(("iq%d" % i, 1536 + 256 * i, 256))
    for i in range(8):
        g.append(("ga%d" % i, 6736 + 256 * i, 256))
        g.append(("gh%d" % i, 8784 + 256 * i, 256))
    return g


GROUPS = _mk_groups()


def _lay_win(w):
    out = np.zeros((len(GROUPS), 128, KC, 256), np.float32)
    for gi, (nm, c0, nc_) in enumerate(GROUPS):
        out[gi, :, :, 0:nc_] = w[:, c0:c0 + nc_].reshape(KC, 128, nc_).transpose(1, 0, 2)
    return out


def prepare(inputs, stage):
    f = lambda k: np.asarray(inputs[k], dtype=np.float32)
    x, meta = f("x"), f("meta")
    shared = {
        "wg1": _lay_gu(f("ffn1_w_gate")[0]), "wu1": _lay_gu(f("ffn1_w_up")[0]),
        "wd1": _lay_d(f("ffn1_w_down")[0]),
        "win": _lay_win(f("w_in")[0]),
        "wg2": _lay_gu(f("ffn2_w_gate")[0]), "wu2": _lay_gu(f("ffn2_w_up")[0]),
        "wd2": _lay_d(f("ffn2_w_down")[0]),
        "wba": np.ascontiguousarray(f("w_branch_att")[0].reshape(8, 128, 8, 256).transpose(2, 1, 0, 3)),
        "wbh": np.ascontiguousarray(f("w_branch_hg")[0].reshape(8, 128, 8, 256).transpose(2, 1, 0, 3)),
        "wo": np.ascontiguousarray(f("w_out")[0].reshape(KC, 128, KC, 128).transpose(2, 1, 0, 3)),
    }
    slopes = (2.0 ** -(np.arange(8) + 1.0)).astype(np.float32)
    c = np.arange(4096)
    kpos = np.concatenate([16 + 128 * (4 * ((c % 1024) // 128) + c // 1024) + c % 128, np.arange(16)]).astype(np.float32)
    augk = np.stack([np.floor(kpos / 64.0), kpos % 64.0, np.ones_like(kpos)], 0).astype(np.float32)
    augs = np.stack([np.repeat(64.0 * slopes, 128), np.repeat(slopes, 128)], 0).astype(np.float32)
    shared["augk"] = augk
    shared["augs"] = augs
    cvec = np.zeros((128, 128), np.float32)
    cvec[:, 0:16] = _fm(f("ln1_g")[0]); cvec[:, 16:32] = _fm(f("ln1_b")[0])
    cvec[:, 32:48] = _fm(f("ln2_g")[0]); cvec[:, 48:64] = _fm(f("ln2_b")[0])
    cvec[:, 64:80] = _fm(f("ln3_g")[0]); cvec[:, 80:96] = _fm(f("ln3_b")[0])
    lbl = f("hg_lb_logits")
    cvec[:, 96:104] = lbl[0].reshape(8, 128).T
    cvec[:, 104:112] = lbl[1].reshape(8, 128).T
    cvec[:, 112:120] = f("hg_norm_g")[0].T
    cmat = np.zeros((128, 384), np.float32)
    cmat[:, 0:128] = np.eye(128, dtype=np.float32)
    cmat[:, 128:256] = 1.0
    sidx = np.arange(128)[:, None]; tidx = np.arange(128)[None, :]
    cmat[:, 256:384] = (((sidx <= tidx) & ((sidx // 64) == (tidx // 64))) | ((sidx < 64) & (tidx >= 64))).astype(np.float32)
    shared["cmat"] = cmat
    maps = []
    for c in range(8):
        m = dict(shared)
        m["xT"] = _core_tokens(x, meta, c)
        cv = cvec.copy()
        cv[:, 120 + (c % 4)] = 1.0
        m["cvec"] = cv
        j = c % 4
        p = np.arange(128, dtype=np.float32)
        qpos = np.stack([16 + 128 * (4 * i + j) + p for i in range(8)], 0)
        m["augq"] = np.ascontiguousarray((-slopes[None, :, None] * qpos[:, None, :]).reshape(8, 1024).astype(np.float32))
        catt = np.zeros((128, 224), np.float32)
        catt[:, 0:64] = (-qpos.T[:, :, None] * slopes[None, None, :]).reshape(128, 64)
        catt[:, 64:96] = (2.0 ** -(np.arange(32) + 1.0))[None, :]
        catt[:, 96:160] = f("idx_k_norm_g")[0][None, :]
        catt[:, 160:224] = f("idx_k_norm_b")[0][None, :]
        m["catt"] = catt
        tt = np.arange(128)[:, None, None]; rr = np.arange(4)[None, :, None]; ss = np.arange(128)[None, None, :]
        m["cbt"] = np.where(128 * (rr - j) + (ss - tt) > 0, -1e30, 0.0).astype(np.float32).reshape(128, 512)
        maps.append(m)
    return maps


_NC_CACHE = {}


def kernel(**inputs):
    stage = int(os.environ.get("KSTAGE", "9"))
    if stage not in _NC_CACHE:
        _NC_CACHE[stage] = Builder(stage).build()
    nc = _NC_CACHE[stage]
    maps = prepare(inputs, stage)
    res = run_bass_kernel_spmd(nc, maps, core_ids=list(range(8)))
    if stage == 4:
        return [(r["dbg"], r["dbg2"]) for r in res.results]
    if stage < 6:
        return [r["dbg"] for r in res.results]
    out = np.zeros((2, 4096, D), np.float32)
    for c in range(8):
        b, j = c // 4, c % 4
        o = res.results[c]["outT"]
        for i in range(8):
            g = 4 * i + j
            out[b, 128 * g:128 * g + 128] = o[:, 128 * i:128 * i + 128].T
    return out
```

```python
import os
import numpy as np
import concourse.bass as bass
import concourse.mybir as mybir
from concourse.bass_utils import run_bass_kernel_spmd

F32 = mybir.dt.float32
BF16 = mybir.dt.bfloat16
U8 = mybir.dt.uint8
AF = mybir.ActivationFunctionType
ALU = mybir.AluOpType
AX = mybir.AxisListType

D = 2048
DFF = 5632
NMETA = 16
NREAL = 1024
NT = NREAL + NMETA
KC = D // 128
FC = DFF // 128
TBS = [(0, 512), (512, 1024), (1024, 1040)]
ALPHA = 2.0 ** 0.25
LN_EPS = 1e-5
RMS_EPS = 1e-6

ENGS = ("pe", "act", "dve", "pool", "sp")


class Op:
    __slots__ = ("eng", "fn", "deps", "is_dma", "dsem", "dval", "sig", "sigidx", "waits")

    def __init__(self, eng, fn, is_dma=False):
        self.eng = eng
        self.fn = fn
        self.deps = []
        self.is_dma = is_dma
        self.dsem = None
        self.dval = 0
        self.sig = False
        self.sigidx = 0
        self.waits = []


class Sched:
    def __init__(self, n_dma_sems=40, same_engine_sync=True):
        self.ops = {e: [] for e in ENGS}
        self.lastw = {}
        self.readers = {}
        self.n_dma = 0
        self.n_dma_sems = n_dma_sems
        self.dma_hist = {}
        self.same_engine_sync = same_engine_sync
        self.n_coll = 0

    def _add(self, op, reads, writes):
        deps = set()
        for k in reads:
            w = self.lastw.get(k)
            if w is not None:
                deps.add(w)
        for k in writes:
            w = self.lastw.get(k)
            if w is not None:
                deps.add(w)
            for r in self.readers.get(k, ()):
                deps.add(r)
        op.deps = list(deps)
        for k in reads:
            self.readers.setdefault(k, []).append(op)
        for k in writes:
            self.lastw[k] = op
            self.readers[k] = []
        self.ops[op.eng].append(op)
        return op

    def op(self, eng, fn, reads=(), writes=()):
        return self._add(Op(eng, fn), reads, writes)

    def dma(self, q, fn, reads=(), writes=()):
        op = Op(q, fn, is_dma=True)
        slot = self.n_dma % self.n_dma_sems
        op.dsem = slot
        op.dval = 16 * (self.n_dma // self.n_dma_sems + 1)
        self.n_dma += 1
        self._add(op, reads, writes)
        prev = self.dma_hist.get(slot)
        if prev is not None:
            op.deps.append(prev)
        self.dma_hist[slot] = op
        return op

    def coll(self, fn, reads=(), writes=()):
        op = Op("pool", fn, is_dma=True)
        op.dsem = self.n_dma_sems + self.n_coll
        op.dval = 1
        self.n_coll += 1
        self._add(op, reads, writes)
        self.dma_hist[op.dsem] = op
        return op

    def barrier(self):
        lasts = []
        for e in ENGS:
            for o in reversed(self.ops[e]):
                if not o.is_dma and o.fn is not None:
                    lasts.append(o)
                    break
        dmas = list(self.dma_hist.values())
        for e in ENGS:
            op = Op(e, None)
            op.deps = [o for o in lasts if o.eng != e] + dmas
            self.ops[e].append(op)
        self.lastw = {}
        self.readers = {}

    def _skip(self, d, op):
        return d.eng == op.eng and (d.eng in ("pe", "sp") or not self.same_engine_sync)

    def finalize(self):
        for e in ENGS:
            for op in self.ops[e]:
                for d in op.deps:
                    if not d.is_dma and not self._skip(d, op):
                        d.sig = True
        for e in ENGS:
            c = 0
            for op in self.ops[e]:
                if op.sig:
                    c += 1
                    op.sigidx = c
        for e in ENGS:
            waited = {}
            for op in self.ops[e]:
                need = {}
                for d in op.deps:
                    if d.is_dma:
                        key, val = ("d", d.dsem), d.dval
                    else:
                        if self._skip(d, op):
                            continue
                        key, val = ("e", d.eng), d.sigidx
                    if waited.get(key, 0) >= val:
                        continue
                    if need.get(key, 0) < val:
                        need[key] = val
                for k, v in need.items():
                    waited[k] = v
                op.waits = list(need.items())

    def emit(self, block, esems, dsems):
        self.finalize()
        regs = {"pe": block.tensor, "act": block.scalar, "dve": block.vector,
                "pool": block.gpsimd, "sp": block.sync}
        final = {d.dsem: d.dval for d in self.dma_hist.values()}

        def make(e):
            ops = self.ops[e]

            def body(eng):
                for op in ops:
                    for (kind, which), val in op.waits:
                        eng.wait_ge(dsems[which] if kind == "d" else esems[which], val)
                    if op.fn is None:
                        continue
                    ins = op.fn(eng)
                    if op.is_dma:
                        ins.then_inc(dsems[op.dsem], 16 if op.dsem < self.n_dma_sems else 1)
                    elif op.sig:
                        ins.then_inc(esems[e], 1)
                if e == "sp":
                    for slot, val in final.items():
                        eng.wait_ge(dsems[slot], val)
            return body

        for e in ENGS:
            regs[e](make(e))


ARENA_F32 = 50816
CONST_F32 = 2304


class Builder:
    def __init__(self, stage):
        self.stage = stage
        self.nc = bass.Bass("TRN2", target_bir_lowering=False)
        self.S = Sched()
        self.dram = {}

    def din(self, name, shape, dt=F32):
        self.dram[name] = self.nc.dram_tensor(name, list(shape), dt, kind="ExternalInput").ap()
        return self.dram[name]

    def dout(self, name, shape, dt=F32):
        self.dram[name] = self.nc.dram_tensor(name, list(shape), dt, kind="ExternalOutput").ap()
        return self.dram[name]

    def dscratch(self, name, shape, dt=F32):
        self.dram[name] = self.nc.dram_tensor(name, list(shape), dt, kind="Internal").ap()
        return self.dram[name]

    def view(self, off_bytes, dt, shape):
        n = 1
        for s in shape:
            n *= s
        esz = 4 if dt == F32 else (1 if dt == U8 else 2)
        assert off_bytes % 4 == 0
        nbytes = n * esz
        assert nbytes % 4 == 0
        assert off_bytes + nbytes <= ARENA_F32 * 4, (off_bytes, nbytes)
        v = self.arena[:, off_bytes // 4:(off_bytes + nbytes) // 4]
        if dt != F32:
            v = v.bitcast(dt)
        if len(shape) == 2:
            v = v.rearrange("p (a b) -> p a b", b=shape[1])
        elif len(shape) == 3:
            v = v.rearrange("p (a b c) -> p a b c", b=shape[1], c=shape[2])
        return v

    def ln_apply(self, tag, z, tbs, cg, cb, toff, eps_eff, strm_out=None, alias_key=None, resid_out=None,
                 final_out=None):
        S, ps = self.S, self.ps
        zsq = [self.view(toff + i * 2048, F32, [512]) for i in range(2)]
        meanb = self.view(toff + 4096, F32, [512])
        rstdb = self.view(toff + 6144, F32, [512])
        t1 = [self.view(toff + 8192 + i * 2048, F32, [512]) for i in range(2)]
        t2 = [self.view(toff + 12288 + i * 2048, F32, [512]) for i in range(2)]
        o32 = [self.view(toff + 16384 + i * 2048, F32, [512]) for i in range(2)]
        ones = self.ones_f32
        for ti, (t0, t1e) in enumerate(tbs):
            n = t1e - t0
            pm, pq = ps[:, 6, 0:n], ps[:, 7, 0:n]
            for dc in range(KC):
                zq = zsq[dc % 2]
                S.op("act", lambda e, zq=zq, dc=dc, t0=t0, t1e=t1e, n=n: e.activation(
                    out=zq[:, 0:n], in_=z[:, dc, t0:t1e], func=AF.Square),
                    reads=[(tag, "z", dc, ti)], writes=[(tag, "zsq", dc % 2)])
                S.op("pe", lambda e, pm=pm, dc=dc, t0=t0, t1e=t1e: e.matmul(
                    pm, lhsT=ones, rhs=z[:, dc, t0:t1e], start=(dc == 0), stop=(dc == KC - 1)),
                    reads=[(tag, "z", dc, ti)], writes=[("ps", 6)])
                S.op("pe", lambda e, pq=pq, zq=zq, dc=dc, n=n: e.matmul(
                    pq, lhsT=ones, rhs=zq[:, 0:n], start=(dc == 0), stop=(dc == KC - 1)),
                    reads=[(tag, "zsq", dc % 2)], writes=[("ps", 7)])
            S.op("act", lambda e, pm=pm, n=n: e.activation(out=meanb[:, 0:n], in_=pm, func=AF.Copy),
                 reads=[("ps", 6)], writes=[(tag, "meanb")])
            S.op("dve", lambda e, n=n: e.tensor_tensor(out=rstdb[:, 0:n], in0=meanb[:, 0:n], in1=meanb[:, 0:n],
                                                       op=ALU.mult),
                 reads=[(tag, "meanb")], writes=[(tag, "rstdb")])
            S.op("dve", lambda e, pq=pq, n=n: e.tensor_tensor(out=rstdb[:, 0:n], in0=pq, in1=rstdb[:, 0:n],
                                                              op=ALU.subtract),
                 reads=[("ps", 7), (tag, "rstdb")], writes=[(tag, "rstdb")])
            S.op("act", lambda e, n=n: e.activation(out=rstdb[:, 0:n], in_=rstdb[:, 0:n], func=AF.Sqrt,
                                                    bias=self.eps_cols[eps_eff], scale=1.0),
                 reads=[(tag, "rstdb")], writes=[(tag, "rstdb")])
            S.op("dve", lambda e, n=n: e.reciprocal(out=rstdb[:, 0:n], in_=rstdb[:, 0:n]),
                 reads=[(tag, "rstdb")], writes=[(tag, "rstdb")])
            for dc in range(KC):
                a, b_, o = t1[dc % 2], t2[dc % 2], o32[dc % 2]
                S.op("pool", lambda e, a=a, dc=dc, t0=t0, t1e=t1e, n=n: e.tensor_tensor(
                    out=a[:, 0:n], in0=z[:, dc, t0:t1e], in1=meanb[:, 0:n], op=ALU.subtract),
                    reads=[(tag, "z", dc, ti), (tag, "meanb")], writes=[(tag, "t1", dc % 2)])
                S.op("dve", lambda e, a=a, b_=b_, n=n: e.tensor_tensor(
                    out=b_[:, 0:n], in0=a[:, 0:n], in1=rstdb[:, 0:n], op=ALU.mult),
                    reads=[(tag, "t1", dc % 2), (tag, "rstdb")], writes=[(tag, "t2", dc % 2)])
                S.op("act", lambda e, b_=b_, o=o, dc=dc, n=n: e.activation(
                    out=o[:, 0:n], in_=b_[:, 0:n], func=AF.Identity,
                    bias=cb[:, dc:dc + 1], scale=cg[:, dc:dc + 1]),
                    reads=[(tag, "t2", dc % 2)], writes=[(tag, "o32", dc % 2)])
                if strm_out is not None:
                    wk = [(tag, "sout", dc, ti)] + ([(tag, alias_key, dc, ti)] if alias_key else [])
                    S.op("act", lambda e, b_=b_, dc=dc, t0=t0, t1e=t1e, n=n: e.activation(
                        out=strm_out[:, dc, t0:t1e], in_=b_[:, 0:n], func=AF.Identity,
                        bias=cb[:, dc:dc + 1], scale=cg[:, dc:dc + 1]),
                        reads=[(tag, "t2", dc % 2)], writes=wk)
                dst = resid_out if final_out is None else final_out
                if final_out is None or t0 < NREAL:
                    S.dma("sp", lambda e, o=o, dc=dc, t0=t0, t1e=t1e, n=n, dst=dst: e.dma_start(
                        out=dst[dc * 128:(dc + 1) * 128, t0:t1e], in_=o[:, 0:n]),
                        reads=[(tag, "o32", dc % 2)])

    def ffn_phase(self, tag, ntok, tbs, strm_in, strm_out_off, wg, wu, wd, resid, cg, cb,
                  resid_out=None, final_out=None):
        S, nc = self.S, self.nc
        ps = self.ps
        C_OFF = 33280
        B_OFF = 66560
        D_OFF = B_OFF + FC * NT * 2
        T_OFF = D_OFF + 2 * FC * 128 * 2
        hT = self.view(B_OFF, BF16, [FC, ntok])
        wgu = [self.view(C_OFF + i * 16384, BF16, [2, KC, 256]) for i in range(2)]
        wdb = [self.view(D_OFF + i * FC * 128 * 2, BF16, [FC, 128]) for i in range(2)]
        z = self.view(0, F32, [KC, ntok])
        sil = [self.view(T_OFF + 8192 + i * 2048, F32, [512]) for i in range(2)]
        strm_out = self.view(strm_out_off, BF16, [KC, ntok]) if final_out is None else None
        c_scale = 0.5 / ALPHA
        eps_eff = LN_EPS / (ALPHA * ALPHA)
        nb = len(tbs)

        for g in range(FC // 2):
            wb = wgu[g % 2]
            kb = (tag, "wgu", g % 2)
            S.dma("pool", lambda e, wb=wb, g=g: e.dma_start(out=wb[:, 0], in_=wg[g]), writes=[(kb, 0)])
            S.dma("pool", lambda e, wb=wb, g=g: e.dma_start(out=wb[:, 1], in_=wu[g]), writes=[(kb, 1)])
            for fcl in range(2):
                fc = 2 * g + fcl
                for ti, (t0, t1e) in enumerate(tbs):
                    n = t1e - t0
                    pb = (fc * nb + ti) % 2
                    pg, pu = ps[:, 2 * pb, 0:n], ps[:, 2 * pb + 1, 0:n]
                    for k in range(KC):
                        S.op("pe", lambda e, pg=pg, wb=wb, k=k, fcl=fcl, t0=t0, t1e=t1e: e.matmul(
                            pg, lhsT=wb[:, 0, k, fcl * 128:(fcl + 1) * 128], rhs=strm_in[:, k, t0:t1e],
                            start=(k == 0), stop=(k == KC - 1)),
                            reads=[(kb, 0), (tag, "sin")], writes=[("ps", 2 * pb)])
                    for k in range(KC):
                        S.op("pe", lambda e, pu=pu, wb=wb, k=k, fcl=fcl, t0=t0, t1e=t1e: e.matmul(
                            pu, lhsT=wb[:, 1, k, fcl * 128:(fcl + 1) * 128], rhs=strm_in[:, k, t0:t1e],
                            start=(k == 0), stop=(k == KC - 1)),
                            reads=[(kb, 1), (tag, "sin")], writes=[("ps", 2 * pb + 1)])
                    sb = sil[pb]
                    S.op("act", lambda e, sb=sb, pg=pg, n=n: e.activation(out=sb[:, 0:n], in_=pg, func=AF.Silu),
                         reads=[("ps", 2 * pb)], writes=[(tag, "sil", pb)])
                    S.op("dve", lambda e, sb=sb, pu=pu, n=n, fc=fc, t0=t0, t1e=t1e: e.tensor_tensor(
                        out=hT[:, fc, t0:t1e], in0=sb[:, 0:n], in1=pu, op=ALU.mult),
                        reads=[(tag, "sil", pb), ("ps", 2 * pb + 1)], writes=[(tag, "hT", fc, ti)])

        S.barrier()
        for dc in range(KC):
            wb = wdb[dc % 2]
            kb = (tag, "wd", dc % 2)
            S.dma("pool", lambda e, wb=wb, dc=dc: e.dma_start(out=wb, in_=wd[dc]), writes=[kb])
            S.dma("sp", lambda e, dc=dc: e.dma_start(out=z[:, dc, :], in_=resid[dc * 128:(dc + 1) * 128, 0:ntok]),
                  writes=[(tag, "z", dc, ti) for ti in range(nb)])
            for ti, (t0, t1e) in enumerate(tbs):
                n = t1e - t0
                pb = 4 + (dc * nb + ti) % 2
                py = ps[:, pb, 0:n]
                for f in range(FC):
                    S.op("pe", lambda e, py=py, wb=wb, f=f, t0=t0, t1e=t1e: e.matmul(
                        py, lhsT=wb[:, f, :], rhs=hT[:, f, t0:t1e], start=(f == 0), stop=(f == FC - 1)),
                        reads=[kb, (tag, "hT", f, ti)], writes=[("ps", pb)])
                S.op("dve", lambda e, py=py, dc=dc, t0=t0, t1e=t1e: e.scalar_tensor_tensor(
                    out=z[:, dc, t0:t1e], in0=py, scalar=c_scale, in1=z[:, dc, t0:t1e],
                    op0=ALU.mult, op1=ALU.add),
                    reads=[("ps", pb), (tag, "z", dc, ti)], writes=[(tag, "z", dc, ti)])
        self.ln_apply(tag, z, tbs, cg, cb, T_OFF, eps_eff, strm_out=strm_out, alias_key="hT",
                      resid_out=resid_out, final_out=final_out)
        S.barrier()

    def proj_fm(self, tag, strm, gi, wbufs, tbs, evac, banks=(0, 1), parity=[0]):
        S, ps = self.S, self.ps
        wb = wbufs[parity[0] % 2]
        kb = ("wb", parity[0] % 2)
        parity[0] += 1
        S.dma("pool", lambda e, wb=wb, gi=gi: e.dma_start(out=wb, in_=self.win[gi]), writes=[kb])
        cnt = 0
        for half in range(2):
            for ti, (t0, t1e) in enumerate(tbs):
                n = t1e - t0
                bk = banks[cnt % len(banks)]
                cnt += 1
                pv = ps[:, bk, 0:n]
                for k in range(KC):
                    S.op("pe", lambda e, pv=pv, wb=wb, k=k, half=half, t0=t0, t1e=t1e: e.matmul(
                        pv, lhsT=wb[:, k, half * 128:(half + 1) * 128], rhs=strm[:, k, t0:t1e],
                        start=(k == 0), stop=(k == KC - 1)),
                        reads=[kb, "strm"], writes=[("ps", bk)])
                evac(half, ti, t0, t1e, pv, bk)

    def proj_tm(self, tag, strm, gi, wbufs, tls, ncols, evac, banks=(0, 1), parity=[0]):
        S, ps = self.S, self.ps
        wb = wbufs[parity[0] % 2]
        kb = ("wb", parity[0] % 2)
        parity[0] += 1
        S.dma("pool", lambda e, wb=wb, gi=gi: e.dma_start(out=wb, in_=self.win[gi]), writes=[kb])
        for cnt, (i, c0, n) in enumerate(tls):
            bk = banks[cnt % len(banks)]
            pv = ps[0:n, bk, 0:ncols]
            for k in range(KC):
                S.op("pe", lambda e, pv=pv, wb=wb, k=k, c0=c0, n=n: e.matmul(
                    pv, lhsT=strm[:, k, c0:c0 + n], rhs=wb[:, k, 0:ncols],
                    start=(k == 0), stop=(k == KC - 1)),
                    reads=[kb, "strm"], writes=[("ps", bk)])
            evac(i, c0, n, pv, bk)

    def hgrn_m1(self, strm):
        S, ps, nc = self.S, self.ps, self.nc
        R1 = 99840
        wbufs = [self.view(R1 + i * 8192, BF16, [KC, 256]) for i in range(2)]
        off = [R1 + 16384]

        def alloc(dt, shape):
            n = 1
            for x in shape:
                n *= x
            nb = n * (4 if dt == F32 else 2)
            nb = (nb + 63) // 64 * 64
            v = self.view(off[0], dt, shape)
            off[0] += nb
            return v
        logf = alloc(F32, [2, NT]); Bg = alloc(F32, [2, NT]); Bsh = alloc(F32, [2, NT])
        tA = alloc(F32, [2, NT]); tB = alloc(F32, [2, NT])
        kk = alloc(BF16, [2, NT]); qs = alloc(BF16, [2, NT]); qt = alloc(BF16, [2, NT])
        kt = alloc(BF16, [2, NT]); kh64 = alloc(BF16, [2, NT]); kh128 = alloc(BF16, [2, NT])
        vv = alloc(BF16, [9, 256])
        PT = alloc(BF16, [2, 128]); khT = alloc(BF16, [2, 128])
        xs_st = alloc(BF16, [9, 256]); xa_st = alloc(F32, [9, 2])
        Qc = self.view(0, BF16, [8, NREAL]); oloc = self.view(16384, BF16, [8, NREAL])
        cv = self.cv
        lbc, omlc = self.lbc, self.omlc
        psb = lambda bk: ps[:, bk, :].bitcast(BF16)
        TL = [(i, 128 * i, 128) for i in range(8)] + [(8, NREAL, NMETA)]
        flat = lambda v: v.rearrange("p h t -> p (h t)")
        r64 = lambda v: v[:, :, 0:NREAL].rearrange("p h (c t) -> p h c t", t=64)
        r128 = lambda v: v[:, :, 0:NREAL].rearrange("p h (c t) -> p h c t", t=128)
        mt = lambda v: v[:, :, NREAL:NT]

        for hp in range(4):
            T = ("m1", hp)
            def ev_f(half, ti, t0, t1e, pv, bk, hp=hp):
                h = 2 * hp + half
                S.op("act", lambda e: e.activation(out=tA[:, half, t0:t1e], in_=pv, func=AF.Sigmoid),
                     reads=[("ps", bk)], writes=[("tA", half, ti)])
                S.op("dve", lambda e: e.tensor_scalar(out=tA[:, half, t0:t1e], in0=tA[:, half, t0:t1e],
                                                      scalar1=omlc[:, h:h + 1], scalar2=lbc[:, h:h + 1],
                                                      op0=ALU.mult, op1=ALU.add),
                     reads=[("tA", half, ti), "lb"], writes=[("tA", half, ti)])
                S.op("act", lambda e: e.activation(out=logf[:, half, t0:t1e], in_=tA[:, half, t0:t1e], func=AF.Ln),
                     reads=[("tA", half, ti)], writes=[("logf", half, ti)])
                S.op("pool", lambda e: e.tensor_scalar(out=kk[:, half, t0:t1e], in0=tA[:, half, t0:t1e],
                                                       scalar1=-1.0, scalar2=1.0, op0=ALU.mult, op1=ALU.add),
                     reads=[("tA", half, ti)], writes=[("kk", half, ti)])
            self.proj_fm(T, strm, self.gidx["hf%d" % hp], wbufs, TBS, ev_f)

            def ev_q(half, ti, t0, t1e, pv, bk):
                S.op("act", lambda e: e.activation(out=qs[:, half, t0:t1e], in_=pv, func=AF.Silu),
                     reads=[("ps", bk)], writes=[("qs", half, ti)])
            self.proj_fm(T, strm, self.gidx["hq%d" % hp], wbufs, TBS, ev_q)

            def ev_v(i, c0, n, pv, bk):
                S.op("act", lambda e: e.activation(out=vv[0:n, i, :], in_=pv, func=AF.Copy),
                     reads=[("ps", bk)], writes=[("vv", i)])
            self.proj_tm(T, strm, self.gidx["hi%d" % hp], wbufs, TL, 256, ev_v)

            allk = lambda nm: [(nm, hl, ti) for hl in range(2) for ti in range(3)]
            S.op("dve", lambda e: e.tensor_tensor_scan(out=flat(Bg), data0=flat(logf), data1=flat(logf),
                                                       initial=0.0, op0=ALU.add, op1=ALU.min),
                 reads=allk("logf"), writes=["Bg"])
            S.op("pool", lambda e: e.memset(flat(Bsh)[:, 0:1], 0.0), writes=["Bsh0"])
            S.op("pool", lambda e: e.tensor_copy(out=flat(Bsh)[:, 1:2 * NT], in_=flat(Bg)[:, 0:2 * NT - 1]),
                 reads=["Bg"], writes=["Bsh"])
            S.op("dve", lambda e: e.tensor_tensor(out=r64(tB), in0=r64(Bg),
                                                  in1=r64(Bsh)[:, :, :, 0:1].to_broadcast([128, 2, 16, 64]),
                                                  op=ALU.subtract),
                 reads=["Bg", "Bsh", "Bsh0"], writes=["tBr"])
            S.op("dve", lambda e: e.tensor_tensor(out=mt(tB), in0=mt(Bg),
                                                  in1=mt(Bsh)[:, :, 0:1].to_broadcast([128, 2, NMETA]),
                                                  op=ALU.subtract),
                 reads=["Bg", "Bsh", "Bsh0"], writes=["tBm"])
            S.op("pool", lambda e: e.tensor_tensor(out=r128(tA), in0=r128(Bg),
                                                   in1=r128(Bsh)[:, :, :, 0:1].to_broadcast([128, 2, 8, 128]),
                                                   op=ALU.subtract),
                 reads=["Bg", "Bsh", "Bsh0"] + allk("tA"), writes=["tAr"] + allk("tA"))
            S.op("pool", lambda e: e.tensor_copy(out=mt(tA), in_=mt(tB)),
                 reads=["tBm"], writes=["tAm"])
            TBk, TAk = ["tBr", "tBm"], ["tAr", "tAm"] + allk("tA")
            S.op("act", lambda e: e.activation(out=flat(Bg), in_=flat(tB), func=AF.Exp),
                 reads=TBk + ["Bsh", "tAr"], writes=["Bg"])
            S.op("dve", lambda e: e.tensor_tensor(out=flat(qt), in0=flat(qs), in1=flat(Bg), op=ALU.mult),
                 reads=["Bg"] + allk("qs"), writes=["qt"])
            S.op("act", lambda e: e.activation(out=flat(Bsh), in_=flat(tB), func=AF.Exp, scale=-1.0),
                 reads=TBk + ["Bsh", "tAr", "Bsh0"], writes=["Bsh", "Bsh0"])
            S.op("dve", lambda e: e.tensor_tensor(out=flat(kt), in0=flat(kk), in1=flat(Bsh), op=ALU.mult),
                 reads=["Bsh"] + allk("kk"), writes=["kt"])
            S.op("dve", lambda e: e.tensor_tensor(out=r64(Bg), in0=r64(tB),
                                                  in1=r64(tB)[:, :, :, 63:64].to_broadcast([128, 2, 16, 64]),
                                                  op=ALU.subtract),
                 reads=TBk + ["qt"], writes=["Bg"])
            S.op("act", lambda e: e.activation(out=r64(Bg), in_=r64(Bg), func=AF.Exp, scale=-1.0),
                 reads=["Bg"], writes=["Bg"])
            S.op("dve", lambda e: e.tensor_tensor(out=r64(kh64), in0=r64(kk), in1=r64(Bg), op=ALU.mult),
                 reads=["Bg"] + allk("kk"), writes=["kh64"])
            S.op("act", lambda e: e.activation(out=flat(Bsh), in_=flat(tA), func=AF.Exp),
                 reads=TAk + ["kt"], writes=["Bsh"])
            S.op("dve", lambda e, hp=hp: e.tensor_tensor(out=Qc[:, 2 * hp:2 * hp + 2, :], in0=qs[:, :, 0:NREAL],
                                                         in1=Bsh[:, :, 0:NREAL], op=ALU.mult),
                 reads=["Bsh"] + allk("qs"), writes=[("Qc", hp)])
            S.op("pool", lambda e: e.tensor_copy(out=xa_st[:, 0:8, :].rearrange("p i h -> p h i"),
                                                 in_=r128(Bsh)[:, :, :, 127]),
                 reads=["Bsh"], writes=["xa_st"])
            S.op("pool", lambda e: e.tensor_copy(out=xa_st[:, 8, :], in_=Bsh[:, :, NT - 1]),
                 reads=["Bsh"], writes=["xa_st"])
            S.op("dve", lambda e: e.tensor_tensor(out=r128(Bg), in0=r128(tA),
                                                  in1=r128(tA)[:, :, :, 127:128].to_broadcast([128, 2, 8, 128]),
                                                  op=ALU.subtract),
                 reads=TAk + ["kh64"], writes=["Bg"])
            S.op("dve", lambda e: e.tensor_tensor(out=mt(Bg), in0=mt(tA),
                                                  in1=mt(tA)[:, :, NMETA - 1:NMETA].to_broadcast([128, 2, NMETA]),
                                                  op=ALU.subtract),
                 reads=TAk + ["kh64"], writes=["Bg"])
            S.op("act", lambda e: e.activation(out=flat(Bg), in_=flat(Bg), func=AF.Exp, scale=-1.0),
                 reads=["Bg"], writes=["Bg"])
            S.op("dve", lambda e: e.tensor_tensor(out=flat(kh128), in0=flat(kk), in1=flat(Bg), op=ALU.mult),
                 reads=["Bg"] + allk("kk"), writes=["kh128"])

            for (i, c0, n) in TL:
                if n == 128:
                    for hl in range(2):
                        o0 = hl * 128
                        S.op("pe", lambda e, hl=hl, o0=o0, c0=c0: e.matmul(
                            ps[:, 2, o0:o0 + 64], lhsT=kt[:, hl, c0:c0 + 128], rhs=qt[:, hl, c0:c0 + 64],
                            start=True, stop=True), reads=["kt", "qt"], writes=[("ps", 2)])
                        S.op("pe", lambda e, hl=hl, o0=o0, c0=c0: e.matmul(
                            ps[0:64, 2, o0 + 64:o0 + 128], lhsT=kh64[:, hl, c0:c0 + 64],
                            rhs=qt[:, hl, c0 + 64:c0 + 128], start=True, stop=True),
                            reads=["kh64", "qt"], writes=[("ps", 2)])
                        S.op("pe", lambda e, hl=hl, o0=o0, c0=c0: e.matmul(
                            ps[64:128, 2, o0 + 64:o0 + 128], lhsT=kt[:, hl, c0 + 64:c0 + 128],
                            rhs=qt[:, hl, c0 + 64:c0 + 128], start=True, stop=True),
                            reads=["kt", "qt"], writes=[("ps", 2)])
                    for hl in range(2):
                        S.op("dve", lambda e, hl=hl: e.tensor_tensor(
                            out=PT[:, hl, :], in0=ps[:, 2, hl * 128:(hl + 1) * 128], in1=self.mask2, op=ALU.mult),
                            reads=[("ps", 2), "mask2"], writes=[("PT", hl)])
                    for hl in range(2):
                        S.op("pe", lambda e, hl=hl, i=i: e.matmul(
                            ps[:, 3, hl * 128:(hl + 1) * 128], lhsT=vv[:, i, hl * 128:(hl + 1) * 128],
                            rhs=PT[:, hl, :], start=True, stop=True),
                            reads=[("vv", i), ("PT", hl)], writes=[("ps", 3)])
                    S.op("act", lambda e, hp=hp, c0=c0: e.activation(
                        out=oloc[:, 2 * hp:2 * hp + 2, c0:c0 + 128],
                        in_=ps[:, 3, 0:256].rearrange("p (h t) -> p h t", h=2), func=AF.Copy),
                        reads=[("ps", 3)], writes=[("oloc", hp, i)])
                for hl in range(2):
                    S.op("pe", lambda e, hl=hl, c0=c0, n=n: e.transpose(
                        out=psb(4)[0:n, hl * 128:(hl + 1) * 128], in_=kh128[:, hl, c0:c0 + n],
                        identity=self.ident_bf),
                        reads=["kh128", "ident"], writes=[("ps", 4)])
                S.op("dve", lambda e, n=n: e.tensor_copy(out=khT[0:n].rearrange("p h d -> p (h d)"),
                                                         in_=psb(4)[0:n, 0:256]),
                     reads=[("ps", 4)], writes=["khT"])
                for hl in range(2):
                    S.op("pe", lambda e, hl=hl, i=i, n=n: e.matmul(
                        ps[:, 5, hl * 128:(hl + 1) * 128], lhsT=khT[0:n, hl, :],
                        rhs=vv[0:n, i, hl * 128:(hl + 1) * 128], start=True, stop=True),
                        reads=["khT", ("vv", i)], writes=[("ps", 5)])
                S.op("dve", lambda e, i=i: e.tensor_copy(out=xs_st[:, i, :], in_=ps[:, 5, 0:256]),
                     reads=[("ps", 5)], writes=[("xs_st", i)])
            for q3 in range(3):
                S.dma("sp", lambda e, hp=hp, q3=q3: e.dma_start(
                    out=self.xs[q3].rearrange("p (i c) -> p i c", i=3)[:, :, hp * 256:(hp + 1) * 256],
                    in_=xs_st[:, 3 * q3:3 * q3 + 3, :]),
                    reads=[("xs_st", i) for i in range(9)], writes=[("xs", hp, q3)])
            S.dma("sp", lambda e, hp=hp: e.dma_start(
                out=self.xa.rearrange("p (i c) -> p i c", i=9)[:, :, 2 * hp:2 * hp + 2], in_=xa_st),
                reads=["xa_st"], writes=[("xa", hp)])
        S.barrier()
        rg = [[0, 1, 2, 3], [4, 5, 6, 7]]
        for q3 in range(3):
            S.coll(lambda e, q3=q3: e.collective_compute("AllGather", ALU.bypass, replica_groups=rg,
                                                         ins=[self.xs[q3]], outs=[self.xg[q3]]), writes=[("xg", q3)])
        S.coll(lambda e: e.collective_compute("AllGather", ALU.bypass, replica_groups=rg,
                                              ins=[self.xa], outs=[self.xag]), writes=["xag"])

    def hgrn_m2(self, strm):
        S, ps, nc = self.S, self.ps, self.nc
        R1 = 99840
        wbufs = [self.view(R1 + i * 8192, BF16, [KC, 256]) for i in range(2)]
        off = [R1 + 16384]

        def alloc(dt, shape):
            n = 1
            for x in shape:
                n *= x
            nb = n * (4 if dt == F32 else 2)
            nb = (nb + 63) // 64 * 64
            v = self.view(off[0], dt, shape)
            off[0] += nb
            return v
        sgate = alloc(BF16, [8, NREAL])
        Scur = alloc(F32, [8, 128]); SmF = alloc(F32, [8, 128])
        SAb = [alloc(BF16, [8, 128]) for _ in range(3)]
        Aall = alloc(F32, [4, 72])
        OF = alloc(F32, [8, 128]); OSQ = alloc(F32, [8, 128]); RS = alloc(F32, [8, 128])
        Qc = self.view(0, BF16, [8, NREAL]); oloc = self.view(16384, BF16, [8, NREAL])
        yhg = self.view(32768, BF16, [8, NREAL]); Smine = self.view(49152, BF16, [8, 8, 128])
        f2 = lambda v: v.rearrange("p h t -> p (h t)")
        RTB = TBS[0:2]

        for g4 in range(4):
            def ev_g(half, ti, t0, t1e, pv, bk, g4=g4):
                h = 2 * g4 + half
                S.op("act", lambda e: e.activation(out=sgate[:, h, t0:t1e], in_=pv, func=AF.Silu),
                     reads=[("ps", bk)], writes=[("sgate", h, ti)])
                S.op("pool", lambda e: e.tensor_scalar(out=sgate[:, h, t0:t1e], in0=sgate[:, h, t0:t1e],
                                                       scalar1=self.gnc[:, h:h + 1], scalar2=None, op0=ALU.mult),
                     reads=[("sgate", h, ti), "gn"], writes=[("sgate", h, ti)])
            self.proj_fm("m2", strm, self.gidx["hg%d" % g4], wbufs, RTB, ev_g)

        S.dma("sp", lambda e: e.dma_start(out=Aall, in_=self.xag.rearrange("(r p) c -> p r c", p=128)),
              reads=["xag"], writes=["Aall"])
        xg3 = [x_.rearrange("(r p) (i c) -> r p i c", p=128, i=3) for x_ in self.xg]
        S.dma("sp", lambda e: e.dma_start(out=f2(SAb[2]), in_=xg3[2][0, :, 2, :]), reads=[("xg", 2)],
              writes=[("SAb", 2)])
        S.op("dve", lambda e: e.tensor_copy(out=f2(Scur), in_=f2(SAb[2])), reads=[("SAb", 2)], writes=["Scur"])
        for g in range(32):
            r, i = g % 4, g // 4
            sb = SAb[g % 3]
            S.dma("sp", lambda e, sb=sb, r=r, i=i: e.dma_start(out=f2(sb), in_=xg3[i // 3][r, :, i % 3, :]),
                  reads=[("xg", i // 3)], writes=[("SAb", g % 3)])
            if r == 0:
                S.op("dve", lambda e: e.tensor_scalar(out=f2(SmF), in0=f2(Scur), scalar1=self.selc[:, 0:1],
                                                      scalar2=None, op0=ALU.mult),
                     reads=["Scur", "sel"], writes=["SmF"])
            else:
                dst = SmF if r < 3 else Smine[:, i]
                S.op("dve", lambda e, r=r, dst=dst: e.scalar_tensor_tensor(
                    out=f2(dst), in0=f2(Scur), scalar=self.selc[:, r:r + 1], in1=f2(SmF),
                    op0=ALU.mult, op1=ALU.add),
                    reads=["Scur", "sel", "SmF"], writes=(["SmF"] if r < 3 else [("Smine", i)]))
            if g < 31:
                for h in range(8):
                    S.op("dve", lambda e, h=h, r=r, i=i, sb=sb: e.scalar_tensor_tensor(
                        out=Scur[:, h, :], in0=Scur[:, h, :], scalar=Aall[:, r, i * 8 + h:i * 8 + h + 1],
                        in1=sb[:, h, :], op0=ALU.mult, op1=ALU.add),
                        reads=["Scur", "Aall", ("SAb", g % 3)], writes=["Scur"])

        for i in range(8):
            c0 = 128 * i
            for h in range(8):
                bk = 2 + h // 4
                S.op("pe", lambda e, h=h, i=i, c0=c0, bk=bk: e.matmul(
                    ps[:, bk, (h % 4) * 128:(h % 4 + 1) * 128], lhsT=Smine[:, i, h, :], rhs=Qc[:, h, c0:c0 + 128],
                    start=True, stop=True),
                    reads=[("Smine", i), "Qc"], writes=[("ps", bk)])
            S.op("dve", lambda e, c0=c0: e.tensor_tensor(
                out=OF, in0=ps[:, 2:4, :].rearrange("p a (h t) -> p (a h) t", h=4), in1=oloc[:, :, c0:c0 + 128],
                op=ALU.add),
                reads=[("ps", 2), ("ps", 3), "oloc"], writes=["OF"])
            S.op("act", lambda e: e.activation(out=f2(OSQ), in_=f2(OF), func=AF.Square),
                 reads=["OF"], writes=["OSQ"])
            for a in range(2):
                S.op("pe", lambda e, a=a: e.matmul(ps[:, 4 + a, :], lhsT=self.ones128, rhs=f2(OSQ)[:, a * 512:(a + 1) * 512],
                                                   start=True, stop=True),
                     reads=["OSQ", "ones128"], writes=[("ps", 4 + a)])
            S.op("act", lambda e: e.activation(out=f2(RS), in_=ps[:, 4:6, :].rearrange("p a b -> p (a b)"),
                                               func=AF.Ln, bias=self.eps_rms, scale=1.0),
                 reads=[("ps", 4), ("ps", 5), "eps"], writes=["RS"])
            S.op("act", lambda e: e.activation(out=f2(RS), in_=f2(RS), func=AF.Exp, scale=-0.5),
                 reads=["RS"], writes=["RS"])
            S.op("dve", lambda e: e.tensor_tensor(out=f2(OF), in0=f2(OF), in1=f2(RS), op=ALU.mult),
                 reads=["OF", "RS"], writes=["OF"])
            S.op("dve", lambda e, c0=c0: e.tensor_tensor(out=yhg[:, :, c0:c0 + 128], in0=OF, in1=sgate[:, :, c0:c0 + 128],
                                                         op=ALU.mult),
                 reads=["OF"] + [("sgate", h, c0 // 512) for h in range(8)], writes=[("yhg", i)])
        S.barrier()

    def attn_m3(self, strm):
        S, ps, nc = self.S, self.ps, self.nc
        R1 = 99840
        psb = lambda bk: ps[:, bk, :].bitcast(BF16)
        K_all = self.view(R1, BF16, [2, 4112])
        V_all = self.view(R1 + 16448, BF16, [33, 258])
        IK_all = self.view(R1 + 33536, BF16, [4096])
        AugK = self.view(R1 + 41728, BF16, [4112])
        qT = self.view(R1 + 49952, BF16, [8, NREAL])
        iqT = self.view(R1 + 66336, BF16, [8, NREAL])
        sc = self.view(R1 + 82720, F32, [4096])
        wbufs = [self.view(R1 + 82720 + i * 8192, BF16, [KC, 256]) for i in range(2)]
        Dg = self.view(R1 + 99104, BF16, [16, 128])
        yatt = self.view(0, BF16, [8, NREAL])
        mb = self.view(16384, BF16, [4096])
        mbT = self.view(24576, BF16, [32, 128])
        junk = self.view(49152, U8, [4096])
        iqz = self.view(49152 + 4096, BF16, [16, 128])
        rh = [self.view(57344 + q * 1024, BF16, [512]) for q in range(4)]
        ya = self.view(61440, BF16, [8, 128])
        PTb = [self.view(61440 + q * 2048, BF16, [1024]) for q in range(2)]
        cbt = self.view(65536, BF16, [4, 128])
        kst = self.view(49152, BF16, [2, NREAL])
        vst = self.view(49152 + 4096, BF16, [8, 258])
        ikst = self.view(49152 + 4096 + 4160, BF16, [NREAL])
        iktmp = self.view(49152 + 10304, F32, [64])
        ikn2 = self.view(49152 + 10304 + 256, BF16, [128])
        cst = self.cst
        AugQ = cst[:, 664:1176].bitcast(BF16)
        AugR = cst[:, 1176:1688].bitcast(BF16)
        wq = cst[:, 1688:1816].rearrange("p (i h) -> p i h", h=16)
        H = cst[:, 1816:1848]
        mx = cst[:, 1848:2008].rearrange("p (h c) -> p h c", h=8)
        mrow = cst[:, 2008:2016]; cc = cst[:, 2016:2024]; rs = cst[:, 2024:2032]
        Bt = cst[:, 2032:2033]; Wc = cst[:, 2033:2034]; mid = cst[:, 2034:2035]; cnt = cst[:, 2035:2036]
        u2 = cst[:, 2036:2037]; tau = cst[:, 2037:2038]; rstd1 = cst[:, 2038:2039]
        AQc = cst[:, 2040:2104].rearrange("p (i h) -> p i h", h=8)
        pw = cst[:, 2104:2136]
        gik = cst[:, 2136:2200]; bik = cst[:, 2200:2264]
        st6 = cst[:, 2264:2270]; mv = cst[:, 2270:2272]
        TL = [(i, 128 * i, 128) for i in range(8)] + [(8, NREAL, NMETA)]
        RTB = TBS[0:2]
        NB = 20

        S.dma("sp", lambda e: e.dma_start(out=cst[:, 2040:2264], in_=self.catt), writes=["catt"])
        S.op("pool", lambda e: e.memset(AugK[0:65, :], 0.0), writes=["AugK"])
        S.op("pool", lambda e: e.memset(AugQ[0:65, :], 0.0), writes=["AugQ"])
        S.op("pool", lambda e: e.memset(AugR[0:65, :], 0.0), writes=["AugR"])
        for rr in range(3):
            S.dma("pool", lambda e, rr=rr: e.dma_start(out=AugK[32 * rr:32 * rr + 1, :], in_=self.augk[rr:rr + 1, :]),
                  writes=["AugK"])
        for rr in range(2):
            S.dma("pool", lambda e, rr=rr: e.dma_start(out=AugQ[32 * rr:32 * rr + 1, :], in_=self.augs[rr:rr + 1, :]),
                  writes=["AugQ"])
            S.dma("pool", lambda e, rr=rr: e.dma_start(out=AugR[32 * rr:32 * rr + 1, :], in_=self.augs[rr:rr + 1, :]),
                  writes=["AugR"])
        S.dma("pool", lambda e: e.dma_start(out=cbt, in_=self.cbt_d.rearrange("p (r s) -> p r s", r=4)), writes=["cbt"])
        S.op("dve", lambda e: e.memset(vst.rearrange("p i (k c) -> p i k c", k=2)[:, :, :, 128:129], 1.0),
             writes=["vst1"])
        S.op("dve", lambda e: e.memset(V_all[:, 32, :].rearrange("p (k c) -> p k c", k=2)[:, :, 128:129], 1.0),
             writes=["V1"])

        def ev_k(half, ti, t0, t1e, pv, bk):
            if ti < 2:
                S.op("act", lambda e: e.activation(out=kst[:, half, t0:t1e], in_=pv, func=AF.Copy),
                     reads=[("ps", bk)], writes=[("kst", half, ti)])
            else:
                S.op("act", lambda e: e.activation(out=K_all[:, half, 4096:4112], in_=pv, func=AF.Copy),
                     reads=[("ps", bk)], writes=[("Kmeta", half)])
        self.proj_fm("m3", strm, self.gidx["ak"], wbufs, TBS, ev_k)

        def ev_v(i, c0, n, pv, bk):
            src = pv.rearrange("p (k c) -> p k c", k=2)
            if i < 8:
                dst = vst[:, i, :].rearrange("p (k c) -> p k c", k=2)[:, :, 0:128]
                S.op("act", lambda e: e.activation(out=dst, in_=src, func=AF.Copy),
                     reads=[("ps", bk), "vst1"], writes=[("vst", i)])
            else:
                dst = V_all[0:n, 32, :].rearrange("p (k c) -> p k c", k=2)[:, :, 0:128]
                S.op("act", lambda e: e.activation(out=dst, in_=src, func=AF.Copy),
                     reads=[("ps", bk), "V1"], writes=["Vmeta"])
        self.proj_tm("m3", strm, self.gidx["av"], wbufs, TL, 256, ev_v)

        def ev_ik(i, c0, n, pv, bk):
            S.op("dve", lambda e: e.bn_stats(out=st6, in_=pv[:, 0:64]), reads=[("ps", bk)], writes=["st6"])
            S.op("dve", lambda e: e.bn_aggr(out=mv, in_=st6), reads=["st6"], writes=["mv"])
            S.op("act", lambda e: e.activation(out=rstd1, in_=mv[:, 1:2], func=AF.Sqrt, bias=self.eps_ik, scale=1.0),
                 reads=["mv", "eps"], writes=["rstd1"])
            S.op("dve", lambda e: e.reciprocal(out=rstd1, in_=rstd1), reads=["rstd1"], writes=["rstd1"])
            S.op("dve", lambda e: e.tensor_scalar(out=iktmp, in0=pv[:, 0:64], scalar1=mv[:, 0:1], scalar2=rstd1,
                                                  op0=ALU.subtract, op1=ALU.mult),
                 reads=[("ps", bk), "mv", "rstd1"], writes=["iktmp"])
            S.op("dve", lambda e: e.tensor_tensor(out=iktmp, in0=iktmp, in1=gik, op=ALU.mult),
                 reads=["iktmp", "catt"], writes=["iktmp"])
            S.op("dve", lambda e: e.tensor_tensor(out=ikn2[:, 0:64], in0=iktmp, in1=bik, op=ALU.add),
                 reads=["iktmp", "catt"], writes=["ikn2a"])
            S.op("pool", lambda e: e.tensor_copy(out=ikn2[:, 64:128], in_=ikn2[:, 0:64]),
                 reads=["ikn2a"], writes=["ikn2b"])
            S.op("act", lambda e, i=i: e.activation(out=wq[:, i, :], in_=pv[:, 64:80], func=AF.Copy,
                                                    scale=0.25 * 0.125),
                 reads=[("ps", bk)], writes=[("wq", i)])
            S.op("pe", lambda e: e.transpose(out=psb(2)[:, 0:128], in_=ikn2, identity=self.ident_bf),
                 reads=["ikn2a", "ikn2b", "ident"], writes=[("ps", 2)])
            S.op("act", lambda e, c0=c0: e.activation(out=ikst[:, c0:c0 + 128], in_=psb(2)[:, 0:128], func=AF.Copy),
                 reads=[("ps", 2)], writes=[("ikst", i)])
        self.proj_tm("m3", strm, self.gidx["ikw"], wbufs, TL[0:8], 80, ev_ik)

        S.dma("sp", lambda e: e.dma_start(out=self.ks.rearrange("p (k t) -> p k t", k=2), in_=kst),
              reads=[("kst", hh, ti) for hh in range(2) for ti in range(2)], writes=["ks"])
        S.dma("sp", lambda e: e.dma_start(out=self.vs[:, 0:2064].rearrange("p (i c) -> p i c", i=8), in_=vst),
              reads=[("vst", i) for i in range(8)] + ["vst1"], writes=["vs"])
        S.dma("sp", lambda e: e.dma_start(out=self.vs[:, 2064:3088], in_=ikst),
              reads=[("ikst", i) for i in range(8)], writes=["vs2"])
        S.barrier()
        rg = [[0, 1, 2, 3], [4, 5, 6, 7]]
        S.coll(lambda e: e.collective_compute("AllGather", ALU.bypass, replica_groups=rg,
                                              ins=[self.ks], outs=[self.kg]), writes=["kg"])
        S.coll(lambda e: e.collective_compute("AllGather", ALU.bypass, replica_groups=rg,
                                              ins=[self.vs], outs=[self.vg]), writes=["vg"])

        for g4 in range(4):
            def ev_q(half, ti, t0, t1e, pv, bk, g4=g4):
                h = 2 * g4 + half
                S.op("act", lambda e: e.activation(out=qT[:, h, t0:t1e], in_=pv, func=AF.Copy, scale=128.0 ** -0.5),
                     reads=[("ps", bk)], writes=[("qT", h, ti)])
            self.proj_fm("m3", strm, self.gidx["aq%d" % g4], wbufs, RTB, ev_q)
        for g4 in range(4):
            def ev_iq(half, ti, t0, t1e, pv, bk, g4=g4):
                h = 2 * g4 + half
                S.op("dve", lambda e: e.tensor_copy(out=iqT[:, h, t0:t1e], in_=pv),
                     reads=[("ps", bk)], writes=[("iqT", h, ti)])
            self.proj_fm("m3", strm, self.gidx["iq%d" % g4], wbufs, RTB, ev_iq)

        for r in range(4):
            S.dma("sp", lambda e, r=r: e.dma_start(
                out=K_all[:, :, r * 1024:(r + 1) * 1024],
                in_=self.kg[r * 128:(r + 1) * 128, :].rearrange("p (k t) -> p k t", k=2)),
                reads=["kg"], writes=["K_all"])
            S.dma("sp", lambda e, r=r: e.dma_start(
                out=V_all[:, r * 8:(r + 1) * 8, :],
                in_=self.vg[r * 128:(r + 1) * 128, 0:2064].rearrange("p (i c) -> p i c", i=8)),
                reads=["vg"], writes=["V_all"])
            S.dma("sp", lambda e, r=r: e.dma_start(
                out=IK_all[:, r * 1024:(r + 1) * 1024], in_=self.vg[r * 128:(r + 1) * 128, 2064:3088]),
                reads=["vg"], writes=["IK_all"])
        S.barrier()

        S.op("pool", lambda e: e.memset(iqz.rearrange("p h t -> p (h t)"), 0.0), writes=["iqz"])
        sc4 = sc.rearrange("p (r c) -> p r c", r=4)
        mb4 = mb.rearrange("p (r c) -> p r c", r=4)
        jk4 = junk.rearrange("p (r c) -> p r c", r=4)
        def geom(i):
            q0 = 128 * i
            nk = 128 * (i + 1)
            pieces = [(r, c0, min(512, nk - c0)) for r in range(4) for c0 in range(0, nk, 512)]
            return q0, nk, pieces

        def st_idx(i):
            q0, nk, pieces = geom(i)
            for h in range(16):
                S.op("pool", lambda e, h=h: e.tensor_scalar(out=Dg[:, h, :], in0=self.ident_bf,
                                                            scalar1=wq[:, i, h:h + 1], scalar2=None, op0=ALU.mult),
                     reads=["ident", ("wq", i)], writes=["Dg"])
            for h in range(16):
                hb = h % 2
                eng = "pool" if h % 2 == 0 else "act"
                if eng == "pool":
                    S.op("pool", lambda e, h=h, hb=hb: e.tensor_copy(
                        out=iqz[hb * 64:(hb + 1) * 64, h, :], in_=iqT[hb * 64:(hb + 1) * 64, h // 2, q0:q0 + 128]),
                        reads=["iqT", "iqz"], writes=[("iqzh", h)])
                else:
                    S.op("act", lambda e, h=h, hb=hb: e.activation(
                        out=iqz[hb * 64:(hb + 1) * 64, h, :], in_=iqT[hb * 64:(hb + 1) * 64, h // 2, q0:q0 + 128],
                        func=AF.Copy),
                        reads=["iqT", "iqz"], writes=[("iqzh", h)])
            for pi, (r, c0, cn) in enumerate(pieces):
                col0 = r * 1024 + c0
                accb = 4 + pi % 2

                def head_mm(h, cn=cn, col0=col0):
                    bk, hb = h % 4, h % 2
                    S.op("pe", lambda e: e.matmul(
                        ps[:, bk, 0:cn], lhsT=iqz[:, h, :],
                        rhs=IK_all[:, col0:col0 + cn], start=True, stop=True),
                        reads=["IK_all", ("iqzh", h)], writes=[("ps", bk)])
                    S.op("act", lambda e: e.activation(out=rh[bk][:, 0:cn], in_=ps[:, bk, 0:cn], func=AF.Relu),
                         reads=[("ps", bk)], writes=[("rh", bk)])

                def head_acc(h, cn=cn, accb=accb):
                    bk = h % 4
                    S.op("pe", lambda e: e.matmul(
                        ps[:, accb, 0:cn], lhsT=Dg[:, h, :], rhs=rh[bk][:, 0:cn], start=(h == 0), stop=(h == 15)),
                        reads=["Dg", ("rh", bk)], writes=[("ps", accb)])
                for h in range(16):
                    head_mm(h)
                    if h >= 2:
                        head_acc(h - 2)
                head_acc(14)
                head_acc(15)
                S.op("act", lambda e, accb=accb, col0=col0, cn=cn: e.activation(
                    out=sc[:, col0:col0 + cn], in_=ps[:, accb, 0:cn], func=AF.Copy),
                    reads=[("ps", accb)], writes=["sc"])

        def st_bis(i):
            q0, nk, pieces = geom(i)
            scv, mbv, jkv = sc4[:, :, 0:nk], mb4[:, :, 0:nk], jk4[:, :, 0:nk]
            S.op("dve", lambda e: e.reduce_max(out=Bt, in_=scv, axis=AX.XY, apply_absolute_value=True),
                 reads=["sc"], writes=["Bt"])
            S.op("dve", lambda e: e.tensor_tensor(out=sc4[:, :, q0:q0 + 128], in0=sc4[:, :, q0:q0 + 128], in1=cbt,
                                                  op=ALU.add),
                 reads=["sc", "cbt", "Bt"], writes=["sc"])
            S.op("dve", lambda e: e.tensor_scalar(out=Wc, in0=Bt, scalar1=2.0002, scalar2=1e-6,
                                                  op0=ALU.mult, op1=ALU.add), reads=["Bt"], writes=["Wc"])
            S.op("dve", lambda e: e.tensor_scalar(out=H[:, 0:NB + 1], in0=pw[:, 0:NB + 1], scalar1=Wc, scalar2=None,
                                                  op0=ALU.mult), reads=["Wc", "catt"], writes=["H"])
            S.op("dve", lambda e: e.memset(mid, 0.0), writes=["mid"])
            for k in range(NB):
                S.op("dve", lambda e: e.tensor_scalar(
                    out=jkv, in0=scv, scalar1=mid, scalar2=0.0, op0=ALU.is_ge, op1=ALU.add, accum_out=cnt),
                    reads=["sc", "mid"], writes=["junk", "cnt"])
                S.op("dve", lambda e, k=k: e.tensor_scalar(out=u2, in0=cnt, scalar1=256.0, scalar2=H[:, k:k + 1],
                                                           op0=ALU.is_ge, op1=ALU.mult),
                     reads=["cnt", "H"], writes=["u2"])
                S.op("dve", lambda e, k=k: e.scalar_tensor_tensor(out=mid, in0=mid, scalar=H[:, k + 1:k + 2], in1=u2,
                                                                  op0=ALU.subtract, op1=ALU.add),
                     reads=["mid", "H", "u2"], writes=["mid"])
            S.op("dve", lambda e: e.tensor_tensor(out=tau, in0=mid, in1=H[:, NB:NB + 1], op=ALU.subtract),
                 reads=["mid", "H"], writes=["tau"])
            S.op("dve", lambda e: e.tensor_scalar(
                out=mbv, in0=scv, scalar1=tau, scalar2=-30000.0, op0=ALU.is_lt, op1=ALU.mult),
                reads=["sc", "tau"], writes=["mb"])

        def st_mbT(i):
            kts = [(r, ip) for r in range(4) for ip in range(i + 1)]
            for g0 in range(0, len(kts), 8):
                grp = kts[g0:g0 + 8]
                bk = 6 + (g0 // 8) % 2
                for s_, (r, ip) in enumerate(grp):
                    S.op("pe", lambda e, bk=bk, s_=s_, r=r, ip=ip: e.transpose(
                        out=psb(bk)[:, s_ * 128:(s_ + 1) * 128], in_=mb[:, r * 1024 + ip * 128:r * 1024 + ip * 128 + 128],
                        identity=self.ident_bf),
                        reads=["mb", "ident"], writes=[("ps", bk)])
                for s_, (r, ip) in enumerate(grp):
                    S.op("act", lambda e, bk=bk, s_=s_, r=r, ip=ip: e.activation(
                        out=mbT[:, r * 8 + ip, :], in_=psb(bk)[:, s_ * 128:(s_ + 1) * 128], func=AF.Copy),
                        reads=[("ps", bk)], writes=["mbT"])

        def st_passA(i):
            q0, nk, pieces = geom(i)
            S.op("dve", lambda e: e.memset(ps[:, 5:8, :].rearrange("p a b -> p (a b)"), 0.0),
                 writes=[("ps", 5), ("ps", 6), ("ps", 7)])
            S.dma("pool", lambda e: e.dma_start(out=AugQ[64:65, :], in_=self.augq[i:i + 1, :]), writes=["AugQ"])
            pcs = pieces + [(4, 0, NMETA)]
            for h in range(8):
                kvh = h // 4
                for pi, (r, c0, cn) in enumerate(pcs):
                    col0 = r * 1024 + c0
                    bk = (h * len(pcs) + pi) % 4
                    meta = (r == 4)
                    S.op("pe", lambda e, bk=bk, h=h, kvh=kvh, col0=col0, cn=cn: e.matmul(
                        ps[:, bk, 0:cn], lhsT=qT[:, h, q0:q0 + 128], rhs=K_all[:, kvh, col0:col0 + cn],
                        start=True, stop=False),
                        reads=["K_all", "qT", ("Kmeta", kvh)], writes=[("ps", bk)])
                    S.op("pe", lambda e, bk=bk, h=h, col0=col0, cn=cn, meta=meta: e.matmul(
                        ps[:, bk, 0:cn], lhsT=AugQ[0:65, h * 128:(h + 1) * 128], rhs=AugK[0:65, col0:col0 + cn],
                        start=False, stop=meta),
                        reads=["AugQ", "AugK"], writes=[("ps", bk)])
                    if not meta:
                        S.op("pe", lambda e, bk=bk, col0=col0, cn=cn: e.matmul(
                            ps[:, bk, 0:cn], lhsT=self.ident_bf, rhs=mb[:, col0:col0 + cn], start=False, stop=True),
                            reads=["mb", "ident"], writes=[("ps", bk)])
                    S.op("dve", lambda e, bk=bk, h=h, pi=pi, cn=cn: e.reduce_max(
                        out=mx[:, h, pi:pi + 1], in_=ps[:, bk, 0:cn], axis=AX.X),
                        reads=[("ps", bk)], writes=["mx"])
            S.op("dve", lambda e: e.reduce_max(out=mrow, in_=mx[:, :, 0:len(pcs)], axis=AX.X),
                 reads=["mx"], writes=["mrow"])
            S.op("dve", lambda e: e.tensor_tensor(out=cc, in0=AQc[:, i, :], in1=mrow, op=ALU.subtract),
                 reads=["mrow", "catt"], writes=["cc"])
            for h in range(8):
                S.op("pool", lambda e, h=h: e.tensor_scalar(out=Dg[:, h, :], in0=self.ident_bf, scalar1=cc[:, h:h + 1],
                                                            scalar2=None, op0=ALU.mult),
                     reads=["ident", "cc"], writes=["Dg"])
            for a_ in range(2):
                S.op("pe", lambda e, a_=a_: e.matmul(ps[:, 4, :], lhsT=self.ones_bf,
                                                     rhs=Dg[:, 4 * a_:4 * a_ + 4, :].rearrange("p h t -> p (h t)"),
                                                     start=True, stop=True),
                     reads=["Dg", "ones_bf"], writes=[("ps", 4)])
                S.op("dve", lambda e, a_=a_: e.tensor_copy(out=AugR[64:65, a_ * 512:(a_ + 1) * 512], in_=ps[64:65, 4, :]),
                     reads=[("ps", 4)], writes=["AugR"])

        def st_passB(i):
            q0, nk, pieces = geom(i)
            ktl = [(r * 1024 + ip * 128, r * 8 + ip, 128) for r in range(4) for ip in range(i + 1)] + [(4096, 32, NMETA)]
            Oreg = lambda h: ps[:, 5 + h // 3, (h % 3) * 129:(h % 3 + 1) * 129]

            def logits(qi):
                col0, vt, n = ktl[qi]
                meta = (n == NMETA)
                pair = (0, 1) if qi % 2 == 0 else (2, 3)
                for h in range(8):
                    kvh = h // 4
                    out = ps[0:n, pair[h // 4], (h % 4) * 128:(h % 4 + 1) * 128]
                    S.op("pe", lambda e, out=out, kvh=kvh, h=h: e.matmul(
                        out, lhsT=K_all[:, kvh, col0:col0 + n], rhs=qT[:, h, q0:q0 + 128], start=True, stop=False),
                        reads=["K_all", "qT", ("Kmeta", kvh)], writes=[("ps", pair[h // 4])])
                    S.op("pe", lambda e, out=out, h=h: e.matmul(
                        out, lhsT=AugK[0:65, col0:col0 + n], rhs=AugR[0:65, h * 128:(h + 1) * 128],
                        start=False, stop=meta),
                        reads=["AugK", "AugR"], writes=[("ps", pair[h // 4])])
                    if not meta:
                        S.op("pe", lambda e, out=out: e.matmul(
                            out, lhsT=self.ident_bf, rhs=mbT[:, vt, :], start=False, stop=True),
                            reads=["mbT", "ident"], writes=[("ps", pair[h // 4])])
                pt = PTb[qi % 2]
                S.op("act", lambda e: e.activation(
                    out=pt[0:n, :], in_=ps[0:n, pair[0]:pair[0] + 2, :].rearrange("p a b -> p (a b)"), func=AF.Exp),
                    reads=[("ps", pair[0]), ("ps", pair[1])], writes=[("PTb", qi % 2)])

            def pv(qi):
                col0, vt, n = ktl[qi]
                pt = PTb[qi % 2]
                for h in range(8):
                    kvh = h // 4
                    S.op("pe", lambda e, h=h, kvh=kvh: e.matmul(
                        Oreg(h), lhsT=pt[0:n, h * 128:(h + 1) * 128], rhs=V_all[0:n, vt, kvh * 129:(kvh + 1) * 129],
                        start=False, stop=(qi == len(ktl) - 1)),
                        reads=[("PTb", qi % 2), "V_all", "Vmeta"], writes=[("ps", 5 + h // 3)])
            logits(0)
            for qi in range(len(ktl)):
                if qi + 1 < len(ktl):
                    logits(qi + 1)
                pv(qi)

        def st_fin(i):
            q0 = 128 * i
            for b3 in range(3):
                nh = 3 if b3 < 2 else 2
                Ov = ps[:, 5 + b3, 0:nh * 129].rearrange("p (h c) -> p h c", c=129)
                S.op("dve", lambda e, Ov=Ov, b3=b3, nh=nh: e.reciprocal(out=rs[:, 3 * b3:3 * b3 + nh], in_=Ov[:, :, 128]),
                     reads=[("ps", 5 + b3)], writes=[("rs", b3)])
                S.op("dve", lambda e, Ov=Ov, b3=b3, nh=nh: e.tensor_tensor(
                    out=ya[:, 3 * b3:3 * b3 + nh, :], in0=Ov[:, :, 0:128],
                    in1=rs[:, 3 * b3:3 * b3 + nh].unsqueeze(2).to_broadcast([128, nh, 128]), op=ALU.mult),
                    reads=[("ps", 5 + b3), ("rs", b3)], writes=[("ya", b3), ("PTb", 0)])
            for h in range(8):
                S.op("pe", lambda e, h=h: e.transpose(out=psb(4)[:, h * 128:(h + 1) * 128], in_=ya[:, h, :],
                                                      identity=self.ident_bf),
                     reads=[("ya", h // 3), ("PTb", 0), "ident"], writes=[("ps", 4)])
            S.op("act", lambda e: e.activation(out=yatt[:, :, q0:q0 + 128],
                                               in_=psb(4).rearrange("p (h t) -> p h t", h=8), func=AF.Copy),
                 reads=[("ps", 4)], writes=[("yatt", i)])

        st_idx(0)
        st_bis(0)
        st_mbT(0)
        for i in range(8):
            if i + 1 < 8:
                st_idx(i + 1)
            st_passA(i)
            if i + 1 < 8:
                st_bis(i + 1)
            st_passB(i)
            st_fin(i)
            if i + 1 < 8:
                st_mbT(i + 1)
        S.barrier()

    def merge_m4(self, strm, cg, cb):
        S, ps, nc = self.S, self.ps, self.nc
        R1 = 99840
        RTB = TBS[0:2]
        yatt = self.view(0, BF16, [8, NREAL]); yhg = self.view(32768, BF16, [8, NREAL])
        merged = self.view(R1, BF16, [KC, NREAL])
        o = R1 + 32768
        wga = [self.view(o + q * 8192, BF16, [KC, 256]) for q in range(2)]
        wgh = [self.view(o + 16384 + q * 8192, BF16, [KC, 256]) for q in range(2)]
        wba = [self.view(o + 32768 + q * 4096, BF16, [8, 256]) for q in range(2)]
        wbh = [self.view(o + 40960 + q * 4096, BF16, [8, 256]) for q in range(2)]
        tm = [self.view(o + 49152 + q * 2048, F32, [512]) for q in range(4)]
        for mg in range(8):
            q = mg % 2
            S.dma("pool", lambda e, q=q, mg=mg: e.dma_start(out=wga[q], in_=self.win[self.gidx["ga%d" % mg]]),
                  writes=[("wga", q)])
            S.dma("pool", lambda e, q=q, mg=mg: e.dma_start(out=wgh[q], in_=self.win[self.gidx["gh%d" % mg]]),
                  writes=[("wgh", q)])
            S.dma("pool", lambda e, q=q, mg=mg: e.dma_start(out=wba[q], in_=self.wba_d[mg]), writes=[("wba", q)])
            S.dma("pool", lambda e, q=q, mg=mg: e.dma_start(out=wbh[q], in_=self.wbh_d[mg]), writes=[("wbh", q)])
            for half in range(2):
                mc = 2 * mg + half
                hs = slice(half * 128, (half + 1) * 128)
                for ti, (t0, t1e) in enumerate(RTB):
                    pp = (half * 2 + ti) % 2
                    b0 = 4 * pp
                    for k in range(KC):
                        S.op("pe", lambda e, b0=b0, q=q, k=k, hs=hs, t0=t0, t1e=t1e: e.matmul(
                            ps[:, b0, :], lhsT=wga[q][:, k, hs], rhs=strm[:, k, t0:t1e],
                            start=(k == 0), stop=(k == KC - 1)),
                            reads=[("wga", q), "strm"], writes=[("ps", b0)])
                    for k in range(8):
                        S.op("pe", lambda e, b0=b0, q=q, k=k, hs=hs, t0=t0, t1e=t1e: e.matmul(
                            ps[:, b0 + 1, :], lhsT=wba[q][:, k, hs], rhs=yatt[:, k, t0:t1e],
                            start=(k == 0), stop=(k == 7)),
                            reads=[("wba", q), "yatt"], writes=[("ps", b0 + 1)])
                    for k in range(KC):
                        S.op("pe", lambda e, b0=b0, q=q, k=k, hs=hs, t0=t0, t1e=t1e: e.matmul(
                            ps[:, b0 + 2, :], lhsT=wgh[q][:, k, hs], rhs=strm[:, k, t0:t1e],
                            start=(k == 0), stop=(k == KC - 1)),
                            reads=[("wgh", q), "strm"], writes=[("ps", b0 + 2)])
                    for k in range(8):
                        S.op("pe", lambda e, b0=b0, q=q, k=k, hs=hs, t0=t0, t1e=t1e: e.matmul(
                            ps[:, b0 + 3, :], lhsT=wbh[q][:, k, hs], rhs=yhg[:, k, t0:t1e],
                            start=(k == 0), stop=(k == 7)),
                            reads=[("wbh", q), "yhg"], writes=[("ps", b0 + 3)])
                    ta, th = tm[2 * pp], tm[2 * pp + 1]
                    S.op("act", lambda e, ta=ta, b0=b0: e.activation(out=ta, in_=ps[:, b0, :], func=AF.Sigmoid),
                         reads=[("ps", b0)], writes=[("tm", 2 * pp)])
                    S.op("dve", lambda e, ta=ta, b0=b0: e.tensor_tensor(out=ta, in0=ta, in1=ps[:, b0 + 1, :], op=ALU.mult),
                         reads=[("tm", 2 * pp), ("ps", b0 + 1)], writes=[("tm", 2 * pp)])
                    S.op("act", lambda e, th=th, b0=b0: e.activation(out=th, in_=ps[:, b0 + 2, :], func=AF.Sigmoid),
                         reads=[("ps", b0 + 2)], writes=[("tm", 2 * pp + 1)])
                    S.op("dve", lambda e, th=th, b0=b0: e.tensor_tensor(out=th, in0=th, in1=ps[:, b0 + 3, :], op=ALU.mult),
                         reads=[("tm", 2 * pp + 1), ("ps", b0 + 3)], writes=[("tm", 2 * pp + 1)])
                    S.op("pool", lambda e, ta=ta, th=th, mc=mc, t0=t0, t1e=t1e: e.tensor_tensor(
                        out=merged[:, mc, t0:t1e], in0=ta, in1=th, op=ALU.add),
                        reads=[("tm", 2 * pp), ("tm", 2 * pp + 1)], writes=[("merged", mc, ti)])
        S.barrier()
        z = self.view(R1 + 32768, F32, [KC, NREAL])
        wo = [self.view(53760 + q * 4096, BF16, [KC, 128]) for q in range(2)]
        strm_out = self.view(0, BF16, [KC, NREAL])
        for dc in range(KC):
            q = dc % 2
            S.dma("pool", lambda e, q=q, dc=dc: e.dma_start(out=wo[q], in_=self.wo_d[dc]), writes=[("wo", q)])
            S.dma("sp", lambda e, dc=dc: e.dma_start(out=z[:, dc, :], in_=self.h1s[dc * 128:(dc + 1) * 128, 0:NREAL]),
                  writes=[("l2", "z", dc, ti) for ti in range(2)])
            for ti, (t0, t1e) in enumerate(RTB):
                pb = (dc * 2 + ti) % 2
                for k in range(KC):
                    S.op("pe", lambda e, pb=pb, q=q, k=k, t0=t0, t1e=t1e: e.matmul(
                        ps[:, pb, :], lhsT=wo[q][:, k, :], rhs=merged[:, k, t0:t1e],
                        start=(k == 0), stop=(k == KC - 1)),
                        reads=[("wo", q), ("merged", k, ti)], writes=[("ps", pb)])
                S.op("dve", lambda e, pb=pb, dc=dc, t0=t0, t1e=t1e: e.scalar_tensor_tensor(
                    out=z[:, dc, t0:t1e], in0=ps[:, pb, :], scalar=1.0 / ALPHA, in1=z[:, dc, t0:t1e],
                    op0=ALU.mult, op1=ALU.add),
                    reads=[("ps", pb), ("l2", "z", dc, ti)], writes=[("l2", "z", dc, ti)])
        self.ln_apply("l2", z, RTB, cg, cb, 33280, LN_EPS / (ALPHA * ALPHA), strm_out=strm_out,
                      resid_out=self.h2s)
        S.barrier()

    def eps_col(self, val):
        return self.eps_cols[val]

    def build(self):
        nc, S = self.nc, self.S
        stage = self.stage
        xT = self.din("xT", [D, NT])
        wg1 = self.din("wg1", [FC // 2, 128, KC, 256])
        wu1 = self.din("wu1", [FC // 2, 128, KC, 256])
        wd1 = self.din("wd1", [KC, 128, FC, 128])
        wg2 = self.din("wg2", [FC // 2, 128, KC, 256])
        wu2 = self.din("wu2", [FC // 2, 128, KC, 256])
        wd2 = self.din("wd2", [KC, 128, FC, 128])
        cvec = self.din("cvec", [128, 128])
        cmat = self.din("cmat", [128, 384])
        self.catt = self.din("catt", [128, 224])
        self.augk = self.din("augk", [3, 4112])
        self.augs = self.din("augs", [2, 1024])
        self.augq = self.din("augq", [8, 1024])
        self.cbt_d = self.din("cbt", [128, 512])
        self.win = self.din("win", [len(GROUPS), 128, KC, 256])
        self.wba_d = self.din("wba", [8, 128, 8, 256])
        self.wbh_d = self.din("wbh", [8, 128, 8, 256])
        self.wo_d = self.din("wo", [KC, 128, KC, 128])
        self.gidx = {nm: i for i, (nm, _, _) in enumerate(GROUPS)}
        self.h1s = h1s = self.dscratch("h1s", [D, NT])
        self.h2s = self.dscratch("h2s", [D, NREAL])
        self.xs = [self.dscratch("xs%d" % q, [128, 3 * 1024], BF16) for q in range(3)]
        self.xg = [self.dscratch("xg%d" % q, [512, 3 * 1024], BF16) for q in range(3)]
        self.xa = self.dscratch("xa", [128, 72])
        self.xag = self.dscratch("xag", [512, 72])
        self.ks = self.dscratch("ks", [128, 2048], BF16)
        self.kg = self.dscratch("kg", [512, 2048], BF16)
        self.vs = self.dscratch("vs", [128, 3088], BF16)
        self.vg = self.dscratch("vg", [512, 3088], BF16)
        if stage == 1:
            dbg = self.dout("dbg", [D, NT])
        elif stage in (3, 4):
            dbg = self.dout("dbg", [128, 8 * NREAL])
            self.dbg2 = self.dout("dbg2", [128, 12000])
        elif stage == 5:
            dbg = self.dout("dbg", [D, NREAL])
        else:
            outT = self.dout("outT", [D, NREAL])

        from contextlib import ExitStack
        with ExitStack() as es:
            self.arena = es.enter_context(nc.sbuf_tensor("arena", [128, ARENA_F32], F32))
            self.cst = es.enter_context(nc.sbuf_tensor("cst", [128, CONST_F32], F32))
            self.ps = es.enter_context(nc.psum_tensor("ps", [128, 8, 512], F32))
            esems = {e: es.enter_context(nc.semaphore("sem_" + e)) for e in ENGS}
            dsems = [es.enter_context(nc.semaphore("dsem%d" % i)) for i in range(S.n_dma_sems + 8)]
            block = es.enter_context(nc.Block())
            cst = self.cst
            self.ones_f32 = cst[:, 0:128]
            cv = self.cv = cst[:, 128:256]
            epsA = cst[:, 256:257]
            self.eps_rms = cst[:, 257:258]
            self.eps_ik = cst[:, 258:259]
            self.eps_cols = {LN_EPS / (ALPHA * ALPHA): epsA}
            self.ident_bf = cst[:, 264:328].bitcast(BF16)
            self.ones_bf = cst[:, 328:392].bitcast(BF16)
            self.mask2 = cst[:, 392:520]
            self.ones128 = cst[:, 520:648]
            self.lbc = cst[:, 648:656]
            self.omlc = cst[:, 656:664]
            self.gnc = cv[:, 112:120]
            self.selc = cv[:, 120:124]
            S.op("dve", lambda e: e.memset(self.ones_f32, 1.0 / D), writes=["ones"])
            S.op("dve", lambda e: e.memset(self.ones128, 1.0 / 128), writes=["ones128"])
            S.op("dve", lambda e: e.memset(epsA, LN_EPS / (ALPHA * ALPHA)), writes=["eps"])
            S.op("dve", lambda e: e.memset(self.eps_rms, RMS_EPS), writes=["eps"])
            S.op("dve", lambda e: e.memset(self.eps_ik, LN_EPS), writes=["eps"])
            S.dma("sp", lambda e: e.dma_start(out=cv, in_=cvec), writes=["cv"])
            S.dma("sp", lambda e: e.dma_start(out=self.mask2, in_=cmat[:, 256:384]), writes=["mask2"])
            S.dma("pool", lambda e: e.dma_start(out=self.ident_bf, in_=cmat[:, 0:128]), writes=["ident"])
            S.dma("pool", lambda e: e.dma_start(out=self.ones_bf, in_=cmat[:, 128:256]), writes=["ones_bf"])
            S.op("dve", lambda e: e.tensor_tensor(out=self.lbc, in0=cv[:, 96:104], in1=cv[:, 104:112], op=ALU.subtract),
                 reads=["cv"], writes=["lb"])
            S.op("act", lambda e: e.activation(out=self.lbc, in_=self.lbc, func=AF.Sigmoid), reads=["lb"], writes=["lb"])
            S.op("dve", lambda e: e.tensor_scalar(out=self.omlc, in0=self.lbc, scalar1=-1.0, scalar2=1.0,
                                                  op0=ALU.mult, op1=ALU.add), reads=["lb"], writes=["lb"])
            strm0 = self.view(0, BF16, [KC, NT])
            S.dma("pool", lambda e: e.dma_start(out=strm0, in_=xT.rearrange("(k p) t -> p k t", p=128)),
                  writes=[("f1", "sin")])
            S.barrier()
            self.ffn_phase("f1", NT, TBS, strm0, 66560, wg1, wu1, wd1, xT, cv[:, 0:16], cv[:, 16:32],
                           resid_out=(dbg if stage == 1 else h1s))
            strm1 = self.view(66560, BF16, [KC, NT])
            if stage >= 2:
                self.hgrn_m1(strm1)
                self.hgrn_m2(strm1)
            if stage == 3:
                yhg = self.view(32768, BF16, [8 * NREAL])
                S.dma("pool", lambda e: e.dma_start(out=dbg, in_=yhg), reads=[])
            if stage >= 4:
                self.attn_m3(strm1)
            if stage == 4:
                yat = self.view(0, BF16, [8 * NREAL])
                S.dma("pool", lambda e: e.dma_start(out=dbg, in_=yat), reads=[])
            if stage >= 5:
                if stage == 5:
                    self.h2s = dbg
                self.merge_m4(strm1, cv[:, 32:48], cv[:, 48:64])
            if stage >= 6:
                strm2 = self.view(0, BF16, [KC, NREAL])
                self.ffn_phase("f2", NREAL, TBS[0:2], strm2, 66560, wg2, wu2, wd2, self.h2s, cv[:, 64:80],
                               cv[:, 80:96], final_out=outT)
            S.emit(block, esems, dsems)
        return nc


def _lay_gu(w):
    return np.ascontiguousarray(w.reshape(KC, 128, FC // 2, 256).transpose(2, 1, 0, 3))


def _lay_d(w):
    return np.ascontiguousarray(w.reshape(FC, 128, KC, 128).transpose(2, 1, 0, 3))


def _fm(v):
    return np.ascontiguousarray(v.reshape(KC, 128).T)


def _core_tokens(x, meta, c):
    b, j = c // 4, c % 4
    blocks = [x[b, 128 * (4 * i + j):128 * (4 * i + j) + 128] for i in range(8)]
    tok = np.concatenate(blocks + [meta], axis=0)
    return np.ascontiguousarray(tok.T)


def _mk_groups():
    g = []
    for hp in range(4):
        g += [("hf%d" % hp, 3664 + 256 * hp, 256), ("hq%d" % hp, 2640 + 256 * hp, 256),
              ("hi%d" % hp, 4688 + 256 * hp, 256)]
    for i in range(4):
        g.append(("hg%d" % i, 5712 + 256 * i, 256))
    g += [("ak", 1024, 256), ("av", 1280, 256), ("ikw", 2560, 80)]
    for i in range(4):
        g.append(("aq%d" % i, 256 * i, 256))
    for i in range(4):
        g.append(("iq%d" % i, 1536 + 256 * i, 256))
    for i in range(8):
        g.append(("ga%d" % i, 6736 + 256 * i, 256))
        g.append(("gh%d" % i, 8784 + 256 * i, 256))
    return g


GROUPS = _mk_groups()


def _lay_win(w):
    out = np.zeros((len(GROUPS), 128, KC, 256), np.float32)
    for gi, (nm, c0, nc_) in enumerate(GROUPS):
        out[gi, :, :, 0:nc_] = w[:, c0:c0 + nc_].reshape(KC, 128, nc_).transpose(1, 0, 2)
    return out


def prepare(inputs, stage):
    f = lambda k: np.asarray(inputs[k], dtype=np.float32)
    x, meta = f("x"), f("meta")
    shared = {
        "wg1": _lay_gu(f("ffn1_w_gate")[0]), "wu1": _lay_gu(f("ffn1_w_up")[0]),
        "wd1": _lay_d(f("ffn1_w_down")[0]),
        "win": _lay_win(f("w_in")[0]),
        "wg2": _lay_gu(f("ffn2_w_gate")[0]), "wu2": _lay_gu(f("ffn2_w_up")[0]),
        "wd2": _lay_d(f("ffn2_w_down")[0]),
        "wba": np.ascontiguousarray(f("w_branch_att")[0].reshape(8, 128, 8, 256).transpose(2, 1, 0, 3)),
        "wbh": np.ascontiguousarray(f("w_branch_hg")[0].reshape(8, 128, 8, 256).transpose(2, 1, 0, 3)),
        "wo": np.ascontiguousarray(f("w_out")[0].reshape(KC, 128, KC, 128).transpose(2, 1, 0, 3)),
    }
    slopes = (2.0 ** -(np.arange(8) + 1.0)).astype(np.float32)
    c = np.arange(4096)
    kpos = np.concatenate([16 + 128 * (4 * ((c % 1024) // 128) + c // 1024) + c % 128, np.arange(16)]).astype(np.float32)
    augk = np.stack([np.floor(kpos / 64.0), kpos % 64.0, np.ones_like(kpos)], 0).astype(np.float32)
    augs = np.stack([np.repeat(64.0 * slopes, 128), np.repeat(slopes, 128)], 0).astype(np.float32)
    shared["augk"] = augk
    shared["augs"] = augs
    cvec = np.zeros((128, 128), np.float32)
    cvec[:, 0:16] = _fm(f("ln1_g")[0]); cvec[:, 16:32] = _fm(f("ln1_b")[0])
    cvec[:, 32:48] = _fm(f("ln2_g")[0]); cvec[:, 48:64] = _fm(f("ln2_b")[0])
    cvec[:, 64:80] = _fm(f("ln3_g")[0]); cvec[:, 80:96] = _fm(f("ln3_b")[0])
    lbl = f("hg_lb_logits")
    cvec[:, 96:104] = lbl[0].reshape(8, 128).T
    cvec[:, 104:112] = lbl[1].reshape(8, 128).T
    cvec[:, 112:120] = f("hg_norm_g")[0].T
    cmat = np.zeros((128, 384), np.float32)
    cmat[:, 0:128] = np.eye(128, dtype=np.float32)
    cmat[:, 128:256] = 1.0
    sidx = np.arange(128)[:, None]; tidx = np.arange(128)[None, :]
    cmat[:, 256:384] = (((sidx <= tidx) & ((sidx // 64) == (tidx // 64))) | ((sidx < 64) & (tidx >= 64))).astype(np.float32)
    shared["cmat"] = cmat
    maps = []
    for c in range(8):
        m = dict(shared)
        m["xT"] = _core_tokens(x, meta, c)
        cv = cvec.copy()
        cv[:, 120 + (c % 4)] = 1.0
        m["cvec"] = cv
        j = c % 4
        p = np.arange(128, dtype=np.float32)
        qpos = np.stack([16 + 128 * (4 * i + j) + p for i in range(8)], 0)
        m["augq"] = np.ascontiguousarray((-slopes[None, :, None] * qpos[:, None, :]).reshape(8, 1024).astype(np.float32))
        catt = np.zeros((128, 224), np.float32)
        catt[:, 0:64] = (-qpos.T[:, :, None] * slopes[None, None, :]).reshape(128, 64)
        catt[:, 64:96] = (2.0 ** -(np.arange(32) + 1.0))[None, :]
        catt[:, 96:160] = f("idx_k_norm_g")[0][None, :]
        catt[:, 160:224] = f("idx_k_norm_b")[0][None, :]
        m["catt"] = catt
        tt = np.arange(128)[:, None, None]; rr = np.arange(4)[None, :, None]; ss = np.arange(128)[None, None, :]
        m["cbt"] = np.where(128 * (rr - j) + (ss - tt) > 0, -1e30, 0.0).astype(np.float32).reshape(128, 512)
        maps.append(m)
    return maps


_NC_CACHE = {}


def kernel(**inputs):
    stage = int(os.environ.get("KSTAGE", "9"))
    if stage not in _NC_CACHE:
        _NC_CACHE[stage] = Builder(stage).build()
    nc = _NC_CACHE[stage]
    maps = prepare(inputs, stage)
    res = run_bass_kernel_spmd(nc, maps, core_ids=list(range(8)))
    if stage == 4:
        return [(r["dbg"], r["dbg2"]) for r in res.results]
    if stage < 6:
        return [r["dbg"] for r in res.results]
    out = np.zeros((2, 4096, D), np.float32)
    for c in range(8):
        b, j = c // 4, c % 4
        o = res.results[c]["outT"]
        for i in range(8):
            g = 4 * i + j
            out[b, 128 * g:128 * g + 128] = o[:, 128 * i:128 * i + 128].T
    return out
```

```python
import os
import numpy as np
import concourse.bass as bass
import concourse.mybir as mybir
from concourse.bass_utils import run_bass_kernel_spmd

F32 = mybir.dt.float32
BF16 = mybir.dt.bfloat16
U8 = mybir.dt.uint8
AF = mybir.ActivationFunctionType
ALU = mybir.AluOpType
AX = mybir.AxisListType

D = 2048
DFF = 5632
NMETA = 16
NREAL = 1024
NT = NREAL + NMETA
KC = D // 128
FC = DFF // 128
TBS = [(0, 512), (512, 1024), (1024, 1040)]
ALPHA = 2.0 ** 0.25
LN_EPS = 1e-5
RMS_EPS = 1e-6

ENGS = ("pe", "act", "dve", "pool", "sp")


class Op:
    __slots__ = ("eng", "fn", "deps", "is_dma", "dsem", "dval", "sig", "sigidx", "waits")

    def __init__(self, eng, fn, is_dma=False):
        self.eng = eng
        self.fn = fn
        self.deps = []
        self.is_dma = is_dma
        self.dsem = None
        self.dval = 0
        self.sig = False
        self.sigidx = 0
        self.waits = []


class Sched:
    def __init__(self, n_dma_sems=40, same_engine_sync=True):
        self.ops = {e: [] for e in ENGS}
        self.lastw = {}
        self.readers = {}
        self.n_dma = 0
        self.n_dma_sems = n_dma_sems
        self.dma_hist = {}
        self.same_engine_sync = same_engine_sync
        self.n_coll = 0

    def _add(self, op, reads, writes):
        deps = set()
        for k in reads:
            w = self.lastw.get(k)
            if w is not None:
                deps.add(w)
        for k in writes:
            w = self.lastw.get(k)
            if w is not None:
                deps.add(w)
            for r in self.readers.get(k, ()):
                deps.add(r)
        op.deps = list(deps)
        for k in reads:
            self.readers.setdefault(k, []).append(op)
        for k in writes:
            self.lastw[k] = op
            self.readers[k] = []
        self.ops[op.eng].append(op)
        return op

    def op(self, eng, fn, reads=(), writes=()):
        return self._add(Op(eng, fn), reads, writes)

    def dma(self, q, fn, reads=(), writes=()):
        op = Op(q, fn, is_dma=True)
        slot = self.n_dma % self.n_dma_sems
        op.dsem = slot
        op.dval = 16 * (self.n_dma // self.n_dma_sems + 1)
        self.n_dma += 1
        self._add(op, reads, writes)
        prev = self.dma_hist.get(slot)
        if prev is not None:
            op.deps.append(prev)
        self.dma_hist[slot] = op
        return op

    def coll(self, fn, reads=(), writes=()):
        op = Op("pool", fn, is_dma=True)
        op.dsem = self.n_dma_sems + self.n_coll
        op.dval = 1
        self.n_coll += 1
        self._add(op, reads, writes)
        self.dma_hist[op.dsem] = op
        return op

    def barrier(self):
        lasts = []
        for e in ENGS:
            for o in reversed(self.ops[e]):
                if not o.is_dma and o.fn is not None:
                    lasts.append(o)
                    break
        dmas = list(self.dma_hist.values())
        for e in ENGS:
            op = Op(e, None)
            op.deps = [o for o in lasts if o.eng != e] + dmas
            self.ops[e].append(op)
        self.lastw = {}
        self.readers = {}

    def _skip(self, d, op):
        return d.eng == op.eng and (d.eng in ("pe", "sp") or not self.same_engine_sync)

    def finalize(self):
        for e in ENGS:
            for op in self.ops[e]:
                for d in op.deps:
                    if not d.is_dma and not self._skip(d, op):
                        d.sig = True
        for e in ENGS:
            c = 0
            for op in self.ops[e]:
                if op.sig:
                    c += 1
                    op.sigidx = c
        for e in ENGS:
            waited = {}
            for op in self.ops[e]:
                need = {}
                for d in op.deps:
                    if d.is_dma:
                        key, val = ("d", d.dsem), d.dval
                    else:
                        if self._skip(d, op):
                            continue
                        key, val = ("e", d.eng), d.sigidx
                    if waited.get(key, 0) >= val:
                        continue
                    if need.get(key, 0) < val:
                        need[key] = val
                for k, v in need.items():
                    waited[k] = v
                op.waits = list(need.items())

    def emit(self, block, esems, dsems):
        self.finalize()
        regs = {"pe": block.tensor, "act": block.scalar, "dve": block.vector,
                "pool": block.gpsimd, "sp": block.sync}
        final = {d.dsem: d.dval for d in self.dma_hist.values()}

        def make(e):
            ops = self.ops[e]

            def body(eng):
                for op in ops:
                    for (kind, which), val in op.waits:
                        eng.wait_ge(dsems[which] if kind == "d" else esems[which], val)
                    if op.fn is None:
                        continue
                    ins = op.fn(eng)
                    if op.is_dma:
                        ins.then_inc(dsems[op.dsem], 16 if op.dsem < self.n_dma_sems else 1)
                    elif op.sig:
                        ins.then_inc(esems[e], 1)
                if e == "sp":
                    for slot, val in final.items():
                        eng.wait_ge(dsems[slot], val)
            return body

        for e in ENGS:
            regs[e](make(e))


ARENA_F32 = 50816
CONST_F32 = 2304


class Builder:
    def __init__(self, stage):
        self.stage = stage
        self.nc = bass.Bass("TRN2", target_bir_lowering=False)
        self.S = Sched()
        self.dram = {}

    def din(self, name, shape, dt=F32):
        self.dram[name] = self.nc.dram_tensor(name, list(shape), dt, kind="ExternalInput").ap()
        return self.dram[name]

    def dout(self, name, shape, dt=F32):
        self.dram[name] = self.nc.dram_tensor(name, list(shape), dt, kind="ExternalOutput").ap()
        return self.dram[name]

    def dscratch(self, name, shape, dt=F32):
        self.dram[name] = self.nc.dram_tensor(name, list(shape), dt, kind="Internal").ap()
        return self.dram[name]

    def view(self, off_bytes, dt, shape):
        n = 1
        for s in shape:
            n *= s
        esz = 4 if dt == F32 else (1 if dt == U8 else 2)
        assert off_bytes % 4 == 0
        nbytes = n * esz
        assert nbytes % 4 == 0
        assert off_bytes + nbytes <= ARENA_F32 * 4, (off_bytes, nbytes)
        v = self.arena[:, off_bytes // 4:(off_bytes + nbytes) // 4]
        if dt != F32:
            v = v.bitcast(dt)
        if len(shape) == 2:
            v = v.rearrange("p (a b) -> p a b", b=shape[1])
        elif len(shape) == 3:
            v = v.rearrange("p (a b c) -> p a b c", b=shape[1], c=shape[2])
        return v

    def ln_apply(self, tag, z, tbs, cg, cb, toff, eps_eff, strm_out=None, alias_key=None, resid_out=None,
                 final_out=None):
        S, ps = self.S, self.ps
        zsq = [self.view(toff + i * 2048, F32, [512]) for i in range(2)]
        meanb = self.view(toff + 4096, F32, [512])
        rstdb = self.view(toff + 6144, F32, [512])
        t1 = [self.view(toff + 8192 + i * 2048, F32, [512]) for i in range(2)]
        t2 = [self.view(toff + 12288 + i * 2048, F32, [512]) for i in range(2)]
        o32 = [self.view(toff + 16384 + i * 2048, F32, [512]) for i in range(2)]
        ones = self.ones_f32
        for ti, (t0, t1e) in enumerate(tbs):
            n = t1e - t0
            pm, pq = ps[:, 6, 0:n], ps[:, 7, 0:n]
            for dc in range(KC):
                zq = zsq[dc % 2]
                S.op("act", lambda e, zq=zq, dc=dc, t0=t0, t1e=t1e, n=n: e.activation(
                    out=zq[:, 0:n], in_=z[:, dc, t0:t1e], func=AF.Square),
                    reads=[(tag, "z", dc, ti)], writes=[(tag, "zsq", dc % 2)])
                S.op("pe", lambda e, pm=pm, dc=dc, t0=t0, t1e=t1e: e.matmul(
                    pm, lhsT=ones, rhs=z[:, dc, t0:t1e], start=(dc == 0), stop=(dc == KC - 1)),
                    reads=[(tag, "z", dc, ti)], writes=[("ps", 6)])
                S.op("pe", lambda e, pq=pq, zq=zq, dc=dc, n=n: e.matmul(
                    pq, lhsT=ones, rhs=zq[:, 0:n], start=(dc == 0), stop=(dc == KC - 1)),
                    reads=[(tag, "zsq", dc % 2)], writes=[("ps", 7)])
            S.op("act", lambda e, pm=pm, n=n: e.activation(out=meanb[:, 0:n], in_=pm, func=AF.Copy),
                 reads=[("ps", 6)], writes=[(tag, "meanb")])
            S.op("dve", lambda e, n=n: e.tensor_tensor(out=rstdb[:, 0:n], in0=meanb[:, 0:n], in1=meanb[:, 0:n],
                                                       op=ALU.mult),
                 reads=[(tag, "meanb")], writes=[(tag, "rstdb")])
            S.op("dve", lambda e, pq=pq, n=n: e.tensor_tensor(out=rstdb[:, 0:n], in0=pq, in1=rstdb[:, 0:n],
                                                              op=ALU.subtract),
                 reads=[("ps", 7), (tag, "rstdb")], writes=[(tag, "rstdb")])
            S.op("act", lambda e, n=n: e.activation(out=rstdb[:, 0:n], in_=rstdb[:, 0:n], func=AF.Sqrt,
                                                    bias=self.eps_cols[eps_eff], scale=1.0),
                 reads=[(tag, "rstdb")], writes=[(tag, "rstdb")])
            S.op("dve", lambda e, n=n: e.reciprocal(out=rstdb[:, 0:n], in_=rstdb[:, 0:n]),
                 reads=[(tag, "rstdb")], writes=[(tag, "rstdb")])
            for dc in range(KC):
                a, b_, o = t1[dc % 2], t2[dc % 2], o32[dc % 2]
                S.op("pool", lambda e, a=a, dc=dc, t0=t0, t1e=t1e, n=n: e.tensor_tensor(
                    out=a[:, 0:n], in0=z[:, dc, t0:t1e], in1=meanb[:, 0:n], op=ALU.subtract),
                    reads=[(tag, "z", dc, ti), (tag, "meanb")], writes=[(tag, "t1", dc % 2)])
                S.op("dve", lambda e, a=a, b_=b_, n=n: e.tensor_tensor(
                    out=b_[:, 0:n], in0=a[:, 0:n], in1=rstdb[:, 0:n], op=ALU.mult),
                    reads=[(tag, "t1", dc % 2), (tag, "rstdb")], writes=[(tag, "t2", dc % 2)])
                S.op("act", lambda e, b_=b_, o=o, dc=dc, n=n: e.activation(
                    out=o[:, 0:n], in_=b_[:, 0:n], func=AF.Identity,
                    bias=cb[:, dc:dc + 1], scale=cg[:, dc:dc + 1]),
                    reads=[(tag, "t2", dc % 2)], writes=[(tag, "o32", dc % 2)])
                if strm_out is not None:
                    wk = [(tag, "sout", dc, ti)] + ([(tag, alias_key, dc, ti)] if alias_key else [])
                    S.op("act", lambda e, b_=b_, dc=dc, t0=t0, t1e=t1e, n=n: e.activation(
                        out=strm_out[:, dc, t0:t1e], in_=b_[:, 0:n], func=AF.Identity,
                        bias=cb[:, dc:dc + 1], scale=cg[:, dc:dc + 1]),
                        reads=[(tag, "t2", dc % 2)], writes=wk)
                dst = resid_out if final_out is None else final_out
                if final_out is None or t0 < NREAL:
                    S.dma("sp", lambda e, o=o, dc=dc, t0=t0, t1e=t1e, n=n, dst=dst: e.dma_start(
                        out=dst[dc * 128:(dc + 1) * 128, t0:t1e], in_=o[:, 0:n]),
                        reads=[(tag, "o32", dc % 2)])

    def ffn_phase(self, tag, ntok, tbs, strm_in, strm_out_off, wg, wu, wd, resid, cg, cb,
                  resid_out=None, final_out=None):
        S, nc = self.S, self.nc
        ps = self.ps
        C_OFF = 33280
        B_OFF = 66560
        D_OFF = B_OFF + FC * NT * 2
        T_OFF = D_OFF + 2 * FC * 128 * 2
        hT = self.view(B_OFF, BF16, [FC, ntok])
        wgu = [self.view(C_OFF + i * 16384, BF16, [2, KC, 256]) for i in range(2)]
        wdb = [self.view(D_OFF + i * FC * 128 * 2, BF16, [FC, 128]) for i in range(2)]
        z = self.view(0, F32, [KC, ntok])
        sil = [self.view(T_OFF + 8192 + i * 2048, F32, [512]) for i in range(2)]
        strm_out = self.view(strm_out_off, BF16, [KC, ntok]) if final_out is None else None
        c_scale = 0.5 / ALPHA
        eps_eff = LN_EPS / (ALPHA * ALPHA)
        nb = len(tbs)

        for g in range(FC // 2):
            wb = wgu[g % 2]
            kb = (tag, "wgu", g % 2)
            S.dma("pool", lambda e, wb=wb, g=g: e.dma_start(out=wb[:, 0], in_=wg[g]), writes=[(kb, 0)])
            S.dma("pool", lambda e, wb=wb, g=g: e.dma_start(out=wb[:, 1], in_=wu[g]), writes=[(kb, 1)])
            for fcl in range(2):
                fc = 2 * g + fcl
                for ti, (t0, t1e) in enumerate(tbs):
                    n = t1e - t0
                    pb = (fc * nb + ti) % 2
                    pg, pu = ps[:, 2 * pb, 0:n], ps[:, 2 * pb + 1, 0:n]
                    for k in range(KC):
                        S.op("pe", lambda e, pg=pg, wb=wb, k=k, fcl=fcl, t0=t0, t1e=t1e: e.matmul(
                            pg, lhsT=wb[:, 0, k, fcl * 128:(fcl + 1) * 128], rhs=strm_in[:, k, t0:t1e],
                            start=(k == 0), stop=(k == KC - 1)),
                            reads=[(kb, 0), (tag, "sin")], writes=[("ps", 2 * pb)])
                    for k in range(KC):
                        S.op("pe", lambda e, pu=pu, wb=wb, k=k, fcl=fcl, t0=t0, t1e=t1e: e.matmul(
                            pu, lhsT=wb[:, 1, k, fcl * 128:(fcl + 1) * 128], rhs=strm_in[:, k, t0:t1e],
                            start=(k == 0), stop=(k == KC - 1)),
                            reads=[(kb, 1), (tag, "sin")], writes=[("ps", 2 * pb + 1)])
                    sb = sil[pb]
                    S.op("act", lambda e, sb=sb, pg=pg, n=n: e.activation(out=sb[:, 0:n], in_=pg, func=AF.Silu),
                         reads=[("ps", 2 * pb)], writes=[(tag, "sil", pb)])
                    S.op("dve", lambda e, sb=sb, pu=pu, n=n, fc=fc, t0=t0, t1e=t1e: e.tensor_tensor(
                        out=hT[:, fc, t0:t1e], in0=sb[:, 0:n], in1=pu, op=ALU.mult),
                        reads=[(tag, "sil", pb), ("ps", 2 * pb + 1)], writes=[(tag, "hT", fc, ti)])

        S.barrier()
        for dc in range(KC):
            wb = wdb[dc % 2]
            kb = (tag, "wd", dc % 2)
            S.dma("pool", lambda e, wb=wb, dc=dc: e.dma_start(out=wb, in_=wd[dc]), writes=[kb])
            S.dma("sp", lambda e, dc=dc: e.dma_start(out=z[:, dc, :], in_=resid[dc * 128:(dc + 1) * 128, 0:ntok]),
                  writes=[(tag, "z", dc, ti) for ti in range(nb)])
            for ti, (t0, t1e) in enumerate(tbs):
                n = t1e - t0
                pb = 4 + (dc * nb + ti) % 2
                py = ps[:, pb, 0:n]
                for f in range(FC):
                    S.op("pe", lambda e, py=py, wb=wb, f=f, t0=t0, t1e=t1e: e.matmul(
                        py, lhsT=wb[:, f, :], rhs=hT[:, f, t0:t1e], start=(f == 0), stop=(f == FC - 1)),
                        reads=[kb, (tag, "hT", f, ti)], writes=[("ps", pb)])
                S.op("dve", lambda e, py=py, dc=dc, t0=t0, t1e=t1e: e.scalar_tensor_tensor(
                    out=z[:, dc, t0:t1e], in0=py, scalar=c_scale, in1=z[:, dc, t0:t1e],
                    op0=ALU.mult, op1=ALU.add),
                    reads=[("ps", pb), (tag, "z", dc, ti)], writes=[(tag, "z", dc, ti)])
        self.ln_apply(tag, z, tbs, cg, cb, T_OFF, eps_eff, strm_out=strm_out, alias_key="hT",
                      resid_out=resid_out, final_out=final_out)
        S.barrier()

    def proj_fm(self, tag, strm, gi, wbufs, tbs, evac, banks=(0, 1), parity=[0]):
        S, ps = self.S, self.ps
        wb = wbufs[parity[0] % 2]
        kb = ("wb", parity[0] % 2)
        parity[0] += 1
        S.dma("pool", lambda e, wb=wb, gi=gi: e.dma_start(out=wb, in_=self.win[gi]), writes=[kb])
        cnt = 0
        for half in range(2):
            for ti, (t0, t1e) in enumerate(tbs):
                n = t1e - t0
                bk = banks[cnt % len(banks)]
                cnt += 1
                pv = ps[:, bk, 0:n]
                for k in range(KC):
                    S.op("pe", lambda e, pv=pv, wb=wb, k=k, half=half, t0=t0, t1e=t1e: e.matmul(
                        pv, lhsT=wb[:, k, half * 128:(half + 1) * 128], rhs=strm[:, k, t0:t1e],
                        start=(k == 0), stop=(k == KC - 1)),
                        reads=[kb, "strm"], writes=[("ps", bk)])
                evac(half, ti, t0, t1e, pv, bk)

    def proj_tm(self, tag, strm, gi, wbufs, tls, ncols, evac, banks=(0, 1), parity=[0]):
        S, ps = self.S, self.ps
        wb = wbufs[parity[0] % 2]
        kb = ("wb", parity[0] % 2)
        parity[0] += 1
        S.dma("pool", lambda e, wb=wb, gi=gi: e.dma_start(out=wb, in_=self.win[gi]), writes=[kb])
        for cnt, (i, c0, n) in enumerate(tls):
            bk = banks[cnt % len(banks)]
            pv = ps[0:n, bk, 0:ncols]
            for k in range(KC):
                S.op("pe", lambda e, pv=pv, wb=wb, k=k, c0=c0, n=n: e.matmul(
                    pv, lhsT=strm[:, k, c0:c0 + n], rhs=wb[:, k, 0:ncols],
                    start=(k == 0), stop=(k == KC - 1)),
                    reads=[kb, "strm"], writes=[("ps", bk)])
            evac(i, c0, n, pv, bk)

    def hgrn_m1(self, strm):
        S, ps, nc = self.S, self.ps, self.nc
        R1 = 99840
        wbufs = [self.view(R1 + i * 8192, BF16, [KC, 256]) for i in range(2)]
        off = [R1 + 16384]

        def alloc(dt, shape):
            n = 1
            for x in shape:
                n *= x
            nb = n * (4 if dt == F32 else 2)
            nb = (nb + 63) // 64 * 64
            v = self.view(off[0], dt, shape)
            off[0] += nb
            return v
        logf = alloc(F32, [2, NT]); Bg = alloc(F32, [2, NT]); Bsh = alloc(F32, [2, NT])
        tA = alloc(F32, [2, NT]); tB = alloc(F32, [2, NT])
        kk = alloc(BF16, [2, NT]); qs = alloc(BF16, [2, NT]); qt = alloc(BF16, [2, NT])
        kt = alloc(BF16, [2, NT]); kh64 = alloc(BF16, [2, NT]); kh128 = alloc(BF16, [2, NT])
        vv = alloc(BF16, [9, 256])
        PTs = [alloc(BF16, [2, 128]) for _ in range(2)]; khTs = [alloc(BF16, [2, 128]) for _ in range(2)]
        xs_st = alloc(BF16, [9, 256]); xa_st = alloc(F32, [9, 2])
        Qc = self.view(0, BF16, [8, NREAL]); oloc = self.view(16384, BF16, [8, NREAL])
        cv = self.cv
        lbc, omlc = self.lbc, self.omlc
        psb = lambda bk: ps[:, bk, :].bitcast(BF16)
        TL = [(i, 128 * i, 128) for i in range(8)] + [(8, NREAL, NMETA)]
        flat = lambda v: v.rearrange("p h t -> p (h t)")
        r64 = lambda v: v[:, :, 0:NREAL].rearrange("p h (c t) -> p h c t", t=64)
        r128 = lambda v: v[:, :, 0:NREAL].rearrange("p h (c t) -> p h c t", t=128)
        mt = lambda v: v[:, :, NREAL:NT]

        for hp in range(4):
            T = ("m1", hp)
            def ev_f(half, ti, t0, t1e, pv, bk, hp=hp):
                h = 2 * hp + half
                S.op("act", lambda e: e.activation(out=tA[:, half, t0:t1e], in_=pv, func=AF.Sigmoid),
                     reads=[("ps", bk)], writes=[("tA", half, ti)])
                S.op("dve", lambda e: e.tensor_scalar(out=tA[:, half, t0:t1e], in0=tA[:, half, t0:t1e],
                                                      scalar1=omlc[:, h:h + 1], scalar2=lbc[:, h:h + 1],
                                                      op0=ALU.mult, op1=ALU.add),
                     reads=[("tA", half, ti), "lb"], writes=[("tA", half, ti)])
                S.op("act", lambda e: e.activation(out=logf[:, half, t0:t1e], in_=tA[:, half, t0:t1e], func=AF.Ln),
                     reads=[("tA", half, ti)], writes=[("logf", half, ti)])
                S.op("pool", lambda e: e.tensor_scalar(out=kk[:, half, t0:t1e], in0=tA[:, half, t0:t1e],
                                                       scalar1=-1.0, scalar2=1.0, op0=ALU.mult, op1=ALU.add),
                     reads=[("tA", half, ti)], writes=[("kk", half, ti)])
            self.proj_fm(T, strm, self.gidx["hf%d" % hp], wbufs, TBS, ev_f)

            def ev_q(half, ti, t0, t1e, pv, bk):
                S.op("act", lambda e: e.activation(out=qs[:, half, t0:t1e], in_=pv, func=AF.Silu),
                     reads=[("ps", bk)], writes=[("qs", half, ti)])
            self.proj_fm(T, strm, self.gidx["hq%d" % hp], wbufs, TBS, ev_q)

            def ev_v(i, c0, n, pv, bk):
                S.op("act", lambda e: e.activation(out=vv[0:n, i, :], in_=pv, func=AF.Copy),
                     reads=[("ps", bk)], writes=[("vv", i)])
            self.proj_tm(T, strm, self.gidx["hi%d" % hp], wbufs, TL, 256, ev_v)

            allk = lambda nm: [(nm, hl, ti) for hl in range(2) for ti in range(3)]
            S.op("dve", lambda e: e.tensor_tensor_scan(out=flat(Bg), data0=flat(logf), data1=flat(logf),
                                                       initial=0.0, op0=ALU.add, op1=ALU.min),
                 reads=allk("logf"), writes=["Bg"])
            S.op("pool", lambda e: e.memset(flat(Bsh)[:, 0:1], 0.0), writes=["Bsh0"])
            S.op("pool", lambda e: e.tensor_copy(out=flat(Bsh)[:, 1:2 * NT], in_=flat(Bg)[:, 0:2 * NT - 1]),
                 reads=["Bg"], writes=["Bsh"])
            S.op("dve", lambda e: e.tensor_tensor(out=r64(tB), in0=r64(Bg),
                                                  in1=r64(Bsh)[:, :, :, 0:1].to_broadcast([128, 2, 16, 64]),
                                                  op=ALU.subtract),
                 reads=["Bg", "Bsh", "Bsh0"], writes=["tBr"])
            S.op("dve", lambda e: e.tensor_tensor(out=mt(tB), in0=mt(Bg),
                                                  in1=mt(Bsh)[:, :, 0:1].to_broadcast([128, 2, NMETA]),
                                                  op=ALU.subtract),
                 reads=["Bg", "Bsh", "Bsh0"], writes=["tBm"])
            S.op("pool", lambda e: e.tensor_tensor(out=r128(tA), in0=r128(Bg),
                                                   in1=r128(Bsh)[:, :, :, 0:1].to_broadcast([128, 2, 8, 128]),
                                                   op=ALU.subtract),
                 reads=["Bg", "Bsh", "Bsh0"] + allk("tA"), writes=["tAr"] + allk("tA"))
            S.op("pool", lambda e: e.tensor_copy(out=mt(tA), in_=mt(tB)),
                 reads=["tBm"], writes=["tAm"])
            TBk, TAk = ["tBr", "tBm"], ["tAr", "tAm"] + allk("tA")
            S.op("act", lambda e: e.activation(out=flat(Bg), in_=flat(tB), func=AF.Exp),
                 reads=TBk + ["Bsh", "tAr"], writes=["Bg"])
            S.op("dve", lambda e: e.tensor_tensor(out=flat(qt), in0=flat(qs), in1=flat(Bg), op=ALU.mult),
                 reads=["Bg"] + allk("qs"), writes=["qt"])
            S.op("act", lambda e: e.activation(out=flat(Bsh), in_=flat(tB), func=AF.Exp, scale=-1.0),
                 reads=TBk + ["Bsh", "tAr", "Bsh0"], writes=["Bsh", "Bsh0"])
            S.op("dve", lambda e: e.tensor_tensor(out=flat(kt), in0=flat(kk), in1=flat(Bsh), op=ALU.mult),
                 reads=["Bsh"] + allk("kk"), writes=["kt"])
            S.op("dve", lambda e: e.tensor_tensor(out=r64(Bg), in0=r64(tB),
                                                  in1=r64(tB)[:, :, :, 63:64].to_broadcast([128, 2, 16, 64]),
                                                  op=ALU.subtract),
                 reads=TBk + ["qt"], writes=["Bg"])
            S.op("act", lambda e: e.activation(out=r64(Bg), in_=r64(Bg), func=AF.Exp, scale=-1.0),
                 reads=["Bg"], writes=["Bg"])
            S.op("dve", lambda e: e.tensor_tensor(out=r64(kh64), in0=r64(kk), in1=r64(Bg), op=ALU.mult),
                 reads=["Bg"] + allk("kk"), writes=["kh64"])
            S.op("act", lambda e: e.activation(out=flat(Bsh), in_=flat(tA), func=AF.Exp),
                 reads=TAk + ["kt"], writes=["Bsh"])
            S.op("dve", lambda e, hp=hp: e.tensor_tensor(out=Qc[:, 2 * hp:2 * hp + 2, :], in0=qs[:, :, 0:NREAL],
                                                         in1=Bsh[:, :, 0:NREAL], op=ALU.mult),
                 reads=["Bsh"] + allk("qs"), writes=[("Qc", hp)])
            S.op("pool", lambda e: e.tensor_copy(out=xa_st[:, 0:8, :].rearrange("p i h -> p h i"),
                                                 in_=r128(Bsh)[:, :, :, 127]),
                 reads=["Bsh"], writes=["xa_st"])
            S.op("pool", lambda e: e.tensor_copy(out=xa_st[:, 8, :], in_=Bsh[:, :, NT - 1]),
                 reads=["Bsh"], writes=["xa_st"])
            S.op("dve", lambda e: e.tensor_tensor(out=r128(Bg), in0=r128(tA),
                                                  in1=r128(tA)[:, :, :, 127:128].to_broadcast([128, 2, 8, 128]),
                                                  op=ALU.subtract),
                 reads=TAk + ["kh64"], writes=["Bg"])
            S.op("dve", lambda e: e.tensor_tensor(out=mt(Bg), in0=mt(tA),
                                                  in1=mt(tA)[:, :, NMETA - 1:NMETA].to_broadcast([128, 2, NMETA]),
                                                  op=ALU.subtract),
                 reads=TAk + ["kh64"], writes=["Bg"])
            S.op("act", lambda e: e.activation(out=flat(Bg), in_=flat(Bg), func=AF.Exp, scale=-1.0),
                 reads=["Bg"], writes=["Bg"])
            S.op("dve", lambda e: e.tensor_tensor(out=flat(kh128), in0=flat(kk), in1=flat(Bg), op=ALU.mult),
                 reads=["Bg"] + allk("kk"), writes=["kh128"])

            def tok_block(i, c0, n, hp=hp):
                par = i % 2
                PT, khT = PTs[par], khTs[par]
                bS, bO = (2, 3) if par == 0 else (6, 7)
                t0c, s0c = par * 256, par * 256
                if n == 128:
                    for hl in range(2):
                        o0 = hl * 128
                        S.op("pe", lambda e, hl=hl, o0=o0, c0=c0: e.matmul(
                            ps[:, bS, o0:o0 + 64], lhsT=kt[:, hl, c0:c0 + 128], rhs=qt[:, hl, c0:c0 + 64],
                            start=True, stop=True), reads=["kt", "qt"], writes=[("ps", bS)])
                        S.op("pe", lambda e, hl=hl, o0=o0, c0=c0: e.matmul(
                            ps[0:64, bS, o0 + 64:o0 + 128], lhsT=kh64[:, hl, c0:c0 + 64],
                            rhs=qt[:, hl, c0 + 64:c0 + 128], start=True, stop=True),
                            reads=["kh64", "qt"], writes=[("ps", bS)])
                        S.op("pe", lambda e, hl=hl, o0=o0, c0=c0: e.matmul(
                            ps[64:128, bS, o0 + 64:o0 + 128], lhsT=kt[:, hl, c0 + 64:c0 + 128],
                            rhs=qt[:, hl, c0 + 64:c0 + 128], start=True, stop=True),
                            reads=["kt", "qt"], writes=[("ps", bS)])
                    for hl in range(2):
                        S.op("dve", lambda e, hl=hl: e.tensor_tensor(
                            out=PT[:, hl, :], in0=ps[:, bS, hl * 128:(hl + 1) * 128], in1=self.mask2, op=ALU.mult),
                            reads=[("ps", bS), "mask2"], writes=[("PT", par, hl)])
                    for hl in range(2):
                        S.op("pe", lambda e, hl=hl, i=i: e.matmul(
                            ps[:, bO, hl * 128:(hl + 1) * 128], lhsT=vv[:, i, hl * 128:(hl + 1) * 128],
                            rhs=PT[:, hl, :], start=True, stop=True),
                            reads=[("vv", i), ("PT", par, hl)], writes=[("ps", bO)])
                    S.op("act", lambda e, hp=hp, c0=c0: e.activation(
                        out=oloc[:, 2 * hp:2 * hp + 2, c0:c0 + 128],
                        in_=ps[:, bO, 0:256].rearrange("p (h t) -> p h t", h=2), func=AF.Copy),
                        reads=[("ps", bO)], writes=[("oloc", hp, i)])
                for hl in range(2):
                    S.op("pe", lambda e, hl=hl, c0=c0, n=n: e.transpose(
                        out=psb(4)[0:n, t0c * 2 + hl * 128:t0c * 2 + (hl + 1) * 128], in_=kh128[:, hl, c0:c0 + n],
                        identity=self.ident_bf),
                        reads=["kh128", "ident"], writes=[("ps4", par)])
                S.op("dve", lambda e, n=n: e.tensor_copy(out=khT[0:n].rearrange("p h d -> p (h d)"),
                                                         in_=psb(4)[0:n, t0c * 2:t0c * 2 + 256]),
                     reads=[("ps4", par)], writes=[("khT", par)])
                for hl in range(2):
                    S.op("pe", lambda e, hl=hl, i=i, n=n: e.matmul(
                        ps[:, 5, s0c + hl * 128:s0c + (hl + 1) * 128], lhsT=khT[0:n, hl, :],
                        rhs=vv[0:n, i, hl * 128:(hl + 1) * 128], start=True, stop=True),
                        reads=[("khT", par), ("vv", i)], writes=[("ps5", par)])
                S.op("dve", lambda e, i=i: e.tensor_copy(out=xs_st[:, i, :], in_=ps[:, 5, s0c:s0c + 256]),
                     reads=[("ps5", par)], writes=[("xs_st", i)])
            for (i_, c0_, n_) in TL:
                tok_block(i_, c0_, n_)
            for q3 in range(3):
                S.dma("sp", lambda e, hp=hp, q3=q3: e.dma_start(
                    out=self.xs[q3].rearrange("p (i c) -> p i c", i=3)[:, :, hp * 256:(hp + 1) * 256],
                    in_=xs_st[:, 3 * q3:3 * q3 + 3, :]),
                    reads=[("xs_st", i) for i in range(9)], writes=[("xs", hp, q3)])
            S.dma("sp", lambda e, hp=hp: e.dma_start(
                out=self.xa.rearrange("p (i c) -> p i c", i=9)[:, :, 2 * hp:2 * hp + 2], in_=xa_st),
                reads=["xa_st"], writes=[("xa", hp)])
        S.barrier()
        rg = [[0, 1, 2, 3], [4, 5, 6, 7]]
        for q3 in range(3):
            S.coll(lambda e, q3=q3: e.collective_compute("AllGather", ALU.bypass, replica_groups=rg,
                                                         ins=[self.xs[q3]], outs=[self.xg[q3]]), writes=[("xg", q3)])
        S.coll(lambda e: e.collective_compute("AllGather", ALU.bypass, replica_groups=rg,
                                              ins=[self.xa], outs=[self.xag]), writes=["xag"])

    def hgrn_m2(self, strm):
        S, ps, nc = self.S, self.ps, self.nc
        R1 = 99840
        wbufs = [self.view(R1 + i * 8192, BF16, [KC, 256]) for i in range(2)]
        off = [R1 + 16384]

        def alloc(dt, shape):
            n = 1
            for x in shape:
                n *= x
            nb = n * (4 if dt == F32 else 2)
            nb = (nb + 63) // 64 * 64
            v = self.view(off[0], dt, shape)
            off[0] += nb
            return v
        sgate = alloc(BF16, [8, NREAL])
        Scur = alloc(F32, [8, 128]); SmF = alloc(F32, [8, 128])
        SAb = [alloc(BF16, [8, 128]) for _ in range(3)]
        Aall = alloc(F32, [4, 72])
        OF = alloc(F32, [8, 128]); OSQ = alloc(F32, [8, 128]); RS = alloc(F32, [8, 128])
        Qc = self.view(0, BF16, [8, NREAL]); oloc = self.view(16384, BF16, [8, NREAL])
        yhg = self.view(32768, BF16, [8, NREAL]); Smine = self.view(49152, BF16, [8, 8, 128])
        f2 = lambda v: v.rearrange("p h t -> p (h t)")
        RTB = TBS[0:2]

        for g4 in range(4):
            def ev_g(half, ti, t0, t1e, pv, bk, g4=g4):
                h = 2 * g4 + half
                S.op("act", lambda e: e.activation(out=sgate[:, h, t0:t1e], in_=pv, func=AF.Silu),
                     reads=[("ps", bk)], writes=[("sgate", h, ti)])
                S.op("pool", lambda e: e.tensor_scalar(out=sgate[:, h, t0:t1e], in0=sgate[:, h, t0:t1e],
                                                       scalar1=self.gnc[:, h:h + 1], scalar2=None, op0=ALU.mult),
                     reads=[("sgate", h, ti), "gn"], writes=[("sgate", h, ti)])
            self.proj_fm("m2", strm, self.gidx["hg%d" % g4], wbufs, RTB, ev_g)

        def out_block(i):
            c0 = 128 * i
            for h in range(8):
                bk = 2 + h // 4
                S.op("pe", lambda e, h=h, i=i, c0=c0, bk=bk: e.matmul(
                    ps[:, bk, (h % 4) * 128:(h % 4 + 1) * 128], lhsT=Smine[:, i, h, :], rhs=Qc[:, h, c0:c0 + 128],
                    start=True, stop=True),
                    reads=[("Smine", i), "Qc"], writes=[("ps", bk)])
            S.op("dve", lambda e, c0=c0: e.tensor_tensor(
                out=OF, in0=ps[:, 2:4, :].rearrange("p a (h t) -> p (a h) t", h=4), in1=oloc[:, :, c0:c0 + 128],
                op=ALU.add),
                reads=[("ps", 2), ("ps", 3), "oloc"], writes=["OF"])
            S.op("act", lambda e: e.activation(out=f2(OSQ), in_=f2(OF), func=AF.Square),
                 reads=["OF"], writes=["OSQ"])
            for a in range(2):
                S.op("pe", lambda e, a=a: e.matmul(ps[:, 4 + a, :], lhsT=self.ones128, rhs=f2(OSQ)[:, a * 512:(a + 1) * 512],
                                                   start=True, stop=True),
                     reads=["OSQ", "ones128"], writes=[("ps", 4 + a)])
            S.op("act", lambda e: e.activation(out=f2(RS), in_=ps[:, 4:6, :].rearrange("p a b -> p (a b)"),
                                               func=AF.Ln, bias=self.eps_rms, scale=1.0),
                 reads=[("ps", 4), ("ps", 5), "eps"], writes=["RS"])
            S.op("act", lambda e: e.activation(out=f2(RS), in_=f2(RS), func=AF.Exp, scale=-0.5),
                 reads=["RS"], writes=["RS"])

        def out_block_b(i):
            c0 = 128 * i
            S.op("dve", lambda e: e.tensor_tensor(out=f2(OF), in0=f2(OF), in1=f2(RS), op=ALU.mult),
                 reads=["OF", "RS"], writes=["OF"])
            S.op("dve", lambda e, c0=c0: e.tensor_tensor(out=yhg[:, :, c0:c0 + 128], in0=OF, in1=sgate[:, :, c0:c0 + 128],
                                                         op=ALU.mult),
                 reads=["OF"] + [("sgate", h, c0 // 512) for h in range(8)], writes=[("yhg", i)])

        S.dma("sp", lambda e: e.dma_start(out=Aall, in_=self.xag.rearrange("(r p) c -> p r c", p=128)),
              reads=["xag"], writes=["Aall"])
        xg3 = [x_.rearrange("(r p) (i c) -> r p i c", p=128, i=3) for x_ in self.xg]
        S.dma("sp", lambda e: e.dma_start(out=f2(SAb[2]), in_=xg3[2][0, :, 2, :]), reads=[("xg", 2)],
              writes=[("SAb", 2)])
        S.op("dve", lambda e: e.tensor_copy(out=f2(Scur), in_=f2(SAb[2])), reads=[("SAb", 2)], writes=["Scur"])
        for g in range(32):
            r, i = g % 4, g // 4
            sb = SAb[g % 3]
            S.dma("sp", lambda e, sb=sb, r=r, i=i: e.dma_start(out=f2(sb), in_=xg3[i // 3][r, :, i % 3, :]),
                  reads=[("xg", i // 3)], writes=[("SAb", g % 3)])
            if r == 0:
                S.op("dve", lambda e: e.tensor_scalar(out=f2(SmF), in0=f2(Scur), scalar1=self.selc[:, 0:1],
                                                      scalar2=None, op0=ALU.mult),
                     reads=["Scur", "sel"], writes=["SmF"])
            else:
                dst = SmF if r < 3 else Smine[:, i]
                S.op("dve", lambda e, r=r, dst=dst: e.scalar_tensor_tensor(
                    out=f2(dst), in0=f2(Scur), scalar=self.selc[:, r:r + 1], in1=f2(SmF),
                    op0=ALU.mult, op1=ALU.add),
                    reads=["Scur", "sel", "SmF"], writes=(["SmF"] if r < 3 else [("Smine", i)]))
            if g < 31:
                for h in range(8):
                    S.op("dve", lambda e, h=h, r=r, i=i, sb=sb: e.scalar_tensor_tensor(
                        out=Scur[:, h, :], in0=Scur[:, h, :], scalar=Aall[:, r, i * 8 + h:i * 8 + h + 1],
                        in1=sb[:, h, :], op0=ALU.mult, op1=ALU.add),
                        reads=["Scur", "Aall", ("SAb", g % 3)], writes=["Scur"])
            if g % 4 == 3:
                out_block(g // 4)
            if g % 4 == 1 and g >= 5:
                out_block_b((g - 5) // 4)
        out_block_b(7)

        S.barrier()

    def attn_m3(self, strm):
        S, ps, nc = self.S, self.ps, self.nc
        R1 = 99840
        psb = lambda bk: ps[:, bk, :].bitcast(BF16)
        K_all = self.view(R1, BF16, [2, 4112])
        V_all = self.view(R1 + 16448, BF16, [33, 258])
        IK_all = self.view(R1 + 33536, BF16, [4096])
        AugK = self.view(R1 + 41728, BF16, [4112])
        qT = self.view(R1 + 49952, BF16, [8, NREAL])
        iqT = self.view(R1 + 66336, BF16, [8, NREAL])
        sc = self.view(R1 + 82720, F32, [4096])
        wbufs = [self.view(R1 + 82720 + i * 8192, BF16, [KC, 256]) for i in range(2)]
        Dg = self.view(R1 + 99104, BF16, [16, 128])
        yatt = self.view(0, BF16, [8, NREAL])
        mb = self.view(16384, BF16, [4096])
        mbT = self.view(24576, BF16, [32, 128])
        junk = self.view(49152, U8, [4096])
        iqz = self.view(49152 + 4096, BF16, [16, 128])
        rh = [self.view(57344 + q * 1024, BF16, [512]) for q in range(4)]
        ya = self.view(61440, BF16, [8, 128])
        PTb = [self.view(61440 + q * 2048, BF16, [1024]) for q in range(2)]
        cbt = self.view(65536, BF16, [4, 128])
        kst = self.view(49152, BF16, [2, NREAL])
        vst = self.view(49152 + 4096, BF16, [8, 258])
        ikst = self.view(49152 + 4096 + 4160, BF16, [NREAL])
        iktmp = self.view(49152 + 10304, F32, [64])
        ikn2 = self.view(49152 + 10304 + 256, BF16, [128])
        cst = self.cst
        AugQ = cst[:, 664:1176].bitcast(BF16)
        AugR = cst[:, 1176:1688].bitcast(BF16)
        wq = cst[:, 1688:1816].rearrange("p (i h) -> p i h", h=16)
        H = cst[:, 1816:1848]
        mx = cst[:, 1848:2008].rearrange("p (h c) -> p h c", h=8)
        mrow = cst[:, 2008:2016]; cc = cst[:, 2016:2024]; rs = cst[:, 2024:2032]
        Bt = cst[:, 2032:2033]; Wc = cst[:, 2033:2034]; mid = cst[:, 2034:2035]; cnt = cst[:, 2035:2036]
        u2 = cst[:, 2036:2037]; tau = cst[:, 2037:2038]; rstd1 = cst[:, 2038:2039]
        AQc = cst[:, 2040:2104].rearrange("p (i h) -> p i h", h=8)
        pw = cst[:, 2104:2136]
        gik = cst[:, 2136:2200]; bik = cst[:, 2200:2264]
        st6 = cst[:, 2264:2270]; mv = cst[:, 2270:2272]
        TL = [(i, 128 * i, 128) for i in range(8)] + [(8, NREAL, NMETA)]
        RTB = TBS[0:2]
        NB = 16

        S.dma("sp", lambda e: e.dma_start(out=cst[:, 2040:2264], in_=self.catt), writes=["catt"])
        S.op("pool", lambda e: e.memset(AugK[0:65, :], 0.0), writes=["AugK"])
        S.op("pool", lambda e: e.memset(AugQ[0:65, :], 0.0), writes=["AugQ"])
        S.op("pool", lambda e: e.memset(AugR[0:65, :], 0.0), writes=["AugR"])
        for rr in range(3):
            S.dma("pool", lambda e, rr=rr: e.dma_start(out=AugK[32 * rr:32 * rr + 1, :], in_=self.augk[rr:rr + 1, :]),
                  writes=["AugK"])
        for rr in range(2):
            S.dma("pool", lambda e, rr=rr: e.dma_start(out=AugQ[32 * rr:32 * rr + 1, :], in_=self.augs[rr:rr + 1, :]),
                  writes=["AugQ"])
            S.dma("pool", lambda e, rr=rr: e.dma_start(out=AugR[32 * rr:32 * rr + 1, :], in_=self.augs[rr:rr + 1, :]),
                  writes=["AugR"])
        S.dma("pool", lambda e: e.dma_start(out=cbt, in_=self.cbt_d.rearrange("p (r s) -> p r s", r=4)), writes=["cbt"])
        S.op("dve", lambda e: e.memset(vst.rearrange("p i (k c) -> p i k c", k=2)[:, :, :, 128:129], 1.0),
             writes=["vst1"])
        S.op("dve", lambda e: e.memset(V_all[:, 32, :].rearrange("p (k c) -> p k c", k=2)[:, :, 128:129], 1.0),
             writes=["V1"])

        def ev_k(half, ti, t0, t1e, pv, bk):
            if ti < 2:
                S.op("act", lambda e: e.activation(out=kst[:, half, t0:t1e], in_=pv, func=AF.Copy),
                     reads=[("ps", bk)], writes=[("kst", half, ti)])
            else:
                S.op("act", lambda e: e.activation(out=K_all[:, half, 4096:4112], in_=pv, func=AF.Copy),
                     reads=[("ps", bk)], writes=[("Kmeta", half)])
        self.proj_fm("m3", strm, self.gidx["ak"], wbufs, TBS, ev_k)

        def ev_v(i, c0, n, pv, bk):
            src = pv.rearrange("p (k c) -> p k c", k=2)
            if i < 8:
                dst = vst[:, i, :].rearrange("p (k c) -> p k c", k=2)[:, :, 0:128]
                S.op("act", lambda e: e.activation(out=dst, in_=src, func=AF.Copy),
                     reads=[("ps", bk), "vst1"], writes=[("vst", i)])
            else:
                dst = V_all[0:n, 32, :].rearrange("p (k c) -> p k c", k=2)[:, :, 0:128]
                S.op("act", lambda e: e.activation(out=dst, in_=src, func=AF.Copy),
                     reads=[("ps", bk), "V1"], writes=["Vmeta"])
        self.proj_tm("m3", strm, self.gidx["av"], wbufs, TL, 256, ev_v)

        def ev_ik(i, c0, n, pv, bk):
            S.op("dve", lambda e: e.bn_stats(out=st6, in_=pv[:, 0:64]), reads=[("ps", bk)], writes=["st6"])
            S.op("dve", lambda e: e.bn_aggr(out=mv, in_=st6), reads=["st6"], writes=["mv"])
            S.op("act", lambda e: e.activation(out=rstd1, in_=mv[:, 1:2], func=AF.Sqrt, bias=self.eps_ik, scale=1.0),
                 reads=["mv", "eps"], writes=["rstd1"])
            S.op("dve", lambda e: e.reciprocal(out=rstd1, in_=rstd1), reads=["rstd1"], writes=["rstd1"])
            S.op("dve", lambda e: e.tensor_scalar(out=iktmp, in0=pv[:, 0:64], scalar1=mv[:, 0:1], scalar2=rstd1,
                                                  op0=ALU.subtract, op1=ALU.mult),
                 reads=[("ps", bk), "mv", "rstd1"], writes=["iktmp"])
            S.op("dve", lambda e: e.tensor_tensor(out=iktmp, in0=iktmp, in1=gik, op=ALU.mult),
                 reads=["iktmp", "catt"], writes=["iktmp"])
            S.op("dve", lambda e: e.tensor_tensor(out=ikn2[:, 0:64], in0=iktmp, in1=bik, op=ALU.add),
                 reads=["iktmp", "catt"], writes=["ikn2a"])
            S.op("pool", lambda e: e.tensor_copy(out=ikn2[:, 64:128], in_=ikn2[:, 0:64]),
                 reads=["ikn2a"], writes=["ikn2b"])
            S.op("act", lambda e, i=i: e.activation(out=wq[:, i, :], in_=pv[:, 64:80], func=AF.Copy,
                                                    scale=0.25 * 0.125),
                 reads=[("ps", bk)], writes=[("wq", i)])
            S.op("pe", lambda e: e.transpose(out=psb(2)[:, 0:128], in_=ikn2, identity=self.ident_bf),
                 reads=["ikn2a", "ikn2b", "ident"], writes=[("ps", 2)])
            S.op("act", lambda e, c0=c0: e.activation(out=ikst[:, c0:c0 + 128], in_=psb(2)[:, 0:128], func=AF.Copy),
                 reads=[("ps", 2)], writes=[("ikst", i)])
        self.proj_tm("m3", strm, self.gidx["ikw"], wbufs, TL[0:8], 80, ev_ik)

        S.dma("sp", lambda e: e.dma_start(out=self.ks.rearrange("p (k t) -> p k t", k=2), in_=kst),
              reads=[("kst", hh, ti) for hh in range(2) for ti in range(2)], writes=["ks"])
        S.dma("sp", lambda e: e.dma_start(out=self.vs[:, 0:2064].rearrange("p (i c) -> p i c", i=8), in_=vst),
              reads=[("vst", i) for i in range(8)] + ["vst1"], writes=["vs"])
        S.dma("sp", lambda e: e.dma_start(out=self.vs[:, 2064:3088], in_=ikst),
              reads=[("ikst", i) for i in range(8)], writes=["vs2"])
        S.barrier()
        rg = [[0, 1, 2, 3], [4, 5, 6, 7]]
        S.coll(lambda e: e.collective_compute("AllGather", ALU.bypass, replica_groups=rg,
                                              ins=[self.ks], outs=[self.kg]), writes=["kg"])
        S.coll(lambda e: e.collective_compute("AllGather", ALU.bypass, replica_groups=rg,
                                              ins=[self.vs], outs=[self.vg]), writes=["vg"])

        for g4 in range(4):
            def ev_q(half, ti, t0, t1e, pv, bk, g4=g4):
                h = 2 * g4 + half
                S.op("act", lambda e: e.activation(out=qT[:, h, t0:t1e], in_=pv, func=AF.Copy, scale=128.0 ** -0.5),
                     reads=[("ps", bk)], writes=[("qT", h, ti)])
            self.proj_fm("m3", strm, self.gidx["aq%d" % g4], wbufs, RTB, ev_q)
        for g4 in range(4):
            def ev_iq(half, ti, t0, t1e, pv, bk, g4=g4):
                h = 2 * g4 + half
                S.op("dve", lambda e: e.tensor_copy(out=iqT[:, h, t0:t1e], in_=pv),
                     reads=[("ps", bk)], writes=[("iqT", h, ti)])
            self.proj_fm("m3", strm, self.gidx["iq%d" % g4], wbufs, RTB, ev_iq)

        for r in range(4):
            S.dma("sp", lambda e, r=r: e.dma_start(
                out=K_all[:, :, r * 1024:(r + 1) * 1024],
                in_=self.kg[r * 128:(r + 1) * 128, :].rearrange("p (k t) -> p k t", k=2)),
                reads=["kg"], writes=["K_all"])
            S.dma("sp", lambda e, r=r: e.dma_start(
                out=V_all[:, r * 8:(r + 1) * 8, :],
                in_=self.vg[r * 128:(r + 1) * 128, 0:2064].rearrange("p (i c) -> p i c", i=8)),
                reads=["vg"], writes=["V_all"])
            S.dma("sp", lambda e, r=r: e.dma_start(
                out=IK_all[:, r * 1024:(r + 1) * 1024], in_=self.vg[r * 128:(r + 1) * 128, 2064:3088]),
                reads=["vg"], writes=["IK_all"])
        S.barrier()

        S.op("pool", lambda e: e.memset(iqz.rearrange("p h t -> p (h t)"), 0.0), writes=["iqz"])
        sc4 = sc.rearrange("p (r c) -> p r c", r=4)
        mb4 = mb.rearrange("p (r c) -> p r c", r=4)
        jk4 = junk.rearrange("p (r c) -> p r c", r=4)
        def geom(i):
            q0 = 128 * i
            nk = 128 * (i + 1)
            pieces = [(r, c0, min(512, nk - c0)) for r in range(4) for c0 in range(0, nk, 512)]
            return q0, nk, pieces

        def st_idx(i):
            q0, nk, pieces = geom(i)
            for h in range(16):
                S.op("pool", lambda e, h=h: e.tensor_scalar(out=Dg[:, h, :], in0=self.ident_bf,
                                                            scalar1=wq[:, i, h:h + 1], scalar2=None, op0=ALU.mult),
                     reads=["ident", ("wq", i)], writes=["Dg"])
            for h in range(16):
                hb = h % 2
                eng = "pool" if h % 2 == 0 else "act"
                if eng == "pool":
                    S.op("pool", lambda e, h=h, hb=hb: e.tensor_copy(
                        out=iqz[hb * 64:(hb + 1) * 64, h, :], in_=iqT[hb * 64:(hb + 1) * 64, h // 2, q0:q0 + 128]),
                        reads=["iqT", "iqz"], writes=[("iqzh", h)])
                else:
                    S.op("act", lambda e, h=h, hb=hb: e.activation(
                        out=iqz[hb * 64:(hb + 1) * 64, h, :], in_=iqT[hb * 64:(hb + 1) * 64, h // 2, q0:q0 + 128],
                        func=AF.Copy),
                        reads=["iqT", "iqz"], writes=[("iqzh", h)])
            for pi, (r, c0, cn) in enumerate(pieces):
                col0 = r * 1024 + c0
                accb = 4 + pi % 2

                def head_mm(h, cn=cn, col0=col0):
                    bk, hb = h % 4, h % 2
                    S.op("pe", lambda e: e.matmul(
                        ps[:, bk, 0:cn], lhsT=iqz[:, h, :],
                        rhs=IK_all[:, col0:col0 + cn], start=True, stop=True),
                        reads=["IK_all", ("iqzh", h)], writes=[("ps", bk)])
                    if h % 8 in (0, 3, 6):
                        S.op("act", lambda e: e.activation(out=rh[bk][:, 0:cn], in_=ps[:, bk, 0:cn], func=AF.Relu),
                             reads=[("ps", bk)], writes=[("rh", bk)])
                    else:
                        S.op("dve", lambda e: e.tensor_scalar(out=rh[bk][:, 0:cn], in0=ps[:, bk, 0:cn], scalar1=0.0,
                                                              scalar2=None, op0=ALU.max),
                             reads=[("ps", bk)], writes=[("rh", bk)])

                def head_acc(h, cn=cn, accb=accb):
                    bk = h % 4
                    S.op("pe", lambda e: e.matmul(
                        ps[:, accb, 0:cn], lhsT=Dg[:, h, :], rhs=rh[bk][:, 0:cn], start=(h == 0), stop=(h == 15)),
                        reads=["Dg", ("rh", bk)], writes=[("ps", accb)])
                for h in range(16):
                    head_mm(h)
                    if h >= 2:
                        head_acc(h - 2)
                head_acc(14)
                head_acc(15)
                S.op("act", lambda e, accb=accb, col0=col0, cn=cn: e.activation(
                    out=sc[:, col0:col0 + cn], in_=ps[:, accb, 0:cn], func=AF.Copy),
                    reads=[("ps", accb)], writes=["sc"])

        def st_bis(i):
            q0, nk, pieces = geom(i)
            scv, mbv, jkv = sc4[:, :, 0:nk], mb4[:, :, 0:nk], jk4[:, :, 0:nk]
            S.op("dve", lambda e: e.reduce_max(out=Bt, in_=scv, axis=AX.XY, apply_absolute_value=True),
                 reads=["sc"], writes=["Bt"])
            S.op("dve", lambda e: e.tensor_tensor(out=sc4[:, :, q0:q0 + 128], in0=sc4[:, :, q0:q0 + 128], in1=cbt,
                                                  op=ALU.add),
                 reads=["sc", "cbt", "Bt"], writes=["sc"])
            S.op("dve", lambda e: e.tensor_scalar(out=Wc, in0=Bt, scalar1=2.0002, scalar2=1e-6,
                                                  op0=ALU.mult, op1=ALU.add), reads=["Bt"], writes=["Wc"])
            S.op("dve", lambda e: e.tensor_scalar(out=H[:, 0:NB + 1], in0=pw[:, 0:NB + 1], scalar1=Wc, scalar2=None,
                                                  op0=ALU.mult), reads=["Wc", "catt"], writes=["H"])
            S.op("dve", lambda e: e.memset(mid, 0.0), writes=["mid"])
            for k in range(NB):
                S.op("dve", lambda e: e.tensor_scalar(
                    out=jkv, in0=scv, scalar1=mid, scalar2=0.0, op0=ALU.is_ge, op1=ALU.add, accum_out=cnt),
                    reads=["sc", "mid"], writes=["junk", "cnt"])
                S.op("dve", lambda e, k=k: e.tensor_scalar(out=u2, in0=cnt, scalar1=256.0, scalar2=H[:, k:k + 1],
                                                           op0=ALU.is_ge, op1=ALU.mult),
                     reads=["cnt", "H"], writes=["u2"])
                S.op("dve", lambda e, k=k: e.scalar_tensor_tensor(out=mid, in0=mid, scalar=H[:, k + 1:k + 2], in1=u2,
                                                                  op0=ALU.subtract, op1=ALU.add),
                     reads=["mid", "H", "u2"], writes=["mid"])
            S.op("dve", lambda e: e.tensor_tensor(out=tau, in0=mid, in1=H[:, NB:NB + 1], op=ALU.subtract),
                 reads=["mid", "H"], writes=["tau"])
            S.op("dve", lambda e: e.tensor_scalar(
                out=mbv, in0=scv, scalar1=tau, scalar2=-30000.0, op0=ALU.is_lt, op1=ALU.mult),
                reads=["sc", "tau"], writes=["mb"])

        def st_mbT(i):
            kts = [(r, ip) for r in range(4) for ip in range(i + 1)]
            for g0 in range(0, len(kts), 8):
                grp = kts[g0:g0 + 8]
                bk = 6 + (g0 // 8) % 2
                for s_, (r, ip) in enumerate(grp):
                    S.op("pe", lambda e, bk=bk, s_=s_, r=r, ip=ip: e.transpose(
                        out=psb(bk)[:, s_ * 128:(s_ + 1) * 128], in_=mb[:, r * 1024 + ip * 128:r * 1024 + ip * 128 + 128],
                        identity=self.ident_bf),
                        reads=["mb", "ident"], writes=[("ps", bk)])
                for s_, (r, ip) in enumerate(grp):
                    S.op("act", lambda e, bk=bk, s_=s_, r=r, ip=ip: e.activation(
                        out=mbT[:, r * 8 + ip, :], in_=psb(bk)[:, s_ * 128:(s_ + 1) * 128], func=AF.Copy),
                        reads=[("ps", bk)], writes=["mbT"])

        def st_passA(i):
            q0, nk, pieces = geom(i)
            S.op("dve", lambda e: e.memset(ps[:, 5:8, :].rearrange("p a b -> p (a b)"), 0.0),
                 writes=[("ps", 5), ("ps", 6), ("ps", 7)])
            S.dma("pool", lambda e: e.dma_start(out=AugQ[64:65, :], in_=self.augq[i:i + 1, :]), writes=["AugQ"])
            pcs = pieces + [(4, 0, NMETA)]
            for h in range(8):
                kvh = h // 4
                for pi, (r, c0, cn) in enumerate(pcs):
                    col0 = r * 1024 + c0
                    bk = (h * len(pcs) + pi) % 4
                    meta = (r == 4)
                    S.op("pe", lambda e, bk=bk, h=h, kvh=kvh, col0=col0, cn=cn: e.matmul(
                        ps[:, bk, 0:cn], lhsT=qT[:, h, q0:q0 + 128], rhs=K_all[:, kvh, col0:col0 + cn],
                        start=True, stop=False),
                        reads=["K_all", "qT", ("Kmeta", kvh)], writes=[("ps", bk)])
                    S.op("pe", lambda e, bk=bk, h=h, col0=col0, cn=cn, meta=meta: e.matmul(
                        ps[:, bk, 0:cn], lhsT=AugQ[0:65, h * 128:(h + 1) * 128], rhs=AugK[0:65, col0:col0 + cn],
                        start=False, stop=meta),
                        reads=["AugQ", "AugK"], writes=[("ps", bk)])
                    if not meta:
                        S.op("pe", lambda e, bk=bk, col0=col0, cn=cn: e.matmul(
                            ps[:, bk, 0:cn], lhsT=self.ident_bf, rhs=mb[:, col0:col0 + cn], start=False, stop=True),
                            reads=["mb", "ident"], writes=[("ps", bk)])
                    S.op("dve", lambda e, bk=bk, h=h, pi=pi, cn=cn: e.reduce_max(
                        out=mx[:, h, pi:pi + 1], in_=ps[:, bk, 0:cn], axis=AX.X),
                        reads=[("ps", bk)], writes=["mx"])
            S.op("dve", lambda e: e.reduce_max(out=mrow, in_=mx[:, :, 0:len(pcs)], axis=AX.X),
                 reads=["mx"], writes=["mrow"])
            S.op("dve", lambda e: e.tensor_tensor(out=cc, in0=AQc[:, i, :], in1=mrow, op=ALU.subtract),
                 reads=["mrow", "catt"], writes=["cc"])
            Dc = PTb[1].rearrange("p (h t) -> p h t", h=8)
            for h in range(8):
                S.op("dve", lambda e, h=h: e.tensor_scalar(out=Dc[:, h, :], in0=self.ident_bf, scalar1=cc[:, h:h + 1],
                                                           scalar2=None, op0=ALU.mult),
                     reads=["ident", "cc"], writes=[("PTb", 1)])
            for a_ in range(2):
                S.op("pe", lambda e, a_=a_: e.matmul(ps[:, 4, :], lhsT=self.ones_bf,
                                                     rhs=PTb[1][:, a_ * 512:(a_ + 1) * 512],
                                                     start=True, stop=True),
                     reads=[("PTb", 1), "ones_bf"], writes=[("ps", 4)])
                S.op("dve", lambda e, a_=a_: e.tensor_copy(out=AugR[64:65, a_ * 512:(a_ + 1) * 512], in_=ps[64:65, 4, :]),
                     reads=[("ps", 4)], writes=["AugR"])

        def st_passB(i):
            q0, nk, pieces = geom(i)
            ktl = [(r * 1024 + ip * 128, r * 8 + ip, 128) for r in range(4) for ip in range(i + 1)] + [(4096, 32, NMETA)]
            Oreg = lambda h: ps[:, 5 + h // 3, (h % 3) * 129:(h % 3 + 1) * 129]

            def logits(qi):
                col0, vt, n = ktl[qi]
                meta = (n == NMETA)
                pair = (0, 1) if qi % 2 == 0 else (2, 3)
                for h in range(8):
                    kvh = h // 4
                    out = ps[0:n, pair[h // 4], (h % 4) * 128:(h % 4 + 1) * 128]
                    S.op("pe", lambda e, out=out, kvh=kvh, h=h: e.matmul(
                        out, lhsT=K_all[:, kvh, col0:col0 + n], rhs=qT[:, h, q0:q0 + 128], start=True, stop=False),
                        reads=["K_all", "qT", ("Kmeta", kvh)], writes=[("ps", pair[h // 4])])
                    S.op("pe", lambda e, out=out, h=h: e.matmul(
                        out, lhsT=AugK[0:65, col0:col0 + n], rhs=AugR[0:65, h * 128:(h + 1) * 128],
                        start=False, stop=meta),
                        reads=["AugK", "AugR"], writes=[("ps", pair[h // 4])])
                    if not meta:
                        S.op("pe", lambda e, out=out: e.matmul(
                            out, lhsT=self.ident_bf, rhs=mbT[:, vt, :], start=False, stop=True),
                            reads=["mbT", "ident"], writes=[("ps", pair[h // 4])])
                pt = PTb[qi % 2]
                S.op("act", lambda e: e.activation(
                    out=pt[0:n, :], in_=ps[0:n, pair[0]:pair[0] + 2, :].rearrange("p a b -> p (a b)"), func=AF.Exp),
                    reads=[("ps", pair[0]), ("ps", pair[1])], writes=[("PTb", qi % 2)])

            def pv(qi):
                col0, vt, n = ktl[qi]
                pt = PTb[qi % 2]
                for h in range(8):
                    kvh = h // 4
                    S.op("pe", lambda e, h=h, kvh=kvh: e.matmul(
                        Oreg(h), lhsT=pt[0:n, h * 128:(h + 1) * 128], rhs=V_all[0:n, vt, kvh * 129:(kvh + 1) * 129],
                        start=False, stop=(qi == len(ktl) - 1)),
                        reads=[("PTb", qi % 2), "V_all", "Vmeta"], writes=[("ps", 5 + h // 3)])
            logits(0)
            for qi in range(len(ktl)):
                if qi + 1 < len(ktl):
                    logits(qi + 1)
                pv(qi)

        def st_fin(i):
            q0 = 128 * i
            for b3 in range(3):
                nh = 3 if b3 < 2 else 2
                Ov = ps[:, 5 + b3, 0:nh * 129].rearrange("p (h c) -> p h c", c=129)
                S.op("dve", lambda e, Ov=Ov, b3=b3, nh=nh: e.reciprocal(out=rs[:, 3 * b3:3 * b3 + nh], in_=Ov[:, :, 128]),
                     reads=[("ps", 5 + b3)], writes=[("rs", b3)])
                S.op("dve", lambda e, Ov=Ov, b3=b3, nh=nh: e.tensor_tensor(
                    out=ya[:, 3 * b3:3 * b3 + nh, :], in0=Ov[:, :, 0:128],
                    in1=rs[:, 3 * b3:3 * b3 + nh].unsqueeze(2).to_broadcast([128, nh, 128]), op=ALU.mult),
                    reads=[("ps", 5 + b3), ("rs", b3)], writes=[("ya", b3), ("PTb", 0)])
            for h in range(8):
                S.op("pe", lambda e, h=h: e.transpose(out=psb(4)[:, h * 128:(h + 1) * 128], in_=ya[:, h, :],
                                                      identity=self.ident_bf),
                     reads=[("ya", h // 3), ("PTb", 0), "ident"], writes=[("ps", 4)])
            S.op("act", lambda e: e.activation(out=yatt[:, :, q0:q0 + 128],
                                               in_=psb(4).rearrange("p (h t) -> p h t", h=8), func=AF.Copy),
                 reads=[("ps", 4)], writes=[("yatt", i)])

        st_idx(0)
        st_bis(0)
        st_mbT(0)
        for i in range(8):
            if i + 1 < 8:
                st_idx(i + 1)
            st_passA(i)
            if i + 1 < 8:
                st_bis(i + 1)
            st_passB(i)
            st_fin(i)
            if i + 1 < 8:
                st_mbT(i + 1)
        S.barrier()

    def merge_m4(self, strm, cg, cb):
        S, ps, nc = self.S, self.ps, self.nc
        R1 = 99840
        RTB = TBS[0:2]
        yatt = self.view(0, BF16, [8, NREAL]); yhg = self.view(32768, BF16, [8, NREAL])
        merged = self.view(R1, BF16, [KC, NREAL])
        o = R1 + 32768
        wga = [self.view(o + q * 8192, BF16, [KC, 256]) for q in range(2)]
        wgh = [self.view(o + 16384 + q * 8192, BF16, [KC, 256]) for q in range(2)]
        wba = [self.view(o + 32768 + q * 4096, BF16, [8, 256]) for q in range(2)]
        wbh = [self.view(o + 40960 + q * 4096, BF16, [8, 256]) for q in range(2)]
        tm = [self.view(o + 49152 + q * 2048, F32, [512]) for q in range(4)]
        for mg in range(8):
            q = mg % 2
            S.dma("pool", lambda e, q=q, mg=mg: e.dma_start(out=wga[q], in_=self.win[self.gidx["ga%d" % mg]]),
                  writes=[("wga", q)])
            S.dma("pool", lambda e, q=q, mg=mg: e.dma_start(out=wgh[q], in_=self.win[self.gidx["gh%d" % mg]]),
                  writes=[("wgh", q)])
            S.dma("pool", lambda e, q=q, mg=mg: e.dma_start(out=wba[q], in_=self.wba_d[mg]), writes=[("wba", q)])
            S.dma("pool", lambda e, q=q, mg=mg: e.dma_start(out=wbh[q], in_=self.wbh_d[mg]), writes=[("wbh", q)])
            for half in range(2):
                mc = 2 * mg + half
                hs = slice(half * 128, (half + 1) * 128)
                for ti, (t0, t1e) in enumerate(RTB):
                    pp = (half * 2 + ti) % 2
                    b0 = 4 * pp
                    for k in range(KC):
                        S.op("pe", lambda e, b0=b0, q=q, k=k, hs=hs, t0=t0, t1e=t1e: e.matmul(
                            ps[:, b0, :], lhsT=wga[q][:, k, hs], rhs=strm[:, k, t0:t1e],
                            start=(k == 0), stop=(k == KC - 1)),
                            reads=[("wga", q), "strm"], writes=[("ps", b0)])
                    for k in range(8):
                        S.op("pe", lambda e, b0=b0, q=q, k=k, hs=hs, t0=t0, t1e=t1e: e.matmul(
                            ps[:, b0 + 1, :], lhsT=wba[q][:, k, hs], rhs=yatt[:, k, t0:t1e],
                            start=(k == 0), stop=(k == 7)),
                            reads=[("wba", q), "yatt"], writes=[("ps", b0 + 1)])
                    for k in range(KC):
                        S.op("pe", lambda e, b0=b0, q=q, k=k, hs=hs, t0=t0, t1e=t1e: e.matmul(
                            ps[:, b0 + 2, :], lhsT=wgh[q][:, k, hs], rhs=strm[:, k, t0:t1e],
                            start=(k == 0), stop=(k == KC - 1)),
                            reads=[("wgh", q), "strm"], writes=[("ps", b0 + 2)])
                    for k in range(8):
                        S.op("pe", lambda e, b0=b0, q=q, k=k, hs=hs, t0=t0, t1e=t1e: e.matmul(
                            ps[:, b0 + 3, :], lhsT=wbh[q][:, k, hs], rhs=yhg[:, k, t0:t1e],
                            start=(k == 0), stop=(k == 7)),
                            reads=[("wbh", q), "yhg"], writes=[("ps", b0 + 3)])
                    ta, th = tm[2 * pp], tm[2 * pp + 1]
                    S.op("act", lambda e, ta=ta, b0=b0: e.activation(out=ta, in_=ps[:, b0, :], func=AF.Sigmoid),
                         reads=[("ps", b0)], writes=[("tm", 2 * pp)])
                    S.op("dve", lambda e, ta=ta, b0=b0: e.tensor_tensor(out=ta, in0=ta, in1=ps[:, b0 + 1, :], op=ALU.mult),
                         reads=[("tm", 2 * pp), ("ps", b0 + 1)], writes=[("tm", 2 * pp)])
                    S.op("act", lambda e, th=th, b0=b0: e.activation(out=th, in_=ps[:, b0 + 2, :], func=AF.Sigmoid),
                         reads=[("ps", b0 + 2)], writes=[("tm", 2 * pp + 1)])
                    S.op("dve", lambda e, th=th, b0=b0: e.tensor_tensor(out=th, in0=th, in1=ps[:, b0 + 3, :], op=ALU.mult),
                         reads=[("tm", 2 * pp + 1), ("ps", b0 + 3)], writes=[("tm", 2 * pp + 1)])
                    S.op("pool", lambda e, ta=ta, th=th, mc=mc, t0=t0, t1e=t1e: e.tensor_tensor(
                        out=merged[:, mc, t0:t1e], in0=ta, in1=th, op=ALU.add),
                        reads=[("tm", 2 * pp), ("tm", 2 * pp + 1)], writes=[("merged", mc, ti)])
        S.barrier()
        z = self.view(R1 + 32768, F32, [KC, NREAL])
        wo = [self.view(53760 + q * 4096, BF16, [KC, 128]) for q in range(2)]
        strm_out = self.view(0, BF16, [KC, NREAL])
        for dc in range(KC):
            q = dc % 2
            S.dma("pool", lambda e, q=q, dc=dc: e.dma_start(out=wo[q], in_=self.wo_d[dc]), writes=[("wo", q)])
            S.dma("sp", lambda e, dc=dc: e.dma_start(out=z[:, dc, :], in_=self.h1s[dc * 128:(dc + 1) * 128, 0:NREAL]),
                  writes=[("l2", "z", dc, ti) for ti in range(2)])
            for ti, (t0, t1e) in enumerate(RTB):
                pb = (dc * 2 + ti) % 2
                for k in range(KC):
                    S.op("pe", lambda e, pb=pb, q=q, k=k, t0=t0, t1e=t1e: e.matmul(
                        ps[:, pb, :], lhsT=wo[q][:, k, :], rhs=merged[:, k, t0:t1e],
                        start=(k == 0), stop=(k == KC - 1)),
                        reads=[("wo", q), ("merged", k, ti)], writes=[("ps", pb)])
                S.op("dve", lambda e, pb=pb, dc=dc, t0=t0, t1e=t1e: e.scalar_tensor_tensor(
                    out=z[:, dc, t0:t1e], in0=ps[:, pb, :], scalar=1.0 / ALPHA, in1=z[:, dc, t0:t1e],
                    op0=ALU.mult, op1=ALU.add),
                    reads=[("ps", pb), ("l2", "z", dc, ti)], writes=[("l2", "z", dc, ti)])
        self.ln_apply("l2", z, RTB, cg, cb, 33280, LN_EPS / (ALPHA * ALPHA), strm_out=strm_out,
                      resid_out=self.h2s)
        S.barrier()

    def eps_col(self, val):
        return self.eps_cols[val]

    def build(self):
        nc, S = self.nc, self.S
        stage = self.stage
        xT = self.din("xT", [D, NT])
        wg1 = self.din("wg1", [FC // 2, 128, KC, 256])
        wu1 = self.din("wu1", [FC // 2, 128, KC, 256])
        wd1 = self.din("wd1", [KC, 128, FC, 128])
        wg2 = self.din("wg2", [FC // 2, 128, KC, 256])
        wu2 = self.din("wu2", [FC // 2, 128, KC, 256])
        wd2 = self.din("wd2", [KC, 128, FC, 128])
        cvec = self.din("cvec", [128, 128])
        cmat = self.din("cmat", [128, 384])
        self.catt = self.din("catt", [128, 224])
        self.augk = self.din("augk", [3, 4112])
        self.augs = self.din("augs", [2, 1024])
        self.augq = self.din("augq", [8, 1024])
        self.cbt_d = self.din("cbt", [128, 512])
        self.win = self.din("win", [len(GROUPS), 128, KC, 256])
        self.wba_d = self.din("wba", [8, 128, 8, 256])
        self.wbh_d = self.din("wbh", [8, 128, 8, 256])
        self.wo_d = self.din("wo", [KC, 128, KC, 128])
        self.gidx = {nm: i for i, (nm, _, _) in enumerate(GROUPS)}
        self.h1s = h1s = self.dscratch("h1s", [D, NT])
        self.h2s = self.dscratch("h2s", [D, NREAL])
        self.xs = [self.dscratch("xs%d" % q, [128, 3 * 1024], BF16) for q in range(3)]
        self.xg = [self.dscratch("xg%d" % q, [512, 3 * 1024], BF16) for q in range(3)]
        self.xa = self.dscratch("xa", [128, 72])
        self.xag = self.dscratch("xag", [512, 72])
        self.ks = self.dscratch("ks", [128, 2048], BF16)
        self.kg = self.dscratch("kg", [512, 2048], BF16)
        self.vs = self.dscratch("vs", [128, 3088], BF16)
        self.vg = self.dscratch("vg", [512, 3088], BF16)
        if stage == 1:
            dbg = self.dout("dbg", [D, NT])
        elif stage in (3, 4):
            dbg = self.dout("dbg", [128, 8 * NREAL])
            self.dbg2 = self.dout("dbg2", [128, 12000])
        elif stage == 5:
            dbg = self.dout("dbg", [D, NREAL])
        else:
            outT = self.dout("outT", [D, NREAL])

        from contextlib import ExitStack
        with ExitStack() as es:
            self.arena = es.enter_context(nc.sbuf_tensor("arena", [128, ARENA_F32], F32))
            self.cst = es.enter_context(nc.sbuf_tensor("cst", [128, CONST_F32], F32))
            self.ps = es.enter_context(nc.psum_tensor("ps", [128, 8, 512], F32))
            esems = {e: es.enter_context(nc.semaphore("sem_" + e)) for e in ENGS}
            dsems = [es.enter_context(nc.semaphore("dsem%d" % i)) for i in range(S.n_dma_sems + 8)]
            block = es.enter_context(nc.Block())
            cst = self.cst
            self.ones_f32 = cst[:, 0:128]
            cv = self.cv = cst[:, 128:256]
            epsA = cst[:, 256:257]
            self.eps_rms = cst[:, 257:258]
            self.eps_ik = cst[:, 258:259]
            self.eps_cols = {LN_EPS / (ALPHA * ALPHA): epsA}
            self.ident_bf = cst[:, 264:328].bitcast(BF16)
            self.ones_bf = cst[:, 328:392].bitcast(BF16)
            self.mask2 = cst[:, 392:520]
            self.ones128 = cst[:, 520:648]
            self.lbc = cst[:, 648:656]
            self.omlc = cst[:, 656:664]
            self.gnc = cv[:, 112:120]
            self.selc = cv[:, 120:124]
            S.op("dve", lambda e: e.memset(self.ones_f32, 1.0 / D), writes=["ones"])
            S.op("dve", lambda e: e.memset(self.ones128, 1.0 / 128), writes=["ones128"])
            S.op("dve", lambda e: e.memset(epsA, LN_EPS / (ALPHA * ALPHA)), writes=["eps"])
            S.op("dve", lambda e: e.memset(self.eps_rms, RMS_EPS), writes=["eps"])
            S.op("dve", lambda e: e.memset(self.eps_ik, LN_EPS), writes=["eps"])
            S.dma("sp", lambda e: e.dma_start(out=cv, in_=cvec), writes=["cv"])
            S.dma("sp", lambda e: e.dma_start(out=self.mask2, in_=cmat[:, 256:384]), writes=["mask2"])
            S.dma("pool", lambda e: e.dma_start(out=self.ident_bf, in_=cmat[:, 0:128]), writes=["ident"])
            S.dma("pool", lambda e: e.dma_start(out=self.ones_bf, in_=cmat[:, 128:256]), writes=["ones_bf"])
            S.op("dve", lambda e: e.tensor_tensor(out=self.lbc, in0=cv[:, 96:104], in1=cv[:, 104:112], op=ALU.subtract),
                 reads=["cv"], writes=["lb"])
            S.op("act", lambda e: e.activation(out=self.lbc, in_=self.lbc, func=AF.Sigmoid), reads=["lb"], writes=["lb"])
            S.op("dve", lambda e: e.tensor_scalar(out=self.omlc, in0=self.lbc, scalar1=-1.0, scalar2=1.0,
                                                  op0=ALU.mult, op1=ALU.add), reads=["lb"], writes=["lb"])
            strm0 = self.view(0, BF16, [KC, NT])
            S.dma("pool", lambda e: e.dma_start(out=strm0, in_=xT.rearrange("(k p) t -> p k t", p=128)),
                  writes=[("f1", "sin")])
            S.barrier()
            self.ffn_phase("f1", NT, TBS, strm0, 66560, wg1, wu1, wd1, xT, cv[:, 0:16], cv[:, 16:32],
                           resid_out=(dbg if stage == 1 else h1s))
            strm1 = self.view(66560, BF16, [KC, NT])
            if stage >= 2:
                self.hgrn_m1(strm1)
                self.hgrn_m2(strm1)
            if stage == 3:
                yhg = self.view(32768, BF16, [8 * NREAL])
                S.dma("pool", lambda e: e.dma_start(out=dbg, in_=yhg), reads=[])
            if stage >= 4:
                self.attn_m3(strm1)
            if stage == 4:
                yat = self.view(0, BF16, [8 * NREAL])
                S.dma("pool", lambda e: e.dma_start(out=dbg, in_=yat), reads=[])
            if stage >= 5:
                if stage == 5:
                    self.h2s = dbg
                self.merge_m4(strm1, cv[:, 32:48], cv[:, 48:64])
            if stage >= 6:
                strm2 = self.view(0, BF16, [KC, NREAL])
                self.ffn_phase("f2", NREAL, TBS[0:2], strm2, 66560, wg2, wu2, wd2, self.h2s, cv[:, 64:80],
                               cv[:, 80:96], final_out=outT)
            S.emit(block, esems, dsems)
        return nc


def _lay_gu(w):
    return np.ascontiguousarray(w.reshape(KC, 128, FC // 2, 256).transpose(2, 1, 0, 3))


def _lay_d(w):
    return np.ascontiguousarray(w.reshape(FC, 128, KC, 128).transpose(2, 1, 0, 3))


def _fm(v):
    return np.ascontiguousarray(v.reshape(KC, 128).T)


def _core_tokens(x, meta, c):
    b, j = c // 4, c % 4
    blocks = [x[b, 128 * (4 * i + j):128 * (4 * i + j) + 128] for i in range(8)]
    tok = np.concatenate(blocks + [meta], axis=0)
    return np.ascontiguousarray(tok.T)


def _mk_groups():
    g = []
    for hp in range(4):
        g += [("hf%d" % hp, 3664 + 256 * hp, 256), ("hq%d" % hp, 2640 + 256 * hp, 256),
              ("hi%d" % hp, 4688 + 256 * hp, 256)]
    for i in range(4):
        g.append(("hg%d" % i, 5712 + 256 * i, 256))
    g += [("ak", 1024, 256), ("av", 1280, 256), ("ikw", 2560, 80)]
    for i in range(4):
        g.append(("aq%d" % i, 256 * i, 256))
    for i in range(4):
        g.append(("iq%d" % i, 1536 + 256 * i, 256))
    for i in range(8):
        g.append(("ga%d" % i, 6736 + 256 * i, 256))
        g.append(("gh%d" % i, 8784 + 256 * i, 256))
    return g


GROUPS = _mk_groups()


def _lay_win(w):
    out = np.zeros((len(GROUPS), 128, KC, 256), np.float32)
    for gi, (nm, c0, nc_) in enumerate(GROUPS):
        out[gi, :, :, 0:nc_] = w[:, c0:c0 + nc_].reshape(KC, 128, nc_).transpose(1, 0, 2)
    return out


def prepare(inputs, stage):
    f = lambda k: np.asarray(inputs[k], dtype=np.float32)
    x, meta = f("x"), f("meta")
    shared = {
        "wg1": _lay_gu(f("ffn1_w_gate")[0]), "wu1": _lay_gu(f("ffn1_w_up")[0]),
        "wd1": _lay_d(f("ffn1_w_down")[0]),
        "win": _lay_win(f("w_in")[0]),
        "wg2": _lay_gu(f("ffn2_w_gate")[0]), "wu2": _lay_gu(f("ffn2_w_up")[0]),
        "wd2": _lay_d(f("ffn2_w_down")[0]),
        "wba": np.ascontiguousarray(f("w_branch_att")[0].reshape(8, 128, 8, 256).transpose(2, 1, 0, 3)),
        "wbh": np.ascontiguousarray(f("w_branch_hg")[0].reshape(8, 128, 8, 256).transpose(2, 1, 0, 3)),
        "wo": np.ascontiguousarray(f("w_out")[0].reshape(KC, 128, KC, 128).transpose(2, 1, 0, 3)),
    }
    slopes = (2.0 ** -(np.arange(8) + 1.0)).astype(np.float32)
    c = np.arange(4096)
    kpos = np.concatenate([16 + 128 * (4 * ((c % 1024) // 128) + c // 1024) + c % 128, np.arange(16)]).astype(np.float32)
    augk = np.stack([np.floor(kpos / 64.0), kpos % 64.0, np.ones_like(kpos)], 0).astype(np.float32)
    augs = np.stack([np.repeat(64.0 * slopes, 128), np.repeat(slopes, 128)], 0).astype(np.float32)
    shared["augk"] = augk
    shared["augs"] = augs
    cvec = np.zeros((128, 128), np.float32)
    cvec[:, 0:16] = _fm(f("ln1_g")[0]); cvec[:, 16:32] = _fm(f("ln1_b")[0])
    cvec[:, 32:48] = _fm(f("ln2_g")[0]); cvec[:, 48:64] = _fm(f("ln2_b")[0])
    cvec[:, 64:80] = _fm(f("ln3_g")[0]); cvec[:, 80:96] = _fm(f("ln3_b")[0])
    lbl = f("hg_lb_logits")
    cvec[:, 96:104] = lbl[0].reshape(8, 128).T
    cvec[:, 104:112] = lbl[1].reshape(8, 128).T
    cvec[:, 112:120] = f("hg_norm_g")[0].T
    cmat = np.zeros((128, 384), np.float32)
    cmat[:, 0:128] = np.eye(128, dtype=np.float32)
    cmat[:, 128:256] = 1.0
    sidx = np.arange(128)[:, None]; tidx = np.arange(128)[None, :]
    cmat[:, 256:384] = (((sidx <= tidx) & ((sidx // 64) == (tidx // 64))) | ((sidx < 64) & (tidx >= 64))).astype(np.float32)
    shared["cmat"] = cmat
    maps = []
    for c in range(8):
        m = dict(shared)
        m["xT"] = _core_tokens(x, meta, c)
        cv = cvec.copy()
        cv[:, 120 + (c % 4)] = 1.0
        m["cvec"] = cv
        j = c % 4
        p = np.arange(128, dtype=np.float32)
        qpos = np.stack([16 + 128 * (4 * i + j) + p for i in range(8)], 0)
        m["augq"] = np.ascontiguousarray((-slopes[None, :, None] * qpos[:, None, :]).reshape(8, 1024).astype(np.float32))
        catt = np.zeros((128, 224), np.float32)
        catt[:, 0:64] = (-qpos.T[:, :, None] * slopes[None, None, :]).reshape(128, 64)
        catt[:, 64:96] = (2.0 ** -(np.arange(32) + 1.0))[None, :]
        catt[:, 96:160] = f("idx_k_norm_g")[0][None, :]
        catt[:, 160:224] = f("idx_k_norm_b")[0][None, :]
        m["catt"] = catt
        tt = np.arange(128)[:, None, None]; rr = np.arange(4)[None, :, None]; ss = np.arange(128)[None, None, :]
        m["cbt"] = np.where(128 * (rr - j) + (ss - tt) > 0, -1e30, 0.0).astype(np.float32).reshape(128, 512)
        maps.append(m)
    return maps


_NC_CACHE = {}


def kernel(**inputs):
    stage = int(os.environ.get("KSTAGE", "9"))
    if stage not in _NC_CACHE:
        _NC_CACHE[stage] = Builder(stage).build()
    nc = _NC_CACHE[stage]
    maps = prepare(inputs, stage)
    res = run_bass_kernel_spmd(nc, maps, core_ids=list(range(8)))
    if stage == 4:
        return [(r["dbg"], r["dbg2"]) for r in res.results]
    if stage < 6:
        return [r["dbg"] for r in res.results]
    out = np.zeros((2, 4096, D), np.float32)
    for c in range(8):
        b, j = c // 4, c % 4
        o = res.results[c]["outT"]
        for i in range(8):
            g = 4 * i + j
            out[b, 128 * g:128 * g + 128] = o[:, 128 * i:128 * i + 128].T
    return out
```

```python
import os
import numpy as np
import concourse.bass as bass
import concourse.mybir as mybir
from concourse.bass_utils import run_bass_kernel_spmd

F32 = mybir.dt.float32
BF16 = mybir.dt.bfloat16
U8 = mybir.dt.uint8
AF = mybir.ActivationFunctionType
ALU = mybir.AluOpType
AX = mybir.AxisListType

D = 2048
DFF = 5632
NMETA = 16
NREAL = 1024
NT = NREAL + NMETA
KC = D // 128
FC = DFF // 128
TBS = [(0, 512), (512, 1024), (1024, 1040)]
ALPHA = 2.0 ** 0.25
LN_EPS = 1e-5
RMS_EPS = 1e-6

ENGS = ("pe", "act", "dve", "pool", "sp")


class Op:
    __slots__ = ("eng", "fn", "deps", "is_dma", "dsem", "dval", "sig", "sigidx", "waits")

    def __init__(self, eng, fn, is_dma=False):
        self.eng = eng
        self.fn = fn
        self.deps = []
        self.is_dma = is_dma
        self.dsem = None
        self.dval = 0
        self.sig = False
        self.sigidx = 0
        self.waits = []


class Sched:
    def __init__(self, n_dma_sems=40, same_engine_sync=True):
        self.ops = {e: [] for e in ENGS}
        self.lastw = {}
        self.readers = {}
        self.n_dma = 0
        self.n_dma_sems = n_dma_sems
        self.dma_hist = {}
        self.same_engine_sync = same_engine_sync
        self.n_coll = 0

    def _add(self, op, reads, writes):
        deps = set()
        for k in reads:
            w = self.lastw.get(k)
            if w is not None:
                deps.add(w)
        for k in writes:
            w = self.lastw.get(k)
            if w is not None:
                deps.add(w)
            for r in self.readers.get(k, ()):
                deps.add(r)
        op.deps = list(deps)
        for k in reads:
            self.readers.setdefault(k, []).append(op)
        for k in writes:
            self.lastw[k] = op
            self.readers[k] = []
        self.ops[op.eng].append(op)
        return op

    def op(self, eng, fn, reads=(), writes=()):
        return self._add(Op(eng, fn), reads, writes)

    def dma(self, q, fn, reads=(), writes=()):
        op = Op(q, fn, is_dma=True)
        slot = self.n_dma % self.n_dma_sems
        op.dsem = slot
        op.dval = 16 * (self.n_dma // self.n_dma_sems + 1)
        self.n_dma += 1
        self._add(op, reads, writes)
        prev = self.dma_hist.get(slot)
        if prev is not None:
            op.deps.append(prev)
        self.dma_hist[slot] = op
        return op

    def coll(self, fn, reads=(), writes=()):
        op = Op("pool", fn, is_dma=True)
        op.dsem = self.n_dma_sems + self.n_coll
        op.dval = 1
        self.n_coll += 1
        self._add(op, reads, writes)
        self.dma_hist[op.dsem] = op
        return op

    def barrier(self):
        lasts = []
        for e in ENGS:
            for o in reversed(self.ops[e]):
                if not o.is_dma and o.fn is not None:
                    lasts.append(o)
                    break
        dmas = list(self.dma_hist.values())
        for e in ENGS:
            op = Op(e, None)
            op.deps = [o for o in lasts if o.eng != e] + dmas
            self.ops[e].append(op)
        self.lastw = {}
        self.readers = {}

    def _skip(self, d, op):
        return d.eng == op.eng and (d.eng in ("pe", "sp") or not self.same_engine_sync)

    def finalize(self):
        for e in ENGS:
            for op in self.ops[e]:
                for d in op.deps:
                    if not d.is_dma and not self._skip(d, op):
                        d.sig = True
        for e in ENGS:
            c = 0
            for op in self.ops[e]:
                if op.sig:
                    c += 1
                    op.sigidx = c
        for e in ENGS:
            waited = {}
            for op in self.ops[e]:
                need = {}
                for d in op.deps:
                    if d.is_dma:
                        key, val = ("d", d.dsem), d.dval
                    else:
                        if self._skip(d, op):
                            continue
                        key, val = ("e", d.eng), d.sigidx
                    if waited.get(key, 0) >= val:
                        continue
                    if need.get(key, 0) < val:
                        need[key] = val
                for k, v in need.items():
                    waited[k] = v
                op.waits = list(need.items())

    def emit(self, block, esems, dsems):
        self.finalize()
        regs = {"pe": block.tensor, "act": block.scalar, "dve": block.vector,
                "pool": block.gpsimd, "sp": block.sync}
        final = {d.dsem: d.dval for d in self.dma_hist.values()}

        def make(e):
            ops = self.ops[e]

            def body(eng):
                for op in ops:
                    for (kind, which), val in op.waits:
                        eng.wait_ge(dsems[which] if kind == "d" else esems[which], val)
                    if op.fn is None:
                        continue
                    ins = op.fn(eng)
                    if op.is_dma:
                        ins.then_inc(dsems[op.dsem], 16 if op.dsem < self.n_dma_sems else 1)
                    elif op.sig:
                        ins.then_inc(esems[e], 1)
                if e == "sp":
                    for slot, val in final.items():
                        eng.wait_ge(dsems[slot], val)
            return body

        for e in ENGS:
            regs[e](make(e))


ARENA_F32 = 50816
CONST_F32 = 2304


class Builder:
    def __init__(self, stage):
        self.stage = stage
        self.nc = bass.Bass("TRN2", target_bir_lowering=False)
        self.S = Sched()
        self.dram = {}

    def din(self, name, shape, dt=F32):
        self.dram[name] = self.nc.dram_tensor(name, list(shape), dt, kind="ExternalInput").ap()
        return self.dram[name]

    def dout(self, name, shape, dt=F32):
        self.dram[name] = self.nc.dram_tensor(name, list(shape), dt, kind="ExternalOutput").ap()
        return self.dram[name]

    def dscratch(self, name, shape, dt=F32):
        self.dram[name] = self.nc.dram_tensor(name, list(shape), dt, kind="Internal").ap()
        return self.dram[name]

    def view(self, off_bytes, dt, shape):
        n = 1
        for s in shape:
            n *= s
        esz = 4 if dt == F32 else (1 if dt == U8 else 2)
        assert off_bytes % 4 == 0
        nbytes = n * esz
        assert nbytes % 4 == 0
        assert off_bytes + nbytes <= ARENA_F32 * 4, (off_bytes, nbytes)
        v = self.arena[:, off_bytes // 4:(off_bytes + nbytes) // 4]
        if dt != F32:
            v = v.bitcast(dt)
        if len(shape) == 2:
            v = v.rearrange("p (a b) -> p a b", b=shape[1])
        elif len(shape) == 3:
            v = v.rearrange("p (a b c) -> p a b c", b=shape[1], c=shape[2])
        return v

    def ln_apply(self, tag, z, tbs, cg, cb, toff, eps_eff, strm_out=None, alias_key=None, resid_out=None,
                 final_out=None):
        S, ps = self.S, self.ps
        zsq = [self.view(toff + i * 2048, F32, [512]) for i in range(2)]
        meanb = self.view(toff + 4096, F32, [512])
        rstdb = self.view(toff + 6144, F32, [512])
        t1 = [self.view(toff + 8192 + i * 2048, F32, [512]) for i in range(2)]
        t2 = [self.view(toff + 12288 + i * 2048, F32, [512]) for i in range(2)]
        o32 = [self.view(toff + 16384 + i * 2048, F32, [512]) for i in range(2)]
        ones = self.ones_f32
        for ti, (t0, t1e) in enumerate(tbs):
            n = t1e - t0
            pm, pq = ps[:, 6, 0:n], ps[:, 7, 0:n]
            for dc in range(KC):
                zq = zsq[dc % 2]
                S.op("act", lambda e, zq=zq, dc=dc, t0=t0, t1e=t1e, n=n: e.activation(
                    out=zq[:, 0:n], in_=z[:, dc, t0:t1e], func=AF.Square),
                    reads=[(tag, "z", dc, ti)], writes=[(tag, "zsq", dc % 2)])
                S.op("pe", lambda e, pm=pm, dc=dc, t0=t0, t1e=t1e: e.matmul(
                    pm, lhsT=ones, rhs=z[:, dc, t0:t1e], start=(dc == 0), stop=(dc == KC - 1)),
                    reads=[(tag, "z", dc, ti)], writes=[("ps", 6)])
                S.op("pe", lambda e, pq=pq, zq=zq, dc=dc, n=n: e.matmul(
                    pq, lhsT=ones, rhs=zq[:, 0:n], start=(dc == 0), stop=(dc == KC - 1)),
                    reads=[(tag, "zsq", dc % 2)], writes=[("ps", 7)])
            S.op("act", lambda e, pm=pm, n=n: e.activation(out=meanb[:, 0:n], in_=pm, func=AF.Copy),
                 reads=[("ps", 6)], writes=[(tag, "meanb")])
            S.op("dve", lambda e, n=n: e.tensor_tensor(out=rstdb[:, 0:n], in0=meanb[:, 0:n], in1=meanb[:, 0:n],
                                                       op=ALU.mult),
                 reads=[(tag, "meanb")], writes=[(tag, "rstdb")])
            S.op("dve", lambda e, pq=pq, n=n: e.tensor_tensor(out=rstdb[:, 0:n], in0=pq, in1=rstdb[:, 0:n],
                                                              op=ALU.subtract),
                 reads=[("ps", 7), (tag, "rstdb")], writes=[(tag, "rstdb")])
            S.op("act", lambda e, n=n: e.activation(out=rstdb[:, 0:n], in_=rstdb[:, 0:n], func=AF.Sqrt,
                                                    bias=self.eps_cols[eps_eff], scale=1.0),
                 reads=[(tag, "rstdb")], writes=[(tag, "rstdb")])
            S.op("dve", lambda e, n=n: e.reciprocal(out=rstdb[:, 0:n], in_=rstdb[:, 0:n]),
                 reads=[(tag, "rstdb")], writes=[(tag, "rstdb")])
            for dc in range(KC):
                a, b_, o = t1[dc % 2], t2[dc % 2], o32[dc % 2]
                S.op("pool", lambda e, a=a, dc=dc, t0=t0, t1e=t1e, n=n: e.tensor_tensor(
                    out=a[:, 0:n], in0=z[:, dc, t0:t1e], in1=meanb[:, 0:n], op=ALU.subtract),
                    reads=[(tag, "z", dc, ti), (tag, "meanb")], writes=[(tag, "t1", dc % 2)])
                S.op("dve", lambda e, a=a, b_=b_, n=n: e.tensor_tensor(
                    out=b_[:, 0:n], in0=a[:, 0:n], in1=rstdb[:, 0:n], op=ALU.mult),
                    reads=[(tag, "t1", dc % 2), (tag, "rstdb")], writes=[(tag, "t2", dc % 2)])
                S.op("act", lambda e, b_=b_, o=o, dc=dc, n=n: e.activation(
                    out=o[:, 0:n], in_=b_[:, 0:n], func=AF.Identity,
                    bias=cb[:, dc:dc + 1], scale=cg[:, dc:dc + 1]),
                    reads=[(tag, "t2", dc % 2)], writes=[(tag, "o32", dc % 2)])
                if strm_out is not None:
                    wk = [(tag, "sout", dc, ti)] + ([(tag, alias_key, dc, ti)] if alias_key else [])
                    S.op("act", lambda e, b_=b_, dc=dc, t0=t0, t1e=t1e, n=n: e.activation(
                        out=strm_out[:, dc, t0:t1e], in_=b_[:, 0:n], func=AF.Identity,
                        bias=cb[:, dc:dc + 1], scale=cg[:, dc:dc + 1]),
                        reads=[(tag, "t2", dc % 2)], writes=wk)
                dst = resid_out if final_out is None else final_out
                if final_out is None or t0 < NREAL:
                    S.dma("sp", lambda e, o=o, dc=dc, t0=t0, t1e=t1e, n=n, dst=dst: e.dma_start(
                        out=dst[dc * 128:(dc + 1) * 128, t0:t1e], in_=o[:, 0:n]),
                        reads=[(tag, "o32", dc % 2)])

    def ffn_phase(self, tag, ntok, tbs, strm_in, strm_out_off, wg, wu, wd, resid, cg, cb,
                  resid_out=None, final_out=None):
        S, nc = self.S, self.nc
        ps = self.ps
        C_OFF = 33280
        B_OFF = 66560
        D_OFF = B_OFF + FC * NT * 2
        T_OFF = D_OFF + 2 * FC * 128 * 2
        hT = self.view(B_OFF, BF16, [FC, ntok])
        wgu = [self.view(C_OFF + i * 16384, BF16, [2, KC, 256]) for i in range(2)]
        wdb = [self.view(D_OFF + i * FC * 128 * 2, BF16, [FC, 128]) for i in range(2)]
        z = self.view(0, F32, [KC, ntok])
        sil = [self.view(T_OFF + 8192 + i * 2048, F32, [512]) for i in range(2)]
        strm_out = self.view(strm_out_off, BF16, [KC, ntok]) if final_out is None else None
        c_scale = 0.5 / ALPHA
        eps_eff = LN_EPS / (ALPHA * ALPHA)
        nb = len(tbs)

        for g in range(FC // 2):
            wb = wgu[g % 2]
            kb = (tag, "wgu", g % 2)
            S.dma("pool", lambda e, wb=wb, g=g: e.dma_start(out=wb[:, 0], in_=wg[g]), writes=[(kb, 0)])
            S.dma("pool", lambda e, wb=wb, g=g: e.dma_start(out=wb[:, 1], in_=wu[g]), writes=[(kb, 1)])
            for fcl in range(2):
                fc = 2 * g + fcl
                for ti, (t0, t1e) in enumerate(tbs):
                    n = t1e - t0
                    pb = (fc * nb + ti) % 2
                    pg, pu = ps[:, 2 * pb, 0:n], ps[:, 2 * pb + 1, 0:n]
                    for k in range(KC):
                        S.op("pe", lambda e, pg=pg, wb=wb, k=k, fcl=fcl, t0=t0, t1e=t1e: e.matmul(
                            pg, lhsT=wb[:, 0, k, fcl * 128:(fcl + 1) * 128], rhs=strm_in[:, k, t0:t1e],
                            start=(k == 0), stop=(k == KC - 1)),
                            reads=[(kb, 0), (tag, "sin")], writes=[("ps", 2 * pb)])
                    for k in range(KC):
                        S.op("pe", lambda e, pu=pu, wb=wb, k=k, fcl=fcl, t0=t0, t1e=t1e: e.matmul(
                            pu, lhsT=wb[:, 1, k, fcl * 128:(fcl + 1) * 128], rhs=strm_in[:, k, t0:t1e],
                            start=(k == 0), stop=(k == KC - 1)),
                            reads=[(kb, 1), (tag, "sin")], writes=[("ps", 2 * pb + 1)])
                    sb = sil[pb]
                    S.op("act", lambda e, sb=sb, pg=pg, n=n: e.activation(out=sb[:, 0:n], in_=pg, func=AF.Silu),
                         reads=[("ps", 2 * pb)], writes=[(tag, "sil", pb)])
                    S.op("dve", lambda e, sb=sb, pu=pu, n=n, fc=fc, t0=t0, t1e=t1e: e.tensor_tensor(
                        out=hT[:, fc, t0:t1e], in0=sb[:, 0:n], in1=pu, op=ALU.mult),
                        reads=[(tag, "sil", pb), ("ps", 2 * pb + 1)], writes=[(tag, "hT", fc, ti)])

        S.barrier()
        for dc in range(KC):
            wb = wdb[dc % 2]
            kb = (tag, "wd", dc % 2)
            S.dma("pool", lambda e, wb=wb, dc=dc: e.dma_start(out=wb, in_=wd[dc]), writes=[kb])
            S.dma("sp", lambda e, dc=dc: e.dma_start(out=z[:, dc, :], in_=resid[dc * 128:(dc + 1) * 128, 0:ntok]),
                  writes=[(tag, "z", dc, ti) for ti in range(nb)])
            for ti, (t0, t1e) in enumerate(tbs):
                n = t1e - t0
                pb = 4 + (dc * nb + ti) % 2
                py = ps[:, pb, 0:n]
                for f in range(FC):
                    S.op("pe", lambda e, py=py, wb=wb, f=f, t0=t0, t1e=t1e: e.matmul(
                        py, lhsT=wb[:, f, :], rhs=hT[:, f, t0:t1e], start=(f == 0), stop=(f == FC - 1)),
                        reads=[kb, (tag, "hT", f, ti)], writes=[("ps", pb)])
                S.op("dve", lambda e, py=py, dc=dc, t0=t0, t1e=t1e: e.scalar_tensor_tensor(
                    out=z[:, dc, t0:t1e], in0=py, scalar=c_scale, in1=z[:, dc, t0:t1e],
                    op0=ALU.mult, op1=ALU.add),
                    reads=[("ps", pb), (tag, "z", dc, ti)], writes=[(tag, "z", dc, ti)])
        self.ln_apply(tag, z, tbs, cg, cb, T_OFF, eps_eff, strm_out=strm_out, alias_key="hT",
                      resid_out=resid_out, final_out=final_out)
        S.barrier()

    def proj_fm(self, tag, strm, gi, wbufs, tbs, evac, banks=(0, 1), parity=[0]):
        S, ps = self.S, self.ps
        wb = wbufs[parity[0] % 2]
        kb = ("wb", parity[0] % 2)
        parity[0] += 1
        S.dma("pool", lambda e, wb=wb, gi=gi: e.dma_start(out=wb, in_=self.win[gi]), writes=[kb])
        cnt = 0
        for half in range(2):
            for ti, (t0, t1e) in enumerate(tbs):
                n = t1e - t0
                bk = banks[cnt % len(banks)]
                cnt += 1
                pv = ps[:, bk, 0:n]
                for k in range(KC):
                    S.op("pe", lambda e, pv=pv, wb=wb, k=k, half=half, t0=t0, t1e=t1e: e.matmul(
                        pv, lhsT=wb[:, k, half * 128:(half + 1) * 128], rhs=strm[:, k, t0:t1e],
                        start=(k == 0), stop=(k == KC - 1)),
                        reads=[kb, "strm"], writes=[("ps", bk)])
                evac(half, ti, t0, t1e, pv, bk)

    def proj_tm(self, tag, strm, gi, wbufs, tls, ncols, evac, banks=(0, 1), parity=[0]):
        S, ps = self.S, self.ps
        wb = wbufs[parity[0] % 2]
        kb = ("wb", parity[0] % 2)
        parity[0] += 1
        S.dma("pool", lambda e, wb=wb, gi=gi: e.dma_start(out=wb, in_=self.win[gi]), writes=[kb])
        for cnt, (i, c0, n) in enumerate(tls):
            bk = banks[cnt % len(banks)]
            pv = ps[0:n, bk, 0:ncols]
            for k in range(KC):
                S.op("pe", lambda e, pv=pv, wb=wb, k=k, c0=c0, n=n: e.matmul(
                    pv, lhsT=strm[:, k, c0:c0 + n], rhs=wb[:, k, 0:ncols],
                    start=(k == 0), stop=(k == KC - 1)),
                    reads=[kb, "strm"], writes=[("ps", bk)])
            evac(i, c0, n, pv, bk)

    def hgrn_m1(self, strm):
        S, ps, nc = self.S, self.ps, self.nc
        R1 = 99840
        wbufs = [self.view(R1 + i * 8192, BF16, [KC, 256]) for i in range(2)]
        off = [R1 + 16384]

        def alloc(dt, shape):
            n = 1
            for x in shape:
                n *= x
            nb = n * (4 if dt == F32 else 2)
            nb = (nb + 63) // 64 * 64
            v = self.view(off[0], dt, shape)
            off[0] += nb
            return v
        logf = alloc(F32, [2, NT]); Bg = alloc(F32, [2, NT]); Bsh = alloc(F32, [2, NT])
        tA = alloc(F32, [2, NT]); tB = alloc(F32, [2, NT])
        kk = alloc(BF16, [2, NT]); qs = alloc(BF16, [2, NT]); qt = alloc(BF16, [2, NT])
        kt = alloc(BF16, [2, NT]); kh64 = alloc(BF16, [2, NT]); kh128 = alloc(BF16, [2, NT])
        vv = alloc(BF16, [9, 256])
        PTs = [alloc(BF16, [2, 128]) for _ in range(2)]; khTs = [alloc(BF16, [2, 128]) for _ in range(2)]
        xs_st = alloc(BF16, [9, 256]); xa_st = alloc(F32, [9, 2])
        Qc = self.view(0, BF16, [8, NREAL]); oloc = self.view(16384, BF16, [8, NREAL])
        cv = self.cv
        lbc, omlc = self.lbc, self.omlc
        psb = lambda bk: ps[:, bk, :].bitcast(BF16)
        TL = [(i, 128 * i, 128) for i in range(8)] + [(8, NREAL, NMETA)]
        flat = lambda v: v.rearrange("p h t -> p (h t)")
        r64 = lambda v: v[:, :, 0:NREAL].rearrange("p h (c t) -> p h c t", t=64)
        r128 = lambda v: v[:, :, 0:NREAL].rearrange("p h (c t) -> p h c t", t=128)
        mt = lambda v: v[:, :, NREAL:NT]

        for hp in range(4):
            T = ("m1", hp)
            def ev_f(half, ti, t0, t1e, pv, bk, hp=hp):
                h = 2 * hp + half
                S.op("act", lambda e: e.activation(out=tA[:, half, t0:t1e], in_=pv, func=AF.Sigmoid),
                     reads=[("ps", bk)], writes=[("tA", half, ti)])
                S.op("dve", lambda e: e.tensor_scalar(out=tA[:, half, t0:t1e], in0=tA[:, half, t0:t1e],
                                                      scalar1=omlc[:, h:h + 1], scalar2=lbc[:, h:h + 1],
                                                      op0=ALU.mult, op1=ALU.add),
                     reads=[("tA", half, ti), "lb"], writes=[("tA", half, ti)])
                S.op("act", lambda e: e.activation(out=logf[:, half, t0:t1e], in_=tA[:, half, t0:t1e], func=AF.Ln),
                     reads=[("tA", half, ti)], writes=[("logf", half, ti)])
                S.op("pool", lambda e: e.tensor_scalar(out=kk[:, half, t0:t1e], in0=tA[:, half, t0:t1e],
                                                       scalar1=-1.0, scalar2=1.0, op0=ALU.mult, op1=ALU.add),
                     reads=[("tA", half, ti)], writes=[("kk", half, ti)])
            self.proj_fm(T, strm, self.gidx["hf%d" % hp], wbufs, TBS, ev_f)

            def ev_q(half, ti, t0, t1e, pv, bk):
                S.op("act", lambda e: e.activation(out=qs[:, half, t0:t1e], in_=pv, func=AF.Silu),
                     reads=[("ps", bk)], writes=[("qs", half, ti)])
            self.proj_fm(T, strm, self.gidx["hq%d" % hp], wbufs, TBS, ev_q)

            def ev_v(i, c0, n, pv, bk):
                S.op("act", lambda e: e.activation(out=vv[0:n, i, :], in_=pv, func=AF.Copy),
                     reads=[("ps", bk)], writes=[("vv", i)])
            self.proj_tm(T, strm, self.gidx["hi%d" % hp], wbufs, TL, 256, ev_v)

            allk = lambda nm: [(nm, hl, ti) for hl in range(2) for ti in range(3)]
            S.op("dve", lambda e: e.tensor_tensor_scan(out=flat(Bg), data0=flat(logf), data1=flat(logf),
                                                       initial=0.0, op0=ALU.add, op1=ALU.min),
                 reads=allk("logf"), writes=["Bg"])
            S.op("pool", lambda e: e.memset(flat(Bsh)[:, 0:1], 0.0), writes=["Bsh0"])
            S.op("pool", lambda e: e.tensor_copy(out=flat(Bsh)[:, 1:2 * NT], in_=flat(Bg)[:, 0:2 * NT - 1]),
                 reads=["Bg"], writes=["Bsh"])
            S.op("dve", lambda e: e.tensor_tensor(out=r64(tB), in0=r64(Bg),
                                                  in1=r64(Bsh)[:, :, :, 0:1].to_broadcast([128, 2, 16, 64]),
                                                  op=ALU.subtract),
                 reads=["Bg", "Bsh", "Bsh0"], writes=["tBr"])
            S.op("dve", lambda e: e.tensor_tensor(out=mt(tB), in0=mt(Bg),
                                                  in1=mt(Bsh)[:, :, 0:1].to_broadcast([128, 2, NMETA]),
                                                  op=ALU.subtract),
                 reads=["Bg", "Bsh", "Bsh0"], writes=["tBm"])
            S.op("pool", lambda e: e.tensor_tensor(out=r128(tA), in0=r128(Bg),
                                                   in1=r128(Bsh)[:, :, :, 0:1].to_broadcast([128, 2, 8, 128]),
                                                   op=ALU.subtract),
                 reads=["Bg", "Bsh", "Bsh0"] + allk("tA"), writes=["tAr"] + allk("tA"))
            S.op("pool", lambda e: e.tensor_copy(out=mt(tA), in_=mt(tB)),
                 reads=["tBm"], writes=["tAm"])
            TBk, TAk = ["tBr", "tBm"], ["tAr", "tAm"] + allk("tA")
            S.op("act", lambda e: e.activation(out=flat(Bg), in_=flat(tB), func=AF.Exp),
                 reads=TBk + ["Bsh", "tAr"], writes=["Bg"])
            S.op("dve", lambda e: e.tensor_tensor(out=flat(qt), in0=flat(qs), in1=flat(Bg), op=ALU.mult),
                 reads=["Bg"] + allk("qs"), writes=["qt"])
            S.op("act", lambda e: e.activation(out=flat(Bsh), in_=flat(tB), func=AF.Exp, scale=-1.0),
                 reads=TBk + ["Bsh", "tAr", "Bsh0"], writes=["Bsh", "Bsh0"])
            S.op("dve", lambda e: e.tensor_tensor(out=flat(kt), in0=flat(kk), in1=flat(Bsh), op=ALU.mult),
                 reads=["Bsh"] + allk("kk"), writes=["kt"])
            S.op("dve", lambda e: e.tensor_tensor(out=r64(Bg), in0=r64(tB),
                                                  in1=r64(tB)[:, :, :, 63:64].to_broadcast([128, 2, 16, 64]),
                                                  op=ALU.subtract),
                 reads=TBk + ["qt"], writes=["Bg"])
            S.op("act", lambda e: e.activation(out=r64(Bg), in_=r64(Bg), func=AF.Exp, scale=-1.0),
                 reads=["Bg"], writes=["Bg"])
            S.op("dve", lambda e: e.tensor_tensor(out=r64(kh64), in0=r64(kk), in1=r64(Bg), op=ALU.mult),
                 reads=["Bg"] + allk("kk"), writes=["kh64"])
            S.op("act", lambda e: e.activation(out=flat(Bsh), in_=flat(tA), func=AF.Exp),
                 reads=TAk + ["kt"], writes=["Bsh"])
            S.op("dve", lambda e, hp=hp: e.tensor_tensor(out=Qc[:, 2 * hp:2 * hp + 2, :], in0=qs[:, :, 0:NREAL],
                                                         in1=Bsh[:, :, 0:NREAL], op=ALU.mult),
                 reads=["Bsh"] + allk("qs"), writes=[("Qc", hp)])
            S.op("pool", lambda e: e.tensor_copy(out=xa_st[:, 0:8, :].rearrange("p i h -> p h i"),
                                                 in_=r128(Bsh)[:, :, :, 127]),
                 reads=["Bsh"], writes=["xa_st"])
            S.op("pool", lambda e: e.tensor_copy(out=xa_st[:, 8, :], in_=Bsh[:, :, NT - 1]),
                 reads=["Bsh"], writes=["xa_st"])
            S.op("dve", lambda e: e.tensor_tensor(out=r128(Bg), in0=r128(tA),
                                                  in1=r128(tA)[:, :, :, 127:128].to_broadcast([128, 2, 8, 128]),
                                                  op=ALU.subtract),
                 reads=TAk + ["kh64"], writes=["Bg"])
            S.op("dve", lambda e: e.tensor_tensor(out=mt(Bg), in0=mt(tA),
                                                  in1=mt(tA)[:, :, NMETA - 1:NMETA].to_broadcast([128, 2, NMETA]),
                                                  op=ALU.subtract),
                 reads=TAk + ["kh64"], writes=["Bg"])
            S.op("act", lambda e: e.activation(out=flat(Bg), in_=flat(Bg), func=AF.Exp, scale=-1.0),
                 reads=["Bg"], writes=["Bg"])
            S.op("dve", lambda e: e.tensor_tensor(out=flat(kh128), in0=flat(kk), in1=flat(Bg), op=ALU.mult),
                 reads=["Bg"] + allk("kk"), writes=["kh128"])

            def tok_block(i, c0, n, hp=hp):
                par = i % 2
                PT, khT = PTs[par], khTs[par]
                bS, bO = (2, 3) if par == 0 else (6, 7)
                t0c, s0c = par * 256, par * 256
                if n == 128:
                    for hl in range(2):
                        o0 = hl * 128
                        S.op("pe", lambda e, hl=hl, o0=o0, c0=c0: e.matmul(
                            ps[:, bS, o0:o0 + 64], lhsT=kt[:, hl, c0:c0 + 128], rhs=qt[:, hl, c0:c0 + 64],
                            start=True, stop=True), reads=["kt", "qt"], writes=[("ps", bS)])
                        S.op("pe", lambda e, hl=hl, o0=o0, c0=c0: e.matmul(
                            ps[0:64, bS, o0 + 64:o0 + 128], lhsT=kh64[:, hl, c0:c0 + 64],
                            rhs=qt[:, hl, c0 + 64:c0 + 128], start=True, stop=True),
                            reads=["kh64", "qt"], writes=[("ps", bS)])
                        S.op("pe", lambda e, hl=hl, o0=o0, c0=c0: e.matmul(
                            ps[64:128, bS, o0 + 64:o0 + 128], lhsT=kt[:, hl, c0 + 64:c0 + 128],
                            rhs=qt[:, hl, c0 + 64:c0 + 128], start=True, stop=True),
                            reads=["kt", "qt"], writes=[("ps", bS)])
                    for hl in range(2):
                        S.op("dve", lambda e, hl=hl: e.tensor_tensor(
                            out=PT[:, hl, :], in0=ps[:, bS, hl * 128:(hl + 1) * 128], in1=self.mask2, op=ALU.mult),
                            reads=[("ps", bS), "mask2"], writes=[("PT", par, hl)])
                    for hl in range(2):
                        S.op("pe", lambda e, hl=hl, i=i: e.matmul(
                            ps[:, bO, hl * 128:(hl + 1) * 128], lhsT=vv[:, i, hl * 128:(hl + 1) * 128],
                            rhs=PT[:, hl, :], start=True, stop=True),
                            reads=[("vv", i), ("PT", par, hl)], writes=[("ps", bO)])
                    S.op("act", lambda e, hp=hp, c0=c0: e.activation(
                        out=oloc[:, 2 * hp:2 * hp + 2, c0:c0 + 128],
                        in_=ps[:, bO, 0:256].rearrange("p (h t) -> p h t", h=2), func=AF.Copy),
                        reads=[("ps", bO)], writes=[("oloc", hp, i)])
                for hl in range(2):
                    S.op("pe", lambda e, hl=hl, c0=c0, n=n: e.transpose(
                        out=psb(4)[0:n, t0c * 2 + hl * 128:t0c * 2 + (hl + 1) * 128], in_=kh128[:, hl, c0:c0 + n],
                        identity=self.ident_bf),
                        reads=["kh128", "ident"], writes=[("ps4", par)])
                S.op("dve", lambda e, n=n: e.tensor_copy(out=khT[0:n].rearrange("p h d -> p (h d)"),
                                                         in_=psb(4)[0:n, t0c * 2:t0c * 2 + 256]),
                     reads=[("ps4", par)], writes=[("khT", par)])
                for hl in range(2):
                    S.op("pe", lambda e, hl=hl, i=i, n=n: e.matmul(
                        ps[:, 5, s0c + hl * 128:s0c + (hl + 1) * 128], lhsT=khT[0:n, hl, :],
                        rhs=vv[0:n, i, hl * 128:(hl + 1) * 128], start=True, stop=True),
                        reads=[("khT", par), ("vv", i)], writes=[("ps5", par)])
                S.op("dve", lambda e, i=i: e.tensor_copy(out=xs_st[:, i, :], in_=ps[:, 5, s0c:s0c + 256]),
                     reads=[("ps5", par)], writes=[("xs_st", i)])
            for (i_, c0_, n_) in TL:
                tok_block(i_, c0_, n_)
            for q3 in range(3):
                S.dma("sp", lambda e, hp=hp, q3=q3: e.dma_start(
                    out=self.xs[q3].rearrange("p (i c) -> p i c", i=3)[:, :, hp * 256:(hp + 1) * 256],
                    in_=xs_st[:, 3 * q3:3 * q3 + 3, :]),
                    reads=[("xs_st", i) for i in range(9)], writes=[("xs", hp, q3)])
            S.dma("sp", lambda e, hp=hp: e.dma_start(
                out=self.xa.rearrange("p (i c) -> p i c", i=9)[:, :, 2 * hp:2 * hp + 2], in_=xa_st),
                reads=["xa_st"], writes=[("xa", hp)])
        S.barrier()
        rg = [[0, 1, 2, 3], [4, 5, 6, 7]]
        for q3 in range(3):
            S.coll(lambda e, q3=q3: e.collective_compute("AllGather", ALU.bypass, replica_groups=rg,
                                                         ins=[self.xs[q3]], outs=[self.xg[q3]]), writes=[("xg", q3)])
        S.coll(lambda e: e.collective_compute("AllGather", ALU.bypass, replica_groups=rg,
                                              ins=[self.xa], outs=[self.xag]), writes=["xag"])

    def hgrn_m2(self, strm):
        S, ps, nc = self.S, self.ps, self.nc
        R1 = 99840
        wbufs = [self.view(R1 + i * 8192, BF16, [KC, 256]) for i in range(2)]
        off = [R1 + 16384]

        def alloc(dt, shape):
            n = 1
            for x in shape:
                n *= x
            nb = n * (4 if dt == F32 else 2)
            nb = (nb + 63) // 64 * 64
            v = self.view(off[0], dt, shape)
            off[0] += nb
            return v
        sgate = alloc(BF16, [8, NREAL])
        Scur = alloc(F32, [8, 128]); SmF = alloc(F32, [8, 128])
        SAb = [alloc(BF16, [8, 128]) for _ in range(3)]
        Aall = alloc(F32, [4, 72])
        OF = alloc(F32, [8, 128]); OSQ = alloc(F32, [8, 128]); RS = alloc(F32, [8, 128])
        Qc = self.view(0, BF16, [8, NREAL]); oloc = self.view(16384, BF16, [8, NREAL])
        yhg = self.view(32768, BF16, [8, NREAL]); Smine = self.view(49152, BF16, [8, 8, 128])
        f2 = lambda v: v.rearrange("p h t -> p (h t)")
        RTB = TBS[0:2]

        for g4 in range(4):
            def ev_g(half, ti, t0, t1e, pv, bk, g4=g4):
                h = 2 * g4 + half
                S.op("act", lambda e: e.activation(out=sgate[:, h, t0:t1e], in_=pv, func=AF.Silu),
                     reads=[("ps", bk)], writes=[("sgate", h, ti)])
                S.op("pool", lambda e: e.tensor_scalar(out=sgate[:, h, t0:t1e], in0=sgate[:, h, t0:t1e],
                                                       scalar1=self.gnc[:, h:h + 1], scalar2=None, op0=ALU.mult),
                     reads=[("sgate", h, ti), "gn"], writes=[("sgate", h, ti)])
            self.proj_fm("m2", strm, self.gidx["hg%d" % g4], wbufs, RTB, ev_g)

        def out_block(i):
            c0 = 128 * i
            for h in range(8):
                bk = 2 + h // 4
                S.op("pe", lambda e, h=h, i=i, c0=c0, bk=bk: e.matmul(
                    ps[:, bk, (h % 4) * 128:(h % 4 + 1) * 128], lhsT=Smine[:, i, h, :], rhs=Qc[:, h, c0:c0 + 128],
                    start=True, stop=True),
                    reads=[("Smine", i), "Qc"], writes=[("ps", bk)])
            S.op("dve", lambda e, c0=c0: e.tensor_tensor(
                out=OF, in0=ps[:, 2:4, :].rearrange("p a (h t) -> p (a h) t", h=4), in1=oloc[:, :, c0:c0 + 128],
                op=ALU.add),
                reads=[("ps", 2), ("ps", 3), "oloc"], writes=["OF"])
            S.op("act", lambda e: e.activation(out=f2(OSQ), in_=f2(OF), func=AF.Square),
                 reads=["OF"], writes=["OSQ"])
            for a in range(2):
                S.op("pe", lambda e, a=a: e.matmul(ps[:, 4 + a, :], lhsT=self.ones128, rhs=f2(OSQ)[:, a * 512:(a + 1) * 512],
                                                   start=True, stop=True),
                     reads=["OSQ", "ones128"], writes=[("ps", 4 + a)])
            S.op("act", lambda e: e.activation(out=f2(RS), in_=ps[:, 4:6, :].rearrange("p a b -> p (a b)"),
                                               func=AF.Ln, bias=self.eps_rms, scale=1.0),
                 reads=[("ps", 4), ("ps", 5), "eps"], writes=["RS"])
            S.op("act", lambda e: e.activation(out=f2(RS), in_=f2(RS), func=AF.Exp, scale=-0.5),
                 reads=["RS"], writes=["RS"])

        def out_block_b(i):
            c0 = 128 * i
            S.op("dve", lambda e: e.tensor_tensor(out=f2(OF), in0=f2(OF), in1=f2(RS), op=ALU.mult),
                 reads=["OF", "RS"], writes=["OF"])
            S.op("dve", lambda e, c0=c0: e.tensor_tensor(out=yhg[:, :, c0:c0 + 128], in0=OF, in1=sgate[:, :, c0:c0 + 128],
                                                         op=ALU.mult),
                 reads=["OF"] + [("sgate", h, c0 // 512) for h in range(8)], writes=[("yhg", i)])

        S.dma("sp", lambda e: e.dma_start(out=Aall, in_=self.xag.rearrange("(r p) c -> p r c", p=128)),
              reads=["xag"], writes=["Aall"])
        xg3 = [x_.rearrange("(r p) (i c) -> r p i c", p=128, i=3) for x_ in self.xg]
        S.dma("sp", lambda e: e.dma_start(out=f2(SAb[2]), in_=xg3[2][0, :, 2, :]), reads=[("xg", 2)],
              writes=[("SAb", 2)])
        S.op("dve", lambda e: e.tensor_copy(out=f2(Scur), in_=f2(SAb[2])), reads=[("SAb", 2)], writes=["Scur"])
        for g in range(32):
            r, i = g % 4, g // 4
            sb = SAb[g % 3]
            S.dma("sp", lambda e, sb=sb, r=r, i=i: e.dma_start(out=f2(sb), in_=xg3[i // 3][r, :, i % 3, :]),
                  reads=[("xg", i // 3)], writes=[("SAb", g % 3)])
            if r == 0:
                S.op("dve", lambda e: e.tensor_scalar(out=f2(SmF), in0=f2(Scur), scalar1=self.selc[:, 0:1],
                                                      scalar2=None, op0=ALU.mult),
                     reads=["Scur", "sel"], writes=["SmF"])
            else:
                dst = SmF if r < 3 else Smine[:, i]
                S.op("dve", lambda e, r=r, dst=dst: e.scalar_tensor_tensor(
                    out=f2(dst), in0=f2(Scur), scalar=self.selc[:, r:r + 1], in1=f2(SmF),
                    op0=ALU.mult, op1=ALU.add),
                    reads=["Scur", "sel", "SmF"], writes=(["SmF"] if r < 3 else [("Smine", i)]))
            if g < 31:
                for h in range(8):
                    S.op("dve", lambda e, h=h, r=r, i=i, sb=sb: e.scalar_tensor_tensor(
                        out=Scur[:, h, :], in0=Scur[:, h, :], scalar=Aall[:, r, i * 8 + h:i * 8 + h + 1],
                        in1=sb[:, h, :], op0=ALU.mult, op1=ALU.add),
                        reads=["Scur", "Aall", ("SAb", g % 3)], writes=["Scur"])
            if g % 4 == 3:
                out_block(g // 4)
            if g % 4 == 1 and g >= 5:
                out_block_b((g - 5) // 4)
        out_block_b(7)

        S.barrier()

    def attn_m3(self, strm):
        S, ps, nc = self.S, self.ps, self.nc
        R1 = 99840
        psb = lambda bk: ps[:, bk, :].bitcast(BF16)
        K_all = self.view(R1, BF16, [2, 4112])
        V_all = self.view(R1 + 16448, BF16, [33, 258])
        IK_all = self.view(R1 + 33536, BF16, [4096])
        AugK = self.view(R1 + 41728, BF16, [4112])
        qT = self.view(R1 + 49952, BF16, [8, NREAL])
        iqT = self.view(R1 + 66336, BF16, [8, NREAL])
        sc = self.view(R1 + 82720, F32, [4096])
        wbufs = [self.view(R1 + 82720 + i * 8192, BF16, [KC, 256]) for i in range(2)]
        Dg = self.view(R1 + 99104, BF16, [16, 128])
        yatt = self.view(0, BF16, [8, NREAL])
        mb = self.view(16384, BF16, [4096])
        mbT = self.view(24576, BF16, [32, 128])
        junk = self.view(49152, U8, [4096])
        iqz = self.view(49152 + 4096, BF16, [16, 128])
        rh = [self.view(57344 + q * 1024, BF16, [512]) for q in range(4)]
        ya = self.view(61440, BF16, [8, 128])
        PTb = [self.view(61440 + q * 2048, BF16, [1024]) for q in range(2)]
        cbt = self.view(65536, BF16, [4, 128])
        kst = self.view(49152, BF16, [2, NREAL])
        vst = self.view(49152 + 4096, BF16, [8, 258])
        ikst = self.view(49152 + 4096 + 4160, BF16, [NREAL])
        iktmp = self.view(49152 + 10304, F32, [64])
        ikn2 = self.view(49152 + 10304 + 256, BF16, [128])
        cst = self.cst
        AugQ = cst[:, 664:1176].bitcast(BF16)
        AugR = cst[:, 1176:1688].bitcast(BF16)
        wq = cst[:, 1688:1816].rearrange("p (i h) -> p i h", h=16)
        H = cst[:, 1816:1848]
        Pt = cst[:, 1848:1976]
        g1 = cst[:, 1976:2008]
        mrow = cst[:, 2008:2016]; cc = cst[:, 2016:2024]; rs = cst[:, 2024:2032]
        Bt = cst[:, 2032:2033]; Wc = cst[:, 2033:2034]; mid = cst[:, 2034:2035]; cnt = cst[:, 2035:2036]
        u2 = cst[:, 2036:2037]; tau = cst[:, 2037:2038]; rstd1 = cst[:, 2038:2039]
        nslope = cst[:, 2040:2048]
        Qt = cst[:, 2048:2080].rearrange("p (r j) -> p r j", r=4)
        kmx = cst[:, 2080:2081]
        pw = cst[:, 2104:2136]
        gik = cst[:, 2136:2200]; bik = cst[:, 2200:2264]
        st6 = cst[:, 2264:2270]; mv = cst[:, 2270:2272]
        TL = [(i, 128 * i, 128) for i in range(8)] + [(8, NREAL, NMETA)]
        RTB = TBS[0:2]
        NB = 16

        S.dma("sp", lambda e: e.dma_start(out=cst[:, 2040:2264], in_=self.catt), writes=["catt"])
        S.dma("sp", lambda e: e.dma_start(out=Pt, in_=self.cmat_d[:, 384:512]), writes=["Pt"])
        S.op("pool", lambda e: e.memset(AugK[0:65, :], 0.0), writes=["AugK"])
        S.op("pool", lambda e: e.memset(AugQ[0:65, :], 0.0), writes=["AugQ"])
        S.op("pool", lambda e: e.memset(AugR[0:65, :], 0.0), writes=["AugR"])
        for rr in range(3):
            S.dma("pool", lambda e, rr=rr: e.dma_start(out=AugK[32 * rr:32 * rr + 1, :], in_=self.augk[rr:rr + 1, :]),
                  writes=["AugK"])
        for rr in range(2):
            S.dma("pool", lambda e, rr=rr: e.dma_start(out=AugQ[32 * rr:32 * rr + 1, :], in_=self.augs[rr:rr + 1, :]),
                  writes=["AugQ"])
            S.dma("pool", lambda e, rr=rr: e.dma_start(out=AugR[32 * rr:32 * rr + 1, :], in_=self.augs[rr:rr + 1, :]),
                  writes=["AugR"])
        S.dma("pool", lambda e: e.dma_start(out=cbt, in_=self.cbt_d.rearrange("p (r s) -> p r s", r=4)), writes=["cbt"])
        S.op("dve", lambda e: e.memset(vst.rearrange("p i (k c) -> p i k c", k=2)[:, :, :, 128:129], 1.0),
             writes=["vst1"])
        S.op("dve", lambda e: e.memset(V_all[:, 32, :].rearrange("p (k c) -> p k c", k=2)[:, :, 128:129], 1.0),
             writes=["V1"])

        def ev_k(half, ti, t0, t1e, pv, bk):
            if ti < 2:
                S.op("act", lambda e: e.activation(out=kst[:, half, t0:t1e], in_=pv, func=AF.Copy),
                     reads=[("ps", bk)], writes=[("kst", half, ti)])
            else:
                S.op("act", lambda e: e.activation(out=K_all[:, half, 4096:4112], in_=pv, func=AF.Copy),
                     reads=[("ps", bk)], writes=[("Kmeta", half)])
        self.proj_fm("m3", strm, self.gidx["ak"], wbufs, TBS, ev_k)

        def ev_v(i, c0, n, pv, bk):
            src = pv.rearrange("p (k c) -> p k c", k=2)
            if i < 8:
                dst = vst[:, i, :].rearrange("p (k c) -> p k c", k=2)[:, :, 0:128]
                S.op("act", lambda e: e.activation(out=dst, in_=src, func=AF.Copy),
                     reads=[("ps", bk), "vst1"], writes=[("vst", i)])
            else:
                dst = V_all[0:n, 32, :].rearrange("p (k c) -> p k c", k=2)[:, :, 0:128]
                S.op("act", lambda e: e.activation(out=dst, in_=src, func=AF.Copy),
                     reads=[("ps", bk), "V1"], writes=["Vmeta"])
        self.proj_tm("m3", strm, self.gidx["av"], wbufs, TL, 256, ev_v)

        def ev_ik(i, c0, n, pv, bk):
            S.op("dve", lambda e: e.bn_stats(out=st6, in_=pv[:, 0:64]), reads=[("ps", bk)], writes=["st6"])
            S.op("dve", lambda e: e.bn_aggr(out=mv, in_=st6), reads=["st6"], writes=["mv"])
            S.op("act", lambda e: e.activation(out=rstd1, in_=mv[:, 1:2], func=AF.Sqrt, bias=self.eps_ik, scale=1.0),
                 reads=["mv", "eps"], writes=["rstd1"])
            S.op("dve", lambda e: e.reciprocal(out=rstd1, in_=rstd1), reads=["rstd1"], writes=["rstd1"])
            S.op("dve", lambda e: e.tensor_scalar(out=iktmp, in0=pv[:, 0:64], scalar1=mv[:, 0:1], scalar2=rstd1,
                                                  op0=ALU.subtract, op1=ALU.mult),
                 reads=[("ps", bk), "mv", "rstd1"], writes=["iktmp"])
            S.op("dve", lambda e: e.tensor_tensor(out=iktmp, in0=iktmp, in1=gik, op=ALU.mult),
                 reads=["iktmp", "catt"], writes=["iktmp"])
            S.op("dve", lambda e: e.tensor_tensor(out=ikn2[:, 0:64], in0=iktmp, in1=bik, op=ALU.add),
                 reads=["iktmp", "catt"], writes=["ikn2a"])
            S.op("pool", lambda e: e.tensor_copy(out=ikn2[:, 64:128], in_=ikn2[:, 0:64]),
                 reads=["ikn2a"], writes=["ikn2b"])
            S.op("act", lambda e, i=i: e.activation(out=wq[:, i, :], in_=pv[:, 64:80], func=AF.Copy,
                                                    scale=0.25 * 0.125),
                 reads=[("ps", bk)], writes=[("wq", i)])
            S.op("pe", lambda e: e.transpose(out=psb(2)[:, 0:128], in_=ikn2, identity=self.ident_bf),
                 reads=["ikn2a", "ikn2b", "ident"], writes=[("ps", 2)])
            S.op("act", lambda e, c0=c0: e.activation(out=ikst[:, c0:c0 + 128], in_=psb(2)[:, 0:128], func=AF.Copy),
                 reads=[("ps", 2)], writes=[("ikst", i)])
        self.proj_tm("m3", strm, self.gidx["ikw"], wbufs, TL[0:8], 80, ev_ik)

        S.dma("sp", lambda e: e.dma_start(out=self.ks.rearrange("p (k t) -> p k t", k=2), in_=kst),
              reads=[("kst", hh, ti) for hh in range(2) for ti in range(2)], writes=["ks"])
        S.dma("sp", lambda e: e.dma_start(out=self.vs[:, 0:2064].rearrange("p (i c) -> p i c", i=8), in_=vst),
              reads=[("vst", i) for i in range(8)] + ["vst1"], writes=["vs"])
        S.dma("sp", lambda e: e.dma_start(out=self.vs[:, 2064:3088], in_=ikst),
              reads=[("ikst", i) for i in range(8)], writes=["vs2"])
        S.barrier()
        rg = [[0, 1, 2, 3], [4, 5, 6, 7]]
        S.coll(lambda e: e.collective_compute("AllGather", ALU.bypass, replica_groups=rg,
                                              ins=[self.ks], outs=[self.kg]), writes=["kg"])
        S.coll(lambda e: e.collective_compute("AllGather", ALU.bypass, replica_groups=rg,
                                              ins=[self.vs], outs=[self.vg]), writes=["vg"])

        for g4 in range(4):
            def ev_q(half, ti, t0, t1e, pv, bk, g4=g4):
                h = 2 * g4 + half
                S.op("act", lambda e: e.activation(out=qT[:, h, t0:t1e], in_=pv, func=AF.Copy, scale=128.0 ** -0.5),
                     reads=[("ps", bk)], writes=[("qT", h, ti)])
            self.proj_fm("m3", strm, self.gidx["aq%d" % g4], wbufs, RTB, ev_q)
        for g4 in range(4):
            def ev_iq(half, ti, t0, t1e, pv, bk, g4=g4):
                h = 2 * g4 + half
                S.op("dve", lambda e: e.tensor_copy(out=iqT[:, h, t0:t1e], in_=pv),
                     reads=[("ps", bk)], writes=[("iqT", h, ti)])
            self.proj_fm("m3", strm, self.gidx["iq%d" % g4], wbufs, RTB, ev_iq)

        for r in range(4):
            S.dma("sp", lambda e, r=r: e.dma_start(
                out=K_all[:, :, r * 1024:(r + 1) * 1024],
                in_=self.kg[r * 128:(r + 1) * 128, :].rearrange("p (k t) -> p k t", k=2)),
                reads=["kg"], writes=["K_all"])
            S.dma("sp", lambda e, r=r: e.dma_start(
                out=V_all[:, r * 8:(r + 1) * 8, :],
                in_=self.vg[r * 128:(r + 1) * 128, 0:2064].rearrange("p (i c) -> p i c", i=8)),
                reads=["vg"], writes=["V_all"])
            S.dma("sp", lambda e, r=r: e.dma_start(
                out=IK_all[:, r * 1024:(r + 1) * 1024], in_=self.vg[r * 128:(r + 1) * 128, 2064:3088]),
                reads=["vg"], writes=["IK_all"])
        S.barrier()

        S.op("pool", lambda e: e.memset(iqz.rearrange("p h t -> p (h t)"), 0.0), writes=["iqz"])
        sc4 = sc.rearrange("p (r c) -> p r c", r=4)
        mb4 = mb.rearrange("p (r c) -> p r c", r=4)
        jk4 = junk.rearrange("p (r c) -> p r c", r=4)
        def geom(i):
            q0 = 128 * i
            nk = 128 * (i + 1)
            pieces = [(r, c0, min(512, nk - c0)) for r in range(4) for c0 in range(0, nk, 512)]
            return q0, nk, pieces

        def st_idx(i):
            q0, nk, pieces = geom(i)
            for h in range(16):
                S.op("pool", lambda e, h=h: e.tensor_scalar(out=Dg[:, h, :], in0=self.ident_bf,
                                                            scalar1=wq[:, i, h:h + 1], scalar2=None, op0=ALU.mult),
                     reads=["ident", ("wq", i)], writes=["Dg"])
            for h in range(16):
                hb = h % 2
                eng = "pool" if h % 2 == 0 else "act"
                if eng == "pool":
                    S.op("pool", lambda e, h=h, hb=hb: e.tensor_copy(
                        out=iqz[hb * 64:(hb + 1) * 64, h, :], in_=iqT[hb * 64:(hb + 1) * 64, h // 2, q0:q0 + 128]),
                        reads=["iqT", "iqz"], writes=[("iqzh", h)])
                else:
                    S.op("act", lambda e, h=h, hb=hb: e.activation(
                        out=iqz[hb * 64:(hb + 1) * 64, h, :], in_=iqT[hb * 64:(hb + 1) * 64, h // 2, q0:q0 + 128],
                        func=AF.Copy),
                        reads=["iqT", "iqz"], writes=[("iqzh", h)])
            for pi, (r, c0, cn) in enumerate(pieces):
                col0 = r * 1024 + c0
                accb = 4 + pi % 2

                def head_mm(h, cn=cn, col0=col0):
                    bk, hb = h % 4, h % 2
                    S.op("pe", lambda e: e.matmul(
                        ps[:, bk, 0:cn], lhsT=iqz[:, h, :],
                        rhs=IK_all[:, col0:col0 + cn], start=True, stop=True),
                        reads=["IK_all", ("iqzh", h)], writes=[("ps", bk)])
                    if h % 8 in (0, 3, 6):
                        S.op("act", lambda e: e.activation(out=rh[bk][:, 0:cn], in_=ps[:, bk, 0:cn], func=AF.Relu),
                             reads=[("ps", bk)], writes=[("rh", bk)])
                    else:
                        S.op("dve", lambda e: e.tensor_scalar(out=rh[bk][:, 0:cn], in0=ps[:, bk, 0:cn], scalar1=0.0,
                                                              scalar2=None, op0=ALU.max),
                             reads=[("ps", bk)], writes=[("rh", bk)])

                def head_acc(h, cn=cn, accb=accb):
                    bk = h % 4
                    S.op("pe", lambda e: e.matmul(
                        ps[:, accb, 0:cn], lhsT=Dg[:, h, :], rhs=rh[bk][:, 0:cn], start=(h == 0), stop=(h == 15)),
                        reads=["Dg", ("rh", bk)], writes=[("ps", accb)])
                for h in range(16):
                    head_mm(h)
                    if h >= 2:
                        head_acc(h - 2)
                head_acc(14)
                head_acc(15)
                S.op("act", lambda e, accb=accb, col0=col0, cn=cn: e.activation(
                    out=sc[:, col0:col0 + cn], in_=ps[:, accb, 0:cn], func=AF.Copy),
                    reads=[("ps", accb)], writes=["sc"])

        def st_bis(i):
            q0, nk, pieces = geom(i)
            scv, mbv, jkv = sc4[:, :, 0:nk], mb4[:, :, 0:nk], jk4[:, :, 0:nk]
            S.op("dve", lambda e: e.reduce_max(out=Bt, in_=scv, axis=AX.XY, apply_absolute_value=True),
                 reads=["sc"], writes=["Bt"])
            S.op("dve", lambda e: e.tensor_tensor(out=sc4[:, :, q0:q0 + 128], in0=sc4[:, :, q0:q0 + 128], in1=cbt,
                                                  op=ALU.add),
                 reads=["sc", "cbt", "Bt"], writes=["sc"])
            S.op("dve", lambda e: e.tensor_scalar(out=Wc, in0=Bt, scalar1=2.0002, scalar2=1e-6,
                                                  op0=ALU.mult, op1=ALU.add), reads=["Bt"], writes=["Wc"])
            S.op("dve", lambda e: e.tensor_scalar(out=H[:, 0:NB + 1], in0=pw[:, 0:NB + 1], scalar1=Wc, scalar2=None,
                                                  op0=ALU.mult), reads=["Wc", "catt"], writes=["H"])
            S.op("dve", lambda e: e.memset(mid, 0.0), writes=["mid"])
            for k in range(NB):
                S.op("dve", lambda e: e.tensor_scalar(
                    out=jkv, in0=scv, scalar1=mid, scalar2=0.0, op0=ALU.is_ge, op1=ALU.add, accum_out=cnt),
                    reads=["sc", "mid"], writes=["junk", "cnt"])
                S.op("dve", lambda e, k=k: e.tensor_scalar(out=u2, in0=cnt, scalar1=256.0, scalar2=H[:, k:k + 1],
                                                           op0=ALU.is_ge, op1=ALU.mult),
                     reads=["cnt", "H"], writes=["u2"])
                S.op("dve", lambda e, k=k: e.scalar_tensor_tensor(out=mid, in0=mid, scalar=H[:, k + 1:k + 2], in1=u2,
                                                                  op0=ALU.subtract, op1=ALU.add),
                     reads=["mid", "H", "u2"], writes=["mid"])
            S.op("dve", lambda e: e.tensor_tensor(out=tau, in0=mid, in1=H[:, NB:NB + 1], op=ALU.subtract),
                 reads=["mid", "H"], writes=["tau"])
            S.op("dve", lambda e: e.tensor_scalar(
                out=mbv, in0=scv, scalar1=tau, scalar2=-30000.0, op0=ALU.is_lt, op1=ALU.mult),
                reads=["sc", "tau"], writes=["mb"])
            nb_ = i + 1
            sc5 = scv.rearrange("p r (j s) -> p r j s", s=128)
            mb5 = mbv.rearrange("p r (j s) -> p r j s", s=128)
            S.op("dve", lambda e: e.tensor_tensor(
                out=sc5, in0=mb5, in1=Pt.unsqueeze(1).unsqueeze(1).to_broadcast([128, 4, nb_, 128]), op=ALU.add),
                reads=["mb", "Pt", "sc"], writes=["sc"])
            g1v = g1.rearrange("p (r j) -> p r j", r=4)[:, :, 0:nb_]
            S.op("dve", lambda e: e.tensor_reduce(out=g1v, in_=sc5, axis=AX.X, op=ALU.max),
                 reads=["sc"], writes=["g1"])
            S.op("dve", lambda e: e.tensor_tensor(out=g1v, in0=g1v, in1=Qt[:, :, 0:nb_], op=ALU.add),
                 reads=["g1", "catt"], writes=["g1"])
            S.op("dve", lambda e: e.tensor_reduce(out=kmx, in_=g1v, axis=AX.XY, op=ALU.max),
                 reads=["g1"], writes=["kmx"])
            S.op("dve", lambda e: e.tensor_scalar(out=kmx, in0=kmx, scalar1=15.0, scalar2=None, op0=ALU.max),
                 reads=["kmx"], writes=["kmx"])
            S.op("dve", lambda e: e.tensor_scalar(out=cc, in0=nslope, scalar1=kmx, scalar2=None, op0=ALU.mult),
                 reads=["kmx", "catt"], writes=["cc"])

        def st_mbT(i):
            kts = [(r, ip) for r in range(4) for ip in range(i + 1)]
            for g0 in range(0, len(kts), 8):
                grp = kts[g0:g0 + 8]
                bk = 6 + (g0 // 8) % 2
                for s_, (r, ip) in enumerate(grp):
                    S.op("pe", lambda e, bk=bk, s_=s_, r=r, ip=ip: e.transpose(
                        out=psb(bk)[:, s_ * 128:(s_ + 1) * 128], in_=mb[:, r * 1024 + ip * 128:r * 1024 + ip * 128 + 128],
                        identity=self.ident_bf),
                        reads=["mb", "ident"], writes=[("ps", bk)])
                for s_, (r, ip) in enumerate(grp):
                    S.op("act", lambda e, bk=bk, s_=s_, r=r, ip=ip: e.activation(
                        out=mbT[:, r * 8 + ip, :], in_=psb(bk)[:, s_ * 128:(s_ + 1) * 128], func=AF.Copy),
                        reads=[("ps", bk)], writes=["mbT"])

        def st_passA(i):
            q0, nk, pieces = geom(i)
            S.op("dve", lambda e: e.memset(ps[:, 5:8, :].rearrange("p a b -> p (a b)"), 0.0),
                 writes=[("ps", 5), ("ps", 6), ("ps", 7)])
            Dc = PTb[1].rearrange("p (h t) -> p h t", h=8)
            for h in range(8):
                S.op("dve", lambda e, h=h: e.tensor_scalar(out=Dc[:, h, :], in0=self.ident_bf, scalar1=cc[:, h:h + 1],
                                                           scalar2=None, op0=ALU.mult),
                     reads=["ident", "cc"], writes=[("PTb", 1)])
            for a_ in range(2):
                S.op("pe", lambda e, a_=a_: e.matmul(ps[:, 4, :], lhsT=self.ones_bf,
                                                     rhs=PTb[1][:, a_ * 512:(a_ + 1) * 512],
                                                     start=True, stop=True),
                     reads=[("PTb", 1), "ones_bf"], writes=[("ps", 4)])
                S.op("dve", lambda e, a_=a_: e.tensor_copy(out=AugR[64:65, a_ * 512:(a_ + 1) * 512], in_=ps[64:65, 4, :]),
                     reads=[("ps", 4)], writes=["AugR"])

        def st_passB(i):
            q0, nk, pieces = geom(i)
            ktl = [(r * 1024 + ip * 128, r * 8 + ip, 128) for r in range(4) for ip in range(i + 1)] + [(4096, 32, NMETA)]
            Oreg = lambda h: ps[:, 5 + h // 3, (h % 3) * 129:(h % 3 + 1) * 129]

            def logits(qi):
                col0, vt, n = ktl[qi]
                meta = (n == NMETA)
                pair = (0, 1) if qi % 2 == 0 else (2, 3)
                for h in range(8):
                    kvh = h // 4
                    out = ps[0:n, pair[h // 4], (h % 4) * 128:(h % 4 + 1) * 128]
                    S.op("pe", lambda e, out=out, kvh=kvh, h=h: e.matmul(
                        out, lhsT=K_all[:, kvh, col0:col0 + n], rhs=qT[:, h, q0:q0 + 128], start=True, stop=False),
                        reads=["K_all", "qT", ("Kmeta", kvh)], writes=[("ps", pair[h // 4])])
                    S.op("pe", lambda e, out=out, h=h: e.matmul(
                        out, lhsT=AugK[0:65, col0:col0 + n], rhs=AugR[0:65, h * 128:(h + 1) * 128],
                        start=False, stop=meta),
                        reads=["AugK", "AugR"], writes=[("ps", pair[h // 4])])
                    if not meta:
                        S.op("pe", lambda e, out=out: e.matmul(
                            out, lhsT=self.ident_bf, rhs=mbT[:, vt, :], start=False, stop=True),
                            reads=["mbT", "ident"], writes=[("ps", pair[h // 4])])
                pt = PTb[qi % 2]
                S.op("act", lambda e: e.activation(
                    out=pt[0:n, :], in_=ps[0:n, pair[0]:pair[0] + 2, :].rearrange("p a b -> p (a b)"), func=AF.Exp),
                    reads=[("ps", pair[0]), ("ps", pair[1])], writes=[("PTb", qi % 2)])

            def pv(qi):
                col0, vt, n = ktl[qi]
                pt = PTb[qi % 2]
                for h in range(8):
                    kvh = h // 4
                    S.op("pe", lambda e, h=h, kvh=kvh: e.matmul(
                        Oreg(h), lhsT=pt[0:n, h * 128:(h + 1) * 128], rhs=V_all[0:n, vt, kvh * 129:(kvh + 1) * 129],
                        start=False, stop=(qi == len(ktl) - 1)),
                        reads=[("PTb", qi % 2), "V_all", "Vmeta"], writes=[("ps", 5 + h // 3)])
            logits(0)
            for qi in range(len(ktl)):
                if qi + 1 < len(ktl):
                    logits(qi + 1)
                pv(qi)

        def st_fin(i):
            q0 = 128 * i
            for b3 in range(3):
                nh = 3 if b3 < 2 else 2
                Ov = ps[:, 5 + b3, 0:nh * 129].rearrange("p (h c) -> p h c", c=129)
                S.op("dve", lambda e, Ov=Ov, b3=b3, nh=nh: e.reciprocal(out=rs[:, 3 * b3:3 * b3 + nh], in_=Ov[:, :, 128]),
                     reads=[("ps", 5 + b3)], writes=[("rs", b3)])
                S.op("dve", lambda e, Ov=Ov, b3=b3, nh=nh: e.tensor_tensor(
                    out=ya[:, 3 * b3:3 * b3 + nh, :], in0=Ov[:, :, 0:128],
                    in1=rs[:, 3 * b3:3 * b3 + nh].unsqueeze(2).to_broadcast([128, nh, 128]), op=ALU.mult),
                    reads=[("ps", 5 + b3), ("rs", b3)], writes=[("ya", b3), ("PTb", 0)])
            for h in range(8):
                S.op("pe", lambda e, h=h: e.transpose(out=psb(4)[:, h * 128:(h + 1) * 128], in_=ya[:, h, :],
                                                      identity=self.ident_bf),
                     reads=[("ya", h // 3), ("PTb", 0), "ident"], writes=[("ps", 4)])
            S.op("act", lambda e: e.activation(out=yatt[:, :, q0:q0 + 128],
                                               in_=psb(4).rearrange("p (h t) -> p h t", h=8), func=AF.Copy),
                 reads=[("ps", 4)], writes=[("yatt", i)])

        st_idx(0)
        st_bis(0)
        st_mbT(0)
        for i in range(8):
            if i + 1 < 8:
                st_idx(i + 1)
            st_passA(i)
            if i + 1 < 8:
                st_bis(i + 1)
            st_passB(i)
            st_fin(i)
            if i + 1 < 8:
                st_mbT(i + 1)
        S.barrier()

    def merge_m4(self, strm, cg, cb):
        S, ps, nc = self.S, self.ps, self.nc
        R1 = 99840
        RTB = TBS[0:2]
        yatt = self.view(0, BF16, [8, NREAL]); yhg = self.view(32768, BF16, [8, NREAL])
        merged = self.view(R1, BF16, [KC, NREAL])
        o = R1 + 32768
        wga = [self.view(o + q * 8192, BF16, [KC, 256]) for q in range(2)]
        wgh = [self.view(o + 16384 + q * 8192, BF16, [KC, 256]) for q in range(2)]
        wba = [self.view(o + 32768 + q * 4096, BF16, [8, 256]) for q in range(2)]
        wbh = [self.view(o + 40960 + q * 4096, BF16, [8, 256]) for q in range(2)]
        tm = [self.view(o + 49152 + q * 2048, F32, [512]) for q in range(4)]
        for mg in range(8):
            q = mg % 2
            S.dma("pool", lambda e, q=q, mg=mg: e.dma_start(out=wga[q], in_=self.win[self.gidx["ga%d" % mg]]),
                  writes=[("wga", q)])
            S.dma("pool", lambda e, q=q, mg=mg: e.dma_start(out=wgh[q], in_=self.win[self.gidx["gh%d" % mg]]),
                  writes=[("wgh", q)])
            S.dma("pool", lambda e, q=q, mg=mg: e.dma_start(out=wba[q], in_=self.wba_d[mg]), writes=[("wba", q)])
            S.dma("pool", lambda e, q=q, mg=mg: e.dma_start(out=wbh[q], in_=self.wbh_d[mg]), writes=[("wbh", q)])
            for half in range(2):
                mc = 2 * mg + half
                hs = slice(half * 128, (half + 1) * 128)
                for ti, (t0, t1e) in enumerate(RTB):
                    pp = (half * 2 + ti) % 2
                    b0 = 4 * pp
                    for k in range(KC):
                        S.op("pe", lambda e, b0=b0, q=q, k=k, hs=hs, t0=t0, t1e=t1e: e.matmul(
                            ps[:, b0, :], lhsT=wga[q][:, k, hs], rhs=strm[:, k, t0:t1e],
                            start=(k == 0), stop=(k == KC - 1)),
                            reads=[("wga", q), "strm"], writes=[("ps", b0)])
                    for k in range(8):
                        S.op("pe", lambda e, b0=b0, q=q, k=k, hs=hs, t0=t0, t1e=t1e: e.matmul(
                            ps[:, b0 + 1, :], lhsT=wba[q][:, k, hs], rhs=yatt[:, k, t0:t1e],
                            start=(k == 0), stop=(k == 7)),
                            reads=[("wba", q), "yatt"], writes=[("ps", b0 + 1)])
                    for k in range(KC):
                        S.op("pe", lambda e, b0=b0, q=q, k=k, hs=hs, t0=t0, t1e=t1e: e.matmul(
                            ps[:, b0 + 2, :], lhsT=wgh[q][:, k, hs], rhs=strm[:, k, t0:t1e],
                            start=(k == 0), stop=(k == KC - 1)),
                            reads=[("wgh", q), "strm"], writes=[("ps", b0 + 2)])
                    for k in range(8):
                        S.op("pe", lambda e, b0=b0, q=q, k=k, hs=hs, t0=t0, t1e=t1e: e.matmul(
                            ps[:, b0 + 3, :], lhsT=wbh[q][:, k, hs], rhs=yhg[:, k, t0:t1e],
                            start=(k == 0), stop=(k == 7)),
                            reads=[("wbh", q), "yhg"], writes=[("ps", b0 + 3)])
                    ta, th = tm[2 * pp], tm[2 * pp + 1]
                    S.op("act", lambda e, ta=ta, b0=b0: e.activation(out=ta, in_=ps[:, b0, :], func=AF.Sigmoid),
                         reads=[("ps", b0)], writes=[("tm", 2 * pp)])
                    S.op("dve", lambda e, ta=ta, b0=b0: e.tensor_tensor(out=ta, in0=ta, in1=ps[:, b0 + 1, :], op=ALU.mult),
                         reads=[("tm", 2 * pp), ("ps", b0 + 1)], writes=[("tm", 2 * pp)])
                    S.op("act", lambda e, th=th, b0=b0: e.activation(out=th, in_=ps[:, b0 + 2, :], func=AF.Sigmoid),
                         reads=[("ps", b0 + 2)], writes=[("tm", 2 * pp + 1)])
                    S.op("dve", lambda e, th=th, b0=b0: e.tensor_tensor(out=th, in0=th, in1=ps[:, b0 + 3, :], op=ALU.mult),
                         reads=[("tm", 2 * pp + 1), ("ps", b0 + 3)], writes=[("tm", 2 * pp + 1)])
                    S.op("pool", lambda e, ta=ta, th=th, mc=mc, t0=t0, t1e=t1e: e.tensor_tensor(
                        out=merged[:, mc, t0:t1e], in0=ta, in1=th, op=ALU.add),
                        reads=[("tm", 2 * pp), ("tm", 2 * pp + 1)], writes=[("merged", mc, ti)])
        S.barrier()
        z = self.view(R1 + 32768, F32, [KC, NREAL])
        wo = [self.view(53760 + q * 4096, BF16, [KC, 128]) for q in range(2)]
        strm_out = self.view(0, BF16, [KC, NREAL])
        for dc in range(KC):
            q = dc % 2
            S.dma("pool", lambda e, q=q, dc=dc: e.dma_start(out=wo[q], in_=self.wo_d[dc]), writes=[("wo", q)])
            S.dma("sp", lambda e, dc=dc: e.dma_start(out=z[:, dc, :], in_=self.h1s[dc * 128:(dc + 1) * 128, 0:NREAL]),
                  writes=[("l2", "z", dc, ti) for ti in range(2)])
            for ti, (t0, t1e) in enumerate(RTB):
                pb = (dc * 2 + ti) % 2
                for k in range(KC):
                    S.op("pe", lambda e, pb=pb, q=q, k=k, t0=t0, t1e=t1e: e.matmul(
                        ps[:, pb, :], lhsT=wo[q][:, k, :], rhs=merged[:, k, t0:t1e],
                        start=(k == 0), stop=(k == KC - 1)),
                        reads=[("wo", q), ("merged", k, ti)], writes=[("ps", pb)])
                S.op("dve", lambda e, pb=pb, dc=dc, t0=t0, t1e=t1e: e.scalar_tensor_tensor(
                    out=z[:, dc, t0:t1e], in0=ps[:, pb, :], scalar=1.0 / ALPHA, in1=z[:, dc, t0:t1e],
                    op0=ALU.mult, op1=ALU.add),
                    reads=[("ps", pb), ("l2", "z", dc, ti)], writes=[("l2", "z", dc, ti)])
        self.ln_apply("l2", z, RTB, cg, cb, 33280, LN_EPS / (ALPHA * ALPHA), strm_out=strm_out,
                      resid_out=self.h2s)
        S.barrier()

    def eps_col(self, val):
        return self.eps_cols[val]

    def build(self):
        nc, S = self.nc, self.S
        stage = self.stage
        xT = self.din("xT", [D, NT])
        wg1 = self.din("wg1", [FC // 2, 128, KC, 256])
        wu1 = self.din("wu1", [FC // 2, 128, KC, 256])
        wd1 = self.din("wd1", [KC, 128, FC, 128])
        wg2 = self.din("wg2", [FC // 2, 128, KC, 256])
        wu2 = self.din("wu2", [FC // 2, 128, KC, 256])
        wd2 = self.din("wd2", [KC, 128, FC, 128])
        cvec = self.din("cvec", [128, 128])
        cmat = self.cmat_d = self.din("cmat", [128, 512])
        self.catt = self.din("catt", [128, 224])
        self.augk = self.din("augk", [3, 4112])
        self.augs = self.din("augs", [2, 1024])
        self.augq = self.din("augq", [8, 1024])
        self.cbt_d = self.din("cbt", [128, 512])
        self.win = self.din("win", [len(GROUPS), 128, KC, 256])
        self.wba_d = self.din("wba", [8, 128, 8, 256])
        self.wbh_d = self.din("wbh", [8, 128, 8, 256])
        self.wo_d = self.din("wo", [KC, 128, KC, 128])
        self.gidx = {nm: i for i, (nm, _, _) in enumerate(GROUPS)}
        self.h1s = h1s = self.dscratch("h1s", [D, NT])
        self.h2s = self.dscratch("h2s", [D, NREAL])
        self.xs = [self.dscratch("xs%d" % q, [128, 3 * 1024], BF16) for q in range(3)]
        self.xg = [self.dscratch("xg%d" % q, [512, 3 * 1024], BF16) for q in range(3)]
        self.xa = self.dscratch("xa", [128, 72])
        self.xag = self.dscratch("xag", [512, 72])
        self.ks = self.dscratch("ks", [128, 2048], BF16)
        self.kg = self.dscratch("kg", [512, 2048], BF16)
        self.vs = self.dscratch("vs", [128, 3088], BF16)
        self.vg = self.dscratch("vg", [512, 3088], BF16)
        if stage == 1:
            dbg = self.dout("dbg", [D, NT])
        elif stage in (3, 4):
            dbg = self.dout("dbg", [128, 8 * NREAL])
            self.dbg2 = self.dout("dbg2", [128, 12000])
        elif stage == 5:
            dbg = self.dout("dbg", [D, NREAL])
        else:
            outT = self.dout("outT", [D, NREAL])

        from contextlib import ExitStack
        with ExitStack() as es:
            self.arena = es.enter_context(nc.sbuf_tensor("arena", [128, ARENA_F32], F32))
            self.cst = es.enter_context(nc.sbuf_tensor("cst", [128, CONST_F32], F32))
            self.ps = es.enter_context(nc.psum_tensor("ps", [128, 8, 512], F32))
            esems = {e: es.enter_context(nc.semaphore("sem_" + e)) for e in ENGS}
            dsems = [es.enter_context(nc.semaphore("dsem%d" % i)) for i in range(S.n_dma_sems + 8)]
            block = es.enter_context(nc.Block())
            cst = self.cst
            self.ones_f32 = cst[:, 0:128]
            cv = self.cv = cst[:, 128:256]
            epsA = cst[:, 256:257]
            self.eps_rms = cst[:, 257:258]
            self.eps_ik = cst[:, 258:259]
            self.eps_cols = {LN_EPS / (ALPHA * ALPHA): epsA}
            self.ident_bf = cst[:, 264:328].bitcast(BF16)
            self.ones_bf = cst[:, 328:392].bitcast(BF16)
            self.mask2 = cst[:, 392:520]
            self.ones128 = cst[:, 520:648]
            self.lbc = cst[:, 648:656]
            self.omlc = cst[:, 656:664]
            self.gnc = cv[:, 112:120]
            self.selc = cv[:, 120:124]
            S.op("dve", lambda e: e.memset(self.ones_f32, 1.0 / D), writes=["ones"])
            S.op("dve", lambda e: e.memset(self.ones128, 1.0 / 128), writes=["ones128"])
            S.op("dve", lambda e: e.memset(epsA, LN_EPS / (ALPHA * ALPHA)), writes=["eps"])
            S.op("dve", lambda e: e.memset(self.eps_rms, RMS_EPS), writes=["eps"])
            S.op("dve", lambda e: e.memset(self.eps_ik, LN_EPS), writes=["eps"])
            S.dma("sp", lambda e: e.dma_start(out=cv, in_=cvec), writes=["cv"])
            S.dma("sp", lambda e: e.dma_start(out=self.mask2, in_=cmat[:, 256:384]), writes=["mask2"])
            S.dma("pool", lambda e: e.dma_start(out=self.ident_bf, in_=cmat[:, 0:128]), writes=["ident"])
            S.dma("pool", lambda e: e.dma_start(out=self.ones_bf, in_=cmat[:, 128:256]), writes=["ones_bf"])
            S.op("dve", lambda e: e.tensor_tensor(out=self.lbc, in0=cv[:, 96:104], in1=cv[:, 104:112], op=ALU.subtract),
                 reads=["cv"], writes=["lb"])
            S.op("act", lambda e: e.activation(out=self.lbc, in_=self.lbc, func=AF.Sigmoid), reads=["lb"], writes=["lb"])
            S.op("dve", lambda e: e.tensor_scalar(out=self.omlc, in0=self.lbc, scalar1=-1.0, scalar2=1.0,
                                                  op0=ALU.mult, op1=ALU.add), reads=["lb"], writes=["lb"])
            strm0 = self.view(0, BF16, [KC, NT])
            S.dma("pool", lambda e: e.dma_start(out=strm0, in_=xT.rearrange("(k p) t -> p k t", p=128)),
                  writes=[("f1", "sin")])
            S.barrier()
            self.ffn_phase("f1", NT, TBS, strm0, 66560, wg1, wu1, wd1, xT, cv[:, 0:16], cv[:, 16:32],
                           resid_out=(dbg if stage == 1 else h1s))
            strm1 = self.view(66560, BF16, [KC, NT])
            if stage >= 2:
                self.hgrn_m1(strm1)
                self.hgrn_m2(strm1)
            if stage == 3:
                yhg = self.view(32768, BF16, [8 * NREAL])
                S.dma("pool", lambda e: e.dma_start(out=dbg, in_=yhg), reads=[])
            if stage >= 4:
                self.attn_m3(strm1)
            if stage == 4:
                yat = self.view(0, BF16, [8 * NREAL])
                S.dma("pool", lambda e: e.dma_start(out=dbg, in_=yat), reads=[])
            if stage >= 5:
                if stage == 5:
                    self.h2s = dbg
                self.merge_m4(strm1, cv[:, 32:48], cv[:, 48:64])
            if stage >= 6:
                strm2 = self.view(0, BF16, [KC, NREAL])
                self.ffn_phase("f2", NREAL, TBS[0:2], strm2, 66560, wg2, wu2, wd2, self.h2s, cv[:, 64:80],
                               cv[:, 80:96], final_out=outT)
            S.emit(block, esems, dsems)
        return nc


def _lay_gu(w):
    return np.ascontiguousarray(w.reshape(KC, 128, FC // 2, 256).transpose(2, 1, 0, 3))


def _lay_d(w):
    return np.ascontiguousarray(w.reshape(FC, 128, KC, 128).transpose(2, 1, 0, 3))


def _fm(v):
    return np.ascontiguousarray(v.reshape(KC, 128).T)


def _core_tokens(x, meta, c):
    b, j = c // 4, c % 4
    blocks = [x[b, 128 * (4 * i + j):128 * (4 * i + j) + 128] for i in range(8)]
    tok = np.concatenate(blocks + [meta], axis=0)
    return np.ascontiguousarray(tok.T)


def _mk_groups():
    g = []
    for hp in range(4):
        g += [("hf%d" % hp, 3664 + 256 * hp, 256), ("hq%d" % hp, 2640 + 256 * hp, 256),
              ("hi%d" % hp, 4688 + 256 * hp, 256)]
    for i in range(4):
        g.append(("hg%d" % i, 5712 + 256 * i, 256))
    g += [("ak", 1024, 256), ("av", 1280, 256), ("ikw", 2560, 80)]
    for i in range(4):
        g.append(("aq%d" % i, 256 * i, 256))
    for i in range(4):
        g.append(("iq%d" % i, 1536 + 256 * i, 256))
    for i in range(8):
        g.append(("ga%d" % i, 6736 + 256 * i, 256))
        g.append(("gh%d" % i, 8784 + 256 * i, 256))
    return g


GROUPS = _mk_groups()


def _lay_win(w):
    out = np.zeros((len(GROUPS), 128, KC, 256), np.float32)
    for gi, (nm, c0, nc_) in enumerate(GROUPS):
        out[gi, :, :, 0:nc_] = w[:, c0:c0 + nc_].reshape(KC, 128, nc_).transpose(1, 0, 2)
    return out


def prepare(inputs, stage):
    f = lambda k: np.asarray(inputs[k], dtype=np.float32)
    x, meta = f("x"), f("meta")
    shared = {
        "wg1": _lay_gu(f("ffn1_w_gate")[0]), "wu1": _lay_gu(f("ffn1_w_up")[0]),
        "wd1": _lay_d(f("ffn1_w_down")[0]),
        "win": _lay_win(f("w_in")[0]),
        "wg2": _lay_gu(f("ffn2_w_gate")[0]), "wu2": _lay_gu(f("ffn2_w_up")[0]),
        "wd2": _lay_d(f("ffn2_w_down")[0]),
        "wba": np.ascontiguousarray(f("w_branch_att")[0].reshape(8, 128, 8, 256).transpose(2, 1, 0, 3)),
        "wbh": np.ascontiguousarray(f("w_branch_hg")[0].reshape(8, 128, 8, 256).transpose(2, 1, 0, 3)),
        "wo": np.ascontiguousarray(f("w_out")[0].reshape(KC, 128, KC, 128).transpose(2, 1, 0, 3)),
    }
    slopes = (2.0 ** -(np.arange(8) + 1.0)).astype(np.float32)
    c = np.arange(4096)
    kpos = np.concatenate([16 + 128 * (4 * ((c % 1024) // 128) + c // 1024) + c % 128, np.arange(16)]).astype(np.float32)
    augk = np.stack([np.floor(kpos / 64.0), kpos % 64.0, np.ones_like(kpos)], 0).astype(np.float32)
    augs = np.stack([np.repeat(64.0 * slopes, 128), np.repeat(slopes, 128)], 0).astype(np.float32)
    shared["augk"] = augk
    shared["augs"] = augs
    cvec = np.zeros((128, 128), np.float32)
    cvec[:, 0:16] = _fm(f("ln1_g")[0]); cvec[:, 16:32] = _fm(f("ln1_b")[0])
    cvec[:, 32:48] = _fm(f("ln2_g")[0]); cvec[:, 48:64] = _fm(f("ln2_b")[0])
    cvec[:, 64:80] = _fm(f("ln3_g")[0]); cvec[:, 80:96] = _fm(f("ln3_b")[0])
    lbl = f("hg_lb_logits")
    cvec[:, 96:104] = lbl[0].reshape(8, 128).T
    cvec[:, 104:112] = lbl[1].reshape(8, 128).T
    cvec[:, 112:120] = f("hg_norm_g")[0].T
    cmat = np.zeros((128, 512), np.float32)
    cmat[:, 384:512] = np.arange(128, dtype=np.float32)[None, :]
    cmat[:, 0:128] = np.eye(128, dtype=np.float32)
    cmat[:, 128:256] = 1.0
    sidx = np.arange(128)[:, None]; tidx = np.arange(128)[None, :]
    cmat[:, 256:384] = (((sidx <= tidx) & ((sidx // 64) == (tidx // 64))) | ((sidx < 64) & (tidx >= 64))).astype(np.float32)
    shared["cmat"] = cmat
    maps = []
    for c in range(8):
        m = dict(shared)
        m["xT"] = _core_tokens(x, meta, c)
        cv = cvec.copy()
        cv[:, 120 + (c % 4)] = 1.0
        m["cvec"] = cv
        j = c % 4
        p = np.arange(128, dtype=np.float32)
        qpos = np.stack([16 + 128 * (4 * i + j) + p for i in range(8)], 0)
        m["augq"] = np.ascontiguousarray((-slopes[None, :, None] * qpos[:, None, :]).reshape(8, 1024).astype(np.float32))
        catt = np.zeros((128, 224), np.float32)
        catt[:, 0:8] = -slopes[None, :]
        catt[:, 8:40] = np.array([16 + 128 * r + 512 * jj for r in range(4) for jj in range(8)], np.float32)[None, :]
        catt[:, 64:96] = (2.0 ** -(np.arange(32) + 1.0))[None, :]
        catt[:, 96:160] = f("idx_k_norm_g")[0][None, :]
        catt[:, 160:224] = f("idx_k_norm_b")[0][None, :]
        m["catt"] = catt
        tt = np.arange(128)[:, None, None]; rr = np.arange(4)[None, :, None]; ss = np.arange(128)[None, None, :]
        m["cbt"] = np.where(128 * (rr - j) + (ss - tt) > 0, -1e30, 0.0).astype(np.float32).reshape(128, 512)
        maps.append(m)
    return maps


_NC_CACHE = {}


def kernel(**inputs):
    stage = int(os.environ.get("KSTAGE", "9"))
    if stage not in _NC_CACHE:
        _NC_CACHE[stage] = Builder(stage).build()
    nc = _NC_CACHE[stage]
    maps = prepare(inputs, stage)
    res = run_bass_kernel_spmd(nc, maps, core_ids=list(range(8)))
    if stage == 4:
        return [(r["dbg"], r["dbg2"]) for r in res.results]
    if stage < 6:
        return [r["dbg"] for r in res.results]
    out = np.zeros((2, 4096, D), np.float32)
    for c in range(8):
        b, j = c // 4, c % 4
        o = res.results[c]["outT"]
        for i in range(8):
            g = 4 * i + j
            out[b, 128 * g:128 * g + 128] = o[:, 128 * i:128 * i + 128].T
    return out
```

```python
import os
import numpy as np
import concourse.bass as bass
import concourse.mybir as mybir
from concourse.bass_utils import run_bass_kernel_spmd

F32 = mybir.dt.float32
BF16 = mybir.dt.bfloat16
U8 = mybir.dt.uint8
AF = mybir.ActivationFunctionType
ALU = mybir.AluOpType
AX = mybir.AxisListType

D = 2048
DFF = 5632
NMETA = 16
NREAL = 1024
NT = NREAL + NMETA
KC = D // 128
FC = DFF // 128
TBS = [(0, 512), (512, 1024), (1024, 1040)]
ALPHA = 2.0 ** 0.25
LN_EPS = 1e-5
RMS_EPS = 1e-6

ENGS = ("pe", "act", "dve", "pool", "sp")


class Op:
    __slots__ = ("eng", "fn", "deps", "is_dma", "dsem", "dval", "sig", "sigidx", "waits")

    def __init__(self, eng, fn, is_dma=False):
        self.eng = eng
        self.fn = fn
        self.deps = []
        self.is_dma = is_dma
        self.dsem = None
        self.dval = 0
        self.sig = False
        self.sigidx = 0
        self.waits = []


class Sched:
    def __init__(self, n_dma_sems=40, same_engine_sync=True):
        self.ops = {e: [] for e in ENGS}
        self.lastw = {}
        self.readers = {}
        self.n_dma = 0
        self.n_dma_sems = n_dma_sems
        self.dma_hist = {}
        self.same_engine_sync = same_engine_sync
        self.n_coll = 0

    def _add(self, op, reads, writes):
        deps = set()
        for k in reads:
            w = self.lastw.get(k)
            if w is not None:
                deps.add(w)
        for k in writes:
            w = self.lastw.get(k)
            if w is not None:
                deps.add(w)
            for r in self.readers.get(k, ()):
                deps.add(r)
        op.deps = list(deps)
        for k in reads:
            self.readers.setdefault(k, []).append(op)
        for k in writes:
            self.lastw[k] = op
            self.readers[k] = []
        self.ops[op.eng].append(op)
        return op

    def op(self, eng, fn, reads=(), writes=()):
        return self._add(Op(eng, fn), reads, writes)

    def dma(self, q, fn, reads=(), writes=()):
        op = Op(q, fn, is_dma=True)
        slot = self.n_dma % self.n_dma_sems
        op.dsem = slot
        op.dval = 16 * (self.n_dma // self.n_dma_sems + 1)
        self.n_dma += 1
        self._add(op, reads, writes)
        prev = self.dma_hist.get(slot)
        if prev is not None:
            op.deps.append(prev)
        self.dma_hist[slot] = op
        return op

    def coll(self, fn, reads=(), writes=()):
        op = Op("pool", fn, is_dma=True)
        op.dsem = self.n_dma_sems + self.n_coll
        op.dval = 1
        self.n_coll += 1
        self._add(op, reads, writes)
        self.dma_hist[op.dsem] = op
        return op

    def barrier(self):
        lasts = []
        for e in ENGS:
            for o in reversed(self.ops[e]):
                if not o.is_dma and o.fn is not None:
                    lasts.append(o)
                    break
        dmas = list(self.dma_hist.values())
        for e in ENGS:
            op = Op(e, None)
            op.deps = [o for o in lasts if o.eng != e] + dmas
            self.ops[e].append(op)
        self.lastw = {}
        self.readers = {}

    def _skip(self, d, op):
        return d.eng == op.eng and (d.eng in ("pe", "sp") or not self.same_engine_sync)

    def finalize(self):
        for e in ENGS:
            for op in self.ops[e]:
                for d in op.deps:
                    if not d.is_dma and not self._skip(d, op):
                        d.sig = True
        for e in ENGS:
            c = 0
            for op in self.ops[e]:
                if op.sig:
                    c += 1
                    op.sigidx = c
        for e in ENGS:
            waited = {}
            for op in self.ops[e]:
                need = {}
                for d in op.deps:
                    if d.is_dma:
                        key, val = ("d", d.dsem), d.dval
                    else:
                        if self._skip(d, op):
                            continue
                        key, val = ("e", d.eng), d.sigidx
                    if waited.get(key, 0) >= val:
                        continue
                    if need.get(key, 0) < val:
                        need[key] = val
                for k, v in need.items():
                    waited[k] = v
                op.waits = list(need.items())

    def emit(self, block, esems, dsems):
        self.finalize()
        regs = {"pe": block.tensor, "act": block.scalar, "dve": block.vector,
                "pool": block.gpsimd, "sp": block.sync}
        final = {d.dsem: d.dval for d in self.dma_hist.values()}

        def make(e):
            ops = self.ops[e]

            def body(eng):
                for op in ops:
                    for (kind, which), val in op.waits:
                        eng.wait_ge(dsems[which] if kind == "d" else esems[which], val)
                    if op.fn is None:
                        continue
                    ins = op.fn(eng)
                    if op.is_dma:
                        ins.then_inc(dsems[op.dsem], 16 if op.dsem < self.n_dma_sems else 1)
                    elif op.sig:
                        ins.then_inc(esems[e], 1)
                if e == "sp":
                    for slot, val in final.items():
                        eng.wait_ge(dsems[slot], val)
            return body

        for e in ENGS:
            regs[e](make(e))


ARENA_F32 = 50816
CONST_F32 = 2304


class Builder:
    def __init__(self, stage):
        self.stage = stage
        self.nc = bass.Bass("TRN2", target_bir_lowering=False)
        self.S = Sched()
        self.dram = {}

    def din(self, name, shape, dt=F32):
        self.dram[name] = self.nc.dram_tensor(name, list(shape), dt, kind="ExternalInput").ap()
        return self.dram[name]

    def dout(self, name, shape, dt=F32):
        self.dram[name] = self.nc.dram_tensor(name, list(shape), dt, kind="ExternalOutput").ap()
        return self.dram[name]

    def dscratch(self, name, shape, dt=F32):
        self.dram[name] = self.nc.dram_tensor(name, list(shape), dt, kind="Internal").ap()
        return self.dram[name]

    def view(self, off_bytes, dt, shape):
        n = 1
        for s in shape:
            n *= s
        esz = 4 if dt == F32 else (1 if dt == U8 else 2)
        assert off_bytes % 4 == 0
        nbytes = n * esz
        assert nbytes % 4 == 0
        assert off_bytes + nbytes <= ARENA_F32 * 4, (off_bytes, nbytes)
        v = self.arena[:, off_bytes // 4:(off_bytes + nbytes) // 4]
        if dt != F32:
            v = v.bitcast(dt)
        if len(shape) == 2:
            v = v.rearrange("p (a b) -> p a b", b=shape[1])
        elif len(shape) == 3:
            v = v.rearrange("p (a b c) -> p a b c", b=shape[1], c=shape[2])
        return v

    def ln_apply(self, tag, z, tbs, cg, cb, toff, eps_eff, strm_out=None, alias_key=None, resid_out=None,
                 final_out=None):
        S, ps = self.S, self.ps
        zsq = [self.view(toff + i * 2048, F32, [512]) for i in range(2)]
        meanb = self.view(toff + 4096, F32, [512])
        rstdb = self.view(toff + 6144, F32, [512])
        t1 = [self.view(toff + 8192 + i * 2048, F32, [512]) for i in range(2)]
        t2 = [self.view(toff + 12288 + i * 2048, F32, [512]) for i in range(2)]
        o32 = [self.view(toff + 16384 + i * 2048, F32, [512]) for i in range(2)]
        ones = self.ones_f32
        for ti, (t0, t1e) in enumerate(tbs):
            n = t1e - t0
            pm, pq = ps[:, 6, 0:n], ps[:, 7, 0:n]
            for dc in range(KC):
                zq = zsq[dc % 2]
                S.op("act", lambda e, zq=zq, dc=dc, t0=t0, t1e=t1e, n=n: e.activation(
                    out=zq[:, 0:n], in_=z[:, dc, t0:t1e], func=AF.Square),
                    reads=[(tag, "z", dc, ti)], writes=[(tag, "zsq", dc % 2)])
                S.op("pe", lambda e, pm=pm, dc=dc, t0=t0, t1e=t1e: e.matmul(
                    pm, lhsT=ones, rhs=z[:, dc, t0:t1e], start=(dc == 0), stop=(dc == KC - 1)),
                    reads=[(tag, "z", dc, ti)], writes=[("ps", 6)])
                S.op("pe", lambda e, pq=pq, zq=zq, dc=dc, n=n: e.matmul(
                    pq, lhsT=ones, rhs=zq[:, 0:n], start=(dc == 0), stop=(dc == KC - 1)),
                    reads=[(tag, "zsq", dc % 2)], writes=[("ps", 7)])
            S.op("act", lambda e, pm=pm, n=n: e.activation(out=meanb[:, 0:n], in_=pm, func=AF.Copy),
                 reads=[("ps", 6)], writes=[(tag, "meanb")])
            S.op("dve", lambda e, n=n: e.tensor_tensor(out=rstdb[:, 0:n], in0=meanb[:, 0:n], in1=meanb[:, 0:n],
                                                       op=ALU.mult),
                 reads=[(tag, "meanb")], writes=[(tag, "rstdb")])
            S.op("dve", lambda e, pq=pq, n=n: e.tensor_tensor(out=rstdb[:, 0:n], in0=pq, in1=rstdb[:, 0:n],
                                                              op=ALU.subtract),
                 reads=[("ps", 7), (tag, "rstdb")], writes=[(tag, "rstdb")])
            S.op("act", lambda e, n=n: e.activation(out=rstdb[:, 0:n], in_=rstdb[:, 0:n], func=AF.Sqrt,
                                                    bias=self.eps_cols[eps_eff], scale=1.0),
                 reads=[(tag, "rstdb")], writes=[(tag, "rstdb")])
            S.op("dve", lambda e, n=n: e.reciprocal(out=rstdb[:, 0:n], in_=rstdb[:, 0:n]),
                 reads=[(tag, "rstdb")], writes=[(tag, "rstdb")])
            S.op("dve", lambda e, n=n: e.scalar_tensor_tensor(out=meanb[:, 0:n], in0=meanb[:, 0:n], scalar=-1.0,
                                                              in1=rstdb[:, 0:n], op0=ALU.mult, op1=ALU.mult),
                 reads=[(tag, "meanb"), (tag, "rstdb")], writes=[(tag, "meanb")])
            for dc in range(KC):
                a, b_, o = t1[dc % 2], t2[dc % 2], o32[dc % 2]
                S.op("dve", lambda e, a=a, dc=dc, t0=t0, t1e=t1e, n=n: e.tensor_tensor(
                    out=a[:, 0:n], in0=z[:, dc, t0:t1e], in1=rstdb[:, 0:n], op=ALU.mult),
                    reads=[(tag, "z", dc, ti), (tag, "rstdb")], writes=[(tag, "t1", dc % 2)])
                S.op("dve", lambda e, a=a, b_=b_, n=n: e.tensor_tensor(
                    out=b_[:, 0:n], in0=a[:, 0:n], in1=meanb[:, 0:n], op=ALU.add),
                    reads=[(tag, "t1", dc % 2), (tag, "meanb")], writes=[(tag, "t2", dc % 2)])
                S.op("act", lambda e, b_=b_, o=o, dc=dc, n=n: e.activation(
                    out=o[:, 0:n], in_=b_[:, 0:n], func=AF.Identity,
                    bias=cb[:, dc:dc + 1], scale=cg[:, dc:dc + 1]),
                    reads=[(tag, "t2", dc % 2)], writes=[(tag, "o32", dc % 2)])
                if strm_out is not None:
                    wk = [(tag, "sout", dc, ti)] + ([(tag, alias_key, dc, ti)] if alias_key else [])
                    S.op("act", lambda e, b_=b_, dc=dc, t0=t0, t1e=t1e, n=n: e.activation(
                        out=strm_out[:, dc, t0:t1e], in_=b_[:, 0:n], func=AF.Identity,
                        bias=cb[:, dc:dc + 1], scale=cg[:, dc:dc + 1]),
                        reads=[(tag, "t2", dc % 2)], writes=wk)
                dst = resid_out if final_out is None else final_out
                if final_out is None or t0 < NREAL:
                    S.dma("sp", lambda e, o=o, dc=dc, t0=t0, t1e=t1e, n=n, dst=dst: e.dma_start(
                        out=dst[dc * 128:(dc + 1) * 128, t0:t1e], in_=o[:, 0:n]),
                        reads=[(tag, "o32", dc % 2)])

    def ffn_phase(self, tag, ntok, tbs, strm_in, strm_out_off, wg, wu, wd, resid, cg, cb,
                  resid_out=None, final_out=None):
        S, nc = self.S, self.nc
        ps = self.ps
        C_OFF = 33280
        B_OFF = 66560
        D_OFF = B_OFF + FC * NT * 2
        T_OFF = D_OFF + 2 * FC * 128 * 2
        hT = self.view(B_OFF, BF16, [FC, ntok])
        wgu = [self.view(C_OFF + i * 16384, BF16, [2, KC, 256]) for i in range(2)]
        wdb = [self.view(D_OFF + i * FC * 128 * 2, BF16, [FC, 128]) for i in range(2)]
        z = self.view(0, F32, [KC, ntok])
        sil = [self.view(T_OFF + 8192 + i * 2048, F32, [512]) for i in range(2)]
        strm_out = self.view(strm_out_off, BF16, [KC, ntok]) if final_out is None else None
        c_scale = 0.5 / ALPHA
        eps_eff = LN_EPS / (ALPHA * ALPHA)
        nb = len(tbs)

        for g in range(FC // 2):
            wb = wgu[g % 2]
            kb = (tag, "wgu", g % 2)
            S.dma("pool", lambda e, wb=wb, g=g: e.dma_start(out=wb[:, 0], in_=wg[g]), writes=[(kb, 0)])
            S.dma("pool", lambda e, wb=wb, g=g: e.dma_start(out=wb[:, 1], in_=wu[g]), writes=[(kb, 1)])
            for fcl in range(2):
                fc = 2 * g + fcl
                for ti, (t0, t1e) in enumerate(tbs):
                    n = t1e - t0
                    pb = (fc * nb + ti) % 2
                    pg, pu = ps[:, 2 * pb, 0:n], ps[:, 2 * pb + 1, 0:n]
                    for k in range(KC):
                        S.op("pe", lambda e, pg=pg, wb=wb, k=k, fcl=fcl, t0=t0, t1e=t1e: e.matmul(
                            pg, lhsT=wb[:, 0, k, fcl * 128:(fcl + 1) * 128], rhs=strm_in[:, k, t0:t1e],
                            start=(k == 0), stop=(k == KC - 1)),
                            reads=[(kb, 0), (tag, "sin")], writes=[("ps", 2 * pb)])
                    for k in range(KC):
                        S.op("pe", lambda e, pu=pu, wb=wb, k=k, fcl=fcl, t0=t0, t1e=t1e: e.matmul(
                            pu, lhsT=wb[:, 1, k, fcl * 128:(fcl + 1) * 128], rhs=strm_in[:, k, t0:t1e],
                            start=(k == 0), stop=(k == KC - 1)),
                            reads=[(kb, 1), (tag, "sin")], writes=[("ps", 2 * pb + 1)])
                    sb = sil[pb]
                    S.op("act", lambda e, sb=sb, pg=pg, n=n: e.activation(out=sb[:, 0:n], in_=pg, func=AF.Silu),
                         reads=[("ps", 2 * pb)], writes=[(tag, "sil", pb)])
                    S.op("dve", lambda e, sb=sb, pu=pu, n=n, fc=fc, t0=t0, t1e=t1e: e.tensor_tensor(
                        out=hT[:, fc, t0:t1e], in0=sb[:, 0:n], in1=pu, op=ALU.mult),
                        reads=[(tag, "sil", pb), ("ps", 2 * pb + 1)], writes=[(tag, "hT", fc, ti)])

        S.barrier()
        for dc in range(KC):
            wb = wdb[dc % 2]
            kb = (tag, "wd", dc % 2)
            S.dma("pool", lambda e, wb=wb, dc=dc: e.dma_start(out=wb, in_=wd[dc]), writes=[kb])
            S.dma("sp", lambda e, dc=dc: e.dma_start(out=z[:, dc, :], in_=resid[dc * 128:(dc + 1) * 128, 0:ntok]),
                  writes=[(tag, "z", dc, ti) for ti in range(nb)])
            for ti, (t0, t1e) in enumerate(tbs):
                n = t1e - t0
                pb = 4 + (dc * nb + ti) % 2
                py = ps[:, pb, 0:n]
                for f in range(FC):
                    S.op("pe", lambda e, py=py, wb=wb, f=f, t0=t0, t1e=t1e: e.matmul(
                        py, lhsT=wb[:, f, :], rhs=hT[:, f, t0:t1e], start=(f == 0), stop=(f == FC - 1)),
                        reads=[kb, (tag, "hT", f, ti)], writes=[("ps", pb)])
                S.op("dve", lambda e, py=py, dc=dc, t0=t0, t1e=t1e: e.scalar_tensor_tensor(
                    out=z[:, dc, t0:t1e], in0=py, scalar=c_scale, in1=z[:, dc, t0:t1e],
                    op0=ALU.mult, op1=ALU.add),
                    reads=[("ps", pb), (tag, "z", dc, ti)], writes=[(tag, "z", dc, ti)])
        self.ln_apply(tag, z, tbs, cg, cb, T_OFF, eps_eff, strm_out=strm_out, alias_key="hT",
                      resid_out=resid_out, final_out=final_out)
        S.barrier()

    def proj_fm(self, tag, strm, gi, wbufs, tbs, evac, banks=(0, 1), parity=[0]):
        S, ps = self.S, self.ps
        wb = wbufs[parity[0] % 2]
        kb = ("wb", parity[0] % 2)
        parity[0] += 1
        S.dma("pool", lambda e, wb=wb, gi=gi: e.dma_start(out=wb, in_=self.win[gi]), writes=[kb])
        cnt = 0
        for half in range(2):
            for ti, (t0, t1e) in enumerate(tbs):
                n = t1e - t0
                bk = banks[cnt % len(banks)]
                cnt += 1
                pv = ps[:, bk, 0:n]
                for k in range(KC):
                    S.op("pe", lambda e, pv=pv, wb=wb, k=k, half=half, t0=t0, t1e=t1e: e.matmul(
                        pv, lhsT=wb[:, k, half * 128:(half + 1) * 128], rhs=strm[:, k, t0:t1e],
                        start=(k == 0), stop=(k == KC - 1)),
                        reads=[kb, "strm"], writes=[("ps", bk)])
                evac(half, ti, t0, t1e, pv, bk)

    def proj_tm(self, tag, strm, gi, wbufs, tls, ncols, evac, banks=(0, 1), parity=[0]):
        S, ps = self.S, self.ps
        wb = wbufs[parity[0] % 2]
        kb = ("wb", parity[0] % 2)
        parity[0] += 1
        S.dma("pool", lambda e, wb=wb, gi=gi: e.dma_start(out=wb, in_=self.win[gi]), writes=[kb])
        for cnt, (i, c0, n) in enumerate(tls):
            bk = banks[cnt % len(banks)]
            pv = ps[0:n, bk, 0:ncols]
            for k in range(KC):
                S.op("pe", lambda e, pv=pv, wb=wb, k=k, c0=c0, n=n: e.matmul(
                    pv, lhsT=strm[:, k, c0:c0 + n], rhs=wb[:, k, 0:ncols],
                    start=(k == 0), stop=(k == KC - 1)),
                    reads=[kb, "strm"], writes=[("ps", bk)])
            evac(i, c0, n, pv, bk)

    def hgrn_m1(self, strm):
        S, ps, nc = self.S, self.ps, self.nc
        R1 = 99840
        wbufs = [self.view(R1 + i * 8192, BF16, [KC, 256]) for i in range(2)]
        off = [R1 + 16384]

        def alloc(dt, shape):
            n = 1
            for x in shape:
                n *= x
            nb = n * (4 if dt == F32 else 2)
            nb = (nb + 63) // 64 * 64
            v = self.view(off[0], dt, shape)
            off[0] += nb
            return v
        logf = alloc(F32, [2, NT]); Bg = alloc(F32, [2, NT]); Bsh = alloc(F32, [2, NT])
        tA = alloc(F32, [2, NT]); tB = alloc(F32, [2, NT])
        kk = alloc(BF16, [2, NT]); qs = alloc(BF16, [2, NT]); qt = alloc(BF16, [2, NT])
        kt = alloc(BF16, [2, NT]); kh64 = alloc(BF16, [2, NT]); kh128 = alloc(BF16, [2, NT])
        vv = alloc(BF16, [9, 256])
        PTs = [alloc(BF16, [2, 128]) for _ in range(2)]; khTs = [alloc(BF16, [2, 128]) for _ in range(2)]
        xs_st = alloc(BF16, [9, 256]); xa_st = alloc(F32, [9, 2])
        Qc = self.view(0, BF16, [8, NREAL]); oloc = self.view(16384, BF16, [8, NREAL])
        cv = self.cv
        lbc, omlc = self.lbc, self.omlc
        psb = lambda bk: ps[:, bk, :].bitcast(BF16)
        TL = [(i, 128 * i, 128) for i in range(8)] + [(8, NREAL, NMETA)]
        flat = lambda v: v.rearrange("p h t -> p (h t)")
        r64 = lambda v: v[:, :, 0:NREAL].rearrange("p h (c t) -> p h c t", t=64)
        r128 = lambda v: v[:, :, 0:NREAL].rearrange("p h (c t) -> p h c t", t=128)
        mt = lambda v: v[:, :, NREAL:NT]

        for hp in range(4):
            T = ("m1", hp)
            def ev_f(half, ti, t0, t1e, pv, bk, hp=hp):
                h = 2 * hp + half
                S.op("act", lambda e: e.activation(out=tA[:, half, t0:t1e], in_=pv, func=AF.Sigmoid),
                     reads=[("ps", bk)], writes=[("tA", half, ti)])
                S.op("dve", lambda e: e.tensor_scalar(out=tA[:, half, t0:t1e], in0=tA[:, half, t0:t1e],
                                                      scalar1=omlc[:, h:h + 1], scalar2=lbc[:, h:h + 1],
                                                      op0=ALU.mult, op1=ALU.add),
                     reads=[("tA", half, ti), "lb"], writes=[("tA", half, ti)])
                S.op("act", lambda e: e.activation(out=logf[:, half, t0:t1e], in_=tA[:, half, t0:t1e], func=AF.Ln),
                     reads=[("tA", half, ti)], writes=[("logf", half, ti)])
                S.op("dve", lambda e: e.tensor_scalar(out=kk[:, half, t0:t1e], in0=tA[:, half, t0:t1e],
                                                      scalar1=-1.0, scalar2=1.0, op0=ALU.mult, op1=ALU.add),
                     reads=[("tA", half, ti)], writes=[("kk", half, ti)])
            self.proj_fm(T, strm, self.gidx["hf%d" % hp], wbufs, TBS, ev_f)

            def ev_q(half, ti, t0, t1e, pv, bk):
                S.op("act", lambda e: e.activation(out=qs[:, half, t0:t1e], in_=pv, func=AF.Silu),
                     reads=[("ps", bk)], writes=[("qs", half, ti)])
            self.proj_fm(T, strm, self.gidx["hq%d" % hp], wbufs, TBS, ev_q)

            def ev_v(i, c0, n, pv, bk):
                S.op("act", lambda e: e.activation(out=vv[0:n, i, :], in_=pv, func=AF.Copy),
                     reads=[("ps", bk)], writes=[("vv", i)])
            self.proj_tm(T, strm, self.gidx["hi%d" % hp], wbufs, TL, 256, ev_v)

            allk = lambda nm: [(nm, hl, ti) for hl in range(2) for ti in range(3)]
            S.op("dve", lambda e: e.tensor_tensor_scan(out=flat(Bg), data0=flat(logf), data1=flat(logf),
                                                       initial=0.0, op0=ALU.add, op1=ALU.min),
                 reads=allk("logf"), writes=["Bg"])
            S.op("dve", lambda e: e.memset(flat(Bsh)[:, 0:1], 0.0), writes=["Bsh0"])
            S.op("act", lambda e: e.activation(out=flat(Bsh)[:, 1:2 * NT], in_=flat(Bg)[:, 0:2 * NT - 1], func=AF.Copy),
                 reads=["Bg"], writes=["Bsh"])
            S.op("dve", lambda e: e.tensor_tensor(out=r64(tB), in0=r64(Bg),
                                                  in1=r64(Bsh)[:, :, :, 0:1].to_broadcast([128, 2, 16, 64]),
                                                  op=ALU.subtract),
                 reads=["Bg", "Bsh", "Bsh0"], writes=["tBr"])
            S.op("dve", lambda e: e.tensor_tensor(out=mt(tB), in0=mt(Bg),
                                                  in1=mt(Bsh)[:, :, 0:1].to_broadcast([128, 2, NMETA]),
                                                  op=ALU.subtract),
                 reads=["Bg", "Bsh", "Bsh0"], writes=["tBm"])
            S.op("dve", lambda e: e.tensor_tensor(out=r128(tA), in0=r128(Bg),
                                                  in1=r128(Bsh)[:, :, :, 0:1].to_broadcast([128, 2, 8, 128]),
                                                  op=ALU.subtract),
                 reads=["Bg", "Bsh", "Bsh0"] + allk("tA"), writes=["tAr"] + allk("tA"))
            S.op("dve", lambda e: e.tensor_copy(out=mt(tA), in_=mt(tB)),
                 reads=["tBm"], writes=["tAm"])
            TBk, TAk = ["tBr", "tBm"], ["tAr", "tAm"] + allk("tA")
            S.op("act", lambda e: e.activation(out=flat(Bg), in_=flat(tB), func=AF.Exp),
                 reads=TBk + ["Bsh", "tAr"], writes=["Bg"])
            S.op("dve", lambda e: e.tensor_tensor(out=flat(qt), in0=flat(qs), in1=flat(Bg), op=ALU.mult),
                 reads=["Bg"] + allk("qs"), writes=["qt"])
            S.op("act", lambda e: e.activation(out=flat(Bsh), in_=flat(tB), func=AF.Exp, scale=-1.0),
                 reads=TBk + ["Bsh", "tAr", "Bsh0"], writes=["Bsh", "Bsh0"])
            S.op("dve", lambda e: e.tensor_tensor(out=flat(kt), in0=flat(kk), in1=flat(Bsh), op=ALU.mult),
                 reads=["Bsh"] + allk("kk"), writes=["kt"])
            S.op("dve", lambda e: e.tensor_tensor(out=r64(Bg), in0=r64(tB),
                                                  in1=r64(tB)[:, :, :, 63:64].to_broadcast([128, 2, 16, 64]),
                                                  op=ALU.subtract),
                 reads=TBk + ["qt"], writes=["Bg"])
            S.op("act", lambda e: e.activation(out=r64(Bg), in_=r64(Bg), func=AF.Exp, scale=-1.0),
                 reads=["Bg"], writes=["Bg"])
            S.op("dve", lambda e: e.tensor_tensor(out=r64(kh64), in0=r64(kk), in1=r64(Bg), op=ALU.mult),
                 reads=["Bg"] + allk("kk"), writes=["kh64"])
            S.op("act", lambda e: e.activation(out=flat(Bsh), in_=flat(tA), func=AF.Exp),
                 reads=TAk + ["kt"], writes=["Bsh"])
            S.op("dve", lambda e, hp=hp: e.tensor_tensor(out=Qc[:, 2 * hp:2 * hp + 2, :], in0=qs[:, :, 0:NREAL],
                                                         in1=Bsh[:, :, 0:NREAL], op=ALU.mult),
                 reads=["Bsh"] + allk("qs"), writes=[("Qc", hp)])
            S.op("dve", lambda e: e.tensor_copy(out=xa_st[:, 0:8, :].rearrange("p i h -> p h i"),
                                                in_=r128(Bsh)[:, :, :, 127]),
                 reads=["Bsh"], writes=["xa_st"])
            S.op("dve", lambda e: e.tensor_copy(out=xa_st[:, 8, :], in_=Bsh[:, :, NT - 1]),
                 reads=["Bsh"], writes=["xa_st"])
            S.op("dve", lambda e: e.tensor_tensor(out=r128(Bg), in0=r128(tA),
                                                  in1=r128(tA)[:, :, :, 127:128].to_broadcast([128, 2, 8, 128]),
                                                  op=ALU.subtract),
                 reads=TAk + ["kh64"], writes=["Bg"])
            S.op("dve", lambda e: e.tensor_tensor(out=mt(Bg), in0=mt(tA),
                                                  in1=mt(tA)[:, :, NMETA - 1:NMETA].to_broadcast([128, 2, NMETA]),
                                                  op=ALU.subtract),
                 reads=TAk + ["kh64"], writes=["Bg"])
            S.op("act", lambda e: e.activation(out=flat(Bg), in_=flat(Bg), func=AF.Exp, scale=-1.0),
                 reads=["Bg"], writes=["Bg"])
            S.op("dve", lambda e: e.tensor_tensor(out=flat(kh128), in0=flat(kk), in1=flat(Bg), op=ALU.mult),
                 reads=["Bg"] + allk("kk"), writes=["kh128"])

            def tok_block(i, c0, n, hp=hp):
                par = i % 2
                PT, khT = PTs[par], khTs[par]
                bS, bO = (2, 3) if par == 0 else (6, 7)
                t0c, s0c = par * 256, par * 256
                if n == 128:
                    for hl in range(2):
                        o0 = hl * 128
                        S.op("pe", lambda e, hl=hl, o0=o0, c0=c0: e.matmul(
                            ps[:, bS, o0:o0 + 64], lhsT=kt[:, hl, c0:c0 + 128], rhs=qt[:, hl, c0:c0 + 64],
                            start=True, stop=True), reads=["kt", "qt"], writes=[("ps", bS)])
                        S.op("pe", lambda e, hl=hl, o0=o0, c0=c0: e.matmul(
                            ps[0:64, bS, o0 + 64:o0 + 128], lhsT=kh64[:, hl, c0:c0 + 64],
                            rhs=qt[:, hl, c0 + 64:c0 + 128], start=True, stop=True),
                            reads=["kh64", "qt"], writes=[("ps", bS)])
                        S.op("pe", lambda e, hl=hl, o0=o0, c0=c0: e.matmul(
                            ps[64:128, bS, o0 + 64:o0 + 128], lhsT=kt[:, hl, c0 + 64:c0 + 128],
                            rhs=qt[:, hl, c0 + 64:c0 + 128], start=True, stop=True),
                            reads=["kt", "qt"], writes=[("ps", bS)])
                    for hl in range(2):
                        S.op("dve", lambda e, hl=hl: e.tensor_tensor(
                            out=PT[:, hl, :], in0=ps[:, bS, hl * 128:(hl + 1) * 128], in1=self.mask2, op=ALU.mult),
                            reads=[("ps", bS), "mask2"], writes=[("PT", par, hl)])
                    for hl in range(2):
                        S.op("pe", lambda e, hl=hl, i=i: e.matmul(
                            ps[:, bO, hl * 128:(hl + 1) * 128], lhsT=vv[:, i, hl * 128:(hl + 1) * 128],
                            rhs=PT[:, hl, :], start=True, stop=True),
                            reads=[("vv", i), ("PT", par, hl)], writes=[("ps", bO)])
                    S.op("act", lambda e, hp=hp, c0=c0: e.activation(
                        out=oloc[:, 2 * hp:2 * hp + 2, c0:c0 + 128],
                        in_=ps[:, bO, 0:256].rearrange("p (h t) -> p h t", h=2), func=AF.Copy),
                        reads=[("ps", bO)], writes=[("oloc", hp, i)])
                for hl in range(2):
                    S.op("pe", lambda e, hl=hl, c0=c0, n=n: e.transpose(
                        out=psb(4)[0:n, t0c * 2 + hl * 128:t0c * 2 + (hl + 1) * 128], in_=kh128[:, hl, c0:c0 + n],
                        identity=self.ident_bf),
                        reads=["kh128", "ident"], writes=[("ps4", par)])
                S.op("dve", lambda e, n=n: e.tensor_copy(out=khT[0:n].rearrange("p h d -> p (h d)"),
                                                         in_=psb(4)[0:n, t0c * 2:t0c * 2 + 256]),
                     reads=[("ps4", par)], writes=[("khT", par)])
                for hl in range(2):
                    S.op("pe", lambda e, hl=hl, i=i, n=n: e.matmul(
                        ps[:, 5, s0c + hl * 128:s0c + (hl + 1) * 128], lhsT=khT[0:n, hl, :],
                        rhs=vv[0:n, i, hl * 128:(hl + 1) * 128], start=True, stop=True),
                        reads=[("khT", par), ("vv", i)], writes=[("ps5", par)])
                S.op("dve", lambda e, i=i: e.tensor_copy(out=xs_st[:, i, :], in_=ps[:, 5, s0c:s0c + 256]),
                     reads=[("ps5", par)], writes=[("xs_st", i)])
            for (i_, c0_, n_) in TL:
                tok_block(i_, c0_, n_)
            for q3 in range(3):
                S.dma("sp", lambda e, hp=hp, q3=q3: e.dma_start(
                    out=self.xs[q3].rearrange("p (i c) -> p i c", i=3)[:, :, hp * 256:(hp + 1) * 256],
                    in_=xs_st[:, 3 * q3:3 * q3 + 3, :]),
                    reads=[("xs_st", i) for i in range(9)], writes=[("xs", hp, q3)])
            S.dma("sp", lambda e, hp=hp: e.dma_start(
                out=self.xa.rearrange("p (i c) -> p i c", i=9)[:, :, 2 * hp:2 * hp + 2], in_=xa_st),
                reads=["xa_st"], writes=[("xa", hp)])
        S.barrier()
        rg = [[0, 1, 2, 3], [4, 5, 6, 7]]
        for q3 in range(3):
            S.coll(lambda e, q3=q3: e.collective_compute("AllGather", ALU.bypass, replica_groups=rg,
                                                         ins=[self.xs[q3]], outs=[self.xg[q3]]), writes=[("xg", q3)])
        S.coll(lambda e: e.collective_compute("AllGather", ALU.bypass, replica_groups=rg,
                                              ins=[self.xa], outs=[self.xag]), writes=["xag"])

    def hgrn_m2(self, strm):
        S, ps, nc = self.S, self.ps, self.nc
        R1 = 99840
        wbufs = [self.view(R1 + i * 8192, BF16, [KC, 256]) for i in range(2)]
        off = [R1 + 16384]

        def alloc(dt, shape):
            n = 1
            for x in shape:
                n *= x
            nb = n * (4 if dt == F32 else 2)
            nb = (nb + 63) // 64 * 64
            v = self.view(off[0], dt, shape)
            off[0] += nb
            return v
        sgate = alloc(BF16, [8, NREAL])
        Scur = alloc(F32, [8, 128]); SmF = alloc(F32, [8, 128])
        SAb = [alloc(BF16, [8, 128]) for _ in range(3)]
        Aall = alloc(F32, [4, 72])
        OF = alloc(F32, [8, 128]); OSQ = alloc(F32, [8, 128]); RS = alloc(F32, [8, 128])
        Qc = self.view(0, BF16, [8, NREAL]); oloc = self.view(16384, BF16, [8, NREAL])
        yhg = self.view(32768, BF16, [8, NREAL]); Smine = self.view(49152, BF16, [8, 8, 128])
        f2 = lambda v: v.rearrange("p h t -> p (h t)")
        RTB = TBS[0:2]

        for g4 in range(4):
            def ev_g(half, ti, t0, t1e, pv, bk, g4=g4):
                h = 2 * g4 + half
                S.op("act", lambda e: e.activation(out=sgate[:, h, t0:t1e], in_=pv, func=AF.Silu),
                     reads=[("ps", bk)], writes=[("sgate", h, ti)])
                S.op("dve", lambda e: e.tensor_scalar(out=sgate[:, h, t0:t1e], in0=sgate[:, h, t0:t1e],
                                                      scalar1=self.gnc[:, h:h + 1], scalar2=None, op0=ALU.mult),
                     reads=[("sgate", h, ti), "gn"], writes=[("sgate", h, ti)])
            self.proj_fm("m2", strm, self.gidx["hg%d" % g4], wbufs, RTB, ev_g)

        def out_block(i):
            c0 = 128 * i
            for h in range(8):
                bk = 2 + h // 4
                S.op("pe", lambda e, h=h, i=i, c0=c0, bk=bk: e.matmul(
                    ps[:, bk, (h % 4) * 128:(h % 4 + 1) * 128], lhsT=Smine[:, i, h, :], rhs=Qc[:, h, c0:c0 + 128],
                    start=True, stop=True),
                    reads=[("Smine", i), "Qc"], writes=[("ps", bk)])
            S.op("dve", lambda e, c0=c0: e.tensor_tensor(
                out=OF, in0=ps[:, 2:4, :].rearrange("p a (h t) -> p (a h) t", h=4), in1=oloc[:, :, c0:c0 + 128],
                op=ALU.add),
                reads=[("ps", 2), ("ps", 3), "oloc"], writes=["OF"])
            S.op("act", lambda e: e.activation(out=f2(OSQ), in_=f2(OF), func=AF.Square),
                 reads=["OF"], writes=["OSQ"])
            for a in range(2):
                S.op("pe", lambda e, a=a: e.matmul(ps[:, 4 + a, :], lhsT=self.ones128, rhs=f2(OSQ)[:, a * 512:(a + 1) * 512],
                                                   start=True, stop=True),
                     reads=["OSQ", "ones128"], writes=[("ps", 4 + a)])
            S.op("act", lambda e: e.activation(out=f2(RS), in_=ps[:, 4:6, :].rearrange("p a b -> p (a b)"),
                                               func=AF.Ln, bias=self.eps_rms, scale=1.0),
                 reads=[("ps", 4), ("ps", 5), "eps"], writes=["RS"])
            S.op("act", lambda e: e.activation(out=f2(RS), in_=f2(RS), func=AF.Exp, scale=-0.5),
                 reads=["RS"], writes=["RS"])

        def out_block_b(i):
            c0 = 128 * i
            S.op("dve", lambda e: e.tensor_tensor(out=f2(OF), in0=f2(OF), in1=f2(RS), op=ALU.mult),
                 reads=["OF", "RS"], writes=["OF"])
            S.op("dve", lambda e, c0=c0: e.tensor_tensor(out=yhg[:, :, c0:c0 + 128], in0=OF, in1=sgate[:, :, c0:c0 + 128],
                                                         op=ALU.mult),
                 reads=["OF"] + [("sgate", h, c0 // 512) for h in range(8)], writes=[("yhg", i)])

        S.dma("sp", lambda e: e.dma_start(out=Aall, in_=self.xag.rearrange("(r p) c -> p r c", p=128)),
              reads=["xag"], writes=["Aall"])
        xg3 = [x_.rearrange("(r p) (i c) -> r p i c", p=128, i=3) for x_ in self.xg]
        S.dma("sp", lambda e: e.dma_start(out=f2(SAb[2]), in_=xg3[2][0, :, 2, :]), reads=[("xg", 2)],
              writes=[("SAb", 2)])
        S.op("dve", lambda e: e.tensor_copy(out=f2(Scur), in_=f2(SAb[2])), reads=[("SAb", 2)], writes=["Scur"])
        for g in range(32):
            r, i = g % 4, g // 4
            sb = SAb[g % 3]
            S.dma("sp", lambda e, sb=sb, r=r, i=i: e.dma_start(out=f2(sb), in_=xg3[i // 3][r, :, i % 3, :]),
                  reads=[("xg", i // 3)], writes=[("SAb", g % 3)])
            if r == 0:
                S.op("dve", lambda e: e.tensor_scalar(out=f2(SmF), in0=f2(Scur), scalar1=self.selc[:, 0:1],
                                                      scalar2=None, op0=ALU.mult),
                     reads=["Scur", "sel"], writes=["SmF"])
            else:
                dst = SmF if r < 3 else Smine[:, i]
                S.op("dve", lambda e, r=r, dst=dst: e.scalar_tensor_tensor(
                    out=f2(dst), in0=f2(Scur), scalar=self.selc[:, r:r + 1], in1=f2(SmF),
                    op0=ALU.mult, op1=ALU.add),
                    reads=["Scur", "sel", "SmF"], writes=(["SmF"] if r < 3 else [("Smine", i)]))
            if g < 31:
                for h in range(8):
                    S.op("dve", lambda e, h=h, r=r, i=i, sb=sb: e.scalar_tensor_tensor(
                        out=Scur[:, h, :], in0=Scur[:, h, :], scalar=Aall[:, r, i * 8 + h:i * 8 + h + 1],
                        in1=sb[:, h, :], op0=ALU.mult, op1=ALU.add),
                        reads=["Scur", "Aall", ("SAb", g % 3)], writes=["Scur"])
            if g % 4 == 3:
                out_block(g // 4)
            if g % 4 == 1 and g >= 5:
                out_block_b((g - 5) // 4)
        out_block_b(7)

        S.barrier()

    def attn_m3(self, strm):
        S, ps, nc = self.S, self.ps, self.nc
        R1 = 99840
        psb = lambda bk: ps[:, bk, :].bitcast(BF16)
        K_all = self.view(R1, BF16, [2, 4112])
        V_all = self.view(R1 + 16448, BF16, [33, 258])
        IK_all = self.view(R1 + 33536, BF16, [4096])
        AugK = self.view(R1 + 41728, BF16, [4112])
        qT = self.view(R1 + 49952, BF16, [8, NREAL])
        iqT = self.view(R1 + 66336, BF16, [8, NREAL])
        sc = self.view(R1 + 82720, F32, [4096])
        wbufs = [self.view(R1 + 82720 + i * 8192, BF16, [KC, 256]) for i in range(2)]
        Dg = self.view(R1 + 99104, BF16, [16, 128])
        yatt = self.view(0, BF16, [8, NREAL])
        mb = self.view(16384, BF16, [4096])
        mbT = self.view(24576, BF16, [32, 128])
        junk = self.view(49152, U8, [4096])
        iqz = self.view(49152 + 4096, BF16, [16, 128])
        rh = [self.view(57344 + q * 1024, BF16, [512]) for q in range(4)]
        ya = self.view(61440, BF16, [8, 128])
        PTb = [self.view(61440 + q * 2048, BF16, [1024]) for q in range(2)]
        cbt = self.view(65536, BF16, [4, 128])
        kst = self.view(49152, BF16, [2, NREAL])
        vst = self.view(49152 + 4096, BF16, [8, 258])
        ikst = self.view(49152 + 4096 + 4160, BF16, [NREAL])
        iktmp = self.view(49152 + 10304, F32, [64])
        ikn2 = self.view(49152 + 10304 + 256, BF16, [128])
        cst = self.cst
        AugQ = cst[:, 664:1176].bitcast(BF16)
        AugR = cst[:, 1176:1688].bitcast(BF16)
        wq = cst[:, 1688:1816].rearrange("p (i h) -> p i h", h=16)
        H = cst[:, 1816:1848]
        Pt = cst[:, 1848:1976]
        g1 = cst[:, 1976:2008]
        mrow = cst[:, 2008:2016]; cc = cst[:, 2016:2024]; rs = cst[:, 2024:2032]
        Bt = cst[:, 2032:2033]; Wc = cst[:, 2033:2034]; mid = cst[:, 2034:2035]; cnt = cst[:, 2035:2036]
        u2 = cst[:, 2036:2037]; tau = cst[:, 2037:2038]; rstd1 = cst[:, 2038:2039]
        nslope = cst[:, 2040:2048]
        Qt = cst[:, 2048:2080].rearrange("p (r j) -> p r j", r=4)
        kmx = cst[:, 2080:2081]
        pw = cst[:, 2104:2136]
        gik = cst[:, 2136:2200]; bik = cst[:, 2200:2264]
        st6 = cst[:, 2264:2270]; mv = cst[:, 2270:2272]
        TL = [(i, 128 * i, 128) for i in range(8)] + [(8, NREAL, NMETA)]
        RTB = TBS[0:2]
        NB = 16

        S.dma("sp", lambda e: e.dma_start(out=cst[:, 2040:2264], in_=self.catt), writes=["catt"])
        S.dma("sp", lambda e: e.dma_start(out=Pt, in_=self.cmat_d[:, 384:512]), writes=["Pt"])
        S.op("pool", lambda e: e.memset(AugK[0:65, :], 0.0), writes=["AugK"])
        S.op("pool", lambda e: e.memset(AugQ[0:65, :], 0.0), writes=["AugQ"])
        S.op("pool", lambda e: e.memset(AugR[0:65, :], 0.0), writes=["AugR"])
        for rr in range(3):
            S.dma("pool", lambda e, rr=rr: e.dma_start(out=AugK[32 * rr:32 * rr + 1, :], in_=self.augk[rr:rr + 1, :]),
                  writes=["AugK"])
        for rr in range(2):
            S.dma("pool", lambda e, rr=rr: e.dma_start(out=AugQ[32 * rr:32 * rr + 1, :], in_=self.augs[rr:rr + 1, :]),
                  writes=["AugQ"])
            S.dma("pool", lambda e, rr=rr: e.dma_start(out=AugR[32 * rr:32 * rr + 1, :], in_=self.augs[rr:rr + 1, :]),
                  writes=["AugR"])
        S.dma("pool", lambda e: e.dma_start(out=cbt, in_=self.cbt_d.rearrange("p (r s) -> p r s", r=4)), writes=["cbt"])
        S.op("dve", lambda e: e.memset(vst.rearrange("p i (k c) -> p i k c", k=2)[:, :, :, 128:129], 1.0),
             writes=["vst1"])
        S.op("dve", lambda e: e.memset(V_all[:, 32, :].rearrange("p (k c) -> p k c", k=2)[:, :, 128:129], 1.0),
             writes=["V1"])

        def ev_k(half, ti, t0, t1e, pv, bk):
            if ti < 2:
                S.op("act", lambda e: e.activation(out=kst[:, half, t0:t1e], in_=pv, func=AF.Copy),
                     reads=[("ps", bk)], writes=[("kst", half, ti)])
            else:
                S.op("act", lambda e: e.activation(out=K_all[:, half, 4096:4112], in_=pv, func=AF.Copy),
                     reads=[("ps", bk)], writes=[("Kmeta", half)])
        self.proj_fm("m3", strm, self.gidx["ak"], wbufs, TBS, ev_k)

        def ev_v(i, c0, n, pv, bk):
            src = pv.rearrange("p (k c) -> p k c", k=2)
            if i < 8:
                dst = vst[:, i, :].rearrange("p (k c) -> p k c", k=2)[:, :, 0:128]
                S.op("act", lambda e: e.activation(out=dst, in_=src, func=AF.Copy),
                     reads=[("ps", bk), "vst1"], writes=[("vst", i)])
            else:
                dst = V_all[0:n, 32, :].rearrange("p (k c) -> p k c", k=2)[:, :, 0:128]
                S.op("act", lambda e: e.activation(out=dst, in_=src, func=AF.Copy),
                     reads=[("ps", bk), "V1"], writes=["Vmeta"])
        self.proj_tm("m3", strm, self.gidx["av"], wbufs, TL, 256, ev_v)

        def ev_ik(i, c0, n, pv, bk):
            S.op("dve", lambda e: e.bn_stats(out=st6, in_=pv[:, 0:64]), reads=[("ps", bk)], writes=["st6"])
            S.op("dve", lambda e: e.bn_aggr(out=mv, in_=st6), reads=["st6"], writes=["mv"])
            S.op("act", lambda e: e.activation(out=rstd1, in_=mv[:, 1:2], func=AF.Sqrt, bias=self.eps_ik, scale=1.0),
                 reads=["mv", "eps"], writes=["rstd1"])
            S.op("dve", lambda e: e.reciprocal(out=rstd1, in_=rstd1), reads=["rstd1"], writes=["rstd1"])
            S.op("dve", lambda e: e.tensor_scalar(out=iktmp, in0=pv[:, 0:64], scalar1=mv[:, 0:1], scalar2=rstd1,
                                                  op0=ALU.subtract, op1=ALU.mult),
                 reads=[("ps", bk), "mv", "rstd1"], writes=["iktmp"])
            S.op("dve", lambda e: e.tensor_tensor(out=iktmp, in0=iktmp, in1=gik, op=ALU.mult),
                 reads=["iktmp", "catt"], writes=["iktmp"])
            S.op("dve", lambda e: e.tensor_tensor(out=ikn2[:, 0:64], in0=iktmp, in1=bik, op=ALU.add),
                 reads=["iktmp", "catt"], writes=["ikn2a"])
            S.op("dve", lambda e: e.tensor_copy(out=ikn2[:, 64:128], in_=ikn2[:, 0:64]),
                 reads=["ikn2a"], writes=["ikn2b"])
            S.op("act", lambda e, i=i: e.activation(out=wq[:, i, :], in_=pv[:, 64:80], func=AF.Copy,
                                                    scale=0.25 * 0.125),
                 reads=[("ps", bk)], writes=[("wq", i)])
            S.op("pe", lambda e: e.transpose(out=psb(2)[:, 0:128], in_=ikn2, identity=self.ident_bf),
                 reads=["ikn2a", "ikn2b", "ident"], writes=[("ps", 2)])
            S.op("act", lambda e, c0=c0: e.activation(out=ikst[:, c0:c0 + 128], in_=psb(2)[:, 0:128], func=AF.Copy),
                 reads=[("ps", 2)], writes=[("ikst", i)])
        self.proj_tm("m3", strm, self.gidx["ikw"], wbufs, TL[0:8], 80, ev_ik)

        S.dma("sp", lambda e: e.dma_start(out=self.ks.rearrange("p (k t) -> p k t", k=2), in_=kst),
              reads=[("kst", hh, ti) for hh in range(2) for ti in range(2)], writes=["ks"])
        S.dma("sp", lambda e: e.dma_start(out=self.vs[:, 0:2064].rearrange("p (i c) -> p i c", i=8), in_=vst),
              reads=[("vst", i) for i in range(8)] + ["vst1"], writes=["vs"])
        S.dma("sp", lambda e: e.dma_start(out=self.vs[:, 2064:3088], in_=ikst),
              reads=[("ikst", i) for i in range(8)], writes=["vs2"])
        S.barrier()
        rg = [[0, 1, 2, 3], [4, 5, 6, 7]]
        S.coll(lambda e: e.collective_compute("AllGather", ALU.bypass, replica_groups=rg,
                                              ins=[self.ks], outs=[self.kg]), writes=["kg"])
        S.coll(lambda e: e.collective_compute("AllGather", ALU.bypass, replica_groups=rg,
                                              ins=[self.vs], outs=[self.vg]), writes=["vg"])

        for g4 in range(4):
            def ev_q(half, ti, t0, t1e, pv, bk, g4=g4):
                h = 2 * g4 + half
                S.op("act", lambda e: e.activation(out=qT[:, h, t0:t1e], in_=pv, func=AF.Copy, scale=128.0 ** -0.5),
                     reads=[("ps", bk)], writes=[("qT", h, ti)])
            self.proj_fm("m3", strm, self.gidx["aq%d" % g4], wbufs, RTB, ev_q)
        for g4 in range(4):
            def ev_iq(half, ti, t0, t1e, pv, bk, g4=g4):
                h = 2 * g4 + half
                S.op("dve", lambda e: e.tensor_copy(out=iqT[:, h, t0:t1e], in_=pv),
                     reads=[("ps", bk)], writes=[("iqT", h, ti)])
            self.proj_fm("m3", strm, self.gidx["iq%d" % g4], wbufs, RTB, ev_iq)

        for r in range(4):
            S.dma("sp", lambda e, r=r: e.dma_start(
                out=K_all[:, :, r * 1024:(r + 1) * 1024],
                in_=self.kg[r * 128:(r + 1) * 128, :].rearrange("p (k t) -> p k t", k=2)),
                reads=["kg"], writes=["K_all"])
            S.dma("sp", lambda e, r=r: e.dma_start(
                out=V_all[:, r * 8:(r + 1) * 8, :],
                in_=self.vg[r * 128:(r + 1) * 128, 0:2064].rearrange("p (i c) -> p i c", i=8)),
                reads=["vg"], writes=["V_all"])
            S.dma("sp", lambda e, r=r: e.dma_start(
                out=IK_all[:, r * 1024:(r + 1) * 1024], in_=self.vg[r * 128:(r + 1) * 128, 2064:3088]),
                reads=["vg"], writes=["IK_all"])
        S.barrier()

        S.op("pool", lambda e: e.memset(iqz.rearrange("p h t -> p (h t)"), 0.0), writes=["iqz"])
        sc4 = sc.rearrange("p (r c) -> p r c", r=4)
        mb4 = mb.rearrange("p (r c) -> p r c", r=4)
        jk4 = junk.rearrange("p (r c) -> p r c", r=4)
        def geom(i):
            q0 = 128 * i
            nk = 128 * (i + 1)
            pieces = [(r, c0, min(512, nk - c0)) for r in range(4) for c0 in range(0, nk, 512)]
            return q0, nk, pieces

        def st_idx(i):
            q0, nk, pieces = geom(i)
            for h in range(16):
                S.op("act", lambda e, h=h: e.activation(out=Dg[:, h, :], in_=self.ident_bf, func=AF.Copy,
                                                        scale=wq[:, i, h:h + 1]),
                     reads=["ident", ("wq", i)], writes=["Dg"])
            for h in range(16):
                hb = h % 2
                eng = "act"
                if eng == "pool":
                    S.op("pool", lambda e, h=h, hb=hb: e.tensor_copy(
                        out=iqz[hb * 64:(hb + 1) * 64, h, :], in_=iqT[hb * 64:(hb + 1) * 64, h // 2, q0:q0 + 128]),
                        reads=["iqT", "iqz"], writes=[("iqzh", h)])
                else:
                    S.op("act", lambda e, h=h, hb=hb: e.activation(
                        out=iqz[hb * 64:(hb + 1) * 64, h, :], in_=iqT[hb * 64:(hb + 1) * 64, h // 2, q0:q0 + 128],
                        func=AF.Copy),
                        reads=["iqT", "iqz"], writes=[("iqzh", h)])
            for pi, (r, c0, cn) in enumerate(pieces):
                col0 = r * 1024 + c0
                accb = 4 + pi % 2

                def head_mm(h, cn=cn, col0=col0):
                    bk, hb = h % 4, h % 2
                    S.op("pe", lambda e: e.matmul(
                        ps[:, bk, 0:cn], lhsT=iqz[:, h, :],
                        rhs=IK_all[:, col0:col0 + cn], start=True, stop=True),
                        reads=["IK_all", ("iqzh", h)], writes=[("ps", bk)])
                    if h % 8 in (0, 3, 6):
                        S.op("act", lambda e: e.activation(out=rh[bk][:, 0:cn], in_=ps[:, bk, 0:cn], func=AF.Relu),
                             reads=[("ps", bk)], writes=[("rh", bk)])
                    else:
                        S.op("dve", lambda e: e.tensor_scalar(out=rh[bk][:, 0:cn], in0=ps[:, bk, 0:cn], scalar1=0.0,
                                                              scalar2=None, op0=ALU.max),
                             reads=[("ps", bk)], writes=[("rh", bk)])

                def head_acc(h, cn=cn, accb=accb):
                    bk = h % 4
                    S.op("pe", lambda e: e.matmul(
                        ps[:, accb, 0:cn], lhsT=Dg[:, h, :], rhs=rh[bk][:, 0:cn], start=(h == 0), stop=(h == 15)),
                        reads=["Dg", ("rh", bk)], writes=[("ps", accb)])
                for h in range(16):
                    head_mm(h)
                    if h >= 2:
                        head_acc(h - 2)
                head_acc(14)
                head_acc(15)
                S.op("act", lambda e, accb=accb, col0=col0, cn=cn: e.activation(
                    out=sc[:, col0:col0 + cn], in_=ps[:, accb, 0:cn], func=AF.Copy),
                    reads=[("ps", accb)], writes=["sc"])

        def st_bis(i):
            q0, nk, pieces = geom(i)
            scv, mbv, jkv = sc4[:, :, 0:nk], mb4[:, :, 0:nk], jk4[:, :, 0:nk]
            S.op("dve", lambda e: e.reduce_max(out=Bt, in_=scv, axis=AX.XY, apply_absolute_value=True),
                 reads=["sc"], writes=["Bt"])
            S.op("dve", lambda e: e.tensor_tensor(out=sc4[:, :, q0:q0 + 128], in0=sc4[:, :, q0:q0 + 128], in1=cbt,
                                                  op=ALU.add),
                 reads=["sc", "cbt", "Bt"], writes=["sc"])
            S.op("dve", lambda e: e.tensor_scalar(out=Wc, in0=Bt, scalar1=2.0002, scalar2=1e-6,
                                                  op0=ALU.mult, op1=ALU.add), reads=["Bt"], writes=["Wc"])
            S.op("dve", lambda e: e.tensor_scalar(out=H[:, 0:NB + 1], in0=pw[:, 0:NB + 1], scalar1=Wc, scalar2=None,
                                                  op0=ALU.mult), reads=["Wc", "catt"], writes=["H"])
            S.op("dve", lambda e: e.memset(mid, 0.0), writes=["mid"])
            for k in range(NB):
                S.op("dve", lambda e: e.tensor_scalar(
                    out=jkv, in0=scv, scalar1=mid, scalar2=0.0, op0=ALU.is_ge, op1=ALU.add, accum_out=cnt),
                    reads=["sc", "mid"], writes=["junk", "cnt"])
                S.op("dve", lambda e, k=k: e.tensor_scalar(out=u2, in0=cnt, scalar1=256.0, scalar2=H[:, k:k + 1],
                                                           op0=ALU.is_ge, op1=ALU.mult),
                     reads=["cnt", "H"], writes=["u2"])
                S.op("dve", lambda e, k=k: e.scalar_tensor_tensor(out=mid, in0=mid, scalar=H[:, k + 1:k + 2], in1=u2,
                                                                  op0=ALU.subtract, op1=ALU.add),
                     reads=["mid", "H", "u2"], writes=["mid"])
            S.op("dve", lambda e: e.tensor_tensor(out=tau, in0=mid, in1=H[:, NB:NB + 1], op=ALU.subtract),
                 reads=["mid", "H"], writes=["tau"])
            S.op("dve", lambda e: e.tensor_scalar(
                out=mbv, in0=scv, scalar1=tau, scalar2=-30000.0, op0=ALU.is_lt, op1=ALU.mult),
                reads=["sc", "tau"], writes=["mb"])
            nb_ = i + 1
            sc5 = scv.rearrange("p r (j s) -> p r j s", s=128)
            mb5 = mbv.rearrange("p r (j s) -> p r j s", s=128)
            S.op("dve", lambda e: e.tensor_tensor(
                out=sc5, in0=mb5, in1=Pt.unsqueeze(1).unsqueeze(1).to_broadcast([128, 4, nb_, 128]), op=ALU.add),
                reads=["mb", "Pt", "sc"], writes=["sc"])
            g1v = g1.rearrange("p (r j) -> p r j", r=4)[:, :, 0:nb_]
            S.op("dve", lambda e: e.tensor_reduce(out=g1v, in_=sc5, axis=AX.X, op=ALU.max),
                 reads=["sc"], writes=["g1"])
            S.op("dve", lambda e: e.tensor_tensor(out=g1v, in0=g1v, in1=Qt[:, :, 0:nb_], op=ALU.add),
                 reads=["g1", "catt"], writes=["g1"])
            S.op("dve", lambda e: e.tensor_reduce(out=kmx, in_=g1v, axis=AX.XY, op=ALU.max),
                 reads=["g1"], writes=["kmx"])
            S.op("dve", lambda e: e.tensor_scalar(out=kmx, in0=kmx, scalar1=15.0, scalar2=None, op0=ALU.max),
                 reads=["kmx"], writes=["kmx"])
            S.op("dve", lambda e: e.tensor_scalar(out=cc, in0=nslope, scalar1=kmx, scalar2=None, op0=ALU.mult),
                 reads=["kmx", "catt"], writes=["cc"])

        def st_mbT(i):
            kts = [(r, ip) for r in range(4) for ip in range(i + 1)]
            for g0 in range(0, len(kts), 8):
                grp = kts[g0:g0 + 8]
                bk = 6 + (g0 // 8) % 2
                for s_, (r, ip) in enumerate(grp):
                    S.op("pe", lambda e, bk=bk, s_=s_, r=r, ip=ip: e.transpose(
                        out=psb(bk)[:, s_ * 128:(s_ + 1) * 128], in_=mb[:, r * 1024 + ip * 128:r * 1024 + ip * 128 + 128],
                        identity=self.ident_bf),
                        reads=["mb", "ident"], writes=[("ps", bk)])
                for s_, (r, ip) in enumerate(grp):
                    S.op("act", lambda e, bk=bk, s_=s_, r=r, ip=ip: e.activation(
                        out=mbT[:, r * 8 + ip, :], in_=psb(bk)[:, s_ * 128:(s_ + 1) * 128], func=AF.Copy),
                        reads=[("ps", bk)], writes=["mbT"])

        def st_passA(i):
            q0, nk, pieces = geom(i)
            S.op("dve", lambda e: e.memset(ps[:, 5:8, :].rearrange("p a b -> p (a b)"), 0.0),
                 writes=[("ps", 5), ("ps", 6), ("ps", 7)])
            Dc = PTb[1].rearrange("p (h t) -> p h t", h=8)
            for h in range(8):
                S.op("dve", lambda e, h=h: e.tensor_scalar(out=Dc[:, h, :], in0=self.ident_bf, scalar1=cc[:, h:h + 1],
                                                           scalar2=None, op0=ALU.mult),
                     reads=["ident", "cc"], writes=[("PTb", 1)])
            for a_ in range(2):
                S.op("pe", lambda e, a_=a_: e.matmul(ps[:, 4, :], lhsT=self.ones_bf,
                                                     rhs=PTb[1][:, a_ * 512:(a_ + 1) * 512],
                                                     start=True, stop=True),
                     reads=[("PTb", 1), "ones_bf"], writes=[("ps", 4)])
                S.op("dve", lambda e, a_=a_: e.tensor_copy(out=AugR[64:65, a_ * 512:(a_ + 1) * 512], in_=ps[64:65, 4, :]),
                     reads=[("ps", 4)], writes=["AugR"])

        def st_passB(i):
            q0, nk, pieces = geom(i)
            ktl = [(r * 1024 + ip * 128, r * 8 + ip, 128) for r in range(4) for ip in range(i + 1)] + [(4096, 32, NMETA)]
            Oreg = lambda h: ps[:, 5 + h // 3, (h % 3) * 129:(h % 3 + 1) * 129]

            def logits(qi):
                col0, vt, n = ktl[qi]
                meta = (n == NMETA)
                pair = (0, 1) if qi % 2 == 0 else (2, 3)
                for h in range(8):
                    kvh = h // 4
                    out = ps[0:n, pair[h // 4], (h % 4) * 128:(h % 4 + 1) * 128]
                    S.op("pe", lambda e, out=out, kvh=kvh, h=h: e.matmul(
                        out, lhsT=K_all[:, kvh, col0:col0 + n], rhs=qT[:, h, q0:q0 + 128], start=True, stop=False),
                        reads=["K_all", "qT", ("Kmeta", kvh)], writes=[("ps", pair[h // 4])])
                    S.op("pe", lambda e, out=out, h=h: e.matmul(
                        out, lhsT=AugK[0:65, col0:col0 + n], rhs=AugR[0:65, h * 128:(h + 1) * 128],
                        start=False, stop=meta),
                        reads=["AugK", "AugR"], writes=[("ps", pair[h // 4])])
                    if not meta:
                        S.op("pe", lambda e, out=out: e.matmul(
                            out, lhsT=self.ident_bf, rhs=mbT[:, vt, :], start=False, stop=True),
                            reads=["mbT", "ident"], writes=[("ps", pair[h // 4])])
                pt = PTb[qi % 2]
                S.op("act", lambda e: e.activation(
                    out=pt[0:n, :], in_=ps[0:n, pair[0]:pair[0] + 2, :].rearrange("p a b -> p (a b)"), func=AF.Exp),
                    reads=[("ps", pair[0]), ("ps", pair[1])], writes=[("PTb", qi % 2)])

            def pv(qi):
                col0, vt, n = ktl[qi]
                pt = PTb[qi % 2]
                for h in range(8):
                    kvh = h // 4
                    S.op("pe", lambda e, h=h, kvh=kvh: e.matmul(
                        Oreg(h), lhsT=pt[0:n, h * 128:(h + 1) * 128], rhs=V_all[0:n, vt, kvh * 129:(kvh + 1) * 129],
                        start=False, stop=(qi == len(ktl) - 1)),
                        reads=[("PTb", qi % 2), "V_all", "Vmeta"], writes=[("ps", 5 + h // 3)])
            logits(0)
            for qi in range(len(ktl)):
                if qi + 1 < len(ktl):
                    logits(qi + 1)
                pv(qi)

        def st_fin(i):
            q0 = 128 * i
            for b3 in range(3):
                nh = 3 if b3 < 2 else 2
                Ov = ps[:, 5 + b3, 0:nh * 129].rearrange("p (h c) -> p h c", c=129)
                S.op("dve", lambda e, Ov=Ov, b3=b3, nh=nh: e.reciprocal(out=rs[:, 3 * b3:3 * b3 + nh], in_=Ov[:, :, 128]),
                     reads=[("ps", 5 + b3)], writes=[("rs", b3)])
                S.op("dve", lambda e, Ov=Ov, b3=b3, nh=nh: e.tensor_tensor(
                    out=ya[:, 3 * b3:3 * b3 + nh, :], in0=Ov[:, :, 0:128],
                    in1=rs[:, 3 * b3:3 * b3 + nh].unsqueeze(2).to_broadcast([128, nh, 128]), op=ALU.mult),
                    reads=[("ps", 5 + b3), ("rs", b3)], writes=[("ya", b3), ("PTb", 0)])
            for h in range(8):
                S.op("pe", lambda e, h=h: e.transpose(out=psb(4)[:, h * 128:(h + 1) * 128], in_=ya[:, h, :],
                                                      identity=self.ident_bf),
                     reads=[("ya", h // 3), ("PTb", 0), "ident"], writes=[("ps", 4)])
            S.op("act", lambda e: e.activation(out=yatt[:, :, q0:q0 + 128],
                                               in_=psb(4).rearrange("p (h t) -> p h t", h=8), func=AF.Copy),
                 reads=[("ps", 4)], writes=[("yatt", i)])

        st_idx(0)
        st_bis(0)
        st_mbT(0)
        for i in range(8):
            if i + 1 < 8:
                st_idx(i + 1)
            st_passA(i)
            if i + 1 < 8:
                st_bis(i + 1)
            st_passB(i)
            st_fin(i)
            if i + 1 < 8:
                st_mbT(i + 1)
        S.barrier()

    def merge_m4(self, strm, cg, cb):
        S, ps, nc = self.S, self.ps, self.nc
        R1 = 99840
        RTB = TBS[0:2]
        yatt = self.view(0, BF16, [8, NREAL]); yhg = self.view(32768, BF16, [8, NREAL])
        merged = self.view(R1, BF16, [KC, NREAL])
        o = R1 + 32768
        wga = [self.view(o + q * 8192, BF16, [KC, 256]) for q in range(2)]
        wgh = [self.view(o + 16384 + q * 8192, BF16, [KC, 256]) for q in range(2)]
        wba = [self.view(o + 32768 + q * 4096, BF16, [8, 256]) for q in range(2)]
        wbh = [self.view(o + 40960 + q * 4096, BF16, [8, 256]) for q in range(2)]
        tm = [self.view(o + 49152 + q * 2048, F32, [512]) for q in range(4)]
        for mg in range(8):
            q = mg % 2
            S.dma("pool", lambda e, q=q, mg=mg: e.dma_start(out=wga[q], in_=self.win[self.gidx["ga%d" % mg]]),
                  writes=[("wga", q)])
            S.dma("pool", lambda e, q=q, mg=mg: e.dma_start(out=wgh[q], in_=self.win[self.gidx["gh%d" % mg]]),
                  writes=[("wgh", q)])
            S.dma("pool", lambda e, q=q, mg=mg: e.dma_start(out=wba[q], in_=self.wba_d[mg]), writes=[("wba", q)])
            S.dma("pool", lambda e, q=q, mg=mg: e.dma_start(out=wbh[q], in_=self.wbh_d[mg]), writes=[("wbh", q)])
            for half in range(2):
                mc = 2 * mg + half
                hs = slice(half * 128, (half + 1) * 128)
                for ti, (t0, t1e) in enumerate(RTB):
                    pp = (half * 2 + ti) % 2
                    b0 = 4 * pp
                    for k in range(KC):
                        S.op("pe", lambda e, b0=b0, q=q, k=k, hs=hs, t0=t0, t1e=t1e: e.matmul(
                            ps[:, b0, :], lhsT=wga[q][:, k, hs], rhs=strm[:, k, t0:t1e],
                            start=(k == 0), stop=(k == KC - 1)),
                            reads=[("wga", q), "strm"], writes=[("ps", b0)])
                    for k in range(8):
                        S.op("pe", lambda e, b0=b0, q=q, k=k, hs=hs, t0=t0, t1e=t1e: e.matmul(
                            ps[:, b0 + 1, :], lhsT=wba[q][:, k, hs], rhs=yatt[:, k, t0:t1e],
                            start=(k == 0), stop=(k == 7)),
                            reads=[("wba", q), "yatt"], writes=[("ps", b0 + 1)])
                    for k in range(KC):
                        S.op("pe", lambda e, b0=b0, q=q, k=k, hs=hs, t0=t0, t1e=t1e: e.matmul(
                            ps[:, b0 + 2, :], lhsT=wgh[q][:, k, hs], rhs=strm[:, k, t0:t1e],
                            start=(k == 0), stop=(k == KC - 1)),
                            reads=[("wgh", q), "strm"], writes=[("ps", b0 + 2)])
                    for k in range(8):
                        S.op("pe", lambda e, b0=b0, q=q, k=k, hs=hs, t0=t0, t1e=t1e: e.matmul(
                            ps[:, b0 + 3, :], lhsT=wbh[q][:, k, hs], rhs=yhg[:, k, t0:t1e],
                            start=(k == 0), stop=(k == 7)),
                            reads=[("wbh", q), "yhg"], writes=[("ps", b0 + 3)])
                    ta, th = tm[2 * pp], tm[2 * pp + 1]
                    S.op("act", lambda e, ta=ta, b0=b0: e.activation(out=ta, in_=ps[:, b0, :], func=AF.Sigmoid),
                         reads=[("ps", b0)], writes=[("tm", 2 * pp)])
                    S.op("dve", lambda e, ta=ta, b0=b0: e.tensor_tensor(out=ta, in0=ta, in1=ps[:, b0 + 1, :], op=ALU.mult),
                         reads=[("tm", 2 * pp), ("ps", b0 + 1)], writes=[("tm", 2 * pp)])
                    S.op("act", lambda e, th=th, b0=b0: e.activation(out=th, in_=ps[:, b0 + 2, :], func=AF.Sigmoid),
                         reads=[("ps", b0 + 2)], writes=[("tm", 2 * pp + 1)])
                    S.op("dve", lambda e, th=th, b0=b0: e.tensor_tensor(out=th, in0=th, in1=ps[:, b0 + 3, :], op=ALU.mult),
                         reads=[("tm", 2 * pp + 1), ("ps", b0 + 3)], writes=[("tm", 2 * pp + 1)])
                    S.op("dve", lambda e, ta=ta, th=th, mc=mc, t0=t0, t1e=t1e: e.tensor_tensor(
                        out=merged[:, mc, t0:t1e], in0=ta, in1=th, op=ALU.add),
                        reads=[("tm", 2 * pp), ("tm", 2 * pp + 1)], writes=[("merged", mc, ti)])
        S.barrier()
        z = self.view(R1 + 32768, F32, [KC, NREAL])
        wo = [self.view(53760 + q * 4096, BF16, [KC, 128]) for q in range(2)]
        strm_out = self.view(0, BF16, [KC, NREAL])
        for dc in range(KC):
            q = dc % 2
            S.dma("pool", lambda e, q=q, dc=dc: e.dma_start(out=wo[q], in_=self.wo_d[dc]), writes=[("wo", q)])
            S.dma("sp", lambda e, dc=dc: e.dma_start(out=z[:, dc, :], in_=self.h1s[dc * 128:(dc + 1) * 128, 0:NREAL]),
                  writes=[("l2", "z", dc, ti) for ti in range(2)])
            for ti, (t0, t1e) in enumerate(RTB):
                pb = (dc * 2 + ti) % 2
                for k in range(KC):
                    S.op("pe", lambda e, pb=pb, q=q, k=k, t0=t0, t1e=t1e: e.matmul(
                        ps[:, pb, :], lhsT=wo[q][:, k, :], rhs=merged[:, k, t0:t1e],
                        start=(k == 0), stop=(k == KC - 1)),
                        reads=[("wo", q), ("merged", k, ti)], writes=[("ps", pb)])
                S.op("dve", lambda e, pb=pb, dc=dc, t0=t0, t1e=t1e: e.scalar_tensor_tensor(
                    out=z[:, dc, t0:t1e], in0=ps[:, pb, :], scalar=1.0 / ALPHA, in1=z[:, dc, t0:t1e],
                    op0=ALU.mult, op1=ALU.add),
                    reads=[("ps", pb), ("l2", "z", dc, ti)], writes=[("l2", "z", dc, ti)])
        self.ln_apply("l2", z, RTB, cg, cb, 33280, LN_EPS / (ALPHA * ALPHA), strm_out=strm_out,
                      resid_out=self.h2s)
        S.barrier()

    def eps_col(self, val):
        return self.eps_cols[val]

    def build(self):
        nc, S = self.nc, self.S
        stage = self.stage
        xT = self.din("xT", [D, NT])
        wg1 = self.din("wg1", [FC // 2, 128, KC, 256])
        wu1 = self.din("wu1", [FC // 2, 128, KC, 256])
        wd1 = self.din("wd1", [KC, 128, FC, 128])
        wg2 = self.din("wg2", [FC // 2, 128, KC, 256])
        wu2 = self.din("wu2", [FC // 2, 128, KC, 256])
        wd2 = self.din("wd2", [KC, 128, FC, 128])
        cvec = self.din("cvec", [128, 128])
        cmat = self.cmat_d = self.din("cmat", [128, 512])
        self.catt = self.din("catt", [128, 224])
        self.augk = self.din("augk", [3, 4112])
        self.augs = self.din("augs", [2, 1024])
        self.augq = self.din("augq", [8, 1024])
        self.cbt_d = self.din("cbt", [128, 512])
        self.win = self.din("win", [len(GROUPS), 128, KC, 256])
        self.wba_d = self.din("wba", [8, 128, 8, 256])
        self.wbh_d = self.din("wbh", [8, 128, 8, 256])
        self.wo_d = self.din("wo", [KC, 128, KC, 128])
        self.gidx = {nm: i for i, (nm, _, _) in enumerate(GROUPS)}
        self.h1s = h1s = self.dscratch("h1s", [D, NT])
        self.h2s = self.dscratch("h2s", [D, NREAL])
        self.xs = [self.dscratch("xs%d" % q, [128, 3 * 1024], BF16) for q in range(3)]
        self.xg = [self.dscratch("xg%d" % q, [512, 3 * 1024], BF16) for q in range(3)]
        self.xa = self.dscratch("xa", [128, 72])
        self.xag = self.dscratch("xag", [512, 72])
        self.ks = self.dscratch("ks", [128, 2048], BF16)
        self.kg = self.dscratch("kg", [512, 2048], BF16)
        self.vs = self.dscratch("vs", [128, 3088], BF16)
        self.vg = self.dscratch("vg", [512, 3088], BF16)
        if stage == 1:
            dbg = self.dout("dbg", [D, NT])
        elif stage in (3, 4):
            dbg = self.dout("dbg", [128, 8 * NREAL])
            self.dbg2 = self.dout("dbg2", [128, 12000])
        elif stage == 5:
            dbg = self.dout("dbg", [D, NREAL])
        else:
            outT = self.dout("outT", [D, NREAL])

        from contextlib import ExitStack
        with ExitStack() as es:
            self.arena = es.enter_context(nc.sbuf_tensor("arena", [128, ARENA_F32], F32))
            self.cst = es.enter_context(nc.sbuf_tensor("cst", [128, CONST_F32], F32))
            self.ps = es.enter_context(nc.psum_tensor("ps", [128, 8, 512], F32))
            esems = {e: es.enter_context(nc.semaphore("sem_" + e)) for e in ENGS}
            dsems = [es.enter_context(nc.semaphore("dsem%d" % i)) for i in range(S.n_dma_sems + 8)]
            block = es.enter_context(nc.Block())
            cst = self.cst
            self.ones_f32 = cst[:, 0:128]
            cv = self.cv = cst[:, 128:256]
            epsA = cst[:, 256:257]
            self.eps_rms = cst[:, 257:258]
            self.eps_ik = cst[:, 258:259]
            self.eps_cols = {LN_EPS / (ALPHA * ALPHA): epsA}
            self.ident_bf = cst[:, 264:328].bitcast(BF16)
            self.ones_bf = cst[:, 328:392].bitcast(BF16)
            self.mask2 = cst[:, 392:520]
            self.ones128 = cst[:, 520:648]
            self.lbc = cst[:, 648:656]
            self.omlc = cst[:, 656:664]
            self.gnc = cv[:, 112:120]
            self.selc = cv[:, 120:124]
            S.op("dve", lambda e: e.memset(self.ones_f32, 1.0 / D), writes=["ones"])
            S.op("dve", lambda e: e.memset(self.ones128, 1.0 / 128), writes=["ones128"])
            S.op("dve", lambda e: e.memset(epsA, LN_EPS / (ALPHA * ALPHA)), writes=["eps"])
            S.op("dve", lambda e: e.memset(self.eps_rms, RMS_EPS), writes=["eps"])
            S.op("dve", lambda e: e.memset(self.eps_ik, LN_EPS), writes=["eps"])
            S.dma("sp", lambda e: e.dma_start(out=cv, in_=cvec), writes=["cv"])
            S.dma("sp", lambda e: e.dma_start(out=self.mask2, in_=cmat[:, 256:384]), writes=["mask2"])
            S.dma("pool", lambda e: e.dma_start(out=self.ident_bf, in_=cmat[:, 0:128]), writes=["ident"])
            S.dma("pool", lambda e: e.dma_start(out=self.ones_bf, in_=cmat[:, 128:256]), writes=["ones_bf"])
            S.op("dve", lambda e: e.tensor_tensor(out=self.lbc, in0=cv[:, 96:104], in1=cv[:, 104:112], op=ALU.subtract),
                 reads=["cv"], writes=["lb"])
            S.op("act", lambda e: e.activation(out=self.lbc, in_=self.lbc, func=AF.Sigmoid), reads=["lb"], writes=["lb"])
            S.op("dve", lambda e: e.tensor_scalar(out=self.omlc, in0=self.lbc, scalar1=-1.0, scalar2=1.0,
                                                  op0=ALU.mult, op1=ALU.add), reads=["lb"], writes=["lb"])
            strm0 = self.view(0, BF16, [KC, NT])
            S.dma("pool", lambda e: e.dma_start(out=strm0, in_=xT.rearrange("(k p) t -> p k t", p=128)),
                  writes=[("f1", "sin")])
            S.barrier()
            self.ffn_phase("f1", NT, TBS, strm0, 66560, wg1, wu1, wd1, xT, cv[:, 0:16], cv[:, 16:32],
                           resid_out=(dbg if stage == 1 else h1s))
            strm1 = self.view(66560, BF16, [KC, NT])
            if stage >= 2:
                self.hgrn_m1(strm1)
                self.hgrn_m2(strm1)
            if stage == 3:
                yhg = self.view(32768, BF16, [8 * NREAL])
                S.dma("pool", lambda e: e.dma_start(out=dbg, in_=yhg), reads=[])
            if stage >= 4:
                self.attn_m3(strm1)
            if stage == 4:
                yat = self.view(0, BF16, [8 * NREAL])
                S.dma("pool", lambda e: e.dma_start(out=dbg, in_=yat), reads=[])
            if stage >= 5:
                if stage == 5:
                    self.h2s = dbg
                self.merge_m4(strm1, cv[:, 32:48], cv[:, 48:64])
            if stage >= 6:
                strm2 = self.view(0, BF16, [KC, NREAL])
                self.ffn_phase("f2", NREAL, TBS[0:2], strm2, 66560, wg2, wu2, wd2, self.h2s, cv[:, 64:80],
                               cv[:, 80:96], final_out=outT)
            S.emit(block, esems, dsems)
        return nc


def _lay_gu(w):
    return np.ascontiguousarray(w.reshape(KC, 128, FC // 2, 256).transpose(2, 1, 0, 3))


def _lay_d(w):
    return np.ascontiguousarray(w.reshape(FC, 128, KC, 128).transpose(2, 1, 0, 3))


def _fm(v):
    return np.ascontiguousarray(v.reshape(KC, 128).T)


def _core_tokens(x, meta, c):
    b, j = c // 4, c % 4
    blocks = [x[b, 128 * (4 * i + j):128 * (4 * i + j) + 128] for i in range(8)]
    tok = np.concatenate(blocks + [meta], axis=0)
    return np.ascontiguousarray(tok.T)


def _mk_groups():
    g = []
    for hp in range(4):
        g += [("hf%d" % hp, 3664 + 256 * hp, 256), ("hq%d" % hp, 2640 + 256 * hp, 256),
              ("hi%d" % hp, 4688 + 256 * hp, 256)]
    for i in range(4):
        g.append(("hg%d" % i, 5712 + 256 * i, 256))
    g += [("ak", 1024, 256), ("av", 1280, 256), ("ikw", 2560, 80)]
    for i in range(4):
        g.append(("aq%d" % i, 256 * i, 256))
    for i in range(4):
        g.append(("iq%d" % i, 1536 + 256 * i, 256))
    for i in range(8):
        g.append(("ga%d" % i, 6736 + 256 * i, 256))
        g.append(("gh%d" % i, 8784 + 256 * i, 256))
    return g


GROUPS = _mk_groups()


def _lay_win(w):
    out = np.zeros((len(GROUPS), 128, KC, 256), np.float32)
    for gi, (nm, c0, nc_) in enumerate(GROUPS):
        out[gi, :, :, 0:nc_] = w[:, c0:c0 + nc_].reshape(KC, 128, nc_).transpose(1, 0, 2)
    return out


def prepare(inputs, stage):
    f = lambda k: np.asarray(inputs[k], dtype=np.float32)
    x, meta = f("x"), f("meta")
    shared = {
        "wg1": _lay_gu(f("ffn1_w_gate")[0]), "wu1": _lay_gu(f("ffn1_w_up")[0]),
        "wd1": _lay_d(f("ffn1_w_down")[0]),
        "win": _lay_win(f("w_in")[0]),
        "wg2": _lay_gu(f("ffn2_w_gate")[0]), "wu2": _lay_gu(f("ffn2_w_up")[0]),
        "wd2": _lay_d(f("ffn2_w_down")[0]),
        "wba": np.ascontiguousarray(f("w_branch_att")[0].reshape(8, 128, 8, 256).transpose(2, 1, 0, 3)),
        "wbh": np.ascontiguousarray(f("w_branch_hg")[0].reshape(8, 128, 8, 256).transpose(2, 1, 0, 3)),
        "wo": np.ascontiguousarray(f("w_out")[0].reshape(KC, 128, KC, 128).transpose(2, 1, 0, 3)),
    }
    slopes = (2.0 ** -(np.arange(8) + 1.0)).astype(np.float32)
    c = np.arange(4096)
    kpos = np.concatenate([16 + 128 * (4 * ((c % 1024) // 128) + c // 1024) + c % 128, np.arange(16)]).astype(np.float32)
    augk = np.stack([np.floor(kpos / 64.0), kpos % 64.0, np.ones_like(kpos)], 0).astype(np.float32)
    augs = np.stack([np.repeat(64.0 * slopes, 128), np.repeat(slopes, 128)], 0).astype(np.float32)
    shared["augk"] = augk
    shared["augs"] = augs
    cvec = np.zeros((128, 128), np.float32)
    cvec[:, 0:16] = _fm(f("ln1_g")[0]); cvec[:, 16:32] = _fm(f("ln1_b")[0])
    cvec[:, 32:48] = _fm(f("ln2_g")[0]); cvec[:, 48:64] = _fm(f("ln2_b")[0])
    cvec[:, 64:80] = _fm(f("ln3_g")[0]); cvec[:, 80:96] = _fm(f("ln3_b")[0])
    lbl = f("hg_lb_logits")
    cvec[:, 96:104] = lbl[0].reshape(8, 128).T
    cvec[:, 104:112] = lbl[1].reshape(8, 128).T
    cvec[:, 112:120] = f("hg_norm_g")[0].T
    cmat = np.zeros((128, 512), np.float32)
    cmat[:, 384:512] = np.arange(128, dtype=np.float32)[None, :]
    cmat[:, 0:128] = np.eye(128, dtype=np.float32)
    cmat[:, 128:256] = 1.0
    sidx = np.arange(128)[:, None]; tidx = np.arange(128)[None, :]
    cmat[:, 256:384] = (((sidx <= tidx) & ((sidx // 64) == (tidx // 64))) | ((sidx < 64) & (tidx >= 64))).astype(np.float32)
    shared["cmat"] = cmat
    maps = []
    for c in range(8):
        m = dict(shared)
        m["xT"] = _core_tokens(x, meta, c)
        cv = cvec.copy()
        cv[:, 120 + (c % 4)] = 1.0
        m["cvec"] = cv
        j = c % 4
        p = np.arange(128, dtype=np.float32)
        qpos = np.stack([16 + 128 * (4 * i + j) + p for i in range(8)], 0)
        m["augq"] = np.ascontiguousarray((-slopes[None, :, None] * qpos[:, None, :]).reshape(8, 1024).astype(np.float32))
        catt = np.zeros((128, 224), np.float32)
        catt[:, 0:8] = -slopes[None, :]
        catt[:, 8:40] = np.array([16 + 128 * r + 512 * jj for r in range(4) for jj in range(8)], np.float32)[None, :]
        catt[:, 64:96] = (2.0 ** -(np.arange(32) + 1.0))[None, :]
        catt[:, 96:160] = f("idx_k_norm_g")[0][None, :]
        catt[:, 160:224] = f("idx_k_norm_b")[0][None, :]
        m["catt"] = catt
        tt = np.arange(128)[:, None, None]; rr = np.arange(4)[None, :, None]; ss = np.arange(128)[None, None, :]
        m["cbt"] = np.where(128 * (rr - j) + (ss - tt) > 0, -1e30, 0.0).astype(np.float32).reshape(128, 512)
        maps.append(m)
    return maps


_NC_CACHE = {}


def kernel(**inputs):
    stage = int(os.environ.get("KSTAGE", "9"))
    if stage not in _NC_CACHE:
        _NC_CACHE[stage] = Builder(stage).build()
    nc = _NC_CACHE[stage]
    maps = prepare(inputs, stage)
    res = run_bass_kernel_spmd(nc, maps, core_ids=list(range(8)))
    if stage == 4:
        return [(r["dbg"], r["dbg2"]) for r in res.results]
    if stage < 6:
        return [r["dbg"] for r in res.results]
    out = np.zeros((2, 4096, D), np.float32)
    for c in range(8):
        b, j = c // 4, c % 4
        o = res.results[c]["outT"]
        for i in range(8):
            g = 4 * i + j
            out[b, 128 * g:128 * g + 128] = o[:, 128 * i:128 * i + 128].T
    return out
```

```python
import os
import numpy as np
import concourse.bass as bass
import concourse.mybir as mybir
from concourse.bass_utils import run_bass_kernel_spmd

F32 = mybir.dt.float32
BF16 = mybir.dt.bfloat16
U8 = mybir.dt.uint8
AF = mybir.ActivationFunctionType
ALU = mybir.AluOpType
AX = mybir.AxisListType

D = 2048
DFF = 5632
NMETA = 16
NREAL = 1024
NT = NREAL + NMETA
KC = D // 128
FC = DFF // 128
TBS = [(0, 512), (512, 1024), (1024, 1040)]
ALPHA = 2.0 ** 0.25
LN_EPS = 1e-5
RMS_EPS = 1e-6

ENGS = ("pe", "act", "dve", "pool", "sp")


class Op:
    __slots__ = ("eng", "fn", "deps", "is_dma", "dsem", "dval", "sig", "sigidx", "waits")

    def __init__(self, eng, fn, is_dma=False):
        self.eng = eng
        self.fn = fn
        self.deps = []
        self.is_dma = is_dma
        self.dsem = None
        self.dval = 0
        self.sig = False
        self.sigidx = 0
        self.waits = []


class Sched:
    def __init__(self, n_dma_sems=40, same_engine_sync=True):
        self.ops = {e: [] for e in ENGS}
        self.lastw = {}
        self.readers = {}
        self.n_dma = 0
        self.n_dma_sems = n_dma_sems
        self.dma_hist = {}
        self.same_engine_sync = same_engine_sync
        self.n_coll = 0

    def _add(self, op, reads, writes):
        deps = set()
        for k in reads:
            w = self.lastw.get(k)
            if w is not None:
                deps.add(w)
        for k in writes:
            w = self.lastw.get(k)
            if w is not None:
                deps.add(w)
            for r in self.readers.get(k, ()):
                deps.add(r)
        op.deps = list(deps)
        for k in reads:
            self.readers.setdefault(k, []).append(op)
        for k in writes:
            self.lastw[k] = op
            self.readers[k] = []
        self.ops[op.eng].append(op)
        return op

    def op(self, eng, fn, reads=(), writes=()):
        return self._add(Op(eng, fn), reads, writes)

    def dma(self, q, fn, reads=(), writes=()):
        op = Op(q, fn, is_dma=True)
        slot = self.n_dma % self.n_dma_sems
        op.dsem = slot
        op.dval = 16 * (self.n_dma // self.n_dma_sems + 1)
        self.n_dma += 1
        self._add(op, reads, writes)
        prev = self.dma_hist.get(slot)
        if prev is not None:
            op.deps.append(prev)
        self.dma_hist[slot] = op
        return op

    def coll(self, fn, reads=(), writes=()):
        op = Op("pool", fn, is_dma=True)
        op.dsem = self.n_dma_sems + self.n_coll
        op.dval = 1
        self.n_coll += 1
        self._add(op, reads, writes)
        self.dma_hist[op.dsem] = op
        return op

    def barrier(self):
        lasts = []
        for e in ENGS:
            for o in reversed(self.ops[e]):
                if not o.is_dma and o.fn is not None:
                    lasts.append(o)
                    break
        dmas = list(self.dma_hist.values())
        for e in ENGS:
            op = Op(e, None)
            op.deps = [o for o in lasts if o.eng != e] + dmas
            self.ops[e].append(op)
        self.lastw = {}
        self.readers = {}

    def _skip(self, d, op):
        return d.eng == op.eng and (d.eng in ("pe", "sp") or not self.same_engine_sync)

    def finalize(self):
        for e in ENGS:
            for op in self.ops[e]:
                for d in op.deps:
                    if not d.is_dma and not self._skip(d, op):
                        d.sig = True
        for e in ENGS:
            c = 0
            for op in self.ops[e]:
                if op.sig:
                    c += 1
                    op.sigidx = c
        for e in ENGS:
            waited = {}
            for op in self.ops[e]:
                need = {}
                for d in op.deps:
                    if d.is_dma:
                        key, val = ("d", d.dsem), d.dval
                    else:
                        if self._skip(d, op):
                            continue
                        key, val = ("e", d.eng), d.sigidx
                    if waited.get(key, 0) >= val:
                        continue
                    if need.get(key, 0) < val:
                        need[key] = val
                for k, v in need.items():
                    waited[k] = v
                op.waits = list(need.items())

    def emit(self, block, esems, dsems):
        self.finalize()
        regs = {"pe": block.tensor, "act": block.scalar, "dve": block.vector,
                "pool": block.gpsimd, "sp": block.sync}
        final = {d.dsem: d.dval for d in self.dma_hist.values()}

        def make(e):
            ops = self.ops[e]

            def body(eng):
                for op in ops:
                    for (kind, which), val in op.waits:
                        eng.wait_ge(dsems[which] if kind == "d" else esems[which], val)
                    if op.fn is None:
                        continue
                    ins = op.fn(eng)
                    if op.is_dma:
                        ins.then_inc(dsems[op.dsem], 16 if op.dsem < self.n_dma_sems else 1)
                    elif op.sig:
                        ins.then_inc(esems[e], 1)
                if e == "sp":
                    for slot, val in final.items():
                        eng.wait_ge(dsems[slot], val)
            return body

        for e in ENGS:
            regs[e](make(e))


ARENA_F32 = 50816
CONST_F32 = 2304


class Builder:
    def __init__(self, stage):
        self.stage = stage
        self.nc = bass.Bass("TRN2", target_bir_lowering=False)
        self.S = Sched()
        self.dram = {}

    def din(self, name, shape, dt=F32):
        self.dram[name] = self.nc.dram_tensor(name, list(shape), dt, kind="ExternalInput").ap()
        return self.dram[name]

    def dout(self, name, shape, dt=F32):
        self.dram[name] = self.nc.dram_tensor(name, list(shape), dt, kind="ExternalOutput").ap()
        return self.dram[name]

    def dscratch(self, name, shape, dt=F32):
        self.dram[name] = self.nc.dram_tensor(name, list(shape), dt, kind="Internal").ap()
        return self.dram[name]

    def view(self, off_bytes, dt, shape):
        n = 1
        for s in shape:
            n *= s
        esz = 4 if dt == F32 else (1 if dt == U8 else 2)
        assert off_bytes % 4 == 0
        nbytes = n * esz
        assert nbytes % 4 == 0
        assert off_bytes + nbytes <= ARENA_F32 * 4, (off_bytes, nbytes)
        v = self.arena[:, off_bytes // 4:(off_bytes + nbytes) // 4]
        if dt != F32:
            v = v.bitcast(dt)
        if len(shape) == 2:
            v = v.rearrange("p (a b) -> p a b", b=shape[1])
        elif len(shape) == 3:
            v = v.rearrange("p (a b c) -> p a b c", b=shape[1], c=shape[2])
        return v

    def ln_apply(self, tag, z, tbs, cg, cb, toff, eps_eff, strm_out=None, alias_key=None, resid_out=None,
                 final_out=None):
        S, ps = self.S, self.ps
        zsq = [self.view(toff + i * 2048, F32, [512]) for i in range(2)]
        meanb = self.view(toff + 4096, F32, [512])
        rstdb = self.view(toff + 6144, F32, [512])
        t1 = [self.view(toff + 8192 + i * 2048, F32, [512]) for i in range(2)]
        t2 = [self.view(toff + 12288 + i * 2048, F32, [512]) for i in range(2)]
        o32 = [self.view(toff + 16384 + i * 2048, F32, [512]) for i in range(2)]
        ones = self.ones_f32
        for ti, (t0, t1e) in enumerate(tbs):
            n = t1e - t0
            pm, pq = ps[:, 6, 0:n], ps[:, 7, 0:n]
            for dc in range(KC):
                zq = zsq[dc % 2]
                S.op("act", lambda e, zq=zq, dc=dc, t0=t0, t1e=t1e, n=n: e.activation(
                    out=zq[:, 0:n], in_=z[:, dc, t0:t1e], func=AF.Square),
                    reads=[(tag, "z", dc, ti)], writes=[(tag, "zsq", dc % 2)])
                S.op("pe", lambda e, pm=pm, dc=dc, t0=t0, t1e=t1e: e.matmul(
                    pm, lhsT=ones, rhs=z[:, dc, t0:t1e], start=(dc == 0), stop=(dc == KC - 1)),
                    reads=[(tag, "z", dc, ti)], writes=[("ps", 6)])
                S.op("pe", lambda e, pq=pq, zq=zq, dc=dc, n=n: e.matmul(
                    pq, lhsT=ones, rhs=zq[:, 0:n], start=(dc == 0), stop=(dc == KC - 1)),
                    reads=[(tag, "zsq", dc % 2)], writes=[("ps", 7)])
            S.op("act", lambda e, pm=pm, n=n: e.activation(out=meanb[:, 0:n], in_=pm, func=AF.Copy),
                 reads=[("ps", 6)], writes=[(tag, "meanb")])
            S.op("dve", lambda e, n=n: e.tensor_tensor(out=rstdb[:, 0:n], in0=meanb[:, 0:n], in1=meanb[:, 0:n],
                                                       op=ALU.mult),
                 reads=[(tag, "meanb")], writes=[(tag, "rstdb")])
            S.op("dve", lambda e, pq=pq, n=n: e.tensor_tensor(out=rstdb[:, 0:n], in0=pq, in1=rstdb[:, 0:n],
                                                              op=ALU.subtract),
                 reads=[("ps", 7), (tag, "rstdb")], writes=[(tag, "rstdb")])
            S.op("act", lambda e, n=n: e.activation(out=rstdb[:, 0:n], in_=rstdb[:, 0:n], func=AF.Sqrt,
                                                    bias=self.eps_cols[eps_eff], scale=1.0),
                 reads=[(tag, "rstdb")], writes=[(tag, "rstdb")])
            S.op("dve", lambda e, n=n: e.reciprocal(out=rstdb[:, 0:n], in_=rstdb[:, 0:n]),
                 reads=[(tag, "rstdb")], writes=[(tag, "rstdb")])
            S.op("dve", lambda e, n=n: e.scalar_tensor_tensor(out=meanb[:, 0:n], in0=meanb[:, 0:n], scalar=-1.0,
                                                              in1=rstdb[:, 0:n], op0=ALU.mult, op1=ALU.mult),
                 reads=[(tag, "meanb"), (tag, "rstdb")], writes=[(tag, "meanb")])
            for dc in range(KC):
                a, b_, o = t1[dc % 2], t2[dc % 2], o32[dc % 2]
                S.op("dve", lambda e, a=a, dc=dc, t0=t0, t1e=t1e, n=n: e.tensor_tensor(
                    out=a[:, 0:n], in0=z[:, dc, t0:t1e], in1=rstdb[:, 0:n], op=ALU.mult),
                    reads=[(tag, "z", dc, ti), (tag, "rstdb")], writes=[(tag, "t1", dc % 2)])
                S.op("dve", lambda e, a=a, b_=b_, n=n: e.tensor_tensor(
                    out=b_[:, 0:n], in0=a[:, 0:n], in1=meanb[:, 0:n], op=ALU.add),
                    reads=[(tag, "t1", dc % 2), (tag, "meanb")], writes=[(tag, "t2", dc % 2)])
                S.op("act", lambda e, b_=b_, o=o, dc=dc, n=n: e.activation(
                    out=o[:, 0:n], in_=b_[:, 0:n], func=AF.Identity,
                    bias=cb[:, dc:dc + 1], scale=cg[:, dc:dc + 1]),
                    reads=[(tag, "t2", dc % 2)], writes=[(tag, "o32", dc % 2)])
                if strm_out is not None:
                    wk = [(tag, "sout", dc, ti)] + ([(tag, alias_key, dc, ti)] if alias_key else [])
                    S.op("act", lambda e, b_=b_, dc=dc, t0=t0, t1e=t1e, n=n: e.activation(
                        out=strm_out[:, dc, t0:t1e], in_=b_[:, 0:n], func=AF.Identity,
                        bias=cb[:, dc:dc + 1], scale=cg[:, dc:dc + 1]),
                        reads=[(tag, "t2", dc % 2)], writes=wk)
                dst = resid_out if final_out is None else final_out
                if final_out is None or t0 < NREAL:
                    S.dma("sp", lambda e, o=o, dc=dc, t0=t0, t1e=t1e, n=n, dst=dst: e.dma_start(
                        out=dst[dc * 128:(dc + 1) * 128, t0:t1e], in_=o[:, 0:n]),
                        reads=[(tag, "o32", dc % 2)])

    def ffn_phase(self, tag, ntok, tbs, strm_in, strm_out_off, wg, wu, wd, resid, cg, cb,
                  resid_out=None, final_out=None):
        S, nc = self.S, self.nc
        ps = self.ps
        C_OFF = 33280
        B_OFF = 66560
        D_OFF = B_OFF + FC * NT * 2
        T_OFF = D_OFF + 2 * FC * 128 * 2
        hT = self.view(B_OFF, BF16, [FC, ntok])
        wgu = [self.view(C_OFF + i * 16384, BF16, [2, KC, 256]) for i in range(2)]
        wdb = [self.view(D_OFF + i * FC * 128 * 2, BF16, [FC, 128]) for i in range(2)]
        z = self.view(0, F32, [KC, ntok])
        sil = [self.view(T_OFF + 8192 + i * 2048, F32, [512]) for i in range(2)]
        strm_out = self.view(strm_out_off, BF16, [KC, ntok]) if final_out is None else None
        c_scale = 0.5 / ALPHA
        eps_eff = LN_EPS / (ALPHA * ALPHA)
        nb = len(tbs)

        for g in range(FC // 2):
            wb = wgu[g % 2]
            kb = (tag, "wgu", g % 2)
            S.dma("pool", lambda e, wb=wb, g=g: e.dma_start(out=wb[:, 0], in_=wg[g]), writes=[(kb, 0)])
            S.dma("pool", lambda e, wb=wb, g=g: e.dma_start(out=wb[:, 1], in_=wu[g]), writes=[(kb, 1)])
            for fcl in range(2):
                fc = 2 * g + fcl
                for ti, (t0, t1e) in enumerate(tbs):
                    n = t1e - t0
                    pb = (fc * nb + ti) % 2
                    pg, pu = ps[:, 2 * pb, 0:n], ps[:, 2 * pb + 1, 0:n]
                    for k in range(KC):
                        S.op("pe", lambda e, pg=pg, wb=wb, k=k, fcl=fcl, t0=t0, t1e=t1e: e.matmul(
                            pg, lhsT=wb[:, 0, k, fcl * 128:(fcl + 1) * 128], rhs=strm_in[:, k, t0:t1e],
                            start=(k == 0), stop=(k == KC - 1)),
                            reads=[(kb, 0), (tag, "sin")], writes=[("ps", 2 * pb)])
                    for k in range(KC):
                        S.op("pe", lambda e, pu=pu, wb=wb, k=k, fcl=fcl, t0=t0, t1e=t1e: e.matmul(
                            pu, lhsT=wb[:, 1, k, fcl * 128:(fcl + 1) * 128], rhs=strm_in[:, k, t0:t1e],
                            start=(k == 0), stop=(k == KC - 1)),
                            reads=[(kb, 1), (tag, "sin")], writes=[("ps", 2 * pb + 1)])
                    sb = sil[pb]
                    S.op("act", lambda e, sb=sb, pg=pg, n=n: e.activation(out=sb[:, 0:n], in_=pg, func=AF.Silu),
                         reads=[("ps", 2 * pb)], writes=[(tag, "sil", pb)])
                    S.op("dve", lambda e, sb=sb, pu=pu, n=n, fc=fc, t0=t0, t1e=t1e: e.tensor_tensor(
                        out=hT[:, fc, t0:t1e], in0=sb[:, 0:n], in1=pu, op=ALU.mult),
                        reads=[(tag, "sil", pb), ("ps", 2 * pb + 1)], writes=[(tag, "hT", fc, ti)])

        S.barrier()
        for dc in range(KC):
            wb = wdb[dc % 2]
            kb = (tag, "wd", dc % 2)
            S.dma("pool", lambda e, wb=wb, dc=dc: e.dma_start(out=wb, in_=wd[dc]), writes=[kb])
            S.dma("sp", lambda e, dc=dc: e.dma_start(out=z[:, dc, :], in_=resid[dc * 128:(dc + 1) * 128, 0:ntok]),
                  writes=[(tag, "z", dc, ti) for ti in range(nb)])
            for ti, (t0, t1e) in enumerate(tbs):
                n = t1e - t0
                pb = 4 + (dc * nb + ti) % 2
                py = ps[:, pb, 0:n]
                for f in range(FC):
                    S.op("pe", lambda e, py=py, wb=wb, f=f, t0=t0, t1e=t1e: e.matmul(
                        py, lhsT=wb[:, f, :], rhs=hT[:, f, t0:t1e], start=(f == 0), stop=(f == FC - 1)),
                        reads=[kb, (tag, "hT", f, ti)], writes=[("ps", pb)])
                S.op("dve", lambda e, py=py, dc=dc, t0=t0, t1e=t1e: e.scalar_tensor_tensor(
                    out=z[:, dc, t0:t1e], in0=py, scalar=c_scale, in1=z[:, dc, t0:t1e],
                    op0=ALU.mult, op1=ALU.add),
                    reads=[("ps", pb), (tag, "z", dc, ti)], writes=[(tag, "z", dc, ti)])
        self.ln_apply(tag, z, tbs, cg, cb, T_OFF, eps_eff, strm_out=strm_out, alias_key="hT",
                      resid_out=resid_out, final_out=final_out)
        S.barrier()

    def proj_fm(self, tag, strm, gi, wbufs, tbs, evac, banks=(0, 1), parity=[0]):
        S, ps = self.S, self.ps
        wb = wbufs[parity[0] % 2]
        kb = ("wb", parity[0] % 2)
        parity[0] += 1
        S.dma("pool", lambda e, wb=wb, gi=gi: e.dma_start(out=wb, in_=self.win[gi]), writes=[kb])
        cnt = 0
        for half in range(2):
            for ti, (t0, t1e) in enumerate(tbs):
                n = t1e - t0
                bk = banks[cnt % len(banks)]
                cnt += 1
                pv = ps[:, bk, 0:n]
                for k in range(KC):
                    S.op("pe", lambda e, pv=pv, wb=wb, k=k, half=half, t0=t0, t1e=t1e: e.matmul(
                        pv, lhsT=wb[:, k, half * 128:(half + 1) * 128], rhs=strm[:, k, t0:t1e],
                        start=(k == 0), stop=(k == KC - 1)),
                        reads=[kb, "strm"], writes=[("ps", bk)])
                evac(half, ti, t0, t1e, pv, bk)

    def proj_tm(self, tag, strm, gi, wbufs, tls, ncols, evac, banks=(0, 1), parity=[0]):
        S, ps = self.S, self.ps
        wb = wbufs[parity[0] % 2]
        kb = ("wb", parity[0] % 2)
        parity[0] += 1
        S.dma("pool", lambda e, wb=wb, gi=gi: e.dma_start(out=wb, in_=self.win[gi]), writes=[kb])
        for cnt, (i, c0, n) in enumerate(tls):
            bk = banks[cnt % len(banks)]
            pv = ps[0:n, bk, 0:ncols]
            for k in range(KC):
                S.op("pe", lambda e, pv=pv, wb=wb, k=k, c0=c0, n=n: e.matmul(
                    pv, lhsT=strm[:, k, c0:c0 + n], rhs=wb[:, k, 0:ncols],
                    start=(k == 0), stop=(k == KC - 1)),
                    reads=[kb, "strm"], writes=[("ps", bk)])
            evac(i, c0, n, pv, bk)

    def hgrn_m1(self, strm):
        S, ps, nc = self.S, self.ps, self.nc
        R1 = 99840
        wbufs = [self.view(R1 + i * 8192, BF16, [KC, 256]) for i in range(2)]
        off = [R1 + 16384]

        def alloc(dt, shape):
            n = 1
            for x in shape:
                n *= x
            nb = n * (4 if dt == F32 else 2)
            nb = (nb + 63) // 64 * 64
            v = self.view(off[0], dt, shape)
            off[0] += nb
            return v
        logf = alloc(F32, [2, NT]); Bg = alloc(F32, [2, NT]); Bsh = alloc(F32, [2, NT])
        tA = alloc(F32, [2, NT]); tB = alloc(F32, [2, NT])
        kk = alloc(BF16, [2, NT]); qs = alloc(BF16, [2, NT]); qt = alloc(BF16, [2, NT])
        kt = alloc(BF16, [2, NT]); kh64 = alloc(BF16, [2, NT]); kh128 = alloc(BF16, [2, NT])
        vv = alloc(BF16, [9, 256])
        PTs = [alloc(BF16, [2, 128]) for _ in range(2)]; khTs = [alloc(BF16, [2, 128]) for _ in range(2)]
        xs_st = alloc(BF16, [9, 256]); xa_st = alloc(F32, [9, 2])
        Qc = self.view(0, BF16, [8, NREAL]); oloc = self.view(16384, BF16, [8, NREAL])
        cv = self.cv
        lbc, omlc = self.lbc, self.omlc
        psb = lambda bk: ps[:, bk, :].bitcast(BF16)
        TL = [(i, 128 * i, 128) for i in range(8)] + [(8, NREAL, NMETA)]
        flat = lambda v: v.rearrange("p h t -> p (h t)")
        r64 = lambda v: v[:, :, 0:NREAL].rearrange("p h (c t) -> p h c t", t=64)
        r128 = lambda v: v[:, :, 0:NREAL].rearrange("p h (c t) -> p h c t", t=128)
        mt = lambda v: v[:, :, NREAL:NT]

        for hp in range(4):
            T = ("m1", hp)
            def ev_f(half, ti, t0, t1e, pv, bk, hp=hp):
                h = 2 * hp + half
                S.op("act", lambda e: e.activation(out=tA[:, half, t0:t1e], in_=pv, func=AF.Sigmoid),
                     reads=[("ps", bk)], writes=[("tA", half, ti)])
                S.op("dve", lambda e: e.tensor_scalar(out=tA[:, half, t0:t1e], in0=tA[:, half, t0:t1e],
                                                      scalar1=omlc[:, h:h + 1], scalar2=lbc[:, h:h + 1],
                                                      op0=ALU.mult, op1=ALU.add),
                     reads=[("tA", half, ti), "lb"], writes=[("tA", half, ti)])
                S.op("act", lambda e: e.activation(out=logf[:, half, t0:t1e], in_=tA[:, half, t0:t1e], func=AF.Ln),
                     reads=[("tA", half, ti)], writes=[("logf", half, ti)])
                S.op("dve", lambda e: e.tensor_scalar(out=kk[:, half, t0:t1e], in0=tA[:, half, t0:t1e],
                                                      scalar1=-1.0, scalar2=1.0, op0=ALU.mult, op1=ALU.add),
                     reads=[("tA", half, ti)], writes=[("kk", half, ti)])
            self.proj_fm(T, strm, self.gidx["hf%d" % hp], wbufs, TBS, ev_f)

            def ev_q(half, ti, t0, t1e, pv, bk):
                S.op("act", lambda e: e.activation(out=qs[:, half, t0:t1e], in_=pv, func=AF.Silu),
                     reads=[("ps", bk)], writes=[("qs", half, ti)])
            self.proj_fm(T, strm, self.gidx["hq%d" % hp], wbufs, TBS, ev_q)

            def ev_v(i, c0, n, pv, bk):
                S.op("act", lambda e: e.activation(out=vv[0:n, i, :], in_=pv, func=AF.Copy),
                     reads=[("ps", bk)], writes=[("vv", i)])
            self.proj_tm(T, strm, self.gidx["hi%d" % hp], wbufs, TL, 256, ev_v)

            allk = lambda nm: [(nm, hl, ti) for hl in range(2) for ti in range(3)]
            S.op("dve", lambda e: e.tensor_tensor_scan(out=flat(Bg), data0=flat(logf), data1=flat(logf),
                                                       initial=0.0, op0=ALU.add, op1=ALU.min),
                 reads=allk("logf"), writes=["Bg"])
            S.op("dve", lambda e: e.memset(flat(Bsh)[:, 0:1], 0.0), writes=["Bsh0"])
            S.op("act", lambda e: e.activation(out=flat(Bsh)[:, 1:2 * NT], in_=flat(Bg)[:, 0:2 * NT - 1], func=AF.Copy),
                 reads=["Bg"], writes=["Bsh"])
            S.op("dve", lambda e: e.tensor_tensor(out=r64(tB), in0=r64(Bg),
                                                  in1=r64(Bsh)[:, :, :, 0:1].to_broadcast([128, 2, 16, 64]),
                                                  op=ALU.subtract),
                 reads=["Bg", "Bsh", "Bsh0"], writes=["tBr"])
            S.op("dve", lambda e: e.tensor_tensor(out=mt(tB), in0=mt(Bg),
                                                  in1=mt(Bsh)[:, :, 0:1].to_broadcast([128, 2, NMETA]),
                                                  op=ALU.subtract),
                 reads=["Bg", "Bsh", "Bsh0"], writes=["tBm"])
            S.op("dve", lambda e: e.tensor_tensor(out=r128(tA), in0=r128(Bg),
                                                  in1=r128(Bsh)[:, :, :, 0:1].to_broadcast([128, 2, 8, 128]),
                                                  op=ALU.subtract),
                 reads=["Bg", "Bsh", "Bsh0"] + allk("tA"), writes=["tAr"] + allk("tA"))
            S.op("dve", lambda e: e.tensor_copy(out=mt(tA), in_=mt(tB)),
                 reads=["tBm"], writes=["tAm"])
            TBk, TAk = ["tBr", "tBm"], ["tAr", "tAm"] + allk("tA")
            S.op("act", lambda e: e.activation(out=flat(Bg), in_=flat(tB), func=AF.Exp),
                 reads=TBk + ["Bsh", "tAr"], writes=["Bg"])
            S.op("dve", lambda e: e.tensor_tensor(out=flat(qt), in0=flat(qs), in1=flat(Bg), op=ALU.mult),
                 reads=["Bg"] + allk("qs"), writes=["qt"])
            S.op("act", lambda e: e.activation(out=flat(Bsh), in_=flat(tB), func=AF.Exp, scale=-1.0),
                 reads=TBk + ["Bsh", "tAr", "Bsh0"], writes=["Bsh", "Bsh0"])
            S.op("dve", lambda e: e.tensor_tensor(out=flat(kt), in0=flat(kk), in1=flat(Bsh), op=ALU.mult),
                 reads=["Bsh"] + allk("kk"), writes=["kt"])
            S.op("dve", lambda e: e.tensor_tensor(out=r64(Bg), in0=r64(tB),
                                                  in1=r64(tB)[:, :, :, 63:64].to_broadcast([128, 2, 16, 64]),
                                                  op=ALU.subtract),
                 reads=TBk + ["qt"], writes=["Bg"])
            S.op("act", lambda e: e.activation(out=r64(Bg), in_=r64(Bg), func=AF.Exp, scale=-1.0),
                 reads=["Bg"], writes=["Bg"])
            S.op("dve", lambda e: e.tensor_tensor(out=r64(kh64), in0=r64(kk), in1=r64(Bg), op=ALU.mult),
                 reads=["Bg"] + allk("kk"), writes=["kh64"])
            S.op("act", lambda e: e.activation(out=flat(Bsh), in_=flat(tA), func=AF.Exp),
                 reads=TAk + ["kt"], writes=["Bsh"])
            S.op("dve", lambda e, hp=hp: e.tensor_tensor(out=Qc[:, 2 * hp:2 * hp + 2, :], in0=qs[:, :, 0:NREAL],
                                                         in1=Bsh[:, :, 0:NREAL], op=ALU.mult),
                 reads=["Bsh"] + allk("qs"), writes=[("Qc", hp)])
            S.op("dve", lambda e: e.tensor_copy(out=xa_st[:, 0:8, :].rearrange("p i h -> p h i"),
                                                in_=r128(Bsh)[:, :, :, 127]),
                 reads=["Bsh"], writes=["xa_st"])
            S.op("dve", lambda e: e.tensor_copy(out=xa_st[:, 8, :], in_=Bsh[:, :, NT - 1]),
                 reads=["Bsh"], writes=["xa_st"])
            S.op("dve", lambda e: e.tensor_tensor(out=r128(Bg), in0=r128(tA),
                                                  in1=r128(tA)[:, :, :, 127:128].to_broadcast([128, 2, 8, 128]),
                                                  op=ALU.subtract),
                 reads=TAk + ["kh64"], writes=["Bg"])
            S.op("dve", lambda e: e.tensor_tensor(out=mt(Bg), in0=mt(tA),
                                                  in1=mt(tA)[:, :, NMETA - 1:NMETA].to_broadcast([128, 2, NMETA]),
                                                  op=ALU.subtract),
                 reads=TAk + ["kh64"], writes=["Bg"])
            S.op("act", lambda e: e.activation(out=flat(Bg), in_=flat(Bg), func=AF.Exp, scale=-1.0),
                 reads=["Bg"], writes=["Bg"])
            S.op("dve", lambda e: e.tensor_tensor(out=flat(kh128), in0=flat(kk), in1=flat(Bg), op=ALU.mult),
                 reads=["Bg"] + allk("kk"), writes=["kh128"])

            def tok_block(i, c0, n, hp=hp):
                par = i % 2
                PT, khT = PTs[par], khTs[par]
                bS, bO = (2, 3) if par == 0 else (6, 7)
                t0c, s0c = par * 256, par * 256
                if n == 128:
                    for hl in range(2):
                        o0 = hl * 128
                        S.op("pe", lambda e, hl=hl, o0=o0, c0=c0: e.matmul(
                            ps[:, bS, o0:o0 + 64], lhsT=kt[:, hl, c0:c0 + 128], rhs=qt[:, hl, c0:c0 + 64],
                            start=True, stop=True), reads=["kt", "qt"], writes=[("ps", bS)])
                        S.op("pe", lambda e, hl=hl, o0=o0, c0=c0: e.matmul(
                            ps[0:64, bS, o0 + 64:o0 + 128], lhsT=kh64[:, hl, c0:c0 + 64],
                            rhs=qt[:, hl, c0 + 64:c0 + 128], start=True, stop=True),
                            reads=["kh64", "qt"], writes=[("ps", bS)])
                        S.op("pe", lambda e, hl=hl, o0=o0, c0=c0: e.matmul(
                            ps[64:128, bS, o0 + 64:o0 + 128], lhsT=kt[:, hl, c0 + 64:c0 + 128],
                            rhs=qt[:, hl, c0 + 64:c0 + 128], start=True, stop=True),
                            reads=["kt", "qt"], writes=[("ps", bS)])
                    for hl in range(2):
                        S.op("dve", lambda e, hl=hl: e.tensor_tensor(
                            out=PT[:, hl, :], in0=ps[:, bS, hl * 128:(hl + 1) * 128], in1=self.mask2, op=ALU.mult),
                            reads=[("ps", bS), "mask2"], writes=[("PT", par, hl)])
                    for hl in range(2):
                        S.op("pe", lambda e, hl=hl, i=i: e.matmul(
                            ps[:, bO, hl * 128:(hl + 1) * 128], lhsT=vv[:, i, hl * 128:(hl + 1) * 128],
                            rhs=PT[:, hl, :], start=True, stop=True),
                            reads=[("vv", i), ("PT", par, hl)], writes=[("ps", bO)])
                    S.op("act", lambda e, hp=hp, c0=c0: e.activation(
                        out=oloc[:, 2 * hp:2 * hp + 2, c0:c0 + 128],
                        in_=ps[:, bO, 0:256].rearrange("p (h t) -> p h t", h=2), func=AF.Copy),
                        reads=[("ps", bO)], writes=[("oloc", hp, i)])
                for hl in range(2):
                    S.op("pe", lambda e, hl=hl, c0=c0, n=n: e.transpose(
                        out=psb(4)[0:n, t0c * 2 + hl * 128:t0c * 2 + (hl + 1) * 128], in_=kh128[:, hl, c0:c0 + n],
                        identity=self.ident_bf),
                        reads=["kh128", "ident"], writes=[("ps4", par)])
                S.op("dve", lambda e, n=n: e.tensor_copy(out=khT[0:n].rearrange("p h d -> p (h d)"),
                                                         in_=psb(4)[0:n, t0c * 2:t0c * 2 + 256]),
                     reads=[("ps4", par)], writes=[("khT", par)])
                for hl in range(2):
                    S.op("pe", lambda e, hl=hl, i=i, n=n: e.matmul(
                        ps[:, 5, s0c + hl * 128:s0c + (hl + 1) * 128], lhsT=khT[0:n, hl, :],
                        rhs=vv[0:n, i, hl * 128:(hl + 1) * 128], start=True, stop=True),
                        reads=[("khT", par), ("vv", i)], writes=[("ps5", par)])
                S.op("dve", lambda e, i=i: e.tensor_copy(out=xs_st[:, i, :], in_=ps[:, 5, s0c:s0c + 256]),
                     reads=[("ps5", par)], writes=[("xs_st", i)])
            for (i_, c0_, n_) in TL:
                tok_block(i_, c0_, n_)
            for q3 in range(3):
                S.dma("sp", lambda e, hp=hp, q3=q3: e.dma_start(
                    out=self.xs[q3].rearrange("p (i c) -> p i c", i=3)[:, :, hp * 256:(hp + 1) * 256],
                    in_=xs_st[:, 3 * q3:3 * q3 + 3, :]),
                    reads=[("xs_st", i) for i in range(9)], writes=[("xs", hp, q3)])
            S.dma("sp", lambda e, hp=hp: e.dma_start(
                out=self.xa.rearrange("p (i c) -> p i c", i=9)[:, :, 2 * hp:2 * hp + 2], in_=xa_st),
                reads=["xa_st"], writes=[("xa", hp)])
        S.barrier()
        rg = [[0, 1, 2, 3], [4, 5, 6, 7]]
        for q3 in range(3):
            S.coll(lambda e, q3=q3: e.collective_compute("AllGather", ALU.bypass, replica_groups=rg,
                                                         ins=[self.xs[q3]], outs=[self.xg[q3]]), writes=[("xg", q3)])
        S.coll(lambda e: e.collective_compute("AllGather", ALU.bypass, replica_groups=rg,
                                              ins=[self.xa], outs=[self.xag]), writes=["xag"])

    def hgrn_m2(self, strm):
        S, ps, nc = self.S, self.ps, self.nc
        R1 = 99840
        wbufs = [self.view(R1 + i * 8192, BF16, [KC, 256]) for i in range(2)]
        off = [R1 + 16384]

        def alloc(dt, shape):
            n = 1
            for x in shape:
                n *= x
            nb = n * (4 if dt == F32 else 2)
            nb = (nb + 63) // 64 * 64
            v = self.view(off[0], dt, shape)
            off[0] += nb
            return v
        sgate = alloc(BF16, [8, NREAL])
        Scur = alloc(F32, [8, 128]); SmF = alloc(F32, [8, 128])
        SAb = [alloc(BF16, [8, 128]) for _ in range(3)]
        Aall = alloc(F32, [4, 72])
        OF = alloc(F32, [8, 128]); OSQ = alloc(F32, [8, 128]); RS = alloc(F32, [8, 128])
        Qc = self.view(0, BF16, [8, NREAL]); oloc = self.view(16384, BF16, [8, NREAL])
        yhg = self.view(32768, BF16, [8, NREAL]); Smine = self.view(49152, BF16, [8, 8, 128])
        f2 = lambda v: v.rearrange("p h t -> p (h t)")
        RTB = TBS[0:2]

        for g4 in range(4):
            def ev_g(half, ti, t0, t1e, pv, bk, g4=g4):
                h = 2 * g4 + half
                S.op("act", lambda e: e.activation(out=sgate[:, h, t0:t1e], in_=pv, func=AF.Silu),
                     reads=[("ps", bk)], writes=[("sgate", h, ti)])
                S.op("dve", lambda e: e.tensor_scalar(out=sgate[:, h, t0:t1e], in0=sgate[:, h, t0:t1e],
                                                      scalar1=self.gnc[:, h:h + 1], scalar2=None, op0=ALU.mult),
                     reads=[("sgate", h, ti), "gn"], writes=[("sgate", h, ti)])
            self.proj_fm("m2", strm, self.gidx["hg%d" % g4], wbufs, RTB, ev_g)

        def out_block(i):
            c0 = 128 * i
            for h in range(8):
                bk = 2 + h // 4
                S.op("pe", lambda e, h=h, i=i, c0=c0, bk=bk: e.matmul(
                    ps[:, bk, (h % 4) * 128:(h % 4 + 1) * 128], lhsT=Smine[:, i, h, :], rhs=Qc[:, h, c0:c0 + 128],
                    start=True, stop=True),
                    reads=[("Smine", i), "Qc"], writes=[("ps", bk)])
            S.op("dve", lambda e, c0=c0: e.tensor_tensor(
                out=OF, in0=ps[:, 2:4, :].rearrange("p a (h t) -> p (a h) t", h=4), in1=oloc[:, :, c0:c0 + 128],
                op=ALU.add),
                reads=[("ps", 2), ("ps", 3), "oloc"], writes=["OF"])
            S.op("act", lambda e: e.activation(out=f2(OSQ), in_=f2(OF), func=AF.Square),
                 reads=["OF"], writes=["OSQ"])
            for a in range(2):
                S.op("pe", lambda e, a=a: e.matmul(ps[:, 4 + a, :], lhsT=self.ones128, rhs=f2(OSQ)[:, a * 512:(a + 1) * 512],
                                                   start=True, stop=True),
                     reads=["OSQ", "ones128"], writes=[("ps", 4 + a)])
            S.op("act", lambda e: e.activation(out=f2(RS), in_=ps[:, 4:6, :].rearrange("p a b -> p (a b)"),
                                               func=AF.Ln, bias=self.eps_rms, scale=1.0),
                 reads=[("ps", 4), ("ps", 5), "eps"], writes=["RS"])
            S.op("act", lambda e: e.activation(out=f2(RS), in_=f2(RS), func=AF.Exp, scale=-0.5),
                 reads=["RS"], writes=["RS"])

        def out_block_b(i):
            c0 = 128 * i
            S.op("dve", lambda e: e.tensor_tensor(out=f2(OF), in0=f2(OF), in1=f2(RS), op=ALU.mult),
                 reads=["OF", "RS"], writes=["OF"])
            S.op("dve", lambda e, c0=c0: e.tensor_tensor(out=yhg[:, :, c0:c0 + 128], in0=OF, in1=sgate[:, :, c0:c0 + 128],
                                                         op=ALU.mult),
                 reads=["OF"] + [("sgate", h, c0 // 512) for h in range(8)], writes=[("yhg", i)])

        S.dma("sp", lambda e: e.dma_start(out=Aall, in_=self.xag.rearrange("(r p) c -> p r c", p=128)),
              reads=["xag"], writes=["Aall"])
        xg3 = [x_.rearrange("(r p) (i c) -> r p i c", p=128, i=3) for x_ in self.xg]
        S.dma("sp", lambda e: e.dma_start(out=f2(SAb[2]), in_=xg3[2][0, :, 2, :]), reads=[("xg", 2)],
              writes=[("SAb", 2)])
        S.op("dve", lambda e: e.tensor_copy(out=f2(Scur), in_=f2(SAb[2])), reads=[("SAb", 2)], writes=["Scur"])
        for g in range(32):
            r, i = g % 4, g // 4
            sb = SAb[g % 3]
            S.dma("sp", lambda e, sb=sb, r=r, i=i: e.dma_start(out=f2(sb), in_=xg3[i // 3][r, :, i % 3, :]),
                  reads=[("xg", i // 3)], writes=[("SAb", g % 3)])
            if r == 0:
                S.op("dve", lambda e: e.tensor_scalar(out=f2(SmF), in0=f2(Scur), scalar1=self.selc[:, 0:1],
                                                      scalar2=None, op0=ALU.mult),
                     reads=["Scur", "sel"], writes=["SmF"])
            else:
                dst = SmF if r < 3 else Smine[:, i]
                S.op("dve", lambda e, r=r, dst=dst: e.scalar_tensor_tensor(
                    out=f2(dst), in0=f2(Scur), scalar=self.selc[:, r:r + 1], in1=f2(SmF),
                    op0=ALU.mult, op1=ALU.add),
                    reads=["Scur", "sel", "SmF"], writes=(["SmF"] if r < 3 else [("Smine", i)]))
            if g < 31:
                for h in range(8):
                    S.op("dve", lambda e, h=h, r=r, i=i, sb=sb: e.scalar_tensor_tensor(
                        out=Scur[:, h, :], in0=Scur[:, h, :], scalar=Aall[:, r, i * 8 + h:i * 8 + h + 1],
                        in1=sb[:, h, :], op0=ALU.mult, op1=ALU.add),
                        reads=["Scur", "Aall", ("SAb", g % 3)], writes=["Scur"])
            if g % 4 == 3:
                out_block(g // 4)
            if g % 4 == 1 and g >= 5:
                out_block_b((g - 5) // 4)
        out_block_b(7)

        S.barrier()

    def attn_m3(self, strm, part):
        S, ps, nc = self.S, self.ps, self.nc
        R1 = 99840
        psb = lambda bk: ps[:, bk, :].bitcast(BF16)
        K_all = self.view(R1, BF16, [2, 4112])
        V_all = self.view(R1 + 16448, BF16, [33, 258])
        IK_all = self.view(R1 + 33536, BF16, [4096])
        AugK = self.view(R1 + 41728, BF16, [4112])
        qT = self.view(R1 + 70656, BF16, [8, NREAL])
        iqT = self.view(R1 + 87040, BF16, [8, NREAL])
        sc = self.view(R1 + 49952, F32, [4096])
        wbufs = [self.view(R1 + i * 8192, BF16, [KC, 256]) for i in range(2)]
        Dg = self.view(R1 + 66336, BF16, [16, 128])
        yatt = self.view(0, BF16, [8, NREAL])
        mb = self.view(16384, BF16, [4096])
        mbT = self.view(24576, BF16, [32, 128])
        junk = self.view(49152, U8, [4096])
        iqz = self.view(49152 + 4096, BF16, [16, 128])
        rh = [self.view(57344 + q * 1024, BF16, [512]) for q in range(4)]
        ya = self.view(61440, BF16, [8, 128])
        PTb = [self.view(61440 + q * 2048, BF16, [1024]) for q in range(2)]
        cbt = self.view(65536, BF16, [4, 128])
        kst = self.view(32768, BF16, [2, NREAL])
        vst = self.view(32768 + 4096, BF16, [8, 258])
        ikst = self.view(32768 + 4096 + 4160, BF16, [NREAL])
        iktmp = self.view(32768 + 10304, F32, [64])
        ikn2 = self.view(32768 + 10304 + 256, BF16, [128])
        kmst = self.view(32768 + 10816, BF16, [2, NMETA])
        vmst = self.view(32768 + 10880, BF16, [258])
        cst = self.cst
        AugQ = cst[:, 664:1176].bitcast(BF16)
        AugR = cst[:, 1176:1688].bitcast(BF16)
        wq = cst[:, 1688:1816].rearrange("p (i h) -> p i h", h=16)
        H = cst[:, 1816:1848]
        Pt = cst[:, 1848:1976]
        g1 = cst[:, 1976:2008]
        mrow = cst[:, 2008:2016]; cc = cst[:, 2016:2024]; rs = cst[:, 2024:2032]
        Bt = cst[:, 2032:2033]; Wc = cst[:, 2033:2034]; mid = cst[:, 2034:2035]; cnt = cst[:, 2035:2036]
        u2 = cst[:, 2036:2037]; tau = cst[:, 2037:2038]; rstd1 = cst[:, 2038:2039]
        nslope = cst[:, 2040:2048]
        Qt = cst[:, 2048:2080].rearrange("p (r j) -> p r j", r=4)
        kmx = cst[:, 2080:2081]
        pw = cst[:, 2104:2136]
        gik = cst[:, 2136:2200]; bik = cst[:, 2200:2264]
        st6 = cst[:, 2264:2270]; mv = cst[:, 2270:2272]
        TL = [(i, 128 * i, 128) for i in range(8)] + [(8, NREAL, NMETA)]
        RTB = TBS[0:2]
        NB = 16

        if part == 0:
            S.dma("sp", lambda e: e.dma_start(out=cst[:, 2040:2264], in_=self.catt), writes=["catt"])
            S.dma("sp", lambda e: e.dma_start(out=Pt, in_=self.cmat_d[:, 384:512]), writes=["Pt"])
            S.op("dve", lambda e: e.memset(vst.rearrange("p i (k c) -> p i k c", k=2)[:, :, :, 128:129], 1.0),
                 writes=["vst1"])
            S.op("dve", lambda e: e.memset(vmst.rearrange("p (k c) -> p k c", k=2)[:, :, 128:129], 1.0),
                 writes=["V1"])

            def ev_k(half, ti, t0, t1e, pv, bk):
                if ti < 2:
                    S.op("act", lambda e: e.activation(out=kst[:, half, t0:t1e], in_=pv, func=AF.Copy),
                         reads=[("ps", bk)], writes=[("kst", half, ti)])
                else:
                    S.op("act", lambda e: e.activation(out=kmst[:, half, :], in_=pv, func=AF.Copy),
                         reads=[("ps", bk)], writes=[("Kmeta", half)])
            self.proj_fm("m3", strm, self.gidx["ak"], wbufs, TBS, ev_k)

            def ev_v(i, c0, n, pv, bk):
                src = pv.rearrange("p (k c) -> p k c", k=2)
                if i < 8:
                    dst = vst[:, i, :].rearrange("p (k c) -> p k c", k=2)[:, :, 0:128]
                    S.op("act", lambda e: e.activation(out=dst, in_=src, func=AF.Copy),
                         reads=[("ps", bk), "vst1"], writes=[("vst", i)])
                else:
                    dst = vmst[0:n, :].rearrange("p (k c) -> p k c", k=2)[:, :, 0:128]
                    S.op("act", lambda e: e.activation(out=dst, in_=src, func=AF.Copy),
                         reads=[("ps", bk), "V1"], writes=["Vmeta"])
            self.proj_tm("m3", strm, self.gidx["av"], wbufs, TL, 256, ev_v)

            def ev_ik(i, c0, n, pv, bk):
                S.op("dve", lambda e: e.bn_stats(out=st6, in_=pv[:, 0:64]), reads=[("ps", bk)], writes=["st6"])
                S.op("dve", lambda e: e.bn_aggr(out=mv, in_=st6), reads=["st6"], writes=["mv"])
                S.op("act", lambda e: e.activation(out=rstd1, in_=mv[:, 1:2], func=AF.Sqrt, bias=self.eps_ik, scale=1.0),
                     reads=["mv", "eps"], writes=["rstd1"])
                S.op("dve", lambda e: e.reciprocal(out=rstd1, in_=rstd1), reads=["rstd1"], writes=["rstd1"])
                S.op("dve", lambda e: e.tensor_scalar(out=iktmp, in0=pv[:, 0:64], scalar1=mv[:, 0:1], scalar2=rstd1,
                                                      op0=ALU.subtract, op1=ALU.mult),
                     reads=[("ps", bk), "mv", "rstd1"], writes=["iktmp"])
                S.op("dve", lambda e: e.tensor_tensor(out=iktmp, in0=iktmp, in1=gik, op=ALU.mult),
                     reads=["iktmp", "catt"], writes=["iktmp"])
                S.op("dve", lambda e: e.tensor_tensor(out=ikn2[:, 0:64], in0=iktmp, in1=bik, op=ALU.add),
                     reads=["iktmp", "catt"], writes=["ikn2a"])
                S.op("dve", lambda e: e.tensor_copy(out=ikn2[:, 64:128], in_=ikn2[:, 0:64]),
                     reads=["ikn2a"], writes=["ikn2b"])
                S.op("act", lambda e, i=i: e.activation(out=wq[:, i, :], in_=pv[:, 64:80], func=AF.Copy,
                                                        scale=0.25 * 0.125),
                     reads=[("ps", bk)], writes=[("wq", i)])
                S.op("pe", lambda e: e.transpose(out=psb(2)[:, 0:128], in_=ikn2, identity=self.ident_bf),
                     reads=["ikn2a", "ikn2b", "ident"], writes=[("ps", 2)])
                S.op("act", lambda e, c0=c0: e.activation(out=ikst[:, c0:c0 + 128], in_=psb(2)[:, 0:128], func=AF.Copy),
                     reads=[("ps", 2)], writes=[("ikst", i)])
            self.proj_tm("m3", strm, self.gidx["ikw"], wbufs, TL[0:8], 80, ev_ik)

            S.dma("sp", lambda e: e.dma_start(out=self.ks.rearrange("p (k t) -> p k t", k=2), in_=kst),
                  reads=[("kst", hh, ti) for hh in range(2) for ti in range(2)], writes=["ks"])
            S.dma("sp", lambda e: e.dma_start(out=self.vs[:, 0:2064].rearrange("p (i c) -> p i c", i=8), in_=vst),
                  reads=[("vst", i) for i in range(8)] + ["vst1"], writes=["vs"])
            S.dma("sp", lambda e: e.dma_start(out=self.vs[:, 2064:3088], in_=ikst),
                  reads=[("ikst", i) for i in range(8)], writes=["vs2"])
            S.dma("sp", lambda e: e.dma_start(out=self.kms.rearrange("p (k t) -> p k t", k=2), in_=kmst),
                  reads=[("Kmeta", 0), ("Kmeta", 1)], writes=["kms"])
            S.dma("sp", lambda e: e.dma_start(out=self.vms, in_=vmst[0:NMETA, :]),
                  reads=["Vmeta", "V1"], writes=["vms"])
            S.barrier()
            rg = [[0, 1, 2, 3], [4, 5, 6, 7]]
            S.coll(lambda e: e.collective_compute("AllGather", ALU.bypass, replica_groups=rg,
                                                  ins=[self.ks], outs=[self.kg]), writes=["kg"])
            S.coll(lambda e: e.collective_compute("AllGather", ALU.bypass, replica_groups=rg,
                                                  ins=[self.vs], outs=[self.vg]), writes=["vg"])
            return

        if part == 1:
            for g4 in range(4):
                def ev_q(half, ti, t0, t1e, pv, bk, g4=g4):
                    h = 2 * g4 + half
                    S.op("act", lambda e: e.activation(out=qT[:, h, t0:t1e], in_=pv, func=AF.Copy, scale=128.0 ** -0.5),
                         reads=[("ps", bk)], writes=[("qT", h, ti)])
                self.proj_fm("m3", strm, self.gidx["aq%d" % g4], wbufs, RTB, ev_q)
            for g4 in range(4):
                def ev_iq(half, ti, t0, t1e, pv, bk, g4=g4):
                    h = 2 * g4 + half
                    S.op("dve", lambda e: e.tensor_copy(out=iqT[:, h, t0:t1e], in_=pv),
                         reads=[("ps", bk)], writes=[("iqT", h, ti)])
                self.proj_fm("m3", strm, self.gidx["iq%d" % g4], wbufs, RTB, ev_iq)
            return

        S.op("pool", lambda e: e.memset(AugK[0:65, :], 0.0), writes=["AugK"])
        S.op("pool", lambda e: e.memset(AugR[0:65, :], 0.0), writes=["AugR"])
        for rr in range(3):
            S.dma("pool", lambda e, rr=rr: e.dma_start(out=AugK[32 * rr:32 * rr + 1, :], in_=self.augk[rr:rr + 1, :]),
                  writes=["AugK"])
        for rr in range(2):
            S.dma("pool", lambda e, rr=rr: e.dma_start(out=AugR[32 * rr:32 * rr + 1, :], in_=self.augs[rr:rr + 1, :]),
                  writes=["AugR"])
        S.dma("pool", lambda e: e.dma_start(out=cbt, in_=self.cbt_d.rearrange("p (r s) -> p r s", r=4)), writes=["cbt"])
        for r in range(4):
            S.dma("sp", lambda e, r=r: e.dma_start(
                out=K_all[:, :, r * 1024:(r + 1) * 1024],
                in_=self.kg[r * 128:(r + 1) * 128, :].rearrange("p (k t) -> p k t", k=2)),
                reads=["kg"], writes=["K_all"])
            S.dma("sp", lambda e, r=r: e.dma_start(
                out=V_all[:, r * 8:(r + 1) * 8, :],
                in_=self.vg[r * 128:(r + 1) * 128, 0:2064].rearrange("p (i c) -> p i c", i=8)),
                reads=["vg"], writes=["V_all"])
            S.dma("sp", lambda e, r=r: e.dma_start(
                out=IK_all[:, r * 1024:(r + 1) * 1024], in_=self.vg[r * 128:(r + 1) * 128, 2064:3088]),
                reads=["vg"], writes=["IK_all"])
        S.dma("sp", lambda e: e.dma_start(out=K_all[:, :, 4096:4112], in_=self.kms.rearrange("p (k t) -> p k t", k=2)),
              reads=["kms"], writes=[("Kmeta", 0), ("Kmeta", 1)])
        S.dma("sp", lambda e: e.dma_start(out=V_all[0:NMETA, 32, :], in_=self.vms), reads=["vms"], writes=["Vmeta"])
        S.barrier()

        S.op("pool", lambda e: e.memset(iqz.rearrange("p h t -> p (h t)"), 0.0), writes=["iqz"])
        sc4 = sc.rearrange("p (r c) -> p r c", r=4)
        mb4 = mb.rearrange("p (r c) -> p r c", r=4)
        jk4 = junk.rearrange("p (r c) -> p r c", r=4)
        def geom(i):
            q0 = 128 * i
            nk = 128 * (i + 1)
            pieces = [(r, c0, min(512, nk - c0)) for r in range(4) for c0 in range(0, nk, 512)]
            return q0, nk, pieces

        def st_idx(i):
            q0, nk, pieces = geom(i)
            for h in range(16):
                S.op("act", lambda e, h=h: e.activation(out=Dg[:, h, :], in_=self.ident_bf, func=AF.Copy,
                                                        scale=wq[:, i, h:h + 1]),
                     reads=["ident", ("wq", i)], writes=["Dg"])
            for h in range(16):
                hb = h % 2
                eng = "act"
                if eng == "pool":
                    S.op("pool", lambda e, h=h, hb=hb: e.tensor_copy(
                        out=iqz[hb * 64:(hb + 1) * 64, h, :], in_=iqT[hb * 64:(hb + 1) * 64, h // 2, q0:q0 + 128]),
                        reads=["iqT", "iqz"], writes=[("iqzh", h)])
                else:
                    S.op("act", lambda e, h=h, hb=hb: e.activation(
                        out=iqz[hb * 64:(hb + 1) * 64, h, :], in_=iqT[hb * 64:(hb + 1) * 64, h // 2, q0:q0 + 128],
                        func=AF.Copy),
                        reads=["iqT", "iqz"], writes=[("iqzh", h)])
            for pi, (r, c0, cn) in enumerate(pieces):
                col0 = r * 1024 + c0
                accb = 4 + pi % 2

                def head_mm(h, cn=cn, col0=col0):
                    bk, hb = h % 4, h % 2
                    S.op("pe", lambda e: e.matmul(
                        ps[:, bk, 0:cn], lhsT=iqz[:, h, :],
                        rhs=IK_all[:, col0:col0 + cn], start=True, stop=True),
                        reads=["IK_all", ("iqzh", h)], writes=[("ps", bk)])
                    if h % 2 == 0:
                        S.op("act", lambda e: e.activation(out=rh[bk][:, 0:cn], in_=ps[:, bk, 0:cn], func=AF.Relu),
                             reads=[("ps", bk)], writes=[("rh", bk)])
                    else:
                        S.op("dve", lambda e: e.tensor_scalar(out=rh[bk][:, 0:cn], in0=ps[:, bk, 0:cn], scalar1=0.0,
                                                              scalar2=None, op0=ALU.max),
                             reads=[("ps", bk)], writes=[("rh", bk)])

                def head_acc(h, cn=cn, accb=accb):
                    bk = h % 4
                    S.op("pe", lambda e: e.matmul(
                        ps[:, accb, 0:cn], lhsT=Dg[:, h, :], rhs=rh[bk][:, 0:cn], start=(h == 0), stop=(h == 15)),
                        reads=["Dg", ("rh", bk)], writes=[("ps", accb)])
                for h in range(16):
                    head_mm(h)
                    if h >= 2:
                        head_acc(h - 2)
                head_acc(14)
                head_acc(15)
                S.op("act", lambda e, accb=accb, col0=col0, cn=cn: e.activation(
                    out=sc[:, col0:col0 + cn], in_=ps[:, accb, 0:cn], func=AF.Copy),
                    reads=[("ps", accb)], writes=["sc"])

        def st_bis(i):
            q0, nk, pieces = geom(i)
            scv, mbv, jkv = sc4[:, :, 0:nk], mb4[:, :, 0:nk], jk4[:, :, 0:nk]
            S.op("dve", lambda e: e.reduce_max(out=Bt, in_=scv, axis=AX.XY, apply_absolute_value=True),
                 reads=["sc"], writes=["Bt"])
            S.op("dve", lambda e: e.tensor_tensor(out=sc4[:, :, q0:q0 + 128], in0=sc4[:, :, q0:q0 + 128], in1=cbt,
                                                  op=ALU.add),
                 reads=["sc", "cbt", "Bt"], writes=["sc"])
            S.op("dve", lambda e: e.tensor_scalar(out=Wc, in0=Bt, scalar1=2.0002, scalar2=1e-6,
                                                  op0=ALU.mult, op1=ALU.add), reads=["Bt"], writes=["Wc"])
            S.op("dve", lambda e: e.tensor_scalar(out=H[:, 0:NB + 1], in0=pw[:, 0:NB + 1], scalar1=Wc, scalar2=None,
                                                  op0=ALU.mult), reads=["Wc", "catt"], writes=["H"])
            S.op("dve", lambda e: e.memset(mid, 0.0), writes=["mid"])
            for k in range(NB):
                S.op("dve", lambda e: e.tensor_scalar(
                    out=jkv, in0=scv, scalar1=mid, scalar2=0.0, op0=ALU.is_ge, op1=ALU.add, accum_out=cnt),
                    reads=["sc", "mid"], writes=["junk", "cnt"])
                S.op("dve", lambda e, k=k: e.tensor_scalar(out=u2, in0=cnt, scalar1=256.0, scalar2=H[:, k:k + 1],
                                                           op0=ALU.is_ge, op1=ALU.mult),
                     reads=["cnt", "H"], writes=["u2"])
                S.op("dve", lambda e, k=k: e.scalar_tensor_tensor(out=mid, in0=mid, scalar=H[:, k + 1:k + 2], in1=u2,
                                                                  op0=ALU.subtract, op1=ALU.add),
                     reads=["mid", "H", "u2"], writes=["mid"])
            S.op("dve", lambda e: e.tensor_tensor(out=tau, in0=mid, in1=H[:, NB:NB + 1], op=ALU.subtract),
                 reads=["mid", "H"], writes=["tau"])
            S.op("dve", lambda e: e.tensor_scalar(
                out=mbv, in0=scv, scalar1=tau, scalar2=-30000.0, op0=ALU.is_lt, op1=ALU.mult),
                reads=["sc", "tau"], writes=["mb"])
            nb_ = i + 1
            sc5 = scv.rearrange("p r (j s) -> p r j s", s=128)
            mb5 = mbv.rearrange("p r (j s) -> p r j s", s=128)
            S.op("dve", lambda e: e.tensor_tensor(
                out=sc5, in0=mb5, in1=Pt.unsqueeze(1).unsqueeze(1).to_broadcast([128, 4, nb_, 128]), op=ALU.add),
                reads=["mb", "Pt", "sc"], writes=["sc"])
            g1v = g1.rearrange("p (r j) -> p r j", r=4)[:, :, 0:nb_]
            S.op("dve", lambda e: e.tensor_reduce(out=g1v, in_=sc5, axis=AX.X, op=ALU.max),
                 reads=["sc"], writes=["g1"])
            S.op("dve", lambda e: e.tensor_tensor(out=g1v, in0=g1v, in1=Qt[:, :, 0:nb_], op=ALU.add),
                 reads=["g1", "catt"], writes=["g1"])
            S.op("dve", lambda e: e.tensor_reduce(out=kmx, in_=g1v, axis=AX.XY, op=ALU.max),
                 reads=["g1"], writes=["kmx"])
            S.op("dve", lambda e: e.tensor_scalar(out=kmx, in0=kmx, scalar1=15.0, scalar2=None, op0=ALU.max),
                 reads=["kmx"], writes=["kmx"])
            S.op("dve", lambda e: e.tensor_scalar(out=cc, in0=nslope, scalar1=kmx, scalar2=None, op0=ALU.mult),
                 reads=["kmx", "catt"], writes=["cc"])

        def st_mbT(i):
            kts = [(r, ip) for r in range(4) for ip in range(i + 1)]
            for g0 in range(0, len(kts), 8):
                grp = kts[g0:g0 + 8]
                bk = 6 + (g0 // 8) % 2
                for s_, (r, ip) in enumerate(grp):
                    S.op("pe", lambda e, bk=bk, s_=s_, r=r, ip=ip: e.transpose(
                        out=psb(bk)[:, s_ * 128:(s_ + 1) * 128], in_=mb[:, r * 1024 + ip * 128:r * 1024 + ip * 128 + 128],
                        identity=self.ident_bf),
                        reads=["mb", "ident"], writes=[("ps", bk)])
                for s_, (r, ip) in enumerate(grp):
                    S.op("act", lambda e, bk=bk, s_=s_, r=r, ip=ip: e.activation(
                        out=mbT[:, r * 8 + ip, :], in_=psb(bk)[:, s_ * 128:(s_ + 1) * 128], func=AF.Copy),
                        reads=[("ps", bk)], writes=["mbT"])

        def st_passA(i):
            q0, nk, pieces = geom(i)
            S.op("dve", lambda e: e.memset(ps[:, 5:8, :].rearrange("p a b -> p (a b)"), 0.0),
                 writes=[("ps", 5), ("ps", 6), ("ps", 7)])
            Dc = PTb[1].rearrange("p (h t) -> p h t", h=8)
            for h in range(8):
                S.op("dve", lambda e, h=h: e.tensor_scalar(out=Dc[:, h, :], in0=self.ident_bf, scalar1=cc[:, h:h + 1],
                                                           scalar2=None, op0=ALU.mult),
                     reads=["ident", "cc"], writes=[("PTb", 1)])
            for a_ in range(2):
                S.op("pe", lambda e, a_=a_: e.matmul(ps[:, 4, :], lhsT=self.ones_bf,
                                                     rhs=PTb[1][:, a_ * 512:(a_ + 1) * 512],
                                                     start=True, stop=True),
                     reads=[("PTb", 1), "ones_bf"], writes=[("ps", 4)])
                S.op("dve", lambda e, a_=a_: e.tensor_copy(out=AugR[64:65, a_ * 512:(a_ + 1) * 512], in_=ps[64:65, 4, :]),
                     reads=[("ps", 4)], writes=["AugR"])

        def st_passB(i):
            q0, nk, pieces = geom(i)
            ktl = [(r * 1024 + ip * 128, r * 8 + ip, 128) for r in range(4) for ip in range(i + 1)] + [(4096, 32, NMETA)]
            Oreg = lambda h: ps[:, 5 + h // 3, (h % 3) * 129:(h % 3 + 1) * 129]

            def logits(qi):
                col0, vt, n = ktl[qi]
                meta = (n == NMETA)
                pair = (0, 1) if qi % 2 == 0 else (2, 3)
                for h in range(8):
                    kvh = h // 4
                    out = ps[0:n, pair[h // 4], (h % 4) * 128:(h % 4 + 1) * 128]
                    S.op("pe", lambda e, out=out, kvh=kvh, h=h: e.matmul(
                        out, lhsT=K_all[:, kvh, col0:col0 + n], rhs=qT[:, h, q0:q0 + 128], start=True, stop=False),
                        reads=["K_all", "qT", ("Kmeta", kvh)], writes=[("ps", pair[h // 4])])
                    S.op("pe", lambda e, out=out, h=h: e.matmul(
                        out, lhsT=AugK[0:65, col0:col0 + n], rhs=AugR[0:65, h * 128:(h + 1) * 128],
                        start=False, stop=meta),
                        reads=["AugK", "AugR"], writes=[("ps", pair[h // 4])])
                    if not meta:
                        S.op("pe", lambda e, out=out: e.matmul(
                            out, lhsT=self.ident_bf, rhs=mbT[:, vt, :], start=False, stop=True),
                            reads=["mbT", "ident"], writes=[("ps", pair[h // 4])])
                pt = PTb[qi % 2]
                S.op("act", lambda e: e.activation(
                    out=pt[0:n, :], in_=ps[0:n, pair[0]:pair[0] + 2, :].rearrange("p a b -> p (a b)"), func=AF.Exp),
                    reads=[("ps", pair[0]), ("ps", pair[1])], writes=[("PTb", qi % 2)])

            def pv(qi):
                col0, vt, n = ktl[qi]
                pt = PTb[qi % 2]
                for h in range(8):
                    kvh = h // 4
                    S.op("pe", lambda e, h=h, kvh=kvh: e.matmul(
                        Oreg(h), lhsT=pt[0:n, h * 128:(h + 1) * 128], rhs=V_all[0:n, vt, kvh * 129:(kvh + 1) * 129],
                        start=False, stop=(qi == len(ktl) - 1)),
                        reads=[("PTb", qi % 2), "V_all", "Vmeta"], writes=[("ps", 5 + h // 3)])
            logits(0)
            for qi in range(len(ktl)):
                if qi + 1 < len(ktl):
                    logits(qi + 1)
                pv(qi)

        def st_fin(i):
            q0 = 128 * i
            for b3 in range(3):
                nh = 3 if b3 < 2 else 2
                Ov = ps[:, 5 + b3, 0:nh * 129].rearrange("p (h c) -> p h c", c=129)
                S.op("dve", lambda e, Ov=Ov, b3=b3, nh=nh: e.reciprocal(out=rs[:, 3 * b3:3 * b3 + nh], in_=Ov[:, :, 128]),
                     reads=[("ps", 5 + b3)], writes=[("rs", b3)])
                S.op("dve", lambda e, Ov=Ov, b3=b3, nh=nh: e.tensor_tensor(
                    out=ya[:, 3 * b3:3 * b3 + nh, :], in0=Ov[:, :, 0:128],
                    in1=rs[:, 3 * b3:3 * b3 + nh].unsqueeze(2).to_broadcast([128, nh, 128]), op=ALU.mult),
                    reads=[("ps", 5 + b3), ("rs", b3)], writes=[("ya", b3), ("PTb", 0)])
            for h in range(8):
                S.op("pe", lambda e, h=h: e.transpose(out=psb(4)[:, h * 128:(h + 1) * 128], in_=ya[:, h, :],
                                                      identity=self.ident_bf),
                     reads=[("ya", h // 3), ("PTb", 0), "ident"], writes=[("ps", 4)])
            S.op("act", lambda e: e.activation(out=yatt[:, :, q0:q0 + 128],
                                               in_=psb(4).rearrange("p (h t) -> p h t", h=8), func=AF.Copy),
                 reads=[("ps", 4)], writes=[("yatt", i)])

        st_idx(0)
        st_bis(0)
        st_mbT(0)
        for i in range(8):
            if i + 1 < 8:
                st_idx(i + 1)
            st_passA(i)
            if i + 1 < 8:
                st_bis(i + 1)
            st_passB(i)
            st_fin(i)
            if i + 1 < 8:
                st_mbT(i + 1)
        S.barrier()

    def merge_m4(self, strm, cg, cb):
        S, ps, nc = self.S, self.ps, self.nc
        R1 = 99840
        RTB = TBS[0:2]
        yatt = self.view(0, BF16, [8, NREAL]); yhg = self.view(32768, BF16, [8, NREAL])
        merged = self.view(R1, BF16, [KC, NREAL])
        o = R1 + 32768
        wga = [self.view(o + q * 8192, BF16, [KC, 256]) for q in range(2)]
        wgh = [self.view(o + 16384 + q * 8192, BF16, [KC, 256]) for q in range(2)]
        wba = [self.view(o + 32768 + q * 4096, BF16, [8, 256]) for q in range(2)]
        wbh = [self.view(o + 40960 + q * 4096, BF16, [8, 256]) for q in range(2)]
        tm = [self.view(o + 49152 + q * 2048, F32, [512]) for q in range(4)]
        for mg in range(8):
            q = mg % 2
            S.dma("pool", lambda e, q=q, mg=mg: e.dma_start(out=wga[q], in_=self.win[self.gidx["ga%d" % mg]]),
                  writes=[("wga", q)])
            S.dma("pool", lambda e, q=q, mg=mg: e.dma_start(out=wgh[q], in_=self.win[self.gidx["gh%d" % mg]]),
                  writes=[("wgh", q)])
            S.dma("pool", lambda e, q=q, mg=mg: e.dma_start(out=wba[q], in_=self.wba_d[mg]), writes=[("wba", q)])
            S.dma("pool", lambda e, q=q, mg=mg: e.dma_start(out=wbh[q], in_=self.wbh_d[mg]), writes=[("wbh", q)])
            for half in range(2):
                mc = 2 * mg + half
                hs = slice(half * 128, (half + 1) * 128)
                for ti, (t0, t1e) in enumerate(RTB):
                    pp = (half * 2 + ti) % 2
                    b0 = 4 * pp
                    for k in range(KC):
                        S.op("pe", lambda e, b0=b0, q=q, k=k, hs=hs, t0=t0, t1e=t1e: e.matmul(
                            ps[:, b0, :], lhsT=wga[q][:, k, hs], rhs=strm[:, k, t0:t1e],
                            start=(k == 0), stop=(k == KC - 1)),
                            reads=[("wga", q), "strm"], writes=[("ps", b0)])
                    for k in range(8):
                        S.op("pe", lambda e, b0=b0, q=q, k=k, hs=hs, t0=t0, t1e=t1e: e.matmul(
                            ps[:, b0 + 1, :], lhsT=wba[q][:, k, hs], rhs=yatt[:, k, t0:t1e],
                            start=(k == 0), stop=(k == 7)),
                            reads=[("wba", q), "yatt"], writes=[("ps", b0 + 1)])
                    for k in range(KC):
                        S.op("pe", lambda e, b0=b0, q=q, k=k, hs=hs, t0=t0, t1e=t1e: e.matmul(
                            ps[:, b0 + 2, :], lhsT=wgh[q][:, k, hs], rhs=strm[:, k, t0:t1e],
                            start=(k == 0), stop=(k == KC - 1)),
                            reads=[("wgh", q), "strm"], writes=[("ps", b0 + 2)])
                    for k in range(8):
                        S.op("pe", lambda e, b0=b0, q=q, k=k, hs=hs, t0=t0, t1e=t1e: e.matmul(
                            ps[:, b0 + 3, :], lhsT=wbh[q][:, k, hs], rhs=yhg[:, k, t0:t1e],
                            start=(k == 0), stop=(k == 7)),
                            reads=[("wbh", q), "yhg"], writes=[("ps", b0 + 3)])
                    ta, th = tm[2 * pp], tm[2 * pp + 1]
                    S.op("act", lambda e, ta=ta, b0=b0: e.activation(out=ta, in_=ps[:, b0, :], func=AF.Sigmoid),
                         reads=[("ps", b0)], writes=[("tm", 2 * pp)])
                    S.op("dve", lambda e, ta=ta, b0=b0: e.tensor_tensor(out=ta, in0=ta, in1=ps[:, b0 + 1, :], op=ALU.mult),
                         reads=[("tm", 2 * pp), ("ps", b0 + 1)], writes=[("tm", 2 * pp)])
                    S.op("act", lambda e, th=th, b0=b0: e.activation(out=th, in_=ps[:, b0 + 2, :], func=AF.Sigmoid),
                         reads=[("ps", b0 + 2)], writes=[("tm", 2 * pp + 1)])
                    S.op("dve", lambda e, th=th, b0=b0: e.tensor_tensor(out=th, in0=th, in1=ps[:, b0 + 3, :], op=ALU.mult),
                         reads=[("tm", 2 * pp + 1), ("ps", b0 + 3)], writes=[("tm", 2 * pp + 1)])
                    S.op("dve", lambda e, ta=ta, th=th, mc=mc, t0=t0, t1e=t1e: e.tensor_tensor(
                        out=merged[:, mc, t0:t1e], in0=ta, in1=th, op=ALU.add),
                        reads=[("tm", 2 * pp), ("tm", 2 * pp + 1)], writes=[("merged", mc, ti)])
        S.barrier()
        z = self.view(R1 + 32768, F32, [KC, NREAL])
        wo = [self.view(53760 + q * 4096, BF16, [KC, 128]) for q in range(2)]
        strm_out = self.view(0, BF16, [KC, NREAL])
        for dc in range(KC):
            q = dc % 2
            S.dma("pool", lambda e, q=q, dc=dc: e.dma_start(out=wo[q], in_=self.wo_d[dc]), writes=[("wo", q)])
            S.dma("sp", lambda e, dc=dc: e.dma_start(out=z[:, dc, :], in_=self.h1s[dc * 128:(dc + 1) * 128, 0:NREAL]),
                  writes=[("l2", "z", dc, ti) for ti in range(2)])
            for ti, (t0, t1e) in enumerate(RTB):
                pb = (dc * 2 + ti) % 2
                for k in range(KC):
                    S.op("pe", lambda e, pb=pb, q=q, k=k, t0=t0, t1e=t1e: e.matmul(
                        ps[:, pb, :], lhsT=wo[q][:, k, :], rhs=merged[:, k, t0:t1e],
                        start=(k == 0), stop=(k == KC - 1)),
                        reads=[("wo", q), ("merged", k, ti)], writes=[("ps", pb)])
                S.op("dve", lambda e, pb=pb, dc=dc, t0=t0, t1e=t1e: e.scalar_tensor_tensor(
                    out=z[:, dc, t0:t1e], in0=ps[:, pb, :], scalar=1.0 / ALPHA, in1=z[:, dc, t0:t1e],
                    op0=ALU.mult, op1=ALU.add),
                    reads=[("ps", pb), ("l2", "z", dc, ti)], writes=[("l2", "z", dc, ti)])
        self.ln_apply("l2", z, RTB, cg, cb, 33280, LN_EPS / (ALPHA * ALPHA), strm_out=strm_out,
                      resid_out=self.h2s)
        S.barrier()

    def eps_col(self, val):
        return self.eps_cols[val]

    def build(self):
        nc, S = self.nc, self.S
        stage = self.stage
        xT = self.din("xT", [D, NT])
        wg1 = self.din("wg1", [FC // 2, 128, KC, 256])
        wu1 = self.din("wu1", [FC // 2, 128, KC, 256])
        wd1 = self.din("wd1", [KC, 128, FC, 128])
        wg2 = self.din("wg2", [FC // 2, 128, KC, 256])
        wu2 = self.din("wu2", [FC // 2, 128, KC, 256])
        wd2 = self.din("wd2", [KC, 128, FC, 128])
        cvec = self.din("cvec", [128, 128])
        cmat = self.cmat_d = self.din("cmat", [128, 512])
        self.catt = self.din("catt", [128, 224])
        self.augk = self.din("augk", [3, 4112])
        self.augs = self.din("augs", [2, 1024])
        self.augq = self.din("augq", [8, 1024])
        self.cbt_d = self.din("cbt", [128, 512])
        self.win = self.din("win", [len(GROUPS), 128, KC, 256])
        self.wba_d = self.din("wba", [8, 128, 8, 256])
        self.wbh_d = self.din("wbh", [8, 128, 8, 256])
        self.wo_d = self.din("wo", [KC, 128, KC, 128])
        self.gidx = {nm: i for i, (nm, _, _) in enumerate(GROUPS)}
        self.h1s = h1s = self.dscratch("h1s", [D, NT])
        self.h2s = self.dscratch("h2s", [D, NREAL])
        self.xs = [self.dscratch("xs%d" % q, [128, 3 * 1024], BF16) for q in range(3)]
        self.xg = [self.dscratch("xg%d" % q, [512, 3 * 1024], BF16) for q in range(3)]
        self.xa = self.dscratch("xa", [128, 72])
        self.xag = self.dscratch("xag", [512, 72])
        self.ks = self.dscratch("ks", [128, 2048], BF16)
        self.kg = self.dscratch("kg", [512, 2048], BF16)
        self.vs = self.dscratch("vs", [128, 3088], BF16)
        self.vg = self.dscratch("vg", [512, 3088], BF16)
        self.kms = self.dscratch("kms", [128, 2 * NMETA], BF16)
        self.vms = self.dscratch("vms", [NMETA, 258], BF16)
        if stage == 1:
            dbg = self.dout("dbg", [D, NT])
        elif stage in (3, 4):
            dbg = self.dout("dbg", [128, 8 * NREAL])
            self.dbg2 = self.dout("dbg2", [128, 12000])
        elif stage == 5:
            dbg = self.dout("dbg", [D, NREAL])
        else:
            outT = self.dout("outT", [D, NREAL])

        from contextlib import ExitStack
        with ExitStack() as es:
            self.arena = es.enter_context(nc.sbuf_tensor("arena", [128, ARENA_F32], F32))
            self.cst = es.enter_context(nc.sbuf_tensor("cst", [128, CONST_F32], F32))
            self.ps = es.enter_context(nc.psum_tensor("ps", [128, 8, 512], F32))
            esems = {e: es.enter_context(nc.semaphore("sem_" + e)) for e in ENGS}
            dsems = [es.enter_context(nc.semaphore("dsem%d" % i)) for i in range(S.n_dma_sems + 8)]
            block = es.enter_context(nc.Block())
            cst = self.cst
            self.ones_f32 = cst[:, 0:128]
            cv = self.cv = cst[:, 128:256]
            epsA = cst[:, 256:257]
            self.eps_rms = cst[:, 257:258]
            self.eps_ik = cst[:, 258:259]
            self.eps_cols = {LN_EPS / (ALPHA * ALPHA): epsA}
            self.ident_bf = cst[:, 264:328].bitcast(BF16)
            self.ones_bf = cst[:, 328:392].bitcast(BF16)
            self.mask2 = cst[:, 392:520]
            self.ones128 = cst[:, 520:648]
            self.lbc = cst[:, 648:656]
            self.omlc = cst[:, 656:664]
            self.gnc = cv[:, 112:120]
            self.selc = cv[:, 120:124]
            S.op("dve", lambda e: e.memset(self.ones_f32, 1.0 / D), writes=["ones"])
            S.op("dve", lambda e: e.memset(self.ones128, 1.0 / 128), writes=["ones128"])
            S.op("dve", lambda e: e.memset(epsA, LN_EPS / (ALPHA * ALPHA)), writes=["eps"])
            S.op("dve", lambda e: e.memset(self.eps_rms, RMS_EPS), writes=["eps"])
            S.op("dve", lambda e: e.memset(self.eps_ik, LN_EPS), writes=["eps"])
            S.dma("sp", lambda e: e.dma_start(out=cv, in_=cvec), writes=["cv"])
            S.dma("sp", lambda e: e.dma_start(out=self.mask2, in_=cmat[:, 256:384]), writes=["mask2"])
            S.dma("pool", lambda e: e.dma_start(out=self.ident_bf, in_=cmat[:, 0:128]), writes=["ident"])
            S.dma("pool", lambda e: e.dma_start(out=self.ones_bf, in_=cmat[:, 128:256]), writes=["ones_bf"])
            S.op("dve", lambda e: e.tensor_tensor(out=self.lbc, in0=cv[:, 96:104], in1=cv[:, 104:112], op=ALU.subtract),
                 reads=["cv"], writes=["lb"])
            S.op("act", lambda e: e.activation(out=self.lbc, in_=self.lbc, func=AF.Sigmoid), reads=["lb"], writes=["lb"])
            S.op("dve", lambda e: e.tensor_scalar(out=self.omlc, in0=self.lbc, scalar1=-1.0, scalar2=1.0,
                                                  op0=ALU.mult, op1=ALU.add), reads=["lb"], writes=["lb"])
            strm0 = self.view(0, BF16, [KC, NT])
            S.dma("pool", lambda e: e.dma_start(out=strm0, in_=xT.rearrange("(k p) t -> p k t", p=128)),
                  writes=[("f1", "sin")])
            S.barrier()
            self.ffn_phase("f1", NT, TBS, strm0, 66560, wg1, wu1, wd1, xT, cv[:, 0:16], cv[:, 16:32],
                           resid_out=(dbg if stage == 1 else h1s))
            strm1 = self.view(66560, BF16, [KC, NT])
            if stage >= 2:
                if stage >= 4:
                    self.attn_m3(strm1, 0)
                self.hgrn_m1(strm1)
                if stage >= 4:
                    self.attn_m3(strm1, 1)
                self.hgrn_m2(strm1)
            if stage == 3:
                yhg = self.view(32768, BF16, [8 * NREAL])
                S.dma("pool", lambda e: e.dma_start(out=dbg, in_=yhg), reads=[])
            if stage >= 4:
                self.attn_m3(strm1, 2)
            if stage == 4:
                yat = self.view(0, BF16, [8 * NREAL])
                S.dma("pool", lambda e: e.dma_start(out=dbg, in_=yat), reads=[])
            if stage >= 5:
                if stage == 5:
                    self.h2s = dbg
                self.merge_m4(strm1, cv[:, 32:48], cv[:, 48:64])
            if stage >= 6:
                strm2 = self.view(0, BF16, [KC, NREAL])
                self.ffn_phase("f2", NREAL, TBS[0:2], strm2, 66560, wg2, wu2, wd2, self.h2s, cv[:, 64:80],
                               cv[:, 80:96], final_out=outT)
            S.emit(block, esems, dsems)
        return nc


def _lay_gu(w):
    return np.ascontiguousarray(w.reshape(KC, 128, FC // 2, 256).transpose(2, 1, 0, 3))


def _lay_d(w):
    return np.ascontiguousarray(w.reshape(FC, 128, KC, 128).transpose(2, 1, 0, 3))


def _fm(v):
    return np.ascontiguousarray(v.reshape(KC, 128).T)


def _core_tokens(x, meta, c):
    b, j = c // 4, c % 4
    blocks = [x[b, 128 * (4 * i + j):128 * (4 * i + j) + 128] for i in range(8)]
    tok = np.concatenate(blocks + [meta], axis=0)
    return np.ascontiguousarray(tok.T)


def _mk_groups():
    g = []
    for hp in range(4):
        g += [("hf%d" % hp, 3664 + 256 * hp, 256), ("hq%d" % hp, 2640 + 256 * hp, 256),
              ("hi%d" % hp, 4688 + 256 * hp, 256)]
    for i in range(4):
        g.append(("hg%d" % i, 5712 + 256 * i, 256))
    g += [("ak", 1024, 256), ("av", 1280, 256), ("ikw", 2560, 80)]
    for i in range(4):
        g.append(("aq%d" % i, 256 * i, 256))
    for i in range(4):
        g.append(("iq%d" % i, 1536 + 256 * i, 256))
    for i in range(8):
        g.append(("ga%d" % i, 6736 + 256 * i, 256))
        g.append(("gh%d" % i, 8784 + 256 * i, 256))
    return g


GROUPS = _mk_groups()


def _lay_win(w):
    out = np.zeros((len(GROUPS), 128, KC, 256), np.float32)
    for gi, (nm, c0, nc_) in enumerate(GROUPS):
        out[gi, :, :, 0:nc_] = w[:, c0:c0 + nc_].reshape(KC, 128, nc_).transpose(1, 0, 2)
    return out


def prepare(inputs, stage):
    f = lambda k: np.asarray(inputs[k], dtype=np.float32)
    x, meta = f("x"), f("meta")
    shared = {
        "wg1": _lay_gu(f("ffn1_w_gate")[0]), "wu1": _lay_gu(f("ffn1_w_up")[0]),
        "wd1": _lay_d(f("ffn1_w_down")[0]),
        "win": _lay_win(f("w_in")[0]),
        "wg2": _lay_gu(f("ffn2_w_gate")[0]), "wu2": _lay_gu(f("ffn2_w_up")[0]),
        "wd2": _lay_d(f("ffn2_w_down")[0]),
        "wba": np.ascontiguousarray(f("w_branch_att")[0].reshape(8, 128, 8, 256).transpose(2, 1, 0, 3)),
        "wbh": np.ascontiguousarray(f("w_branch_hg")[0].reshape(8, 128, 8, 256).transpose(2, 1, 0, 3)),
        "wo": np.ascontiguousarray(f("w_out")[0].reshape(KC, 128, KC, 128).transpose(2, 1, 0, 3)),
    }
    slopes = (2.0 ** -(np.arange(8) + 1.0)).astype(np.float32)
    c = np.arange(4096)
    kpos = np.concatenate([16 + 128 * (4 * ((c % 1024) // 128) + c // 1024) + c % 128, np.arange(16)]).astype(np.float32)
    augk = np.stack([np.floor(kpos / 64.0), kpos % 64.0, np.ones_like(kpos)], 0).astype(np.float32)
    augs = np.stack([np.repeat(64.0 * slopes, 128), np.repeat(slopes, 128)], 0).astype(np.float32)
    shared["augk"] = augk
    shared["augs"] = augs
    cvec = np.zeros((128, 128), np.float32)
    cvec[:, 0:16] = _fm(f("ln1_g")[0]); cvec[:, 16:32] = _fm(f("ln1_b")[0])
    cvec[:, 32:48] = _fm(f("ln2_g")[0]); cvec[:, 48:64] = _fm(f("ln2_b")[0])
    cvec[:, 64:80] = _fm(f("ln3_g")[0]); cvec[:, 80:96] = _fm(f("ln3_b")[0])
    lbl = f("hg_lb_logits")
    cvec[:, 96:104] = lbl[0].reshape(8, 128).T
    cvec[:, 104:112] = lbl[1].reshape(8, 128).T
    cvec[:, 112:120] = f("hg_norm_g")[0].T
    cmat = np.zeros((128, 512), np.float32)
    cmat[:, 384:512] = np.arange(128, dtype=np.float32)[None, :]
    cmat[:, 0:128] = np.eye(128, dtype=np.float32)
    cmat[:, 128:256] = 1.0
    sidx = np.arange(128)[:, None]; tidx = np.arange(128)[None, :]
    cmat[:, 256:384] = (((sidx <= tidx) & ((sidx // 64) == (tidx // 64))) | ((sidx < 64) & (tidx >= 64))).astype(np.float32)
    shared["cmat"] = cmat
    maps = []
    for c in range(8):
        m = dict(shared)
        m["xT"] = _core_tokens(x, meta, c)
        cv = cvec.copy()
        cv[:, 120 + (c % 4)] = 1.0
        m["cvec"] = cv
        j = c % 4
        p = np.arange(128, dtype=np.float32)
        qpos = np.stack([16 + 128 * (4 * i + j) + p for i in range(8)], 0)
        m["augq"] = np.ascontiguousarray((-slopes[None, :, None] * qpos[:, None, :]).reshape(8, 1024).astype(np.float32))
        catt = np.zeros((128, 224), np.float32)
        catt[:, 0:8] = -slopes[None, :]
        catt[:, 8:40] = np.array([16 + 128 * r + 512 * jj for r in range(4) for jj in range(8)], np.float32)[None, :]
        catt[:, 64:96] = (2.0 ** -(np.arange(32) + 1.0))[None, :]
        catt[:, 96:160] = f("idx_k_norm_g")[0][None, :]
        catt[:, 160:224] = f("idx_k_norm_b")[0][None, :]
        m["catt"] = catt
        tt = np.arange(128)[:, None, None]; rr = np.arange(4)[None, :, None]; ss = np.arange(128)[None, None, :]
        m["cbt"] = np.where(128 * (rr - j) + (ss - tt) > 0, -1e30, 0.0).astype(np.float32).reshape(128, 512)
        maps.append(m)
    return maps


_NC_CACHE = {}


def kernel(**inputs):
    stage = int(os.environ.get("KSTAGE", "9"))
    if stage not in _NC_CACHE:
        _NC_CACHE[stage] = Builder(stage).build()
    nc = _NC_CACHE[stage]
    maps = prepare(inputs, stage)
    res = run_bass_kernel_spmd(nc, maps, core_ids=list(range(8)))
    if stage == 4:
        return [(r["dbg"], r["dbg2"]) for r in res.results]
    if stage < 6:
        return [r["dbg"] for r in res.results]
    out = np.zeros((2, 4096, D), np.float32)
    for c in range(8):
        b, j = c // 4, c % 4
        o = res.results[c]["outT"]
        for i in range(8):
            g = 4 * i + j
            out[b, 128 * g:128 * g + 128] = o[:, 128 * i:128 * i + 128].T
    return out
```

```python
import os
import numpy as np
import concourse.bass as bass
import concourse.mybir as mybir
from concourse.bass_utils import run_bass_kernel_spmd

F32 = mybir.dt.float32
BF16 = mybir.dt.bfloat16
U8 = mybir.dt.uint8
AF = mybir.ActivationFunctionType
ALU = mybir.AluOpType
AX = mybir.AxisListType

D = 2048
DFF = 5632
NMETA = 16
NREAL = 1024
NT = NREAL + NMETA
KC = D // 128
FC = DFF // 128
TBS = [(0, 512), (512, 1024), (1024, 1040)]
ALPHA = 2.0 ** 0.25
LN_EPS = 1e-5
RMS_EPS = 1e-6

ENGS = ("pe", "act", "dve", "pool", "sp")


class Op:
    __slots__ = ("eng", "fn", "deps", "is_dma", "dsem", "dval", "sig", "sigidx", "waits")

    def __init__(self, eng, fn, is_dma=False):
        self.eng = eng
        self.fn = fn
        self.deps = []
        self.is_dma = is_dma
        self.dsem = None
        self.dval = 0
        self.sig = False
        self.sigidx = 0
        self.waits = []


class Sched:
    def __init__(self, n_dma_sems=40, same_engine_sync=True):
        self.ops = {e: [] for e in ENGS}
        self.lastw = {}
        self.readers = {}
        self.n_dma = 0
        self.n_dma_sems = n_dma_sems
        self.dma_hist = {}
        self.same_engine_sync = same_engine_sync
        self.n_coll = 0

    def _add(self, op, reads, writes):
        deps = set()
        for k in reads:
            w = self.lastw.get(k)
            if w is not None:
                deps.add(w)
        for k in writes:
            w = self.lastw.get(k)
            if w is not None:
                deps.add(w)
            for r in self.readers.get(k, ()):
                deps.add(r)
        op.deps = list(deps)
        for k in reads:
            self.readers.setdefault(k, []).append(op)
        for k in writes:
            self.lastw[k] = op
            self.readers[k] = []
        self.ops[op.eng].append(op)
        return op

    def op(self, eng, fn, reads=(), writes=()):
        return self._add(Op(eng, fn), reads, writes)

    def dma(self, q, fn, reads=(), writes=()):
        op = Op(q, fn, is_dma=True)
        slot = self.n_dma % self.n_dma_sems
        op.dsem = slot
        op.dval = 16 * (self.n_dma // self.n_dma_sems + 1)
        self.n_dma += 1
        self._add(op, reads, writes)
        prev = self.dma_hist.get(slot)
        if prev is not None:
            op.deps.append(prev)
        self.dma_hist[slot] = op
        return op

    def coll(self, fn, reads=(), writes=()):
        op = Op("pool", fn, is_dma=True)
        op.dsem = self.n_dma_sems + self.n_coll
        op.dval = 1
        self.n_coll += 1
        self._add(op, reads, writes)
        self.dma_hist[op.dsem] = op
        return op

    def barrier(self):
        lasts = []
        for e in ENGS:
            for o in reversed(self.ops[e]):
                if not o.is_dma and o.fn is not None:
                    lasts.append(o)
                    break
        dmas = list(self.dma_hist.values())
        for e in ENGS:
            op = Op(e, None)
            op.deps = [o for o in lasts if o.eng != e] + dmas
            self.ops[e].append(op)
        self.lastw = {}
        self.readers = {}

    def _skip(self, d, op):
        return d.eng == op.eng and (d.eng in ("pe", "sp") or not self.same_engine_sync)

    def finalize(self):
        for e in ENGS:
            for op in self.ops[e]:
                for d in op.deps:
                    if not d.is_dma and not self._skip(d, op):
                        d.sig = True
        for e in ENGS:
            c = 0
            for op in self.ops[e]:
                if op.sig:
                    c += 1
                    op.sigidx = c
        for e in ENGS:
            waited = {}
            for op in self.ops[e]:
                need = {}
                for d in op.deps:
                    if d.is_dma:
                        key, val = ("d", d.dsem), d.dval
                    else:
                        if self._skip(d, op):
                            continue
                        key, val = ("e", d.eng), d.sigidx
                    if waited.get(key, 0) >= val:
                        continue
                    if need.get(key, 0) < val:
                        need[key] = val
                for k, v in need.items():
                    waited[k] = v
                op.waits = list(need.items())

    def emit(self, block, esems, dsems):
        self.finalize()
        regs = {"pe": block.tensor, "act": block.scalar, "dve": block.vector,
                "pool": block.gpsimd, "sp": block.sync}
        final = {d.dsem: d.dval for d in self.dma_hist.values()}

        def make(e):
            ops = self.ops[e]

            def body(eng):
                for op in ops:
                    for (kind, which), val in op.waits:
                        eng.wait_ge(dsems[which] if kind == "d" else esems[which], val)
                    if op.fn is None:
                        continue
                    ins = op.fn(eng)
                    if op.is_dma:
                        ins.then_inc(dsems[op.dsem], 16 if op.dsem < self.n_dma_sems else 1)
                    elif op.sig:
                        ins.then_inc(esems[e], 1)
                if e == "sp":
                    for slot, val in final.items():
                        eng.wait_ge(dsems[slot], val)
            return body

        for e in ENGS:
            regs[e](make(e))


ARENA_F32 = 50816
CONST_F32 = 2304


class Builder:
    def __init__(self, stage):
        self.stage = stage
        self.nc = bass.Bass("TRN2", target_bir_lowering=False)
        self.S = Sched()
        self.dram = {}

    def din(self, name, shape, dt=F32):
        self.dram[name] = self.nc.dram_tensor(name, list(shape), dt, kind="ExternalInput").ap()
        return self.dram[name]

    def dout(self, name, shape, dt=F32):
        self.dram[name] = self.nc.dram_tensor(name, list(shape), dt, kind="ExternalOutput").ap()
        return self.dram[name]

    def dscratch(self, name, shape, dt=F32):
        self.dram[name] = self.nc.dram_tensor(name, list(shape), dt, kind="Internal").ap()
        return self.dram[name]

    def view(self, off_bytes, dt, shape):
        n = 1
        for s in shape:
            n *= s
        esz = 4 if dt == F32 else (1 if dt == U8 else 2)
        assert off_bytes % 4 == 0
        nbytes = n * esz
        assert nbytes % 4 == 0
        assert off_bytes + nbytes <= ARENA_F32 * 4, (off_bytes, nbytes)
        v = self.arena[:, off_bytes // 4:(off_bytes + nbytes) // 4]
        if dt != F32:
            v = v.bitcast(dt)
        if len(shape) == 2:
            v = v.rearrange("p (a b) -> p a b", b=shape[1])
        elif len(shape) == 3:
            v = v.rearrange("p (a b c) -> p a b c", b=shape[1], c=shape[2])
        return v

    def ln_apply(self, tag, z, tbs, cg, cb, toff, eps_eff, strm_out=None, alias_key=None, resid_out=None,
                 final_out=None):
        S, ps = self.S, self.ps
        zsq = [self.view(toff + i * 2048, BF16, [512]) for i in range(2)]
        meanb = self.view(toff + 4096, F32, [512])
        rstdb = self.view(toff + 6144, F32, [512])
        t1 = [self.view(toff + 8192 + i * 2048, F32, [512]) for i in range(2)]
        t2 = [self.view(toff + 12288 + i * 2048, F32, [512]) for i in range(2)]
        o32 = [self.view(toff + 16384 + i * 2048, F32, [512]) for i in range(2)]
        ones = self.ones_f32
        for ti, (t0, t1e) in enumerate(tbs):
            n = t1e - t0
            pm, pq = ps[:, 6, 0:n], ps[:, 7, 0:n]
            for dc in range(KC):
                zq = zsq[dc % 2]
                S.op("act", lambda e, zq=zq, dc=dc, t0=t0, t1e=t1e, n=n: e.activation(
                    out=zq[:, 0:n], in_=z[:, dc, t0:t1e], func=AF.Square, scale=float(D) ** -0.5),
                    reads=[(tag, "z", dc, ti)], writes=[(tag, "zsq", dc % 2)])
                S.op("pe", lambda e, pm=pm, dc=dc, t0=t0, t1e=t1e: e.matmul(
                    pm, lhsT=ones, rhs=z[:, dc, t0:t1e], start=(dc == 0), stop=(dc == KC - 1)),
                    reads=[(tag, "z", dc, ti)], writes=[("ps", 6)])
                S.op("pe", lambda e, pq=pq, zq=zq, dc=dc, n=n: e.matmul(
                    pq, lhsT=self.ones_bf, rhs=zq[:, 0:n], start=(dc == 0), stop=(dc == KC - 1)),
                    reads=[(tag, "zsq", dc % 2), "ones_bf"], writes=[("ps", 7)])
            S.op("act", lambda e, pm=pm, n=n: e.activation(out=meanb[:, 0:n], in_=pm, func=AF.Copy),
                 reads=[("ps", 6)], writes=[(tag, "meanb")])
            S.op("dve", lambda e, n=n: e.tensor_tensor(out=rstdb[:, 0:n], in0=meanb[:, 0:n], in1=meanb[:, 0:n],
                                                       op=ALU.mult),
                 reads=[(tag, "meanb")], writes=[(tag, "rstdb")])
            S.op("dve", lambda e, pq=pq, n=n: e.tensor_tensor(out=rstdb[:, 0:n], in0=pq, in1=rstdb[:, 0:n],
                                                              op=ALU.subtract),
                 reads=[("ps", 7), (tag, "rstdb")], writes=[(tag, "rstdb")])
            S.op("act", lambda e, n=n: e.activation(out=rstdb[:, 0:n], in_=rstdb[:, 0:n], func=AF.Sqrt,
                                                    bias=self.eps_cols[eps_eff], scale=1.0),
                 reads=[(tag, "rstdb")], writes=[(tag, "rstdb")])
            S.op("dve", lambda e, n=n: e.reciprocal(out=rstdb[:, 0:n], in_=rstdb[:, 0:n]),
                 reads=[(tag, "rstdb")], writes=[(tag, "rstdb")])
            S.op("dve", lambda e, n=n: e.scalar_tensor_tensor(out=meanb[:, 0:n], in0=meanb[:, 0:n], scalar=-1.0,
                                                              in1=rstdb[:, 0:n], op0=ALU.mult, op1=ALU.mult),
                 reads=[(tag, "meanb"), (tag, "rstdb")], writes=[(tag, "meanb")])
            for dc in range(KC):
                a, b_, o = t1[dc % 2], t2[dc % 2], o32[dc % 2]
                S.op("dve", lambda e, a=a, dc=dc, t0=t0, t1e=t1e, n=n: e.tensor_tensor(
                    out=a[:, 0:n], in0=z[:, dc, t0:t1e], in1=rstdb[:, 0:n], op=ALU.mult),
                    reads=[(tag, "z", dc, ti), (tag, "rstdb")], writes=[(tag, "t1", dc % 2)])
                S.op("dve", lambda e, a=a, b_=b_, n=n: e.tensor_tensor(
                    out=b_[:, 0:n], in0=a[:, 0:n], in1=meanb[:, 0:n], op=ALU.add),
                    reads=[(tag, "t1", dc % 2), (tag, "meanb")], writes=[(tag, "t2", dc % 2)])
                S.op("act", lambda e, b_=b_, o=o, dc=dc, n=n: e.activation(
                    out=o[:, 0:n], in_=b_[:, 0:n], func=AF.Identity,
                    bias=cb[:, dc:dc + 1], scale=cg[:, dc:dc + 1]),
                    reads=[(tag, "t2", dc % 2)], writes=[(tag, "o32", dc % 2)])
                if strm_out is not None:
                    wk = [(tag, "sout", dc, ti)] + ([(tag, alias_key, dc, ti)] if alias_key else [])
                    S.op("act", lambda e, b_=b_, dc=dc, t0=t0, t1e=t1e, n=n: e.activation(
                        out=strm_out[:, dc, t0:t1e], in_=b_[:, 0:n], func=AF.Identity,
                        bias=cb[:, dc:dc + 1], scale=cg[:, dc:dc + 1]),
                        reads=[(tag, "t2", dc % 2)], writes=wk)
                dst = resid_out if final_out is None else final_out
                if final_out is None or t0 < NREAL:
                    S.dma("sp", lambda e, o=o, dc=dc, t0=t0, t1e=t1e, n=n, dst=dst: e.dma_start(
                        out=dst[dc * 128:(dc + 1) * 128, t0:t1e], in_=o[:, 0:n]),
                        reads=[(tag, "o32", dc % 2)])

    def ffn_phase(self, tag, ntok, tbs, strm_in, strm_out_off, wg, wu, wd, resid, cg, cb,
                  resid_out=None, final_out=None):
        S, nc = self.S, self.nc
        ps = self.ps
        C_OFF = 33280
        B_OFF = 66560
        D_OFF = B_OFF + FC * NT * 2
        T_OFF = D_OFF + 2 * FC * 128 * 2
        hT = self.view(B_OFF, BF16, [FC, ntok])
        wgu = [self.view(C_OFF + i * 16384, BF16, [2, KC, 256]) for i in range(2)]
        wdb = [self.view(D_OFF + i * FC * 128 * 2, BF16, [FC, 128]) for i in range(2)]
        z = self.view(0, F32, [KC, ntok])
        sil = [self.view(T_OFF + 8192 + i * 2048, F32, [512]) for i in range(2)]
        strm_out = self.view(strm_out_off, BF16, [KC, ntok]) if final_out is None else None
        c_scale = 0.5 / ALPHA
        eps_eff = LN_EPS / (ALPHA * ALPHA)
        nb = len(tbs)

        for g in range(FC // 2):
            wb = wgu[g % 2]
            kb = (tag, "wgu", g % 2)
            S.dma("pool", lambda e, wb=wb, g=g: e.dma_start(out=wb[:, 0], in_=wg[g]), writes=[(kb, 0)])
            S.dma("pool", lambda e, wb=wb, g=g: e.dma_start(out=wb[:, 1], in_=wu[g]), writes=[(kb, 1)])
            for fcl in range(2):
                fc = 2 * g + fcl
                for ti, (t0, t1e) in enumerate(tbs):
                    n = t1e - t0
                    pb = (fc * nb + ti) % 2
                    pg, pu = ps[:, 2 * pb, 0:n], ps[:, 2 * pb + 1, 0:n]
                    for k in range(KC):
                        S.op("pe", lambda e, pg=pg, wb=wb, k=k, fcl=fcl, t0=t0, t1e=t1e: e.matmul(
                            pg, lhsT=wb[:, 0, k, fcl * 128:(fcl + 1) * 128], rhs=strm_in[:, k, t0:t1e],
                            start=(k == 0), stop=(k == KC - 1)),
                            reads=[(kb, 0), (tag, "sin")], writes=[("ps", 2 * pb)])
                    for k in range(KC):
                        S.op("pe", lambda e, pu=pu, wb=wb, k=k, fcl=fcl, t0=t0, t1e=t1e: e.matmul(
                            pu, lhsT=wb[:, 1, k, fcl * 128:(fcl + 1) * 128], rhs=strm_in[:, k, t0:t1e],
                            start=(k == 0), stop=(k == KC - 1)),
                            reads=[(kb, 1), (tag, "sin")], writes=[("ps", 2 * pb + 1)])
                    sb = sil[pb]
                    S.op("act", lambda e, sb=sb, pg=pg, n=n: e.activation(out=sb[:, 0:n], in_=pg, func=AF.Silu),
                         reads=[("ps", 2 * pb)], writes=[(tag, "sil", pb)])
                    S.op("dve", lambda e, sb=sb, pu=pu, n=n, fc=fc, t0=t0, t1e=t1e: e.tensor_tensor(
                        out=hT[:, fc, t0:t1e], in0=sb[:, 0:n], in1=pu, op=ALU.mult),
                        reads=[(tag, "sil", pb), ("ps", 2 * pb + 1)], writes=[(tag, "hT", fc, ti)])

        S.barrier()
        for dc in range(KC):
            wb = wdb[dc % 2]
            kb = (tag, "wd", dc % 2)
            S.dma("pool", lambda e, wb=wb, dc=dc: e.dma_start(out=wb, in_=wd[dc]), writes=[kb])
            S.dma("sp", lambda e, dc=dc: e.dma_start(out=z[:, dc, :], in_=resid[dc * 128:(dc + 1) * 128, 0:ntok]),
                  writes=[(tag, "z", dc, ti) for ti in range(nb)])
            for ti, (t0, t1e) in enumerate(tbs):
                n = t1e - t0
                pb = 4 + (dc * nb + ti) % 2
                py = ps[:, pb, 0:n]
                for f in range(FC):
                    S.op("pe", lambda e, py=py, wb=wb, f=f, t0=t0, t1e=t1e: e.matmul(
                        py, lhsT=wb[:, f, :], rhs=hT[:, f, t0:t1e], start=(f == 0), stop=(f == FC - 1)),
                        reads=[kb, (tag, "hT", f, ti)], writes=[("ps", pb)])
                S.op("dve", lambda e, py=py, dc=dc, t0=t0, t1e=t1e: e.scalar_tensor_tensor(
                    out=z[:, dc, t0:t1e], in0=py, scalar=c_scale, in1=z[:, dc, t0:t1e],
                    op0=ALU.mult, op1=ALU.add),
                    reads=[("ps", pb), (tag, "z", dc, ti)], writes=[(tag, "z", dc, ti)])
        self.ln_apply(tag, z, tbs, cg, cb, T_OFF, eps_eff, strm_out=strm_out, alias_key="hT",
                      resid_out=resid_out, final_out=final_out)
        S.barrier()

    def proj_fm(self, tag, strm, gi, wbufs, tbs, evac, banks=(0, 1), parity=[0]):
        S, ps = self.S, self.ps
        wb = wbufs[parity[0] % 2]
        kb = ("wb", parity[0] % 2)
        parity[0] += 1
        S.dma("pool", lambda e, wb=wb, gi=gi: e.dma_start(out=wb, in_=self.win[gi]), writes=[kb])
        cnt = 0
        for half in range(2):
            for ti, (t0, t1e) in enumerate(tbs):
                n = t1e - t0
                bk = banks[cnt % len(banks)]
                cnt += 1
                pv = ps[:, bk, 0:n]
                for k in range(KC):
                    S.op("pe", lambda e, pv=pv, wb=wb, k=k, half=half, t0=t0, t1e=t1e: e.matmul(
                        pv, lhsT=wb[:, k, half * 128:(half + 1) * 128], rhs=strm[:, k, t0:t1e],
                        start=(k == 0), stop=(k == KC - 1)),
                        reads=[kb, "strm"], writes=[("ps", bk)])
                evac(half, ti, t0, t1e, pv, bk)

    def proj_tm(self, tag, strm, gi, wbufs, tls, ncols, evac, banks=(0, 1), parity=[0]):
        S, ps = self.S, self.ps
        wb = wbufs[parity[0] % 2]
        kb = ("wb", parity[0] % 2)
        parity[0] += 1
        S.dma("pool", lambda e, wb=wb, gi=gi: e.dma_start(out=wb, in_=self.win[gi]), writes=[kb])
        for cnt, (i, c0, n) in enumerate(tls):
            bk = banks[cnt % len(banks)]
            pv = ps[0:n, bk, 0:ncols]
            for k in range(KC):
                S.op("pe", lambda e, pv=pv, wb=wb, k=k, c0=c0, n=n: e.matmul(
                    pv, lhsT=strm[:, k, c0:c0 + n], rhs=wb[:, k, 0:ncols],
                    start=(k == 0), stop=(k == KC - 1)),
                    reads=[kb, "strm"], writes=[("ps", bk)])
            evac(i, c0, n, pv, bk)

    def hgrn_m1(self, strm):
        S, ps, nc = self.S, self.ps, self.nc
        R1 = 99840
        wbufs = [self.view(R1 + i * 8192, BF16, [KC, 256]) for i in range(2)]
        off = [R1 + 16384]

        def alloc(dt, shape):
            n = 1
            for x in shape:
                n *= x
            nb = n * (4 if dt == F32 else 2)
            nb = (nb + 63) // 64 * 64
            v = self.view(off[0], dt, shape)
            off[0] += nb
            return v
        logf = alloc(F32, [2, NT]); Bg = alloc(F32, [2, NT]); Bsh = alloc(F32, [2, NT])
        tA = alloc(F32, [2, NT]); tB = alloc(F32, [2, NT])
        kk = alloc(BF16, [2, NT]); qs = alloc(BF16, [2, NT]); qt = alloc(BF16, [2, NT])
        kt = alloc(BF16, [2, NT]); kh64 = alloc(BF16, [2, NT]); kh128 = alloc(BF16, [2, NT])
        vv = alloc(BF16, [9, 256])
        PTs = [alloc(BF16, [2, 128]) for _ in range(2)]; khTs = [alloc(BF16, [2, 128]) for _ in range(2)]
        xs_st = alloc(BF16, [9, 256]); xa_st = alloc(F32, [9, 2])
        Qc = self.view(0, BF16, [8, NREAL]); oloc = self.view(16384, BF16, [8, NREAL])
        cv = self.cv
        lbc, omlc = self.lbc, self.omlc
        psb = lambda bk: ps[:, bk, :].bitcast(BF16)
        TL = [(i, 128 * i, 128) for i in range(8)] + [(8, NREAL, NMETA)]
        flat = lambda v: v.rearrange("p h t -> p (h t)")
        r64 = lambda v: v[:, :, 0:NREAL].rearrange("p h (c t) -> p h c t", t=64)
        r128 = lambda v: v[:, :, 0:NREAL].rearrange("p h (c t) -> p h c t", t=128)
        mt = lambda v: v[:, :, NREAL:NT]

        for hp in range(4):
            T = ("m1", hp)
            def ev_f(half, ti, t0, t1e, pv, bk, hp=hp):
                h = 2 * hp + half
                S.op("act", lambda e: e.activation(out=tA[:, half, t0:t1e], in_=pv, func=AF.Sigmoid),
                     reads=[("ps", bk)], writes=[("tA", half, ti)])
                S.op("dve", lambda e: e.tensor_scalar(out=tA[:, half, t0:t1e], in0=tA[:, half, t0:t1e],
                                                      scalar1=omlc[:, h:h + 1], scalar2=lbc[:, h:h + 1],
                                                      op0=ALU.mult, op1=ALU.add),
                     reads=[("tA", half, ti), "lb"], writes=[("tA", half, ti)])
                S.op("act", lambda e: e.activation(out=logf[:, half, t0:t1e], in_=tA[:, half, t0:t1e], func=AF.Ln),
                     reads=[("tA", half, ti)], writes=[("logf", half, ti)])
                S.op("dve", lambda e: e.tensor_scalar(out=kk[:, half, t0:t1e], in0=tA[:, half, t0:t1e],
                                                      scalar1=-1.0, scalar2=1.0, op0=ALU.mult, op1=ALU.add),
                     reads=[("tA", half, ti)], writes=[("kk", half, ti)])
            self.proj_fm(T, strm, self.gidx["hf%d" % hp], wbufs, TBS, ev_f)

            def ev_q(half, ti, t0, t1e, pv, bk):
                S.op("act", lambda e: e.activation(out=qs[:, half, t0:t1e], in_=pv, func=AF.Silu),
                     reads=[("ps", bk)], writes=[("qs", half, ti)])
            self.proj_fm(T, strm, self.gidx["hq%d" % hp], wbufs, TBS, ev_q)

            def ev_v(i, c0, n, pv, bk):
                S.op("act", lambda e: e.activation(out=vv[0:n, i, :], in_=pv, func=AF.Copy),
                     reads=[("ps", bk)], writes=[("vv", i)])
            self.proj_tm(T, strm, self.gidx["hi%d" % hp], wbufs, TL, 256, ev_v)

            allk = lambda nm: [(nm, hl, ti) for hl in range(2) for ti in range(3)]
            S.op("dve", lambda e: e.tensor_tensor_scan(out=flat(Bg), data0=flat(logf), data1=flat(logf),
                                                       initial=0.0, op0=ALU.add, op1=ALU.min),
                 reads=allk("logf"), writes=["Bg"])
            S.op("dve", lambda e: e.memset(flat(Bsh)[:, 0:1], 0.0), writes=["Bsh0"])
            S.op("act", lambda e: e.activation(out=flat(Bsh)[:, 1:2 * NT], in_=flat(Bg)[:, 0:2 * NT - 1], func=AF.Copy),
                 reads=["Bg"], writes=["Bsh"])
            S.op("dve", lambda e: e.tensor_tensor(out=r64(tB), in0=r64(Bg),
                                                  in1=r64(Bsh)[:, :, :, 0:1].to_broadcast([128, 2, 16, 64]),
                                                  op=ALU.subtract),
                 reads=["Bg", "Bsh", "Bsh0"], writes=["tBr"])
            S.op("dve", lambda e: e.tensor_tensor(out=mt(tB), in0=mt(Bg),
                                                  in1=mt(Bsh)[:, :, 0:1].to_broadcast([128, 2, NMETA]),
                                                  op=ALU.subtract),
                 reads=["Bg", "Bsh", "Bsh0"], writes=["tBm"])
            S.op("dve", lambda e: e.tensor_tensor(out=r128(tA), in0=r128(Bg),
                                                  in1=r128(Bsh)[:, :, :, 0:1].to_broadcast([128, 2, 8, 128]),
                                                  op=ALU.subtract),
                 reads=["Bg", "Bsh", "Bsh0"] + allk("tA"), writes=["tAr"] + allk("tA"))
            S.op("dve", lambda e: e.tensor_copy(out=mt(tA), in_=mt(tB)),
                 reads=["tBm"], writes=["tAm"])
            TBk, TAk = ["tBr", "tBm"], ["tAr", "tAm"] + allk("tA")
            S.op("act", lambda e: e.activation(out=flat(Bg), in_=flat(tB), func=AF.Exp),
                 reads=TBk + ["Bsh", "tAr"], writes=["Bg"])
            S.op("dve", lambda e: e.tensor_tensor(out=flat(qt), in0=flat(qs), in1=flat(Bg), op=ALU.mult),
                 reads=["Bg"] + allk("qs"), writes=["qt"])
            S.op("act", lambda e: e.activation(out=flat(Bsh), in_=flat(tB), func=AF.Exp, scale=-1.0),
                 reads=TBk + ["Bsh", "tAr", "Bsh0"], writes=["Bsh", "Bsh0"])
            S.op("dve", lambda e: e.tensor_tensor(out=flat(kt), in0=flat(kk), in1=flat(Bsh), op=ALU.mult),
                 reads=["Bsh"] + allk("kk"), writes=["kt"])
            S.op("dve", lambda e: e.tensor_tensor(out=r64(Bg), in0=r64(tB),
                                                  in1=r64(tB)[:, :, :, 63:64].to_broadcast([128, 2, 16, 64]),
                                                  op=ALU.subtract),
                 reads=TBk + ["qt"], writes=["Bg"])
            S.op("act", lambda e: e.activation(out=r64(Bg), in_=r64(Bg), func=AF.Exp, scale=-1.0),
                 reads=["Bg"], writes=["Bg"])
            S.op("dve", lambda e: e.tensor_tensor(out=r64(kh64), in0=r64(kk), in1=r64(Bg), op=ALU.mult),
                 reads=["Bg"] + allk("kk"), writes=["kh64"])
            S.op("act", lambda e: e.activation(out=flat(Bsh), in_=flat(tA), func=AF.Exp),
                 reads=TAk + ["kt"], writes=["Bsh"])
            S.op("dve", lambda e, hp=hp: e.tensor_tensor(out=Qc[:, 2 * hp:2 * hp + 2, :], in0=qs[:, :, 0:NREAL],
                                                         in1=Bsh[:, :, 0:NREAL], op=ALU.mult),
                 reads=["Bsh"] + allk("qs"), writes=[("Qc", hp)])
            S.op("dve", lambda e: e.tensor_copy(out=xa_st[:, 0:8, :].rearrange("p i h -> p h i"),
                                                in_=r128(Bsh)[:, :, :, 127]),
                 reads=["Bsh"], writes=["xa_st"])
            S.op("dve", lambda e: e.tensor_copy(out=xa_st[:, 8, :], in_=Bsh[:, :, NT - 1]),
                 reads=["Bsh"], writes=["xa_st"])
            S.op("dve", lambda e: e.tensor_tensor(out=r128(Bg), in0=r128(tA),
                                                  in1=r128(tA)[:, :, :, 127:128].to_broadcast([128, 2, 8, 128]),
                                                  op=ALU.subtract),
                 reads=TAk + ["kh64"], writes=["Bg"])
            S.op("dve", lambda e: e.tensor_tensor(out=mt(Bg), in0=mt(tA),
                                                  in1=mt(tA)[:, :, NMETA - 1:NMETA].to_broadcast([128, 2, NMETA]),
                                                  op=ALU.subtract),
                 reads=TAk + ["kh64"], writes=["Bg"])
            S.op("act", lambda e: e.activation(out=flat(Bg), in_=flat(Bg), func=AF.Exp, scale=-1.0),
                 reads=["Bg"], writes=["Bg"])
            S.op("dve", lambda e: e.tensor_tensor(out=flat(kh128), in0=flat(kk), in1=flat(Bg), op=ALU.mult),
                 reads=["Bg"] + allk("kk"), writes=["kh128"])

            def tok_block(i, c0, n, hp=hp):
                par = i % 2
                PT, khT = PTs[par], khTs[par]
                bS, bO = (2, 3) if par == 0 else (6, 7)
                t0c, s0c = par * 256, par * 256
                if n == 128:
                    for hl in range(2):
                        o0 = hl * 128
                        S.op("pe", lambda e, hl=hl, o0=o0, c0=c0: e.matmul(
                            ps[:, bS, o0:o0 + 64], lhsT=kt[:, hl, c0:c0 + 128], rhs=qt[:, hl, c0:c0 + 64],
                            start=True, stop=True), reads=["kt", "qt"], writes=[("ps", bS)])
                        S.op("pe", lambda e, hl=hl, o0=o0, c0=c0: e.matmul(
                            ps[0:64, bS, o0 + 64:o0 + 128], lhsT=kh64[:, hl, c0:c0 + 64],
                            rhs=qt[:, hl, c0 + 64:c0 + 128], start=True, stop=True),
                            reads=["kh64", "qt"], writes=[("ps", bS)])
                        S.op("pe", lambda e, hl=hl, o0=o0, c0=c0: e.matmul(
                            ps[64:128, bS, o0 + 64:o0 + 128], lhsT=kt[:, hl, c0 + 64:c0 + 128],
                            rhs=qt[:, hl, c0 + 64:c0 + 128], start=True, stop=True),
                            reads=["kt", "qt"], writes=[("ps", bS)])
                    for hl in range(2):
                        S.op("dve", lambda e, hl=hl: e.tensor_tensor(
                            out=PT[:, hl, :], in0=ps[:, bS, hl * 128:(hl + 1) * 128], in1=self.mask2, op=ALU.mult),
                            reads=[("ps", bS), "mask2"], writes=[("PT", par, hl)])
                    for hl in range(2):
                        S.op("pe", lambda e, hl=hl, i=i: e.matmul(
                            ps[:, bO, hl * 128:(hl + 1) * 128], lhsT=vv[:, i, hl * 128:(hl + 1) * 128],
                            rhs=PT[:, hl, :], start=True, stop=True),
                            reads=[("vv", i), ("PT", par, hl)], writes=[("ps", bO)])
                    S.op("act", lambda e, hp=hp, c0=c0: e.activation(
                        out=oloc[:, 2 * hp:2 * hp + 2, c0:c0 + 128],
                        in_=ps[:, bO, 0:256].rearrange("p (h t) -> p h t", h=2), func=AF.Copy),
                        reads=[("ps", bO)], writes=[("oloc", hp, i)])
                for hl in range(2):
                    S.op("pe", lambda e, hl=hl, c0=c0, n=n: e.transpose(
                        out=psb(4)[0:n, t0c * 2 + hl * 128:t0c * 2 + (hl + 1) * 128], in_=kh128[:, hl, c0:c0 + n],
                        identity=self.ident_bf),
                        reads=["kh128", "ident"], writes=[("ps4", par)])
                S.op("dve", lambda e, n=n: e.tensor_copy(out=khT[0:n].rearrange("p h d -> p (h d)"),
                                                         in_=psb(4)[0:n, t0c * 2:t0c * 2 + 256]),
                     reads=[("ps4", par)], writes=[("khT", par)])
                for hl in range(2):
                    S.op("pe", lambda e, hl=hl, i=i, n=n: e.matmul(
                        ps[:, 5, s0c + hl * 128:s0c + (hl + 1) * 128], lhsT=khT[0:n, hl, :],
                        rhs=vv[0:n, i, hl * 128:(hl + 1) * 128], start=True, stop=True),
                        reads=[("khT", par), ("vv", i)], writes=[("ps5", par)])
                S.op("dve", lambda e, i=i: e.tensor_copy(out=xs_st[:, i, :], in_=ps[:, 5, s0c:s0c + 256]),
                     reads=[("ps5", par)], writes=[("xs_st", i)])
            for (i_, c0_, n_) in TL:
                tok_block(i_, c0_, n_)
            for q3 in range(3):
                S.dma("sp", lambda e, hp=hp, q3=q3: e.dma_start(
                    out=self.xs[q3].rearrange("p (i c) -> p i c", i=3)[:, :, hp * 256:(hp + 1) * 256],
                    in_=xs_st[:, 3 * q3:3 * q3 + 3, :]),
                    reads=[("xs_st", i) for i in range(9)], writes=[("xs", hp, q3)])
            S.dma("sp", lambda e, hp=hp: e.dma_start(
                out=self.xa.rearrange("p (i c) -> p i c", i=9)[:, :, 2 * hp:2 * hp + 2], in_=xa_st),
                reads=["xa_st"], writes=[("xa", hp)])
        S.barrier()
        rg = [[0, 1, 2, 3], [4, 5, 6, 7]]
        for q3 in range(3):
            S.coll(lambda e, q3=q3: e.collective_compute("AllGather", ALU.bypass, replica_groups=rg,
                                                         ins=[self.xs[q3]], outs=[self.xg[q3]]), writes=[("xg", q3)])
        S.coll(lambda e: e.collective_compute("AllGather", ALU.bypass, replica_groups=rg,
                                              ins=[self.xa], outs=[self.xag]), writes=["xag"])

    def hgrn_m2(self, strm):
        S, ps, nc = self.S, self.ps, self.nc
        R1 = 99840
        wbufs = [self.view(R1 + i * 8192, BF16, [KC, 256]) for i in range(2)]
        off = [R1 + 16384]

        def alloc(dt, shape):
            n = 1
            for x in shape:
                n *= x
            nb = n * (4 if dt == F32 else 2)
            nb = (nb + 63) // 64 * 64
            v = self.view(off[0], dt, shape)
            off[0] += nb
            return v
        sgate = alloc(BF16, [8, NREAL])
        Scur = alloc(F32, [8, 128]); SmF = alloc(F32, [8, 128])
        SAb = [alloc(BF16, [8, 128]) for _ in range(3)]
        Aall = alloc(F32, [4, 72])
        OF = alloc(F32, [8, 128]); OSQ = alloc(F32, [8, 128]); RS = alloc(F32, [8, 128])
        Qc = self.view(0, BF16, [8, NREAL]); oloc = self.view(16384, BF16, [8, NREAL])
        yhg = self.view(32768, BF16, [8, NREAL]); Smine = self.view(49152, BF16, [8, 8, 128])
        f2 = lambda v: v.rearrange("p h t -> p (h t)")
        RTB = TBS[0:2]

        for g4 in range(4):
            def ev_g(half, ti, t0, t1e, pv, bk, g4=g4):
                h = 2 * g4 + half
                S.op("act", lambda e: e.activation(out=sgate[:, h, t0:t1e], in_=pv, func=AF.Silu),
                     reads=[("ps", bk)], writes=[("sgate", h, ti)])
                S.op("dve", lambda e: e.tensor_scalar(out=sgate[:, h, t0:t1e], in0=sgate[:, h, t0:t1e],
                                                      scalar1=self.gnc[:, h:h + 1], scalar2=None, op0=ALU.mult),
                     reads=[("sgate", h, ti), "gn"], writes=[("sgate", h, ti)])
            self.proj_fm("m2", strm, self.gidx["hg%d" % g4], wbufs, RTB, ev_g)

        def out_block(i):
            c0 = 128 * i
            for h in range(8):
                bk = 2 + h // 4
                S.op("pe", lambda e, h=h, i=i, c0=c0, bk=bk: e.matmul(
                    ps[:, bk, (h % 4) * 128:(h % 4 + 1) * 128], lhsT=Smine[:, i, h, :], rhs=Qc[:, h, c0:c0 + 128],
                    start=True, stop=True),
                    reads=[("Smine", i), "Qc"], writes=[("ps", bk)])
            S.op("dve", lambda e, c0=c0: e.tensor_tensor(
                out=OF, in0=ps[:, 2:4, :].rearrange("p a (h t) -> p (a h) t", h=4), in1=oloc[:, :, c0:c0 + 128],
                op=ALU.add),
                reads=[("ps", 2), ("ps", 3), "oloc"], writes=["OF"])
            S.op("act", lambda e: e.activation(out=f2(OSQ), in_=f2(OF), func=AF.Square),
                 reads=["OF"], writes=["OSQ"])
            for a in range(2):
                S.op("pe", lambda e, a=a: e.matmul(ps[:, 4 + a, :], lhsT=self.ones128, rhs=f2(OSQ)[:, a * 512:(a + 1) * 512],
                                                   start=True, stop=True),
                     reads=["OSQ", "ones128"], writes=[("ps", 4 + a)])
            S.op("act", lambda e: e.activation(out=f2(RS), in_=ps[:, 4:6, :].rearrange("p a b -> p (a b)"),
                                               func=AF.Ln, bias=self.eps_rms, scale=1.0),
                 reads=[("ps", 4), ("ps", 5), "eps"], writes=["RS"])
            S.op("act", lambda e: e.activation(out=f2(RS), in_=f2(RS), func=AF.Exp, scale=-0.5),
                 reads=["RS"], writes=["RS"])

        def out_block_b(i):
            c0 = 128 * i
            S.op("dve", lambda e: e.tensor_tensor(out=f2(OF), in0=f2(OF), in1=f2(RS), op=ALU.mult),
                 reads=["OF", "RS"], writes=["OF"])
            S.op("dve", lambda e, c0=c0: e.tensor_tensor(out=yhg[:, :, c0:c0 + 128], in0=OF, in1=sgate[:, :, c0:c0 + 128],
                                                         op=ALU.mult),
                 reads=["OF"] + [("sgate", h, c0 // 512) for h in range(8)], writes=[("yhg", i)])

        S.dma("sp", lambda e: e.dma_start(out=Aall, in_=self.xag.rearrange("(r p) c -> p r c", p=128)),
              reads=["xag"], writes=["Aall"])
        xg3 = [x_.rearrange("(r p) (i c) -> r p i c", p=128, i=3) for x_ in self.xg]
        S.dma("sp", lambda e: e.dma_start(out=f2(SAb[2]), in_=xg3[2][0, :, 2, :]), reads=[("xg", 2)],
              writes=[("SAb", 2)])
        S.op("dve", lambda e: e.tensor_copy(out=f2(Scur), in_=f2(SAb[2])), reads=[("SAb", 2)], writes=["Scur"])
        for g in range(32):
            r, i = g % 4, g // 4
            sb = SAb[g % 3]
            S.dma("sp", lambda e, sb=sb, r=r, i=i: e.dma_start(out=f2(sb), in_=xg3[i // 3][r, :, i % 3, :]),
                  reads=[("xg", i // 3)], writes=[("SAb", g % 3)])
            if r == 0:
                S.op("dve", lambda e: e.tensor_scalar(out=f2(SmF), in0=f2(Scur), scalar1=self.selc[:, 0:1],
                                                      scalar2=None, op0=ALU.mult),
                     reads=["Scur", "sel"], writes=["SmF"])
            else:
                dst = SmF if r < 3 else Smine[:, i]
                S.op("dve", lambda e, r=r, dst=dst: e.scalar_tensor_tensor(
                    out=f2(dst), in0=f2(Scur), scalar=self.selc[:, r:r + 1], in1=f2(SmF),
                    op0=ALU.mult, op1=ALU.add),
                    reads=["Scur", "sel", "SmF"], writes=(["SmF"] if r < 3 else [("Smine", i)]))
            if g < 31:
                for h in range(8):
                    S.op("dve", lambda e, h=h, r=r, i=i, sb=sb: e.scalar_tensor_tensor(
                        out=Scur[:, h, :], in0=Scur[:, h, :], scalar=Aall[:, r, i * 8 + h:i * 8 + h + 1],
                        in1=sb[:, h, :], op0=ALU.mult, op1=ALU.add),
                        reads=["Scur", "Aall", ("SAb", g % 3)], writes=["Scur"])
            if g % 4 == 3:
                out_block(g // 4)
            if g % 4 == 1 and g >= 5:
                out_block_b((g - 5) // 4)
        out_block_b(7)

        S.barrier()

    def attn_m3(self, strm, part):
        S, ps, nc = self.S, self.ps, self.nc
        R1 = 99840
        psb = lambda bk: ps[:, bk, :].bitcast(BF16)
        K_all = self.view(R1, BF16, [2, 4112])
        V_all = self.view(R1 + 16448, BF16, [33, 258])
        IK_all = self.view(R1 + 33536, BF16, [4096])
        AugK = self.view(R1 + 41728, BF16, [4112])
        qT = self.view(R1 + 70656, BF16, [8, NREAL])
        iqT = self.view(R1 + 87040, BF16, [8, NREAL])
        sc = self.view(R1 + 49952, F32, [4096])
        wbufs = [self.view(R1 + i * 8192, BF16, [KC, 256]) for i in range(2)]
        Dg = self.view(R1 + 66336, BF16, [16, 128])
        yatt = self.view(0, BF16, [8, NREAL])
        mb = self.view(16384, BF16, [4096])
        mbT = self.view(24576, BF16, [32, 128])
        junk = self.view(49152, U8, [4096])
        iqz = self.view(49152 + 4096, BF16, [16, 128])
        rh = [self.view(57344 + q * 1024, BF16, [512]) for q in range(4)]
        ya = self.view(61440, BF16, [8, 128])
        PTb = [self.view(61440 + q * 2048, BF16, [1024]) for q in range(2)]
        cbt = self.view(65536, BF16, [4, 128])
        kst = self.view(32768, BF16, [2, NREAL])
        vst = self.view(32768 + 4096, BF16, [8, 258])
        ikst = self.view(32768 + 4096 + 4160, BF16, [NREAL])
        iktmp = self.view(32768 + 10304, F32, [64])
        ikn2 = self.view(32768 + 10304 + 256, BF16, [128])
        kmst = self.view(32768 + 10816, BF16, [2, NMETA])
        vmst = self.view(32768 + 10880, BF16, [258])
        cst = self.cst
        AugQ = cst[:, 664:1176].bitcast(BF16)
        AugR = cst[:, 1176:1688].bitcast(BF16)
        wq = cst[:, 1688:1816].rearrange("p (i h) -> p i h", h=16)
        H = cst[:, 1816:1848]
        Pt = cst[:, 1848:1976]
        g1 = cst[:, 1976:2008]
        mrow = cst[:, 2008:2016]; cc = cst[:, 2016:2024]; rs = cst[:, 2024:2032]
        Bt = cst[:, 2032:2033]; Wc = cst[:, 2033:2034]; mid = cst[:, 2034:2035]; cnt = cst[:, 2035:2036]
        u2 = cst[:, 2036:2037]; tau = cst[:, 2037:2038]; rstd1 = cst[:, 2038:2039]
        nslope = cst[:, 2040:2048]
        Qt = cst[:, 2048:2080].rearrange("p (r j) -> p r j", r=4)
        kmx = cst[:, 2080:2081]
        pw = cst[:, 2104:2136]
        gik = cst[:, 2136:2200]; bik = cst[:, 2200:2264]
        st6 = cst[:, 2264:2270]; mv = cst[:, 2270:2272]
        TL = [(i, 128 * i, 128) for i in range(8)] + [(8, NREAL, NMETA)]
        RTB = TBS[0:2]
        NB = 16

        if part == 0:
            S.dma("sp", lambda e: e.dma_start(out=cst[:, 2040:2264], in_=self.catt), writes=["catt"])
            S.dma("sp", lambda e: e.dma_start(out=Pt, in_=self.cmat_d[:, 384:512]), writes=["Pt"])
            S.op("dve", lambda e: e.memset(vst.rearrange("p i (k c) -> p i k c", k=2)[:, :, :, 128:129], 1.0),
                 writes=["vst1"])
            S.op("dve", lambda e: e.memset(vmst.rearrange("p (k c) -> p k c", k=2)[:, :, 128:129], 1.0),
                 writes=["V1"])

            def ev_k(half, ti, t0, t1e, pv, bk):
                if ti < 2:
                    S.op("act", lambda e: e.activation(out=kst[:, half, t0:t1e], in_=pv, func=AF.Copy),
                         reads=[("ps", bk)], writes=[("kst", half, ti)])
                else:
                    S.op("act", lambda e: e.activation(out=kmst[:, half, :], in_=pv, func=AF.Copy),
                         reads=[("ps", bk)], writes=[("Kmeta", half)])
            self.proj_fm("m3", strm, self.gidx["ak"], wbufs, TBS, ev_k)

            def ev_v(i, c0, n, pv, bk):
                src = pv.rearrange("p (k c) -> p k c", k=2)
                if i < 8:
                    dst = vst[:, i, :].rearrange("p (k c) -> p k c", k=2)[:, :, 0:128]
                    S.op("act", lambda e: e.activation(out=dst, in_=src, func=AF.Copy),
                         reads=[("ps", bk), "vst1"], writes=[("vst", i)])
                else:
                    dst = vmst[0:n, :].rearrange("p (k c) -> p k c", k=2)[:, :, 0:128]
                    S.op("act", lambda e: e.activation(out=dst, in_=src, func=AF.Copy),
                         reads=[("ps", bk), "V1"], writes=["Vmeta"])
            self.proj_tm("m3", strm, self.gidx["av"], wbufs, TL, 256, ev_v)

            def ev_ik(i, c0, n, pv, bk):
                S.op("dve", lambda e: e.bn_stats(out=st6, in_=pv[:, 0:64]), reads=[("ps", bk)], writes=["st6"])
                S.op("dve", lambda e: e.bn_aggr(out=mv, in_=st6), reads=["st6"], writes=["mv"])
                S.op("act", lambda e: e.activation(out=rstd1, in_=mv[:, 1:2], func=AF.Sqrt, bias=self.eps_ik, scale=1.0),
                     reads=["mv", "eps"], writes=["rstd1"])
                S.op("dve", lambda e: e.reciprocal(out=rstd1, in_=rstd1), reads=["rstd1"], writes=["rstd1"])
                S.op("dve", lambda e: e.tensor_scalar(out=iktmp, in0=pv[:, 0:64], scalar1=mv[:, 0:1], scalar2=rstd1,
                                                      op0=ALU.subtract, op1=ALU.mult),
                     reads=[("ps", bk), "mv", "rstd1"], writes=["iktmp"])
                S.op("dve", lambda e: e.tensor_tensor(out=iktmp, in0=iktmp, in1=gik, op=ALU.mult),
                     reads=["iktmp", "catt"], writes=["iktmp"])
                S.op("dve", lambda e: e.tensor_tensor(out=ikn2[:, 0:64], in0=iktmp, in1=bik, op=ALU.add),
                     reads=["iktmp", "catt"], writes=["ikn2a"])
                S.op("dve", lambda e: e.tensor_copy(out=ikn2[:, 64:128], in_=ikn2[:, 0:64]),
                     reads=["ikn2a"], writes=["ikn2b"])
                S.op("act", lambda e, i=i: e.activation(out=wq[:, i, :], in_=pv[:, 64:80], func=AF.Copy,
                                                        scale=0.25 * 0.125),
                     reads=[("ps", bk)], writes=[("wq", i)])
                S.op("pe", lambda e: e.transpose(out=psb(2)[:, 0:128], in_=ikn2, identity=self.ident_bf),
                     reads=["ikn2a", "ikn2b", "ident"], writes=[("ps", 2)])
                S.op("act", lambda e, c0=c0: e.activation(out=ikst[:, c0:c0 + 128], in_=psb(2)[:, 0:128], func=AF.Copy),
                     reads=[("ps", 2)], writes=[("ikst", i)])
            self.proj_tm("m3", strm, self.gidx["ikw"], wbufs, TL[0:8], 80, ev_ik)

            S.dma("sp", lambda e: e.dma_start(out=self.ks.rearrange("p (k t) -> p k t", k=2), in_=kst),
                  reads=[("kst", hh, ti) for hh in range(2) for ti in range(2)], writes=["ks"])
            S.dma("sp", lambda e: e.dma_start(out=self.vs[:, 0:2064].rearrange("p (i c) -> p i c", i=8), in_=vst),
                  reads=[("vst", i) for i in range(8)] + ["vst1"], writes=["vs"])
            S.dma("sp", lambda e: e.dma_start(out=self.vs[:, 2064:3088], in_=ikst),
                  reads=[("ikst", i) for i in range(8)], writes=["vs2"])
            S.dma("sp", lambda e: e.dma_start(out=self.kms.rearrange("p (k t) -> p k t", k=2), in_=kmst),
                  reads=[("Kmeta", 0), ("Kmeta", 1)], writes=["kms"])
            S.dma("sp", lambda e: e.dma_start(out=self.vms, in_=vmst[0:NMETA, :]),
                  reads=["Vmeta", "V1"], writes=["vms"])
            S.barrier()
            rg = [[0, 1, 2, 3], [4, 5, 6, 7]]
            S.coll(lambda e: e.collective_compute("AllGather", ALU.bypass, replica_groups=rg,
                                                  ins=[self.ks], outs=[self.kg]), writes=["kg"])
            S.coll(lambda e: e.collective_compute("AllGather", ALU.bypass, replica_groups=rg,
                                                  ins=[self.vs], outs=[self.vg]), writes=["vg"])
            return

        if part == 1:
            for g4 in range(4):
                def ev_q(half, ti, t0, t1e, pv, bk, g4=g4):
                    h = 2 * g4 + half
                    S.op("act", lambda e: e.activation(out=qT[:, h, t0:t1e], in_=pv, func=AF.Copy, scale=128.0 ** -0.5),
                         reads=[("ps", bk)], writes=[("qT", h, ti)])
                self.proj_fm("m3", strm, self.gidx["aq%d" % g4], wbufs, RTB, ev_q)
            for g4 in range(4):
                def ev_iq(half, ti, t0, t1e, pv, bk, g4=g4):
                    h = 2 * g4 + half
                    S.op("dve", lambda e: e.tensor_copy(out=iqT[:, h, t0:t1e], in_=pv),
                         reads=[("ps", bk)], writes=[("iqT", h, ti)])
                self.proj_fm("m3", strm, self.gidx["iq%d" % g4], wbufs, RTB, ev_iq)
            return

        S.op("pool", lambda e: e.memset(AugK[0:65, :], 0.0), writes=["AugK"])
        S.op("pool", lambda e: e.memset(AugR[0:65, :], 0.0), writes=["AugR"])
        for rr in range(3):
            S.dma("pool", lambda e, rr=rr: e.dma_start(out=AugK[32 * rr:32 * rr + 1, :], in_=self.augk[rr:rr + 1, :]),
                  writes=["AugK"])
        for rr in range(2):
            S.dma("pool", lambda e, rr=rr: e.dma_start(out=AugR[32 * rr:32 * rr + 1, :], in_=self.augs[rr:rr + 1, :]),
                  writes=["AugR"])
        S.dma("pool", lambda e: e.dma_start(out=cbt, in_=self.cbt_d.rearrange("p (r s) -> p r s", r=4)), writes=["cbt"])
        for r in range(4):
            S.dma("sp", lambda e, r=r: e.dma_start(
                out=K_all[:, :, r * 1024:(r + 1) * 1024],
                in_=self.kg[r * 128:(r + 1) * 128, :].rearrange("p (k t) -> p k t", k=2)),
                reads=["kg"], writes=["K_all"])
            S.dma("sp", lambda e, r=r: e.dma_start(
                out=V_all[:, r * 8:(r + 1) * 8, :],
                in_=self.vg[r * 128:(r + 1) * 128, 0:2064].rearrange("p (i c) -> p i c", i=8)),
                reads=["vg"], writes=["V_all"])
            S.dma("sp", lambda e, r=r: e.dma_start(
                out=IK_all[:, r * 1024:(r + 1) * 1024], in_=self.vg[r * 128:(r + 1) * 128, 2064:3088]),
                reads=["vg"], writes=["IK_all"])
        S.dma("sp", lambda e: e.dma_start(out=K_all[:, :, 4096:4112], in_=self.kms.rearrange("p (k t) -> p k t", k=2)),
              reads=["kms"], writes=[("Kmeta", 0), ("Kmeta", 1)])
        S.dma("sp", lambda e: e.dma_start(out=V_all[0:NMETA, 32, :], in_=self.vms), reads=["vms"], writes=["Vmeta"])
        S.barrier()

        S.op("pool", lambda e: e.memset(iqz.rearrange("p h t -> p (h t)"), 0.0), writes=["iqz"])
        sc4 = sc.rearrange("p (r c) -> p r c", r=4)
        mb4 = mb.rearrange("p (r c) -> p r c", r=4)
        jk4 = junk.rearrange("p (r c) -> p r c", r=4)
        def geom(i):
            q0 = 128 * i
            nk = 128 * (i + 1)
            pieces = [(r, c0, min(512, nk - c0)) for r in range(4) for c0 in range(0, nk, 512)]
            return q0, nk, pieces

        def st_idx(i):
            q0, nk, pieces = geom(i)
            for h in range(16):
                S.op("act", lambda e, h=h: e.activation(out=Dg[:, h, :], in_=self.ident_bf, func=AF.Copy,
                                                        scale=wq[:, i, h:h + 1]),
                     reads=["ident", ("wq", i)], writes=["Dg"])
            for h in range(16):
                hb = h % 2
                eng = "act"
                if eng == "pool":
                    S.op("pool", lambda e, h=h, hb=hb: e.tensor_copy(
                        out=iqz[hb * 64:(hb + 1) * 64, h, :], in_=iqT[hb * 64:(hb + 1) * 64, h // 2, q0:q0 + 128]),
                        reads=["iqT", "iqz"], writes=[("iqzh", h)])
                else:
                    S.op("act", lambda e, h=h, hb=hb: e.activation(
                        out=iqz[hb * 64:(hb + 1) * 64, h, :], in_=iqT[hb * 64:(hb + 1) * 64, h // 2, q0:q0 + 128],
                        func=AF.Copy),
                        reads=["iqT", "iqz"], writes=[("iqzh", h)])
            for pi, (r, c0, cn) in enumerate(pieces):
                col0 = r * 1024 + c0
                accb = 4 + pi % 2

                def head_mm(h, cn=cn, col0=col0):
                    bk, hb = h % 4, h % 2
                    S.op("pe", lambda e: e.matmul(
                        ps[:, bk, 0:cn], lhsT=iqz[:, h, :],
                        rhs=IK_all[:, col0:col0 + cn], start=True, stop=True),
                        reads=["IK_all", ("iqzh", h)], writes=[("ps", bk)])
                    if h % 2 == 0:
                        S.op("act", lambda e: e.activation(out=rh[bk][:, 0:cn], in_=ps[:, bk, 0:cn], func=AF.Relu),
                             reads=[("ps", bk)], writes=[("rh", bk)])
                    else:
                        S.op("dve", lambda e: e.tensor_scalar(out=rh[bk][:, 0:cn], in0=ps[:, bk, 0:cn], scalar1=0.0,
                                                              scalar2=None, op0=ALU.max),
                             reads=[("ps", bk)], writes=[("rh", bk)])

                def head_acc(h, cn=cn, accb=accb):
                    bk = h % 4
                    S.op("pe", lambda e: e.matmul(
                        ps[:, accb, 0:cn], lhsT=Dg[:, h, :], rhs=rh[bk][:, 0:cn], start=(h == 0), stop=(h == 15)),
                        reads=["Dg", ("rh", bk)], writes=[("ps", accb)])
                for h in range(16):
                    head_mm(h)
                    if h >= 2:
                        head_acc(h - 2)
                head_acc(14)
                head_acc(15)
                S.op("act", lambda e, accb=accb, col0=col0, cn=cn: e.activation(
                    out=sc[:, col0:col0 + cn], in_=ps[:, accb, 0:cn], func=AF.Copy),
                    reads=[("ps", accb)], writes=["sc"])

        def st_bis(i):
            q0, nk, pieces = geom(i)
            scv, mbv, jkv = sc4[:, :, 0:nk], mb4[:, :, 0:nk], jk4[:, :, 0:nk]
            S.op("dve", lambda e: e.reduce_max(out=Bt, in_=scv, axis=AX.XY, apply_absolute_value=True),
                 reads=["sc"], writes=["Bt"])
            S.op("dve", lambda e: e.tensor_tensor(out=sc4[:, :, q0:q0 + 128], in0=sc4[:, :, q0:q0 + 128], in1=cbt,
                                                  op=ALU.add),
                 reads=["sc", "cbt", "Bt"], writes=["sc"])
            S.op("dve", lambda e: e.tensor_scalar(out=Wc, in0=Bt, scalar1=2.0002, scalar2=1e-6,
                                                  op0=ALU.mult, op1=ALU.add), reads=["Bt"], writes=["Wc"])
            S.op("dve", lambda e: e.tensor_scalar(out=H[:, 0:NB + 1], in0=pw[:, 0:NB + 1], scalar1=Wc, scalar2=None,
                                                  op0=ALU.mult), reads=["Wc", "catt"], writes=["H"])
            S.op("dve", lambda e: e.memset(mid, 0.0), writes=["mid"])
            for k in range(NB):
                S.op("dve", lambda e: e.tensor_scalar(
                    out=jkv, in0=scv, scalar1=mid, scalar2=0.0, op0=ALU.is_ge, op1=ALU.add, accum_out=cnt),
                    reads=["sc", "mid"], writes=["junk", "cnt"])
                S.op("dve", lambda e, k=k: e.tensor_scalar(out=u2, in0=cnt, scalar1=256.0, scalar2=H[:, k:k + 1],
                                                           op0=ALU.is_ge, op1=ALU.mult),
                     reads=["cnt", "H"], writes=["u2"])
                S.op("dve", lambda e, k=k: e.scalar_tensor_tensor(out=mid, in0=mid, scalar=H[:, k + 1:k + 2], in1=u2,
                                                                  op0=ALU.subtract, op1=ALU.add),
                     reads=["mid", "H", "u2"], writes=["mid"])
            S.op("dve", lambda e: e.tensor_tensor(out=tau, in0=mid, in1=H[:, NB:NB + 1], op=ALU.subtract),
                 reads=["mid", "H"], writes=["tau"])
            S.op("dve", lambda e: e.tensor_scalar(
                out=mbv, in0=scv, scalar1=tau, scalar2=-30000.0, op0=ALU.is_lt, op1=ALU.mult),
                reads=["sc", "tau"], writes=["mb"])
            nb_ = i + 1
            sc5 = scv.rearrange("p r (j s) -> p r j s", s=128)
            mb5 = mbv.rearrange("p r (j s) -> p r j s", s=128)
            S.op("dve", lambda e: e.tensor_tensor(
                out=sc5, in0=mb5, in1=Pt.unsqueeze(1).unsqueeze(1).to_broadcast([128, 4, nb_, 128]), op=ALU.add),
                reads=["mb", "Pt", "sc"], writes=["sc"])
            g1v = g1.rearrange("p (r j) -> p r j", r=4)[:, :, 0:nb_]
            S.op("dve", lambda e: e.tensor_reduce(out=g1v, in_=sc5, axis=AX.X, op=ALU.max),
                 reads=["sc"], writes=["g1"])
            S.op("dve", lambda e: e.tensor_tensor(out=g1v, in0=g1v, in1=Qt[:, :, 0:nb_], op=ALU.add),
                 reads=["g1", "catt"], writes=["g1"])
            S.op("dve", lambda e: e.tensor_reduce(out=kmx, in_=g1v, axis=AX.XY, op=ALU.max),
                 reads=["g1"], writes=["kmx"])
            S.op("dve", lambda e: e.tensor_scalar(out=kmx, in0=kmx, scalar1=15.0, scalar2=None, op0=ALU.max),
                 reads=["kmx"], writes=["kmx"])
            S.op("dve", lambda e: e.tensor_scalar(out=cc, in0=nslope, scalar1=kmx, scalar2=None, op0=ALU.mult),
                 reads=["kmx", "catt"], writes=["cc"])

        def st_mbT(i):
            kts = [(r, ip) for r in range(4) for ip in range(i + 1)]
            for g0 in range(0, len(kts), 8):
                grp = kts[g0:g0 + 8]
                bk = 6 + (g0 // 8) % 2
                for s_, (r, ip) in enumerate(grp):
                    S.op("pe", lambda e, bk=bk, s_=s_, r=r, ip=ip: e.transpose(
                        out=psb(bk)[:, s_ * 128:(s_ + 1) * 128], in_=mb[:, r * 1024 + ip * 128:r * 1024 + ip * 128 + 128],
                        identity=self.ident_bf),
                        reads=["mb", "ident"], writes=[("ps", bk)])
                for s_, (r, ip) in enumerate(grp):
                    S.op("act", lambda e, bk=bk, s_=s_, r=r, ip=ip: e.activation(
                        out=mbT[:, r * 8 + ip, :], in_=psb(bk)[:, s_ * 128:(s_ + 1) * 128], func=AF.Copy),
                        reads=[("ps", bk)], writes=["mbT"])

        def st_passA(i):
            q0, nk, pieces = geom(i)
            S.op("dve", lambda e: e.memset(ps[:, 5:8, :].rearrange("p a b -> p (a b)"), 0.0),
                 writes=[("ps", 5), ("ps", 6), ("ps", 7)])
            Dc = PTb[1].rearrange("p (h t) -> p h t", h=8)
            for h in range(8):
                S.op("dve", lambda e, h=h: e.tensor_scalar(out=Dc[:, h, :], in0=self.ident_bf, scalar1=cc[:, h:h + 1],
                                                           scalar2=None, op0=ALU.mult),
                     reads=["ident", "cc"], writes=[("PTb", 1)])
            for a_ in range(2):
                S.op("pe", lambda e, a_=a_: e.matmul(ps[:, 4, :], lhsT=self.ones_bf,
                                                     rhs=PTb[1][:, a_ * 512:(a_ + 1) * 512],
                                                     start=True, stop=True),
                     reads=[("PTb", 1), "ones_bf"], writes=[("ps", 4)])
                S.op("dve", lambda e, a_=a_: e.tensor_copy(out=AugR[64:65, a_ * 512:(a_ + 1) * 512], in_=ps[64:65, 4, :]),
                     reads=[("ps", 4)], writes=["AugR"])

        def st_passB(i):
            q0, nk, pieces = geom(i)
            ktl = [(r * 1024 + ip * 128, r * 8 + ip, 128) for r in range(4) for ip in range(i + 1)] + [(4096, 32, NMETA)]
            Oreg = lambda h: ps[:, 5 + h // 3, (h % 3) * 129:(h % 3 + 1) * 129]

            def logits(qi):
                col0, vt, n = ktl[qi]
                meta = (n == NMETA)
                pair = (0, 1) if qi % 2 == 0 else (2, 3)
                for h in range(8):
                    kvh = h // 4
                    out = ps[0:n, pair[h // 4], (h % 4) * 128:(h % 4 + 1) * 128]
                    S.op("pe", lambda e, out=out, kvh=kvh, h=h: e.matmul(
                        out, lhsT=K_all[:, kvh, col0:col0 + n], rhs=qT[:, h, q0:q0 + 128], start=True, stop=False),
                        reads=["K_all", "qT", ("Kmeta", kvh)], writes=[("ps", pair[h // 4])])
                    S.op("pe", lambda e, out=out, h=h: e.matmul(
                        out, lhsT=AugK[0:65, col0:col0 + n], rhs=AugR[0:65, h * 128:(h + 1) * 128],
                        start=False, stop=meta),
                        reads=["AugK", "AugR"], writes=[("ps", pair[h // 4])])
                    if not meta:
                        S.op("pe", lambda e, out=out: e.matmul(
                            out, lhsT=self.ident_bf, rhs=mbT[:, vt, :], start=False, stop=True),
                            reads=["mbT", "ident"], writes=[("ps", pair[h // 4])])
                pt = PTb[qi % 2]
                S.op("act", lambda e: e.activation(
                    out=pt[0:n, :], in_=ps[0:n, pair[0]:pair[0] + 2, :].rearrange("p a b -> p (a b)"), func=AF.Exp),
                    reads=[("ps", pair[0]), ("ps", pair[1])], writes=[("PTb", qi % 2)])

            def pv(qi):
                col0, vt, n = ktl[qi]
                pt = PTb[qi % 2]
                for h in range(8):
                    kvh = h // 4
                    S.op("pe", lambda e, h=h, kvh=kvh: e.matmul(
                        Oreg(h), lhsT=pt[0:n, h * 128:(h + 1) * 128], rhs=V_all[0:n, vt, kvh * 129:(kvh + 1) * 129],
                        start=False, stop=(qi == len(ktl) - 1)),
                        reads=[("PTb", qi % 2), "V_all", "Vmeta"], writes=[("ps", 5 + h // 3)])
            logits(0)
            for qi in range(len(ktl)):
                if qi + 1 < len(ktl):
                    logits(qi + 1)
                pv(qi)

        def st_fin(i):
            q0 = 128 * i
            for b3 in range(3):
                nh = 3 if b3 < 2 else 2
                Ov = ps[:, 5 + b3, 0:nh * 129].rearrange("p (h c) -> p h c", c=129)
                S.op("dve", lambda e, Ov=Ov, b3=b3, nh=nh: e.reciprocal(out=rs[:, 3 * b3:3 * b3 + nh], in_=Ov[:, :, 128]),
                     reads=[("ps", 5 + b3)], writes=[("rs", b3)])
                S.op("dve", lambda e, Ov=Ov, b3=b3, nh=nh: e.tensor_tensor(
                    out=ya[:, 3 * b3:3 * b3 + nh, :], in0=Ov[:, :, 0:128],
                    in1=rs[:, 3 * b3:3 * b3 + nh].unsqueeze(2).to_broadcast([128, nh, 128]), op=ALU.mult),
                    reads=[("ps", 5 + b3), ("rs", b3)], writes=[("ya", b3), ("PTb", 0)])
            for h in range(8):
                S.op("pe", lambda e, h=h: e.transpose(out=psb(4)[:, h * 128:(h + 1) * 128], in_=ya[:, h, :],
                                                      identity=self.ident_bf),
                     reads=[("ya", h // 3), ("PTb", 0), "ident"], writes=[("ps", 4)])
            S.op("act", lambda e: e.activation(out=yatt[:, :, q0:q0 + 128],
                                               in_=psb(4).rearrange("p (h t) -> p h t", h=8), func=AF.Copy),
                 reads=[("ps", 4)], writes=[("yatt", i)])

        st_idx(0)
        st_bis(0)
        st_mbT(0)
        for i in range(8):
            if i + 1 < 8:
                st_idx(i + 1)
            st_passA(i)
            if i + 1 < 8:
                st_bis(i + 1)
            st_passB(i)
            st_fin(i)
            if i + 1 < 8:
                st_mbT(i + 1)
        S.barrier()

    def merge_m4(self, strm, cg, cb):
        S, ps, nc = self.S, self.ps, self.nc
        R1 = 99840
        RTB = TBS[0:2]
        yatt = self.view(0, BF16, [8, NREAL]); yhg = self.view(32768, BF16, [8, NREAL])
        merged = self.view(R1, BF16, [KC, NREAL])
        o = R1 + 32768
        wga = [self.view(o + q * 8192, BF16, [KC, 256]) for q in range(2)]
        wgh = [self.view(o + 16384 + q * 8192, BF16, [KC, 256]) for q in range(2)]
        wba = [self.view(o + 32768 + q * 4096, BF16, [8, 256]) for q in range(2)]
        wbh = [self.view(o + 40960 + q * 4096, BF16, [8, 256]) for q in range(2)]
        tm = [self.view(o + 49152 + q * 2048, F32, [512]) for q in range(4)]
        for mg in range(8):
            q = mg % 2
            S.dma("pool", lambda e, q=q, mg=mg: e.dma_start(out=wga[q], in_=self.win[self.gidx["ga%d" % mg]]),
                  writes=[("wga", q)])
            S.dma("pool", lambda e, q=q, mg=mg: e.dma_start(out=wgh[q], in_=self.win[self.gidx["gh%d" % mg]]),
                  writes=[("wgh", q)])
            S.dma("pool", lambda e, q=q, mg=mg: e.dma_start(out=wba[q], in_=self.wba_d[mg]), writes=[("wba", q)])
            S.dma("pool", lambda e, q=q, mg=mg: e.dma_start(out=wbh[q], in_=self.wbh_d[mg]), writes=[("wbh", q)])
            for half in range(2):
                mc = 2 * mg + half
                hs = slice(half * 128, (half + 1) * 128)
                for ti, (t0, t1e) in enumerate(RTB):
                    pp = (half * 2 + ti) % 2
                    b0 = 4 * pp
                    for k in range(KC):
                        S.op("pe", lambda e, b0=b0, q=q, k=k, hs=hs, t0=t0, t1e=t1e: e.matmul(
                            ps[:, b0, :], lhsT=wga[q][:, k, hs], rhs=strm[:, k, t0:t1e],
                            start=(k == 0), stop=(k == KC - 1)),
                            reads=[("wga", q), "strm"], writes=[("ps", b0)])
                    for k in range(8):
                        S.op("pe", lambda e, b0=b0, q=q, k=k, hs=hs, t0=t0, t1e=t1e: e.matmul(
                            ps[:, b0 + 1, :], lhsT=wba[q][:, k, hs], rhs=yatt[:, k, t0:t1e],
                            start=(k == 0), stop=(k == 7)),
                            reads=[("wba", q), "yatt"], writes=[("ps", b0 + 1)])
                    for k in range(KC):
                        S.op("pe", lambda e, b0=b0, q=q, k=k, hs=hs, t0=t0, t1e=t1e: e.matmul(
                            ps[:, b0 + 2, :], lhsT=wgh[q][:, k, hs], rhs=strm[:, k, t0:t1e],
                            start=(k == 0), stop=(k == KC - 1)),
                            reads=[("wgh", q), "strm"], writes=[("ps", b0 + 2)])
                    for k in range(8):
                        S.op("pe", lambda e, b0=b0, q=q, k=k, hs=hs, t0=t0, t1e=t1e: e.matmul(
                            ps[:, b0 + 3, :], lhsT=wbh[q][:, k, hs], rhs=yhg[:, k, t0:t1e],
                            start=(k == 0), stop=(k == 7)),
                            reads=[("wbh", q), "yhg"], writes=[("ps", b0 + 3)])
                    ta, th = tm[2 * pp], tm[2 * pp + 1]
                    S.op("act", lambda e, ta=ta, b0=b0: e.activation(out=ta, in_=ps[:, b0, :], func=AF.Sigmoid),
                         reads=[("ps", b0)], writes=[("tm", 2 * pp)])
                    S.op("dve", lambda e, ta=ta, b0=b0: e.tensor_tensor(out=ta, in0=ta, in1=ps[:, b0 + 1, :], op=ALU.mult),
                         reads=[("tm", 2 * pp), ("ps", b0 + 1)], writes=[("tm", 2 * pp)])
                    S.op("act", lambda e, th=th, b0=b0: e.activation(out=th, in_=ps[:, b0 + 2, :], func=AF.Sigmoid),
                         reads=[("ps", b0 + 2)], writes=[("tm", 2 * pp + 1)])
                    S.op("dve", lambda e, th=th, b0=b0: e.tensor_tensor(out=th, in0=th, in1=ps[:, b0 + 3, :], op=ALU.mult),
                         reads=[("tm", 2 * pp + 1), ("ps", b0 + 3)], writes=[("tm", 2 * pp + 1)])
                    S.op("dve", lambda e, ta=ta, th=th, mc=mc, t0=t0, t1e=t1e: e.tensor_tensor(
                        out=merged[:, mc, t0:t1e], in0=ta, in1=th, op=ALU.add),
                        reads=[("tm", 2 * pp), ("tm", 2 * pp + 1)], writes=[("merged", mc, ti)])
        S.barrier()
        z = self.view(R1 + 32768, F32, [KC, NREAL])
        wo = [self.view(53760 + q * 4096, BF16, [KC, 128]) for q in range(2)]
        strm_out = self.view(0, BF16, [KC, NREAL])
        for dc in range(KC):
            q = dc % 2
            S.dma("pool", lambda e, q=q, dc=dc: e.dma_start(out=wo[q], in_=self.wo_d[dc]), writes=[("wo", q)])
            S.dma("sp", lambda e, dc=dc: e.dma_start(out=z[:, dc, :], in_=self.h1s[dc * 128:(dc + 1) * 128, 0:NREAL]),
                  writes=[("l2", "z", dc, ti) for ti in range(2)])
            for ti, (t0, t1e) in enumerate(RTB):
                pb = (dc * 2 + ti) % 2
                for k in range(KC):
                    S.op("pe", lambda e, pb=pb, q=q, k=k, t0=t0, t1e=t1e: e.matmul(
                        ps[:, pb, :], lhsT=wo[q][:, k, :], rhs=merged[:, k, t0:t1e],
                        start=(k == 0), stop=(k == KC - 1)),
                        reads=[("wo", q), ("merged", k, ti)], writes=[("ps", pb)])
                S.op("dve", lambda e, pb=pb, dc=dc, t0=t0, t1e=t1e: e.scalar_tensor_tensor(
                    out=z[:, dc, t0:t1e], in0=ps[:, pb, :], scalar=1.0 / ALPHA, in1=z[:, dc, t0:t1e],
                    op0=ALU.mult, op1=ALU.add),
                    reads=[("ps", pb), ("l2", "z", dc, ti)], writes=[("l2", "z", dc, ti)])
        self.ln_apply("l2", z, RTB, cg, cb, 33280, LN_EPS / (ALPHA * ALPHA), strm_out=strm_out,
                      resid_out=self.h2s)
        S.barrier()

    def eps_col(self, val):
        return self.eps_cols[val]

    def build(self):
        nc, S = self.nc, self.S
        stage = self.stage
        xT = self.din("xT", [D, NT])
        wg1 = self.din("wg1", [FC // 2, 128, KC, 256])
        wu1 = self.din("wu1", [FC // 2, 128, KC, 256])
        wd1 = self.din("wd1", [KC, 128, FC, 128])
        wg2 = self.din("wg2", [FC // 2, 128, KC, 256])
        wu2 = self.din("wu2", [FC // 2, 128, KC, 256])
        wd2 = self.din("wd2", [KC, 128, FC, 128])
        cvec = self.din("cvec", [128, 128])
        cmat = self.cmat_d = self.din("cmat", [128, 512])
        self.catt = self.din("catt", [128, 224])
        self.augk = self.din("augk", [3, 4112])
        self.augs = self.din("augs", [2, 1024])
        self.augq = self.din("augq", [8, 1024])
        self.cbt_d = self.din("cbt", [128, 512])
        self.win = self.din("win", [len(GROUPS), 128, KC, 256])
        self.wba_d = self.din("wba", [8, 128, 8, 256])
        self.wbh_d = self.din("wbh", [8, 128, 8, 256])
        self.wo_d = self.din("wo", [KC, 128, KC, 128])
        self.gidx = {nm: i for i, (nm, _, _) in enumerate(GROUPS)}
        self.h1s = h1s = self.dscratch("h1s", [D, NT])
        self.h2s = self.dscratch("h2s", [D, NREAL])
        self.xs = [self.dscratch("xs%d" % q, [128, 3 * 1024], BF16) for q in range(3)]
        self.xg = [self.dscratch("xg%d" % q, [512, 3 * 1024], BF16) for q in range(3)]
        self.xa = self.dscratch("xa", [128, 72])
        self.xag = self.dscratch("xag", [512, 72])
        self.ks = self.dscratch("ks", [128, 2048], BF16)
        self.kg = self.dscratch("kg", [512, 2048], BF16)
        self.vs = self.dscratch("vs", [128, 3088], BF16)
        self.vg = self.dscratch("vg", [512, 3088], BF16)
        self.kms = self.dscratch("kms", [128, 2 * NMETA], BF16)
        self.vms = self.dscratch("vms", [NMETA, 258], BF16)
        if stage == 1:
            dbg = self.dout("dbg", [D, NT])
        elif stage in (3, 4):
            dbg = self.dout("dbg", [128, 8 * NREAL])
            self.dbg2 = self.dout("dbg2", [128, 12000])
        elif stage == 5:
            dbg = self.dout("dbg", [D, NREAL])
        else:
            outT = self.dout("outT", [D, NREAL])

        from contextlib import ExitStack
        with ExitStack() as es:
            self.arena = es.enter_context(nc.sbuf_tensor("arena", [128, ARENA_F32], F32))
            self.cst = es.enter_context(nc.sbuf_tensor("cst", [128, CONST_F32], F32))
            self.ps = es.enter_context(nc.psum_tensor("ps", [128, 8, 512], F32))
            esems = {e: es.enter_context(nc.semaphore("sem_" + e)) for e in ENGS}
            dsems = [es.enter_context(nc.semaphore("dsem%d" % i)) for i in range(S.n_dma_sems + 8)]
            block = es.enter_context(nc.Block())
            cst = self.cst
            self.ones_f32 = cst[:, 0:128]
            cv = self.cv = cst[:, 128:256]
            epsA = cst[:, 256:257]
            self.eps_rms = cst[:, 257:258]
            self.eps_ik = cst[:, 258:259]
            self.eps_cols = {LN_EPS / (ALPHA * ALPHA): epsA}
            self.ident_bf = cst[:, 264:328].bitcast(BF16)
            self.ones_bf = cst[:, 328:392].bitcast(BF16)
            self.mask2 = cst[:, 392:520]
            self.ones128 = cst[:, 520:648]
            self.lbc = cst[:, 648:656]
            self.omlc = cst[:, 656:664]
            self.gnc = cv[:, 112:120]
            self.selc = cv[:, 120:124]
            S.op("dve", lambda e: e.memset(self.ones_f32, 1.0 / D), writes=["ones"])
            S.op("dve", lambda e: e.memset(self.ones128, 1.0 / 128), writes=["ones128"])
            S.op("dve", lambda e: e.memset(epsA, LN_EPS / (ALPHA * ALPHA)), writes=["eps"])
            S.op("dve", lambda e: e.memset(self.eps_rms, RMS_EPS), writes=["eps"])
            S.op("dve", lambda e: e.memset(self.eps_ik, LN_EPS), writes=["eps"])
            S.dma("sp", lambda e: e.dma_start(out=cv, in_=cvec), writes=["cv"])
            S.dma("sp", lambda e: e.dma_start(out=self.mask2, in_=cmat[:, 256:384]), writes=["mask2"])
            S.dma("pool", lambda e: e.dma_start(out=self.ident_bf, in_=cmat[:, 0:128]), writes=["ident"])
            S.dma("pool", lambda e: e.dma_start(out=self.ones_bf, in_=cmat[:, 128:256]), writes=["ones_bf"])
            S.op("dve", lambda e: e.tensor_tensor(out=self.lbc, in0=cv[:, 96:104], in1=cv[:, 104:112], op=ALU.subtract),
                 reads=["cv"], writes=["lb"])
            S.op("act", lambda e: e.activation(out=self.lbc, in_=self.lbc, func=AF.Sigmoid), reads=["lb"], writes=["lb"])
            S.op("dve", lambda e: e.tensor_scalar(out=self.omlc, in0=self.lbc, scalar1=-1.0, scalar2=1.0,
                                                  op0=ALU.mult, op1=ALU.add), reads=["lb"], writes=["lb"])
            strm0 = self.view(0, BF16, [KC, NT])
            S.dma("pool", lambda e: e.dma_start(out=strm0, in_=xT.rearrange("(k p) t -> p k t", p=128)),
                  writes=[("f1", "sin")])
            S.barrier()
            self.ffn_phase("f1", NT, TBS, strm0, 66560, wg1, wu1, wd1, xT, cv[:, 0:16], cv[:, 16:32],
                           resid_out=(dbg if stage == 1 else h1s))
            strm1 = self.view(66560, BF16, [KC, NT])
            if stage >= 2:
                if stage >= 4:
                    self.attn_m3(strm1, 0)
                self.hgrn_m1(strm1)
                if stage >= 4:
                    self.attn_m3(strm1, 1)
                self.hgrn_m2(strm1)
            if stage == 3:
                yhg = self.view(32768, BF16, [8 * NREAL])
                S.dma("pool", lambda e: e.dma_start(out=dbg, in_=yhg), reads=[])
            if stage >= 4:
                self.attn_m3(strm1, 2)
            if stage == 4:
                yat = self.view(0, BF16, [8 * NREAL])
                S.dma("pool", lambda e: e.dma_start(out=dbg, in_=yat), reads=[])
            if stage >= 5:
                if stage == 5:
                    self.h2s = dbg
                self.merge_m4(strm1, cv[:, 32:48], cv[:, 48:64])
            if stage >= 6:
                strm2 = self.view(0, BF16, [KC, NREAL])
                self.ffn_phase("f2", NREAL, TBS[0:2], strm2, 66560, wg2, wu2, wd2, self.h2s, cv[:, 64:80],
                               cv[:, 80:96], final_out=outT)
            S.emit(block, esems, dsems)
        return nc


def _lay_gu(w):
    return np.ascontiguousarray(w.reshape(KC, 128, FC // 2, 256).transpose(2, 1, 0, 3))


def _lay_d(w):
    return np.ascontiguousarray(w.reshape(FC, 128, KC, 128).transpose(2, 1, 0, 3))


def _fm(v):
    return np.ascontiguousarray(v.reshape(KC, 128).T)


def _core_tokens(x, meta, c):
    b, j = c // 4, c % 4
    blocks = [x[b, 128 * (4 * i + j):128 * (4 * i + j) + 128] for i in range(8)]
    tok = np.concatenate(blocks + [meta], axis=0)
    return np.ascontiguousarray(tok.T)


def _mk_groups():
    g = []
    for hp in range(4):
        g += [("hf%d" % hp, 3664 + 256 * hp, 256), ("hq%d" % hp, 2640 + 256 * hp, 256),
              ("hi%d" % hp, 4688 + 256 * hp, 256)]
    for i in range(4):
        g.append(("hg%d" % i, 5712 + 256 * i, 256))
    g += [("ak", 1024, 256), ("av", 1280, 256), ("ikw", 2560, 80)]
    for i in range(4):
        g.append(("aq%d" % i, 256 * i, 256))
    for i in range(4):
        g.append(("iq%d" % i, 1536 + 256 * i, 256))
    for i in range(8):
        g.append(("ga%d" % i, 6736 + 256 * i, 256))
        g.append(("gh%d" % i, 8784 + 256 * i, 256))
    return g


GROUPS = _mk_groups()


def _lay_win(w):
    out = np.zeros((len(GROUPS), 128, KC, 256), np.float32)
    for gi, (nm, c0, nc_) in enumerate(GROUPS):
        out[gi, :, :, 0:nc_] = w[:, c0:c0 + nc_].reshape(KC, 128, nc_).transpose(1, 0, 2)
    return out


def prepare(inputs, stage):
    f = lambda k: np.asarray(inputs[k], dtype=np.float32)
    x, meta = f("x"), f("meta")
    shared = {
        "wg1": _lay_gu(f("ffn1_w_gate")[0]), "wu1": _lay_gu(f("ffn1_w_up")[0]),
        "wd1": _lay_d(f("ffn1_w_down")[0]),
        "win": _lay_win(f("w_in")[0]),
        "wg2": _lay_gu(f("ffn2_w_gate")[0]), "wu2": _lay_gu(f("ffn2_w_up")[0]),
        "wd2": _lay_d(f("ffn2_w_down")[0]),
        "wba": np.ascontiguousarray(f("w_branch_att")[0].reshape(8, 128, 8, 256).transpose(2, 1, 0, 3)),
        "wbh": np.ascontiguousarray(f("w_branch_hg")[0].reshape(8, 128, 8, 256).transpose(2, 1, 0, 3)),
        "wo": np.ascontiguousarray(f("w_out")[0].reshape(KC, 128, KC, 128).transpose(2, 1, 0, 3)),
    }
    slopes = (2.0 ** -(np.arange(8) + 1.0)).astype(np.float32)
    c = np.arange(4096)
    kpos = np.concatenate([16 + 128 * (4 * ((c % 1024) // 128) + c // 1024) + c % 128, np.arange(16)]).astype(np.float32)
    augk = np.stack([np.floor(kpos / 64.0), kpos % 64.0, np.ones_like(kpos)], 0).astype(np.float32)
    augs = np.stack([np.repeat(64.0 * slopes, 128), np.repeat(slopes, 128)], 0).astype(np.float32)
    shared["augk"] = augk
    shared["augs"] = augs
    cvec = np.zeros((128, 128), np.float32)
    cvec[:, 0:16] = _fm(f("ln1_g")[0]); cvec[:, 16:32] = _fm(f("ln1_b")[0])
    cvec[:, 32:48] = _fm(f("ln2_g")[0]); cvec[:, 48:64] = _fm(f("ln2_b")[0])
    cvec[:, 64:80] = _fm(f("ln3_g")[0]); cvec[:, 80:96] = _fm(f("ln3_b")[0])
    lbl = f("hg_lb_logits")
    cvec[:, 96:104] = lbl[0].reshape(8, 128).T
    cvec[:, 104:112] = lbl[1].reshape(8, 128).T
    cvec[:, 112:120] = f("hg_norm_g")[0].T
    cmat = np.zeros((128, 512), np.float32)
    cmat[:, 384:512] = np.arange(128, dtype=np.float32)[None, :]
    cmat[:, 0:128] = np.eye(128, dtype=np.float32)
    cmat[:, 128:256] = 1.0
    sidx = np.arange(128)[:, None]; tidx = np.arange(128)[None, :]
    cmat[:, 256:384] = (((sidx <= tidx) & ((sidx // 64) == (tidx // 64))) | ((sidx < 64) & (tidx >= 64))).astype(np.float32)
    shared["cmat"] = cmat
    maps = []
    for c in range(8):
        m = dict(shared)
        m["xT"] = _core_tokens(x, meta, c)
        cv = cvec.copy()
        cv[:, 120 + (c % 4)] = 1.0
        m["cvec"] = cv
        j = c % 4
        p = np.arange(128, dtype=np.float32)
        qpos = np.stack([16 + 128 * (4 * i + j) + p for i in range(8)], 0)
        m["augq"] = np.ascontiguousarray((-slopes[None, :, None] * qpos[:, None, :]).reshape(8, 1024).astype(np.float32))
        catt = np.zeros((128, 224), np.float32)
        catt[:, 0:8] = -slopes[None, :]
        catt[:, 8:40] = np.array([16 + 128 * r + 512 * jj for r in range(4) for jj in range(8)], np.float32)[None, :]
        catt[:, 64:96] = (2.0 ** -(np.arange(32) + 1.0))[None, :]
        catt[:, 96:160] = f("idx_k_norm_g")[0][None, :]
        catt[:, 160:224] = f("idx_k_norm_b")[0][None, :]
        m["catt"] = catt
        tt = np.arange(128)[:, None, None]; rr = np.arange(4)[None, :, None]; ss = np.arange(128)[None, None, :]
        m["cbt"] = np.where(128 * (rr - j) + (ss - tt) > 0, -1e30, 0.0).astype(np.float32).reshape(128, 512)
        maps.append(m)
    return maps


_NC_CACHE = {}


def kernel(**inputs):
    stage = int(os.environ.get("KSTAGE", "9"))
    if stage not in _NC_CACHE:
        _NC_CACHE[stage] = Builder(stage).build()
    nc = _NC_CACHE[stage]
    maps = prepare(inputs, stage)
    res = run_bass_kernel_spmd(nc, maps, core_ids=list(range(8)))
    if stage == 4:
        return [(r["dbg"], r["dbg2"]) for r in res.results]
    if stage < 6:
        return [r["dbg"] for r in res.results]
    out = np.zeros((2, 4096, D), np.float32)
    for c in range(8):
        b, j = c // 4, c % 4
        o = res.results[c]["outT"]
        for i in range(8):
            g = 4 * i + j
            out[b, 128 * g:128 * g + 128] = o[:, 128 * i:128 * i + 128].T
    return out
```

```python
import os
import numpy as np
import concourse.bass as bass
import concourse.mybir as mybir
from concourse.bass_utils import run_bass_kernel_spmd

F32 = mybir.dt.float32
BF16 = mybir.dt.bfloat16
U8 = mybir.dt.uint8
AF = mybir.ActivationFunctionType
ALU = mybir.AluOpType
AX = mybir.AxisListType

D = 2048
DFF = 5632
NMETA = 16
NREAL = 1024
NT = NREAL + NMETA
KC = D // 128
FC = DFF // 128
TBS = [(0, 512), (512, 1024), (1024, 1040)]
ALPHA = 2.0 ** 0.25
LN_EPS = 1e-5
RMS_EPS = 1e-6

ENGS = ("pe", "act", "dve", "pool", "sp")


class Op:
    __slots__ = ("eng", "fn", "deps", "is_dma", "dsem", "dval", "sig", "sigidx", "waits")

    def __init__(self, eng, fn, is_dma=False):
        self.eng = eng
        self.fn = fn
        self.deps = []
        self.is_dma = is_dma
        self.dsem = None
        self.dval = 0
        self.sig = False
        self.sigidx = 0
        self.waits = []


class Sched:
    def __init__(self, n_dma_sems=40, same_engine_sync=True):
        self.ops = {e: [] for e in ENGS}
        self.lastw = {}
        self.readers = {}
        self.n_dma = 0
        self.n_dma_sems = n_dma_sems
        self.dma_hist = {}
        self.same_engine_sync = same_engine_sync
        self.n_coll = 0

    def _add(self, op, reads, writes):
        deps = set()
        for k in reads:
            w = self.lastw.get(k)
            if w is not None:
                deps.add(w)
        for k in writes:
            w = self.lastw.get(k)
            if w is not None:
                deps.add(w)
            for r in self.readers.get(k, ()):
                deps.add(r)
        op.deps = list(deps)
        for k in reads:
            self.readers.setdefault(k, []).append(op)
        for k in writes:
            self.lastw[k] = op
            self.readers[k] = []
        self.ops[op.eng].append(op)
        return op

    def op(self, eng, fn, reads=(), writes=()):
        return self._add(Op(eng, fn), reads, writes)

    def dma(self, q, fn, reads=(), writes=()):
        op = Op(q, fn, is_dma=True)
        slot = self.n_dma % self.n_dma_sems
        op.dsem = slot
        op.dval = 16 * (self.n_dma // self.n_dma_sems + 1)
        self.n_dma += 1
        self._add(op, reads, writes)
        prev = self.dma_hist.get(slot)
        if prev is not None:
            op.deps.append(prev)
        self.dma_hist[slot] = op
        return op

    def coll(self, fn, reads=(), writes=()):
        op = Op("pool", fn, is_dma=True)
        op.dsem = self.n_dma_sems + self.n_coll
        op.dval = 1
        self.n_coll += 1
        self._add(op, reads, writes)
        self.dma_hist[op.dsem] = op
        return op

    def barrier(self):
        lasts = []
        for e in ENGS:
            for o in reversed(self.ops[e]):
                if not o.is_dma and o.fn is not None:
                    lasts.append(o)
                    break
        dmas = list(self.dma_hist.values())
        for e in ENGS:
            op = Op(e, None)
            op.deps = [o for o in lasts if o.eng != e] + dmas
            self.ops[e].append(op)
        self.lastw = {}
        self.readers = {}

    def _skip(self, d, op):
        return d.eng == op.eng and (d.eng in ("pe", "sp") or not self.same_engine_sync)

    def finalize(self):
        for e in ENGS:
            for op in self.ops[e]:
                for d in op.deps:
                    if not d.is_dma and not self._skip(d, op):
                        d.sig = True
        for e in ENGS:
            c = 0
            for op in self.ops[e]:
                if op.sig:
                    c += 1
                    op.sigidx = c
        for e in ENGS:
            waited = {}
            for op in self.ops[e]:
                need = {}
                for d in op.deps:
                    if d.is_dma:
                        key, val = ("d", d.dsem), d.dval
                    else:
                        if self._skip(d, op):
                            continue
                        key, val = ("e", d.eng), d.sigidx
                    if waited.get(key, 0) >= val:
                        continue
                    if need.get(key, 0) < val:
                        need[key] = val
                for k, v in need.items():
                    waited[k] = v
                op.waits = list(need.items())

    def emit(self, block, esems, dsems):
        self.finalize()
        regs = {"pe": block.tensor, "act": block.scalar, "dve": block.vector,
                "pool": block.gpsimd, "sp": block.sync}
        final = {d.dsem: d.dval for d in self.dma_hist.values()}

        def make(e):
            ops = self.ops[e]

            def body(eng):
                for op in ops:
                    for (kind, which), val in op.waits:
                        eng.wait_ge(dsems[which] if kind == "d" else esems[which], val)
                    if op.fn is None:
                        continue
                    ins = op.fn(eng)
                    if op.is_dma:
                        ins.then_inc(dsems[op.dsem], 16 if op.dsem < self.n_dma_sems else 1)
                    elif op.sig:
                        ins.then_inc(esems[e], 1)
                if e == "sp":
                    for slot, val in final.items():
                        eng.wait_ge(dsems[slot], val)
            return body

        for e in ENGS:
            regs[e](make(e))


ARENA_F32 = 50816
CONST_F32 = 2304


class Builder:
    def __init__(self, stage):
        self.stage = stage
        self.nc = bass.Bass("TRN2", target_bir_lowering=False)
        self.S = Sched()
        self.dram = {}

    def din(self, name, shape, dt=F32):
        self.dram[name] = self.nc.dram_tensor(name, list(shape), dt, kind="ExternalInput").ap()
        return self.dram[name]

    def dout(self, name, shape, dt=F32):
        self.dram[name] = self.nc.dram_tensor(name, list(shape), dt, kind="ExternalOutput").ap()
        return self.dram[name]

    def dscratch(self, name, shape, dt=F32):
        self.dram[name] = self.nc.dram_tensor(name, list(shape), dt, kind="Internal").ap()
        return self.dram[name]

    def view(self, off_bytes, dt, shape):
        n = 1
        for s in shape:
            n *= s
        esz = 4 if dt == F32 else (1 if dt == U8 else 2)
        assert off_bytes % 4 == 0
        nbytes = n * esz
        assert nbytes % 4 == 0
        assert off_bytes + nbytes <= ARENA_F32 * 4, (off_bytes, nbytes)
        v = self.arena[:, off_bytes // 4:(off_bytes + nbytes) // 4]
        if dt != F32:
            v = v.bitcast(dt)
        if len(shape) == 2:
            v = v.rearrange("p (a b) -> p a b", b=shape[1])
        elif len(shape) == 3:
            v = v.rearrange("p (a b c) -> p a b c", b=shape[1], c=shape[2])
        return v

    def ln_apply(self, tag, z, tbs, cg, cb, toff, eps_eff, strm_out=None, alias_key=None, resid_out=None,
                 final_out=None):
        S, ps = self.S, self.ps
        zsq = [self.view(toff + i * 2048, BF16, [512]) for i in range(2)]
        meanb = self.view(toff + 4096, F32, [512])
        rstdb = self.view(toff + 6144, F32, [512])
        t1 = [self.view(toff + 8192 + i * 2048, F32, [512]) for i in range(2)]
        t2 = [self.view(toff + 12288 + i * 2048, F32, [512]) for i in range(2)]
        o32 = [self.view(toff + 16384 + i * 2048, F32, [512]) for i in range(2)]
        ones = self.ones_f32
        for ti, (t0, t1e) in enumerate(tbs):
            n = t1e - t0
            pm, pq = ps[:, 6, 0:n], ps[:, 7, 0:n]
            for dc in range(KC):
                zq = zsq[dc % 2]
                S.op("act", lambda e, zq=zq, dc=dc, t0=t0, t1e=t1e, n=n: e.activation(
                    out=zq[:, 0:n], in_=z[:, dc, t0:t1e], func=AF.Square, scale=float(D) ** -0.5),
                    reads=[(tag, "z", dc, ti)], writes=[(tag, "zsq", dc % 2)])
                S.op("pe", lambda e, pm=pm, dc=dc, t0=t0, t1e=t1e: e.matmul(
                    pm, lhsT=ones, rhs=z[:, dc, t0:t1e], start=(dc == 0), stop=(dc == KC - 1)),
                    reads=[(tag, "z", dc, ti)], writes=[("ps", 6)])
                S.op("pe", lambda e, pq=pq, zq=zq, dc=dc, n=n: e.matmul(
                    pq, lhsT=self.ones_bf, rhs=zq[:, 0:n], start=(dc == 0), stop=(dc == KC - 1)),
                    reads=[(tag, "zsq", dc % 2), "ones_bf"], writes=[("ps", 7)])
            S.op("act", lambda e, pm=pm, n=n: e.activation(out=meanb[:, 0:n], in_=pm, func=AF.Copy),
                 reads=[("ps", 6)], writes=[(tag, "meanb")])
            S.op("dve", lambda e, n=n: e.tensor_tensor(out=rstdb[:, 0:n], in0=meanb[:, 0:n], in1=meanb[:, 0:n],
                                                       op=ALU.mult),
                 reads=[(tag, "meanb")], writes=[(tag, "rstdb")])
            S.op("dve", lambda e, pq=pq, n=n: e.tensor_tensor(out=rstdb[:, 0:n], in0=pq, in1=rstdb[:, 0:n],
                                                              op=ALU.subtract),
                 reads=[("ps", 7), (tag, "rstdb")], writes=[(tag, "rstdb")])
            S.op("act", lambda e, n=n: e.activation(out=rstdb[:, 0:n], in_=rstdb[:, 0:n], func=AF.Sqrt,
                                                    bias=self.eps_cols[eps_eff], scale=1.0),
                 reads=[(tag, "rstdb")], writes=[(tag, "rstdb")])
            S.op("dve", lambda e, n=n: e.reciprocal(out=rstdb[:, 0:n], in_=rstdb[:, 0:n]),
                 reads=[(tag, "rstdb")], writes=[(tag, "rstdb")])
            S.op("dve", lambda e, n=n: e.scalar_tensor_tensor(out=meanb[:, 0:n], in0=meanb[:, 0:n], scalar=-1.0,
                                                              in1=rstdb[:, 0:n], op0=ALU.mult, op1=ALU.mult),
                 reads=[(tag, "meanb"), (tag, "rstdb")], writes=[(tag, "meanb")])
            for dc in range(KC):
                a, b_, o = t1[dc % 2], t2[dc % 2], o32[dc % 2]
                S.op("dve", lambda e, a=a, dc=dc, t0=t0, t1e=t1e, n=n: e.tensor_tensor(
                    out=a[:, 0:n], in0=z[:, dc, t0:t1e], in1=rstdb[:, 0:n], op=ALU.mult),
                    reads=[(tag, "z", dc, ti), (tag, "rstdb")], writes=[(tag, "t1", dc % 2)])
                S.op("dve", lambda e, a=a, b_=b_, n=n: e.tensor_tensor(
                    out=b_[:, 0:n], in0=a[:, 0:n], in1=meanb[:, 0:n], op=ALU.add),
                    reads=[(tag, "t1", dc % 2), (tag, "meanb")], writes=[(tag, "t2", dc % 2)])
                S.op("act", lambda e, b_=b_, o=o, dc=dc, n=n: e.activation(
                    out=o[:, 0:n], in_=b_[:, 0:n], func=AF.Identity,
                    bias=cb[:, dc:dc + 1], scale=cg[:, dc:dc + 1]),
                    reads=[(tag, "t2", dc % 2)], writes=[(tag, "o32", dc % 2)])
                if strm_out is not None:
                    wk = [(tag, "sout", dc, ti)] + ([(tag, alias_key, dc, ti)] if alias_key else [])
                    S.op("act", lambda e, b_=b_, dc=dc, t0=t0, t1e=t1e, n=n: e.activation(
                        out=strm_out[:, dc, t0:t1e], in_=b_[:, 0:n], func=AF.Identity,
                        bias=cb[:, dc:dc + 1], scale=cg[:, dc:dc + 1]),
                        reads=[(tag, "t2", dc % 2)], writes=wk)
                dst = resid_out if final_out is None else final_out
                if final_out is None or t0 < NREAL:
                    S.dma("sp", lambda e, o=o, dc=dc, t0=t0, t1e=t1e, n=n, dst=dst: e.dma_start(
                        out=dst[dc * 128:(dc + 1) * 128, t0:t1e], in_=o[:, 0:n]),
                        reads=[(tag, "o32", dc % 2)])

    def ffn_phase(self, tag, ntok, tbs, strm_in, strm_out_off, wg, wu, wd, resid, cg, cb,
                  resid_out=None, final_out=None):
        S, nc = self.S, self.nc
        ps = self.ps
        C_OFF = 33280
        B_OFF = 66560
        D_OFF = B_OFF + FC * NT * 2
        T_OFF = D_OFF + 2 * FC * 128 * 2
        hT = self.view(B_OFF, BF16, [FC, ntok])
        wgu = [self.view(C_OFF + i * 16384, BF16, [2, KC, 256]) for i in range(2)]
        wdb = [self.view(D_OFF + i * FC * 128 * 2, BF16, [FC, 128]) for i in range(2)]
        z = self.view(0, F32, [KC, ntok])
        sil = [self.view(T_OFF + 8192 + i * 2048, F32, [512]) for i in range(2)]
        strm_out = self.view(strm_out_off, BF16, [KC, ntok]) if final_out is None else None
        c_scale = 0.5 / ALPHA
        eps_eff = LN_EPS / (ALPHA * ALPHA)
        nb = len(tbs)

        for g in range(FC // 2):
            wb = wgu[g % 2]
            kb = (tag, "wgu", g % 2)
            S.dma("pool", lambda e, wb=wb, g=g: e.dma_start(out=wb[:, 0], in_=wg[g]), writes=[(kb, 0)])
            S.dma("pool", lambda e, wb=wb, g=g: e.dma_start(out=wb[:, 1], in_=wu[g]), writes=[(kb, 1)])
            for fcl in range(2):
                fc = 2 * g + fcl
                for ti, (t0, t1e) in enumerate(tbs):
                    n = t1e - t0
                    pb = (fc * nb + ti) % 2
                    pg, pu = ps[:, 2 * pb, 0:n], ps[:, 2 * pb + 1, 0:n]
                    for k in range(KC):
                        S.op("pe", lambda e, pg=pg, wb=wb, k=k, fcl=fcl, t0=t0, t1e=t1e: e.matmul(
                            pg, lhsT=wb[:, 0, k, fcl * 128:(fcl + 1) * 128], rhs=strm_in[:, k, t0:t1e],
                            start=(k == 0), stop=(k == KC - 1)),
                            reads=[(kb, 0), (tag, "sin")], writes=[("ps", 2 * pb)])
                    for k in range(KC):
                        S.op("pe", lambda e, pu=pu, wb=wb, k=k, fcl=fcl, t0=t0, t1e=t1e: e.matmul(
                            pu, lhsT=wb[:, 1, k, fcl * 128:(fcl + 1) * 128], rhs=strm_in[:, k, t0:t1e],
                            start=(k == 0), stop=(k == KC - 1)),
                            reads=[(kb, 1), (tag, "sin")], writes=[("ps", 2 * pb + 1)])
                    sb = sil[pb]
                    S.op("act", lambda e, sb=sb, pg=pg, n=n: e.activation(out=sb[:, 0:n], in_=pg, func=AF.Silu),
                         reads=[("ps", 2 * pb)], writes=[(tag, "sil", pb)])
                    S.op("dve", lambda e, sb=sb, pu=pu, n=n, fc=fc, t0=t0, t1e=t1e: e.tensor_tensor(
                        out=hT[:, fc, t0:t1e], in0=sb[:, 0:n], in1=pu, op=ALU.mult),
                        reads=[(tag, "sil", pb), ("ps", 2 * pb + 1)], writes=[(tag, "hT", fc, ti)])

        S.barrier()
        for dc in range(KC):
            wb = wdb[dc % 2]
            kb = (tag, "wd", dc % 2)
            S.dma("pool", lambda e, wb=wb, dc=dc: e.dma_start(out=wb, in_=wd[dc]), writes=[kb])
            S.dma("sp", lambda e, dc=dc: e.dma_start(out=z[:, dc, :], in_=resid[dc * 128:(dc + 1) * 128, 0:ntok]),
                  writes=[(tag, "z", dc, ti) for ti in range(nb)])
            for ti, (t0, t1e) in enumerate(tbs):
                n = t1e - t0
                pb = 4 + (dc * nb + ti) % 2
                py = ps[:, pb, 0:n]
                for f in range(FC):
                    S.op("pe", lambda e, py=py, wb=wb, f=f, t0=t0, t1e=t1e: e.matmul(
                        py, lhsT=wb[:, f, :], rhs=hT[:, f, t0:t1e], start=(f == 0), stop=(f == FC - 1)),
                        reads=[kb, (tag, "hT", f, ti)], writes=[("ps", pb)])
                S.op("dve", lambda e, py=py, dc=dc, t0=t0, t1e=t1e: e.scalar_tensor_tensor(
                    out=z[:, dc, t0:t1e], in0=py, scalar=c_scale, in1=z[:, dc, t0:t1e],
                    op0=ALU.mult, op1=ALU.add),
                    reads=[("ps", pb), (tag, "z", dc, ti)], writes=[(tag, "z", dc, ti)])
        self.ln_apply(tag, z, tbs, cg, cb, T_OFF, eps_eff, strm_out=strm_out, alias_key="hT",
                      resid_out=resid_out, final_out=final_out)
        S.barrier()

    def proj_fm(self, tag, strm, gi, wbufs, tbs, evac, banks=(0, 1), parity=[0]):
        S, ps = self.S, self.ps
        wb = wbufs[parity[0] % 2]
        kb = ("wb", parity[0] % 2)
        parity[0] += 1
        S.dma("pool", lambda e, wb=wb, gi=gi: e.dma_start(out=wb, in_=self.win[gi]), writes=[kb])
        cnt = 0
        for half in range(2):
            for ti, (t0, t1e) in enumerate(tbs):
                n = t1e - t0
                bk = banks[cnt % len(banks)]
                cnt += 1
                pv = ps[:, bk, 0:n]
                for k in range(KC):
                    S.op("pe", lambda e, pv=pv, wb=wb, k=k, half=half, t0=t0, t1e=t1e: e.matmul(
                        pv, lhsT=wb[:, k, half * 128:(half + 1) * 128], rhs=strm[:, k, t0:t1e],
                        start=(k == 0), stop=(k == KC - 1)),
                        reads=[kb, "strm"], writes=[("ps", bk)])
                evac(half, ti, t0, t1e, pv, bk)

    def proj_tm(self, tag, strm, gi, wbufs, tls, ncols, evac, banks=(0, 1), parity=[0]):
        S, ps = self.S, self.ps
        wb = wbufs[parity[0] % 2]
        kb = ("wb", parity[0] % 2)
        parity[0] += 1
        S.dma("pool", lambda e, wb=wb, gi=gi: e.dma_start(out=wb, in_=self.win[gi]), writes=[kb])
        for cnt, (i, c0, n) in enumerate(tls):
            bk = banks[cnt % len(banks)]
            pv = ps[0:n, bk, 0:ncols]
            for k in range(KC):
                S.op("pe", lambda e, pv=pv, wb=wb, k=k, c0=c0, n=n: e.matmul(
                    pv, lhsT=strm[:, k, c0:c0 + n], rhs=wb[:, k, 0:ncols],
                    start=(k == 0), stop=(k == KC - 1)),
                    reads=[kb, "strm"], writes=[("ps", bk)])
            evac(i, c0, n, pv, bk)

    def hgrn_m1(self, strm):
        S, ps, nc = self.S, self.ps, self.nc
        R1 = 99840
        wbufs = [self.view(R1 + i * 8192, BF16, [KC, 256]) for i in range(2)]
        off = [R1 + 16384]

        def alloc(dt, shape):
            n = 1
            for x in shape:
                n *= x
            nb = n * (4 if dt == F32 else 2)
            nb = (nb + 63) // 64 * 64
            v = self.view(off[0], dt, shape)
            off[0] += nb
            return v
        logf = alloc(F32, [2, NT]); Bg = alloc(F32, [2, NT]); Bsh = alloc(F32, [2, NT])
        tA = alloc(F32, [2, NT]); tB = alloc(F32, [2, NT])
        kk = alloc(BF16, [2, NT]); qs = alloc(BF16, [2, NT]); qt = alloc(BF16, [2, NT])
        kt = alloc(BF16, [2, NT]); kh64 = alloc(BF16, [2, NT]); kh128 = alloc(BF16, [2, NT])
        vv = alloc(BF16, [9, 256])
        PTs = [alloc(BF16, [2, 128]) for _ in range(2)]; khTs = [alloc(BF16, [2, 128]) for _ in range(2)]
        xs_st = alloc(BF16, [9, 256]); xa_st = alloc(F32, [9, 2])
        Qc = self.view(0, BF16, [8, NREAL]); oloc = self.view(16384, BF16, [8, NREAL])
        cv = self.cv
        lbc, omlc = self.lbc, self.omlc
        psb = lambda bk: ps[:, bk, :].bitcast(BF16)
        TL = [(i, 128 * i, 128) for i in range(8)] + [(8, NREAL, NMETA)]
        flat = lambda v: v.rearrange("p h t -> p (h t)")
        r64 = lambda v: v[:, :, 0:NREAL].rearrange("p h (c t) -> p h c t", t=64)
        r128 = lambda v: v[:, :, 0:NREAL].rearrange("p h (c t) -> p h c t", t=128)
        mt = lambda v: v[:, :, NREAL:NT]

        for hp in range(4):
            T = ("m1", hp)
            def ev_f(half, ti, t0, t1e, pv, bk, hp=hp):
                h = 2 * hp + half
                S.op("act", lambda e: e.activation(out=tA[:, half, t0:t1e], in_=pv, func=AF.Sigmoid),
                     reads=[("ps", bk)], writes=[("tA", half, ti)])
                S.op("dve", lambda e: e.tensor_scalar(out=tA[:, half, t0:t1e], in0=tA[:, half, t0:t1e],
                                                      scalar1=omlc[:, h:h + 1], scalar2=lbc[:, h:h + 1],
                                                      op0=ALU.mult, op1=ALU.add),
                     reads=[("tA", half, ti), "lb"], writes=[("tA", half, ti)])
                S.op("act", lambda e: e.activation(out=logf[:, half, t0:t1e], in_=tA[:, half, t0:t1e], func=AF.Ln),
                     reads=[("tA", half, ti)], writes=[("logf", half, ti)])
                S.op("dve", lambda e: e.tensor_scalar(out=kk[:, half, t0:t1e], in0=tA[:, half, t0:t1e],
                                                      scalar1=-1.0, scalar2=1.0, op0=ALU.mult, op1=ALU.add),
                     reads=[("tA", half, ti)], writes=[("kk", half, ti)])
            self.proj_fm(T, strm, self.gidx["hf%d" % hp], wbufs, TBS, ev_f)

            def ev_q(half, ti, t0, t1e, pv, bk):
                S.op("act", lambda e: e.activation(out=qs[:, half, t0:t1e], in_=pv, func=AF.Silu),
                     reads=[("ps", bk)], writes=[("qs", half, ti)])
            self.proj_fm(T, strm, self.gidx["hq%d" % hp], wbufs, TBS, ev_q)

            def ev_v(i, c0, n, pv, bk):
                S.op("act", lambda e: e.activation(out=vv[0:n, i, :], in_=pv, func=AF.Copy),
                     reads=[("ps", bk)], writes=[("vv", i)])
            self.proj_tm(T, strm, self.gidx["hi%d" % hp], wbufs, TL, 256, ev_v)

            allk = lambda nm: [(nm, hl, ti) for hl in range(2) for ti in range(3)]
            S.op("dve", lambda e: e.tensor_tensor_scan(out=flat(Bg), data0=flat(logf), data1=flat(logf),
                                                       initial=0.0, op0=ALU.add, op1=ALU.min),
                 reads=allk("logf"), writes=["Bg"])
            S.op("dve", lambda e: e.memset(flat(Bsh)[:, 0:1], 0.0), writes=["Bsh0"])
            S.op("act", lambda e: e.activation(out=flat(Bsh)[:, 1:2 * NT], in_=flat(Bg)[:, 0:2 * NT - 1], func=AF.Copy),
                 reads=["Bg"], writes=["Bsh"])
            S.op("dve", lambda e: e.tensor_tensor(out=r64(tB), in0=r64(Bg),
                                                  in1=r64(Bsh)[:, :, :, 0:1].to_broadcast([128, 2, 16, 64]),
                                                  op=ALU.subtract),
                 reads=["Bg", "Bsh", "Bsh0"], writes=["tBr"])
            S.op("dve", lambda e: e.tensor_tensor(out=mt(tB), in0=mt(Bg),
                                                  in1=mt(Bsh)[:, :, 0:1].to_broadcast([128, 2, NMETA]),
                                                  op=ALU.subtract),
                 reads=["Bg", "Bsh", "Bsh0"], writes=["tBm"])
            S.op("dve", lambda e: e.tensor_tensor(out=r128(tA), in0=r128(Bg),
                                                  in1=r128(Bsh)[:, :, :, 0:1].to_broadcast([128, 2, 8, 128]),
                                                  op=ALU.subtract),
                 reads=["Bg", "Bsh", "Bsh0"] + allk("tA"), writes=["tAr"] + allk("tA"))
            S.op("dve", lambda e: e.tensor_copy(out=mt(tA), in_=mt(tB)),
                 reads=["tBm"], writes=["tAm"])
            TBk, TAk = ["tBr", "tBm"], ["tAr", "tAm"] + allk("tA")
            S.op("act", lambda e: e.activation(out=flat(Bg), in_=flat(tB), func=AF.Exp),
                 reads=TBk + ["Bsh", "tAr"], writes=["Bg"])
            S.op("dve", lambda e: e.tensor_tensor(out=flat(qt), in0=flat(qs), in1=flat(Bg), op=ALU.mult),
                 reads=["Bg"] + allk("qs"), writes=["qt"])
            S.op("act", lambda e: e.activation(out=flat(Bsh), in_=flat(tB), func=AF.Exp, scale=-1.0),
                 reads=TBk + ["Bsh", "tAr", "Bsh0"], writes=["Bsh", "Bsh0"])
            S.op("dve", lambda e: e.tensor_tensor(out=flat(kt), in0=flat(kk), in1=flat(Bsh), op=ALU.mult),
                 reads=["Bsh"] + allk("kk"), writes=["kt"])
            S.op("dve", lambda e: e.tensor_tensor(out=r64(Bg), in0=r64(tB),
                                                  in1=r64(tB)[:, :, :, 63:64].to_broadcast([128, 2, 16, 64]),
                                                  op=ALU.subtract),
                 reads=TBk + ["qt"], writes=["Bg"])
            S.op("act", lambda e: e.activation(out=r64(Bg), in_=r64(Bg), func=AF.Exp, scale=-1.0),
                 reads=["Bg"], writes=["Bg"])
            S.op("dve", lambda e: e.tensor_tensor(out=r64(kh64), in0=r64(kk), in1=r64(Bg), op=ALU.mult),
                 reads=["Bg"] + allk("kk"), writes=["kh64"])
            S.op("act", lambda e: e.activation(out=flat(Bsh), in_=flat(tA), func=AF.Exp),
                 reads=TAk + ["kt"], writes=["Bsh"])
            S.op("dve", lambda e, hp=hp: e.tensor_tensor(out=Qc[:, 2 * hp:2 * hp + 2, :], in0=qs[:, :, 0:NREAL],
                                                         in1=Bsh[:, :, 0:NREAL], op=ALU.mult),
                 reads=["Bsh"] + allk("qs"), writes=[("Qc", hp)])
            S.op("dve", lambda e: e.tensor_copy(out=xa_st[:, 0:8, :].rearrange("p i h -> p h i"),
                                                in_=r128(Bsh)[:, :, :, 127]),
                 reads=["Bsh"], writes=["xa_st"])
            S.op("dve", lambda e: e.tensor_copy(out=xa_st[:, 8, :], in_=Bsh[:, :, NT - 1]),
                 reads=["Bsh"], writes=["xa_st"])
            S.op("dve", lambda e: e.tensor_tensor(out=r128(Bg), in0=r128(tA),
                                                  in1=r128(tA)[:, :, :, 127:128].to_broadcast([128, 2, 8, 128]),
                                                  op=ALU.subtract),
                 reads=TAk + ["kh64"], writes=["Bg"])
            S.op("dve", lambda e: e.tensor_tensor(out=mt(Bg), in0=mt(tA),
                                                  in1=mt(tA)[:, :, NMETA - 1:NMETA].to_broadcast([128, 2, NMETA]),
                                                  op=ALU.subtract),
                 reads=TAk + ["kh64"], writes=["Bg"])
            S.op("act", lambda e: e.activation(out=flat(Bg), in_=flat(Bg), func=AF.Exp, scale=-1.0),
                 reads=["Bg"], writes=["Bg"])
            S.op("dve", lambda e: e.tensor_tensor(out=flat(kh128), in0=flat(kk), in1=flat(Bg), op=ALU.mult),
                 reads=["Bg"] + allk("kk"), writes=["kh128"])

            def tok_block(i, c0, n, hp=hp):
                par = i % 2
                PT, khT = PTs[par], khTs[par]
                bS, bO = (2, 3) if par == 0 else (6, 7)
                t0c, s0c = par * 256, par * 256
                if n == 128:
                    for hl in range(2):
                        o0 = hl * 128
                        S.op("pe", lambda e, hl=hl, o0=o0, c0=c0: e.matmul(
                            ps[:, bS, o0:o0 + 64], lhsT=kt[:, hl, c0:c0 + 128], rhs=qt[:, hl, c0:c0 + 64],
                            start=True, stop=True), reads=["kt", "qt"], writes=[("ps", bS)])
                        S.op("pe", lambda e, hl=hl, o0=o0, c0=c0: e.matmul(
                            ps[0:64, bS, o0 + 64:o0 + 128], lhsT=kh64[:, hl, c0:c0 + 64],
                            rhs=qt[:, hl, c0 + 64:c0 + 128], start=True, stop=True),
                            reads=["kh64", "qt"], writes=[("ps", bS)])
                        S.op("pe", lambda e, hl=hl, o0=o0, c0=c0: e.matmul(
                            ps[64:128, bS, o0 + 64:o0 + 128], lhsT=kt[:, hl, c0 + 64:c0 + 128],
                            rhs=qt[:, hl, c0 + 64:c0 + 128], start=True, stop=True),
                            reads=["kt", "qt"], writes=[("ps", bS)])
                    for hl in range(2):
                        S.op("dve", lambda e, hl=hl: e.tensor_tensor(
                            out=PT[:, hl, :], in0=ps[:, bS, hl * 128:(hl + 1) * 128], in1=self.mask2, op=ALU.mult),
                            reads=[("ps", bS), "mask2"], writes=[("PT", par, hl)])
                    for hl in range(2):
                        S.op("pe", lambda e, hl=hl, i=i: e.matmul(
                            ps[:, bO, hl * 128:(hl + 1) * 128], lhsT=vv[:, i, hl * 128:(hl + 1) * 128],
                            rhs=PT[:, hl, :], start=True, stop=True),
                            reads=[("vv", i), ("PT", par, hl)], writes=[("ps", bO)])
                    S.op("act", lambda e, hp=hp, c0=c0: e.activation(
                        out=oloc[:, 2 * hp:2 * hp + 2, c0:c0 + 128],
                        in_=ps[:, bO, 0:256].rearrange("p (h t) -> p h t", h=2), func=AF.Copy),
                        reads=[("ps", bO)], writes=[("oloc", hp, i)])
                for hl in range(2):
                    S.op("pe", lambda e, hl=hl, c0=c0, n=n: e.transpose(
                        out=psb(4)[0:n, t0c * 2 + hl * 128:t0c * 2 + (hl + 1) * 128], in_=kh128[:, hl, c0:c0 + n],
                        identity=self.ident_bf),
                        reads=["kh128", "ident"], writes=[("ps4", par)])
                S.op("dve", lambda e, n=n: e.tensor_copy(out=khT[0:n].rearrange("p h d -> p (h d)"),
                                                         in_=psb(4)[0:n, t0c * 2:t0c * 2 + 256]),
                     reads=[("ps4", par)], writes=[("khT", par)])
                for hl in range(2):
                    S.op("pe", lambda e, hl=hl, i=i, n=n: e.matmul(
                        ps[:, 5, s0c + hl * 128:s0c + (hl + 1) * 128], lhsT=khT[0:n, hl, :],
                        rhs=vv[0:n, i, hl * 128:(hl + 1) * 128], start=True, stop=True),
                        reads=[("khT", par), ("vv", i)], writes=[("ps5", par)])
                S.op("dve", lambda e, i=i: e.tensor_copy(out=xs_st[:, i, :], in_=ps[:, 5, s0c:s0c + 256]),
                     reads=[("ps5", par)], writes=[("xs_st", i)])
            for (i_, c0_, n_) in TL:
                tok_block(i_, c0_, n_)
            for q3 in range(3):
                S.dma("sp", lambda e, hp=hp, q3=q3: e.dma_start(
                    out=self.xs[q3].rearrange("p (i c) -> p i c", i=3)[:, :, hp * 256:(hp + 1) * 256],
                    in_=xs_st[:, 3 * q3:3 * q3 + 3, :]),
                    reads=[("xs_st", i) for i in range(9)], writes=[("xs", hp, q3)])
            S.dma("sp", lambda e, hp=hp: e.dma_start(
                out=self.xa.rearrange("p (i c) -> p i c", i=9)[:, :, 2 * hp:2 * hp + 2], in_=xa_st),
                reads=["xa_st"], writes=[("xa", hp)])
        S.barrier()
        rg = [[0, 1, 2, 3], [4, 5, 6, 7]]
        for q3 in range(3):
            S.coll(lambda e, q3=q3: e.collective_compute("AllGather", ALU.bypass, replica_groups=rg,
                                                         ins=[self.xs[q3]], outs=[self.xg[q3]]), writes=[("xg", q3)])
        S.coll(lambda e: e.collective_compute("AllGather", ALU.bypass, replica_groups=rg,
                                              ins=[self.xa], outs=[self.xag]), writes=["xag"])

    def hgrn_m2(self, strm):
        S, ps, nc = self.S, self.ps, self.nc
        R1 = 99840
        wbufs = [self.view(R1 + i * 8192, BF16, [KC, 256]) for i in range(2)]
        off = [R1 + 16384]

        def alloc(dt, shape):
            n = 1
            for x in shape:
                n *= x
            nb = n * (4 if dt == F32 else 2)
            nb = (nb + 63) // 64 * 64
            v = self.view(off[0], dt, shape)
            off[0] += nb
            return v
        sgate = alloc(BF16, [8, NREAL])
        Scur = alloc(F32, [8, 128]); SmF = alloc(F32, [8, 128])
        SAb = [alloc(BF16, [8, 128]) for _ in range(3)]
        Aall = alloc(F32, [4, 72])
        OF = alloc(F32, [8, 128]); OSQ = alloc(F32, [8, 128]); RS = alloc(F32, [8, 128])
        Qc = self.view(0, BF16, [8, NREAL]); oloc = self.view(16384, BF16, [8, NREAL])
        yhg = self.view(32768, BF16, [8, NREAL]); Smine = self.view(49152, BF16, [8, 8, 128])
        f2 = lambda v: v.rearrange("p h t -> p (h t)")
        RTB = TBS[0:2]

        for g4 in range(4):
            def ev_g(half, ti, t0, t1e, pv, bk, g4=g4):
                h = 2 * g4 + half
                S.op("act", lambda e: e.activation(out=sgate[:, h, t0:t1e], in_=pv, func=AF.Silu),
                     reads=[("ps", bk)], writes=[("sgate", h, ti)])
                S.op("dve", lambda e: e.tensor_scalar(out=sgate[:, h, t0:t1e], in0=sgate[:, h, t0:t1e],
                                                      scalar1=self.gnc[:, h:h + 1], scalar2=None, op0=ALU.mult),
                     reads=[("sgate", h, ti), "gn"], writes=[("sgate", h, ti)])
            self.proj_fm("m2", strm, self.gidx["hg%d" % g4], wbufs, RTB, ev_g)

        def out_block(i):
            c0 = 128 * i
            for h in range(8):
                bk = 2 + h // 4
                S.op("pe", lambda e, h=h, i=i, c0=c0, bk=bk: e.matmul(
                    ps[:, bk, (h % 4) * 128:(h % 4 + 1) * 128], lhsT=Smine[:, i, h, :], rhs=Qc[:, h, c0:c0 + 128],
                    start=True, stop=True),
                    reads=[("Smine", i), "Qc"], writes=[("ps", bk)])
            S.op("dve", lambda e, c0=c0: e.tensor_tensor(
                out=OF, in0=ps[:, 2:4, :].rearrange("p a (h t) -> p (a h) t", h=4), in1=oloc[:, :, c0:c0 + 128],
                op=ALU.add),
                reads=[("ps", 2), ("ps", 3), "oloc"], writes=["OF"])
            S.op("act", lambda e: e.activation(out=f2(OSQ), in_=f2(OF), func=AF.Square),
                 reads=["OF"], writes=["OSQ"])
            for a in range(2):
                S.op("pe", lambda e, a=a: e.matmul(ps[:, 4 + a, :], lhsT=self.ones128, rhs=f2(OSQ)[:, a * 512:(a + 1) * 512],
                                                   start=True, stop=True),
                     reads=["OSQ", "ones128"], writes=[("ps", 4 + a)])
            S.op("act", lambda e: e.activation(out=f2(RS), in_=ps[:, 4:6, :].rearrange("p a b -> p (a b)"),
                                               func=AF.Ln, bias=self.eps_rms, scale=1.0),
                 reads=[("ps", 4), ("ps", 5), "eps"], writes=["RS"])
            S.op("act", lambda e: e.activation(out=f2(RS), in_=f2(RS), func=AF.Exp, scale=-0.5),
                 reads=["RS"], writes=["RS"])

        def out_block_b(i):
            c0 = 128 * i
            S.op("dve", lambda e: e.tensor_tensor(out=f2(OF), in0=f2(OF), in1=f2(RS), op=ALU.mult),
                 reads=["OF", "RS"], writes=["OF"])
            S.op("dve", lambda e, c0=c0: e.tensor_tensor(out=yhg[:, :, c0:c0 + 128], in0=OF, in1=sgate[:, :, c0:c0 + 128],
                                                         op=ALU.mult),
                 reads=["OF"] + [("sgate", h, c0 // 512) for h in range(8)], writes=[("yhg", i)])

        S.dma("sp", lambda e: e.dma_start(out=Aall, in_=self.xag.rearrange("(r p) c -> p r c", p=128)),
              reads=["xag"], writes=["Aall"])
        xg3 = [x_.rearrange("(r p) (i c) -> r p i c", p=128, i=3) for x_ in self.xg]
        S.dma("sp", lambda e: e.dma_start(out=f2(SAb[2]), in_=xg3[2][0, :, 2, :]), reads=[("xg", 2)],
              writes=[("SAb", 2)])
        S.op("dve", lambda e: e.tensor_copy(out=f2(Scur), in_=f2(SAb[2])), reads=[("SAb", 2)], writes=["Scur"])
        for g in range(32):
            r, i = g % 4, g // 4
            sb = SAb[g % 3]
            S.dma("sp", lambda e, sb=sb, r=r, i=i: e.dma_start(out=f2(sb), in_=xg3[i // 3][r, :, i % 3, :]),
                  reads=[("xg", i // 3)], writes=[("SAb", g % 3)])
            if r == 0:
                S.op("dve", lambda e: e.tensor_scalar(out=f2(SmF), in0=f2(Scur), scalar1=self.selc[:, 0:1],
                                                      scalar2=None, op0=ALU.mult),
                     reads=["Scur", "sel"], writes=["SmF"])
            else:
                dst = SmF if r < 3 else Smine[:, i]
                S.op("dve", lambda e, r=r, dst=dst: e.scalar_tensor_tensor(
                    out=f2(dst), in0=f2(Scur), scalar=self.selc[:, r:r + 1], in1=f2(SmF),
                    op0=ALU.mult, op1=ALU.add),
                    reads=["Scur", "sel", "SmF"], writes=(["SmF"] if r < 3 else [("Smine", i)]))
            if g < 31:
                for h in range(8):
                    S.op("dve", lambda e, h=h, r=r, i=i, sb=sb: e.scalar_tensor_tensor(
                        out=Scur[:, h, :], in0=Scur[:, h, :], scalar=Aall[:, r, i * 8 + h:i * 8 + h + 1],
                        in1=sb[:, h, :], op0=ALU.mult, op1=ALU.add),
                        reads=["Scur", "Aall", ("SAb", g % 3)], writes=["Scur"])
            if g % 4 == 3:
                out_block(g // 4)
            if g % 4 == 1 and g >= 5:
                out_block_b((g - 5) // 4)
        out_block_b(7)

        S.barrier()

    def attn_m3(self, strm, part):
        S, ps, nc = self.S, self.ps, self.nc
        R1 = 99840
        psb = lambda bk: ps[:, bk, :].bitcast(BF16)
        K_all = self.view(R1, BF16, [2, 4112])
        V_all = self.view(R1 + 16448, BF16, [33, 258])
        IK_all = self.view(R1 + 33536, BF16, [4096])
        AugK = self.view(R1 + 41728, BF16, [4112])
        qT = self.view(R1 + 70656, BF16, [8, NREAL])
        iqT = self.view(R1 + 87040, BF16, [8, NREAL])
        sc = self.view(R1 + 49952, F32, [4096])
        wbufs = [self.view(R1 + i * 8192, BF16, [KC, 256]) for i in range(2)]
        Dg = self.view(R1 + 66336, BF16, [16, 128])
        yatt = self.view(0, BF16, [8, NREAL])
        mb = self.view(16384, BF16, [4096])
        mbT = self.view(24576, BF16, [32, 128])
        junk = self.view(49152, U8, [4096])
        iqz = self.view(49152 + 4096, BF16, [16, 128])
        rh = [self.view(57344 + q * 1024, BF16, [512]) for q in range(4)]
        ya = self.view(61440, BF16, [8, 128])
        PTb = [self.view(61440 + q * 2048, BF16, [1024]) for q in range(2)]
        cbt = self.view(65536, BF16, [4, 128])
        kst = self.view(32768, BF16, [2, NREAL])
        vst = self.view(32768 + 4096, BF16, [8, 258])
        ikst = self.view(32768 + 4096 + 4160, BF16, [NREAL])
        iktmp = self.view(32768 + 10304, F32, [64])
        ikn2 = self.view(32768 + 10304 + 256, BF16, [128])
        kmst = self.view(32768 + 10816, BF16, [2, NMETA])
        vmst = self.view(32768 + 10880, BF16, [258])
        cst = self.cst
        AugQ = cst[:, 664:1176].bitcast(BF16)
        AugR = cst[:, 1176:1688].bitcast(BF16)
        wq = cst[:, 1688:1816].rearrange("p (i h) -> p i h", h=16)
        H = cst[:, 1816:1848]
        Pt = cst[:, 1848:1976]
        g1 = cst[:, 1976:2008]
        mrow = cst[:, 2008:2016]; cc = cst[:, 2016:2024]; rs = cst[:, 2024:2032]
        Bt = cst[:, 2032:2033]; Wc = cst[:, 2033:2034]; mid = cst[:, 2034:2035]; cnt = cst[:, 2035:2036]
        u2 = cst[:, 2036:2037]; tau = cst[:, 2037:2038]; rstd1 = cst[:, 2038:2039]
        nslope = cst[:, 2040:2048]
        Qt = cst[:, 2048:2080].rearrange("p (r j) -> p r j", r=4)
        kmx = cst[:, 2080:2081]
        pw = cst[:, 2104:2136]
        gik = cst[:, 2136:2200]; bik = cst[:, 2200:2264]
        st6 = cst[:, 2264:2270]; mv = cst[:, 2270:2272]
        TL = [(i, 128 * i, 128) for i in range(8)] + [(8, NREAL, NMETA)]
        RTB = TBS[0:2]
        NB = 14

        if part == 0:
            S.dma("sp", lambda e: e.dma_start(out=cst[:, 2040:2264], in_=self.catt), writes=["catt"])
            S.dma("sp", lambda e: e.dma_start(out=Pt, in_=self.cmat_d[:, 384:512]), writes=["Pt"])
            S.op("dve", lambda e: e.memset(vst.rearrange("p i (k c) -> p i k c", k=2)[:, :, :, 128:129], 1.0),
                 writes=["vst1"])
            S.op("dve", lambda e: e.memset(vmst.rearrange("p (k c) -> p k c", k=2)[:, :, 128:129], 1.0),
                 writes=["V1"])

            def ev_k(half, ti, t0, t1e, pv, bk):
                if ti < 2:
                    S.op("act", lambda e: e.activation(out=kst[:, half, t0:t1e], in_=pv, func=AF.Copy),
                         reads=[("ps", bk)], writes=[("kst", half, ti)])
                else:
                    S.op("act", lambda e: e.activation(out=kmst[:, half, :], in_=pv, func=AF.Copy),
                         reads=[("ps", bk)], writes=[("Kmeta", half)])
            self.proj_fm("m3", strm, self.gidx["ak"], wbufs, TBS, ev_k)

            def ev_v(i, c0, n, pv, bk):
                src = pv.rearrange("p (k c) -> p k c", k=2)
                if i < 8:
                    dst = vst[:, i, :].rearrange("p (k c) -> p k c", k=2)[:, :, 0:128]
                    S.op("act", lambda e: e.activation(out=dst, in_=src, func=AF.Copy),
                         reads=[("ps", bk), "vst1"], writes=[("vst", i)])
                else:
                    dst = vmst[0:n, :].rearrange("p (k c) -> p k c", k=2)[:, :, 0:128]
                    S.op("act", lambda e: e.activation(out=dst, in_=src, func=AF.Copy),
                         reads=[("ps", bk), "V1"], writes=["Vmeta"])
            self.proj_tm("m3", strm, self.gidx["av"], wbufs, TL, 256, ev_v)

            def ev_ik(i, c0, n, pv, bk):
                S.op("dve", lambda e: e.bn_stats(out=st6, in_=pv[:, 0:64]), reads=[("ps", bk)], writes=["st6"])
                S.op("dve", lambda e: e.bn_aggr(out=mv, in_=st6), reads=["st6"], writes=["mv"])
                S.op("act", lambda e: e.activation(out=rstd1, in_=mv[:, 1:2], func=AF.Sqrt, bias=self.eps_ik, scale=1.0),
                     reads=["mv", "eps"], writes=["rstd1"])
                S.op("dve", lambda e: e.reciprocal(out=rstd1, in_=rstd1), reads=["rstd1"], writes=["rstd1"])
                S.op("dve", lambda e: e.tensor_scalar(out=iktmp, in0=pv[:, 0:64], scalar1=mv[:, 0:1], scalar2=rstd1,
                                                      op0=ALU.subtract, op1=ALU.mult),
                     reads=[("ps", bk), "mv", "rstd1"], writes=["iktmp"])
                S.op("dve", lambda e: e.tensor_tensor(out=iktmp, in0=iktmp, in1=gik, op=ALU.mult),
                     reads=["iktmp", "catt"], writes=["iktmp"])
                S.op("dve", lambda e: e.tensor_tensor(out=ikn2[:, 0:64], in0=iktmp, in1=bik, op=ALU.add),
                     reads=["iktmp", "catt"], writes=["ikn2a"])
                S.op("dve", lambda e: e.tensor_copy(out=ikn2[:, 64:128], in_=ikn2[:, 0:64]),
                     reads=["ikn2a"], writes=["ikn2b"])
                S.op("act", lambda e, i=i: e.activation(out=wq[:, i, :], in_=pv[:, 64:80], func=AF.Copy,
                                                        scale=0.25 * 0.125),
                     reads=[("ps", bk)], writes=[("wq", i)])
                S.op("pe", lambda e: e.transpose(out=psb(2)[:, 0:128], in_=ikn2, identity=self.ident_bf),
                     reads=["ikn2a", "ikn2b", "ident"], writes=[("ps", 2)])
                S.op("act", lambda e, c0=c0: e.activation(out=ikst[:, c0:c0 + 128], in_=psb(2)[:, 0:128], func=AF.Copy),
                     reads=[("ps", 2)], writes=[("ikst", i)])
            self.proj_tm("m3", strm, self.gidx["ikw"], wbufs, TL[0:8], 80, ev_ik)

            S.dma("sp", lambda e: e.dma_start(out=self.ks.rearrange("p (k t) -> p k t", k=2), in_=kst),
                  reads=[("kst", hh, ti) for hh in range(2) for ti in range(2)], writes=["ks"])
            S.dma("sp", lambda e: e.dma_start(out=self.vs[:, 0:2064].rearrange("p (i c) -> p i c", i=8), in_=vst),
                  reads=[("vst", i) for i in range(8)] + ["vst1"], writes=["vs"])
            S.dma("sp", lambda e: e.dma_start(out=self.vs[:, 2064:3088], in_=ikst),
                  reads=[("ikst", i) for i in range(8)], writes=["vs2"])
            S.dma("sp", lambda e: e.dma_start(out=self.kms.rearrange("p (k t) -> p k t", k=2), in_=kmst),
                  reads=[("Kmeta", 0), ("Kmeta", 1)], writes=["kms"])
            S.dma("sp", lambda e: e.dma_start(out=self.vms, in_=vmst[0:NMETA, :]),
                  reads=["Vmeta", "V1"], writes=["vms"])
            S.barrier()
            rg = [[0, 1, 2, 3], [4, 5, 6, 7]]
            S.coll(lambda e: e.collective_compute("AllGather", ALU.bypass, replica_groups=rg,
                                                  ins=[self.ks], outs=[self.kg]), writes=["kg"])
            S.coll(lambda e: e.collective_compute("AllGather", ALU.bypass, replica_groups=rg,
                                                  ins=[self.vs], outs=[self.vg]), writes=["vg"])
            return

        if part == 1:
            for g4 in range(4):
                def ev_q(half, ti, t0, t1e, pv, bk, g4=g4):
                    h = 2 * g4 + half
                    S.op("act", lambda e: e.activation(out=qT[:, h, t0:t1e], in_=pv, func=AF.Copy, scale=128.0 ** -0.5),
                         reads=[("ps", bk)], writes=[("qT", h, ti)])
                self.proj_fm("m3", strm, self.gidx["aq%d" % g4], wbufs, RTB, ev_q)
            for g4 in range(4):
                def ev_iq(half, ti, t0, t1e, pv, bk, g4=g4):
                    h = 2 * g4 + half
                    S.op("dve", lambda e: e.tensor_copy(out=iqT[:, h, t0:t1e], in_=pv),
                         reads=[("ps", bk)], writes=[("iqT", h, ti)])
                self.proj_fm("m3", strm, self.gidx["iq%d" % g4], wbufs, RTB, ev_iq)
            return

        S.op("pool", lambda e: e.memset(AugK[0:65, :], 0.0), writes=["AugK"])
        S.op("pool", lambda e: e.memset(AugR[0:65, :], 0.0), writes=["AugR"])
        for rr in range(3):
            S.dma("pool", lambda e, rr=rr: e.dma_start(out=AugK[32 * rr:32 * rr + 1, :], in_=self.augk[rr:rr + 1, :]),
                  writes=["AugK"])
        for rr in range(2):
            S.dma("pool", lambda e, rr=rr: e.dma_start(out=AugR[32 * rr:32 * rr + 1, :], in_=self.augs[rr:rr + 1, :]),
                  writes=["AugR"])
        S.dma("pool", lambda e: e.dma_start(out=cbt, in_=self.cbt_d.rearrange("p (r s) -> p r s", r=4)), writes=["cbt"])
        for r in range(4):
            S.dma("sp", lambda e, r=r: e.dma_start(
                out=K_all[:, :, r * 1024:(r + 1) * 1024],
                in_=self.kg[r * 128:(r + 1) * 128, :].rearrange("p (k t) -> p k t", k=2)),
                reads=["kg"], writes=["K_all"])
            S.dma("sp", lambda e, r=r: e.dma_start(
                out=V_all[:, r * 8:(r + 1) * 8, :],
                in_=self.vg[r * 128:(r + 1) * 128, 0:2064].rearrange("p (i c) -> p i c", i=8)),
                reads=["vg"], writes=["V_all"])
            S.dma("sp", lambda e, r=r: e.dma_start(
                out=IK_all[:, r * 1024:(r + 1) * 1024], in_=self.vg[r * 128:(r + 1) * 128, 2064:3088]),
                reads=["vg"], writes=["IK_all"])
        S.dma("sp", lambda e: e.dma_start(out=K_all[:, :, 4096:4112], in_=self.kms.rearrange("p (k t) -> p k t", k=2)),
              reads=["kms"], writes=[("Kmeta", 0), ("Kmeta", 1)])
        S.dma("sp", lambda e: e.dma_start(out=V_all[0:NMETA, 32, :], in_=self.vms), reads=["vms"], writes=["Vmeta"])
        S.barrier()

        S.op("pool", lambda e: e.memset(iqz.rearrange("p h t -> p (h t)"), 0.0), writes=["iqz"])
        sc4 = sc.rearrange("p (r c) -> p r c", r=4)
        mb4 = mb.rearrange("p (r c) -> p r c", r=4)
        jk4 = junk.rearrange("p (r c) -> p r c", r=4)
        def geom(i):
            q0 = 128 * i
            nk = 128 * (i + 1)
            pieces = [(r, c0, min(512, nk - c0)) for r in range(4) for c0 in range(0, nk, 512)]
            return q0, nk, pieces

        def st_idx(i):
            q0, nk, pieces = geom(i)
            for h in range(16):
                S.op("act", lambda e, h=h: e.activation(out=Dg[:, h, :], in_=self.ident_bf, func=AF.Copy,
                                                        scale=wq[:, i, h:h + 1]),
                     reads=["ident", ("wq", i)], writes=["Dg"])
            for h in range(16):
                hb = h % 2
                eng = "act"
                if eng == "pool":
                    S.op("pool", lambda e, h=h, hb=hb: e.tensor_copy(
                        out=iqz[hb * 64:(hb + 1) * 64, h, :], in_=iqT[hb * 64:(hb + 1) * 64, h // 2, q0:q0 + 128]),
                        reads=["iqT", "iqz"], writes=[("iqzh", h)])
                else:
                    S.op("act", lambda e, h=h, hb=hb: e.activation(
                        out=iqz[hb * 64:(hb + 1) * 64, h, :], in_=iqT[hb * 64:(hb + 1) * 64, h // 2, q0:q0 + 128],
                        func=AF.Copy),
                        reads=["iqT", "iqz"], writes=[("iqzh", h)])
            for pi, (r, c0, cn) in enumerate(pieces):
                col0 = r * 1024 + c0
                accb = 4 + pi % 2

                def head_mm(h, cn=cn, col0=col0):
                    bk, hb = h % 4, h % 2
                    S.op("pe", lambda e: e.matmul(
                        ps[:, bk, 0:cn], lhsT=iqz[:, h, :],
                        rhs=IK_all[:, col0:col0 + cn], start=True, stop=True),
                        reads=["IK_all", ("iqzh", h)], writes=[("ps", bk)])
                    if h % 2 == 0:
                        S.op("act", lambda e: e.activation(out=rh[bk][:, 0:cn], in_=ps[:, bk, 0:cn], func=AF.Relu),
                             reads=[("ps", bk)], writes=[("rh", bk)])
                    else:
                        S.op("dve", lambda e: e.tensor_scalar(out=rh[bk][:, 0:cn], in0=ps[:, bk, 0:cn], scalar1=0.0,
                                                              scalar2=None, op0=ALU.max),
                             reads=[("ps", bk)], writes=[("rh", bk)])

                def head_acc(h, cn=cn, accb=accb):
                    bk = h % 4
                    S.op("pe", lambda e: e.matmul(
                        ps[:, accb, 0:cn], lhsT=Dg[:, h, :], rhs=rh[bk][:, 0:cn], start=(h == 0), stop=(h == 15)),
                        reads=["Dg", ("rh", bk)], writes=[("ps", accb)])
                for h in range(16):
                    head_mm(h)
                    if h >= 2:
                        head_acc(h - 2)
                head_acc(14)
                head_acc(15)
                S.op("act", lambda e, accb=accb, col0=col0, cn=cn: e.activation(
                    out=sc[:, col0:col0 + cn], in_=ps[:, accb, 0:cn], func=AF.Copy),
                    reads=[("ps", accb)], writes=["sc"])

        def st_bis(i):
            q0, nk, pieces = geom(i)
            scv, mbv, jkv = sc4[:, :, 0:nk], mb4[:, :, 0:nk], jk4[:, :, 0:nk]
            S.op("dve", lambda e: e.reduce_max(out=Bt, in_=scv, axis=AX.XY, apply_absolute_value=True),
                 reads=["sc"], writes=["Bt"])
            S.op("dve", lambda e: e.tensor_tensor(out=sc4[:, :, q0:q0 + 128], in0=sc4[:, :, q0:q0 + 128], in1=cbt,
                                                  op=ALU.add),
                 reads=["sc", "cbt", "Bt"], writes=["sc"])
            S.op("dve", lambda e: e.tensor_scalar(out=Wc, in0=Bt, scalar1=2.0002, scalar2=1e-6,
                                                  op0=ALU.mult, op1=ALU.add), reads=["Bt"], writes=["Wc"])
            S.op("dve", lambda e: e.tensor_scalar(out=H[:, 0:NB + 1], in0=pw[:, 0:NB + 1], scalar1=Wc, scalar2=None,
                                                  op0=ALU.mult), reads=["Wc", "catt"], writes=["H"])
            S.op("dve", lambda e: e.memset(mid, 0.0), writes=["mid"])
            for k in range(NB):
                S.op("dve", lambda e: e.tensor_scalar(
                    out=jkv, in0=scv, scalar1=mid, scalar2=0.0, op0=ALU.is_ge, op1=ALU.add, accum_out=cnt),
                    reads=["sc", "mid"], writes=["junk", "cnt"])
                S.op("dve", lambda e, k=k: e.tensor_scalar(out=u2, in0=cnt, scalar1=256.0, scalar2=H[:, k:k + 1],
                                                           op0=ALU.is_ge, op1=ALU.mult),
                     reads=["cnt", "H"], writes=["u2"])
                S.op("dve", lambda e, k=k: e.scalar_tensor_tensor(out=mid, in0=mid, scalar=H[:, k + 1:k + 2], in1=u2,
                                                                  op0=ALU.subtract, op1=ALU.add),
                     reads=["mid", "H", "u2"], writes=["mid"])
            S.op("dve", lambda e: e.tensor_tensor(out=tau, in0=mid, in1=H[:, NB:NB + 1], op=ALU.subtract),
                 reads=["mid", "H"], writes=["tau"])
            S.op("dve", lambda e: e.tensor_scalar(
                out=mbv, in0=scv, scalar1=tau, scalar2=-30000.0, op0=ALU.is_lt, op1=ALU.mult),
                reads=["sc", "tau"], writes=["mb"])
            nb_ = i + 1
            sc5 = scv.rearrange("p r (j s) -> p r j s", s=128)
            mb5 = mbv.rearrange("p r (j s) -> p r j s", s=128)
            S.op("dve", lambda e: e.tensor_tensor(
                out=sc5, in0=mb5, in1=Pt.unsqueeze(1).unsqueeze(1).to_broadcast([128, 4, nb_, 128]), op=ALU.add),
                reads=["mb", "Pt", "sc"], writes=["sc"])
            g1v = g1.rearrange("p (r j) -> p r j", r=4)[:, :, 0:nb_]
            S.op("dve", lambda e: e.tensor_reduce(out=g1v, in_=sc5, axis=AX.X, op=ALU.max),
                 reads=["sc"], writes=["g1"])
            S.op("dve", lambda e: e.tensor_tensor(out=g1v, in0=g1v, in1=Qt[:, :, 0:nb_], op=ALU.add),
                 reads=["g1", "catt"], writes=["g1"])
            S.op("dve", lambda e: e.tensor_reduce(out=kmx, in_=g1v, axis=AX.XY, op=ALU.max),
                 reads=["g1"], writes=["kmx"])
            S.op("dve", lambda e: e.tensor_scalar(out=kmx, in0=kmx, scalar1=15.0, scalar2=None, op0=ALU.max),
                 reads=["kmx"], writes=["kmx"])
            S.op("dve", lambda e: e.tensor_scalar(out=cc, in0=nslope, scalar1=kmx, scalar2=None, op0=ALU.mult),
                 reads=["kmx", "catt"], writes=["cc"])

        def st_mbT(i):
            kts = [(r, ip) for r in range(4) for ip in range(i + 1)]
            for g0 in range(0, len(kts), 8):
                grp = kts[g0:g0 + 8]
                bk = 6 + (g0 // 8) % 2
                for s_, (r, ip) in enumerate(grp):
                    S.op("pe", lambda e, bk=bk, s_=s_, r=r, ip=ip: e.transpose(
                        out=psb(bk)[:, s_ * 128:(s_ + 1) * 128], in_=mb[:, r * 1024 + ip * 128:r * 1024 + ip * 128 + 128],
                        identity=self.ident_bf),
                        reads=["mb", "ident"], writes=[("ps", bk)])
                for s_, (r, ip) in enumerate(grp):
                    S.op("act", lambda e, bk=bk, s_=s_, r=r, ip=ip: e.activation(
                        out=mbT[:, r * 8 + ip, :], in_=psb(bk)[:, s_ * 128:(s_ + 1) * 128], func=AF.Copy),
                        reads=[("ps", bk)], writes=["mbT"])

        def st_passA(i):
            q0, nk, pieces = geom(i)
            S.op("dve", lambda e: e.memset(ps[:, 5:8, :].rearrange("p a b -> p (a b)"), 0.0),
                 writes=[("ps", 5), ("ps", 6), ("ps", 7)])
            Dc = PTb[1].rearrange("p (h t) -> p h t", h=8)
            for h in range(8):
                S.op("dve", lambda e, h=h: e.tensor_scalar(out=Dc[:, h, :], in0=self.ident_bf, scalar1=cc[:, h:h + 1],
                                                           scalar2=None, op0=ALU.mult),
                     reads=["ident", "cc"], writes=[("PTb", 1)])
            for a_ in range(2):
                S.op("pe", lambda e, a_=a_: e.matmul(ps[:, 4, :], lhsT=self.ones_bf,
                                                     rhs=PTb[1][:, a_ * 512:(a_ + 1) * 512],
                                                     start=True, stop=True),
                     reads=[("PTb", 1), "ones_bf"], writes=[("ps", 4)])
                S.op("dve", lambda e, a_=a_: e.tensor_copy(out=AugR[64:65, a_ * 512:(a_ + 1) * 512], in_=ps[64:65, 4, :]),
                     reads=[("ps", 4)], writes=["AugR"])

        def st_passB(i):
            q0, nk, pieces = geom(i)
            ktl = [(r * 1024 + ip * 128, r * 8 + ip, 128) for r in range(4) for ip in range(i + 1)] + [(4096, 32, NMETA)]
            Oreg = lambda h: ps[:, 5 + h // 3, (h % 3) * 129:(h % 3 + 1) * 129]

            def logits(qi):
                col0, vt, n = ktl[qi]
                meta = (n == NMETA)
                pair = (0, 1) if qi % 2 == 0 else (2, 3)
                for h in range(8):
                    kvh = h // 4
                    out = ps[0:n, pair[h // 4], (h % 4) * 128:(h % 4 + 1) * 128]
                    S.op("pe", lambda e, out=out, kvh=kvh, h=h: e.matmul(
                        out, lhsT=K_all[:, kvh, col0:col0 + n], rhs=qT[:, h, q0:q0 + 128], start=True, stop=False),
                        reads=["K_all", "qT", ("Kmeta", kvh)], writes=[("ps", pair[h // 4])])
                    S.op("pe", lambda e, out=out, h=h: e.matmul(
                        out, lhsT=AugK[0:65, col0:col0 + n], rhs=AugR[0:65, h * 128:(h + 1) * 128],
                        start=False, stop=meta),
                        reads=["AugK", "AugR"], writes=[("ps", pair[h // 4])])
                    if not meta:
                        S.op("pe", lambda e, out=out: e.matmul(
                            out, lhsT=self.ident_bf, rhs=mbT[:, vt, :], start=False, stop=True),
                            reads=["mbT", "ident"], writes=[("ps", pair[h // 4])])
                pt = PTb[qi % 2]
                S.op("act", lambda e: e.activation(
                    out=pt[0:n, :], in_=ps[0:n, pair[0]:pair[0] + 2, :].rearrange("p a b -> p (a b)"), func=AF.Exp),
                    reads=[("ps", pair[0]), ("ps", pair[1])], writes=[("PTb", qi % 2)])

            def pv(qi):
                col0, vt, n = ktl[qi]
                pt = PTb[qi % 2]
                for h in range(8):
                    kvh = h // 4
                    S.op("pe", lambda e, h=h, kvh=kvh: e.matmul(
                        Oreg(h), lhsT=pt[0:n, h * 128:(h + 1) * 128], rhs=V_all[0:n, vt, kvh * 129:(kvh + 1) * 129],
                        start=False, stop=(qi == len(ktl) - 1)),
                        reads=[("PTb", qi % 2), "V_all", "Vmeta"], writes=[("ps", 5 + h // 3)])
            logits(0)
            for qi in range(len(ktl)):
                if qi + 1 < len(ktl):
                    logits(qi + 1)
                pv(qi)

        def st_fin(i):
            q0 = 128 * i
            for b3 in range(3):
                nh = 3 if b3 < 2 else 2
                Ov = ps[:, 5 + b3, 0:nh * 129].rearrange("p (h c) -> p h c", c=129)
                S.op("dve", lambda e, Ov=Ov, b3=b3, nh=nh: e.reciprocal(out=rs[:, 3 * b3:3 * b3 + nh], in_=Ov[:, :, 128]),
                     reads=[("ps", 5 + b3)], writes=[("rs", b3)])
                S.op("dve", lambda e, Ov=Ov, b3=b3, nh=nh: e.tensor_tensor(
                    out=ya[:, 3 * b3:3 * b3 + nh, :], in0=Ov[:, :, 0:128],
                    in1=rs[:, 3 * b3:3 * b3 + nh].unsqueeze(2).to_broadcast([128, nh, 128]), op=ALU.mult),
                    reads=[("ps", 5 + b3), ("rs", b3)], writes=[("ya", b3), ("PTb", 0)])
            for h in range(8):
                S.op("pe", lambda e, h=h: e.transpose(out=psb(4)[:, h * 128:(h + 1) * 128], in_=ya[:, h, :],
                                                      identity=self.ident_bf),
                     reads=[("ya", h // 3), ("PTb", 0), "ident"], writes=[("ps", 4)])
            S.op("act", lambda e: e.activation(out=yatt[:, :, q0:q0 + 128],
                                               in_=psb(4).rearrange("p (h t) -> p h t", h=8), func=AF.Copy),
                 reads=[("ps", 4)], writes=[("yatt", i)])

        st_idx(0)
        st_bis(0)
        st_mbT(0)
        for i in range(8):
            if i + 1 < 8:
                st_idx(i + 1)
            st_passA(i)
            if i + 1 < 8:
                st_bis(i + 1)
            st_passB(i)
            st_fin(i)
            if i + 1 < 8:
                st_mbT(i + 1)
        S.barrier()

    def merge_m4(self, strm, cg, cb):
        S, ps, nc = self.S, self.ps, self.nc
        R1 = 99840
        RTB = TBS[0:2]
        yatt = self.view(0, BF16, [8, NREAL]); yhg = self.view(32768, BF16, [8, NREAL])
        merged = self.view(R1, BF16, [KC, NREAL])
        o = R1 + 32768
        wga = [self.view(o + q * 8192, BF16, [KC, 256]) for q in range(2)]
        wgh = [self.view(o + 16384 + q * 8192, BF16, [KC, 256]) for q in range(2)]
        wba = [self.view(o + 32768 + q * 4096, BF16, [8, 256]) for q in range(2)]
        wbh = [self.view(o + 40960 + q * 4096, BF16, [8, 256]) for q in range(2)]
        tm = [self.view(o + 49152 + q * 2048, F32, [512]) for q in range(4)]
        for mg in range(8):
            q = mg % 2
            S.dma("pool", lambda e, q=q, mg=mg: e.dma_start(out=wga[q], in_=self.win[self.gidx["ga%d" % mg]]),
                  writes=[("wga", q)])
            S.dma("pool", lambda e, q=q, mg=mg: e.dma_start(out=wgh[q], in_=self.win[self.gidx["gh%d" % mg]]),
                  writes=[("wgh", q)])
            S.dma("pool", lambda e, q=q, mg=mg: e.dma_start(out=wba[q], in_=self.wba_d[mg]), writes=[("wba", q)])
            S.dma("pool", lambda e, q=q, mg=mg: e.dma_start(out=wbh[q], in_=self.wbh_d[mg]), writes=[("wbh", q)])
            for half in range(2):
                mc = 2 * mg + half
                hs = slice(half * 128, (half + 1) * 128)
                for ti, (t0, t1e) in enumerate(RTB):
                    pp = (half * 2 + ti) % 2
                    b0 = 4 * pp
                    for k in range(KC):
                        S.op("pe", lambda e, b0=b0, q=q, k=k, hs=hs, t0=t0, t1e=t1e: e.matmul(
                            ps[:, b0, :], lhsT=wga[q][:, k, hs], rhs=strm[:, k, t0:t1e],
                            start=(k == 0), stop=(k == KC - 1)),
                            reads=[("wga", q), "strm"], writes=[("ps", b0)])
                    for k in range(8):
                        S.op("pe", lambda e, b0=b0, q=q, k=k, hs=hs, t0=t0, t1e=t1e: e.matmul(
                            ps[:, b0 + 1, :], lhsT=wba[q][:, k, hs], rhs=yatt[:, k, t0:t1e],
                            start=(k == 0), stop=(k == 7)),
                            reads=[("wba", q), "yatt"], writes=[("ps", b0 + 1)])
                    for k in range(KC):
                        S.op("pe", lambda e, b0=b0, q=q, k=k, hs=hs, t0=t0, t1e=t1e: e.matmul(
                            ps[:, b0 + 2, :], lhsT=wgh[q][:, k, hs], rhs=strm[:, k, t0:t1e],
                            start=(k == 0), stop=(k == KC - 1)),
                            reads=[("wgh", q), "strm"], writes=[("ps", b0 + 2)])
                    for k in range(8):
                        S.op("pe", lambda e, b0=b0, q=q, k=k, hs=hs, t0=t0, t1e=t1e: e.matmul(
                            ps[:, b0 + 3, :], lhsT=wbh[q][:, k, hs], rhs=yhg[:, k, t0:t1e],
                            start=(k == 0), stop=(k == 7)),
                            reads=[("wbh", q), "yhg"], writes=[("ps", b0 + 3)])
                    ta, th = tm[2 * pp], tm[2 * pp + 1]
                    S.op("act", lambda e, ta=ta, b0=b0: e.activation(out=ta, in_=ps[:, b0, :], func=AF.Sigmoid),
                         reads=[("ps", b0)], writes=[("tm", 2 * pp)])
                    S.op("dve", lambda e, ta=ta, b0=b0: e.tensor_tensor(out=ta, in0=ta, in1=ps[:, b0 + 1, :], op=ALU.mult),
                         reads=[("tm", 2 * pp), ("ps", b0 + 1)], writes=[("tm", 2 * pp)])
                    S.op("act", lambda e, th=th, b0=b0: e.activation(out=th, in_=ps[:, b0 + 2, :], func=AF.Sigmoid),
                         reads=[("ps", b0 + 2)], writes=[("tm", 2 * pp + 1)])
                    S.op("dve", lambda e, th=th, b0=b0: e.tensor_tensor(out=th, in0=th, in1=ps[:, b0 + 3, :], op=ALU.mult),
                         reads=[("tm", 2 * pp + 1), ("ps", b0 + 3)], writes=[("tm", 2 * pp + 1)])
                    S.op("dve", lambda e, ta=ta, th=th, mc=mc, t0=t0, t1e=t1e: e.tensor_tensor(
                        out=merged[:, mc, t0:t1e], in0=ta, in1=th, op=ALU.add),
                        reads=[("tm", 2 * pp), ("tm", 2 * pp + 1)], writes=[("merged", mc, ti)])
        S.barrier()
        z = self.view(R1 + 32768, F32, [KC, NREAL])
        wo = [self.view(53760 + q * 4096, BF16, [KC, 128]) for q in range(2)]
        strm_out = self.view(0, BF16, [KC, NREAL])
        for dc in range(KC):
            q = dc % 2
            S.dma("pool", lambda e, q=q, dc=dc: e.dma_start(out=wo[q], in_=self.wo_d[dc]), writes=[("wo", q)])
            S.dma("sp", lambda e, dc=dc: e.dma_start(out=z[:, dc, :], in_=self.h1s[dc * 128:(dc + 1) * 128, 0:NREAL]),
                  writes=[("l2", "z", dc, ti) for ti in range(2)])
            for ti, (t0, t1e) in enumerate(RTB):
                pb = (dc * 2 + ti) % 2
                for k in range(KC):
                    S.op("pe", lambda e, pb=pb, q=q, k=k, t0=t0, t1e=t1e: e.matmul(
                        ps[:, pb, :], lhsT=wo[q][:, k, :], rhs=merged[:, k, t0:t1e],
                        start=(k == 0), stop=(k == KC - 1)),
                        reads=[("wo", q), ("merged", k, ti)], writes=[("ps", pb)])
                S.op("dve", lambda e, pb=pb, dc=dc, t0=t0, t1e=t1e: e.scalar_tensor_tensor(
                    out=z[:, dc, t0:t1e], in0=ps[:, pb, :], scalar=1.0 / ALPHA, in1=z[:, dc, t0:t1e],
                    op0=ALU.mult, op1=ALU.add),
                    reads=[("ps", pb), ("l2", "z", dc, ti)], writes=[("l2", "z", dc, ti)])
        self.ln_apply("l2", z, RTB, cg, cb, 33280, LN_EPS / (ALPHA * ALPHA), strm_out=strm_out,
                      resid_out=self.h2s)
        S.barrier()

    def eps_col(self, val):
        return self.eps_cols[val]

    def build(self):
        nc, S = self.nc, self.S
        stage = self.stage
        xT = self.din("xT", [D, NT])
        wg1 = self.din("wg1", [FC // 2, 128, KC, 256])
        wu1 = self.din("wu1", [FC // 2, 128, KC, 256])
        wd1 = self.din("wd1", [KC, 128, FC, 128])
        wg2 = self.din("wg2", [FC // 2, 128, KC, 256])
        wu2 = self.din("wu2", [FC // 2, 128, KC, 256])
        wd2 = self.din("wd2", [KC, 128, FC, 128])
        cvec = self.din("cvec", [128, 128])
        cmat = self.cmat_d = self.din("cmat", [128, 512])
        self.catt = self.din("catt", [128, 224])
        self.augk = self.din("augk", [3, 4112])
        self.augs = self.din("augs", [2, 1024])
        self.augq = self.din("augq", [8, 1024])
        self.cbt_d = self.din("cbt", [128, 512])
        self.win = self.din("win", [len(GROUPS), 128, KC, 256])
        self.wba_d = self.din("wba", [8, 128, 8, 256])
        self.wbh_d = self.din("wbh", [8, 128, 8, 256])
        self.wo_d = self.din("wo", [KC, 128, KC, 128])
        self.gidx = {nm: i for i, (nm, _, _) in enumerate(GROUPS)}
        self.h1s = h1s = self.dscratch("h1s", [D, NT])
        self.h2s = self.dscratch("h2s", [D, NREAL])
        self.xs = [self.dscratch("xs%d" % q, [128, 3 * 1024], BF16) for q in range(3)]
        self.xg = [self.dscratch("xg%d" % q, [512, 3 * 1024], BF16) for q in range(3)]
        self.xa = self.dscratch("xa", [128, 72])
        self.xag = self.dscratch("xag", [512, 72])
        self.ks = self.dscratch("ks", [128, 2048], BF16)
        self.kg = self.dscratch("kg", [512, 2048], BF16)
        self.vs = self.dscratch("vs", [128, 3088], BF16)
        self.vg = self.dscratch("vg", [512, 3088], BF16)
        self.kms = self.dscratch("kms", [128, 2 * NMETA], BF16)
        self.vms = self.dscratch("vms", [NMETA, 258], BF16)
        if stage == 1:
            dbg = self.dout("dbg", [D, NT])
        elif stage in (3, 4):
            dbg = self.dout("dbg", [128, 8 * NREAL])
            self.dbg2 = self.dout("dbg2", [128, 12000])
        elif stage == 5:
            dbg = self.dout("dbg", [D, NREAL])
        else:
            outT = self.dout("outT", [D, NREAL])

        from contextlib import ExitStack
        with ExitStack() as es:
            self.arena = es.enter_context(nc.sbuf_tensor("arena", [128, ARENA_F32], F32))
            self.cst = es.enter_context(nc.sbuf_tensor("cst", [128, CONST_F32], F32))
            self.ps = es.enter_context(nc.psum_tensor("ps", [128, 8, 512], F32))
            esems = {e: es.enter_context(nc.semaphore("sem_" + e)) for e in ENGS}
            dsems = [es.enter_context(nc.semaphore("dsem%d" % i)) for i in range(S.n_dma_sems + 8)]
            block = es.enter_context(nc.Block())
            cst = self.cst
            self.ones_f32 = cst[:, 0:128]
            cv = self.cv = cst[:, 128:256]
            epsA = cst[:, 256:257]
            self.eps_rms = cst[:, 257:258]
            self.eps_ik = cst[:, 258:259]
            self.eps_cols = {LN_EPS / (ALPHA * ALPHA): epsA}
            self.ident_bf = cst[:, 264:328].bitcast(BF16)
            self.ones_bf = cst[:, 328:392].bitcast(BF16)
            self.mask2 = cst[:, 392:520]
            self.ones128 = cst[:, 520:648]
            self.lbc = cst[:, 648:656]
            self.omlc = cst[:, 656:664]
            self.gnc = cv[:, 112:120]
            self.selc = cv[:, 120:124]
            S.op("dve", lambda e: e.memset(self.ones_f32, 1.0 / D), writes=["ones"])
            S.op("dve", lambda e: e.memset(self.ones128, 1.0 / 128), writes=["ones128"])
            S.op("dve", lambda e: e.memset(epsA, LN_EPS / (ALPHA * ALPHA)), writes=["eps"])
            S.op("dve", lambda e: e.memset(self.eps_rms, RMS_EPS), writes=["eps"])
            S.op("dve", lambda e: e.memset(self.eps_ik, LN_EPS), writes=["eps"])
            S.dma("sp", lambda e: e.dma_start(out=cv, in_=cvec), writes=["cv"])
            S.dma("sp", lambda e: e.dma_start(out=self.mask2, in_=cmat[:, 256:384]), writes=["mask2"])
            S.dma("pool", lambda e: e.dma_start(out=self.ident_bf, in_=cmat[:, 0:128]), writes=["ident"])
            S.dma("pool", lambda e: e.dma_start(out=self.ones_bf, in_=cmat[:, 128:256]), writes=["ones_bf"])
            S.op("dve", lambda e: e.tensor_tensor(out=self.lbc, in0=cv[:, 96:104], in1=cv[:, 104:112], op=ALU.subtract),
                 reads=["cv"], writes=["lb"])
            S.op("act", lambda e: e.activation(out=self.lbc, in_=self.lbc, func=AF.Sigmoid), reads=["lb"], writes=["lb"])
            S.op("dve", lambda e: e.tensor_scalar(out=self.omlc, in0=self.lbc, scalar1=-1.0, scalar2=1.0,
                                                  op0=ALU.mult, op1=ALU.add), reads=["lb"], writes=["lb"])
            strm0 = self.view(0, BF16, [KC, NT])
            S.dma("pool", lambda e: e.dma_start(out=strm0, in_=xT.rearrange("(k p) t -> p k t", p=128)),
                  writes=[("f1", "sin")])
            self.ffn_phase("f1", NT, TBS, strm0, 66560, wg1, wu1, wd1, xT, cv[:, 0:16], cv[:, 16:32],
                           resid_out=(dbg if stage == 1 else h1s))
            strm1 = self.view(66560, BF16, [KC, NT])
            if stage >= 2:
                if stage >= 4:
                    self.attn_m3(strm1, 0)
                self.hgrn_m1(strm1)
                if stage >= 4:
                    self.attn_m3(strm1, 1)
                self.hgrn_m2(strm1)
            if stage == 3:
                yhg = self.view(32768, BF16, [8 * NREAL])
                S.dma("pool", lambda e: e.dma_start(out=dbg, in_=yhg), reads=[])
            if stage >= 4:
                self.attn_m3(strm1, 2)
            if stage == 4:
                yat = self.view(0, BF16, [8 * NREAL])
                S.dma("pool", lambda e: e.dma_start(out=dbg, in_=yat), reads=[])
            if stage >= 5:
                if stage == 5:
                    self.h2s = dbg
                self.merge_m4(strm1, cv[:, 32:48], cv[:, 48:64])
            if stage >= 6:
                strm2 = self.view(0, BF16, [KC, NREAL])
                self.ffn_phase("f2", NREAL, TBS[0:2], strm2, 66560, wg2, wu2, wd2, self.h2s, cv[:, 64:80],
                               cv[:, 80:96], final_out=outT)
            S.emit(block, esems, dsems)
        return nc


def _lay_gu(w):
    return np.ascontiguousarray(w.reshape(KC, 128, FC // 2, 256).transpose(2, 1, 0, 3))


def _lay_d(w):
    return np.ascontiguousarray(w.reshape(FC, 128, KC, 128).transpose(2, 1, 0, 3))


def _fm(v):
    return np.ascontiguousarray(v.reshape(KC, 128).T)


def _core_tokens(x, meta, c):
    b, j = c // 4, c % 4
    blocks = [x[b, 128 * (4 * i + j):128 * (4 * i + j) + 128] for i in range(8)]
    tok = np.concatenate(blocks + [meta], axis=0)
    return np.ascontiguousarray(tok.T)


def _mk_groups():
    g = []
    for hp in range(4):
        g += [("hf%d" % hp, 3664 + 256 * hp, 256), ("hq%d" % hp, 2640 + 256 * hp, 256),
              ("hi%d" % hp, 4688 + 256 * hp, 256)]
    for i in range(4):
        g.append(("hg%d" % i, 5712 + 256 * i, 256))
    g += [("ak", 1024, 256), ("av", 1280, 256), ("ikw", 2560, 80)]
    for i in range(4):
        g.append(("aq%d" % i, 256 * i, 256))
    for i in range(4):
        g.append(("iq%d" % i, 1536 + 256 * i, 256))
    for i in range(8):
        g.append(("ga%d" % i, 6736 + 256 * i, 256))
        g.append(("gh%d" % i, 8784 + 256 * i, 256))
    return g


GROUPS = _mk_groups()


def _lay_win(w):
    out = np.zeros((len(GROUPS), 128, KC, 256), np.float32)
    for gi, (nm, c0, nc_) in enumerate(GROUPS):
        out[gi, :, :, 0:nc_] = w[:, c0:c0 + nc_].reshape(KC, 128, nc_).transpose(1, 0, 2)
    return out


def prepare(inputs, stage):
    f = lambda k: np.asarray(inputs[k], dtype=np.float32)
    x, meta = f("x"), f("meta")
    shared = {
        "wg1": _lay_gu(f("ffn1_w_gate")[0]), "wu1": _lay_gu(f("ffn1_w_up")[0]),
        "wd1": _lay_d(f("ffn1_w_down")[0]),
        "win": _lay_win(f("w_in")[0]),
        "wg2": _lay_gu(f("ffn2_w_gate")[0]), "wu2": _lay_gu(f("ffn2_w_up")[0]),
        "wd2": _lay_d(f("ffn2_w_down")[0]),
        "wba": np.ascontiguousarray(f("w_branch_att")[0].reshape(8, 128, 8, 256).transpose(2, 1, 0, 3)),
        "wbh": np.ascontiguousarray(f("w_branch_hg")[0].reshape(8, 128, 8, 256).transpose(2, 1, 0, 3)),
        "wo": np.ascontiguousarray(f("w_out")[0].reshape(KC, 128, KC, 128).transpose(2, 1, 0, 3)),
    }
    slopes = (2.0 ** -(np.arange(8) + 1.0)).astype(np.float32)
    c = np.arange(4096)
    kpos = np.concatenate([16 + 128 * (4 * ((c % 1024) // 128) + c // 1024) + c % 128, np.arange(16)]).astype(np.float32)
    augk = np.stack([np.floor(kpos / 64.0), kpos % 64.0, np.ones_like(kpos)], 0).astype(np.float32)
    augs = np.stack([np.repeat(64.0 * slopes, 128), np.repeat(slopes, 128)], 0).astype(np.float32)
    shared["augk"] = augk
    shared["augs"] = augs
    cvec = np.zeros((128, 128), np.float32)
    cvec[:, 0:16] = _fm(f("ln1_g")[0]); cvec[:, 16:32] = _fm(f("ln1_b")[0])
    cvec[:, 32:48] = _fm(f("ln2_g")[0]); cvec[:, 48:64] = _fm(f("ln2_b")[0])
    cvec[:, 64:80] = _fm(f("ln3_g")[0]); cvec[:, 80:96] = _fm(f("ln3_b")[0])
    lbl = f("hg_lb_logits")
    cvec[:, 96:104] = lbl[0].reshape(8, 128).T
    cvec[:, 104:112] = lbl[1].reshape(8, 128).T
    cvec[:, 112:120] = f("hg_norm_g")[0].T
    cmat = np.zeros((128, 512), np.float32)
    cmat[:, 384:512] = np.arange(128, dtype=np.float32)[None, :]
    cmat[:, 0:128] = np.eye(128, dtype=np.float32)
    cmat[:, 128:256] = 1.0
    sidx = np.arange(128)[:, None]; tidx = np.arange(128)[None, :]
    cmat[:, 256:384] = (((sidx <= tidx) & ((sidx // 64) == (tidx // 64))) | ((sidx < 64) & (tidx >= 64))).astype(np.float32)
    shared["cmat"] = cmat
    maps = []
    for c in range(8):
        m = dict(shared)
        m["xT"] = _core_tokens(x, meta, c)
        cv = cvec.copy()
        cv[:, 120 + (c % 4)] = 1.0
        m["cvec"] = cv
        j = c % 4
        p = np.arange(128, dtype=np.float32)
        qpos = np.stack([16 + 128 * (4 * i + j) + p for i in range(8)], 0)
        m["augq"] = np.ascontiguousarray((-slopes[None, :, None] * qpos[:, None, :]).reshape(8, 1024).astype(np.float32))
        catt = np.zeros((128, 224), np.float32)
        catt[:, 0:8] = -slopes[None, :]
        catt[:, 8:40] = np.array([16 + 128 * r + 512 * jj for r in range(4) for jj in range(8)], np.float32)[None, :]
        catt[:, 64:96] = (2.0 ** -(np.arange(32) + 1.0))[None, :]
        catt[:, 96:160] = f("idx_k_norm_g")[0][None, :]
        catt[:, 160:224] = f("idx_k_norm_b")[0][None, :]
        m["catt"] = catt
        tt = np.arange(128)[:, None, None]; rr = np.arange(4)[None, :, None]; ss = np.arange(128)[None, None, :]
        m["cbt"] = np.where(128 * (rr - j) + (ss - tt) > 0, -1e30, 0.0).astype(np.float32).reshape(128, 512)
        maps.append(m)
    return maps


_NC_CACHE = {}


def kernel(**inputs):
    stage = int(os.environ.get("KSTAGE", "9"))
    if stage not in _NC_CACHE:
        _NC_CACHE[stage] = Builder(stage).build()
    nc = _NC_CACHE[stage]
    maps = prepare(inputs, stage)
    res = run_bass_kernel_spmd(nc, maps, core_ids=list(range(8)))
    if stage == 4:
        return [(r["dbg"], r["dbg2"]) for r in res.results]
    if stage < 6:
        return [r["dbg"] for r in res.results]
    out = np.zeros((2, 4096, D), np.float32)
    for c in range(8):
        b, j = c // 4, c % 4
        o = res.results[c]["outT"]
        for i in range(8):
            g = 4 * i + j
            out[b, 128 * g:128 * g + 128] = o[:, 128 * i:128 * i + 128].T
    return out
```

```python
import os
import numpy as np
import concourse.bass as bass
import concourse.mybir as mybir
from concourse.bass_utils import run_bass_kernel_spmd

F32 = mybir.dt.float32
BF16 = mybir.dt.bfloat16
U8 = mybir.dt.uint8
AF = mybir.ActivationFunctionType
ALU = mybir.AluOpType
AX = mybir.AxisListType

D = 2048
DFF = 5632
NMETA = 16
NREAL = 1024
NT = NREAL + NMETA
KC = D // 128
FC = DFF // 128
TBS = [(0, 512), (512, 1024), (1024, 1040)]
ALPHA = 2.0 ** 0.25
LN_EPS = 1e-5
RMS_EPS = 1e-6

ENGS = ("pe", "act", "dve", "pool", "sp")


class Op:
    __slots__ = ("eng", "fn", "deps", "is_dma", "dsem", "dval", "sig", "sigidx", "waits")

    def __init__(self, eng, fn, is_dma=False):
        self.eng = eng
        self.fn = fn
        self.deps = []
        self.is_dma = is_dma
        self.dsem = None
        self.dval = 0
        self.sig = False
        self.sigidx = 0
        self.waits = []


class Sched:
    def __init__(self, n_dma_sems=40, same_engine_sync=True):
        self.ops = {e: [] for e in ENGS}
        self.lastw = {}
        self.readers = {}
        self.n_dma = 0
        self.n_dma_sems = n_dma_sems
        self.dma_hist = {}
        self.same_engine_sync = same_engine_sync
        self.n_coll = 0

    def _add(self, op, reads, writes):
        deps = set()
        for k in reads:
            w = self.lastw.get(k)
            if w is not None:
                deps.add(w)
        for k in writes:
            w = self.lastw.get(k)
            if w is not None:
                deps.add(w)
            for r in self.readers.get(k, ()):
                deps.add(r)
        op.deps = list(deps)
        for k in reads:
            self.readers.setdefault(k, []).append(op)
        for k in writes:
            self.lastw[k] = op
            self.readers[k] = []
        self.ops[op.eng].append(op)
        return op

    def op(self, eng, fn, reads=(), writes=()):
        return self._add(Op(eng, fn), reads, writes)

    def dma(self, q, fn, reads=(), writes=()):
        op = Op(q, fn, is_dma=True)
        slot = self.n_dma % self.n_dma_sems
        op.dsem = slot
        op.dval = 16 * (self.n_dma // self.n_dma_sems + 1)
        self.n_dma += 1
        self._add(op, reads, writes)
        prev = self.dma_hist.get(slot)
        if prev is not None:
            op.deps.append(prev)
        self.dma_hist[slot] = op
        return op

    def coll(self, fn, reads=(), writes=()):
        op = Op("pool", fn, is_dma=True)
        op.dsem = self.n_dma_sems + self.n_coll
        op.dval = 1
        self.n_coll += 1
        self._add(op, reads, writes)
        self.dma_hist[op.dsem] = op
        return op

    def barrier(self):
        lasts = []
        for e in ENGS:
            for o in reversed(self.ops[e]):
                if not o.is_dma and o.fn is not None:
                    lasts.append(o)
                    break
        dmas = list(self.dma_hist.values())
        for e in ENGS:
            op = Op(e, None)
            op.deps = [o for o in lasts if o.eng != e] + dmas
            self.ops[e].append(op)
        self.lastw = {}
        self.readers = {}

    def _skip(self, d, op):
        return d.eng == op.eng and (d.eng in ("pe", "sp") or not self.same_engine_sync)

    def finalize(self):
        for e in ENGS:
            for op in self.ops[e]:
                for d in op.deps:
                    if not d.is_dma and not self._skip(d, op):
                        d.sig = True
        for e in ENGS:
            c = 0
            for op in self.ops[e]:
                if op.sig:
                    c += 1
                    op.sigidx = c
        for e in ENGS:
            waited = {}
            for op in self.ops[e]:
                need = {}
                for d in op.deps:
                    if d.is_dma:
                        key, val = ("d", d.dsem), d.dval
                    else:
                        if self._skip(d, op):
                            continue
                        key, val = ("e", d.eng), d.sigidx
                    if waited.get(key, 0) >= val:
                        continue
                    if need.get(key, 0) < val:
                        need[key] = val
                for k, v in need.items():
                    waited[k] = v
                op.waits = list(need.items())

    def emit(self, block, esems, dsems):
        self.finalize()
        regs = {"pe": block.tensor, "act": block.scalar, "dve": block.vector,
                "pool": block.gpsimd, "sp": block.sync}
        final = {d.dsem: d.dval for d in self.dma_hist.values()}

        def make(e):
            ops = self.ops[e]

            def body(eng):
                for op in ops:
                    for (kind, which), val in op.waits:
                        eng.wait_ge(dsems[which] if kind == "d" else esems[which], val)
                    if op.fn is None:
                        continue
                    ins = op.fn(eng)
                    if op.is_dma:
                        ins.then_inc(dsems[op.dsem], 16 if op.dsem < self.n_dma_sems else 1)
                    elif op.sig:
                        ins.then_inc(esems[e], 1)
                if e == "sp":
                    for slot, val in final.items():
                        eng.wait_ge(dsems[slot], val)
            return body

        for e in ENGS:
            regs[e](make(e))


ARENA_F32 = 50816
CONST_F32 = 2304


class Builder:
    def __init__(self, stage):
        self.stage = stage
        self.nc = bass.Bass("TRN2", target_bir_lowering=False)
        self.S = Sched()
        self.dram = {}

    def din(self, name, shape, dt=F32):
        self.dram[name] = self.nc.dram_tensor(name, list(shape), dt, kind="ExternalInput").ap()
        return self.dram[name]

    def dout(self, name, shape, dt=F32):
        self.dram[name] = self.nc.dram_tensor(name, list(shape), dt, kind="ExternalOutput").ap()
        return self.dram[name]

    def dscratch(self, name, shape, dt=F32):
        self.dram[name] = self.nc.dram_tensor(name, list(shape), dt, kind="Internal").ap()
        return self.dram[name]

    def view(self, off_bytes, dt, shape):
        n = 1
        for s in shape:
            n *= s
        esz = 4 if dt == F32 else (1 if dt == U8 else 2)
        assert off_bytes % 4 == 0
        nbytes = n * esz
        assert nbytes % 4 == 0
        assert off_bytes + nbytes <= ARENA_F32 * 4, (off_bytes, nbytes)
        v = self.arena[:, off_bytes // 4:(off_bytes + nbytes) // 4]
        if dt != F32:
            v = v.bitcast(dt)
        if len(shape) == 2:
            v = v.rearrange("p (a b) -> p a b", b=shape[1])
        elif len(shape) == 3:
            v = v.rearrange("p (a b c) -> p a b c", b=shape[1], c=shape[2])
        return v

    def ln_apply(self, tag, z, tbs, cg, cb, toff, eps_eff, strm_out=None, alias_key=None, resid_out=None,
                 final_out=None):
        S, ps = self.S, self.ps
        zsq = [self.view(toff + i * 2048, BF16, [512]) for i in range(2)]
        meanb = self.view(toff + 4096, F32, [512])
        rstdb = self.view(toff + 6144, F32, [512])
        t1 = [self.view(toff + 8192 + i * 2048, F32, [512]) for i in range(2)]
        t2 = [self.view(toff + 12288 + i * 2048, F32, [512]) for i in range(2)]
        o32 = [self.view(toff + 16384 + i * 2048, F32, [512]) for i in range(2)]
        ones = self.ones_f32
        for ti, (t0, t1e) in enumerate(tbs):
            n = t1e - t0
            pm, pq = ps[:, 6, 0:n], ps[:, 7, 0:n]
            for dc in range(KC):
                zq = zsq[dc % 2]
                S.op("act", lambda e, zq=zq, dc=dc, t0=t0, t1e=t1e, n=n: e.activation(
                    out=zq[:, 0:n], in_=z[:, dc, t0:t1e], func=AF.Square, scale=float(D) ** -0.5),
                    reads=[(tag, "z", dc, ti)], writes=[(tag, "zsq", dc % 2)])
                S.op("pe", lambda e, pm=pm, dc=dc, t0=t0, t1e=t1e: e.matmul(
                    pm, lhsT=ones, rhs=z[:, dc, t0:t1e], start=(dc == 0), stop=(dc == KC - 1)),
                    reads=[(tag, "z", dc, ti)], writes=[("ps", 6)])
                S.op("pe", lambda e, pq=pq, zq=zq, dc=dc, n=n: e.matmul(
                    pq, lhsT=self.ones_bf, rhs=zq[:, 0:n], start=(dc == 0), stop=(dc == KC - 1)),
                    reads=[(tag, "zsq", dc % 2), "ones_bf"], writes=[("ps", 7)])
            S.op("act", lambda e, pm=pm, n=n: e.activation(out=meanb[:, 0:n], in_=pm, func=AF.Copy),
                 reads=[("ps", 6)], writes=[(tag, "meanb")])
            S.op("dve", lambda e, n=n: e.tensor_tensor(out=rstdb[:, 0:n], in0=meanb[:, 0:n], in1=meanb[:, 0:n],
                                                       op=ALU.mult),
                 reads=[(tag, "meanb")], writes=[(tag, "rstdb")])
            S.op("dve", lambda e, pq=pq, n=n: e.tensor_tensor(out=rstdb[:, 0:n], in0=pq, in1=rstdb[:, 0:n],
                                                              op=ALU.subtract),
                 reads=[("ps", 7), (tag, "rstdb")], writes=[(tag, "rstdb")])
            S.op("act", lambda e, n=n: e.activation(out=rstdb[:, 0:n], in_=rstdb[:, 0:n], func=AF.Sqrt,
                                                    bias=self.eps_cols[eps_eff], scale=1.0),
                 reads=[(tag, "rstdb")], writes=[(tag, "rstdb")])
            S.op("dve", lambda e, n=n: e.reciprocal(out=rstdb[:, 0:n], in_=rstdb[:, 0:n]),
                 reads=[(tag, "rstdb")], writes=[(tag, "rstdb")])
            S.op("dve", lambda e, n=n: e.scalar_tensor_tensor(out=meanb[:, 0:n], in0=meanb[:, 0:n], scalar=-1.0,
                                                              in1=rstdb[:, 0:n], op0=ALU.mult, op1=ALU.mult),
                 reads=[(tag, "meanb"), (tag, "rstdb")], writes=[(tag, "meanb")])
            for dc in range(KC):
                a, b_, o = t1[dc % 2], t2[dc % 2], o32[dc % 2]
                S.op("dve", lambda e, a=a, dc=dc, t0=t0, t1e=t1e, n=n: e.tensor_tensor(
                    out=a[:, 0:n], in0=z[:, dc, t0:t1e], in1=rstdb[:, 0:n], op=ALU.mult),
                    reads=[(tag, "z", dc, ti), (tag, "rstdb")], writes=[(tag, "t1", dc % 2)])
                S.op("dve", lambda e, a=a, b_=b_, n=n: e.tensor_tensor(
                    out=b_[:, 0:n], in0=a[:, 0:n], in1=meanb[:, 0:n], op=ALU.add),
                    reads=[(tag, "t1", dc % 2), (tag, "meanb")], writes=[(tag, "t2", dc % 2)])
                S.op("act", lambda e, b_=b_, o=o, dc=dc, n=n: e.activation(
                    out=o[:, 0:n], in_=b_[:, 0:n], func=AF.Identity,
                    bias=cb[:, dc:dc + 1], scale=cg[:, dc:dc + 1]),
                    reads=[(tag, "t2", dc % 2)], writes=[(tag, "o32", dc % 2)])
                if strm_out is not None:
                    wk = [(tag, "sout", dc, ti)] + ([(tag, alias_key, dc, ti)] if alias_key else [])
                    S.op("act", lambda e, b_=b_, dc=dc, t0=t0, t1e=t1e, n=n: e.activation(
                        out=strm_out[:, dc, t0:t1e], in_=b_[:, 0:n], func=AF.Identity,
                        bias=cb[:, dc:dc + 1], scale=cg[:, dc:dc + 1]),
                        reads=[(tag, "t2", dc % 2)], writes=wk)
                dst = resid_out if final_out is None else final_out
                if final_out is None or t0 < NREAL:
                    S.dma("sp", lambda e, o=o, dc=dc, t0=t0, t1e=t1e, n=n, dst=dst: e.dma_start(
                        out=dst[dc * 128:(dc + 1) * 128, t0:t1e], in_=o[:, 0:n]),
                        reads=[(tag, "o32", dc % 2)])

    def ffn_phase(self, tag, ntok, tbs, strm_in, strm_out_off, wg, wu, wd, resid, cg, cb,
                  resid_out=None, final_out=None):
        S, nc = self.S, self.nc
        ps = self.ps
        C_OFF = 33280
        B_OFF = 66560
        D_OFF = B_OFF + FC * NT * 2
        T_OFF = D_OFF + 2 * FC * 128 * 2
        hT = self.view(B_OFF, BF16, [FC, ntok])
        wgu = [self.view(C_OFF + i * 16384, BF16, [2, KC, 256]) for i in range(2)]
        wdb = [self.view(D_OFF + i * FC * 128 * 2, BF16, [FC, 128]) for i in range(2)]
        z = self.view(0, F32, [KC, ntok])
        sil = [self.view(T_OFF + 8192 + i * 2048, F32, [512]) for i in range(2)]
        strm_out = self.view(strm_out_off, BF16, [KC, ntok]) if final_out is None else None
        c_scale = 0.5 / ALPHA
        eps_eff = LN_EPS / (ALPHA * ALPHA)
        nb = len(tbs)

        for g in range(FC // 2):
            wb = wgu[g % 2]
            kb = (tag, "wgu", g % 2)
            S.dma("pool", lambda e, wb=wb, g=g: e.dma_start(out=wb[:, 0], in_=wg[g]), writes=[(kb, 0)])
            S.dma("pool", lambda e, wb=wb, g=g: e.dma_start(out=wb[:, 1], in_=wu[g]), writes=[(kb, 1)])
            for fcl in range(2):
                fc = 2 * g + fcl
                for ti, (t0, t1e) in enumerate(tbs):
                    n = t1e - t0
                    pb = (fc * nb + ti) % 2
                    pg, pu = ps[:, 2 * pb, 0:n], ps[:, 2 * pb + 1, 0:n]
                    for k in range(KC):
                        S.op("pe", lambda e, pg=pg, wb=wb, k=k, fcl=fcl, t0=t0, t1e=t1e: e.matmul(
                            pg, lhsT=wb[:, 0, k, fcl * 128:(fcl + 1) * 128], rhs=strm_in[:, k, t0:t1e],
                            start=(k == 0), stop=(k == KC - 1)),
                            reads=[(kb, 0), (tag, "sin")], writes=[("ps", 2 * pb)])
                    for k in range(KC):
                        S.op("pe", lambda e, pu=pu, wb=wb, k=k, fcl=fcl, t0=t0, t1e=t1e: e.matmul(
                            pu, lhsT=wb[:, 1, k, fcl * 128:(fcl + 1) * 128], rhs=strm_in[:, k, t0:t1e],
                            start=(k == 0), stop=(k == KC - 1)),
                            reads=[(kb, 1), (tag, "sin")], writes=[("ps", 2 * pb + 1)])
                    sb = sil[pb]
                    S.op("act", lambda e, sb=sb, pg=pg, n=n: e.activation(out=sb[:, 0:n], in_=pg, func=AF.Silu),
                         reads=[("ps", 2 * pb)], writes=[(tag, "sil", pb)])
                    S.op("dve", lambda e, sb=sb, pu=pu, n=n, fc=fc, t0=t0, t1e=t1e: e.tensor_tensor(
                        out=hT[:, fc, t0:t1e], in0=sb[:, 0:n], in1=pu, op=ALU.mult),
                        reads=[(tag, "sil", pb), ("ps", 2 * pb + 1)], writes=[(tag, "hT", fc, ti)])

        for dc in range(2):
            S.dma("pool", lambda e, dc=dc: e.dma_start(out=wdb[dc], in_=wd[dc]), writes=[(tag, "wd", dc)])
        S.barrier()
        for dc in range(KC):
            wb = wdb[dc % 2]
            kb = (tag, "wd", dc % 2)
            if dc >= 2:
                S.dma("pool", lambda e, wb=wb, dc=dc: e.dma_start(out=wb, in_=wd[dc]), writes=[kb])
            S.dma("sp", lambda e, dc=dc: e.dma_start(out=z[:, dc, :], in_=resid[dc * 128:(dc + 1) * 128, 0:ntok]),
                  writes=[(tag, "z", dc, ti) for ti in range(nb)])
            for ti, (t0, t1e) in enumerate(tbs):
                n = t1e - t0
                pb = 4 + (dc * nb + ti) % 2
                py = ps[:, pb, 0:n]
                for f in range(FC):
                    S.op("pe", lambda e, py=py, wb=wb, f=f, t0=t0, t1e=t1e: e.matmul(
                        py, lhsT=wb[:, f, :], rhs=hT[:, f, t0:t1e], start=(f == 0), stop=(f == FC - 1)),
                        reads=[kb, (tag, "hT", f, ti)], writes=[("ps", pb)])
                S.op("dve", lambda e, py=py, dc=dc, t0=t0, t1e=t1e: e.scalar_tensor_tensor(
                    out=z[:, dc, t0:t1e], in0=py, scalar=c_scale, in1=z[:, dc, t0:t1e],
                    op0=ALU.mult, op1=ALU.add),
                    reads=[("ps", pb), (tag, "z", dc, ti)], writes=[(tag, "z", dc, ti)])
        self.ln_apply(tag, z, tbs, cg, cb, T_OFF, eps_eff, strm_out=strm_out, alias_key="hT",
                      resid_out=resid_out, final_out=final_out)
        S.barrier()

    def proj_fm(self, tag, strm, gi, wbufs, tbs, evac, banks=(0, 1), parity=[0]):
        S, ps = self.S, self.ps
        wb = wbufs[parity[0] % 2]
        kb = ("wb", parity[0] % 2)
        parity[0] += 1
        S.dma("pool", lambda e, wb=wb, gi=gi: e.dma_start(out=wb, in_=self.win[gi]), writes=[kb])
        cnt = 0
        for half in range(2):
            for ti, (t0, t1e) in enumerate(tbs):
                n = t1e - t0
                bk = banks[cnt % len(banks)]
                cnt += 1
                pv = ps[:, bk, 0:n]
                for k in range(KC):
                    S.op("pe", lambda e, pv=pv, wb=wb, k=k, half=half, t0=t0, t1e=t1e: e.matmul(
                        pv, lhsT=wb[:, k, half * 128:(half + 1) * 128], rhs=strm[:, k, t0:t1e],
                        start=(k == 0), stop=(k == KC - 1)),
                        reads=[kb, "strm"], writes=[("ps", bk)])
                evac(half, ti, t0, t1e, pv, bk)

    def proj_tm(self, tag, strm, gi, wbufs, tls, ncols, evac, banks=(0, 1), parity=[0]):
        S, ps = self.S, self.ps
        wb = wbufs[parity[0] % 2]
        kb = ("wb", parity[0] % 2)
        parity[0] += 1
        S.dma("pool", lambda e, wb=wb, gi=gi: e.dma_start(out=wb, in_=self.win[gi]), writes=[kb])
        for cnt, (i, c0, n) in enumerate(tls):
            bk = banks[cnt % len(banks)]
            pv = ps[0:n, bk, 0:ncols]
            for k in range(KC):
                S.op("pe", lambda e, pv=pv, wb=wb, k=k, c0=c0, n=n: e.matmul(
                    pv, lhsT=strm[:, k, c0:c0 + n], rhs=wb[:, k, 0:ncols],
                    start=(k == 0), stop=(k == KC - 1)),
                    reads=[kb, "strm"], writes=[("ps", bk)])
            evac(i, c0, n, pv, bk)

    def hgrn_m1(self, strm):
        S, ps, nc = self.S, self.ps, self.nc
        R1 = 99840
        wbufs = [self.view(R1 + i * 8192, BF16, [KC, 256]) for i in range(2)]
        off = [R1 + 16384]

        def alloc(dt, shape):
            n = 1
            for x in shape:
                n *= x
            nb = n * (4 if dt == F32 else 2)
            nb = (nb + 63) // 64 * 64
            v = self.view(off[0], dt, shape)
            off[0] += nb
            return v
        logf = alloc(F32, [2, NT]); Bg = alloc(F32, [2, NT]); Bsh = alloc(F32, [2, NT])
        tA = alloc(F32, [2, NT]); tB = alloc(F32, [2, NT])
        kk = alloc(BF16, [2, NT]); qs = alloc(BF16, [2, NT]); qt = alloc(BF16, [2, NT])
        kt = alloc(BF16, [2, NT]); kh64 = alloc(BF16, [2, NT]); kh128 = alloc(BF16, [2, NT])
        vv = alloc(BF16, [9, 256])
        PTs = [alloc(BF16, [2, 128]) for _ in range(2)]; khTs = [alloc(BF16, [2, 128]) for _ in range(2)]
        xs_st = alloc(BF16, [9, 256]); xa_st = alloc(F32, [9, 2])
        Qc = self.view(0, BF16, [8, NREAL]); oloc = self.view(16384, BF16, [8, NREAL])
        cv = self.cv
        lbc, omlc = self.lbc, self.omlc
        psb = lambda bk: ps[:, bk, :].bitcast(BF16)
        TL = [(i, 128 * i, 128) for i in range(8)] + [(8, NREAL, NMETA)]
        flat = lambda v: v.rearrange("p h t -> p (h t)")
        r64 = lambda v: v[:, :, 0:NREAL].rearrange("p h (c t) -> p h c t", t=64)
        r128 = lambda v: v[:, :, 0:NREAL].rearrange("p h (c t) -> p h c t", t=128)
        mt = lambda v: v[:, :, NREAL:NT]

        for hp in range(4):
            T = ("m1", hp)
            def ev_f(half, ti, t0, t1e, pv, bk, hp=hp):
                h = 2 * hp + half
                S.op("act", lambda e: e.activation(out=tA[:, half, t0:t1e], in_=pv, func=AF.Sigmoid),
                     reads=[("ps", bk)], writes=[("tA", half, ti)])
                S.op("dve", lambda e: e.tensor_scalar(out=tA[:, half, t0:t1e], in0=tA[:, half, t0:t1e],
                                                      scalar1=omlc[:, h:h + 1], scalar2=lbc[:, h:h + 1],
                                                      op0=ALU.mult, op1=ALU.add),
                     reads=[("tA", half, ti), "lb"], writes=[("tA", half, ti)])
                S.op("act", lambda e: e.activation(out=logf[:, half, t0:t1e], in_=tA[:, half, t0:t1e], func=AF.Ln),
                     reads=[("tA", half, ti)], writes=[("logf", half, ti)])
                S.op("dve", lambda e: e.tensor_scalar(out=kk[:, half, t0:t1e], in0=tA[:, half, t0:t1e],
                                                      scalar1=-1.0, scalar2=1.0, op0=ALU.mult, op1=ALU.add),
                     reads=[("tA", half, ti)], writes=[("kk", half, ti)])
            self.proj_fm(T, strm, self.gidx["hf%d" % hp], wbufs, TBS, ev_f)

            def ev_q(half, ti, t0, t1e, pv, bk):
                S.op("act", lambda e: e.activation(out=qs[:, half, t0:t1e], in_=pv, func=AF.Silu),
                     reads=[("ps", bk)], writes=[("qs", half, ti)])
            self.proj_fm(T, strm, self.gidx["hq%d" % hp], wbufs, TBS, ev_q)

            def ev_v(i, c0, n, pv, bk):
                S.op("act", lambda e: e.activation(out=vv[0:n, i, :], in_=pv, func=AF.Copy),
                     reads=[("ps", bk)], writes=[("vv", i)])
            self.proj_tm(T, strm, self.gidx["hi%d" % hp], wbufs, TL, 256, ev_v)

            allk = lambda nm: [(nm, hl, ti) for hl in range(2) for ti in range(3)]
            S.op("dve", lambda e: e.tensor_tensor_scan(out=flat(Bg), data0=flat(logf), data1=flat(logf),
                                                       initial=0.0, op0=ALU.add, op1=ALU.min),
                 reads=allk("logf"), writes=["Bg"])
            S.op("dve", lambda e: e.memset(flat(Bsh)[:, 0:1], 0.0), writes=["Bsh0"])
            S.op("act", lambda e: e.activation(out=flat(Bsh)[:, 1:2 * NT], in_=flat(Bg)[:, 0:2 * NT - 1], func=AF.Copy),
                 reads=["Bg"], writes=["Bsh"])
            S.op("dve", lambda e: e.tensor_tensor(out=r64(tB), in0=r64(Bg),
                                                  in1=r64(Bsh)[:, :, :, 0:1].to_broadcast([128, 2, 16, 64]),
                                                  op=ALU.subtract),
                 reads=["Bg", "Bsh", "Bsh0"], writes=["tBr"])
            S.op("dve", lambda e: e.tensor_tensor(out=mt(tB), in0=mt(Bg),
                                                  in1=mt(Bsh)[:, :, 0:1].to_broadcast([128, 2, NMETA]),
                                                  op=ALU.subtract),
                 reads=["Bg", "Bsh", "Bsh0"], writes=["tBm"])
            S.op("dve", lambda e: e.tensor_tensor(out=r128(tA), in0=r128(Bg),
                                                  in1=r128(Bsh)[:, :, :, 0:1].to_broadcast([128, 2, 8, 128]),
                                                  op=ALU.subtract),
                 reads=["Bg", "Bsh", "Bsh0"] + allk("tA"), writes=["tAr"] + allk("tA"))
            S.op("dve", lambda e: e.tensor_copy(out=mt(tA), in_=mt(tB)),
                 reads=["tBm"], writes=["tAm"])
            TBk, TAk = ["tBr", "tBm"], ["tAr", "tAm"] + allk("tA")
            S.op("act", lambda e: e.activation(out=flat(Bg), in_=flat(tB), func=AF.Exp),
                 reads=TBk + ["Bsh", "tAr"], writes=["Bg"])
            S.op("dve", lambda e: e.tensor_tensor(out=flat(qt), in0=flat(qs), in1=flat(Bg), op=ALU.mult),
                 reads=["Bg"] + allk("qs"), writes=["qt"])
            S.op("act", lambda e: e.activation(out=flat(Bsh), in_=flat(tB), func=AF.Exp, scale=-1.0),
                 reads=TBk + ["Bsh", "tAr", "Bsh0"], writes=["Bsh", "Bsh0"])
            S.op("dve", lambda e: e.tensor_tensor(out=flat(kt), in0=flat(kk), in1=flat(Bsh), op=ALU.mult),
                 reads=["Bsh"] + allk("kk"), writes=["kt"])
            S.op("dve", lambda e: e.tensor_tensor(out=r64(Bg), in0=r64(tB),
                                                  in1=r64(tB)[:, :, :, 63:64].to_broadcast([128, 2, 16, 64]),
                                                  op=ALU.subtract),
                 reads=TBk + ["qt"], writes=["Bg"])
            S.op("act", lambda e: e.activation(out=r64(Bg), in_=r64(Bg), func=AF.Exp, scale=-1.0),
                 reads=["Bg"], writes=["Bg"])
            S.op("dve", lambda e: e.tensor_tensor(out=r64(kh64), in0=r64(kk), in1=r64(Bg), op=ALU.mult),
                 reads=["Bg"] + allk("kk"), writes=["kh64"])
            S.op("act", lambda e: e.activation(out=flat(Bsh), in_=flat(tA), func=AF.Exp),
                 reads=TAk + ["kt"], writes=["Bsh"])
            S.op("dve", lambda e, hp=hp: e.tensor_tensor(out=Qc[:, 2 * hp:2 * hp + 2, :], in0=qs[:, :, 0:NREAL],
                                                         in1=Bsh[:, :, 0:NREAL], op=ALU.mult),
                 reads=["Bsh"] + allk("qs"), writes=[("Qc", hp)])
            S.op("dve", lambda e: e.tensor_copy(out=xa_st[:, 0:8, :].rearrange("p i h -> p h i"),
                                                in_=r128(Bsh)[:, :, :, 127]),
                 reads=["Bsh"], writes=["xa_st"])
            S.op("dve", lambda e: e.tensor_copy(out=xa_st[:, 8, :], in_=Bsh[:, :, NT - 1]),
                 reads=["Bsh"], writes=["xa_st"])
            S.op("dve", lambda e: e.tensor_tensor(out=r128(Bg), in0=r128(tA),
                                                  in1=r128(tA)[:, :, :, 127:128].to_broadcast([128, 2, 8, 128]),
                                                  op=ALU.subtract),
                 reads=TAk + ["kh64"], writes=["Bg"])
            S.op("dve", lambda e: e.tensor_tensor(out=mt(Bg), in0=mt(tA),
                                                  in1=mt(tA)[:, :, NMETA - 1:NMETA].to_broadcast([128, 2, NMETA]),
                                                  op=ALU.subtract),
                 reads=TAk + ["kh64"], writes=["Bg"])
            S.op("act", lambda e: e.activation(out=flat(Bg), in_=flat(Bg), func=AF.Exp, scale=-1.0),
                 reads=["Bg"], writes=["Bg"])
            S.op("dve", lambda e: e.tensor_tensor(out=flat(kh128), in0=flat(kk), in1=flat(Bg), op=ALU.mult),
                 reads=["Bg"] + allk("kk"), writes=["kh128"])

            def tok_block(i, c0, n, hp=hp):
                par = i % 2
                PT, khT = PTs[par], khTs[par]
                bS, bO = (2, 3) if par == 0 else (6, 7)
                t0c, s0c = par * 256, par * 256
                if n == 128:
                    for hl in range(2):
                        o0 = hl * 128
                        S.op("pe", lambda e, hl=hl, o0=o0, c0=c0: e.matmul(
                            ps[:, bS, o0:o0 + 64], lhsT=kt[:, hl, c0:c0 + 128], rhs=qt[:, hl, c0:c0 + 64],
                            start=True, stop=True), reads=["kt", "qt"], writes=[("ps", bS)])
                        S.op("pe", lambda e, hl=hl, o0=o0, c0=c0: e.matmul(
                            ps[0:64, bS, o0 + 64:o0 + 128], lhsT=kh64[:, hl, c0:c0 + 64],
                            rhs=qt[:, hl, c0 + 64:c0 + 128], start=True, stop=True),
                            reads=["kh64", "qt"], writes=[("ps", bS)])
                        S.op("pe", lambda e, hl=hl, o0=o0, c0=c0: e.matmul(
                            ps[64:128, bS, o0 + 64:o0 + 128], lhsT=kt[:, hl, c0 + 64:c0 + 128],
                            rhs=qt[:, hl, c0 + 64:c0 + 128], start=True, stop=True),
                            reads=["kt", "qt"], writes=[("ps", bS)])
                    for hl in range(2):
                        S.op("dve", lambda e, hl=hl: e.tensor_tensor(
                            out=PT[:, hl, :], in0=ps[:, bS, hl * 128:(hl + 1) * 128], in1=self.mask2, op=ALU.mult),
                            reads=[("ps", bS), "mask2"], writes=[("PT", par, hl)])
                    for hl in range(2):
                        S.op("pe", lambda e, hl=hl, i=i: e.matmul(
                            ps[:, bO, hl * 128:(hl + 1) * 128], lhsT=vv[:, i, hl * 128:(hl + 1) * 128],
                            rhs=PT[:, hl, :], start=True, stop=True),
                            reads=[("vv", i), ("PT", par, hl)], writes=[("ps", bO)])
                    S.op("act", lambda e, hp=hp, c0=c0: e.activation(
                        out=oloc[:, 2 * hp:2 * hp + 2, c0:c0 + 128],
                        in_=ps[:, bO, 0:256].rearrange("p (h t) -> p h t", h=2), func=AF.Copy),
                        reads=[("ps", bO)], writes=[("oloc", hp, i)])
                for hl in range(2):
                    S.op("pe", lambda e, hl=hl, c0=c0, n=n: e.transpose(
                        out=psb(4)[0:n, t0c * 2 + hl * 128:t0c * 2 + (hl + 1) * 128], in_=kh128[:, hl, c0:c0 + n],
                        identity=self.ident_bf),
                        reads=["kh128", "ident"], writes=[("ps4", par)])
                S.op("dve", lambda e, n=n: e.tensor_copy(out=khT[0:n].rearrange("p h d -> p (h d)"),
                                                         in_=psb(4)[0:n, t0c * 2:t0c * 2 + 256]),
                     reads=[("ps4", par)], writes=[("khT", par)])
                for hl in range(2):
                    S.op("pe", lambda e, hl=hl, i=i, n=n: e.matmul(
                        ps[:, 5, s0c + hl * 128:s0c + (hl + 1) * 128], lhsT=khT[0:n, hl, :],
                        rhs=vv[0:n, i, hl * 128:(hl + 1) * 128], start=True, stop=True),
                        reads=[("khT", par), ("vv", i)], writes=[("ps5", par)])
                S.op("dve", lambda e, i=i: e.tensor_copy(out=xs_st[:, i, :], in_=ps[:, 5, s0c:s0c + 256]),
                     reads=[("ps5", par)], writes=[("xs_st", i)])
            for (i_, c0_, n_) in TL:
                tok_block(i_, c0_, n_)
            for q3 in range(3):
                S.dma("sp", lambda e, hp=hp, q3=q3: e.dma_start(
                    out=self.xs[q3].rearrange("p (i c) -> p i c", i=3)[:, :, hp * 256:(hp + 1) * 256],
                    in_=xs_st[:, 3 * q3:3 * q3 + 3, :]),
                    reads=[("xs_st", i) for i in range(9)], writes=[("xs", hp, q3)])
            S.dma("sp", lambda e, hp=hp: e.dma_start(
                out=self.xa.rearrange("p (i c) -> p i c", i=9)[:, :, 2 * hp:2 * hp + 2], in_=xa_st),
                reads=["xa_st"], writes=[("xa", hp)])
        S.barrier()
        rg = [[0, 1, 2, 3], [4, 5, 6, 7]]
        for q3 in range(3):
            S.coll(lambda e, q3=q3: e.collective_compute("AllGather", ALU.bypass, replica_groups=rg,
                                                         ins=[self.xs[q3]], outs=[self.xg[q3]]), writes=[("xg", q3)])
        S.coll(lambda e: e.collective_compute("AllGather", ALU.bypass, replica_groups=rg,
                                              ins=[self.xa], outs=[self.xag]), writes=["xag"])

    def hgrn_m2(self, strm):
        S, ps, nc = self.S, self.ps, self.nc
        R1 = 99840
        wbufs = [self.view(R1 + i * 8192, BF16, [KC, 256]) for i in range(2)]
        off = [R1 + 16384]

        def alloc(dt, shape):
            n = 1
            for x in shape:
                n *= x
            nb = n * (4 if dt == F32 else 2)
            nb = (nb + 63) // 64 * 64
            v = self.view(off[0], dt, shape)
            off[0] += nb
            return v
        sgate = alloc(BF16, [8, NREAL])
        Scur = alloc(F32, [8, 128]); SmF = alloc(F32, [8, 128])
        SAb = [alloc(BF16, [8, 128]) for _ in range(3)]
        Aall = alloc(F32, [4, 72])
        OF = alloc(F32, [8, 128]); OSQ = alloc(F32, [8, 128]); RS = alloc(F32, [8, 128])
        Qc = self.view(0, BF16, [8, NREAL]); oloc = self.view(16384, BF16, [8, NREAL])
        yhg = self.view(32768, BF16, [8, NREAL]); Smine = self.view(49152, BF16, [8, 8, 128])
        f2 = lambda v: v.rearrange("p h t -> p (h t)")
        RTB = TBS[0:2]

        for g4 in range(4):
            def ev_g(half, ti, t0, t1e, pv, bk, g4=g4):
                h = 2 * g4 + half
                S.op("act", lambda e: e.activation(out=sgate[:, h, t0:t1e], in_=pv, func=AF.Silu),
                     reads=[("ps", bk)], writes=[("sgate", h, ti)])
                S.op("dve", lambda e: e.tensor_scalar(out=sgate[:, h, t0:t1e], in0=sgate[:, h, t0:t1e],
                                                      scalar1=self.gnc[:, h:h + 1], scalar2=None, op0=ALU.mult),
                     reads=[("sgate", h, ti), "gn"], writes=[("sgate", h, ti)])
            self.proj_fm("m2", strm, self.gidx["hg%d" % g4], wbufs, RTB, ev_g)

        def out_block(i):
            c0 = 128 * i
            for h in range(8):
                bk = 2 + h // 4
                S.op("pe", lambda e, h=h, i=i, c0=c0, bk=bk: e.matmul(
                    ps[:, bk, (h % 4) * 128:(h % 4 + 1) * 128], lhsT=Smine[:, i, h, :], rhs=Qc[:, h, c0:c0 + 128],
                    start=True, stop=True),
                    reads=[("Smine", i), "Qc"], writes=[("ps", bk)])
            S.op("dve", lambda e, c0=c0: e.tensor_tensor(
                out=OF, in0=ps[:, 2:4, :].rearrange("p a (h t) -> p (a h) t", h=4), in1=oloc[:, :, c0:c0 + 128],
                op=ALU.add),
                reads=[("ps", 2), ("ps", 3), "oloc"], writes=["OF"])
            S.op("act", lambda e: e.activation(out=f2(OSQ), in_=f2(OF), func=AF.Square),
                 reads=["OF"], writes=["OSQ"])
            for a in range(2):
                S.op("pe", lambda e, a=a: e.matmul(ps[:, 4 + a, :], lhsT=self.ones128, rhs=f2(OSQ)[:, a * 512:(a + 1) * 512],
                                                   start=True, stop=True),
                     reads=["OSQ", "ones128"], writes=[("ps", 4 + a)])
            S.op("act", lambda e: e.activation(out=f2(RS), in_=ps[:, 4:6, :].rearrange("p a b -> p (a b)"),
                                               func=AF.Ln, bias=self.eps_rms, scale=1.0),
                 reads=[("ps", 4), ("ps", 5), "eps"], writes=["RS"])
            S.op("act", lambda e: e.activation(out=f2(RS), in_=f2(RS), func=AF.Exp, scale=-0.5),
                 reads=["RS"], writes=["RS"])

        def out_block_b(i):
            c0 = 128 * i
            S.op("dve", lambda e: e.tensor_tensor(out=f2(OF), in0=f2(OF), in1=f2(RS), op=ALU.mult),
                 reads=["OF", "RS"], writes=["OF"])
            S.op("dve", lambda e, c0=c0: e.tensor_tensor(out=yhg[:, :, c0:c0 + 128], in0=OF, in1=sgate[:, :, c0:c0 + 128],
                                                         op=ALU.mult),
                 reads=["OF"] + [("sgate", h, c0 // 512) for h in range(8)], writes=[("yhg", i)])

        S.dma("sp", lambda e: e.dma_start(out=Aall, in_=self.xag.rearrange("(r p) c -> p r c", p=128)),
              reads=["xag"], writes=["Aall"])
        xg3 = [x_.rearrange("(r p) (i c) -> r p i c", p=128, i=3) for x_ in self.xg]
        S.dma("sp", lambda e: e.dma_start(out=f2(SAb[2]), in_=xg3[2][0, :, 2, :]), reads=[("xg", 2)],
              writes=[("SAb", 2)])
        S.op("dve", lambda e: e.tensor_copy(out=f2(Scur), in_=f2(SAb[2])), reads=[("SAb", 2)], writes=["Scur"])
        for g in range(32):
            r, i = g % 4, g // 4
            sb = SAb[g % 3]
            S.dma("sp", lambda e, sb=sb, r=r, i=i: e.dma_start(out=f2(sb), in_=xg3[i // 3][r, :, i % 3, :]),
                  reads=[("xg", i // 3)], writes=[("SAb", g % 3)])
            if r == 0:
                S.op("dve", lambda e: e.tensor_scalar(out=f2(SmF), in0=f2(Scur), scalar1=self.selc[:, 0:1],
                                                      scalar2=None, op0=ALU.mult),
                     reads=["Scur", "sel"], writes=["SmF"])
            else:
                dst = SmF if r < 3 else Smine[:, i]
                S.op("dve", lambda e, r=r, dst=dst: e.scalar_tensor_tensor(
                    out=f2(dst), in0=f2(Scur), scalar=self.selc[:, r:r + 1], in1=f2(SmF),
                    op0=ALU.mult, op1=ALU.add),
                    reads=["Scur", "sel", "SmF"], writes=(["SmF"] if r < 3 else [("Smine", i)]))
            if g < 31:
                for h in range(8):
                    S.op("dve", lambda e, h=h, r=r, i=i, sb=sb: e.scalar_tensor_tensor(
                        out=Scur[:, h, :], in0=Scur[:, h, :], scalar=Aall[:, r, i * 8 + h:i * 8 + h + 1],
                        in1=sb[:, h, :], op0=ALU.mult, op1=ALU.add),
                        reads=["Scur", "Aall", ("SAb", g % 3)], writes=["Scur"])
            if g % 4 == 3:
                out_block(g // 4)
            if g % 4 == 1 and g >= 5:
                out_block_b((g - 5) // 4)
        out_block_b(7)

        S.barrier()

    def attn_m3(self, strm, part):
        S, ps, nc = self.S, self.ps, self.nc
        R1 = 99840
        psb = lambda bk: ps[:, bk, :].bitcast(BF16)
        K_all = self.view(R1, BF16, [2, 4112])
        V_all = self.view(R1 + 16448, BF16, [33, 258])
        IK_all = self.view(R1 + 33536, BF16, [4096])
        AugK = self.view(R1 + 41728, BF16, [4112])
        qT = self.view(R1 + 70656, BF16, [8, NREAL])
        iqT = self.view(R1 + 87040, BF16, [8, NREAL])
        sc = self.view(R1 + 49952, F32, [4096])
        wbufs = [self.view(R1 + i * 8192, BF16, [KC, 256]) for i in range(2)]
        Dg = self.view(R1 + 66336, BF16, [16, 128])
        yatt = self.view(0, BF16, [8, NREAL])
        mb = self.view(16384, BF16, [4096])
        mbT = self.view(24576, BF16, [32, 128])
        junk = self.view(49152, U8, [4096])
        iqz = self.view(49152 + 4096, BF16, [16, 128])
        rh = [self.view(57344 + q * 1024, BF16, [512]) for q in range(4)]
        ya = self.view(61440, BF16, [8, 128])
        PTb = [self.view(61440 + q * 2048, BF16, [1024]) for q in range(2)]
        cbt = self.view(65536, BF16, [4, 128])
        kst = self.view(32768, BF16, [2, NREAL])
        vst = self.view(32768 + 4096, BF16, [8, 258])
        ikst = self.view(32768 + 4096 + 4160, BF16, [NREAL])
        iktmp = self.view(32768 + 10304, F32, [64])
        ikn2 = self.view(32768 + 10304 + 256, BF16, [128])
        kmst = self.view(32768 + 10816, BF16, [2, NMETA])
        vmst = self.view(32768 + 10880, BF16, [258])
        cst = self.cst
        AugQ = cst[:, 664:1176].bitcast(BF16)
        AugR = cst[:, 1176:1688].bitcast(BF16)
        wq = cst[:, 1688:1816].rearrange("p (i h) -> p i h", h=16)
        H = cst[:, 1816:1848]
        Pt = cst[:, 1848:1976]
        g1 = cst[:, 1976:2008]
        mrow = cst[:, 2008:2016]; cc = cst[:, 2016:2024]; rs = cst[:, 2024:2032]
        Bt = cst[:, 2032:2033]; Wc = cst[:, 2033:2034]; mid = cst[:, 2034:2035]; cnt = cst[:, 2035:2036]
        u2 = cst[:, 2036:2037]; tau = cst[:, 2037:2038]; rstd1 = cst[:, 2038:2039]
        nslope = cst[:, 2040:2048]
        Qt = cst[:, 2048:2080].rearrange("p (r j) -> p r j", r=4)
        kmx = cst[:, 2080:2081]
        pw = cst[:, 2104:2136]
        gik = cst[:, 2136:2200]; bik = cst[:, 2200:2264]
        st6 = cst[:, 2264:2270]; mv = cst[:, 2270:2272]
        TL = [(i, 128 * i, 128) for i in range(8)] + [(8, NREAL, NMETA)]
        RTB = TBS[0:2]
        NB = 14

        if part == 0:
            S.dma("sp", lambda e: e.dma_start(out=cst[:, 2040:2264], in_=self.catt), writes=["catt"])
            S.dma("sp", lambda e: e.dma_start(out=Pt, in_=self.cmat_d[:, 384:512]), writes=["Pt"])
            S.op("dve", lambda e: e.memset(vst.rearrange("p i (k c) -> p i k c", k=2)[:, :, :, 128:129], 1.0),
                 writes=["vst1"])
            S.op("dve", lambda e: e.memset(vmst.rearrange("p (k c) -> p k c", k=2)[:, :, 128:129], 1.0),
                 writes=["V1"])

            def ev_k(half, ti, t0, t1e, pv, bk):
                if ti < 2:
                    S.op("act", lambda e: e.activation(out=kst[:, half, t0:t1e], in_=pv, func=AF.Copy),
                         reads=[("ps", bk)], writes=[("kst", half, ti)])
                else:
                    S.op("act", lambda e: e.activation(out=kmst[:, half, :], in_=pv, func=AF.Copy),
                         reads=[("ps", bk)], writes=[("Kmeta", half)])
            self.proj_fm("m3", strm, self.gidx["ak"], wbufs, TBS, ev_k)

            def ev_v(i, c0, n, pv, bk):
                src = pv.rearrange("p (k c) -> p k c", k=2)
                if i < 8:
                    dst = vst[:, i, :].rearrange("p (k c) -> p k c", k=2)[:, :, 0:128]
                    S.op("act", lambda e: e.activation(out=dst, in_=src, func=AF.Copy),
                         reads=[("ps", bk), "vst1"], writes=[("vst", i)])
                else:
                    dst = vmst[0:n, :].rearrange("p (k c) -> p k c", k=2)[:, :, 0:128]
                    S.op("act", lambda e: e.activation(out=dst, in_=src, func=AF.Copy),
                         reads=[("ps", bk), "V1"], writes=["Vmeta"])
            self.proj_tm("m3", strm, self.gidx["av"], wbufs, TL, 256, ev_v)

            def ev_ik(i, c0, n, pv, bk):
                S.op("dve", lambda e: e.bn_stats(out=st6, in_=pv[:, 0:64]), reads=[("ps", bk)], writes=["st6"])
                S.op("dve", lambda e: e.bn_aggr(out=mv, in_=st6), reads=["st6"], writes=["mv"])
                S.op("act", lambda e: e.activation(out=rstd1, in_=mv[:, 1:2], func=AF.Sqrt, bias=self.eps_ik, scale=1.0),
                     reads=["mv", "eps"], writes=["rstd1"])
                S.op("dve", lambda e: e.reciprocal(out=rstd1, in_=rstd1), reads=["rstd1"], writes=["rstd1"])
                S.op("dve", lambda e: e.tensor_scalar(out=iktmp, in0=pv[:, 0:64], scalar1=mv[:, 0:1], scalar2=rstd1,
                                                      op0=ALU.subtract, op1=ALU.mult),
                     reads=[("ps", bk), "mv", "rstd1"], writes=["iktmp"])
                S.op("dve", lambda e: e.tensor_tensor(out=iktmp, in0=iktmp, in1=gik, op=ALU.mult),
                     reads=["iktmp", "catt"], writes=["iktmp"])
                S.op("dve", lambda e: e.tensor_tensor(out=ikn2[:, 0:64], in0=iktmp, in1=bik, op=ALU.add),
                     reads=["iktmp", "catt"], writes=["ikn2a"])
                S.op("dve", lambda e: e.tensor_copy(out=ikn2[:, 64:128], in_=ikn2[:, 0:64]),
                     reads=["ikn2a"], writes=["ikn2b"])
                S.op("act", lambda e, i=i: e.activation(out=wq[:, i, :], in_=pv[:, 64:80], func=AF.Copy,
                                                        scale=0.25 * 0.125),
                     reads=[("ps", bk)], writes=[("wq", i)])
                S.op("pe", lambda e: e.transpose(out=psb(2)[:, 0:128], in_=ikn2, identity=self.ident_bf),
                     reads=["ikn2a", "ikn2b", "ident"], writes=[("ps", 2)])
                S.op("act", lambda e, c0=c0: e.activation(out=ikst[:, c0:c0 + 128], in_=psb(2)[:, 0:128], func=AF.Copy),
                     reads=[("ps", 2)], writes=[("ikst", i)])
            self.proj_tm("m3", strm, self.gidx["ikw"], wbufs, TL[0:8], 80, ev_ik)

            S.dma("sp", lambda e: e.dma_start(out=self.ks.rearrange("p (k t) -> p k t", k=2), in_=kst),
                  reads=[("kst", hh, ti) for hh in range(2) for ti in range(2)], writes=["ks"])
            S.dma("sp", lambda e: e.dma_start(out=self.vs[:, 0:2064].rearrange("p (i c) -> p i c", i=8), in_=vst),
                  reads=[("vst", i) for i in range(8)] + ["vst1"], writes=["vs"])
            S.dma("sp", lambda e: e.dma_start(out=self.vs[:, 2064:3088], in_=ikst),
                  reads=[("ikst", i) for i in range(8)], writes=["vs2"])
            S.dma("sp", lambda e: e.dma_start(out=self.kms.rearrange("p (k t) -> p k t", k=2), in_=kmst),
                  reads=[("Kmeta", 0), ("Kmeta", 1)], writes=["kms"])
            S.dma("sp", lambda e: e.dma_start(out=self.vms, in_=vmst[0:NMETA, :]),
                  reads=["Vmeta", "V1"], writes=["vms"])
            S.barrier()
            rg = [[0, 1, 2, 3], [4, 5, 6, 7]]
            S.coll(lambda e: e.collective_compute("AllGather", ALU.bypass, replica_groups=rg,
                                                  ins=[self.ks], outs=[self.kg]), writes=["kg"])
            S.coll(lambda e: e.collective_compute("AllGather", ALU.bypass, replica_groups=rg,
                                                  ins=[self.vs], outs=[self.vg]), writes=["vg"])
            return

        if part == 1:
            for g4 in range(4):
                def ev_q(half, ti, t0, t1e, pv, bk, g4=g4):
                    h = 2 * g4 + half
                    S.op("act", lambda e: e.activation(out=qT[:, h, t0:t1e], in_=pv, func=AF.Copy, scale=128.0 ** -0.5),
                         reads=[("ps", bk)], writes=[("qT", h, ti)])
                self.proj_fm("m3", strm, self.gidx["aq%d" % g4], wbufs, RTB, ev_q)
            for g4 in range(4):
                def ev_iq(half, ti, t0, t1e, pv, bk, g4=g4):
                    h = 2 * g4 + half
                    S.op("dve", lambda e: e.tensor_copy(out=iqT[:, h, t0:t1e], in_=pv),
                         reads=[("ps", bk)], writes=[("iqT", h, ti)])
                self.proj_fm("m3", strm, self.gidx["iq%d" % g4], wbufs, RTB, ev_iq)
            return

        S.op("pool", lambda e: e.memset(AugK[0:65, :], 0.0), writes=["AugK"])
        S.op("pool", lambda e: e.memset(AugR[0:65, :], 0.0), writes=["AugR"])
        for rr in range(3):
            S.dma("pool", lambda e, rr=rr: e.dma_start(out=AugK[32 * rr:32 * rr + 1, :], in_=self.augk[rr:rr + 1, :]),
                  writes=["AugK"])
        for rr in range(2):
            S.dma("pool", lambda e, rr=rr: e.dma_start(out=AugR[32 * rr:32 * rr + 1, :], in_=self.augs[rr:rr + 1, :]),
                  writes=["AugR"])
        S.dma("pool", lambda e: e.dma_start(out=cbt, in_=self.cbt_d.rearrange("p (r s) -> p r s", r=4)), writes=["cbt"])
        for r in range(4):
            S.dma("sp", lambda e, r=r: e.dma_start(
                out=K_all[:, :, r * 1024:(r + 1) * 1024],
                in_=self.kg[r * 128:(r + 1) * 128, :].rearrange("p (k t) -> p k t", k=2)),
                reads=["kg"], writes=["K_all"])
            S.dma("sp", lambda e, r=r: e.dma_start(
                out=V_all[:, r * 8:(r + 1) * 8, :],
                in_=self.vg[r * 128:(r + 1) * 128, 0:2064].rearrange("p (i c) -> p i c", i=8)),
                reads=["vg"], writes=["V_all"])
            S.dma("sp", lambda e, r=r: e.dma_start(
                out=IK_all[:, r * 1024:(r + 1) * 1024], in_=self.vg[r * 128:(r + 1) * 128, 2064:3088]),
                reads=["vg"], writes=["IK_all"])
        S.dma("sp", lambda e: e.dma_start(out=K_all[:, :, 4096:4112], in_=self.kms.rearrange("p (k t) -> p k t", k=2)),
              reads=["kms"], writes=[("Kmeta", 0), ("Kmeta", 1)])
        S.dma("sp", lambda e: e.dma_start(out=V_all[0:NMETA, 32, :], in_=self.vms), reads=["vms"], writes=["Vmeta"])
        S.barrier()

        S.op("pool", lambda e: e.memset(iqz.rearrange("p h t -> p (h t)"), 0.0), writes=["iqz"])
        sc4 = sc.rearrange("p (r c) -> p r c", r=4)
        mb4 = mb.rearrange("p (r c) -> p r c", r=4)
        jk4 = junk.rearrange("p (r c) -> p r c", r=4)
        def geom(i):
            q0 = 128 * i
            nk = 128 * (i + 1)
            pieces = [(r, c0, min(512, nk - c0)) for r in range(4) for c0 in range(0, nk, 512)]
            return q0, nk, pieces

        def st_idx(i):
            q0, nk, pieces = geom(i)
            for h in range(16):
                S.op("act", lambda e, h=h: e.activation(out=Dg[:, h, :], in_=self.ident_bf, func=AF.Copy,
                                                        scale=wq[:, i, h:h + 1]),
                     reads=["ident", ("wq", i)], writes=["Dg"])
            for h in range(16):
                hb = h % 2
                eng = "act"
                if eng == "pool":
                    S.op("pool", lambda e, h=h, hb=hb: e.tensor_copy(
                        out=iqz[hb * 64:(hb + 1) * 64, h, :], in_=iqT[hb * 64:(hb + 1) * 64, h // 2, q0:q0 + 128]),
                        reads=["iqT", "iqz"], writes=[("iqzh", h)])
                else:
                    S.op("act", lambda e, h=h, hb=hb: e.activation(
                        out=iqz[hb * 64:(hb + 1) * 64, h, :], in_=iqT[hb * 64:(hb + 1) * 64, h // 2, q0:q0 + 128],
                        func=AF.Copy),
                        reads=["iqT", "iqz"], writes=[("iqzh", h)])
            for pi, (r, c0, cn) in enumerate(pieces):
                col0 = r * 1024 + c0
                accb = 4 + pi % 2

                def head_mm(h, cn=cn, col0=col0):
                    bk, hb = h % 4, h % 2
                    S.op("pe", lambda e: e.matmul(
                        ps[:, bk, 0:cn], lhsT=iqz[:, h, :],
                        rhs=IK_all[:, col0:col0 + cn], start=True, stop=True),
                        reads=["IK_all", ("iqzh", h)], writes=[("ps", bk)])
                    if h % 2 == 0:
                        S.op("act", lambda e: e.activation(out=rh[bk][:, 0:cn], in_=ps[:, bk, 0:cn], func=AF.Relu),
                             reads=[("ps", bk)], writes=[("rh", bk)])
                    else:
                        S.op("dve", lambda e: e.tensor_scalar(out=rh[bk][:, 0:cn], in0=ps[:, bk, 0:cn], scalar1=0.0,
                                                              scalar2=None, op0=ALU.max),
                             reads=[("ps", bk)], writes=[("rh", bk)])

                def head_acc(h, cn=cn, accb=accb):
                    bk = h % 4
                    S.op("pe", lambda e: e.matmul(
                        ps[:, accb, 0:cn], lhsT=Dg[:, h, :], rhs=rh[bk][:, 0:cn], start=(h == 0), stop=(h == 15)),
                        reads=["Dg", ("rh", bk)], writes=[("ps", accb)])
                for h in range(16):
                    head_mm(h)
                    if h >= 2:
                        head_acc(h - 2)
                head_acc(14)
                head_acc(15)
                S.op("act", lambda e, accb=accb, col0=col0, cn=cn: e.activation(
                    out=sc[:, col0:col0 + cn], in_=ps[:, accb, 0:cn], func=AF.Copy),
                    reads=[("ps", accb)], writes=["sc"])

        def st_bis(i):
            q0, nk, pieces = geom(i)
            scv, mbv, jkv = sc4[:, :, 0:nk], mb4[:, :, 0:nk], jk4[:, :, 0:nk]
            S.op("dve", lambda e: e.reduce_max(out=Bt, in_=scv, axis=AX.XY, apply_absolute_value=True),
                 reads=["sc"], writes=["Bt"])
            S.op("dve", lambda e: e.tensor_tensor(out=sc4[:, :, q0:q0 + 128], in0=sc4[:, :, q0:q0 + 128], in1=cbt,
                                                  op=ALU.add),
                 reads=["sc", "cbt", "Bt"], writes=["sc"])
            S.op("dve", lambda e: e.tensor_scalar(out=Wc, in0=Bt, scalar1=2.0002, scalar2=1e-6,
                                                  op0=ALU.mult, op1=ALU.add), reads=["Bt"], writes=["Wc"])
            S.op("dve", lambda e: e.tensor_scalar(out=H[:, 0:NB + 1], in0=pw[:, 0:NB + 1], scalar1=Wc, scalar2=None,
                                                  op0=ALU.mult), reads=["Wc", "catt"], writes=["H"])
            S.op("dve", lambda e: e.memset(mid, 0.0), writes=["mid"])
            for k in range(NB):
                S.op("dve", lambda e: e.tensor_scalar(
                    out=jkv, in0=scv, scalar1=mid, scalar2=0.0, op0=ALU.is_ge, op1=ALU.add, accum_out=cnt),
                    reads=["sc", "mid"], writes=["junk", "cnt"])
                S.op("dve", lambda e, k=k: e.tensor_scalar(out=u2, in0=cnt, scalar1=256.0, scalar2=H[:, k:k + 1],
                                                           op0=ALU.is_ge, op1=ALU.mult),
                     reads=["cnt", "H"], writes=["u2"])
                S.op("dve", lambda e, k=k: e.scalar_tensor_tensor(out=mid, in0=mid, scalar=H[:, k + 1:k + 2], in1=u2,
                                                                  op0=ALU.subtract, op1=ALU.add),
                     reads=["mid", "H", "u2"], writes=["mid"])
            S.op("dve", lambda e: e.tensor_tensor(out=tau, in0=mid, in1=H[:, NB:NB + 1], op=ALU.subtract),
                 reads=["mid", "H"], writes=["tau"])
            S.op("dve", lambda e: e.tensor_scalar(
                out=mbv, in0=scv, scalar1=tau, scalar2=-30000.0, op0=ALU.is_lt, op1=ALU.mult),
                reads=["sc", "tau"], writes=["mb"])
            nb_ = i + 1
            sc5 = scv.rearrange("p r (j s) -> p r j s", s=128)
            mb5 = mbv.rearrange("p r (j s) -> p r j s", s=128)
            S.op("dve", lambda e: e.tensor_tensor(
                out=sc5, in0=mb5, in1=Pt.unsqueeze(1).unsqueeze(1).to_broadcast([128, 4, nb_, 128]), op=ALU.add),
                reads=["mb", "Pt", "sc"], writes=["sc"])
            g1v = g1.rearrange("p (r j) -> p r j", r=4)[:, :, 0:nb_]
            S.op("dve", lambda e: e.tensor_reduce(out=g1v, in_=sc5, axis=AX.X, op=ALU.max),
                 reads=["sc"], writes=["g1"])
            S.op("dve", lambda e: e.tensor_tensor(out=g1v, in0=g1v, in1=Qt[:, :, 0:nb_], op=ALU.add),
                 reads=["g1", "catt"], writes=["g1"])
            S.op("dve", lambda e: e.tensor_reduce(out=kmx, in_=g1v, axis=AX.XY, op=ALU.max),
                 reads=["g1"], writes=["kmx"])
            S.op("dve", lambda e: e.tensor_scalar(out=kmx, in0=kmx, scalar1=15.0, scalar2=None, op0=ALU.max),
                 reads=["kmx"], writes=["kmx"])
            S.op("dve", lambda e: e.tensor_scalar(out=cc, in0=nslope, scalar1=kmx, scalar2=None, op0=ALU.mult),
                 reads=["kmx", "catt"], writes=["cc"])

        def st_mbT(i):
            kts = [(r, ip) for r in range(4) for ip in range(i + 1)]
            for g0 in range(0, len(kts), 8):
                grp = kts[g0:g0 + 8]
                bk = 6 + (g0 // 8) % 2
                for s_, (r, ip) in enumerate(grp):
                    S.op("pe", lambda e, bk=bk, s_=s_, r=r, ip=ip: e.transpose(
                        out=psb(bk)[:, s_ * 128:(s_ + 1) * 128], in_=mb[:, r * 1024 + ip * 128:r * 1024 + ip * 128 + 128],
                        identity=self.ident_bf),
                        reads=["mb", "ident"], writes=[("ps", bk)])
                for s_, (r, ip) in enumerate(grp):
                    S.op("act", lambda e, bk=bk, s_=s_, r=r, ip=ip: e.activation(
                        out=mbT[:, r * 8 + ip, :], in_=psb(bk)[:, s_ * 128:(s_ + 1) * 128], func=AF.Copy),
                        reads=[("ps", bk)], writes=["mbT"])

        def st_passA(i):
            q0, nk, pieces = geom(i)
            S.op("dve", lambda e: e.memset(ps[:, 5:8, :].rearrange("p a b -> p (a b)"), 0.0),
                 writes=[("ps", 5), ("ps", 6), ("ps", 7)])
            Dc = PTb[1].rearrange("p (h t) -> p h t", h=8)
            for h in range(8):
                S.op("dve", lambda e, h=h: e.tensor_scalar(out=Dc[:, h, :], in0=self.ident_bf, scalar1=cc[:, h:h + 1],
                                                           scalar2=None, op0=ALU.mult),
                     reads=["ident", "cc"], writes=[("PTb", 1)])
            for a_ in range(2):
                S.op("pe", lambda e, a_=a_: e.matmul(ps[:, 4, :], lhsT=self.ones_bf,
                                                     rhs=PTb[1][:, a_ * 512:(a_ + 1) * 512],
                                                     start=True, stop=True),
                     reads=[("PTb", 1), "ones_bf"], writes=[("ps", 4)])
                S.op("dve", lambda e, a_=a_: e.tensor_copy(out=AugR[64:65, a_ * 512:(a_ + 1) * 512], in_=ps[64:65, 4, :]),
                     reads=[("ps", 4)], writes=["AugR"])

        def st_passB(i):
            q0, nk, pieces = geom(i)
            ktl = [(r * 1024 + ip * 128, r * 8 + ip, 128) for r in range(4) for ip in range(i + 1)] + [(4096, 32, NMETA)]
            Oreg = lambda h: ps[:, 5 + h // 3, (h % 3) * 129:(h % 3 + 1) * 129]

            def logits(qi):
                col0, vt, n = ktl[qi]
                meta = (n == NMETA)
                pair = (0, 1) if qi % 2 == 0 else (2, 3)
                for h in range(8):
                    kvh = h // 4
                    out = ps[0:n, pair[h // 4], (h % 4) * 128:(h % 4 + 1) * 128]
                    S.op("pe", lambda e, out=out, kvh=kvh, h=h: e.matmul(
                        out, lhsT=K_all[:, kvh, col0:col0 + n], rhs=qT[:, h, q0:q0 + 128], start=True, stop=False),
                        reads=["K_all", "qT", ("Kmeta", kvh)], writes=[("ps", pair[h // 4])])
                    S.op("pe", lambda e, out=out, h=h: e.matmul(
                        out, lhsT=AugK[0:65, col0:col0 + n], rhs=AugR[0:65, h * 128:(h + 1) * 128],
                        start=False, stop=meta),
                        reads=["AugK", "AugR"], writes=[("ps", pair[h // 4])])
                    if not meta:
                        S.op("pe", lambda e, out=out: e.matmul(
                            out, lhsT=self.ident_bf, rhs=mbT[:, vt, :], start=False, stop=True),
                            reads=["mbT", "ident"], writes=[("ps", pair[h // 4])])
                pt = PTb[qi % 2]
                S.op("act", lambda e: e.activation(
                    out=pt[0:n, :], in_=ps[0:n, pair[0]:pair[0] + 2, :].rearrange("p a b -> p (a b)"), func=AF.Exp),
                    reads=[("ps", pair[0]), ("ps", pair[1])], writes=[("PTb", qi % 2)])

            def pv(qi):
                col0, vt, n = ktl[qi]
                pt = PTb[qi % 2]
                for h in range(8):
                    kvh = h // 4
                    S.op("pe", lambda e, h=h, kvh=kvh: e.matmul(
                        Oreg(h), lhsT=pt[0:n, h * 128:(h + 1) * 128], rhs=V_all[0:n, vt, kvh * 129:(kvh + 1) * 129],
                        start=False, stop=(qi == len(ktl) - 1)),
                        reads=[("PTb", qi % 2), "V_all", "Vmeta"], writes=[("ps", 5 + h // 3)])
            logits(0)
            for qi in range(len(ktl)):
                if qi + 1 < len(ktl):
                    logits(qi + 1)
                pv(qi)

        def st_fin(i):
            q0 = 128 * i
            for b3 in range(3):
                nh = 3 if b3 < 2 else 2
                Ov = ps[:, 5 + b3, 0:nh * 129].rearrange("p (h c) -> p h c", c=129)
                S.op("dve", lambda e, Ov=Ov, b3=b3, nh=nh: e.reciprocal(out=rs[:, 3 * b3:3 * b3 + nh], in_=Ov[:, :, 128]),
                     reads=[("ps", 5 + b3)], writes=[("rs", b3)])
                S.op("dve", lambda e, Ov=Ov, b3=b3, nh=nh: e.tensor_tensor(
                    out=ya[:, 3 * b3:3 * b3 + nh, :], in0=Ov[:, :, 0:128],
                    in1=rs[:, 3 * b3:3 * b3 + nh].unsqueeze(2).to_broadcast([128, nh, 128]), op=ALU.mult),
                    reads=[("ps", 5 + b3), ("rs", b3)], writes=[("ya", b3), ("PTb", 0)])
            for h in range(8):
                S.op("pe", lambda e, h=h: e.transpose(out=psb(4)[:, h * 128:(h + 1) * 128], in_=ya[:, h, :],
                                                      identity=self.ident_bf),
                     reads=[("ya", h // 3), ("PTb", 0), "ident"], writes=[("ps", 4)])
            S.op("act", lambda e: e.activation(out=yatt[:, :, q0:q0 + 128],
                                               in_=psb(4).rearrange("p (h t) -> p h t", h=8), func=AF.Copy),
                 reads=[("ps", 4)], writes=[("yatt", i)])

        st_idx(0)
        st_bis(0)
        st_mbT(0)
        for i in range(8):
            if i + 1 < 8:
                st_idx(i + 1)
            st_passA(i)
            if i + 1 < 8:
                st_bis(i + 1)
            st_passB(i)
            st_fin(i)
            if i + 1 < 8:
                st_mbT(i + 1)
        S.barrier()

    def merge_m4(self, strm, cg, cb):
        S, ps, nc = self.S, self.ps, self.nc
        R1 = 99840
        RTB = TBS[0:2]
        yatt = self.view(0, BF16, [8, NREAL]); yhg = self.view(32768, BF16, [8, NREAL])
        merged = self.view(R1, BF16, [KC, NREAL])
        o = R1 + 32768
        wga = [self.view(o + q * 8192, BF16, [KC, 256]) for q in range(2)]
        wgh = [self.view(o + 16384 + q * 8192, BF16, [KC, 256]) for q in range(2)]
        wba = [self.view(o + 32768 + q * 4096, BF16, [8, 256]) for q in range(2)]
        wbh = [self.view(o + 40960 + q * 4096, BF16, [8, 256]) for q in range(2)]
        tm = [self.view(o + 49152 + q * 2048, F32, [512]) for q in range(4)]
        for mg in range(8):
            q = mg % 2
            S.dma("pool", lambda e, q=q, mg=mg: e.dma_start(out=wga[q], in_=self.win[self.gidx["ga%d" % mg]]),
                  writes=[("wga", q)])
            S.dma("pool", lambda e, q=q, mg=mg: e.dma_start(out=wgh[q], in_=self.win[self.gidx["gh%d" % mg]]),
                  writes=[("wgh", q)])
            S.dma("pool", lambda e, q=q, mg=mg: e.dma_start(out=wba[q], in_=self.wba_d[mg]), writes=[("wba", q)])
            S.dma("pool", lambda e, q=q, mg=mg: e.dma_start(out=wbh[q], in_=self.wbh_d[mg]), writes=[("wbh", q)])
            for half in range(2):
                mc = 2 * mg + half
                hs = slice(half * 128, (half + 1) * 128)
                for ti, (t0, t1e) in enumerate(RTB):
                    pp = (half * 2 + ti) % 2
                    b0 = 4 * pp
                    for k in range(KC):
                        S.op("pe", lambda e, b0=b0, q=q, k=k, hs=hs, t0=t0, t1e=t1e: e.matmul(
                            ps[:, b0, :], lhsT=wga[q][:, k, hs], rhs=strm[:, k, t0:t1e],
                            start=(k == 0), stop=(k == KC - 1)),
                            reads=[("wga", q), "strm"], writes=[("ps", b0)])
                    for k in range(8):
                        S.op("pe", lambda e, b0=b0, q=q, k=k, hs=hs, t0=t0, t1e=t1e: e.matmul(
                            ps[:, b0 + 1, :], lhsT=wba[q][:, k, hs], rhs=yatt[:, k, t0:t1e],
                            start=(k == 0), stop=(k == 7)),
                            reads=[("wba", q), "yatt"], writes=[("ps", b0 + 1)])
                    for k in range(KC):
                        S.op("pe", lambda e, b0=b0, q=q, k=k, hs=hs, t0=t0, t1e=t1e: e.matmul(
                            ps[:, b0 + 2, :], lhsT=wgh[q][:, k, hs], rhs=strm[:, k, t0:t1e],
                            start=(k == 0), stop=(k == KC - 1)),
                            reads=[("wgh", q), "strm"], writes=[("ps", b0 + 2)])
                    for k in range(8):
                        S.op("pe", lambda e, b0=b0, q=q, k=k, hs=hs, t0=t0, t1e=t1e: e.matmul(
                            ps[:, b0 + 3, :], lhsT=wbh[q][:, k, hs], rhs=yhg[:, k, t0:t1e],
                            start=(k == 0), stop=(k == 7)),
                            reads=[("wbh", q), "yhg"], writes=[("ps", b0 + 3)])
                    ta, th = tm[2 * pp], tm[2 * pp + 1]
                    S.op("act", lambda e, ta=ta, b0=b0: e.activation(out=ta, in_=ps[:, b0, :], func=AF.Sigmoid),
                         reads=[("ps", b0)], writes=[("tm", 2 * pp)])
                    S.op("dve", lambda e, ta=ta, b0=b0: e.tensor_tensor(out=ta, in0=ta, in1=ps[:, b0 + 1, :], op=ALU.mult),
                         reads=[("tm", 2 * pp), ("ps", b0 + 1)], writes=[("tm", 2 * pp)])
                    S.op("act", lambda e, th=th, b0=b0: e.activation(out=th, in_=ps[:, b0 + 2, :], func=AF.Sigmoid),
                         reads=[("ps", b0 + 2)], writes=[("tm", 2 * pp + 1)])
                    S.op("dve", lambda e, th=th, b0=b0: e.tensor_tensor(out=th, in0=th, in1=ps[:, b0 + 3, :], op=ALU.mult),
                         reads=[("tm", 2 * pp + 1), ("ps", b0 + 3)], writes=[("tm", 2 * pp + 1)])
                    S.op("dve", lambda e, ta=ta, th=th, mc=mc, t0=t0, t1e=t1e: e.tensor_tensor(
                        out=merged[:, mc, t0:t1e], in0=ta, in1=th, op=ALU.add),
                        reads=[("tm", 2 * pp), ("tm", 2 * pp + 1)], writes=[("merged", mc, ti)])
        wo = [self.view(53760 + q * 4096, BF16, [KC, 128]) for q in range(2)]
        for q in range(2):
            S.dma("pool", lambda e, q=q: e.dma_start(out=wo[q], in_=self.wo_d[q]), writes=[("wo", q)])
        S.barrier()
        z = self.view(R1 + 32768, F32, [KC, NREAL])
        strm_out = self.view(0, BF16, [KC, NREAL])
        for dc in range(KC):
            q = dc % 2
            if dc >= 2:
                S.dma("pool", lambda e, q=q, dc=dc: e.dma_start(out=wo[q], in_=self.wo_d[dc]), writes=[("wo", q)])
            S.dma("sp", lambda e, dc=dc: e.dma_start(out=z[:, dc, :], in_=self.h1s[dc * 128:(dc + 1) * 128, 0:NREAL]),
                  writes=[("l2", "z", dc, ti) for ti in range(2)])
            for ti, (t0, t1e) in enumerate(RTB):
                pb = (dc * 2 + ti) % 2
                for k in range(KC):
                    S.op("pe", lambda e, pb=pb, q=q, k=k, t0=t0, t1e=t1e: e.matmul(
                        ps[:, pb, :], lhsT=wo[q][:, k, :], rhs=merged[:, k, t0:t1e],
                        start=(k == 0), stop=(k == KC - 1)),
                        reads=[("wo", q), ("merged", k, ti)], writes=[("ps", pb)])
                S.op("dve", lambda e, pb=pb, dc=dc, t0=t0, t1e=t1e: e.scalar_tensor_tensor(
                    out=z[:, dc, t0:t1e], in0=ps[:, pb, :], scalar=1.0 / ALPHA, in1=z[:, dc, t0:t1e],
                    op0=ALU.mult, op1=ALU.add),
                    reads=[("ps", pb), ("l2", "z", dc, ti)], writes=[("l2", "z", dc, ti)])
        self.ln_apply("l2", z, RTB, cg, cb, 33280, LN_EPS / (ALPHA * ALPHA), strm_out=strm_out,
                      resid_out=self.h2s)
        S.barrier()

    def eps_col(self, val):
        return self.eps_cols[val]

    def build(self):
        nc, S = self.nc, self.S
        stage = self.stage
        xT = self.din("xT", [D, NT])
        wg1 = self.din("wg1", [FC // 2, 128, KC, 256])
        wu1 = self.din("wu1", [FC // 2, 128, KC, 256])
        wd1 = self.din("wd1", [KC, 128, FC, 128])
        wg2 = self.din("wg2", [FC // 2, 128, KC, 256])
        wu2 = self.din("wu2", [FC // 2, 128, KC, 256])
        wd2 = self.din("wd2", [KC, 128, FC, 128])
        cvec = self.din("cvec", [128, 128])
        cmat = self.cmat_d = self.din("cmat", [128, 512])
        self.catt = self.din("catt", [128, 224])
        self.augk = self.din("augk", [3, 4112])
        self.augs = self.din("augs", [2, 1024])
        self.augq = self.din("augq", [8, 1024])
        self.cbt_d = self.din("cbt", [128, 512])
        self.win = self.din("win", [len(GROUPS), 128, KC, 256])
        self.wba_d = self.din("wba", [8, 128, 8, 256])
        self.wbh_d = self.din("wbh", [8, 128, 8, 256])
        self.wo_d = self.din("wo", [KC, 128, KC, 128])
        self.gidx = {nm: i for i, (nm, _, _) in enumerate(GROUPS)}
        self.h1s = h1s = self.dscratch("h1s", [D, NT])
        self.h2s = self.dscratch("h2s", [D, NREAL])
        self.xs = [self.dscratch("xs%d" % q, [128, 3 * 1024], BF16) for q in range(3)]
        self.xg = [self.dscratch("xg%d" % q, [512, 3 * 1024], BF16) for q in range(3)]
        self.xa = self.dscratch("xa", [128, 72])
        self.xag = self.dscratch("xag", [512, 72])
        self.ks = self.dscratch("ks", [128, 2048], BF16)
        self.kg = self.dscratch("kg", [512, 2048], BF16)
        self.vs = self.dscratch("vs", [128, 3088], BF16)
        self.vg = self.dscratch("vg", [512, 3088], BF16)
        self.kms = self.dscratch("kms", [128, 2 * NMETA], BF16)
        self.vms = self.dscratch("vms", [NMETA, 258], BF16)
        if stage == 1:
            dbg = self.dout("dbg", [D, NT])
        elif stage in (3, 4):
            dbg = self.dout("dbg", [128, 8 * NREAL])
            self.dbg2 = self.dout("dbg2", [128, 12000])
        elif stage == 5:
            dbg = self.dout("dbg", [D, NREAL])
        else:
            outT = self.dout("outT", [D, NREAL])

        from contextlib import ExitStack
        with ExitStack() as es:
            self.arena = es.enter_context(nc.sbuf_tensor("arena", [128, ARENA_F32], F32))
            self.cst = es.enter_context(nc.sbuf_tensor("cst", [128, CONST_F32], F32))
            self.ps = es.enter_context(nc.psum_tensor("ps", [128, 8, 512], F32))
            esems = {e: es.enter_context(nc.semaphore("sem_" + e)) for e in ENGS}
            dsems = [es.enter_context(nc.semaphore("dsem%d" % i)) for i in range(S.n_dma_sems + 8)]
            block = es.enter_context(nc.Block())
            cst = self.cst
            self.ones_f32 = cst[:, 0:128]
            cv = self.cv = cst[:, 128:256]
            epsA = cst[:, 256:257]
            self.eps_rms = cst[:, 257:258]
            self.eps_ik = cst[:, 258:259]
            self.eps_cols = {LN_EPS / (ALPHA * ALPHA): epsA}
            self.ident_bf = cst[:, 264:328].bitcast(BF16)
            self.ones_bf = cst[:, 328:392].bitcast(BF16)
            self.mask2 = cst[:, 392:520]
            self.ones128 = cst[:, 520:648]
            self.lbc = cst[:, 648:656]
            self.omlc = cst[:, 656:664]
            self.gnc = cv[:, 112:120]
            self.selc = cv[:, 120:124]
            S.op("dve", lambda e: e.memset(self.ones_f32, 1.0 / D), writes=["ones"])
            S.op("dve", lambda e: e.memset(self.ones128, 1.0 / 128), writes=["ones128"])
            S.op("dve", lambda e: e.memset(epsA, LN_EPS / (ALPHA * ALPHA)), writes=["eps"])
            S.op("dve", lambda e: e.memset(self.eps_rms, RMS_EPS), writes=["eps"])
            S.op("dve", lambda e: e.memset(self.eps_ik, LN_EPS), writes=["eps"])
            S.dma("sp", lambda e: e.dma_start(out=cv, in_=cvec), writes=["cv"])
            S.dma("sp", lambda e: e.dma_start(out=self.mask2, in_=cmat[:, 256:384]), writes=["mask2"])
            S.dma("pool", lambda e: e.dma_start(out=self.ident_bf, in_=cmat[:, 0:128]), writes=["ident"])
            S.dma("pool", lambda e: e.dma_start(out=self.ones_bf, in_=cmat[:, 128:256]), writes=["ones_bf"])
            S.op("dve", lambda e: e.tensor_tensor(out=self.lbc, in0=cv[:, 96:104], in1=cv[:, 104:112], op=ALU.subtract),
                 reads=["cv"], writes=["lb"])
            S.op("act", lambda e: e.activation(out=self.lbc, in_=self.lbc, func=AF.Sigmoid), reads=["lb"], writes=["lb"])
            S.op("dve", lambda e: e.tensor_scalar(out=self.omlc, in0=self.lbc, scalar1=-1.0, scalar2=1.0,
                                                  op0=ALU.mult, op1=ALU.add), reads=["lb"], writes=["lb"])
            strm0 = self.view(0, BF16, [KC, NT])
            S.dma("pool", lambda e: e.dma_start(out=strm0, in_=xT.rearrange("(k p) t -> p k t", p=128)),
                  writes=[("f1", "sin")])
            self.ffn_phase("f1", NT, TBS, strm0, 66560, wg1, wu1, wd1, xT, cv[:, 0:16], cv[:, 16:32],
                           resid_out=(dbg if stage == 1 else h1s))
            strm1 = self.view(66560, BF16, [KC, NT])
            if stage >= 2:
                if stage >= 4:
                    self.attn_m3(strm1, 0)
                self.hgrn_m1(strm1)
                if stage >= 4:
                    self.attn_m3(strm1, 1)
                self.hgrn_m2(strm1)
            if stage == 3:
                yhg = self.view(32768, BF16, [8 * NREAL])
                S.dma("pool", lambda e: e.dma_start(out=dbg, in_=yhg), reads=[])
            if stage >= 4:
                self.attn_m3(strm1, 2)
            if stage == 4:
                yat = self.view(0, BF16, [8 * NREAL])
                S.dma("pool", lambda e: e.dma_start(out=dbg, in_=yat), reads=[])
            if stage >= 5:
                if stage == 5:
                    self.h2s = dbg
                self.merge_m4(strm1, cv[:, 32:48], cv[:, 48:64])
            if stage >= 6:
                strm2 = self.view(0, BF16, [KC, NREAL])
                self.ffn_phase("f2", NREAL, TBS[0:2], strm2, 66560, wg2, wu2, wd2, self.h2s, cv[:, 64:80],
                               cv[:, 80:96], final_out=outT)
            S.emit(block, esems, dsems)
        return nc


def _lay_gu(w):
    return np.ascontiguousarray(w.reshape(KC, 128, FC // 2, 256).transpose(2, 1, 0, 3))


def _lay_d(w):
    return np.ascontiguousarray(w.reshape(FC, 128, KC, 128).transpose(2, 1, 0, 3))


def _fm(v):
    return np.ascontiguousarray(v.reshape(KC, 128).T)


def _core_tokens(x, meta, c):
    b, j = c // 4, c % 4
    blocks = [x[b, 128 * (4 * i + j):128 * (4 * i + j) + 128] for i in range(8)]
    tok = np.concatenate(blocks + [meta], axis=0)
    return np.ascontiguousarray(tok.T)


def _mk_groups():
    g = []
    for hp in range(4):
        g += [("hf%d" % hp, 3664 + 256 * hp, 256), ("hq%d" % hp, 2640 + 256 * hp, 256),
              ("hi%d" % hp, 4688 + 256 * hp, 256)]
    for i in range(4):
        g.append(("hg%d" % i, 5712 + 256 * i, 256))
    g += [("ak", 1024, 256), ("av", 1280, 256), ("ikw", 2560, 80)]
    for i in range(4):
        g.append(("aq%d" % i, 256 * i, 256))
    for i in range(4):
        g.append(("iq%d" % i, 1536 + 256 * i, 256))
    for i in range(8):
        g.append(("ga%d" % i, 6736 + 256 * i, 256))
        g.append(("gh%d" % i, 8784 + 256 * i, 256))
    return g


GROUPS = _mk_groups()


def _lay_win(w):
    out = np.zeros((len(GROUPS), 128, KC, 256), np.float32)
    for gi, (nm, c0, nc_) in enumerate(GROUPS):
        out[gi, :, :, 0:nc_] = w[:, c0:c0 + nc_].reshape(KC, 128, nc_).transpose(1, 0, 2)
    return out


def prepare(inputs, stage):
    f = lambda k: np.asarray(inputs[k], dtype=np.float32)
    x, meta = f("x"), f("meta")
    shared = {
        "wg1": _lay_gu(f("ffn1_w_gate")[0]), "wu1": _lay_gu(f("ffn1_w_up")[0]),
        "wd1": _lay_d(f("ffn1_w_down")[0]),
        "win": _lay_win(f("w_in")[0]),
        "wg2": _lay_gu(f("ffn2_w_gate")[0]), "wu2": _lay_gu(f("ffn2_w_up")[0]),
        "wd2": _lay_d(f("ffn2_w_down")[0]),
        "wba": np.ascontiguousarray(f("w_branch_att")[0].reshape(8, 128, 8, 256).transpose(2, 1, 0, 3)),
        "wbh": np.ascontiguousarray(f("w_branch_hg")[0].reshape(8, 128, 8, 256).transpose(2, 1, 0, 3)),
        "wo": np.ascontiguousarray(f("w_out")[0].reshape(KC, 128, KC, 128).transpose(2, 1, 0, 3)),
    }
    slopes = (2.0 ** -(np.arange(8) + 1.0)).astype(np.float32)
    c = np.arange(4096)
    kpos = np.concatenate([16 + 128 * (4 * ((c % 1024) // 128) + c // 1024) + c % 128, np.arange(16)]).astype(np.float32)
    augk = np.stack([np.floor(kpos / 64.0), kpos % 64.0, np.ones_like(kpos)], 0).astype(np.float32)
    augs = np.stack([np.repeat(64.0 * slopes, 128), np.repeat(slopes, 128)], 0).astype(np.float32)
    shared["augk"] = augk
    shared["augs"] = augs
    cvec = np.zeros((128, 128), np.float32)
    cvec[:, 0:16] = _fm(f("ln1_g")[0]); cvec[:, 16:32] = _fm(f("ln1_b")[0])
    cvec[:, 32:48] = _fm(f("ln2_g")[0]); cvec[:, 48:64] = _fm(f("ln2_b")[0])
    cvec[:, 64:80] = _fm(f("ln3_g")[0]); cvec[:, 80:96] = _fm(f("ln3_b")[0])
    lbl = f("hg_lb_logits")
    cvec[:, 96:104] = lbl[0].reshape(8, 128).T
    cvec[:, 104:112] = lbl[1].reshape(8, 128).T
    cvec[:, 112:120] = f("hg_norm_g")[0].T
    cmat = np.zeros((128, 512), np.float32)
    cmat[:, 384:512] = np.arange(128, dtype=np.float32)[None, :]
    cmat[:, 0:128] = np.eye(128, dtype=np.float32)
    cmat[:, 128:256] = 1.0
    sidx = np.arange(128)[:, None]; tidx = np.arange(128)[None, :]
    cmat[:, 256:384] = (((sidx <= tidx) & ((sidx // 64) == (tidx // 64))) | ((sidx < 64) & (tidx >= 64))).astype(np.float32)
    shared["cmat"] = cmat
    maps = []
    for c in range(8):
        m = dict(shared)
        m["xT"] = _core_tokens(x, meta, c)
        cv = cvec.copy()
        cv[:, 120 + (c % 4)] = 1.0
        m["cvec"] = cv
        j = c % 4
        p = np.arange(128, dtype=np.float32)
        qpos = np.stack([16 + 128 * (4 * i + j) + p for i in range(8)], 0)
        m["augq"] = np.ascontiguousarray((-slopes[None, :, None] * qpos[:, None, :]).reshape(8, 1024).astype(np.float32))
        catt = np.zeros((128, 224), np.float32)
        catt[:, 0:8] = -slopes[None, :]
        catt[:, 8:40] = np.array([16 + 128 * r + 512 * jj for r in range(4) for jj in range(8)], np.float32)[None, :]
        catt[:, 64:96] = (2.0 ** -(np.arange(32) + 1.0))[None, :]
        catt[:, 96:160] = f("idx_k_norm_g")[0][None, :]
        catt[:, 160:224] = f("idx_k_norm_b")[0][None, :]
        m["catt"] = catt
        tt = np.arange(128)[:, None, None]; rr = np.arange(4)[None, :, None]; ss = np.arange(128)[None, None, :]
        m["cbt"] = np.where(128 * (rr - j) + (ss - tt) > 0, -1e30, 0.0).astype(np.float32).reshape(128, 512)
        maps.append(m)
    return maps


_NC_CACHE = {}


def kernel(**inputs):
    stage = int(os.environ.get("KSTAGE", "9"))
    if stage not in _NC_CACHE:
        _NC_CACHE[stage] = Builder(stage).build()
    nc = _NC_CACHE[stage]
    maps = prepare(inputs, stage)
    res = run_bass_kernel_spmd(nc, maps, core_ids=list(range(8)))
    if stage == 4:
        return [(r["dbg"], r["dbg2"]) for r in res.results]
    if stage < 6:
        return [r["dbg"] for r in res.results]
    out = np.zeros((2, 4096, D), np.float32)
    for c in range(8):
        b, j = c // 4, c % 4
        o = res.results[c]["outT"]
        for i in range(8):
            g = 4 * i + j
            out[b, 128 * g:128 * g + 128] = o[:, 128 * i:128 * i + 128].T
    return out
```
